# Optimizing a Trainium2 kernel written in Bass

```python
import math
import jax
import jax.numpy as jnp
from jax import lax
import numpy as np

D_MODEL = 1024
BATCH = 4
SEQ = 8192
DEPTH = 2

CHUNK = 64

SSD_HEAD_DIM = 64
SSD_WIDTH = D_MODEL // 2
SSD_HEADS = SSD_WIDTH // SSD_HEAD_DIM
SSD_GROUPS = 2
SSD_HPG = SSD_HEADS // SSD_GROUPS
SSD_STATE = 64
SSD_CONV = 4
SSD_XBC = SSD_WIDTH + 2 * SSD_GROUPS * SSD_STATE

RWKV_HEAD_DIM = 64
RWKV_WIDTH = D_MODEL // 4
RWKV_HEADS = RWKV_WIDTH // RWKV_HEAD_DIM
RWKV_DECAY_RANK = 32
RWKV_ICLR_RANK = 32
RWKV_GATE_RANK = 64
RWKV_SPLITS = [RWKV_WIDTH, RWKV_WIDTH, RWKV_WIDTH, RWKV_DECAY_RANK, RWKV_ICLR_RANK, RWKV_GATE_RANK]
RWKV_IN = sum(RWKV_SPLITS)
RWKV_GN_EPS = 64e-5

GLA_VALUE_DIM = 64
GLA_WIDTH = D_MODEL // 4
GLA_HEADS = GLA_WIDTH // GLA_VALUE_DIM
GLA_KEY_DIM = GLA_VALUE_DIM // 2
GLA_QK_WIDTH = GLA_HEADS * GLA_KEY_DIM
GLA_GATE_RANK = 16
GLA_GATE_TEMP = 16.0

MIX_WIDTH = SSD_WIDTH + RWKV_WIDTH + GLA_WIDTH
IN_SPLITS = [SSD_WIDTH, SSD_XBC, SSD_HEADS, RWKV_IN, GLA_QK_WIDTH, GLA_QK_WIDTH, GLA_WIDTH, GLA_WIDTH, GLA_GATE_RANK]
N_IN = sum(IN_SPLITS)

N_GROUPS = 4
EXPERTS_PER_GROUP = 8
N_EXPERTS = N_GROUPS * EXPERTS_PER_GROUP
TOP_K = 2
EXPERT_FF = 512
ROW_BLOCK = 256

DEEPNORM_ALPHA = (2 * DEPTH) ** 0.25
DEEPNORM_BETA = (8 * DEPTH) ** -0.25
LN_EPS = 1e-5
RMS_EPS = 1e-6

kernel_name = 'hybrid_ssd_rwkv7_gla_hmoe'

F32 = jnp.float32


def _split(t, sizes):
    idx = [int(i) for i in np.cumsum(sizes)[:-1]]
    return jnp.split(t, idx, axis=-1)


def layer_norm(x, g, b):
    xf = x.astype(F32)
    mu = jnp.mean(xf, -1, keepdims=True)
    var = jnp.mean(jnp.square(xf - mu), -1, keepdims=True)
    return ((xf - mu) * lax.rsqrt(var + LN_EPS) * g + b).astype(x.dtype)


def causal_depthwise_conv(x, w, b):
    k, c = w.shape
    y = lax.conv_general_dilated(x, w[:, None, :], window_strides=(1,), padding=[(k - 1, 0)],
                                 dimension_numbers=('NWC', 'WIO', 'NWC'), feature_group_count=c)
    return y + b


def ssd_chunked_scan(x, a_dt, bm, cm):
    bsz, seq = x.shape[0], x.shape[1]
    nc = seq // CHUNK
    x = x.reshape(bsz, nc, CHUNK, *x.shape[2:])
    a_dt = a_dt.reshape(bsz, nc, CHUNK, *a_dt.shape[2:])
    bm = bm.reshape(bsz, nc, CHUNK, *bm.shape[2:])
    cm = cm.reshape(bsz, nc, CHUNK, *cm.shape[2:])
    a_cs = jnp.cumsum(a_dt, axis=2)
    mask = jnp.tril(jnp.ones((CHUNK, CHUNK), bool))[:, :, None, None]
    seg = jnp.exp(jnp.where(mask, a_cs[:, :, :, None] - a_cs[:, :, None, :], -jnp.inf))
    cb = jnp.einsum('bclgn,bcsgn->bclsg', cm, bm)
    y_diag = jnp.einsum('bclsgh,bcsghp->bclghp', cb[..., None] * seg, x)
    to_end = jnp.exp(a_cs[:, :, -1:] - a_cs)
    states = jnp.einsum('bclgn,bclghp->bcghpn', bm, to_end[..., None] * x)
    chunk_decay = jnp.exp(a_cs[:, :, -1])

    def step(s, inp):
        st, dec = inp
        return s * dec[..., None, None] + st, s

    s0 = jnp.zeros_like(states[:, 0])
    _, prev = lax.scan(step, s0, (jnp.moveaxis(states, 1, 0), jnp.moveaxis(chunk_decay, 1, 0)))
    prev = jnp.moveaxis(prev, 0, 1)
    y_off = jnp.einsum('bclgn,bcghpn->bclghp', cm, prev) * jnp.exp(a_cs)[..., None]
    return (y_diag + y_off).reshape(bsz, seq, *x.shape[3:])


def ssd_mixer(z, xbc, dt_raw, conv_w, conv_b, dt_bias, a_log, d_skip, norm_g):
    bsz, seq, _ = xbc.shape
    xbc = jax.nn.silu(causal_depthwise_conv(xbc, conv_w, conv_b))
    xh, bm, cm = _split(xbc, [SSD_WIDTH, SSD_GROUPS * SSD_STATE, SSD_GROUPS * SSD_STATE])
    xh = xh.astype(F32).reshape(bsz, seq, SSD_GROUPS, SSD_HPG, SSD_HEAD_DIM)
    bm = bm.astype(F32).reshape(bsz, seq, SSD_GROUPS, SSD_STATE)
    cm = cm.astype(F32).reshape(bsz, seq, SSD_GROUPS, SSD_STATE)
    dt = jax.nn.softplus((dt_raw + dt_bias).astype(F32)).reshape(bsz, seq, SSD_GROUPS, SSD_HPG)
    a = -jnp.exp(a_log.astype(F32)).reshape(SSD_GROUPS, SSD_HPG)
    y = ssd_chunked_scan(xh * dt[..., None], dt * a, bm, cm)
    y = y + d_skip.astype(F32).reshape(SSD_GROUPS, SSD_HPG)[:, :, None] * xh
    y = y.reshape(bsz, seq, SSD_WIDTH) * jax.nn.silu(z.astype(F32))
    y = y.reshape(bsz, seq, SSD_GROUPS, SSD_WIDTH // SSD_GROUPS)
    y = y * lax.rsqrt(jnp.mean(y * y, -1, keepdims=True) + RMS_EPS)
    return y.reshape(bsz, seq, SSD_WIDTH) * norm_g


def rwkv7_mixer(p, mu, w0, w2, a0, a2, g2, k_k, k_a, r_k, ln_g, ln_b):
    bsz, seq, _ = p.shape
    prev = jnp.pad(p, ((0, 0), (1, 0), (0, 0)))[:, :seq]
    p = p + (prev - p) * mu
    r, k, v, w_lr, a_lr, g_lr = _split(p, RWKV_SPLITS)
    w = -jax.nn.softplus(-(w0 + jnp.tanh(w_lr) @ w2).astype(F32)) - 0.5
    decay = jnp.exp(-jnp.exp(w))
    a = jax.nn.sigmoid((a0 + a_lr @ a2).astype(F32))
    g = (jax.nn.sigmoid(g_lr) @ g2).astype(F32)

    def heads(t):
        return t.astype(F32).reshape(bsz, seq, RWKV_HEADS, RWKV_HEAD_DIM)

    r, k, v, decay, a = heads(r), heads(k), heads(v), heads(decay), heads(a)
    kk = k * k_k.astype(F32).reshape(RWKV_HEADS, RWKV_HEAD_DIM)
    kk = kk * lax.rsqrt(jnp.sum(kk * kk, -1, keepdims=True) + 1e-12)
    k = k * (1.0 + (a - 1.0) * k_a.astype(F32).reshape(RWKV_HEADS, RWKV_HEAD_DIM))

    def step(state, inp):
        r_t, w_t, k_t, v_t, kk_t, a_t = inp
        sa = jnp.einsum('bhvk,bhk->bhv', state, -kk_t)
        state = (state * w_t[:, :, None, :] + sa[..., None] * (kk_t * a_t)[:, :, None, :]
                 + v_t[..., None] * k_t[:, :, None, :])
        return state, jnp.einsum('bhvk,bhk->bhv', state, r_t)

    s0 = jnp.zeros((bsz, RWKV_HEADS, RWKV_HEAD_DIM, RWKV_HEAD_DIM), F32)
    tm = lambda t: jnp.moveaxis(t, 1, 0)
    _, y = lax.scan(step, s0, (tm(r), tm(decay), tm(k), tm(v), tm(kk), tm(a)))
    y = jnp.moveaxis(y, 0, 1)
    mean = jnp.mean(y, -1, keepdims=True)
    var = jnp.mean(jnp.square(y - mean), -1, keepdims=True)
    y = ((y - mean) * lax.rsqrt(var + RWKV_GN_EPS)).reshape(bsz, seq, RWKV_WIDTH) * ln_g + ln_b
    bonus = jnp.sum(r * k * r_k.astype(F32), -1, keepdims=True) * v
    return (y + bonus.reshape(bsz, seq, RWKV_WIDTH)) * g


def gla_mixer(q, k, v, g, a_lr, w_a2, b_a, norm_g):
    bsz, seq, _ = q.shape
    nc = seq // CHUNK
    log_a = jax.nn.log_sigmoid((a_lr @ w_a2 + b_a).astype(F32)) / GLA_GATE_TEMP

    def chunks(t, d):
        return jnp.moveaxis(t.astype(F32).reshape(bsz, nc, CHUNK, GLA_HEADS, d), 1, 0)

    qc = chunks(q, GLA_KEY_DIM) * (GLA_KEY_DIM ** -0.5)
    kc = chunks(k, GLA_KEY_DIM)
    vc = chunks(v, GLA_VALUE_DIM)
    lac = chunks(log_a, GLA_KEY_DIM)
    mask = jnp.tril(jnp.ones((CHUNK, CHUNK), bool))[None, :, :, None, None]

    def step(state, inp):
        q_c, k_c, v_c, la_c = inp
        cum = jnp.cumsum(la_c, axis=1)
        o_inter = jnp.einsum('bihd,bhde->bihe', q_c * jnp.exp(cum), state)
        rel = jnp.exp(jnp.where(mask, cum[:, :, None] - cum[:, None, :], -jnp.inf))
        scores = jnp.einsum('bijhd,bjhd->bhij', q_c[:, :, None] * rel, k_c)
        o_intra = jnp.einsum('bhij,bjhe->bihe', scores, v_c)
        last = cum[:, -1]
        k_dec = k_c * jnp.exp(last[:, None] - cum)
        state = state * jnp.exp(last)[..., None] + jnp.einsum('bjhd,bjhe->bhde', k_dec, v_c)
        return state, o_inter + o_intra

    s0 = jnp.zeros((bsz, GLA_HEADS, GLA_KEY_DIM, GLA_VALUE_DIM), F32)
    _, o = lax.scan(step, s0, (qc, kc, vc, lac))
    o = jnp.moveaxis(o, 0, 1).reshape(bsz, seq, GLA_HEADS, GLA_VALUE_DIM)
    o = o * lax.rsqrt(jnp.mean(o * o, -1, keepdims=True) + RMS_EPS)
    return o.reshape(bsz, seq, GLA_WIDTH) * norm_g * jax.nn.silu(g.astype(F32))


def hybrid_mixer(x, w_in, ssd_conv_w, ssd_conv_b, ssd_dt_bias, ssd_a_log, ssd_d, ssd_norm_g,
                 rwkv_mu, rwkv_w0, rwkv_w2, rwkv_a0, rwkv_a2, rwkv_g2, rwkv_k_k, rwkv_k_a, rwkv_r_k,
                 rwkv_ln_g, rwkv_ln_b, gla_w_a2, gla_b_a, gla_norm_g, w_out):
    p = x @ w_in
    s_z, s_xbc, s_dt, rw, gq, gk, gv, gg, ga = _split(p, IN_SPLITS)
    y_ssd = ssd_mixer(s_z, s_xbc, s_dt, ssd_conv_w, ssd_conv_b, ssd_dt_bias, ssd_a_log, ssd_d, ssd_norm_g)
    y_rwkv = rwkv7_mixer(rw, rwkv_mu, rwkv_w0, rwkv_w2, rwkv_a0, rwkv_a2, rwkv_g2, rwkv_k_k, rwkv_k_a,
                         rwkv_r_k, rwkv_ln_g, rwkv_ln_b)
    y_gla = gla_mixer(gq, gk, gv, gg, ga, gla_w_a2, gla_b_a, gla_norm_g)
    y = jnp.concatenate([y_ssd, y_rwkv, y_gla], axis=-1).astype(x.dtype)
    return y @ w_out


def grouped_expert_mlp(xt, expert_id, gate, w_gate, w_up, w_down):
    n_tok, d = xt.shape
    n_assign = expert_id.size
    e_flat = expert_id.reshape(-1)
    order = jnp.argsort(e_flat)
    e_sorted = e_flat[order]
    counts = jnp.bincount(e_flat, length=N_EXPERTS)
    padded = (counts + ROW_BLOCK - 1) // ROW_BLOCK * ROW_BLOCK
    pad_end = jnp.cumsum(padded)
    pad_start = pad_end - padded
    start = jnp.cumsum(counts) - counts
    dest = pad_start[e_sorted] + jnp.arange(n_assign, dtype=jnp.int32) - start[e_sorted]
    n_blocks = -(-n_assign // ROW_BLOCK) + N_EXPERTS
    n_rows = n_blocks * ROW_BLOCK
    row_tok = jnp.full((n_rows,), n_tok, jnp.int32).at[dest].set((order // TOP_K).astype(jnp.int32))
    row_gate = jnp.zeros((n_rows,), xt.dtype).at[dest].set(gate.reshape(-1)[order].astype(xt.dtype))
    block_start = jnp.arange(n_blocks, dtype=jnp.int32) * ROW_BLOCK
    block_expert = jnp.minimum(jnp.searchsorted(pad_end, block_start, side='right'), N_EXPERTS - 1)
    x_pad = jnp.concatenate([xt, jnp.zeros((1, d), xt.dtype)], axis=0)
    xs = x_pad[row_tok].reshape(n_blocks, ROW_BLOCK, d)

    def block_mlp(args):
        xb, e = args
        h = jax.nn.silu(xb @ w_gate[e]) * (xb @ w_up[e])
        return h @ w_down[e]

    ys = lax.map(block_mlp, (xs, block_expert)).reshape(n_rows, d)
    out = jnp.zeros((n_tok + 1, d), ys.dtype).at[row_tok].add(ys * row_gate[:, None])
    return out[:n_tok]


def hierarchical_moe(x, w_rg, b_rg, w_re, b_re, w_gate, w_up, w_down):
    bsz, seq, d = x.shape
    xt = x.reshape(-1, d)
    n_tok = xt.shape[0]
    g_prob = jax.nn.softmax((xt @ w_rg + b_rg).astype(F32), axis=-1)
    g_w, g_idx = lax.top_k(g_prob, 1)
    e_logits = (xt @ w_re + b_re).astype(F32).reshape(n_tok, N_GROUPS, EXPERTS_PER_GROUP)
    e_logits = jnp.take_along_axis(e_logits, g_idx[:, :, None], axis=1)[:, 0]
    e_prob = jax.nn.softmax(e_logits, axis=-1)
    e_w, e_local = lax.top_k(e_prob, TOP_K)
    e_w = e_w / jnp.sum(e_w, -1, keepdims=True)
    gate = g_w * e_w
    expert_id = g_idx * EXPERTS_PER_GROUP + e_local
    y = grouped_expert_mlp(xt, expert_id, gate, w_gate, w_up, w_down)
    return y.reshape(bsz, seq, d).astype(x.dtype)


def setup_inputs(seed: int = 0) -> dict:
    key = jax.random.key(seed)
    ks = iter(jax.random.split(key, 48))
    L = DEPTH

    def nrm(shape, scale):
        return scale * jax.random.normal(next(ks), shape, F32)

    def gain(shape):
        return 1.0 + nrm(shape, 0.02)

    x = nrm((BATCH, SEQ, D_MODEL), 1.0)
    ln_in_g = gain((D_MODEL,))
    ln_in_b = nrm((D_MODEL,), 0.02)
    w_in = nrm((L, D_MODEL, N_IN), D_MODEL ** -0.5)
    ssd_conv_w = nrm((L, SSD_CONV, SSD_XBC), SSD_CONV ** -0.5)
    ssd_conv_b = nrm((L, SSD_XBC), 0.02)
    dt = jnp.exp(jax.random.uniform(next(ks), (L, SSD_HEADS), F32, math.log(1e-3), math.log(1e-1)))
    ssd_dt_bias = dt + jnp.log(-jnp.expm1(-dt))
    ssd_a_log = jnp.log(jax.random.uniform(next(ks), (L, SSD_HEADS), F32, 1.0, 16.0))
    ssd_d = 1.0 + nrm((L, SSD_HEADS), 0.1)
    ssd_norm_g = gain((L, SSD_WIDTH))
    rwkv_mu = jax.random.uniform(next(ks), (L, RWKV_IN), F32)
    rwkv_w0 = jnp.linspace(-6.5, -1.5, RWKV_WIDTH, dtype=F32)[None, :] + nrm((L, RWKV_WIDTH), 0.1)
    rwkv_w2 = nrm((L, RWKV_DECAY_RANK, RWKV_WIDTH), 0.1)
    rwkv_a0 = nrm((L, RWKV_WIDTH), 0.1)
    rwkv_a2 = nrm((L, RWKV_ICLR_RANK, RWKV_WIDTH), 0.1)
    rwkv_g2 = nrm((L, RWKV_GATE_RANK, RWKV_WIDTH), RWKV_GATE_RANK ** -0.5)
    rwkv_k_k = 1.0 + nrm((L, RWKV_WIDTH), 0.1)
    rwkv_k_a = 1.0 + nrm((L, RWKV_WIDTH), 0.1)
    rwkv_r_k = nrm((L, RWKV_HEADS, RWKV_HEAD_DIM), 0.1)
    rwkv_ln_g = gain((L, RWKV_WIDTH))
    rwkv_ln_b = nrm((L, RWKV_WIDTH), 0.02)
    gla_w_a2 = nrm((L, GLA_GATE_RANK, GLA_QK_WIDTH), GLA_GATE_RANK ** -0.5)
    gla_b_a = nrm((L, GLA_QK_WIDTH), 0.1)
    gla_norm_g = gain((L, GLA_WIDTH))
    w_out = nrm((L, MIX_WIDTH, D_MODEL), DEEPNORM_BETA * MIX_WIDTH ** -0.5)
    ln1_g = gain((L, D_MODEL))
    ln1_b = nrm((L, D_MODEL), 0.02)
    moe_w_rg = nrm((L, D_MODEL, N_GROUPS), D_MODEL ** -0.5)
    moe_b_rg = nrm((L, N_GROUPS), 0.01)
    moe_w_re = nrm((L, D_MODEL, N_EXPERTS), D_MODEL ** -0.5)
    moe_b_re = nrm((L, N_EXPERTS), 0.01)
    moe_w_gate = nrm((L, N_EXPERTS, D_MODEL, EXPERT_FF), D_MODEL ** -0.5)
    moe_w_up = nrm((L, N_EXPERTS, D_MODEL, EXPERT_FF), D_MODEL ** -0.5)
    moe_w_down = nrm((L, N_EXPERTS, EXPERT_FF, D_MODEL), DEEPNORM_BETA * EXPERT_FF ** -0.5)
    ln2_g = gain((L, D_MODEL))
    ln2_b = nrm((L, D_MODEL), 0.02)
    return {'x': x, 'ln_in_g': ln_in_g, 'ln_in_b': ln_in_b, 'w_in': w_in,
            'ssd_conv_w': ssd_conv_w, 'ssd_conv_b': ssd_conv_b, 'ssd_dt_bias': ssd_dt_bias,
            'ssd_a_log': ssd_a_log, 'ssd_d': ssd_d, 'ssd_norm_g': ssd_norm_g,
            'rwkv_mu': rwkv_mu, 'rwkv_w0': rwkv_w0, 'rwkv_w2': rwkv_w2, 'rwkv_a0': rwkv_a0,
            'rwkv_a2': rwkv_a2, 'rwkv_g2': rwkv_g2, 'rwkv_k_k': rwkv_k_k, 'rwkv_k_a': rwkv_k_a,
            'rwkv_r_k': rwkv_r_k, 'rwkv_ln_g': rwkv_ln_g, 'rwkv_ln_b': rwkv_ln_b,
            'gla_w_a2': gla_w_a2, 'gla_b_a': gla_b_a, 'gla_norm_g': gla_norm_g,
            'w_out': w_out, 'ln1_g': ln1_g, 'ln1_b': ln1_b,
            'moe_w_rg': moe_w_rg, 'moe_b_rg': moe_b_rg, 'moe_w_re': moe_w_re, 'moe_b_re': moe_b_re,
            'moe_w_gate': moe_w_gate, 'moe_w_up': moe_w_up, 'moe_w_down': moe_w_down,
            'ln2_g': ln2_g, 'ln2_b': ln2_b}


def reference(x, ln_in_g, ln_in_b, w_in, ssd_conv_w, ssd_conv_b, ssd_dt_bias, ssd_a_log, ssd_d,
              ssd_norm_g, rwkv_mu, rwkv_w0, rwkv_w2, rwkv_a0, rwkv_a2, rwkv_g2, rwkv_k_k, rwkv_k_a,
              rwkv_r_k, rwkv_ln_g, rwkv_ln_b, gla_w_a2, gla_b_a, gla_norm_g, w_out, ln1_g, ln1_b,
              moe_w_rg, moe_b_rg, moe_w_re, moe_b_re, moe_w_gate, moe_w_up, moe_w_down, ln2_g, ln2_b):
    h = layer_norm(x, ln_in_g, ln_in_b)
    for i in range(DEPTH):
        mix = hybrid_mixer(h, w_in[i], ssd_conv_w[i], ssd_conv_b[i], ssd_dt_bias[i], ssd_a_log[i],
                           ssd_d[i], ssd_norm_g[i], rwkv_mu[i], rwkv_w0[i], rwkv_w2[i], rwkv_a0[i],
                           rwkv_a2[i], rwkv_g2[i], rwkv_k_k[i], rwkv_k_a[i], rwkv_r_k[i], rwkv_ln_g[i],
                           rwkv_ln_b[i], gla_w_a2[i], gla_b_a[i], gla_norm_g[i], w_out[i])
        h = layer_norm(DEEPNORM_ALPHA * h + mix, ln1_g[i], ln1_b[i])
        ffn = hierarchical_moe(h, moe_w_rg[i], moe_b_rg[i], moe_w_re[i], moe_b_re[i],
                               moe_w_gate[i], moe_w_up[i], moe_w_down[i])
        h = layer_norm(DEEPNORM_ALPHA * h + ffn, ln2_g[i], ln2_b[i])
    return h
```

```python
import numpy as np
import concourse.bass as bass
import concourse.mybir as mybir
from concourse.bass_utils import run_bass_kernel_spmd

F32 = mybir.dt.float32
BF16 = mybir.dt.bfloat16
I32 = mybir.dt.int32
U32 = mybir.dt.uint32
AF = mybir.ActivationFunctionType
ALU = mybir.AluOpType
AX = mybir.AxisListType

D = 1024
NIN = 2968
DEPTH = 2
ALPHA = (2 * DEPTH) ** 0.25
LN_EPS = 1e-5
RMS_EPS = 1e-6
GN_EPS = 64e-5
NE = 32
FF = 512
O_Z, O_XBC, O_DT, O_RW, O_GQ, O_GK, O_GV, O_GG, O_GA = 0, 512, 1280, 1288, 2184, 2312, 2440, 2696, 2952


class Prog:
    EPOCH = 8192
    NDMA = 40

    def __init__(self, nc):
        self.nc = nc
        self.eng = {'pe': nc.tensor, 'dve': nc.vector, 'act': nc.scalar, 'pool': nc.gpsimd, 'sp': nc.sync}
        self.esems = {n: [] for n in ('pe', 'dve', 'act', 'pool')}
        self.cnt = {n: 0 for n in ('pe', 'dve', 'act', 'pool')}
        self.dsems, self.dval, self.dkey = [], [], {}
        self.waited = {n: {} for n in self.eng}
        self.lastw, self.readers = {}, {}
        self.ninst = 0

    def _esem(self, X, ep):
        while len(self.esems[X]) <= ep:
            self.esems[X].append(self.nc.alloc_semaphore("s_%s_%d" % (X, len(self.esems[X]))))
        return self.esems[X][ep]

    def _deps(self, reads, writes):
        deps = {}

        def add(ev):
            if ev is not None and deps.get(ev[0], 0) < ev[1]:
                deps[ev[0]] = ev[1]
        for r in reads:
            add(self.lastw.get(r))
        for w in writes:
            add(self.lastw.get(w))
            for k, v in self.readers.get(w, {}).items():
                add((k, v))
        return deps

    def _wait(self, X, deps):
        e = self.eng[X]
        for k, v in deps.items():
            if k == X and X == 'pe':
                continue
            if self.waited[X].get(k, 0) >= v:
                continue
            if isinstance(k, str):
                ep = (v - 1) // self.EPOCH
                e.wait_ge(self._esem(k, ep), v - ep * self.EPOCH)
            else:
                v = self.dval[k]
                e.wait_ge(self.dsems[k], v)
            self.waited[X][k] = v
            self.ninst += 1

    def _record(self, ev, reads, writes):
        for r in reads:
            d = self.readers.setdefault(r, {})
            if d.get(ev[0], 0) < ev[1]:
                d[ev[0]] = ev[1]
        for w in writes:
            self.lastw[w] = ev
            self.readers[w] = {}

    def op(self, X, fn, r=(), w=()):
        r = [k for k in r if k is not None]
        w = [k for k in w if k is not None]
        w = w + [k for k in r if isinstance(k, str) and k.startswith('psb') and k not in w]
        self._wait(X, self._deps(r, w))
        inst = fn(self.eng[X])
        self.cnt[X] += 1
        n = self.cnt[X]
        inst.then_inc(self._esem(X, (n - 1) // self.EPOCH), 1)
        self.ninst += 1
        self._record((X, n), r, w)

    def dma(self, X, fn, semkey, r=(), w=()):
        if semkey not in self.dkey:
            i = len(self.dkey) % self.NDMA
            if i >= len(self.dsems):
                self.dsems.append(self.nc.alloc_semaphore("d_%d" % i))
                self.dval.append(0)
            self.dkey[semkey] = i
        i = self.dkey[semkey]
        self._wait(X, self._deps(r, w))
        inst = fn(self.eng[X])
        self.dval[i] += 16
        inst.then_inc(self.dsems[i], 16)
        self.ninst += 1
        self._record((i, self.dval[i]), r, w)

    def barrier(self):
        deps = {k: v for k, v in self.cnt.items() if v > 0}
        for i, v in enumerate(self.dval):
            if v > 0:
                deps[i] = v
        for X in self.eng:
            self._wait(X, dict(deps))


class Tile:
    def __init__(self, t, key):
        self.t, self.key = t, key

    def __getitem__(self, k):
        return self.t[k]


class Builder:
    def __init__(self, T, depth=DEPTH, debug=False, rb=None):
        import os
        rb = rb or int(os.environ.get('RB', '512'))
        self.T, self.depth, self.debug = T, depth, debug
        self.NT = T // 128
        self.RB = rb
        self.NB = (2 * T) // rb + NE
        nc = self.nc = bass.Bass("TRN2", target_bir_lowering=False)
        self.P = Prog(nc)
        self.nsb = 0
        self.sb_off = 16640
        self.sb_peak = 0
        self.sb_cap = 229376
        self.bank_i = 0
        self.chain_i = {}
        self.banks = [nc.alloc_psum_tensor("psb%d" % i, [128, 512], F32) for i in range(8)]
        self.dbg = {}

    def sb(self, shape, dt=F32, name=None):
        self.nsb += 1
        name = "%s_%d" % (name or "t", self.nsb)
        esz = 2 if dt == BF16 else 4
        n = 1
        for v in shape[1:]:
            n *= v
        nbytes = (n * esz + 31) // 32 * 32
        off = self.sb_off
        self.sb_off += nbytes
        assert self.sb_off <= self.sb_cap, "SBUF overflow %d" % self.sb_off
        self.sb_peak = max(self.sb_peak, self.sb_off)
        return Tile(self.nc.alloc_sbuf_tensor_at(name, list(shape), dt, offset=off), name)

    CHAIN_BANKS = {'s': [0, 1, 2], 'r': [3, 4, 5], 'g': [6, 7]}

    def bank(self, chain=None):
        if chain is None:
            i = self.bank_i
            self.bank_i = (i + 1) % 8
        else:
            lst = self.CHAIN_BANKS[chain]
            j = self.chain_i.get(chain, 0)
            self.chain_i[chain] = (j + 1) % len(lst)
            i = lst[j]
        return self.banks[i], "psb%d" % i

    def V(self, fn, r=(), w=()):
        self.P.op('dve', fn, r, w)

    def A(self, fn, r=(), w=()):
        self.P.op('act', fn, r, w)

    def G(self, fn, r=(), w=()):
        self.P.op('pool', fn, r, w)

    def M(self, fn, r=(), w=()):
        self.P.op('pe', fn, r, w)

    def din(self, name, shape, dt=F32):
        return self.nc.dram_tensor(name, list(shape), dt, kind="ExternalInput")

    def dscr(self, name, shape, dt=F32):
        return self.nc.dram_tensor(name, list(shape), dt, kind="Internal")

    def dout(self, name, shape, dt=F32):
        return self.nc.dram_tensor(name, list(shape), dt, kind="ExternalOutput")

    def load(self, q, out_ap, in_ap, key, dkeys=(), slow=False):
        if slow:
            self.P.dma(q, lambda e: e.dma_start(out=out_ap, in_=in_ap, allow_slow_non_contiguous=True), key, r=list(dkeys), w=[key])
        else:
            self.P.dma(q, lambda e: e.dma_start(out=out_ap, in_=in_ap), key, r=list(dkeys), w=[key])

    def store(self, q, out_ap, in_ap, key, dkeys=()):
        self.P.dma(q, lambda e: e.dma_start(out=out_ap, in_=in_ap), key, r=[key], w=list(dkeys))

    def consts(self):
        nc = self.nc
        c = self.c = {}
        self._ln_st = self.sb([128, 2, 6], name="ln_st")
        self._ln_mv = self.sb([128, 2], name="ln_mv")
        self._ln_rs = self.sb([128, 1], name="ln_rs")
        onesf = c['onesf'] = self.sb([128, 128], name="onesf")
        self.G(lambda e: e.memset(onesf[:], 1.0), w=[onesf.key])
        identf = c['identf'] = self.sb([128, 128], name="identf")
        self.G(lambda e: e.memset(identf[:], 0.0), w=[identf.key])
        self.G(lambda e: e.affine_select(out=identf[:], in_=identf[:], pattern=[[-1, 128]], base=0, channel_multiplier=1,
                                         compare_op=ALU.not_equal, fill=1.0), r=[identf.key], w=[identf.key])
        identb = c['identb'] = self.sb([128, 128], BF16, name="identb")
        self.V(lambda e: e.tensor_copy(identb[:], identf[:]), r=[identf.key], w=[identb.key])
        tri = c['tri'] = self.sb([128, 128], name="tri")
        self.G(lambda e: e.affine_select(out=tri[:], in_=onesf[:], pattern=[[1, 128]], base=0, channel_multiplier=-1,
                                         compare_op=ALU.is_ge, fill=0.0), r=[onesf.key], w=[tri.key])
        su = c['su'] = self.sb([128, 128], name="su")
        self.G(lambda e: e.affine_select(out=su[:], in_=onesf[:], pattern=[[-1, 128]], base=0, channel_multiplier=1,
                                         compare_op=ALU.is_gt, fill=0.0), r=[onesf.key], w=[su.key])
        sl = c['sl'] = self.sb([128, 128], name="sl")
        self.G(lambda e: e.affine_select(out=sl[:], in_=onesf[:], pattern=[[1, 128]], base=0, channel_multiplier=-1,
                                         compare_op=ALU.is_gt, fill=0.0), r=[onesf.key], w=[sl.key])
        maskb = c['maskb'] = self.sb([128, 128], name="maskb")
        self.G(lambda e: e.tensor_copy(maskb[:], tri[:]), r=[tri.key], w=[maskb.key])
        self.G(lambda e: e.memset(maskb[0:64, 64:128], 0.0), w=[maskb.key])
        rmask = c['rmask'] = self.sb([128, 256], name="rmask")
        self.G(lambda e: e.memset(rmask[:], 1.0), w=[rmask.key])
        self.G(lambda e: e.memset(rmask[:].rearrange("p (a b) -> p a b", b=64)[:, :, 0:1], 0.0), w=[rmask.key])
        hm = c['hm'] = self.sb([128, 2], name="hm")
        self.G(lambda e: e.memset(hm[:], 0.0), w=[hm.key])
        self.G(lambda e: e.memset(hm[0:64, 0:1], 1.0), w=[hm.key])
        self.G(lambda e: e.memset(hm[64:128, 1:2], 1.0), w=[hm.key])
        nhm = c['nhm'] = self.sb([128, 2], name="nhm")
        self.V(lambda e: e.tensor_scalar(nhm[:], hm[:], -1.0, None, ALU.mult), r=[hm.key], w=[nhm.key])
        qm = c['qm'] = self.sb([64, 2], name="qm")
        self.G(lambda e: e.memset(qm[:], 0.0), w=[qm.key])
        self.G(lambda e: e.memset(qm[0:32, 0:1], 32.0 ** -0.5), w=[qm.key])
        self.G(lambda e: e.memset(qm[32:64, 1:2], 32.0 ** -0.5), w=[qm.key])
        bones = c['bones'] = self.sb([128, 128], name="bones")
        self.G(lambda e: e.memset(bones[:], 0.0), w=[bones.key])
        self.G(lambda e: e.memset(bones[0:64, 0:64], 1.0), w=[bones.key])
        self.G(lambda e: e.memset(bones[64:128, 64:128], 1.0), w=[bones.key])

    def layernorm(self, xin, gk, bk, out, tmp):
        st, mv, rs = self._ln_st, self._ln_mv, self._ln_rs
        for i in range(2):
            self.V(lambda e, i=i: e.bn_stats(st[:, i, :], xin[:, i * 512:(i + 1) * 512]), r=[xin.key], w=[st.key])
        self.V(lambda e: e.bn_aggr(mv[:], st[:].rearrange("p a b -> p (a b)")), r=[st.key], w=[mv.key])
        self.V(lambda e: e.tensor_scalar(rs[:], mv[:, 1:2], LN_EPS, None, ALU.add), r=[mv.key], w=[rs.key])
        self.A(lambda e: e.activation(out=rs[:], in_=rs[:], func=AF.Sqrt), r=[rs.key], w=[rs.key])
        self.V(lambda e: e.reciprocal(rs[:], rs[:]), r=[rs.key], w=[rs.key])
        self.V(lambda e: e.tensor_scalar(tmp[:], xin[:], mv[:, 0:1], rs[:, 0:1], ALU.subtract, ALU.mult), r=[xin.key, mv.key, rs.key], w=[tmp.key])
        self.G(lambda e: e.tensor_tensor(tmp[:], tmp[:], gk[:], ALU.mult), r=[tmp.key, gk.key], w=[tmp.key])
        self.V(lambda e: e.tensor_tensor(out[:], tmp[:], bk[:], ALU.add), r=[tmp.key, bk.key], w=[out.key])

    def to_fm(self, h_tm, hb, hT):
        c = self.c
        self.A(lambda e: e.copy(out=hb[:], in_=h_tm[:]), r=[h_tm.key], w=[hb.key])
        bk, bkey = self.bank()
        pb = bk[:].bitcast(BF16)
        for kc in range(8):
            self.M(lambda e, kc=kc: e.transpose(pb[:, kc * 128:(kc + 1) * 128], hb[:, kc * 128:(kc + 1) * 128], c['identb'][:]),
                   r=[hb.key, c['identb'].key], w=[bkey])
        self.V(lambda e: e.tensor_copy(hT[:].rearrange("p a b -> p (a b)"), pb), r=[bkey], w=[hT.key])

    def declare_inputs(self):
        L = DEPTH
        d = self.d = {}
        specs = dict(x=[self.T, D], ln_in_g=[D], ln_in_b=[D], w_in=[L, D, NIN], ssd_conv_w=[L, 4, 768], ssd_conv_b=[L, 768],
                     ssd_dt_bias=[L, 8], ssd_a_log=[L, 8], ssd_d=[L, 8], ssd_norm_g=[L, 512], rwkv_mu=[L, 896], rwkv_w0=[L, 256],
                     rwkv_w2=[L, 32, 256], rwkv_a0=[L, 256], rwkv_a2=[L, 32, 256], rwkv_g2=[L, 64, 256], rwkv_k_k=[L, 256],
                     rwkv_k_a=[L, 256], rwkv_r_k=[L, 256], rwkv_ln_g=[L, 256], rwkv_ln_b=[L, 256], gla_w_a2=[L, 16, 128],
                     gla_b_a=[L, 128], gla_norm_g=[L, 256], w_out=[L, D, D], ln1_g=[L, D], ln1_b=[L, D], moe_w_rg=[L, D, 4],
                     moe_b_rg=[L, 4], moe_w_re=[L, D, 32], moe_b_re=[L, 32], moe_w_gate=[L * NE * D, FF], moe_w_up=[L * NE * D, FF],
                     moe_w_down=[L * NE * FF, D], ln2_g=[L, D], ln2_b=[L, D])
        for k, s in specs.items():
            d[k] = self.din(k, s)
        return specs

    def alloc_params(self):
        p = self.p = {}
        sb = self.sb
        p['w_in'] = sb([128, 8, NIN], BF16, "w_in_sb")
        p['w_out'] = sb([128, 8, D], BF16, "w_out_sb")
        p['cw'] = sb([128, 6, 4], name="convw")
        p['cb'] = sb([128, 6], name="convb")
        p['cdiag'] = sb([128, 24, 128], BF16, "cdiag")
        for n, w in (('dtb', 8), ('alog', 8), ('dsk8', 8), ('dsk', 512), ('sng', 512), ('rlg', 256), ('rlb', 256), ('gng', 256),
                     ('l1g', D), ('l1b', D), ('rb36', 36)):
            p[n] = sb([128, w], name="p_" + n)
        for n, w in (('mu', 7), ('omu', 7), ('w0', 2), ('a0', 2), ('kk', 2), ('ka', 2), ('omka', 2), ('rk', 2)):
            p[n] = sb([128, w], name="p_" + n)
        p['ba'] = sb([64, 2], name="p_ba")
        p['w2p'] = sb([128, 256], name="p_w2p")
        p['a2p'] = sb([128, 256], name="p_a2p")
        p['g2p'] = sb([128, 256], name="p_g2p")
        p['wa2'] = sb([32, 128], name="p_wa2")
        p['wr'] = sb([128, 8, 36], name="p_wr")

    def load_params(self, l):
        p, d, c = self.p, self.d, self.c
        q = 'pool'
        win = d['w_in'][l].rearrange("(kc p) n -> p kc n", p=128)
        for kc in range(8):
            for (a, b) in ((0, 1484), (1484, NIN)):
                self.load(q, p['w_in'][:, kc, a:b], win[:, kc, a:b], p['w_in'].key)
        wo = d['w_out'][l].rearrange("(kc p) n -> p kc n", p=128)
        for kc in range(8):
            self.load(q, p['w_out'][:, kc, :], wo[:, kc, :], p['w_out'].key)
        q = 'sp'
        for kk_ in range(4):
            self.load(q, p['cw'][:, :, kk_], d['ssd_conv_w'][l][kk_].rearrange("(cb p) -> p cb", p=128), p['cw'].key, slow=True)
        self.load(q, p['cb'][:], d['ssd_conv_b'][l].rearrange("(cb p) -> p cb", p=128), p['cb'].key, slow=True)
        for n, src in (('dtb', 'ssd_dt_bias'), ('alog', 'ssd_a_log'), ('dsk8', 'ssd_d'), ('sng', 'ssd_norm_g'), ('rlg', 'rwkv_ln_g'),
                       ('rlb', 'rwkv_ln_b'), ('gng', 'gla_norm_g'), ('l1g', 'ln1_g'), ('l1b', 'ln1_b')):
            self.load(q, p[n][:], d[src][l].partition_broadcast(128), p[n].key)
        self.load(q, p['rb36'][:, 0:4], d['moe_b_rg'][l].partition_broadcast(128), p['rb36'].key)
        self.load(q, p['rb36'][:, 4:36], d['moe_b_re'][l].partition_broadcast(128), p['rb36'].key)
        self.load(q, p['mu'][:], d['rwkv_mu'][l].rearrange("(b p) -> p b", p=128), p['mu'].key, slow=True)
        for n, src in (('w0', 'rwkv_w0'), ('a0', 'rwkv_a0'), ('kk', 'rwkv_k_k'), ('ka', 'rwkv_k_a'), ('rk', 'rwkv_r_k')):
            self.load(q, p[n][:], d[src][l].rearrange("(b p) -> p b", p=128), p[n].key, slow=True)
        self.load(q, p['ba'][:], d['gla_b_a'][l].rearrange("(b p) -> p b", p=64), p['ba'].key, slow=True)
        for n in ('w2p', 'a2p', 'g2p'):
            self.G(lambda e, n=n: e.memset(p[n][:], 0.0), w=[p[n].key])
        self.load(q, p['w2p'][0:32, :], d['rwkv_w2'][l], p['w2p'].key)
        self.load(q, p['a2p'][32:64, :], d['rwkv_a2'][l], p['a2p'].key)
        self.load(q, p['g2p'][64:128, :], d['rwkv_g2'][l], p['g2p'].key)
        self.G(lambda e: e.memset(p['wa2'][:], 0.0), w=[p['wa2'].key])
        self.load(q, p['wa2'][16:32, :], d['gla_w_a2'][l], p['wa2'].key)
        self.load(q, p['wr'][:, :, 0:4], d['moe_w_rg'][l].rearrange("(kc p) n -> p kc n", p=128), p['wr'].key, slow=True)
        self.load(q, p['wr'][:, :, 4:36], d['moe_w_re'][l].rearrange("(kc p) n -> p kc n", p=128), p['wr'].key, slow=True)
        self.V(lambda e: e.tensor_scalar(p['omu'][:], p['mu'][:], -1.0, 1.0, ALU.mult, ALU.add), r=[p['mu'].key], w=[p['omu'].key])
        self.V(lambda e: e.tensor_scalar(p['omka'][:], p['ka'][:], -1.0, 1.0, ALU.mult, ALU.add), r=[p['ka'].key], w=[p['omka'].key])
        self.A(lambda e: e.activation(out=p['alog'][:], in_=p['alog'][:], func=AF.Exp), r=[p['alog'].key], w=[p['alog'].key])
        self.V(lambda e: e.tensor_scalar(p['alog'][:], p['alog'][:], -1.0, None, ALU.mult), r=[p['alog'].key], w=[p['alog'].key])
        self.V(lambda e: e.tensor_copy(p['dsk'][:].rearrange("p (h q) -> p h q", q=64), p['dsk8'][:].unsqueeze(2).to_broadcast([128, 8, 64])),
               r=[p['dsk8'].key], w=[p['dsk'].key])
        for cb in range(6):
            for k in range(4):
                self.V(lambda e, cb=cb, k=k: e.tensor_scalar(p['cdiag'][:, cb * 4 + k, :], c['identf'][:], p['cw'][:, cb, k:k + 1], None, ALU.mult),
                       r=[c['identf'].key, p['cw'].key], w=[p['cdiag'].key])

    def alloc_mixer(self):
        s = self.s = {}
        sb = self.sb
        s['hT'] = sb([128, 8, 128], BF16, "m_hT")
        s['htm'] = sb([128, D], name="m_htm")
        s['xbc'] = sb([128, 6, 132], BF16, "m_xbc")
        s['xbB'] = sb([128, 6, 132], BF16, "m_xbB")
        s['xc'] = sb([128, 6, 128], BF16, "m_xc")
        s['xh'] = sb([128, 512], BF16, "m_xh")
        s['xdt'] = sb([128, 512], BF16, "m_xdt")
        s['btm'] = sb([128, 128], BF16, "m_btm")
        s['cm'] = sb([128, 2, 128], BF16, "m_cm")
        s['dt'] = sb([128, 8], name="m_dt")
        s['adt'] = sb([128, 8], name="m_adt")
        s['sp1'] = sb([128, 8], name="m_sp1")
        s['sp2'] = sb([128, 8], name="m_sp2")
        s['R'] = sb([128, 4, 128], name="m_R")
        s['seg'] = sb([128, 8, 128], BF16, "m_seg")
        s['ea'] = sb([128, 8], name="m_ea")
        s['cd'] = sb([128, 4], name="m_cd")
        s['cbm'] = sb([128, 2, 128], BF16, "m_cbm")
        s['toend'] = sb([128, 8], name="m_toend")
        s['S32'] = sb([128, 256], name="m_S32")
        s['Sbf'] = sb([128, 256], BF16, "m_Sbf")
        s['y1'] = sb([128, 512], name="m_y1")
        s['y2'] = sb([128, 512], name="m_y2")
        s['sz'] = sb([128, 512], BF16, "m_sz")
        s['ssq'] = sb([128, 4], name="m_ssq")
        s['ycat'] = sb([128, D], BF16, "m_ycat")
        s['yT'] = sb([128, 8, 128], BF16, "m_yT")
        s['gaT'] = sb([32, 128], name="g_gaT")
        s['gx'] = sb([64, 256], name="g_x")
        s['gt1'] = sb([64, 256], name="g_t1")
        s['gcum'] = sb([64, 256], name="g_cum")
        s['geq'] = sb([64, 256], name="g_eq")
        s['gek'] = sb([64, 256], name="g_ek")
        s['gel'] = sb([64, 4], name="g_el")
        s['gqm'] = sb([64, 2, 256], BF16, "g_qm")
        s['gkT'] = sb([64, 256], BF16, "g_kT")
        s['gktm'] = sb([128, 2, 128], BF16, "g_ktm")
        s['gv'] = sb([128, 256], BF16, "g_v")
        s['gvm'] = sb([128, 2, 256], BF16, "g_vm")
        s['gsm'] = sb([128, 4, 128], BF16, "g_sm")
        s['gS'] = sb([64, 2, 64], name="g_S")
        s['gSb'] = sb([64, 2, 2, 64], BF16, "g_Sb")
        s['gst'] = sb([64, 2, 64], name="g_st")
        s['go'] = sb([128, 256], name="g_o")
        s['gsq'] = sb([128, 256], name="g_sq")
        s['grs'] = sb([128, 4], name="g_rs")
        s['gsg'] = sb([128, 256], name="g_sg")
        s['rw'] = sb([128, 7, 129], name="r_rw")
        s['rsh'] = sb([128, 7, 128], name="r_sh")
        for n in ('ra1', 'ra2', 'ra3', 'rcw', 'recw', 'reicw', 'recwp', 'ra', 'rkk', 'rkp', 'rKt', 'rBt'):
            s[n] = sb([128, 256], name="r_" + n)
        s['rAm'] = sb([128, 2, 256], name="r_Am")
        s['rRm'] = sb([128, 2, 256], name="r_Rm")
        s['rtw'] = sb([128, 128], name="r_tw")
        s['rsg'] = sb([128, 128], name="r_sg")
        s['rvtm'] = sb([128, 256], name="r_vtm")
        s['rvc'] = sb([64, 2, 256], name="r_vc")
        s['rBc'] = sb([64, 2, 256], name="r_Bc")
        s['rKc'] = sb([64, 2, 256], name="r_Kc")
        s['rP'] = sb([64, 8, 64], name="r_P")
        s['rQ'] = sb([64, 8, 64], name="r_Q")
        s['rP2'] = sb([64, 8, 64], name="r_P2")
        s['rQ2'] = sb([64, 8, 64], name="r_Q2")
        s['rTT'] = sb([64, 8, 64], name="r_TT")
        s['rAak'] = sb([64, 8, 64], name="r_Aak")
        s['rArb'] = sb([64, 8, 64], name="r_Arb")
        s['rArk'] = sb([64, 8, 64], name="r_Ark")
        s['rG'] = sb([64, 4, 64], name="r_G")
        s['rU'] = sb([64, 4, 64], name="r_U")
        s['rST'] = sb([128, 2, 64], name="r_ST")
        s['rt2'] = sb([128, 2, 64], name="r_t2")
        s['rewc'] = sb([128, 4], name="r_ewc")
        s['rY1'] = sb([128, 256], name="r_Y1")
        s['rY'] = sb([128, 256], name="r_Y")
        s['rm1'] = sb([128, 4], name="r_m1")
        s['rm2'] = sb([128, 4], name="r_m2")
        s['rvar'] = sb([128, 4], name="r_var")
        s['rbc'] = sb([128, 4], name="r_bc")
        s['rg'] = sb([128, 256], name="r_g")
        s['mix'] = sb([128, D], name="m_mix")
        s['tmp'] = sb([128, D], name="m_tmp")
        s['h1'] = sb([128, D], name="m_h1")
        s['h1b'] = sb([128, D], BF16, "m_h1b")

    def proj_tm(self, out_ap, okey, c0, n):
        s, p = self.s, self.p
        for kc in range(8):
            self.M(lambda e, kc=kc: e.matmul(out_ap, lhsT=s['hT'][:, kc, :], rhs=p['w_in'][:, kc, c0:c0 + n], start=(kc == 0), stop=(kc == 7)),
                   r=[s['hT'].key, p['w_in'].key], w=[okey])

    def proj_fm(self, out_ap, okey, c0, m):
        s, p = self.s, self.p
        for kc in range(8):
            self.M(lambda e, kc=kc: e.matmul(out_ap, lhsT=p['w_in'][:, kc, c0:c0 + m], rhs=s['hT'][:, kc, :], start=(kc == 0), stop=(kc == 7)),
                   r=[s['hT'].key, p['w_in'].key], w=[okey])

    def bc(self, ap, shape, axis):
        return ap.unsqueeze(axis).to_broadcast(list(shape))

    def ssd_tile(self, first):
        s, p, c = self.s, self.p, self.c
        V, A, G, M = self.V, self.A, self.G, self.M
        k = lambda t: t.key
        bz, kz = self.bank('s')
        self.proj_tm(bz[:, :], kz, O_Z, 512)
        A(lambda e: e.activation(out=s['sz'][:], in_=bz[:, :], func=AF.Silu), r=[kz], w=[k(s['sz'])])
        yield
        bd, kd = self.bank('s')
        self.proj_tm(bd[:, 0:8], kd, O_DT, 8)
        V(lambda e: e.tensor_tensor(s['sp1'][:], bd[:, 0:8], p['dtb'][:], ALU.add), r=[kd, k(p['dtb'])], w=[k(s['sp1'])])
        yield
        V(lambda e: e.scalar_tensor_tensor(s['sp2'][:], s['sp1'][:], -1.0, s['sp1'][:], ALU.mult, ALU.max), r=[k(s['sp1'])], w=[k(s['sp2'])])
        yield
        A(lambda e: e.activation(out=s['sp2'][:], in_=s['sp2'][:], func=AF.Exp, scale=-1.0), r=[k(s['sp2'])], w=[k(s['sp2'])])
        yield
        A(lambda e: e.activation(out=s['sp2'][:], in_=s['sp2'][:], func=AF.Ln, bias=1.0), r=[k(s['sp2'])], w=[k(s['sp2'])])
        yield
        V(lambda e: e.scalar_tensor_tensor(s['dt'][:], s['sp1'][:], 0.0, s['sp2'][:], ALU.max, ALU.add), r=[k(s['sp1']), k(s['sp2'])], w=[k(s['dt'])])
        yield
        V(lambda e: e.tensor_tensor(s['adt'][:], s['dt'][:], p['alog'][:], ALU.mult), r=[k(s['dt']), k(p['alog'])], w=[k(s['adt'])])
        yield
        import os
        stop = float(os.environ.get('SSDSTOP', '9'))
        if stop <= 1:
            return
        if first:
            G(lambda e: e.memset(s['xbc'][:, :, 0:4], 0.0), w=[k(s['xbc'])])
            yield
            G(lambda e: e.memset(s['xbB'][:, :, 0:2], 0.0), w=[k(s['xbB'])])
            yield
        else:
            G(lambda e: e.tensor_copy(s['xbc'][:, :, 0:3], s['xbc'][:, :, 128:131]), r=[k(s['xbc'])], w=[k(s['xbc'])])
            yield
            G(lambda e: e.tensor_copy(s['xbB'][:, :, 0:2], s['xbB'][:, :, 128:130]), r=[k(s['xbB'])], w=[k(s['xbB'])])
            yield
        for grp, nb in ((0, 4), (4, 2)):
            bx, kx = self.bank('s')
            for j in range(nb):
                self.proj_fm(bx[:, j * 128:(j + 1) * 128], kx, O_XBC + (grp + j) * 128, 128)
            A(lambda e, bx=bx, grp=grp, nb=nb: e.copy(out=s['xbc'][:, grp:grp + nb, 3:131], in_=bx[:, 0:nb * 128].rearrange("p (a b) -> p a b", b=128)),
              r=[kx], w=[k(s['xbc'])])
            yield
            V(lambda e, bx=bx, grp=grp, nb=nb: e.tensor_copy(s['xbB'][:, grp:grp + nb, 2:130], bx[:, 0:nb * 128].rearrange("p (a b) -> p a b", b=128)),
              r=[kx], w=[k(s['xbB'])])
            yield
        for grp, nb in ((0, 4), (4, 2)):
            bx, kx = self.bank('s')
            for j in range(nb):
                cb = grp + j
                for kk_ in range(4):
                    src = s['xbc'] if kk_ % 2 == 0 else s['xbB']
                    off = kk_ if kk_ % 2 == 0 else kk_ - 1
                    M(lambda e, bx=bx, j=j, cb=cb, kk_=kk_, src=src, off=off: e.matmul(bx[:, j * 128:(j + 1) * 128], lhsT=p['cdiag'][:, cb * 4 + kk_, :],
                                                                                   rhs=src[:, cb, off:off + 128], start=(kk_ == 0), stop=(kk_ == 3)),
                      r=[k(p['cdiag']), k(src)], w=[kx])
            for j in range(nb):
                cb = grp + j
                A(lambda e, bx=bx, j=j, cb=cb: e.activation(out=s['xc'][:, cb, :], in_=bx[:, j * 128:(j + 1) * 128], func=AF.Silu, bias=p['cb'][:, cb:cb + 1]),
                  r=[kx, k(p['cb'])], w=[k(s['xc'])])
                yield
        if stop <= 2:
            return
        bt, kt = self.bank('s')
        pb = bt[:].bitcast(BF16)
        for j in range(5):
            M(lambda e, j=j: e.transpose(pb[:, j * 128:(j + 1) * 128], s['xc'][:, j, :], c['identb'][:]), r=[k(s['xc']), k(c['identb'])], w=[kt])
        V(lambda e: e.tensor_copy(s['xh'][:], pb[:, 0:512]), r=[kt], w=[k(s['xh'])])
        yield
        V(lambda e: e.tensor_copy(s['btm'][:], pb[:, 512:640]), r=[kt], w=[k(s['btm'])])
        yield
        if stop <= 2.2:
            return
        G(lambda e: e.tensor_tensor(s['cm'][:], self.bc(s['xc'][:, 5, :], [128, 2, 128], 1), self.bc(c['hm'][:, :], [128, 2, 128], 2), ALU.mult),
          r=[k(s['xc']), k(c['hm'])], w=[k(s['cm'])])
        yield
        V(lambda e: e.tensor_tensor(s['xdt'][:].rearrange("p (h q) -> p h q", q=64), s['xh'][:].rearrange("p (h q) -> p h q", q=64),
                                    self.bc(s['dt'][:, :], [128, 8, 64], 2), ALU.mult), r=[k(s['xh']), k(s['dt'])], w=[k(s['xdt'])])
        yield
        if stop <= 2.4:
            return
        for half in range(2):
            G(lambda e, half=half: e.tensor_tensor(s['R'][:], self.bc(c['tri'][:, :], [128, 4, 128], 1), self.bc(s['adt'][:, half * 4:(half + 1) * 4], [128, 4, 128], 2), ALU.mult),
              r=[k(c['tri']), k(s['adt'])], w=[k(s['R'])])
            yield
            bD, kD = self.bank('s')
            for q2 in range(2):
                M(lambda e, bD=bD, q2=q2: e.matmul(bD[:, q2 * 256:(q2 + 1) * 256], lhsT=c['su'][:], rhs=s['R'][:, q2 * 2:(q2 + 1) * 2, :].rearrange("p a b -> p (a b)"), start=True, stop=True),
                  r=[k(c['su']), k(s['R'])], w=[kD])
            if stop <= 2.6:
                continue
            A(lambda e, bD=bD, half=half: e.activation(out=s['seg'][:, half * 4:(half + 1) * 4, :].rearrange("p a b -> p (a b)"), in_=bD[:, :], func=AF.Exp),
              r=[kD], w=[k(s['seg'])])
            yield
        if stop <= 2.8:
            return
        V(lambda e: e.tensor_copy(s['toend'][:], s['seg'][:, :, 127]), r=[k(s['seg'])], w=[k(s['toend'])])
        yield
        if stop <= 3:
            return
        be, ke = self.bank('s')
        M(lambda e: e.matmul(be[:, 0:8], lhsT=c['tri'][:], rhs=s['adt'][:], start=True, stop=True), r=[k(c['tri']), k(s['adt'])], w=[ke])
        for g in range(2):
            M(lambda e, g=g: e.matmul(be[g * 64:(g + 1) * 64, 8:12], lhsT=c['onesf'][:, 0:64], rhs=s['adt'][:, g * 4:(g + 1) * 4], start=True, stop=True),
              r=[k(c['onesf']), k(s['adt'])], w=[ke])
        A(lambda e: e.activation(out=s['ea'][:], in_=be[:, 0:8], func=AF.Exp), r=[ke], w=[k(s['ea'])])
        yield
        A(lambda e: e.activation(out=s['cd'][:], in_=be[:, 8:12], func=AF.Exp), r=[ke], w=[k(s['cd'])])
        yield
        if stop <= 4:
            return
        bc_, kc_ = self.bank('s')
        for g in range(2):
            M(lambda e, g=g: e.matmul(bc_[:, g * 128:(g + 1) * 128], lhsT=s['xc'][:, 4, :], rhs=s['cm'][:, g, :], start=True, stop=True),
              r=[k(s['xc']), k(s['cm'])], w=[kc_])
        V(lambda e: e.tensor_tensor(s['cbm'][:], bc_[:, 0:256].rearrange("p (a b) -> p a b", b=128), self.bc(c['tri'][:, :], [128, 2, 128], 1), ALU.mult),
          r=[kc_, k(c['tri'])], w=[k(s['cbm'])])
        yield
        for g in range(2):
            V(lambda e, g=g: e.tensor_tensor(s['seg'][:, g * 4:(g + 1) * 4, :], s['seg'][:, g * 4:(g + 1) * 4, :], self.bc(s['cbm'][:, g, :], [128, 4, 128], 1), ALU.mult),
              r=[k(s['seg']), k(s['cbm'])], w=[k(s['seg'])])
            yield
        by, ky = self.bank('s')
        for h in range(8):
            M(lambda e, h=h: e.matmul(by[:, h * 64:(h + 1) * 64], lhsT=s['seg'][:, h, :], rhs=s['xdt'][:, h * 64:(h + 1) * 64], start=True, stop=True),
              r=[k(s['seg']), k(s['xdt'])], w=[ky])
        bo, ko = self.bank('s')
        if not first:
            for g in range(2):
                M(lambda e, g=g: e.matmul(bo[:, g * 256:(g + 1) * 256], lhsT=s['cm'][:, g, :], rhs=s['Sbf'][:, :], start=True, stop=True),
                  r=[k(s['cm']), k(s['Sbf'])], w=[ko])
            V(lambda e: e.tensor_tensor(s['y1'][:].rearrange("p (h q) -> p h q", q=64), bo[:, :].rearrange("p (h q) -> p h q", q=64),
                                        self.bc(s['ea'][:, :], [128, 8, 64], 2), ALU.mult), r=[ko, k(s['ea'])], w=[k(s['y1'])])
            yield
            V(lambda e: e.tensor_tensor(s['y1'][:], s['y1'][:], by[:, :], ALU.add), r=[k(s['y1']), ky], w=[k(s['y1'])])
            yield
        else:
            V(lambda e: e.tensor_copy(s['y1'][:], by[:, :]), r=[ky], w=[k(s['y1'])])
            yield
        if stop <= 5:
            return
        V(lambda e: e.tensor_tensor(s['xdt'][:].rearrange("p (h q) -> p h q", q=64), s['xdt'][:].rearrange("p (h q) -> p h q", q=64),
                                    self.bc(s['toend'][:, :], [128, 8, 64], 2), ALU.mult), r=[k(s['xdt']), k(s['toend'])], w=[k(s['xdt'])])
        yield
        bs, ks = self.bank('s')
        for g in range(2):
            M(lambda e, g=g: e.matmul(bs[g * 64:(g + 1) * 64, 0:256], lhsT=s['btm'][:, g * 64:(g + 1) * 64], rhs=s['xdt'][:, g * 256:(g + 1) * 256], start=True, stop=True),
              r=[k(s['btm']), k(s['xdt'])], w=[ks])
        if first:
            V(lambda e: e.tensor_copy(s['S32'][:], bs[:, 0:256]), r=[ks], w=[k(s['S32'])])
            yield
        else:
            V(lambda e: e.tensor_tensor(s['S32'][:].rearrange("p (h q) -> p h q", q=64), s['S32'][:].rearrange("p (h q) -> p h q", q=64),
                                        self.bc(s['cd'][:, :], [128, 4, 64], 2), ALU.mult), r=[k(s['S32']), k(s['cd'])], w=[k(s['S32'])])
            yield
            V(lambda e: e.tensor_tensor(s['S32'][:], s['S32'][:], bs[:, 0:256], ALU.add), r=[k(s['S32']), ks], w=[k(s['S32'])])
            yield
        A(lambda e: e.copy(out=s['Sbf'][:], in_=s['S32'][:]), r=[k(s['S32'])], w=[k(s['Sbf'])])
        yield
        G(lambda e: e.tensor_tensor(s['y2'][:], s['xh'][:], p['dsk'][:], ALU.mult), r=[k(s['xh']), k(p['dsk'])], w=[k(s['y2'])])
        yield
        V(lambda e: e.tensor_tensor(s['y1'][:], s['y1'][:], s['y2'][:], ALU.add), r=[k(s['y1']), k(s['y2'])], w=[k(s['y1'])])
        yield
        V(lambda e: e.tensor_tensor(s['y1'][:], s['y1'][:], s['sz'][:], ALU.mult), r=[k(s['y1']), k(s['sz'])], w=[k(s['y1'])])
        yield
        for g in range(2):
            A(lambda e, g=g: e.activation(out=s['y2'][:, g * 256:(g + 1) * 256], in_=s['y1'][:, g * 256:(g + 1) * 256], func=AF.Square, accum_out=s['ssq'][:, g:g + 1]),
              r=[k(s['y1'])], w=[k(s['y2']), k(s['ssq'])])
            yield
        V(lambda e: e.tensor_scalar(s['ssq'][:, 0:2], s['ssq'][:, 0:2], 1.0 / 256, RMS_EPS, ALU.mult, ALU.add), r=[k(s['ssq'])], w=[k(s['ssq'])])
        yield
        A(lambda e: e.activation(out=s['ssq'][:, 0:2], in_=s['ssq'][:, 0:2], func=AF.Sqrt), r=[k(s['ssq'])], w=[k(s['ssq'])])
        yield
        V(lambda e: e.reciprocal(s['ssq'][:, 0:2], s['ssq'][:, 0:2]), r=[k(s['ssq'])], w=[k(s['ssq'])])
        yield
        for g in range(2):
            V(lambda e, g=g: e.scalar_tensor_tensor(s['ycat'][:, g * 256:(g + 1) * 256], s['y1'][:, g * 256:(g + 1) * 256], s['ssq'][:, g:g + 1],
                                                   p['sng'][:, g * 256:(g + 1) * 256], ALU.mult, ALU.mult), r=[k(s['y1']), k(s['ssq']), k(p['sng'])], w=[k(s['ycat'])])
            yield

    def gla_tile(self, first):
        s, p, c = self.s, self.p, self.c
        V, A, G, M = self.V, self.A, self.G, self.M
        k = lambda t: t.key
        bv, kv = self.bank('g')
        self.proj_tm(bv[:, :], kv, O_GV, 512)
        A(lambda e: e.copy(out=s['gv'][:], in_=bv[:, 0:256]), r=[kv], w=[k(s['gv'])])
        yield
        A(lambda e: e.activation(out=s['gsg'][:], in_=bv[:, 256:512], func=AF.Silu), r=[kv], w=[k(s['gsg'])])
        yield
        G(lambda e: e.tensor_tensor(s['gsg'][:], s['gsg'][:], p['gng'][:], ALU.mult), r=[k(s['gsg']), k(p['gng'])], w=[k(s['gsg'])])
        yield
        ba_, ka_ = self.bank('g')
        self.proj_fm(ba_[0:32, 0:128], ka_, O_GA - 16, 32)
        A(lambda e: e.copy(out=s['gaT'][:], in_=ba_[0:32, 0:128]), r=[ka_], w=[k(s['gaT'])])
        yield
        bx, kx = self.bank('g')
        for pr in range(2):
            M(lambda e, pr=pr: e.matmul(bx[0:64, pr * 128:(pr + 1) * 128], lhsT=p['wa2'][:, pr * 64:(pr + 1) * 64], rhs=s['gaT'][:], start=True, stop=True),
              r=[k(p['wa2']), k(s['gaT'])], w=[kx])
        for pr in range(2):
            A(lambda e, pr=pr: e.activation(out=s['gx'][:, pr * 128:(pr + 1) * 128], in_=bx[0:64, pr * 128:(pr + 1) * 128], func=AF.Identity, bias=p['ba'][:, pr:pr + 1]),
              r=[kx, k(p['ba'])], w=[k(s['gx'])])
            yield
        V(lambda e: e.scalar_tensor_tensor(s['gt1'][:], s['gx'][:], -1.0, s['gx'][:], ALU.mult, ALU.max), r=[k(s['gx'])], w=[k(s['gt1'])])
        yield
        A(lambda e: e.activation(out=s['gt1'][:], in_=s['gt1'][:], func=AF.Exp, scale=-1.0), r=[k(s['gt1'])], w=[k(s['gt1'])])
        yield
        A(lambda e: e.activation(out=s['gt1'][:], in_=s['gt1'][:], func=AF.Ln, bias=1.0), r=[k(s['gt1'])], w=[k(s['gt1'])])
        yield
        V(lambda e: e.scalar_tensor_tensor(s['gx'][:], s['gx'][:], 0.0, s['gt1'][:], ALU.min, ALU.subtract), r=[k(s['gx']), k(s['gt1'])], w=[k(s['gx'])])
        yield
        V(lambda e: e.tensor_tensor_scan(s['gcum'][:], c['rmask'][0:64, :], s['gx'][:], 0.0, ALU.mult, ALU.add), r=[k(c['rmask']), k(s['gx'])], w=[k(s['gcum'])])
        yield
        A(lambda e: e.activation(out=s['geq'][:], in_=s['gcum'][:], func=AF.Exp, scale=1.0 / 16), r=[k(s['gcum'])], w=[k(s['geq'])])
        yield
        A(lambda e: e.activation(out=s['gek'][:], in_=s['gcum'][:], func=AF.Exp, scale=-1.0 / 16), r=[k(s['gcum'])], w=[k(s['gek'])])
        yield
        A(lambda e: e.activation(out=s['gel'][:], in_=s['gcum'][:].rearrange("p (a b) -> p a b", b=64)[:, :, 63], func=AF.Exp, scale=1.0 / 16),
          r=[k(s['gcum'])], w=[k(s['gel'])])
        yield
        bq, kq = self.bank('g')
        for pr in range(2):
            self.proj_fm(bq[0:64, pr * 128:(pr + 1) * 128], kq, O_GQ + pr * 64, 64)
        for pr in range(2):
            self.proj_fm(bq[0:64, 256 + pr * 128:256 + (pr + 1) * 128], kq, O_GK + pr * 64, 64)
        for hh in range(2):
            V(lambda e, hh=hh: e.scalar_tensor_tensor(s['gqm'][:, hh, :], bq[0:64, 0:256], c['qm'][:, hh:hh + 1], s['geq'][:], ALU.mult, ALU.mult),
              r=[kq, k(c['qm']), k(s['geq'])], w=[k(s['gqm'])])
            yield
        V(lambda e: e.tensor_tensor(s['gkT'][:], bq[0:64, 256:512], s['gek'][:], ALU.mult), r=[kq, k(s['gek'])], w=[k(s['gkT'])])
        yield
        bt, kt = self.bank('g')
        pb = bt[:].bitcast(BF16)
        for pr in range(2):
            M(lambda e, pr=pr: e.transpose(pb[:, pr * 64:(pr + 1) * 64], s['gkT'][:, pr * 128:(pr + 1) * 128], c['identb'][0:64, 0:64]),
              r=[k(s['gkT']), k(c['identb'])], w=[kt])
        if first:
            G(lambda e: e.memset(s['gktm'][:], 0.0), w=[k(s['gktm'])])
            yield
        for hh in range(2):
            V(lambda e, hh=hh: e.tensor_copy(s['gktm'][:, hh, :].rearrange("p (a b c) -> p a b c", a=2, b=2)[:, :, hh, :],
                                             pb[:, 0:128].rearrange("p (a b c) -> p a b c", a=2, b=2)[:, :, hh, :]), r=[kt], w=[k(s['gktm'])])
            yield
        G(lambda e: e.tensor_tensor(s['gvm'][:], self.bc(s['gv'][:, :], [128, 2, 256], 1), self.bc(c['hm'][:, :], [128, 2, 256], 2), ALU.mult),
          r=[k(s['gv']), k(c['hm'])], w=[k(s['gvm'])])
        yield
        bs_, ks_ = self.bank('g')
        for h in range(4):
            pr, hh = h // 2, h % 2
            M(lambda e, h=h, pr=pr, hh=hh: e.matmul(bs_[:, h * 128:(h + 1) * 128], lhsT=s['gkT'][:, pr * 128:(pr + 1) * 128], rhs=s['gqm'][:, hh, pr * 128:(pr + 1) * 128],
                                                    start=True, stop=True), r=[k(s['gkT']), k(s['gqm'])], w=[ks_])
        V(lambda e: e.tensor_tensor(s['gsm'][:], bs_[:, :].rearrange("p (a b) -> p a b", b=128), self.bc(c['maskb'][:, :], [128, 4, 128], 1), ALU.mult),
          r=[ks_, k(c['maskb'])], w=[k(s['gsm'])])
        yield
        bu, ku = self.bank('g')
        for cc in range(2):
            for pr in range(2):
                for hh in range(2):
                    h = pr * 2 + hh
                    M(lambda e, cc=cc, h=h, pr=pr, hh=hh: e.matmul(bu[0:64, cc * 128 + pr * 64:cc * 128 + (pr + 1) * 64],
                                                                   lhsT=s['gktm'][:, hh, pr * 64:(pr + 1) * 64], rhs=s['gvm'][:, cc, h * 64:(h + 1) * 64],
                                                                   start=(hh == 0), stop=(hh == 1)), r=[k(s['gktm']), k(s['gvm'])], w=[ku])
        if first:
            G(lambda e: e.memset(s['gS'][:], 0.0), w=[k(s['gS'])])
            yield
        for cc in range(2):
            A(lambda e, cc=cc: e.copy(out=s['gSb'][:, cc, :, :], in_=s['gS'][:]), r=[k(s['gS'])], w=[k(s['gSb'])])
            yield
            V(lambda e, cc=cc: e.tensor_tensor(s['gst'][:], s['gS'][:], bu[0:64, cc * 128:(cc + 1) * 128].rearrange("p (a b) -> p a b", b=64), ALU.add),
              r=[k(s['gS']), ku], w=[k(s['gst'])])
            yield
            V(lambda e, cc=cc: e.tensor_tensor(s['gS'][:], s['gst'][:], self.bc(s['gel'][:, cc::2], [64, 2, 64], 2), ALU.mult),
              r=[k(s['gst']), k(s['gel'])], w=[k(s['gS'])])
            yield
        bo, ko = self.bank('g')
        for h in range(4):
            M(lambda e, h=h: e.matmul(bo[:, h * 64:(h + 1) * 64], lhsT=s['gsm'][:, h, :], rhs=s['gv'][:, h * 64:(h + 1) * 64], start=True, stop=True),
              r=[k(s['gsm']), k(s['gv'])], w=[ko])
        bi, ki = self.bank('g')
        for cc in range(2):
            for h in range(4):
                pr, hh = h // 2, h % 2
                M(lambda e, cc=cc, h=h, pr=pr, hh=hh: e.matmul(bi[cc * 64:(cc + 1) * 64, h * 64:(h + 1) * 64], lhsT=s['gqm'][:, hh, pr * 128 + cc * 64:pr * 128 + (cc + 1) * 64],
                                                               rhs=s['gSb'][:, cc, pr, :], start=True, stop=True), r=[k(s['gqm']), k(s['gSb'])], w=[ki])
        A(lambda e: e.copy(out=s['gsq'][:], in_=bi[:, 0:256]), r=[ki], w=[k(s['gsq'])])
        yield
        V(lambda e: e.tensor_tensor(s['go'][:], s['gsq'][:], bo[:, 0:256], ALU.add), r=[k(s['gsq']), ko], w=[k(s['go'])])
        yield
        A(lambda e: e.activation(out=s['gsq'][:], in_=s['go'][:], func=AF.Square), r=[k(s['go'])], w=[k(s['gsq'])])
        yield
        V(lambda e: e.tensor_reduce(s['grs'][:], s['gsq'][:].rearrange("p (h q) -> p h q", q=64), AX.X, ALU.add), r=[k(s['gsq'])], w=[k(s['grs'])])
        yield
        V(lambda e: e.tensor_scalar(s['grs'][:], s['grs'][:], 1.0 / 64, RMS_EPS, ALU.mult, ALU.add), r=[k(s['grs'])], w=[k(s['grs'])])
        yield
        A(lambda e: e.activation(out=s['grs'][:], in_=s['grs'][:], func=AF.Sqrt), r=[k(s['grs'])], w=[k(s['grs'])])
        yield
        V(lambda e: e.reciprocal(s['grs'][:], s['grs'][:]), r=[k(s['grs'])], w=[k(s['grs'])])
        yield
        V(lambda e: e.tensor_tensor(s['go'][:].rearrange("p (h q) -> p h q", q=64), s['go'][:].rearrange("p (h q) -> p h q", q=64),
                                    self.bc(s['grs'][:, :], [128, 4, 64], 2), ALU.mult), r=[k(s['go']), k(s['grs'])], w=[k(s['go'])])
        yield
        V(lambda e: e.tensor_tensor(s['ycat'][:, 768:1024], s['go'][:], s['gsg'][:], ALU.mult), r=[k(s['go']), k(s['gsg'])], w=[k(s['ycat'])])
        yield

    def rwkv_tile(self, first):
        s, p, c = self.s, self.p, self.c
        V, A, G, M = self.V, self.A, self.G, self.M
        k = lambda t: t.key
        f2 = lambda t, a, b: t[:, a:b, :].rearrange("p a b -> p (a b)")
        h3 = lambda ap: ap.rearrange("p (h q) -> p h q", q=64)
        if first:
            G(lambda e: e.memset(s['rw'][:, :, 0:1], 0.0), w=[k(s['rw'])])
            yield
            G(lambda e: e.memset(s['rST'][:], 0.0), w=[k(s['rST'])])
            yield
        else:
            G(lambda e: e.tensor_copy(s['rw'][:, :, 0:1], s['rw'][:, :, 128:129]), r=[k(s['rw'])], w=[k(s['rw'])])
            yield
        for grp, nb in ((0, 4), (4, 3)):
            bx, kx = self.bank('r')
            for j in range(nb):
                self.proj_fm(bx[:, j * 128:(j + 1) * 128], kx, O_RW + (grp + j) * 128, 128)
            A(lambda e, bx=bx, grp=grp, nb=nb: e.copy(out=s['rw'][:, grp:grp + nb, 1:129], in_=bx[:, 0:nb * 128].rearrange("p (a b) -> p a b", b=128)),
              r=[kx], w=[k(s['rw'])])
            yield
        rt1 = s['tmp'][:, 0:896].rearrange("p (a b) -> p a b", b=128)
        G(lambda e: e.tensor_tensor(rt1, s['rw'][:, :, 0:128], self.bc(p['mu'][:, :], [128, 7, 128], 2), ALU.mult), r=[k(s['rw']), k(p['mu'])], w=[k(s['tmp'])])
        yield
        V(lambda e: e.tensor_tensor(s['rsh'][:], s['rw'][:, :, 1:129], self.bc(p['omu'][:, :], [128, 7, 128], 2), ALU.mult), r=[k(s['rw']), k(p['omu'])], w=[k(s['rsh'])])
        yield
        V(lambda e: e.tensor_tensor(s['rsh'][:], s['rsh'][:], rt1, ALU.add), r=[k(s['rsh']), k(s['tmp'])], w=[k(s['rsh'])])
        yield
        rT, kT, vT, lr = f2(s['rsh'], 0, 2), f2(s['rsh'], 2, 4), f2(s['rsh'], 4, 6), s['rsh'][:, 6, :]
        ksh = k(s['rsh'])
        A(lambda e: e.activation(out=s['rtw'][:], in_=lr, func=AF.Tanh), r=[ksh], w=[k(s['rtw'])])
        yield
        bw, kw = self.bank('r')
        for b in range(2):
            M(lambda e, b=b: e.matmul(bw[:, b * 128:(b + 1) * 128], lhsT=p['w2p'][:, b * 128:(b + 1) * 128], rhs=s['rtw'][:], start=True, stop=True),
              r=[k(p['w2p']), k(s['rtw'])], w=[kw])
        for b in range(2):
            A(lambda e, b=b: e.activation(out=s['ra1'][:, b * 128:(b + 1) * 128], in_=bw[:, b * 128:(b + 1) * 128], func=AF.Identity, bias=p['w0'][:, b:b + 1]),
              r=[kw, k(p['w0'])], w=[k(s['ra1'])])
            yield
        V(lambda e: e.scalar_tensor_tensor(s['ra2'][:], s['ra1'][:], -1.0, s['ra1'][:], ALU.mult, ALU.max), r=[k(s['ra1'])], w=[k(s['ra2'])])
        yield
        A(lambda e: e.activation(out=s['ra2'][:], in_=s['ra2'][:], func=AF.Exp, scale=-1.0), r=[k(s['ra2'])], w=[k(s['ra2'])])
        yield
        A(lambda e: e.activation(out=s['ra2'][:], in_=s['ra2'][:], func=AF.Ln, bias=1.0), r=[k(s['ra2'])], w=[k(s['ra2'])])
        yield
        V(lambda e: e.tensor_scalar(s['ra3'][:], s['ra1'][:], -1.0, 0.0, ALU.mult, ALU.max), r=[k(s['ra1'])], w=[k(s['ra3'])])
        yield
        V(lambda e: e.tensor_tensor(s['ra3'][:], s['ra3'][:], s['ra2'][:], ALU.add), r=[k(s['ra3']), k(s['ra2'])], w=[k(s['ra3'])])
        yield
        A(lambda e: e.activation(out=s['ra1'][:], in_=s['ra3'][:], func=AF.Exp, scale=-1.0), r=[k(s['ra3'])], w=[k(s['ra1'])])
        yield
        V(lambda e: e.tensor_scalar(s['ra1'][:], s['ra1'][:], -float(np.exp(-0.5)), None, ALU.mult), r=[k(s['ra1'])], w=[k(s['ra1'])])
        yield
        V(lambda e: e.tensor_tensor_scan(s['rcw'][:], c['rmask'][:], s['ra1'][:], 0.0, ALU.mult, ALU.add), r=[k(c['rmask']), k(s['ra1'])], w=[k(s['rcw'])])
        yield
        V(lambda e: e.tensor_tensor(s['ra2'][:], s['rcw'][:], s['ra1'][:], ALU.subtract), r=[k(s['rcw']), k(s['ra1'])], w=[k(s['ra2'])])
        yield
        A(lambda e: e.activation(out=s['recw'][:], in_=s['rcw'][:], func=AF.Exp), r=[k(s['rcw'])], w=[k(s['recw'])])
        yield
        A(lambda e: e.activation(out=s['reicw'][:], in_=s['rcw'][:], func=AF.Exp, scale=-1.0), r=[k(s['rcw'])], w=[k(s['reicw'])])
        yield
        A(lambda e: e.activation(out=s['recwp'][:], in_=s['ra2'][:], func=AF.Exp), r=[k(s['ra2'])], w=[k(s['recwp'])])
        yield
        ba_, ka_ = self.bank('r')
        for b in range(2):
            M(lambda e, b=b: e.matmul(ba_[:, b * 128:(b + 1) * 128], lhsT=p['a2p'][:, b * 128:(b + 1) * 128], rhs=lr, start=True, stop=True),
              r=[k(p['a2p']), ksh], w=[ka_])
        for b in range(2):
            A(lambda e, b=b: e.activation(out=s['ra'][:, b * 128:(b + 1) * 128], in_=ba_[:, b * 128:(b + 1) * 128], func=AF.Sigmoid, bias=p['a0'][:, b:b + 1]),
              r=[ka_, k(p['a0'])], w=[k(s['ra'])])
            yield
        A(lambda e: e.activation(out=s['rsg'][:], in_=lr, func=AF.Sigmoid), r=[ksh], w=[k(s['rsg'])])
        yield
        bg, kg = self.bank('r')
        M(lambda e: e.matmul(bg[:, 0:256], lhsT=s['rsg'][:], rhs=p['g2p'][:], start=True, stop=True), r=[k(s['rsg']), k(p['g2p'])], w=[kg])
        A(lambda e: e.copy(out=s['rg'][:], in_=bg[:, 0:256]), r=[kg], w=[k(s['rg'])])
        yield
        V(lambda e: e.tensor_tensor(s['rkk'][:].rearrange("p (a b) -> p a b", b=128), kT.rearrange("p (a b) -> p a b", b=128), self.bc(p['kk'][:, :], [128, 2, 128], 2), ALU.mult),
          r=[ksh, k(p['kk'])], w=[k(s['rkk'])])
        yield
        A(lambda e: e.activation(out=s['ra2'][:], in_=s['rkk'][:], func=AF.Square), r=[k(s['rkk'])], w=[k(s['ra2'])])
        yield
        bn, kn = self.bank('r')
        for b in range(2):
            M(lambda e, b=b: e.matmul(bn[:, b * 128:(b + 1) * 128], lhsT=c['bones'][:], rhs=s['ra2'][:, b * 128:(b + 1) * 128], start=True, stop=True),
              r=[k(c['bones']), k(s['ra2'])], w=[kn])
        V(lambda e: e.tensor_scalar(s['ra3'][:], bn[:, 0:256], 1e-12, None, ALU.add), r=[kn], w=[k(s['ra3'])])
        yield
        A(lambda e: e.activation(out=s['ra3'][:], in_=s['ra3'][:], func=AF.Sqrt), r=[k(s['ra3'])], w=[k(s['ra3'])])
        yield
        V(lambda e: e.reciprocal(s['ra3'][:], s['ra3'][:]), r=[k(s['ra3'])], w=[k(s['ra3'])])
        yield
        V(lambda e: e.tensor_tensor(s['rkk'][:], s['rkk'][:], s['ra3'][:], ALU.mult), r=[k(s['rkk']), k(s['ra3'])], w=[k(s['rkk'])])
        yield
        for b in range(2):
            V(lambda e, b=b: e.tensor_scalar(s['ra2'][:, b * 128:(b + 1) * 128], s['ra'][:, b * 128:(b + 1) * 128], p['ka'][:, b:b + 1], p['omka'][:, b:b + 1], ALU.mult, ALU.add),
              r=[k(s['ra']), k(p['ka']), k(p['omka'])], w=[k(s['ra2'])])
            yield
        V(lambda e: e.tensor_tensor(s['rkp'][:], kT, s['ra2'][:], ALU.mult), r=[ksh, k(s['ra2'])], w=[k(s['rkp'])])
        yield
        V(lambda e: e.tensor_tensor(s['ra3'][:], s['rkk'][:], s['ra'][:], ALU.mult), r=[k(s['rkk']), k(s['ra'])], w=[k(s['ra3'])])
        yield
        for hh in range(2):
            V(lambda e, hh=hh: e.scalar_tensor_tensor(s['rAm'][:, hh, :], s['rkk'][:], c['nhm'][:, hh:hh + 1], s['recwp'][:], ALU.mult, ALU.mult),
              r=[k(s['rkk']), k(c['nhm']), k(s['recwp'])], w=[k(s['rAm'])])
            yield
            V(lambda e, hh=hh: e.scalar_tensor_tensor(s['rRm'][:, hh, :], rT, c['hm'][:, hh:hh + 1], s['recw'][:], ALU.mult, ALU.mult),
              r=[ksh, k(c['hm']), k(s['recw'])], w=[k(s['rRm'])])
            yield
        G(lambda e: e.tensor_tensor(s['rBt'][:], s['ra3'][:], s['reicw'][:], ALU.mult), r=[k(s['ra3']), k(s['reicw'])], w=[k(s['rBt'])])
        yield
        G(lambda e: e.tensor_tensor(s['rKt'][:], s['rkp'][:], s['reicw'][:], ALU.mult), r=[k(s['rkp']), k(s['reicw'])], w=[k(s['rKt'])])
        yield
        V(lambda e: e.tensor_tensor(s['ra2'][:], rT, s['rkp'][:], ALU.mult), r=[ksh, k(s['rkp'])], w=[k(s['ra2'])])
        yield
        V(lambda e: e.tensor_tensor(s['ra2'][:].rearrange("p (a b) -> p a b", b=128), s['ra2'][:].rearrange("p (a b) -> p a b", b=128), self.bc(p['rk'][:, :], [128, 2, 128], 2), ALU.mult),
          r=[k(s['ra2']), k(p['rk'])], w=[k(s['ra2'])])
        yield
        bb, kb = self.bank('r')
        for b in range(2):
            M(lambda e, b=b: e.matmul(bb[:, b * 2:(b + 1) * 2], lhsT=s['ra2'][:, b * 128:(b + 1) * 128], rhs=c['hm'][:, :], start=True, stop=True),
              r=[k(s['ra2']), k(c['hm'])], w=[kb])
        A(lambda e: e.copy(out=s['rbc'][:], in_=bb[:, 0:4]), r=[kb], w=[k(s['rbc'])])
        yield
        bt, kt = self.bank('r')
        for b in range(2):
            M(lambda e, b=b: e.transpose(bt[:, b * 128:(b + 1) * 128], s['rsh'][:, 4 + b, :], c['identf'][:]), r=[ksh, k(c['identf'])], w=[kt])
        A(lambda e: e.copy(out=s['rvtm'][:], in_=bt[:, 0:256]), r=[kt], w=[k(s['rvtm'])])
        yield
        for src, dst, rk_ in ((lambda b, cc: s['rsh'][:, 4 + b, cc * 64:(cc + 1) * 64], s['rvc'], ksh),
                              (lambda b, cc: s['rBt'][:, b * 128 + cc * 64:b * 128 + (cc + 1) * 64], s['rBc'], k(s['rBt'])),
                              (lambda b, cc: s['rKt'][:, b * 128 + cc * 64:b * 128 + (cc + 1) * 64], s['rKc'], k(s['rKt']))):
            bt, kt = self.bank('r')
            for cc in range(2):
                for b in range(2):
                    M(lambda e, bt=bt, src=src, cc=cc, b=b: e.transpose(bt[0:64, cc * 256 + b * 128:cc * 256 + (b + 1) * 128], src(b, cc), c['identf'][:]),
                      r=[rk_, k(c['identf'])], w=[kt])
            A(lambda e, bt=bt, dst=dst: e.copy(out=dst[:].rearrange("p a b -> p (a b)"), in_=bt[0:64, :]), r=[kt], w=[k(dst)])
            yield
        def amat(lt, lkey, lhh, rt_, rkey, rhh, mask, dst):
            bA, kA = self.bank('r')
            for cc in range(2):
                for h in range(4):
                    b, hh = h // 2, h % 2
                    sl_ = slice(b * 128 + cc * 64, b * 128 + (cc + 1) * 64)
                    la = lt[:, hh, sl_] if lhh else lt[:, sl_]
                    ra_ = rt_[:, hh, sl_] if rhh else rt_[:, sl_]
                    i8 = cc * 4 + h
                    M(lambda e, bA=bA, la=la, ra_=ra_, i8=i8: e.matmul(bA[0:64, i8 * 64:(i8 + 1) * 64], lhsT=la, rhs=ra_, start=True, stop=True), r=[lkey, rkey], w=[kA])
            V(lambda e, bA=bA: e.tensor_tensor(dst[:], h3(bA[0:64, :]), self.bc(mask, [64, 8, 64], 1), ALU.mult), r=[kA, k(c['su'])], w=[k(dst)])
            yield
        kAm, kRm, kBt, kKt = k(s['rAm']), k(s['rRm']), k(s['rBt']), k(s['rKt'])
        yield from amat(s['rAm'], kAm, True, s['rBt'], kBt, False, c['su'][0:64, 0:64], s['rP'])
        yield from amat(s['rBt'], kBt, False, s['rAm'], kAm, True, c['sl'][0:64, 0:64], s['rQ'])
        yield from amat(s['rKt'], kKt, False, s['rAm'], kAm, True, c['sl'][0:64, 0:64], s['rAak'])
        yield from amat(s['rBt'], kBt, False, s['rRm'], kRm, True, c['tri'][0:64, 0:64], s['rArb'])
        yield from amat(s['rKt'], kKt, False, s['rRm'], kRm, True, c['tri'][0:64, 0:64], s['rArk'])
        V(lambda e: e.tensor_tensor(s['rTT'][:], s['rQ'][:], self.bc(c['identf'][0:64, 0:64], [64, 8, 64], 1), ALU.add), r=[k(s['rQ']), k(c['identf'])], w=[k(s['rTT'])])
        yield
        Pc, Qc, Pn, Qn = s['rP'], s['rQ'], s['rP2'], s['rQ2']
        for lvl in range(5):
            bP, kP = self.bank('r')
            for i8 in range(8):
                M(lambda e, bP=bP, i8=i8, Pc=Pc, Qc=Qc: e.matmul(bP[0:64, i8 * 64:(i8 + 1) * 64], lhsT=Qc[:, i8, :], rhs=Pc[:, i8, :], start=True, stop=True),
                  r=[k(Pc), k(Qc)], w=[kP])
            A(lambda e, bP=bP, Pn=Pn: e.copy(out=Pn[:], in_=h3(bP[0:64, :])), r=[kP], w=[k(Pn)])
            yield
            if lvl < 4:
                bQ, kQ = self.bank('r')
                for i8 in range(8):
                    M(lambda e, bQ=bQ, i8=i8, Pc=Pc, Qc=Qc: e.matmul(bQ[0:64, i8 * 64:(i8 + 1) * 64], lhsT=Pc[:, i8, :], rhs=Qc[:, i8, :], start=True, stop=True),
                      r=[k(Pc), k(Qc)], w=[kQ])
                V(lambda e, bQ=bQ, Qn=Qn: e.tensor_copy(Qn[:], h3(bQ[0:64, :])), r=[kQ], w=[k(Qn)])
                yield
            bT, kT_ = self.bank('r')
            for i8 in range(8):
                M(lambda e, bT=bT, i8=i8, Pn=Pn: e.matmul(bT[0:64, i8 * 64:(i8 + 1) * 64], lhsT=Pn[:, i8, :], rhs=s['rTT'][:, i8, :], start=True, stop=True),
                  r=[k(Pn), k(s['rTT'])], w=[kT_])
            V(lambda e, bT=bT: e.tensor_tensor(s['rTT'][:], s['rTT'][:], h3(bT[0:64, :]), ALU.add), r=[k(s['rTT']), kT_], w=[k(s['rTT'])])
            yield
            Pc, Qc, Pn, Qn = Pn, Qn, Pc, Qc
        bG, kG = self.bank('r')
        for cc in range(2):
            for h in range(4):
                i8 = cc * 4 + h
                M(lambda e, cc=cc, h=h, i8=i8: e.matmul(bG[0:64, i8 * 64:(i8 + 1) * 64], lhsT=s['rAak'][:, i8, :], rhs=s['rvc'][:, cc, h * 64:(h + 1) * 64], start=True, stop=True),
                  r=[k(s['rAak']), k(s['rvc'])], w=[kG])
        A(lambda e: e.copy(out=s['rAak'][:], in_=h3(bG[0:64, :])), r=[kG], w=[k(s['rAak'])])
        yield
        ewc = s['recw'][:].rearrange("p (a b) -> p a b", b=64)[:, :, 63]
        for cc in range(2):
            bG1, kG1 = self.bank('r')
            for h in range(4):
                b, hh = h // 2, h % 2
                sl_ = slice(b * 128 + cc * 64, b * 128 + (cc + 1) * 64)
                M(lambda e, bG1=bG1, h=h, b=b, hh=hh, sl_=sl_: e.matmul(bG1[0:64, h * 64:(h + 1) * 64], lhsT=s['rAm'][:, hh, sl_], rhs=s['rST'][:, b, :], start=True, stop=True),
                  r=[kAm, k(s['rST'])], w=[kG1])
            bY1, kY1 = self.bank('r')
            for h in range(4):
                b, hh = h // 2, h % 2
                sl_ = slice(b * 128 + cc * 64, b * 128 + (cc + 1) * 64)
                M(lambda e, bY1=bY1, h=h, b=b, hh=hh, sl_=sl_, cc=cc: e.matmul(bY1[cc * 64:(cc + 1) * 64, h * 64:(h + 1) * 64], lhsT=s['rRm'][:, hh, sl_], rhs=s['rST'][:, b, :], start=True, stop=True),
                  r=[kRm, k(s['rST'])], w=[kY1])
            A(lambda e, bY1=bY1, cc=cc: e.copy(out=s['rY1'][cc * 64:(cc + 1) * 64, :], in_=bY1[cc * 64:(cc + 1) * 64, 0:256]), r=[kY1], w=[k(s['rY1'])])
            yield
            V(lambda e, bG1=bG1, cc=cc: e.tensor_tensor(s['rG'][:], s['rAak'][:, cc * 4:(cc + 1) * 4, :], h3(bG1[0:64, 0:256]), ALU.add), r=[k(s['rAak']), kG1], w=[k(s['rG'])])
            yield
            bU, kU = self.bank('r')
            for h in range(4):
                i8 = cc * 4 + h
                M(lambda e, bU=bU, h=h, i8=i8: e.matmul(bU[0:64, h * 64:(h + 1) * 64], lhsT=s['rTT'][:, i8, :], rhs=s['rG'][:, h, :], start=True, stop=True),
                  r=[k(s['rTT']), k(s['rG'])], w=[kU])
            A(lambda e, bU=bU: e.copy(out=s['rU'][:], in_=h3(bU[0:64, 0:256])), r=[kU], w=[k(s['rU'])])
            yield
            bY2, kY2 = self.bank('r')
            for h in range(4):
                i8 = cc * 4 + h
                M(lambda e, bY2=bY2, h=h, i8=i8, cc=cc: e.matmul(bY2[cc * 64:(cc + 1) * 64, h * 64:(h + 1) * 64], lhsT=s['rArb'][:, i8, :], rhs=s['rU'][:, h, :], start=True, stop=False),
                  r=[k(s['rArb']), k(s['rU'])], w=[kY2])
                M(lambda e, bY2=bY2, h=h, i8=i8, cc=cc: e.matmul(bY2[cc * 64:(cc + 1) * 64, h * 64:(h + 1) * 64], lhsT=s['rArk'][:, i8, :], rhs=s['rvc'][:, cc, h * 64:(h + 1) * 64], start=False, stop=True),
                  r=[k(s['rArk']), k(s['rvc'])], w=[kY2])
            V(lambda e, bY2=bY2, cc=cc: e.tensor_tensor(s['rY'][cc * 64:(cc + 1) * 64, :], s['rY1'][cc * 64:(cc + 1) * 64, :], bY2[cc * 64:(cc + 1) * 64, 0:256], ALU.add),
              r=[k(s['rY1']), kY2], w=[k(s['rY'])])
            yield
            bS, kS = self.bank('r')
            for h in range(4):
                b, hh = h // 2, h % 2
                i8 = cc * 4 + h
                M(lambda e, bS=bS, h=h, b=b, hh=hh, cc=cc: e.matmul(bS[hh * 64:(hh + 1) * 64, b * 64:(b + 1) * 64], lhsT=s['rBc'][:, cc, h * 64:(h + 1) * 64], rhs=s['rU'][:, h, :], start=True, stop=False),
                  r=[k(s['rBc']), k(s['rU'])], w=[kS])
                M(lambda e, bS=bS, h=h, b=b, hh=hh, cc=cc: e.matmul(bS[hh * 64:(hh + 1) * 64, b * 64:(b + 1) * 64], lhsT=s['rKc'][:, cc, h * 64:(h + 1) * 64], rhs=s['rvc'][:, cc, h * 64:(h + 1) * 64], start=False, stop=True),
                  r=[k(s['rKc']), k(s['rvc'])], w=[kS])
            V(lambda e, bS=bS: e.tensor_tensor(s['rt2'][:], s['rST'][:], h3(bS[:, 0:128]), ALU.add), r=[k(s['rST']), kS], w=[k(s['rt2'])])
            yield
            V(lambda e, cc=cc: e.tensor_tensor(s['rST'][:], s['rt2'][:], self.bc(ewc[:, cc::2], [128, 2, 64], 2), ALU.mult), r=[k(s['rt2']), k(s['recw'])], w=[k(s['rST'])])
            yield
        V(lambda e: e.tensor_reduce(s['rm1'][:], h3(s['rY'][:]), AX.X, ALU.add), r=[k(s['rY'])], w=[k(s['rm1'])])
        yield
        A(lambda e: e.activation(out=s['rY1'][:], in_=s['rY'][:], func=AF.Square), r=[k(s['rY'])], w=[k(s['rY1'])])
        yield
        V(lambda e: e.tensor_reduce(s['rm2'][:], h3(s['rY1'][:]), AX.X, ALU.add), r=[k(s['rY1'])], w=[k(s['rm2'])])
        yield
        V(lambda e: e.tensor_scalar(s['rm1'][:], s['rm1'][:], 1.0 / 64, None, ALU.mult), r=[k(s['rm1'])], w=[k(s['rm1'])])
        yield
        V(lambda e: e.tensor_tensor(s['rvar'][:], s['rm1'][:], s['rm1'][:], ALU.mult), r=[k(s['rm1'])], w=[k(s['rvar'])])
        yield
        V(lambda e: e.scalar_tensor_tensor(s['rvar'][:], s['rm2'][:], 1.0 / 64, s['rvar'][:], ALU.mult, ALU.subtract), r=[k(s['rm2']), k(s['rvar'])], w=[k(s['rvar'])])
        yield
        V(lambda e: e.tensor_scalar(s['rvar'][:], s['rvar'][:], GN_EPS, None, ALU.add), r=[k(s['rvar'])], w=[k(s['rvar'])])
        yield
        A(lambda e: e.activation(out=s['rvar'][:], in_=s['rvar'][:], func=AF.Sqrt), r=[k(s['rvar'])], w=[k(s['rvar'])])
        yield
        V(lambda e: e.reciprocal(s['rvar'][:], s['rvar'][:]), r=[k(s['rvar'])], w=[k(s['rvar'])])
        yield
        V(lambda e: e.tensor_tensor(h3(s['rY'][:]), h3(s['rY'][:]), self.bc(s['rm1'][:, :], [128, 4, 64], 2), ALU.subtract), r=[k(s['rY']), k(s['rm1'])], w=[k(s['rY'])])
        yield
        V(lambda e: e.tensor_tensor(h3(s['rY'][:]), h3(s['rY'][:]), self.bc(s['rvar'][:, :], [128, 4, 64], 2), ALU.mult), r=[k(s['rY']), k(s['rvar'])], w=[k(s['rY'])])
        yield
        G(lambda e: e.tensor_tensor(s['rY'][:], s['rY'][:], p['rlg'][:], ALU.mult), r=[k(s['rY']), k(p['rlg'])], w=[k(s['rY'])])
        yield
        G(lambda e: e.tensor_tensor(s['rY'][:], s['rY'][:], p['rlb'][:], ALU.add), r=[k(s['rY']), k(p['rlb'])], w=[k(s['rY'])])
        yield
        V(lambda e: e.tensor_tensor(h3(s['rY1'][:]), h3(s['rvtm'][:]), self.bc(s['rbc'][:, :], [128, 4, 64], 2), ALU.mult), r=[k(s['rvtm']), k(s['rbc'])], w=[k(s['rY1'])])
        yield
        V(lambda e: e.tensor_tensor(s['rY'][:], s['rY'][:], s['rY1'][:], ALU.add), r=[k(s['rY']), k(s['rY1'])], w=[k(s['rY'])])
        yield
        V(lambda e: e.tensor_tensor(s['ycat'][:, 512:768], s['rY'][:], s['rg'][:], ALU.mult), r=[k(s['rY']), k(s['rg'])], w=[k(s['ycat'])])
        yield

    def mixer_epilogue(self, l, i):
        s, p, c = self.s, self.p, self.c
        V, A, G, M = self.V, self.A, self.G, self.M
        k = lambda t: t.key
        bt, kt = self.bank()
        pb = bt[:].bitcast(BF16)
        for kc in range(8):
            M(lambda e, kc=kc: e.transpose(pb[:, kc * 128:(kc + 1) * 128], s['ycat'][:, kc * 128:(kc + 1) * 128], c['identb'][:]), r=[k(s['ycat']), k(c['identb'])], w=[kt])
        V(lambda e: e.tensor_copy(s['yT'][:].rearrange("p a b -> p (a b)"), pb), r=[kt], w=[k(s['yT'])])
        for half in range(2):
            bo, ko = self.bank()
            for kc in range(8):
                M(lambda e, bo=bo, kc=kc, half=half: e.matmul(bo[:, :], lhsT=s['yT'][:, kc, :], rhs=p['w_out'][:, kc, half * 512:(half + 1) * 512], start=(kc == 0), stop=(kc == 7)),
                  r=[k(s['yT']), k(p['w_out'])], w=[ko])
            V(lambda e, bo=bo, half=half: e.scalar_tensor_tensor(s['mix'][:, half * 512:(half + 1) * 512], s['htm'][:, half * 512:(half + 1) * 512], ALPHA, bo[:, :], ALU.mult, ALU.add),
              r=[k(s['htm']), ko], w=[k(s['mix'])])
        self.layernorm(s['mix'], p['l1g'], p['l1b'], s['h1'], s['tmp'])
        tk = "h1d_%d_%d" % (l, i)
        self.store('sp', self.h1_d[i * 128:(i + 1) * 128, :], s['h1'][:], k(s['h1']), dkeys=[tk])
        A(lambda e: e.copy(out=s['h1b'][:], in_=s['h1'][:]), r=[k(s['h1'])], w=[k(s['h1b'])])
        self.store('sp', self.h1b_d[i * 128:(i + 1) * 128, :], s['h1b'][:], k(s['h1b']), dkeys=["h1bd_%d_%d" % (l, i)])
        if hasattr(self, 'xs_d'):
            self.router_tile(l, i)

    def stage0(self):
        s, p, d = self.s, self.p, self.d
        k = lambda t: t.key
        self.load('sp', p['l1g'][:], d['ln_in_g'].ap().partition_broadcast(128), k(p['l1g']))
        self.load('sp', p['l1b'][:], d['ln_in_b'].ap().partition_broadcast(128), k(p['l1b']))
        for i in range(self.NT):
            self.load('sp', s['mix'][:], d['x'][i * 128:(i + 1) * 128, :], k(s['mix']))
            self.layernorm(s['mix'], p['l1g'], p['l1b'], s['htm'], s['tmp'])
            self.store('sp', self.h_d[i * 128:(i + 1) * 128, :], s['htm'][:], k(s['htm']), dkeys=["hd_%d" % i])
            self.to_fm(s['htm'], s['h1b'], s['hT'])
            self.store('sp', self.hT_d[:, :, i * 128:(i + 1) * 128], s['hT'][:], k(s['hT']), dkeys=["hTd_%d" % i])

    def stageM(self, l):
        s = self.s
        k = lambda t: t.key
        for i in range(self.NT):
            self.load('sp', s['hT'][:], self.hT_d[:, :, i * 128:(i + 1) * 128], k(s['hT']), dkeys=["hTd_%d" % i])
            self.load('sp', s['htm'][:], self.h_d[i * 128:(i + 1) * 128, :], k(s['htm']), dkeys=["hd_%d" % i])
            import os
            only = os.environ.get("ONLY", "srg")
            gens = []
            if 'r' in only:
                gens.append(self.rwkv_tile(i == 0))
            if 's' in only:
                gens.append(self.ssd_tile(i == 0))
            if 'g' in only:
                gens.append(self.gla_tile(i == 0))
            if os.environ.get("NOILV"):
                for g_ in gens:
                    for _ in g_:
                        pass
            else:
                while gens:
                    for g_ in list(gens):
                        try:
                            next(g_)
                        except StopIteration:
                            gens.remove(g_)
            if self.debug:
                self.A(lambda e: e.copy(out=s['tmp'][:], in_=s['ycat'][:]), r=[k(s['ycat'])], w=[k(s['tmp'])])
                self.store('sp', self.dbg_y[i * 128:(i + 1) * 128, :], s['tmp'][:], k(s['tmp']))
            self.mixer_epilogue(l, i)

    def build_mixer_test(self):
        self.declare_inputs()
        T = self.T
        self.h_d = self.dscr("h_d", [T, D])
        self.hT_d = self.dscr("hT_d", [128, 8, T], BF16)
        self.h1_d = self.dout("h1_d", [T, D])
        self.h1b_d = self.dscr("h1b_d", [T, D], BF16)
        self.dbg_y = self.dout("dbg_y", [T, D])
        self.consts()
        self.alloc_params()
        self.alloc_mixer()
        print("sbuf peak", self.sb_peak)
        self.stage0()
        self.P.barrier()
        self.load_params(0)
        self.stageM(0)
        self.P.barrier()
        return self.nc

    def alloc_router(self):
        rt = self.rt = {}
        for n, w in (('lg', 36), ('gmx', 1), ('goh', 4), ('gex', 4), ('gsum', 1), ('t32', 32), ('el8', 8), ('el8m', 8), ('l1', 1), ('l2', 1),
                     ('oh1', 8), ('oh2', 8), ('w1', 1), ('w2', 1), ('E1', 32), ('E2', 32), ('Mm', 32), ('rk', 32)):
            rt[n] = self.sb([128, w], name="rt_" + n)

    def alloc_route_persist(self):
        rp = self.rp = {}
        NT = self.NT
        rp['eid'] = self.sb([128, NT * 2], name="rp_eid")
        rp['rnk'] = self.sb([128, NT * 2], name="rp_rnk")
        rp['gat'] = self.sb([128, NT * 2], name="rp_gat")
        rp['cnt'] = self.sb([128, NE], name="rp_cnt")
        rp['iota32'] = self.sb([128, NE], name="rp_iota32")
        ii = self.sb([128, NE], I32, "rp_iota32i")
        self.G(lambda e: e.iota(ii[:], pattern=[[1, NE]], base=0, channel_multiplier=0), w=[ii.key])
        self.V(lambda e: e.tensor_copy(rp['iota32'][:], ii[:]), r=[ii.key], w=[rp['iota32'].key])

    def router_tile(self, l, i):
        s, p, c = self.s, self.p, self.c
        V, A, G, M = self.V, self.A, self.G, self.M
        k = lambda t: t.key
        rt, rp = self.rt, self.rp
        if i == 0:
            G(lambda e: e.memset(rp['cnt'][:], 0.0), w=[k(rp['cnt'])])
        hT32 = s['tmp'][:].rearrange("p (a b) -> p a b", b=128)
        for half in range(2):
            bt, kt = self.bank()
            for j in range(4):
                kc = half * 4 + j
                M(lambda e, bt=bt, j=j, kc=kc: e.transpose(bt[:, j * 128:(j + 1) * 128], s['h1'][:, kc * 128:(kc + 1) * 128], c['identf'][:]), r=[k(s['h1']), k(c['identf'])], w=[kt])
            A(lambda e, bt=bt, half=half: e.copy(out=s['tmp'][:, half * 512:(half + 1) * 512], in_=bt[:, :]), r=[kt], w=[k(s['tmp'])])
        bl, kl = self.bank()
        for kc in range(8):
            M(lambda e, kc=kc: e.matmul(bl[:, 0:36], lhsT=hT32[:, kc, :], rhs=p['wr'][:, kc, :], start=(kc == 0), stop=(kc == 7)), r=[k(s['tmp']), k(p['wr'])], w=[kl])
        V(lambda e: e.tensor_tensor(rt['lg'][:], bl[:, 0:36], p['rb36'][:], ALU.add), r=[kl, k(p['rb36'])], w=[k(rt['lg'])])
        V(lambda e: e.tensor_reduce(rt['gmx'][:], rt['lg'][:, 0:4], AX.X, ALU.max), r=[k(rt['lg'])], w=[k(rt['gmx'])])
        V(lambda e: e.tensor_scalar(rt['goh'][:], rt['lg'][:, 0:4], rt['gmx'][:, 0:1], None, ALU.is_equal), r=[k(rt['lg']), k(rt['gmx'])], w=[k(rt['goh'])])
        V(lambda e: e.tensor_scalar(rt['gex'][:], rt['lg'][:, 0:4], rt['gmx'][:, 0:1], None, ALU.subtract), r=[k(rt['lg']), k(rt['gmx'])], w=[k(rt['gex'])])
        A(lambda e: e.activation(out=rt['gex'][:], in_=rt['gex'][:], func=AF.Exp), r=[k(rt['gex'])], w=[k(rt['gex'])])
        V(lambda e: e.tensor_reduce(rt['gsum'][:], rt['gex'][:], AX.X, ALU.add), r=[k(rt['gex'])], w=[k(rt['gsum'])])
        V(lambda e: e.reciprocal(rt['gsum'][:], rt['gsum'][:]), r=[k(rt['gsum'])], w=[k(rt['gsum'])])
        V(lambda e: e.tensor_tensor(rt['t32'][:].rearrange("p (g j) -> p g j", j=8), rt['lg'][:, 4:36].rearrange("p (g j) -> p g j", j=8),
                                    self.bc(rt['goh'][:, :], [128, 4, 8], 2), ALU.mult), r=[k(rt['lg']), k(rt['goh'])], w=[k(rt['t32'])])
        V(lambda e: e.tensor_reduce(rt['el8'][:], rt['t32'][:].rearrange("p (g j) -> p j g", j=8), AX.X, ALU.add), r=[k(rt['t32'])], w=[k(rt['el8'])])
        V(lambda e: e.tensor_reduce(rt['l1'][:], rt['el8'][:], AX.X, ALU.max), r=[k(rt['el8'])], w=[k(rt['l1'])])
        V(lambda e: e.tensor_scalar(rt['oh1'][:], rt['el8'][:], rt['l1'][:, 0:1], None, ALU.is_equal), r=[k(rt['el8']), k(rt['l1'])], w=[k(rt['oh1'])])
        V(lambda e: e.scalar_tensor_tensor(rt['el8m'][:], rt['oh1'][:], -1e30, rt['el8'][:], ALU.mult, ALU.add), r=[k(rt['oh1']), k(rt['el8'])], w=[k(rt['el8m'])])
        V(lambda e: e.tensor_reduce(rt['l2'][:], rt['el8m'][:], AX.X, ALU.max), r=[k(rt['el8m'])], w=[k(rt['l2'])])
        V(lambda e: e.tensor_scalar(rt['oh2'][:], rt['el8m'][:], rt['l2'][:, 0:1], None, ALU.is_equal), r=[k(rt['el8m']), k(rt['l2'])], w=[k(rt['oh2'])])
        V(lambda e: e.tensor_tensor(rt['w2'][:], rt['l2'][:], rt['l1'][:], ALU.subtract), r=[k(rt['l2']), k(rt['l1'])], w=[k(rt['w2'])])
        A(lambda e: e.activation(out=rt['w2'][:], in_=rt['w2'][:], func=AF.Exp), r=[k(rt['w2'])], w=[k(rt['w2'])])
        V(lambda e: e.tensor_scalar(rt['w1'][:], rt['w2'][:], 1.0, None, ALU.add), r=[k(rt['w2'])], w=[k(rt['w1'])])
        V(lambda e: e.reciprocal(rt['w1'][:], rt['w1'][:]), r=[k(rt['w1'])], w=[k(rt['w1'])])
        V(lambda e: e.tensor_tensor(rt['w2'][:], rt['w2'][:], rt['w1'][:], ALU.mult), r=[k(rt['w2']), k(rt['w1'])], w=[k(rt['w2'])])
        V(lambda e: e.tensor_tensor(rp['gat'][:, 2 * i:2 * i + 1], rt['w1'][:], rt['gsum'][:], ALU.mult), r=[k(rt['w1']), k(rt['gsum'])], w=[k(rp['gat'])])
        V(lambda e: e.tensor_tensor(rp['gat'][:, 2 * i + 1:2 * i + 2], rt['w2'][:], rt['gsum'][:], ALU.mult), r=[k(rt['w2']), k(rt['gsum'])], w=[k(rp['gat'])])
        for E, oh in ((rt['E1'], rt['oh1']), (rt['E2'], rt['oh2'])):
            V(lambda e, E=E, oh=oh: e.tensor_tensor(E[:].rearrange("p (g j) -> p g j", j=8), self.bc(rt['goh'][:, :], [128, 4, 8], 2), self.bc(oh[:, :], [128, 4, 8], 1), ALU.mult),
              r=[k(rt['goh']), k(oh)], w=[k(E)])
        V(lambda e: e.tensor_tensor(rt['Mm'][:], rt['E1'][:], rt['E2'][:], ALU.add), r=[k(rt['E1']), k(rt['E2'])], w=[k(rt['Mm'])])
        br, kr = self.bank()
        M(lambda e: e.matmul(br[:, 0:32], lhsT=c['sl'][:], rhs=rt['Mm'][:], start=True, stop=True), r=[k(c['sl']), k(rt['Mm'])], w=[kr])
        M(lambda e: e.matmul(br[:, 32:64], lhsT=c['onesf'][:], rhs=rt['Mm'][:], start=True, stop=True), r=[k(c['onesf']), k(rt['Mm'])], w=[kr])
        V(lambda e: e.tensor_tensor(rt['rk'][:], br[:, 0:32], rp['cnt'][:], ALU.add), r=[kr, k(rp['cnt'])], w=[k(rt['rk'])])
        V(lambda e: e.tensor_tensor(rp['cnt'][:], rp['cnt'][:], br[:, 32:64], ALU.add), r=[kr, k(rp['cnt'])], w=[k(rp['cnt'])])
        for j, E in ((0, rt['E1']), (1, rt['E2'])):
            V(lambda e, E=E: e.tensor_tensor(rt['t32'][:], E[:], rt['rk'][:], ALU.mult), r=[k(E), k(rt['rk'])], w=[k(rt['t32'])])
            V(lambda e, j=j: e.tensor_reduce(rp['rnk'][:, 2 * i + j:2 * i + j + 1], rt['t32'][:], AX.X, ALU.add), r=[k(rt['t32'])], w=[k(rp['rnk'])])
            V(lambda e, E=E: e.tensor_tensor(rt['t32'][:], E[:], rp['iota32'][:], ALU.mult), r=[k(E), k(rp['iota32'])], w=[k(rt['t32'])])
            V(lambda e, j=j: e.tensor_reduce(rp['eid'][:, 2 * i + j:2 * i + j + 1], rt['t32'][:], AX.X, ALU.add), r=[k(rt['t32'])], w=[k(rp['eid'])])

    def stageMoE(self, l, last):
        d, c, rp = self.d, self.c, self.rp
        V, A, G, M = self.V, self.A, self.G, self.M
        k = lambda t: t.key
        mark = self.sb_off
        sb = self.sb
        NT, NB, RB = self.NT, self.NB, self.RB
        NR = RB // 128
        NC = NT * 2
        thr_i = sb([128, 64], I32, "f_thri")
        thr = sb([128, 64], name="f_thr")
        G(lambda e: e.iota(thr_i[:], pattern=[[RB, 64]], base=0, channel_multiplier=0), w=[k(thr_i)])
        V(lambda e: e.tensor_copy(thr[:], thr_i[:]), r=[k(thr_i)], w=[k(thr)])
        big = sb([128, max(NC * NE, NE * 64, NB * NE)], name="f_big")
        nblk = sb([128, NE], name="f_nblk")
        padded = sb([128, NE], name="f_padded")
        pend = sb([128, NE], name="f_pend")
        pstart = sb([128, NE], name="f_pstart")
        cmp3 = big[:, 0:NE * 64].rearrange("p (e m) -> p e m", m=64)
        V(lambda e: e.tensor_tensor(cmp3, self.bc(rp['cnt'][:, :], [128, NE, 64], 2), self.bc(thr[:, :], [128, NE, 64], 1), ALU.is_gt), r=[k(rp['cnt']), k(thr)], w=[k(big)])
        V(lambda e: e.tensor_reduce(nblk[:], cmp3, AX.X, ALU.add), r=[k(big)], w=[k(nblk)])
        V(lambda e: e.tensor_scalar(padded[:], nblk[:], float(RB), None, ALU.mult), r=[k(nblk)], w=[k(padded)])
        V(lambda e: e.tensor_tensor_scan(pend[:], c['onesf'][:, 0:NE], padded[:], 0.0, ALU.mult, ALU.add), r=[k(c['onesf']), k(padded)], w=[k(pend)])
        V(lambda e: e.tensor_tensor(pstart[:], pend[:], padded[:], ALU.subtract), r=[k(pend), k(padded)], w=[k(pstart)])
        oh3 = big[:, 0:NC * NE].rearrange("p (n e) -> p n e", e=NE)
        destf = sb([128, NC], name="f_destf")
        dest = sb([128, NC], I32, "f_dest")
        V(lambda e: e.tensor_tensor(oh3, self.bc(rp['iota32'][:, :], [128, NC, NE], 1), self.bc(rp['eid'][:, :], [128, NC, NE], 2), ALU.is_equal), r=[k(rp['iota32']), k(rp['eid'])], w=[k(big)])
        V(lambda e: e.tensor_tensor(oh3, oh3, self.bc(pstart[:, :], [128, NC, NE], 1), ALU.mult), r=[k(big), k(pstart)], w=[k(big)])
        V(lambda e: e.tensor_reduce(destf[:], oh3, AX.X, ALU.add), r=[k(big)], w=[k(destf)])
        V(lambda e: e.tensor_tensor(destf[:], destf[:], rp['rnk'][:], ALU.add), r=[k(destf), k(rp['rnk'])], w=[k(destf)])
        V(lambda e: e.tensor_copy(dest[:], destf[:]), r=[k(destf)], w=[k(dest)])
        bs_i = sb([128, NB], I32, "f_bsi")
        bstart = sb([128, NB], name="f_bstart")
        be = sb([128, NB], name="f_be")
        G(lambda e: e.iota(bs_i[:], pattern=[[RB, NB]], base=0, channel_multiplier=0), w=[k(bs_i)])
        V(lambda e: e.tensor_copy(bstart[:], bs_i[:]), r=[k(bs_i)], w=[k(bstart)])
        cmpb = big[:, 0:NB * NE].rearrange("p (b e) -> p b e", e=NE)
        V(lambda e: e.tensor_tensor(cmpb, self.bc(pend[:, :], [128, NB, NE], 1), self.bc(bstart[:, :], [128, NB, NE], 2), ALU.is_le), r=[k(pend), k(bstart)], w=[k(big)])
        V(lambda e: e.tensor_reduce(be[:], cmpb, AX.X, ALU.add), r=[k(big)], w=[k(be)])
        V(lambda e: e.tensor_scalar(be[:], be[:], float(NE - 1), None, ALU.min), r=[k(be)], w=[k(be)])
        kp_i = sb([128, 8], I32, "f_kpi")
        kp = sb([128, 8], name="f_kp")
        G(lambda e: e.iota(kp_i[:], pattern=[[128, 8]], base=0, channel_multiplier=1), w=[k(kp_i)])
        V(lambda e: e.tensor_copy(kp[:], kp_i[:]), r=[k(kp_i)], w=[k(kp)])
        widf = sb([128, NB, 8], name="f_widf")
        wid = sb([128, NB, 8], I32, "f_wid")
        didf = sb([128, NB, 4], name="f_didf")
        did = sb([128, NB, 4], I32, "f_did")
        bew = sb([128, NB], name="f_bew")
        V(lambda e: e.tensor_scalar(bew[:], be[:], float(D), float(l * NE * D), ALU.mult, ALU.add), r=[k(be)], w=[k(bew)])
        V(lambda e: e.tensor_tensor(widf[:], self.bc(bew[:, :], [128, NB, 8], 2), self.bc(kp[:, :], [128, NB, 8], 1), ALU.add), r=[k(bew), k(kp)], w=[k(widf)])
        V(lambda e: e.tensor_copy(wid[:], widf[:]), r=[k(widf)], w=[k(wid)])
        V(lambda e: e.tensor_scalar(bew[:], be[:], float(FF), float(l * NE * FF), ALU.mult, ALU.add), r=[k(be)], w=[k(bew)])
        V(lambda e: e.tensor_tensor(didf[:], self.bc(bew[:, :], [128, NB, 4], 2), self.bc(kp[:, 0:4], [128, NB, 4], 1), ALU.add), r=[k(bew), k(kp)], w=[k(didf)])
        V(lambda e: e.tensor_copy(did[:], didf[:]), r=[k(didf)], w=[k(did)])
        hb = sb([128, D], BF16, "e_hb")
        for i in range(NT):
            self.load('sp', hb[:], self.h1b_d[i * 128:(i + 1) * 128, :], k(hb), dkeys=["h1bd_%d_%d" % (l, i)])
            for j in range(2):
                col = 2 * i + j
                self.P.dma('pool', lambda e, col=col: e.indirect_dma_start(out=self.xs_d.ap(), out_offset=bass.IndirectOffsetOnAxis(ap=dest[:, col:col + 1], axis=0),
                                                                           in_=hb[:], in_offset=None), k(hb), r=[k(hb), k(dest)], w=["xs_d"])
        self.P.barrier()
        import os
        mstop = int(os.environ.get('MOESTOP', '9'))
        if mstop <= 1:
            self.sb_off = mark
            return
        wg = [sb([128, 8, FF], BF16, "e_wg%d" % j) for j in range(2)]
        wu = [sb([128, 8, FF], BF16, "e_wu%d" % j) for j in range(2)]
        wd = [sb([128, 4, D], BF16, "e_wd%d" % j) for j in range(2)]
        xs = [sb([128, NR, D], BF16, "e_xs%d" % j) for j in range(2)]
        xsT = sb([128, 8, RB], BF16, "e_xsT")
        hT = sb([128, 4, RB], BF16, "e_hT")
        sg = sb([128, RB], name="e_sg")
        ys = [sb([128, D], name="e_ys%d" % j) for j in range(2)]
        tg, tu, td = d['moe_w_gate'].ap(), d['moe_w_up'].ap(), d['moe_w_down'].ap()
        nys = 0
        for b in range(NB):
            j = b % 2
            for kc in range(8):
                self.P.dma('pool', lambda e, j=j, b=b, kc=kc: e.indirect_dma_start(out=wg[j][:, kc, :], out_offset=None, in_=tg,
                                                                                  in_offset=bass.IndirectOffsetOnAxis(ap=wid[:, b, kc:kc + 1], axis=0)), k(wg[j]), r=[k(wid)], w=[k(wg[j])])
                self.P.dma('pool', lambda e, j=j, b=b, kc=kc: e.indirect_dma_start(out=wu[j][:, kc, :], out_offset=None, in_=tu,
                                                                                  in_offset=bass.IndirectOffsetOnAxis(ap=wid[:, b, kc:kc + 1], axis=0)), k(wu[j]), r=[k(wid)], w=[k(wu[j])])
            for fc in range(4):
                self.P.dma('pool', lambda e, j=j, b=b, fc=fc: e.indirect_dma_start(out=wd[j][:, fc, :], out_offset=None, in_=td,
                                                                                  in_offset=bass.IndirectOffsetOnAxis(ap=did[:, b, fc:fc + 1], axis=0)), k(wd[j]), r=[k(did)], w=[k(wd[j])])
            self.load('sp', xs[j][:], self.xs_d[b * RB:(b + 1) * RB, :].rearrange("(r p) n -> p r n", p=128), k(xs[j]), dkeys=["xs_d"])
            for r_ in range(NR):
                bt, kt = self.bank()
                pb = bt[:].bitcast(BF16)
                for kc in range(8):
                    M(lambda e, pb=pb, j=j, r_=r_, kc=kc: e.transpose(pb[:, kc * 128:(kc + 1) * 128], xs[j][:, r_, kc * 128:(kc + 1) * 128], c['identb'][:]), r=[k(xs[j]), k(c['identb'])], w=[kt])
                V(lambda e, pb=pb, r_=r_: e.tensor_copy(xsT[:, :, r_ * 128:(r_ + 1) * 128], pb.rearrange("p (a b) -> p a b", b=128)), r=[kt], w=[k(xsT)])
            for fc in range(4):
                bg, kg = self.bank()
                for kc in range(8):
                    M(lambda e, bg=bg, kc=kc, fc=fc, j=j: e.matmul(bg[:, 0:RB], lhsT=wg[j][:, kc, fc * 128:(fc + 1) * 128], rhs=xsT[:, kc, :], start=(kc == 0), stop=(kc == 7)),
                      r=[k(wg[j]), k(xsT)], w=[kg])
                bu, ku = self.bank()
                for kc in range(8):
                    M(lambda e, bu=bu, kc=kc, fc=fc, j=j: e.matmul(bu[:, 0:RB], lhsT=wu[j][:, kc, fc * 128:(fc + 1) * 128], rhs=xsT[:, kc, :], start=(kc == 0), stop=(kc == 7)),
                      r=[k(wu[j]), k(xsT)], w=[ku])
                A(lambda e, bg=bg: e.activation(out=sg[:], in_=bg[:, 0:RB], func=AF.Silu), r=[kg], w=[k(sg)])
                V(lambda e, bu=bu, fc=fc: e.tensor_tensor(hT[:, fc, :], sg[:], bu[:, 0:RB], ALU.mult), r=[k(sg), ku], w=[k(hT)])
            for r_ in range(NR):
                yb = ys[nys % 2]
                nys += 1
                for half in range(2):
                    bo, ko = self.bank()
                    for fc in range(4):
                        M(lambda e, bo=bo, fc=fc, r_=r_, half=half, j=j: e.matmul(bo[:, :], lhsT=hT[:, fc, r_ * 128:(r_ + 1) * 128], rhs=wd[j][:, fc, half * 512:(half + 1) * 512],
                                                                                 start=(fc == 0), stop=(fc == 3)), r=[k(hT), k(wd[j])], w=[ko])
                    if half == 0:
                        A(lambda e, bo=bo, yb=yb: e.copy(out=yb[:, 0:512], in_=bo[:, :]), r=[ko], w=[k(yb)])
                    else:
                        V(lambda e, bo=bo, yb=yb: e.tensor_copy(yb[:, 512:1024], bo[:, :]), r=[ko], w=[k(yb)])
                r0 = b * RB + r_ * 128
                self.store('sp', self.ys_d[r0:r0 + 128, :], yb[:], k(yb), dkeys=["ys_d"])
        self.P.barrier()
        if mstop <= 2:
            self.sb_off = mark
            return
        h1 = sb([128, D], name="c_h1")
        y0 = sb([128, D], name="c_y0")
        y1 = sb([128, D], name="c_y1")
        tmp = sb([128, D], name="c_tmp")
        h2 = sb([128, D], name="c_h2")
        hb2 = sb([128, D], BF16, "c_hb")
        hTo = sb([128, 8, 128], BF16, "c_hTo")
        l2g = sb([128, D], name="c_l2g")
        l2b = sb([128, D], name="c_l2b")
        self.load('sp', l2g[:], d['ln2_g'][l].partition_broadcast(128), k(l2g))
        self.load('sp', l2b[:], d['ln2_b'][l].partition_broadcast(128), k(l2b))
        for i in range(NT):
            self.load('sp', h1[:], self.h1_d[i * 128:(i + 1) * 128, :], k(h1), dkeys=["h1d_%d_%d" % (l, i)])
            for j, yt in ((0, y0), (1, y1)):
                col = 2 * i + j
                self.P.dma('pool', lambda e, col=col, yt=yt: e.indirect_dma_start(out=yt[:], out_offset=None, in_=self.ys_d.ap(),
                                                                                 in_offset=bass.IndirectOffsetOnAxis(ap=dest[:, col:col + 1], axis=0)), k(yt), r=[k(dest), "ys_d"], w=[k(yt)])
            V(lambda e, i=i: e.tensor_scalar(y0[:], y0[:], rp['gat'][:, 2 * i:2 * i + 1], None, ALU.mult), r=[k(y0), k(rp['gat'])], w=[k(y0)])
            V(lambda e, i=i: e.scalar_tensor_tensor(y0[:], y1[:], rp['gat'][:, 2 * i + 1:2 * i + 2], y0[:], ALU.mult, ALU.add), r=[k(y1), k(y0), k(rp['gat'])], w=[k(y0)])
            V(lambda e: e.scalar_tensor_tensor(h1[:], h1[:], ALPHA, y0[:], ALU.mult, ALU.add), r=[k(h1), k(y0)], w=[k(h1)])
            self.layernorm(h1, l2g, l2b, h2, tmp)
            if last:
                self.store('sp', self.out_d[i * 128:(i + 1) * 128, :], h2[:], k(h2), dkeys=["out_%d" % i])
            else:
                self.store('sp', self.h_d[i * 128:(i + 1) * 128, :], h2[:], k(h2), dkeys=["hd_%d" % i])
                self.to_fm(h2, hb2, hTo)
                self.store('sp', self.hT_d[:, :, i * 128:(i + 1) * 128], hTo[:], k(hTo), dkeys=["hTd_%d" % i])
        self.P.barrier()
        self.sb_off = mark

    def build_full(self):
        self.declare_inputs()
        T = self.T
        self.h_d = self.dscr("h_d", [T, D])
        self.hT_d = self.dscr("hT_d", [128, 8, T], BF16)
        self.h1_d = self.dscr("h1_d", [T, D])
        self.h1b_d = self.dscr("h1b_d", [T, D], BF16)
        self.xs_d = self.dscr("xs_d", [self.NB * self.RB, D], BF16)
        self.ys_d = self.dscr("ys_d", [self.NB * self.RB, D])
        self.out_d = self.dout("out", [T, D])
        if self.debug:
            self.dbg_y = self.dout("dbg_y", [T, D])
        self.consts()
        self.alloc_route_persist()
        base = self.sb_off
        for l in range(self.depth):
            self.sb_off = base
            self.alloc_params()
            self.alloc_mixer()
            self.alloc_router()
            if l == 0:
                self.stage0()
                self.P.barrier()
            self.load_params(l)
            self.stageM(l)
            self.P.barrier()
            self.sb_off = base
            self.stageMoE(l, l == self.depth - 1)
        self.P.barrier()
        return self.nc


def _host_inputs(inputs, b, T):
    m = {}
    for k, v in inputs.items():
        v = np.asarray(v)
        if k == 'x':
            m[k] = np.ascontiguousarray(v[b, :T])
        elif k == 'rwkv_r_k':
            m[k] = np.ascontiguousarray(v.reshape(DEPTH, 256))
        elif k in ('moe_w_gate', 'moe_w_up'):
            m[k] = np.ascontiguousarray(v.reshape(DEPTH * NE * D, FF))
        elif k == 'moe_w_down':
            m[k] = np.ascontiguousarray(v.reshape(DEPTH * NE * FF, D))
        else:
            m[k] = np.ascontiguousarray(v)
    return m


def kernel(**inputs):
    x = np.asarray(inputs['x'])
    Bsz, T, _ = x.shape
    bld = Builder(T)
    nc = bld.build_full()
    in_maps = [_host_inputs(inputs, b, T) for b in range(Bsz)]
    res = run_bass_kernel_spmd(nc, in_maps, core_ids=list(range(Bsz)))
    return np.stack([np.asarray(r["out"]) for r in res.results], axis=0).astype(np.float32)
```

```python
import numpy as np
import concourse.bass as bass
import concourse.mybir as mybir
from concourse.bass_utils import run_bass_kernel_spmd

F32 = mybir.dt.float32
BF16 = mybir.dt.bfloat16
I32 = mybir.dt.int32
U32 = mybir.dt.uint32
AF = mybir.ActivationFunctionType
ALU = mybir.AluOpType
AX = mybir.AxisListType

D = 1024
NIN = 2968
DEPTH = 2
ALPHA = (2 * DEPTH) ** 0.25
LN_EPS = 1e-5
RMS_EPS = 1e-6
GN_EPS = 64e-5
NE = 32
FF = 512
O_Z, O_XBC, O_DT, O_RW, O_GQ, O_GK, O_GV, O_GG, O_GA = 0, 512, 1280, 1288, 2184, 2312, 2440, 2696, 2952


class Prog:
    EPOCH = 8192
    NDMA = 40

    def __init__(self, nc):
        self.nc = nc
        self.eng = {'pe': nc.tensor, 'dve': nc.vector, 'act': nc.scalar, 'pool': nc.gpsimd, 'sp': nc.sync}
        self.esems = {n: [] for n in ('pe', 'dve', 'act', 'pool')}
        self.cnt = {n: 0 for n in ('pe', 'dve', 'act', 'pool')}
        self.dsems, self.dval, self.dkey = [], [], {}
        self.waited = {n: {} for n in self.eng}
        self.lastw, self.readers = {}, {}
        self.ninst = 0

    def _esem(self, X, ep):
        while len(self.esems[X]) <= ep:
            self.esems[X].append(self.nc.alloc_semaphore("s_%s_%d" % (X, len(self.esems[X]))))
        return self.esems[X][ep]

    def _deps(self, reads, writes):
        deps = {}

        def add(ev):
            if ev is not None and deps.get(ev[0], 0) < ev[1]:
                deps[ev[0]] = ev[1]
        for r in reads:
            add(self.lastw.get(r))
        for w in writes:
            add(self.lastw.get(w))
            for k, v in self.readers.get(w, {}).items():
                add((k, v))
        return deps

    def _wait(self, X, deps):
        e = self.eng[X]
        for k, v in deps.items():
            if k == X and X == 'pe':
                continue
            if self.waited[X].get(k, 0) >= v:
                continue
            if isinstance(k, str):
                ep = (v - 1) // self.EPOCH
                e.wait_ge(self._esem(k, ep), v - ep * self.EPOCH)
            else:
                v = self.dval[k]
                e.wait_ge(self.dsems[k], v)
            self.waited[X][k] = v
            self.ninst += 1

    def _record(self, ev, reads, writes):
        for r in reads:
            d = self.readers.setdefault(r, {})
            if d.get(ev[0], 0) < ev[1]:
                d[ev[0]] = ev[1]
        for w in writes:
            self.lastw[w] = ev
            self.readers[w] = {}

    def op(self, X, fn, r=(), w=()):
        r = [k for k in r if k is not None]
        w = [k for k in w if k is not None]
        w = w + [k for k in r if isinstance(k, str) and k.startswith('psb') and k not in w]
        self._wait(X, self._deps(r, w))
        inst = fn(self.eng[X])
        self.cnt[X] += 1
        n = self.cnt[X]
        inst.then_inc(self._esem(X, (n - 1) // self.EPOCH), 1)
        self.ninst += 1
        self._record((X, n), r, w)

    def dma(self, X, fn, semkey, r=(), w=()):
        if semkey not in self.dkey:
            i = len(self.dkey) % self.NDMA
            if i >= len(self.dsems):
                self.dsems.append(self.nc.alloc_semaphore("d_%d" % i))
                self.dval.append(0)
            self.dkey[semkey] = i
        i = self.dkey[semkey]
        self._wait(X, self._deps(r, w))
        inst = fn(self.eng[X])
        self.dval[i] += 16
        inst.then_inc(self.dsems[i], 16)
        self.ninst += 1
        self._record((i, self.dval[i]), r, w)

    def barrier(self):
        deps = {k: v for k, v in self.cnt.items() if v > 0}
        for i, v in enumerate(self.dval):
            if v > 0:
                deps[i] = v
        for X in self.eng:
            self._wait(X, dict(deps))


class Tile:
    def __init__(self, t, key):
        self.t, self.key = t, key

    def __getitem__(self, k):
        return self.t[k]


class Builder:
    def __init__(self, T, depth=DEPTH, debug=False, rb=None):
        import os
        rb = rb or int(os.environ.get('RB', '512'))
        self.T, self.depth, self.debug = T, depth, debug
        self.NT = T // 128
        self.RB = rb
        self.NB = (2 * T) // rb + NE
        nc = self.nc = bass.Bass("TRN2", target_bir_lowering=False)
        self.P = Prog(nc)
        self.nsb = 0
        self.sb_off = 16640
        self.sb_peak = 0
        self.sb_cap = 229376
        self.bank_i = 0
        self.chain_i = {}
        self.banks = [nc.alloc_psum_tensor("psb%d" % i, [128, 512], F32) for i in range(8)]
        self.dbg = {}

    def sb(self, shape, dt=F32, name=None):
        self.nsb += 1
        name = "%s_%d" % (name or "t", self.nsb)
        esz = 2 if dt == BF16 else 4
        n = 1
        for v in shape[1:]:
            n *= v
        nbytes = (n * esz + 31) // 32 * 32
        off = self.sb_off
        self.sb_off += nbytes
        assert self.sb_off <= self.sb_cap, "SBUF overflow %d" % self.sb_off
        self.sb_peak = max(self.sb_peak, self.sb_off)
        return Tile(self.nc.alloc_sbuf_tensor_at(name, list(shape), dt, offset=off), name)

    CHAIN_BANKS = {'s': [0, 1], 'r': [2, 3, 4], 'g': [5, 6], 'e': [7]}

    def bank(self, chain=None):
        if chain is None:
            i = self.bank_i
            self.bank_i = (i + 1) % 8
        else:
            lst = self.CHAIN_BANKS[chain]
            j = self.chain_i.get(chain, 0)
            self.chain_i[chain] = (j + 1) % len(lst)
            i = lst[j]
        return self.banks[i], "psb%d" % i

    def V(self, fn, r=(), w=()):
        self.P.op('dve', fn, r, w)

    def A(self, fn, r=(), w=()):
        self.P.op('act', fn, r, w)

    def G(self, fn, r=(), w=()):
        self.P.op('pool', fn, r, w)

    def M(self, fn, r=(), w=()):
        self.P.op('pe', fn, r, w)

    def din(self, name, shape, dt=F32):
        return self.nc.dram_tensor(name, list(shape), dt, kind="ExternalInput")

    def dscr(self, name, shape, dt=F32):
        return self.nc.dram_tensor(name, list(shape), dt, kind="Internal")

    def dout(self, name, shape, dt=F32):
        return self.nc.dram_tensor(name, list(shape), dt, kind="ExternalOutput")

    def load(self, q, out_ap, in_ap, key, dkeys=(), slow=False):
        if slow:
            self.P.dma(q, lambda e: e.dma_start(out=out_ap, in_=in_ap, allow_slow_non_contiguous=True), key, r=list(dkeys), w=[key])
        else:
            self.P.dma(q, lambda e: e.dma_start(out=out_ap, in_=in_ap), key, r=list(dkeys), w=[key])

    def store(self, q, out_ap, in_ap, key, dkeys=()):
        self.P.dma(q, lambda e: e.dma_start(out=out_ap, in_=in_ap), key, r=[key], w=list(dkeys))

    def consts(self):
        nc = self.nc
        c = self.c = {}
        self._ln_st = self.sb([128, 2, 6], name="ln_st")
        self._ln_mv = self.sb([128, 2], name="ln_mv")
        self._ln_rs = self.sb([128, 1], name="ln_rs")
        onesf = c['onesf'] = self.sb([128, 128], name="onesf")
        self.G(lambda e: e.memset(onesf[:], 1.0), w=[onesf.key])
        identf = c['identf'] = self.sb([128, 128], name="identf")
        self.G(lambda e: e.memset(identf[:], 0.0), w=[identf.key])
        self.G(lambda e: e.affine_select(out=identf[:], in_=identf[:], pattern=[[-1, 128]], base=0, channel_multiplier=1,
                                         compare_op=ALU.not_equal, fill=1.0), r=[identf.key], w=[identf.key])
        identb = c['identb'] = self.sb([128, 128], BF16, name="identb")
        self.V(lambda e: e.tensor_copy(identb[:], identf[:]), r=[identf.key], w=[identb.key])
        tri = c['tri'] = self.sb([128, 128], name="tri")
        self.G(lambda e: e.affine_select(out=tri[:], in_=onesf[:], pattern=[[1, 128]], base=0, channel_multiplier=-1,
                                         compare_op=ALU.is_ge, fill=0.0), r=[onesf.key], w=[tri.key])
        su = c['su'] = self.sb([128, 128], name="su")
        self.G(lambda e: e.affine_select(out=su[:], in_=onesf[:], pattern=[[-1, 128]], base=0, channel_multiplier=1,
                                         compare_op=ALU.is_gt, fill=0.0), r=[onesf.key], w=[su.key])
        sl = c['sl'] = self.sb([128, 128], name="sl")
        self.G(lambda e: e.affine_select(out=sl[:], in_=onesf[:], pattern=[[1, 128]], base=0, channel_multiplier=-1,
                                         compare_op=ALU.is_gt, fill=0.0), r=[onesf.key], w=[sl.key])
        maskb = c['maskb'] = self.sb([128, 128], name="maskb")
        self.G(lambda e: e.tensor_copy(maskb[:], tri[:]), r=[tri.key], w=[maskb.key])
        self.G(lambda e: e.memset(maskb[0:64, 64:128], 0.0), w=[maskb.key])
        rmask = c['rmask'] = self.sb([128, 256], name="rmask")
        self.G(lambda e: e.memset(rmask[:], 1.0), w=[rmask.key])
        self.G(lambda e: e.memset(rmask[:].rearrange("p (a b) -> p a b", b=64)[:, :, 0:1], 0.0), w=[rmask.key])
        hm = c['hm'] = self.sb([128, 2], name="hm")
        self.G(lambda e: e.memset(hm[:], 0.0), w=[hm.key])
        self.G(lambda e: e.memset(hm[0:64, 0:1], 1.0), w=[hm.key])
        self.G(lambda e: e.memset(hm[64:128, 1:2], 1.0), w=[hm.key])
        nhm = c['nhm'] = self.sb([128, 2], name="nhm")
        self.V(lambda e: e.tensor_scalar(nhm[:], hm[:], -1.0, None, ALU.mult), r=[hm.key], w=[nhm.key])
        qm = c['qm'] = self.sb([64, 2], name="qm")
        self.G(lambda e: e.memset(qm[:], 0.0), w=[qm.key])
        self.G(lambda e: e.memset(qm[0:32, 0:1], 32.0 ** -0.5), w=[qm.key])
        self.G(lambda e: e.memset(qm[32:64, 1:2], 32.0 ** -0.5), w=[qm.key])
        bones = c['bones'] = self.sb([128, 128], name="bones")
        self.G(lambda e: e.memset(bones[:], 0.0), w=[bones.key])
        self.G(lambda e: e.memset(bones[0:64, 0:64], 1.0), w=[bones.key])
        self.G(lambda e: e.memset(bones[64:128, 64:128], 1.0), w=[bones.key])

    def layernorm(self, xin, gk, bk, out, tmp):
        st, mv, rs = self._ln_st, self._ln_mv, self._ln_rs
        for i in range(2):
            self.V(lambda e, i=i: e.bn_stats(st[:, i, :], xin[:, i * 512:(i + 1) * 512]), r=[xin.key], w=[st.key])
        self.V(lambda e: e.bn_aggr(mv[:], st[:].rearrange("p a b -> p (a b)")), r=[st.key], w=[mv.key])
        self.V(lambda e: e.tensor_scalar(rs[:], mv[:, 1:2], LN_EPS, None, ALU.add), r=[mv.key], w=[rs.key])
        self.A(lambda e: e.activation(out=rs[:], in_=rs[:], func=AF.Sqrt), r=[rs.key], w=[rs.key])
        self.V(lambda e: e.reciprocal(rs[:], rs[:]), r=[rs.key], w=[rs.key])
        self.V(lambda e: e.tensor_scalar(tmp[:], xin[:], mv[:, 0:1], rs[:, 0:1], ALU.subtract, ALU.mult), r=[xin.key, mv.key, rs.key], w=[tmp.key])
        self.G(lambda e: e.tensor_tensor(tmp[:], tmp[:], gk[:], ALU.mult), r=[tmp.key, gk.key], w=[tmp.key])
        self.V(lambda e: e.tensor_tensor(out[:], tmp[:], bk[:], ALU.add), r=[tmp.key, bk.key], w=[out.key])

    def to_fm(self, h_tm, hb, hT):
        c = self.c
        self.A(lambda e: e.copy(out=hb[:], in_=h_tm[:]), r=[h_tm.key], w=[hb.key])
        bk, bkey = self.bank()
        pb = bk[:].bitcast(BF16)
        for kc in range(8):
            self.M(lambda e, kc=kc: e.transpose(pb[:, kc * 128:(kc + 1) * 128], hb[:, kc * 128:(kc + 1) * 128], c['identb'][:]),
                   r=[hb.key, c['identb'].key], w=[bkey])
        self.V(lambda e: e.tensor_copy(hT[:].rearrange("p a b -> p (a b)"), pb), r=[bkey], w=[hT.key])

    def declare_inputs(self):
        L = DEPTH
        d = self.d = {}
        specs = dict(x=[self.T, D], ln_in_g=[D], ln_in_b=[D], w_in=[L, D, NIN], ssd_conv_w=[L, 4, 768], ssd_conv_b=[L, 768],
                     ssd_dt_bias=[L, 8], ssd_a_log=[L, 8], ssd_d=[L, 8], ssd_norm_g=[L, 512], rwkv_mu=[L, 896], rwkv_w0=[L, 256],
                     rwkv_w2=[L, 32, 256], rwkv_a0=[L, 256], rwkv_a2=[L, 32, 256], rwkv_g2=[L, 64, 256], rwkv_k_k=[L, 256],
                     rwkv_k_a=[L, 256], rwkv_r_k=[L, 256], rwkv_ln_g=[L, 256], rwkv_ln_b=[L, 256], gla_w_a2=[L, 16, 128],
                     gla_b_a=[L, 128], gla_norm_g=[L, 256], w_out=[L, D, D], ln1_g=[L, D], ln1_b=[L, D], moe_w_rg=[L, D, 4],
                     moe_b_rg=[L, 4], moe_w_re=[L, D, 32], moe_b_re=[L, 32], moe_w_gate=[L * NE * D, FF], moe_w_up=[L * NE * D, FF],
                     moe_w_down=[L * NE * FF, D], ln2_g=[L, D], ln2_b=[L, D])
        for k, s in specs.items():
            d[k] = self.din(k, s)
        return specs

    def alloc_params(self):
        p = self.p = {}
        sb = self.sb
        p['w_in'] = sb([128, 8, NIN], BF16, "w_in_sb")
        p['w_out'] = sb([128, 8, D], BF16, "w_out_sb")
        p['cw'] = sb([128, 6, 4], name="convw")
        p['cb'] = sb([128, 6], name="convb")
        p['cdiag'] = sb([128, 24, 128], BF16, "cdiag")
        for n, w in (('dtb', 8), ('alog', 8), ('dsk8', 8), ('dsk', 512), ('sng', 512), ('rlg', 256), ('rlb', 256), ('gng', 256),
                     ('l1g', D), ('l1b', D), ('rb36', 36)):
            p[n] = sb([128, w], name="p_" + n)
        for n, w in (('mu', 7), ('omu', 7), ('w0', 2), ('a0', 2), ('kk', 2), ('ka', 2), ('omka', 2), ('rk', 2)):
            p[n] = sb([128, w], name="p_" + n)
        p['ba'] = sb([64, 2], name="p_ba")
        p['w2p'] = sb([128, 256], name="p_w2p")
        p['a2p'] = sb([128, 256], name="p_a2p")
        p['g2p'] = sb([128, 256], name="p_g2p")
        p['wa2'] = sb([32, 128], name="p_wa2")
        p['wr'] = sb([128, 8, 36], name="p_wr")

    def load_params(self, l):
        p, d, c = self.p, self.d, self.c
        q = 'pool'
        win = d['w_in'][l].rearrange("(kc p) n -> p kc n", p=128)
        for kc in range(8):
            for (a, b) in ((0, 1484), (1484, NIN)):
                self.load(q, p['w_in'][:, kc, a:b], win[:, kc, a:b], p['w_in'].key)
        wo = d['w_out'][l].rearrange("(kc p) n -> p kc n", p=128)
        for kc in range(8):
            self.load(q, p['w_out'][:, kc, :], wo[:, kc, :], p['w_out'].key)
        q = 'sp'
        for kk_ in range(4):
            self.load(q, p['cw'][:, :, kk_], d['ssd_conv_w'][l][kk_].rearrange("(cb p) -> p cb", p=128), p['cw'].key, slow=True)
        self.load(q, p['cb'][:], d['ssd_conv_b'][l].rearrange("(cb p) -> p cb", p=128), p['cb'].key, slow=True)
        for n, src in (('dtb', 'ssd_dt_bias'), ('alog', 'ssd_a_log'), ('dsk8', 'ssd_d'), ('sng', 'ssd_norm_g'), ('rlg', 'rwkv_ln_g'),
                       ('rlb', 'rwkv_ln_b'), ('gng', 'gla_norm_g'), ('l1g', 'ln1_g'), ('l1b', 'ln1_b')):
            self.load(q, p[n][:], d[src][l].partition_broadcast(128), p[n].key)
        self.load(q, p['rb36'][:, 0:4], d['moe_b_rg'][l].partition_broadcast(128), p['rb36'].key)
        self.load(q, p['rb36'][:, 4:36], d['moe_b_re'][l].partition_broadcast(128), p['rb36'].key)
        self.load(q, p['mu'][:], d['rwkv_mu'][l].rearrange("(b p) -> p b", p=128), p['mu'].key, slow=True)
        for n, src in (('w0', 'rwkv_w0'), ('a0', 'rwkv_a0'), ('kk', 'rwkv_k_k'), ('ka', 'rwkv_k_a'), ('rk', 'rwkv_r_k')):
            self.load(q, p[n][:], d[src][l].rearrange("(b p) -> p b", p=128), p[n].key, slow=True)
        self.load(q, p['ba'][:], d['gla_b_a'][l].rearrange("(b p) -> p b", p=64), p['ba'].key, slow=True)
        for n in ('w2p', 'a2p', 'g2p'):
            self.G(lambda e, n=n: e.memset(p[n][:], 0.0), w=[p[n].key])
        self.load(q, p['w2p'][0:32, :], d['rwkv_w2'][l], p['w2p'].key)
        self.load(q, p['a2p'][32:64, :], d['rwkv_a2'][l], p['a2p'].key)
        self.load(q, p['g2p'][64:128, :], d['rwkv_g2'][l], p['g2p'].key)
        self.G(lambda e: e.memset(p['wa2'][:], 0.0), w=[p['wa2'].key])
        self.load(q, p['wa2'][16:32, :], d['gla_w_a2'][l], p['wa2'].key)
        self.load(q, p['wr'][:, :, 0:4], d['moe_w_rg'][l].rearrange("(kc p) n -> p kc n", p=128), p['wr'].key, slow=True)
        self.load(q, p['wr'][:, :, 4:36], d['moe_w_re'][l].rearrange("(kc p) n -> p kc n", p=128), p['wr'].key, slow=True)
        self.V(lambda e: e.tensor_scalar(p['omu'][:], p['mu'][:], -1.0, 1.0, ALU.mult, ALU.add), r=[p['mu'].key], w=[p['omu'].key])
        self.V(lambda e: e.tensor_scalar(p['omka'][:], p['ka'][:], -1.0, 1.0, ALU.mult, ALU.add), r=[p['ka'].key], w=[p['omka'].key])
        self.A(lambda e: e.activation(out=p['alog'][:], in_=p['alog'][:], func=AF.Exp), r=[p['alog'].key], w=[p['alog'].key])
        self.V(lambda e: e.tensor_scalar(p['alog'][:], p['alog'][:], -1.0, None, ALU.mult), r=[p['alog'].key], w=[p['alog'].key])
        self.V(lambda e: e.tensor_copy(p['dsk'][:].rearrange("p (h q) -> p h q", q=64), p['dsk8'][:].unsqueeze(2).to_broadcast([128, 8, 64])),
               r=[p['dsk8'].key], w=[p['dsk'].key])
        for cb in range(6):
            for k in range(4):
                self.V(lambda e, cb=cb, k=k: e.tensor_scalar(p['cdiag'][:, cb * 4 + k, :], c['identf'][:], p['cw'][:, cb, k:k + 1], None, ALU.mult),
                       r=[c['identf'].key, p['cw'].key], w=[p['cdiag'].key])

    def alloc_mixer(self):
        s = self.s = {}
        sb = self.sb
        s['hT'] = sb([128, 8, 128], BF16, "m_hT")
        s['htm'] = sb([128, D], name="m_htm")
        s['xbc'] = sb([128, 6, 132], BF16, "m_xbc")
        s['xbB'] = sb([128, 6, 132], BF16, "m_xbB")
        s['xc'] = sb([128, 6, 128], BF16, "m_xc")
        s['xh'] = sb([128, 512], BF16, "m_xh")
        s['xdt'] = sb([128, 512], BF16, "m_xdt")
        s['btm'] = sb([128, 128], BF16, "m_btm")
        s['cm'] = sb([128, 2, 128], BF16, "m_cm")
        s['dt'] = sb([128, 8], name="m_dt")
        s['adt'] = sb([128, 8], name="m_adt")
        s['sp1'] = sb([128, 8], name="m_sp1")
        s['sp2'] = sb([128, 8], name="m_sp2")
        s['R'] = sb([128, 4, 128], name="m_R")
        s['seg'] = sb([128, 8, 128], BF16, "m_seg")
        s['ea'] = sb([128, 8], name="m_ea")
        s['cd'] = sb([128, 4], name="m_cd")
        s['cbm'] = sb([128, 2, 128], BF16, "m_cbm")
        s['toend'] = sb([128, 8], name="m_toend")
        s['S32'] = sb([128, 256], name="m_S32")
        s['Sbf'] = sb([128, 256], BF16, "m_Sbf")
        s['y1'] = sb([128, 512], name="m_y1")
        s['sz'] = sb([128, 512], BF16, "m_sz")
        s['ssq'] = sb([128, 4], name="m_ssq")
        s['ycat'] = sb([128, D], BF16, "m_ycat")
        s['yT'] = sb([128, 8, 128], BF16, "m_yT")
        s['gaT'] = sb([32, 128], name="g_gaT")
        s['gx'] = sb([64, 256], name="g_x")
        s['gt1'] = sb([64, 256], name="g_t1")
        s['gcum'] = sb([64, 256], name="g_cum")
        s['geq'] = sb([64, 256], name="g_eq")
        s['gek'] = sb([64, 256], name="g_ek")
        s['gel'] = sb([64, 4], name="g_el")
        s['gqm'] = sb([64, 2, 256], BF16, "g_qm")
        s['gkT'] = sb([64, 256], BF16, "g_kT")
        s['gktm'] = sb([128, 2, 128], BF16, "g_ktm")
        s['gv'] = sb([128, 256], BF16, "g_v")
        s['gvm'] = sb([128, 2, 256], BF16, "g_vm")
        s['gsm'] = sb([128, 4, 128], BF16, "g_sm")
        s['gS'] = sb([64, 2, 64], name="g_S")
        s['gSb'] = sb([64, 2, 2, 64], BF16, "g_Sb")
        s['gst'] = sb([64, 2, 64], name="g_st")
        s['go'] = sb([128, 256], name="g_o")
        s['gsq'] = sb([128, 256], name="g_sq")
        s['grs'] = sb([128, 4], name="g_rs")
        s['gsg'] = sb([128, 256], name="g_sg")
        s['rw'] = sb([128, 7, 129], name="r_rw")
        s['rsh'] = sb([128, 7, 128], name="r_sh")
        s['rt1'] = sb([128, 7, 128], name="r_t1")
        for n in ('ra1', 'ra2', 'ra3', 'rcw', 'recw', 'reicw', 'recwp', 'ra', 'rkk', 'rkp', 'rKt', 'rBt'):
            s[n] = sb([128, 256], name="r_" + n)
        s['rAm'] = sb([128, 2, 256], name="r_Am")
        s['rRm'] = sb([128, 2, 256], name="r_Rm")
        s['rtw'] = sb([128, 128], name="r_tw")
        s['rsg'] = sb([128, 128], name="r_sg")
        s['rvtm'] = sb([128, 256], name="r_vtm")
        s['rvc'] = sb([64, 2, 256], name="r_vc")
        s['rBc'] = sb([64, 2, 256], name="r_Bc")
        s['rKc'] = sb([64, 2, 256], name="r_Kc")
        s['rP'] = sb([64, 8, 64], name="r_P")
        s['rQ'] = sb([64, 8, 64], name="r_Q")
        s['rP2'] = sb([64, 8, 64], name="r_P2")
        s['rQ2'] = sb([64, 8, 64], name="r_Q2")
        s['rTT'] = sb([64, 8, 64], name="r_TT")
        s['rAak'] = sb([64, 8, 64], name="r_Aak")
        s['rArb'] = sb([64, 8, 64], name="r_Arb")
        s['rArk'] = sb([64, 8, 64], name="r_Ark")
        s['rG'] = sb([64, 4, 64], name="r_G")
        s['rU'] = sb([64, 4, 64], name="r_U")
        s['rST'] = sb([128, 2, 64], name="r_ST")
        s['rt2'] = sb([128, 2, 64], name="r_t2")
        s['rewc'] = sb([128, 4], name="r_ewc")
        s['rY1'] = sb([128, 256], name="r_Y1")
        s['rY'] = sb([128, 256], name="r_Y")
        s['rm1'] = sb([128, 4], name="r_m1")
        s['rm2'] = sb([128, 4], name="r_m2")
        s['rvar'] = sb([128, 4], name="r_var")
        s['rbc'] = sb([128, 4], name="r_bc")
        s['rg'] = sb([128, 256], name="r_g")
        s['mix'] = sb([128, D], name="m_mix")
        s['tmp'] = sb([128, D], name="m_tmp")
        s['h1'] = sb([128, D], name="m_h1")

    def proj_tm(self, out_ap, okey, c0, n):
        s, p = self.s, self.p
        for kc in range(8):
            self.M(lambda e, kc=kc: e.matmul(out_ap, lhsT=s['hT'][:, kc, :], rhs=p['w_in'][:, kc, c0:c0 + n], start=(kc == 0), stop=(kc == 7)),
                   r=[s['hT'].key, p['w_in'].key], w=[okey])

    def proj_fm(self, out_ap, okey, c0, m):
        s, p = self.s, self.p
        for kc in range(8):
            self.M(lambda e, kc=kc: e.matmul(out_ap, lhsT=p['w_in'][:, kc, c0:c0 + m], rhs=s['hT'][:, kc, :], start=(kc == 0), stop=(kc == 7)),
                   r=[s['hT'].key, p['w_in'].key], w=[okey])

    def bc(self, ap, shape, axis):
        return ap.unsqueeze(axis).to_broadcast(list(shape))

    def ssd_tile(self, first):
        s, p, c = self.s, self.p, self.c
        V, A, G, M = self.V, self.A, self.G, self.M
        k = lambda t: t.key
        bz, kz = self.bank('s')
        self.proj_tm(bz[:, :], kz, O_Z, 512)
        A(lambda e: e.activation(out=s['sz'][:], in_=bz[:, :], func=AF.Silu), r=[kz], w=[k(s['sz'])])
        yield
        bd, kd = self.bank('s')
        self.proj_tm(bd[:, 0:8], kd, O_DT, 8)
        V(lambda e: e.tensor_tensor(s['sp1'][:], bd[:, 0:8], p['dtb'][:], ALU.add), r=[kd, k(p['dtb'])], w=[k(s['sp1'])])
        yield
        V(lambda e: e.scalar_tensor_tensor(s['sp2'][:], s['sp1'][:], -1.0, s['sp1'][:], ALU.mult, ALU.max), r=[k(s['sp1'])], w=[k(s['sp2'])])
        yield
        A(lambda e: e.activation(out=s['sp2'][:], in_=s['sp2'][:], func=AF.Exp, scale=-1.0), r=[k(s['sp2'])], w=[k(s['sp2'])])
        yield
        A(lambda e: e.activation(out=s['sp2'][:], in_=s['sp2'][:], func=AF.Ln, bias=1.0), r=[k(s['sp2'])], w=[k(s['sp2'])])
        yield
        V(lambda e: e.scalar_tensor_tensor(s['dt'][:], s['sp1'][:], 0.0, s['sp2'][:], ALU.max, ALU.add), r=[k(s['sp1']), k(s['sp2'])], w=[k(s['dt'])])
        yield
        V(lambda e: e.tensor_tensor(s['adt'][:], s['dt'][:], p['alog'][:], ALU.mult), r=[k(s['dt']), k(p['alog'])], w=[k(s['adt'])])
        yield
        import os
        stop = float(os.environ.get('SSDSTOP', '9'))
        if stop <= 1:
            return
        if first:
            G(lambda e: e.memset(s['xbc'][:, :, 0:4], 0.0), w=[k(s['xbc'])])
            yield
            G(lambda e: e.memset(s['xbB'][:, :, 0:2], 0.0), w=[k(s['xbB'])])
            yield
        else:
            G(lambda e: e.tensor_copy(s['xbc'][:, :, 0:3], s['xbc'][:, :, 128:131]), r=[k(s['xbc'])], w=[k(s['xbc'])])
            yield
            G(lambda e: e.tensor_copy(s['xbB'][:, :, 0:2], s['xbB'][:, :, 128:130]), r=[k(s['xbB'])], w=[k(s['xbB'])])
            yield
        for grp, nb in ((0, 4), (4, 2)):
            bx, kx = self.bank('s')
            for j in range(nb):
                self.proj_fm(bx[:, j * 128:(j + 1) * 128], kx, O_XBC + (grp + j) * 128, 128)
            A(lambda e, bx=bx, grp=grp, nb=nb: e.copy(out=s['xbc'][:, grp:grp + nb, 3:131], in_=bx[:, 0:nb * 128].rearrange("p (a b) -> p a b", b=128)),
              r=[kx], w=[k(s['xbc'])])
            yield
            V(lambda e, bx=bx, grp=grp, nb=nb: e.tensor_copy(s['xbB'][:, grp:grp + nb, 2:130], bx[:, 0:nb * 128].rearrange("p (a b) -> p a b", b=128)),
              r=[kx], w=[k(s['xbB'])])
            yield
        for grp, nb in ((0, 4), (4, 2)):
            bx, kx = self.bank('s')
            for j in range(nb):
                cb = grp + j
                for kk_ in range(4):
                    src = s['xbc'] if kk_ % 2 == 0 else s['xbB']
                    off = kk_ if kk_ % 2 == 0 else kk_ - 1
                    M(lambda e, bx=bx, j=j, cb=cb, kk_=kk_, src=src, off=off: e.matmul(bx[:, j * 128:(j + 1) * 128], lhsT=p['cdiag'][:, cb * 4 + kk_, :],
                                                                                   rhs=src[:, cb, off:off + 128], start=(kk_ == 0), stop=(kk_ == 3)),
                      r=[k(p['cdiag']), k(src)], w=[kx])
            for j in range(nb):
                cb = grp + j
                A(lambda e, bx=bx, j=j, cb=cb: e.activation(out=s['xc'][:, cb, :], in_=bx[:, j * 128:(j + 1) * 128], func=AF.Silu, bias=p['cb'][:, cb:cb + 1]),
                  r=[kx, k(p['cb'])], w=[k(s['xc'])])
                yield
        if stop <= 2:
            return
        bt, kt = self.bank('s')
        pb = bt[:].bitcast(BF16)
        for j in range(5):
            M(lambda e, j=j: e.transpose(pb[:, j * 128:(j + 1) * 128], s['xc'][:, j, :], c['identb'][:]), r=[k(s['xc']), k(c['identb'])], w=[kt])
        V(lambda e: e.tensor_copy(s['xh'][:], pb[:, 0:512]), r=[kt], w=[k(s['xh'])])
        yield
        V(lambda e: e.tensor_copy(s['btm'][:], pb[:, 512:640]), r=[kt], w=[k(s['btm'])])
        yield
        if stop <= 2.2:
            return
        G(lambda e: e.tensor_tensor(s['cm'][:], self.bc(s['xc'][:, 5, :], [128, 2, 128], 1), self.bc(c['hm'][:, :], [128, 2, 128], 2), ALU.mult),
          r=[k(s['xc']), k(c['hm'])], w=[k(s['cm'])])
        yield
        V(lambda e: e.tensor_tensor(s['xdt'][:].rearrange("p (h q) -> p h q", q=64), s['xh'][:].rearrange("p (h q) -> p h q", q=64),
                                    self.bc(s['dt'][:, :], [128, 8, 64], 2), ALU.mult), r=[k(s['xh']), k(s['dt'])], w=[k(s['xdt'])])
        yield
        if stop <= 2.4:
            return
        for half in range(2):
            G(lambda e, half=half: e.tensor_tensor(s['R'][:], self.bc(c['tri'][:, :], [128, 4, 128], 1), self.bc(s['adt'][:, half * 4:(half + 1) * 4], [128, 4, 128], 2), ALU.mult),
              r=[k(c['tri']), k(s['adt'])], w=[k(s['R'])])
            yield
            bD, kD = self.bank('s')
            for q2 in range(2):
                M(lambda e, bD=bD, q2=q2: e.matmul(bD[:, q2 * 256:(q2 + 1) * 256], lhsT=c['su'][:], rhs=s['R'][:, q2 * 2:(q2 + 1) * 2, :].rearrange("p a b -> p (a b)"), start=True, stop=True),
                  r=[k(c['su']), k(s['R'])], w=[kD])
            if stop <= 2.6:
                continue
            A(lambda e, bD=bD, half=half: e.activation(out=s['seg'][:, half * 4:(half + 1) * 4, :].rearrange("p a b -> p (a b)"), in_=bD[:, :], func=AF.Exp),
              r=[kD], w=[k(s['seg'])])
            yield
        if stop <= 2.8:
            return
        V(lambda e: e.tensor_copy(s['toend'][:], s['seg'][:, :, 127]), r=[k(s['seg'])], w=[k(s['toend'])])
        yield
        if stop <= 3:
            return
        be, ke = self.bank('s')
        M(lambda e: e.matmul(be[:, 0:8], lhsT=c['tri'][:], rhs=s['adt'][:], start=True, stop=True), r=[k(c['tri']), k(s['adt'])], w=[ke])
        for g in range(2):
            M(lambda e, g=g: e.matmul(be[g * 64:(g + 1) * 64, 8:12], lhsT=c['onesf'][:, 0:64], rhs=s['adt'][:, g * 4:(g + 1) * 4], start=True, stop=True),
              r=[k(c['onesf']), k(s['adt'])], w=[ke])
        A(lambda e: e.activation(out=s['ea'][:], in_=be[:, 0:8], func=AF.Exp), r=[ke], w=[k(s['ea'])])
        yield
        A(lambda e: e.activation(out=s['cd'][:], in_=be[:, 8:12], func=AF.Exp), r=[ke], w=[k(s['cd'])])
        yield
        if stop <= 4:
            return
        bc_, kc_ = self.bank('s')
        for g in range(2):
            M(lambda e, g=g: e.matmul(bc_[:, g * 128:(g + 1) * 128], lhsT=s['xc'][:, 4, :], rhs=s['cm'][:, g, :], start=True, stop=True),
              r=[k(s['xc']), k(s['cm'])], w=[kc_])
        V(lambda e: e.tensor_tensor(s['cbm'][:], bc_[:, 0:256].rearrange("p (a b) -> p a b", b=128), self.bc(c['tri'][:, :], [128, 2, 128], 1), ALU.mult),
          r=[kc_, k(c['tri'])], w=[k(s['cbm'])])
        yield
        for g in range(2):
            V(lambda e, g=g: e.tensor_tensor(s['seg'][:, g * 4:(g + 1) * 4, :], s['seg'][:, g * 4:(g + 1) * 4, :], self.bc(s['cbm'][:, g, :], [128, 4, 128], 1), ALU.mult),
              r=[k(s['seg']), k(s['cbm'])], w=[k(s['seg'])])
            yield
        by, ky = self.bank('s')
        for h in range(8):
            M(lambda e, h=h: e.matmul(by[:, h * 64:(h + 1) * 64], lhsT=s['seg'][:, h, :], rhs=s['xdt'][:, h * 64:(h + 1) * 64], start=True, stop=True),
              r=[k(s['seg']), k(s['xdt'])], w=[ky])
        bo, ko = self.bank('s')
        if not first:
            for g in range(2):
                M(lambda e, g=g: e.matmul(bo[:, g * 256:(g + 1) * 256], lhsT=s['cm'][:, g, :], rhs=s['Sbf'][:, :], start=True, stop=True),
                  r=[k(s['cm']), k(s['Sbf'])], w=[ko])
            V(lambda e: e.tensor_tensor(s['y1'][:].rearrange("p (h q) -> p h q", q=64), bo[:, :].rearrange("p (h q) -> p h q", q=64),
                                        self.bc(s['ea'][:, :], [128, 8, 64], 2), ALU.mult), r=[ko, k(s['ea'])], w=[k(s['y1'])])
            yield
            V(lambda e: e.tensor_tensor(s['y1'][:], s['y1'][:], by[:, :], ALU.add), r=[k(s['y1']), ky], w=[k(s['y1'])])
            yield
        else:
            V(lambda e: e.tensor_copy(s['y1'][:], by[:, :]), r=[ky], w=[k(s['y1'])])
            yield
        if stop <= 5:
            return
        V(lambda e: e.tensor_tensor(s['xdt'][:].rearrange("p (h q) -> p h q", q=64), s['xdt'][:].rearrange("p (h q) -> p h q", q=64),
                                    self.bc(s['toend'][:, :], [128, 8, 64], 2), ALU.mult), r=[k(s['xdt']), k(s['toend'])], w=[k(s['xdt'])])
        yield
        bs, ks = self.bank('s')
        for g in range(2):
            M(lambda e, g=g: e.matmul(bs[g * 64:(g + 1) * 64, 0:256], lhsT=s['btm'][:, g * 64:(g + 1) * 64], rhs=s['xdt'][:, g * 256:(g + 1) * 256], start=True, stop=True),
              r=[k(s['btm']), k(s['xdt'])], w=[ks])
        if first:
            V(lambda e: e.tensor_copy(s['S32'][:], bs[:, 0:256]), r=[ks], w=[k(s['S32'])])
            yield
        else:
            V(lambda e: e.tensor_tensor(s['S32'][:].rearrange("p (h q) -> p h q", q=64), s['S32'][:].rearrange("p (h q) -> p h q", q=64),
                                        self.bc(s['cd'][:, :], [128, 4, 64], 2), ALU.mult), r=[k(s['S32']), k(s['cd'])], w=[k(s['S32'])])
            yield
            V(lambda e: e.tensor_tensor(s['S32'][:], s['S32'][:], bs[:, 0:256], ALU.add), r=[k(s['S32']), ks], w=[k(s['S32'])])
            yield
        A(lambda e: e.copy(out=s['Sbf'][:], in_=s['S32'][:]), r=[k(s['S32'])], w=[k(s['Sbf'])])
        yield
        G(lambda e: e.tensor_tensor(s['xdt'][:], s['xh'][:], p['dsk'][:], ALU.mult), r=[k(s['xh']), k(p['dsk'])], w=[k(s['xdt'])])
        yield
        V(lambda e: e.tensor_tensor(s['y1'][:], s['y1'][:], s['xdt'][:], ALU.add), r=[k(s['y1']), k(s['xdt'])], w=[k(s['y1'])])
        yield
        V(lambda e: e.tensor_tensor(s['y1'][:], s['y1'][:], s['sz'][:], ALU.mult), r=[k(s['y1']), k(s['sz'])], w=[k(s['y1'])])
        yield
        for g in range(2):
            A(lambda e, g=g: e.activation(out=s['sz'][:, g * 256:(g + 1) * 256], in_=s['y1'][:, g * 256:(g + 1) * 256], func=AF.Square, accum_out=s['ssq'][:, g:g + 1]),
              r=[k(s['y1'])], w=[k(s['sz']), k(s['ssq'])])
            yield
        V(lambda e: e.tensor_scalar(s['ssq'][:, 0:2], s['ssq'][:, 0:2], 1.0 / 256, RMS_EPS, ALU.mult, ALU.add), r=[k(s['ssq'])], w=[k(s['ssq'])])
        yield
        A(lambda e: e.activation(out=s['ssq'][:, 0:2], in_=s['ssq'][:, 0:2], func=AF.Sqrt), r=[k(s['ssq'])], w=[k(s['ssq'])])
        yield
        V(lambda e: e.reciprocal(s['ssq'][:, 0:2], s['ssq'][:, 0:2]), r=[k(s['ssq'])], w=[k(s['ssq'])])
        yield
        for g in range(2):
            V(lambda e, g=g: e.scalar_tensor_tensor(s['ycat'][:, g * 256:(g + 1) * 256], s['y1'][:, g * 256:(g + 1) * 256], s['ssq'][:, g:g + 1],
                                                   p['sng'][:, g * 256:(g + 1) * 256], ALU.mult, ALU.mult), r=[k(s['y1']), k(s['ssq']), k(p['sng'])], w=[k(s['ycat'])])
            yield

    def gla_tile(self, first):
        s, p, c = self.s, self.p, self.c
        V, A, G, M = self.V, self.A, self.G, self.M
        k = lambda t: t.key
        bv, kv = self.bank('g')
        self.proj_tm(bv[:, :], kv, O_GV, 512)
        A(lambda e: e.copy(out=s['gv'][:], in_=bv[:, 0:256]), r=[kv], w=[k(s['gv'])])
        yield
        A(lambda e: e.activation(out=s['gsg'][:], in_=bv[:, 256:512], func=AF.Silu), r=[kv], w=[k(s['gsg'])])
        yield
        G(lambda e: e.tensor_tensor(s['gsg'][:], s['gsg'][:], p['gng'][:], ALU.mult), r=[k(s['gsg']), k(p['gng'])], w=[k(s['gsg'])])
        yield
        ba_, ka_ = self.bank('g')
        self.proj_fm(ba_[0:32, 0:128], ka_, O_GA - 16, 32)
        A(lambda e: e.copy(out=s['gaT'][:], in_=ba_[0:32, 0:128]), r=[ka_], w=[k(s['gaT'])])
        yield
        bx, kx = self.bank('g')
        for pr in range(2):
            M(lambda e, pr=pr: e.matmul(bx[0:64, pr * 128:(pr + 1) * 128], lhsT=p['wa2'][:, pr * 64:(pr + 1) * 64], rhs=s['gaT'][:], start=True, stop=True),
              r=[k(p['wa2']), k(s['gaT'])], w=[kx])
        for pr in range(2):
            A(lambda e, pr=pr: e.activation(out=s['gx'][:, pr * 128:(pr + 1) * 128], in_=bx[0:64, pr * 128:(pr + 1) * 128], func=AF.Identity, bias=p['ba'][:, pr:pr + 1]),
              r=[kx, k(p['ba'])], w=[k(s['gx'])])
            yield
        V(lambda e: e.scalar_tensor_tensor(s['gt1'][:], s['gx'][:], -1.0, s['gx'][:], ALU.mult, ALU.max), r=[k(s['gx'])], w=[k(s['gt1'])])
        yield
        A(lambda e: e.activation(out=s['gt1'][:], in_=s['gt1'][:], func=AF.Exp, scale=-1.0), r=[k(s['gt1'])], w=[k(s['gt1'])])
        yield
        A(lambda e: e.activation(out=s['gt1'][:], in_=s['gt1'][:], func=AF.Ln, bias=1.0), r=[k(s['gt1'])], w=[k(s['gt1'])])
        yield
        V(lambda e: e.scalar_tensor_tensor(s['gx'][:], s['gx'][:], 0.0, s['gt1'][:], ALU.min, ALU.subtract), r=[k(s['gx']), k(s['gt1'])], w=[k(s['gx'])])
        yield
        V(lambda e: e.tensor_tensor_scan(s['gcum'][:], c['rmask'][0:64, :], s['gx'][:], 0.0, ALU.mult, ALU.add), r=[k(c['rmask']), k(s['gx'])], w=[k(s['gcum'])])
        yield
        A(lambda e: e.activation(out=s['geq'][:], in_=s['gcum'][:], func=AF.Exp, scale=1.0 / 16), r=[k(s['gcum'])], w=[k(s['geq'])])
        yield
        A(lambda e: e.activation(out=s['gek'][:], in_=s['gcum'][:], func=AF.Exp, scale=-1.0 / 16), r=[k(s['gcum'])], w=[k(s['gek'])])
        yield
        A(lambda e: e.activation(out=s['gel'][:], in_=s['gcum'][:].rearrange("p (a b) -> p a b", b=64)[:, :, 63], func=AF.Exp, scale=1.0 / 16),
          r=[k(s['gcum'])], w=[k(s['gel'])])
        yield
        bq, kq = self.bank('g')
        for pr in range(2):
            self.proj_fm(bq[0:64, pr * 128:(pr + 1) * 128], kq, O_GQ + pr * 64, 64)
        for pr in range(2):
            self.proj_fm(bq[0:64, 256 + pr * 128:256 + (pr + 1) * 128], kq, O_GK + pr * 64, 64)
        for hh in range(2):
            V(lambda e, hh=hh: e.scalar_tensor_tensor(s['gqm'][:, hh, :], bq[0:64, 0:256], c['qm'][:, hh:hh + 1], s['geq'][:], ALU.mult, ALU.mult),
              r=[kq, k(c['qm']), k(s['geq'])], w=[k(s['gqm'])])
            yield
        V(lambda e: e.tensor_tensor(s['gkT'][:], bq[0:64, 256:512], s['gek'][:], ALU.mult), r=[kq, k(s['gek'])], w=[k(s['gkT'])])
        yield
        bt, kt = self.bank('g')
        pb = bt[:].bitcast(BF16)
        for pr in range(2):
            M(lambda e, pr=pr: e.transpose(pb[:, pr * 64:(pr + 1) * 64], s['gkT'][:, pr * 128:(pr + 1) * 128], c['identb'][0:64, 0:64]),
              r=[k(s['gkT']), k(c['identb'])], w=[kt])
        if first:
            G(lambda e: e.memset(s['gktm'][:], 0.0), w=[k(s['gktm'])])
            yield
        for hh in range(2):
            V(lambda e, hh=hh: e.tensor_copy(s['gktm'][:, hh, :].rearrange("p (a b c) -> p a b c", a=2, b=2)[:, :, hh, :],
                                             pb[:, 0:128].rearrange("p (a b c) -> p a b c", a=2, b=2)[:, :, hh, :]), r=[kt], w=[k(s['gktm'])])
            yield
        G(lambda e: e.tensor_tensor(s['gvm'][:], self.bc(s['gv'][:, :], [128, 2, 256], 1), self.bc(c['hm'][:, :], [128, 2, 256], 2), ALU.mult),
          r=[k(s['gv']), k(c['hm'])], w=[k(s['gvm'])])
        yield
        bs_, ks_ = self.bank('g')
        for h in range(4):
            pr, hh = h // 2, h % 2
            M(lambda e, h=h, pr=pr, hh=hh: e.matmul(bs_[:, h * 128:(h + 1) * 128], lhsT=s['gkT'][:, pr * 128:(pr + 1) * 128], rhs=s['gqm'][:, hh, pr * 128:(pr + 1) * 128],
                                                    start=True, stop=True), r=[k(s['gkT']), k(s['gqm'])], w=[ks_])
        V(lambda e: e.tensor_tensor(s['gsm'][:], bs_[:, :].rearrange("p (a b) -> p a b", b=128), self.bc(c['maskb'][:, :], [128, 4, 128], 1), ALU.mult),
          r=[ks_, k(c['maskb'])], w=[k(s['gsm'])])
        yield
        bu, ku = self.bank('g')
        for cc in range(2):
            for pr in range(2):
                for hh in range(2):
                    h = pr * 2 + hh
                    M(lambda e, cc=cc, h=h, pr=pr, hh=hh: e.matmul(bu[0:64, cc * 128 + pr * 64:cc * 128 + (pr + 1) * 64],
                                                                   lhsT=s['gktm'][:, hh, pr * 64:(pr + 1) * 64], rhs=s['gvm'][:, cc, h * 64:(h + 1) * 64],
                                                                   start=(hh == 0), stop=(hh == 1)), r=[k(s['gktm']), k(s['gvm'])], w=[ku])
        if first:
            G(lambda e: e.memset(s['gS'][:], 0.0), w=[k(s['gS'])])
            yield
        for cc in range(2):
            A(lambda e, cc=cc: e.copy(out=s['gSb'][:, cc, :, :], in_=s['gS'][:]), r=[k(s['gS'])], w=[k(s['gSb'])])
            yield
            V(lambda e, cc=cc: e.tensor_tensor(s['gst'][:], s['gS'][:], bu[0:64, cc * 128:(cc + 1) * 128].rearrange("p (a b) -> p a b", b=64), ALU.add),
              r=[k(s['gS']), ku], w=[k(s['gst'])])
            yield
            V(lambda e, cc=cc: e.tensor_tensor(s['gS'][:], s['gst'][:], self.bc(s['gel'][:, cc::2], [64, 2, 64], 2), ALU.mult),
              r=[k(s['gst']), k(s['gel'])], w=[k(s['gS'])])
            yield
        bo, ko = self.bank('g')
        for h in range(4):
            M(lambda e, h=h: e.matmul(bo[:, h * 64:(h + 1) * 64], lhsT=s['gsm'][:, h, :], rhs=s['gv'][:, h * 64:(h + 1) * 64], start=True, stop=True),
              r=[k(s['gsm']), k(s['gv'])], w=[ko])
        bi, ki = self.bank('g')
        for cc in range(2):
            for h in range(4):
                pr, hh = h // 2, h % 2
                M(lambda e, cc=cc, h=h, pr=pr, hh=hh: e.matmul(bi[cc * 64:(cc + 1) * 64, h * 64:(h + 1) * 64], lhsT=s['gqm'][:, hh, pr * 128 + cc * 64:pr * 128 + (cc + 1) * 64],
                                                               rhs=s['gSb'][:, cc, pr, :], start=True, stop=True), r=[k(s['gqm']), k(s['gSb'])], w=[ki])
        A(lambda e: e.copy(out=s['gsq'][:], in_=bi[:, 0:256]), r=[ki], w=[k(s['gsq'])])
        yield
        V(lambda e: e.tensor_tensor(s['go'][:], s['gsq'][:], bo[:, 0:256], ALU.add), r=[k(s['gsq']), ko], w=[k(s['go'])])
        yield
        A(lambda e: e.activation(out=s['gsq'][:], in_=s['go'][:], func=AF.Square), r=[k(s['go'])], w=[k(s['gsq'])])
        yield
        V(lambda e: e.tensor_reduce(s['grs'][:], s['gsq'][:].rearrange("p (h q) -> p h q", q=64), AX.X, ALU.add), r=[k(s['gsq'])], w=[k(s['grs'])])
        yield
        V(lambda e: e.tensor_scalar(s['grs'][:], s['grs'][:], 1.0 / 64, RMS_EPS, ALU.mult, ALU.add), r=[k(s['grs'])], w=[k(s['grs'])])
        yield
        A(lambda e: e.activation(out=s['grs'][:], in_=s['grs'][:], func=AF.Sqrt), r=[k(s['grs'])], w=[k(s['grs'])])
        yield
        V(lambda e: e.reciprocal(s['grs'][:], s['grs'][:]), r=[k(s['grs'])], w=[k(s['grs'])])
        yield
        V(lambda e: e.tensor_tensor(s['go'][:].rearrange("p (h q) -> p h q", q=64), s['go'][:].rearrange("p (h q) -> p h q", q=64),
                                    self.bc(s['grs'][:, :], [128, 4, 64], 2), ALU.mult), r=[k(s['go']), k(s['grs'])], w=[k(s['go'])])
        yield
        V(lambda e: e.tensor_tensor(s['ycat'][:, 768:1024], s['go'][:], s['gsg'][:], ALU.mult), r=[k(s['go']), k(s['gsg'])], w=[k(s['ycat'])])
        yield

    def rwkv_tile(self, first):
        s, p, c = self.s, self.p, self.c
        V, A, G, M = self.V, self.A, self.G, self.M
        k = lambda t: t.key
        f2 = lambda t, a, b: t[:, a:b, :].rearrange("p a b -> p (a b)")
        h3 = lambda ap: ap.rearrange("p (h q) -> p h q", q=64)
        if first:
            G(lambda e: e.memset(s['rw'][:, :, 0:1], 0.0), w=[k(s['rw'])])
            yield
            G(lambda e: e.memset(s['rST'][:], 0.0), w=[k(s['rST'])])
            yield
        else:
            G(lambda e: e.tensor_copy(s['rw'][:, :, 0:1], s['rw'][:, :, 128:129]), r=[k(s['rw'])], w=[k(s['rw'])])
            yield
        for grp, nb in ((0, 4), (4, 3)):
            bx, kx = self.bank('r')
            for j in range(nb):
                self.proj_fm(bx[:, j * 128:(j + 1) * 128], kx, O_RW + (grp + j) * 128, 128)
            A(lambda e, bx=bx, grp=grp, nb=nb: e.copy(out=s['rw'][:, grp:grp + nb, 1:129], in_=bx[:, 0:nb * 128].rearrange("p (a b) -> p a b", b=128)),
              r=[kx], w=[k(s['rw'])])
            yield
        rt1 = s['rt1'][:]
        G(lambda e: e.tensor_tensor(rt1, s['rw'][:, :, 0:128], self.bc(p['mu'][:, :], [128, 7, 128], 2), ALU.mult), r=[k(s['rw']), k(p['mu'])], w=[k(s['rt1'])])
        yield
        V(lambda e: e.tensor_tensor(s['rsh'][:], s['rw'][:, :, 1:129], self.bc(p['omu'][:, :], [128, 7, 128], 2), ALU.mult), r=[k(s['rw']), k(p['omu'])], w=[k(s['rsh'])])
        yield
        V(lambda e: e.tensor_tensor(s['rsh'][:], s['rsh'][:], rt1, ALU.add), r=[k(s['rsh']), k(s['rt1'])], w=[k(s['rsh'])])
        yield
        rT, kT, vT, lr = f2(s['rsh'], 0, 2), f2(s['rsh'], 2, 4), f2(s['rsh'], 4, 6), s['rsh'][:, 6, :]
        ksh = k(s['rsh'])
        A(lambda e: e.activation(out=s['rtw'][:], in_=lr, func=AF.Tanh), r=[ksh], w=[k(s['rtw'])])
        yield
        bw, kw = self.bank('r')
        for b in range(2):
            M(lambda e, b=b: e.matmul(bw[:, b * 128:(b + 1) * 128], lhsT=p['w2p'][:, b * 128:(b + 1) * 128], rhs=s['rtw'][:], start=True, stop=True),
              r=[k(p['w2p']), k(s['rtw'])], w=[kw])
        for b in range(2):
            A(lambda e, b=b: e.activation(out=s['ra1'][:, b * 128:(b + 1) * 128], in_=bw[:, b * 128:(b + 1) * 128], func=AF.Identity, bias=p['w0'][:, b:b + 1]),
              r=[kw, k(p['w0'])], w=[k(s['ra1'])])
            yield
        V(lambda e: e.scalar_tensor_tensor(s['ra2'][:], s['ra1'][:], -1.0, s['ra1'][:], ALU.mult, ALU.max), r=[k(s['ra1'])], w=[k(s['ra2'])])
        yield
        A(lambda e: e.activation(out=s['ra2'][:], in_=s['ra2'][:], func=AF.Exp, scale=-1.0), r=[k(s['ra2'])], w=[k(s['ra2'])])
        yield
        A(lambda e: e.activation(out=s['ra2'][:], in_=s['ra2'][:], func=AF.Ln, bias=1.0), r=[k(s['ra2'])], w=[k(s['ra2'])])
        yield
        V(lambda e: e.tensor_scalar(s['ra3'][:], s['ra1'][:], -1.0, 0.0, ALU.mult, ALU.max), r=[k(s['ra1'])], w=[k(s['ra3'])])
        yield
        V(lambda e: e.tensor_tensor(s['ra3'][:], s['ra3'][:], s['ra2'][:], ALU.add), r=[k(s['ra3']), k(s['ra2'])], w=[k(s['ra3'])])
        yield
        A(lambda e: e.activation(out=s['ra1'][:], in_=s['ra3'][:], func=AF.Exp, scale=-1.0), r=[k(s['ra3'])], w=[k(s['ra1'])])
        yield
        V(lambda e: e.tensor_scalar(s['ra1'][:], s['ra1'][:], -float(np.exp(-0.5)), None, ALU.mult), r=[k(s['ra1'])], w=[k(s['ra1'])])
        yield
        V(lambda e: e.tensor_tensor_scan(s['rcw'][:], c['rmask'][:], s['ra1'][:], 0.0, ALU.mult, ALU.add), r=[k(c['rmask']), k(s['ra1'])], w=[k(s['rcw'])])
        yield
        V(lambda e: e.tensor_tensor(s['ra2'][:], s['rcw'][:], s['ra1'][:], ALU.subtract), r=[k(s['rcw']), k(s['ra1'])], w=[k(s['ra2'])])
        yield
        A(lambda e: e.activation(out=s['recw'][:], in_=s['rcw'][:], func=AF.Exp), r=[k(s['rcw'])], w=[k(s['recw'])])
        yield
        A(lambda e: e.activation(out=s['reicw'][:], in_=s['rcw'][:], func=AF.Exp, scale=-1.0), r=[k(s['rcw'])], w=[k(s['reicw'])])
        yield
        A(lambda e: e.activation(out=s['recwp'][:], in_=s['ra2'][:], func=AF.Exp), r=[k(s['ra2'])], w=[k(s['recwp'])])
        yield
        ba_, ka_ = self.bank('r')
        for b in range(2):
            M(lambda e, b=b: e.matmul(ba_[:, b * 128:(b + 1) * 128], lhsT=p['a2p'][:, b * 128:(b + 1) * 128], rhs=lr, start=True, stop=True),
              r=[k(p['a2p']), ksh], w=[ka_])
        for b in range(2):
            A(lambda e, b=b: e.activation(out=s['ra'][:, b * 128:(b + 1) * 128], in_=ba_[:, b * 128:(b + 1) * 128], func=AF.Sigmoid, bias=p['a0'][:, b:b + 1]),
              r=[ka_, k(p['a0'])], w=[k(s['ra'])])
            yield
        A(lambda e: e.activation(out=s['rsg'][:], in_=lr, func=AF.Sigmoid), r=[ksh], w=[k(s['rsg'])])
        yield
        bg, kg = self.bank('r')
        M(lambda e: e.matmul(bg[:, 0:256], lhsT=s['rsg'][:], rhs=p['g2p'][:], start=True, stop=True), r=[k(s['rsg']), k(p['g2p'])], w=[kg])
        A(lambda e: e.copy(out=s['rg'][:], in_=bg[:, 0:256]), r=[kg], w=[k(s['rg'])])
        yield
        V(lambda e: e.tensor_tensor(s['rkk'][:].rearrange("p (a b) -> p a b", b=128), kT.rearrange("p (a b) -> p a b", b=128), self.bc(p['kk'][:, :], [128, 2, 128], 2), ALU.mult),
          r=[ksh, k(p['kk'])], w=[k(s['rkk'])])
        yield
        A(lambda e: e.activation(out=s['ra2'][:], in_=s['rkk'][:], func=AF.Square), r=[k(s['rkk'])], w=[k(s['ra2'])])
        yield
        bn, kn = self.bank('r')
        for b in range(2):
            M(lambda e, b=b: e.matmul(bn[:, b * 128:(b + 1) * 128], lhsT=c['bones'][:], rhs=s['ra2'][:, b * 128:(b + 1) * 128], start=True, stop=True),
              r=[k(c['bones']), k(s['ra2'])], w=[kn])
        V(lambda e: e.tensor_scalar(s['ra3'][:], bn[:, 0:256], 1e-12, None, ALU.add), r=[kn], w=[k(s['ra3'])])
        yield
        A(lambda e: e.activation(out=s['ra3'][:], in_=s['ra3'][:], func=AF.Sqrt), r=[k(s['ra3'])], w=[k(s['ra3'])])
        yield
        V(lambda e: e.reciprocal(s['ra3'][:], s['ra3'][:]), r=[k(s['ra3'])], w=[k(s['ra3'])])
        yield
        V(lambda e: e.tensor_tensor(s['rkk'][:], s['rkk'][:], s['ra3'][:], ALU.mult), r=[k(s['rkk']), k(s['ra3'])], w=[k(s['rkk'])])
        yield
        for b in range(2):
            V(lambda e, b=b: e.tensor_scalar(s['ra2'][:, b * 128:(b + 1) * 128], s['ra'][:, b * 128:(b + 1) * 128], p['ka'][:, b:b + 1], p['omka'][:, b:b + 1], ALU.mult, ALU.add),
              r=[k(s['ra']), k(p['ka']), k(p['omka'])], w=[k(s['ra2'])])
            yield
        V(lambda e: e.tensor_tensor(s['rkp'][:], kT, s['ra2'][:], ALU.mult), r=[ksh, k(s['ra2'])], w=[k(s['rkp'])])
        yield
        V(lambda e: e.tensor_tensor(s['ra3'][:], s['rkk'][:], s['ra'][:], ALU.mult), r=[k(s['rkk']), k(s['ra'])], w=[k(s['ra3'])])
        yield
        for hh in range(2):
            V(lambda e, hh=hh: e.scalar_tensor_tensor(s['rAm'][:, hh, :], s['rkk'][:], c['nhm'][:, hh:hh + 1], s['recwp'][:], ALU.mult, ALU.mult),
              r=[k(s['rkk']), k(c['nhm']), k(s['recwp'])], w=[k(s['rAm'])])
            yield
            V(lambda e, hh=hh: e.scalar_tensor_tensor(s['rRm'][:, hh, :], rT, c['hm'][:, hh:hh + 1], s['recw'][:], ALU.mult, ALU.mult),
              r=[ksh, k(c['hm']), k(s['recw'])], w=[k(s['rRm'])])
            yield
        G(lambda e: e.tensor_tensor(s['rBt'][:], s['ra3'][:], s['reicw'][:], ALU.mult), r=[k(s['ra3']), k(s['reicw'])], w=[k(s['rBt'])])
        yield
        G(lambda e: e.tensor_tensor(s['rKt'][:], s['rkp'][:], s['reicw'][:], ALU.mult), r=[k(s['rkp']), k(s['reicw'])], w=[k(s['rKt'])])
        yield
        V(lambda e: e.tensor_tensor(s['ra2'][:], rT, s['rkp'][:], ALU.mult), r=[ksh, k(s['rkp'])], w=[k(s['ra2'])])
        yield
        V(lambda e: e.tensor_tensor(s['ra2'][:].rearrange("p (a b) -> p a b", b=128), s['ra2'][:].rearrange("p (a b) -> p a b", b=128), self.bc(p['rk'][:, :], [128, 2, 128], 2), ALU.mult),
          r=[k(s['ra2']), k(p['rk'])], w=[k(s['ra2'])])
        yield
        bb, kb = self.bank('r')
        for b in range(2):
            M(lambda e, b=b: e.matmul(bb[:, b * 2:(b + 1) * 2], lhsT=s['ra2'][:, b * 128:(b + 1) * 128], rhs=c['hm'][:, :], start=True, stop=True),
              r=[k(s['ra2']), k(c['hm'])], w=[kb])
        A(lambda e: e.copy(out=s['rbc'][:], in_=bb[:, 0:4]), r=[kb], w=[k(s['rbc'])])
        yield
        bt, kt = self.bank('r')
        for b in range(2):
            M(lambda e, b=b: e.transpose(bt[:, b * 128:(b + 1) * 128], s['rsh'][:, 4 + b, :], c['identf'][:]), r=[ksh, k(c['identf'])], w=[kt])
        A(lambda e: e.copy(out=s['rvtm'][:], in_=bt[:, 0:256]), r=[kt], w=[k(s['rvtm'])])
        yield
        for src, dst, rk_ in ((lambda b, cc: s['rsh'][:, 4 + b, cc * 64:(cc + 1) * 64], s['rvc'], ksh),
                              (lambda b, cc: s['rBt'][:, b * 128 + cc * 64:b * 128 + (cc + 1) * 64], s['rBc'], k(s['rBt'])),
                              (lambda b, cc: s['rKt'][:, b * 128 + cc * 64:b * 128 + (cc + 1) * 64], s['rKc'], k(s['rKt']))):
            bt, kt = self.bank('r')
            for cc in range(2):
                for b in range(2):
                    M(lambda e, bt=bt, src=src, cc=cc, b=b: e.transpose(bt[0:64, cc * 256 + b * 128:cc * 256 + (b + 1) * 128], src(b, cc), c['identf'][:]),
                      r=[rk_, k(c['identf'])], w=[kt])
            A(lambda e, bt=bt, dst=dst: e.copy(out=dst[:].rearrange("p a b -> p (a b)"), in_=bt[0:64, :]), r=[kt], w=[k(dst)])
            yield
        def amat(lt, lkey, lhh, rt_, rkey, rhh, mask, dst):
            bA, kA = self.bank('r')
            for cc in range(2):
                for h in range(4):
                    b, hh = h // 2, h % 2
                    sl_ = slice(b * 128 + cc * 64, b * 128 + (cc + 1) * 64)
                    la = lt[:, hh, sl_] if lhh else lt[:, sl_]
                    ra_ = rt_[:, hh, sl_] if rhh else rt_[:, sl_]
                    i8 = cc * 4 + h
                    M(lambda e, bA=bA, la=la, ra_=ra_, i8=i8: e.matmul(bA[0:64, i8 * 64:(i8 + 1) * 64], lhsT=la, rhs=ra_, start=True, stop=True), r=[lkey, rkey], w=[kA])
            V(lambda e, bA=bA: e.tensor_tensor(dst[:], h3(bA[0:64, :]), self.bc(mask, [64, 8, 64], 1), ALU.mult), r=[kA, k(c['su'])], w=[k(dst)])
            yield
        kAm, kRm, kBt, kKt = k(s['rAm']), k(s['rRm']), k(s['rBt']), k(s['rKt'])
        yield from amat(s['rAm'], kAm, True, s['rBt'], kBt, False, c['su'][0:64, 0:64], s['rP'])
        yield from amat(s['rBt'], kBt, False, s['rAm'], kAm, True, c['sl'][0:64, 0:64], s['rQ'])
        yield from amat(s['rKt'], kKt, False, s['rAm'], kAm, True, c['sl'][0:64, 0:64], s['rAak'])
        yield from amat(s['rBt'], kBt, False, s['rRm'], kRm, True, c['tri'][0:64, 0:64], s['rArb'])
        yield from amat(s['rKt'], kKt, False, s['rRm'], kRm, True, c['tri'][0:64, 0:64], s['rArk'])
        V(lambda e: e.tensor_tensor(s['rTT'][:], s['rQ'][:], self.bc(c['identf'][0:64, 0:64], [64, 8, 64], 1), ALU.add), r=[k(s['rQ']), k(c['identf'])], w=[k(s['rTT'])])
        yield
        Pc, Qc, Pn, Qn = s['rP'], s['rQ'], s['rP2'], s['rQ2']
        for lvl in range(5):
            bP, kP = self.bank('r')
            for i8 in range(8):
                M(lambda e, bP=bP, i8=i8, Pc=Pc, Qc=Qc: e.matmul(bP[0:64, i8 * 64:(i8 + 1) * 64], lhsT=Qc[:, i8, :], rhs=Pc[:, i8, :], start=True, stop=True),
                  r=[k(Pc), k(Qc)], w=[kP])
            A(lambda e, bP=bP, Pn=Pn: e.copy(out=Pn[:], in_=h3(bP[0:64, :])), r=[kP], w=[k(Pn)])
            yield
            if lvl < 4:
                bQ, kQ = self.bank('r')
                for i8 in range(8):
                    M(lambda e, bQ=bQ, i8=i8, Pc=Pc, Qc=Qc: e.matmul(bQ[0:64, i8 * 64:(i8 + 1) * 64], lhsT=Pc[:, i8, :], rhs=Qc[:, i8, :], start=True, stop=True),
                      r=[k(Pc), k(Qc)], w=[kQ])
                V(lambda e, bQ=bQ, Qn=Qn: e.tensor_copy(Qn[:], h3(bQ[0:64, :])), r=[kQ], w=[k(Qn)])
                yield
            bT, kT_ = self.bank('r')
            for i8 in range(8):
                M(lambda e, bT=bT, i8=i8, Pn=Pn: e.matmul(bT[0:64, i8 * 64:(i8 + 1) * 64], lhsT=Pn[:, i8, :], rhs=s['rTT'][:, i8, :], start=True, stop=True),
                  r=[k(Pn), k(s['rTT'])], w=[kT_])
            V(lambda e, bT=bT: e.tensor_tensor(s['rTT'][:], s['rTT'][:], h3(bT[0:64, :]), ALU.add), r=[k(s['rTT']), kT_], w=[k(s['rTT'])])
            yield
            Pc, Qc, Pn, Qn = Pn, Qn, Pc, Qc
        bG, kG = self.bank('r')
        for cc in range(2):
            for h in range(4):
                i8 = cc * 4 + h
                M(lambda e, cc=cc, h=h, i8=i8: e.matmul(bG[0:64, i8 * 64:(i8 + 1) * 64], lhsT=s['rAak'][:, i8, :], rhs=s['rvc'][:, cc, h * 64:(h + 1) * 64], start=True, stop=True),
                  r=[k(s['rAak']), k(s['rvc'])], w=[kG])
        A(lambda e: e.copy(out=s['rAak'][:], in_=h3(bG[0:64, :])), r=[kG], w=[k(s['rAak'])])
        yield
        ewc = s['recw'][:].rearrange("p (a b) -> p a b", b=64)[:, :, 63]
        for cc in range(2):
            bG1, kG1 = self.bank('r')
            for h in range(4):
                b, hh = h // 2, h % 2
                sl_ = slice(b * 128 + cc * 64, b * 128 + (cc + 1) * 64)
                M(lambda e, bG1=bG1, h=h, b=b, hh=hh, sl_=sl_: e.matmul(bG1[0:64, h * 64:(h + 1) * 64], lhsT=s['rAm'][:, hh, sl_], rhs=s['rST'][:, b, :], start=True, stop=True),
                  r=[kAm, k(s['rST'])], w=[kG1])
            bY1, kY1 = self.bank('r')
            for h in range(4):
                b, hh = h // 2, h % 2
                sl_ = slice(b * 128 + cc * 64, b * 128 + (cc + 1) * 64)
                M(lambda e, bY1=bY1, h=h, b=b, hh=hh, sl_=sl_, cc=cc: e.matmul(bY1[cc * 64:(cc + 1) * 64, h * 64:(h + 1) * 64], lhsT=s['rRm'][:, hh, sl_], rhs=s['rST'][:, b, :], start=True, stop=True),
                  r=[kRm, k(s['rST'])], w=[kY1])
            A(lambda e, bY1=bY1, cc=cc: e.copy(out=s['rY1'][cc * 64:(cc + 1) * 64, :], in_=bY1[cc * 64:(cc + 1) * 64, 0:256]), r=[kY1], w=[k(s['rY1'])])
            yield
            V(lambda e, bG1=bG1, cc=cc: e.tensor_tensor(s['rG'][:], s['rAak'][:, cc * 4:(cc + 1) * 4, :], h3(bG1[0:64, 0:256]), ALU.add), r=[k(s['rAak']), kG1], w=[k(s['rG'])])
            yield
            bU, kU = self.bank('r')
            for h in range(4):
                i8 = cc * 4 + h
                M(lambda e, bU=bU, h=h, i8=i8: e.matmul(bU[0:64, h * 64:(h + 1) * 64], lhsT=s['rTT'][:, i8, :], rhs=s['rG'][:, h, :], start=True, stop=True),
                  r=[k(s['rTT']), k(s['rG'])], w=[kU])
            A(lambda e, bU=bU: e.copy(out=s['rU'][:], in_=h3(bU[0:64, 0:256])), r=[kU], w=[k(s['rU'])])
            yield
            bY2, kY2 = self.bank('r')
            for h in range(4):
                i8 = cc * 4 + h
                M(lambda e, bY2=bY2, h=h, i8=i8, cc=cc: e.matmul(bY2[cc * 64:(cc + 1) * 64, h * 64:(h + 1) * 64], lhsT=s['rArb'][:, i8, :], rhs=s['rU'][:, h, :], start=True, stop=False),
                  r=[k(s['rArb']), k(s['rU'])], w=[kY2])
                M(lambda e, bY2=bY2, h=h, i8=i8, cc=cc: e.matmul(bY2[cc * 64:(cc + 1) * 64, h * 64:(h + 1) * 64], lhsT=s['rArk'][:, i8, :], rhs=s['rvc'][:, cc, h * 64:(h + 1) * 64], start=False, stop=True),
                  r=[k(s['rArk']), k(s['rvc'])], w=[kY2])
            V(lambda e, bY2=bY2, cc=cc: e.tensor_tensor(s['rY'][cc * 64:(cc + 1) * 64, :], s['rY1'][cc * 64:(cc + 1) * 64, :], bY2[cc * 64:(cc + 1) * 64, 0:256], ALU.add),
              r=[k(s['rY1']), kY2], w=[k(s['rY'])])
            yield
            bS, kS = self.bank('r')
            for h in range(4):
                b, hh = h // 2, h % 2
                i8 = cc * 4 + h
                M(lambda e, bS=bS, h=h, b=b, hh=hh, cc=cc: e.matmul(bS[hh * 64:(hh + 1) * 64, b * 64:(b + 1) * 64], lhsT=s['rBc'][:, cc, h * 64:(h + 1) * 64], rhs=s['rU'][:, h, :], start=True, stop=False),
                  r=[k(s['rBc']), k(s['rU'])], w=[kS])
                M(lambda e, bS=bS, h=h, b=b, hh=hh, cc=cc: e.matmul(bS[hh * 64:(hh + 1) * 64, b * 64:(b + 1) * 64], lhsT=s['rKc'][:, cc, h * 64:(h + 1) * 64], rhs=s['rvc'][:, cc, h * 64:(h + 1) * 64], start=False, stop=True),
                  r=[k(s['rKc']), k(s['rvc'])], w=[kS])
            V(lambda e, bS=bS: e.tensor_tensor(s['rt2'][:], s['rST'][:], h3(bS[:, 0:128]), ALU.add), r=[k(s['rST']), kS], w=[k(s['rt2'])])
            yield
            V(lambda e, cc=cc: e.tensor_tensor(s['rST'][:], s['rt2'][:], self.bc(ewc[:, cc::2], [128, 2, 64], 2), ALU.mult), r=[k(s['rt2']), k(s['recw'])], w=[k(s['rST'])])
            yield
        V(lambda e: e.tensor_reduce(s['rm1'][:], h3(s['rY'][:]), AX.X, ALU.add), r=[k(s['rY'])], w=[k(s['rm1'])])
        yield
        A(lambda e: e.activation(out=s['rY1'][:], in_=s['rY'][:], func=AF.Square), r=[k(s['rY'])], w=[k(s['rY1'])])
        yield
        V(lambda e: e.tensor_reduce(s['rm2'][:], h3(s['rY1'][:]), AX.X, ALU.add), r=[k(s['rY1'])], w=[k(s['rm2'])])
        yield
        V(lambda e: e.tensor_scalar(s['rm1'][:], s['rm1'][:], 1.0 / 64, None, ALU.mult), r=[k(s['rm1'])], w=[k(s['rm1'])])
        yield
        V(lambda e: e.tensor_tensor(s['rvar'][:], s['rm1'][:], s['rm1'][:], ALU.mult), r=[k(s['rm1'])], w=[k(s['rvar'])])
        yield
        V(lambda e: e.scalar_tensor_tensor(s['rvar'][:], s['rm2'][:], 1.0 / 64, s['rvar'][:], ALU.mult, ALU.subtract), r=[k(s['rm2']), k(s['rvar'])], w=[k(s['rvar'])])
        yield
        V(lambda e: e.tensor_scalar(s['rvar'][:], s['rvar'][:], GN_EPS, None, ALU.add), r=[k(s['rvar'])], w=[k(s['rvar'])])
        yield
        A(lambda e: e.activation(out=s['rvar'][:], in_=s['rvar'][:], func=AF.Sqrt), r=[k(s['rvar'])], w=[k(s['rvar'])])
        yield
        V(lambda e: e.reciprocal(s['rvar'][:], s['rvar'][:]), r=[k(s['rvar'])], w=[k(s['rvar'])])
        yield
        V(lambda e: e.tensor_tensor(h3(s['rY'][:]), h3(s['rY'][:]), self.bc(s['rm1'][:, :], [128, 4, 64], 2), ALU.subtract), r=[k(s['rY']), k(s['rm1'])], w=[k(s['rY'])])
        yield
        V(lambda e: e.tensor_tensor(h3(s['rY'][:]), h3(s['rY'][:]), self.bc(s['rvar'][:, :], [128, 4, 64], 2), ALU.mult), r=[k(s['rY']), k(s['rvar'])], w=[k(s['rY'])])
        yield
        G(lambda e: e.tensor_tensor(s['rY'][:], s['rY'][:], p['rlg'][:], ALU.mult), r=[k(s['rY']), k(p['rlg'])], w=[k(s['rY'])])
        yield
        G(lambda e: e.tensor_tensor(s['rY'][:], s['rY'][:], p['rlb'][:], ALU.add), r=[k(s['rY']), k(p['rlb'])], w=[k(s['rY'])])
        yield
        V(lambda e: e.tensor_tensor(h3(s['rY1'][:]), h3(s['rvtm'][:]), self.bc(s['rbc'][:, :], [128, 4, 64], 2), ALU.mult), r=[k(s['rvtm']), k(s['rbc'])], w=[k(s['rY1'])])
        yield
        V(lambda e: e.tensor_tensor(s['rY'][:], s['rY'][:], s['rY1'][:], ALU.add), r=[k(s['rY']), k(s['rY1'])], w=[k(s['rY'])])
        yield
        V(lambda e: e.tensor_tensor(s['ycat'][:, 512:768], s['rY'][:], s['rg'][:], ALU.mult), r=[k(s['rY']), k(s['rg'])], w=[k(s['ycat'])])
        yield

    def mixer_epilogue(self, l, i):
        s, p, c = self.s, self.p, self.c
        V, A, G, M = self.V, self.A, self.G, self.M
        k = lambda t: t.key
        self.load('sp', s['htm'][:], self.h_d[i * 128:(i + 1) * 128, :], k(s['htm']), dkeys=["hd_%d" % i])
        if self.debug:
            self.A(lambda e: e.copy(out=s['tmp'][:], in_=s['ycat'][:]), r=[k(s['ycat'])], w=[k(s['tmp'])])
            yield
            self.store('sp', self.dbg_y[i * 128:(i + 1) * 128, :], s['tmp'][:], k(s['tmp']))
            yield
        bt, kt = self.bank('e')
        pb = bt[:].bitcast(BF16)
        for kc in range(8):
            M(lambda e, kc=kc: e.transpose(pb[:, kc * 128:(kc + 1) * 128], s['ycat'][:, kc * 128:(kc + 1) * 128], c['identb'][:]), r=[k(s['ycat']), k(c['identb'])], w=[kt])
        V(lambda e: e.tensor_copy(s['yT'][:].rearrange("p a b -> p (a b)"), pb), r=[kt], w=[k(s['yT'])])
        yield
        for half in range(2):
            bo, ko = self.bank('e')
            for kc in range(8):
                M(lambda e, bo=bo, kc=kc, half=half: e.matmul(bo[:, :], lhsT=s['yT'][:, kc, :], rhs=p['w_out'][:, kc, half * 512:(half + 1) * 512], start=(kc == 0), stop=(kc == 7)),
                  r=[k(s['yT']), k(p['w_out'])], w=[ko])
            V(lambda e, bo=bo, half=half: e.scalar_tensor_tensor(s['mix'][:, half * 512:(half + 1) * 512], s['htm'][:, half * 512:(half + 1) * 512], ALPHA, bo[:, :], ALU.mult, ALU.add),
              r=[k(s['htm']), ko], w=[k(s['mix'])])
            yield
        self.layernorm(s['mix'], p['l1g'], p['l1b'], s['h1'], s['tmp'])
        yield
        tk = "h1d_%d_%d" % (l, i)
        self.store('sp', self.h1_d[i * 128:(i + 1) * 128, :], s['h1'][:], k(s['h1']), dkeys=[tk])
        yield
        self.store('pool', self.h1b_d[i * 128:(i + 1) * 128, :], s['h1'][:], k(s['h1']), dkeys=["h1bd_%d_%d" % (l, i)])
        yield
        if hasattr(self, 'xs_d'):
            yield from self.router_tile(l, i)

    def stage0(self):
        s, p, d = self.s, self.p, self.d
        k = lambda t: t.key
        self.load('sp', p['l1g'][:], d['ln_in_g'].ap().partition_broadcast(128), k(p['l1g']))
        self.load('sp', p['l1b'][:], d['ln_in_b'].ap().partition_broadcast(128), k(p['l1b']))
        for i in range(self.NT):
            self.load('sp', s['mix'][:], d['x'][i * 128:(i + 1) * 128, :], k(s['mix']))
            self.layernorm(s['mix'], p['l1g'], p['l1b'], s['htm'], s['tmp'])
            self.store('sp', self.h_d[i * 128:(i + 1) * 128, :], s['htm'][:], k(s['htm']), dkeys=["hd_%d" % i])
            self.to_fm(s['htm'], s['ycat'], s['hT'])
            self.store('sp', self.hT_d[:, :, i * 128:(i + 1) * 128], s['hT'][:], k(s['hT']), dkeys=["hTd_%d" % i])

    def stageM(self, l):
        import os
        s = self.s
        k = lambda t: t.key
        only = os.environ.get("ONLY", "srg")
        prev = None
        for i in range(self.NT + 1):
            gens = []
            if i < self.NT:
                self.load('sp', s['hT'][:], self.hT_d[:, :, i * 128:(i + 1) * 128], k(s['hT']), dkeys=["hTd_%d" % i])
                if 'r' in only:
                    gens.append(self.rwkv_tile(i == 0))
                if 's' in only:
                    gens.append(self.ssd_tile(i == 0))
                if 'g' in only:
                    gens.append(self.gla_tile(i == 0))
            if prev is not None:
                gens.append(self.mixer_epilogue(l, prev))
            prev = i if i < self.NT else None
            while gens:
                for g_ in list(gens):
                    try:
                        next(g_)
                    except StopIteration:
                        gens.remove(g_)

    def build_mixer_test(self):
        self.declare_inputs()
        T = self.T
        self.h_d = self.dscr("h_d", [T, D])
        self.hT_d = self.dscr("hT_d", [128, 8, T], BF16)
        self.h1_d = self.dout("h1_d", [T, D])
        self.h1b_d = self.dscr("h1b_d", [T, D], BF16)
        self.dbg_y = self.dout("dbg_y", [T, D])
        self.consts()
        self.alloc_params()
        self.alloc_mixer()
        print("sbuf peak", self.sb_peak)
        self.stage0()
        self.P.barrier()
        self.load_params(0)
        self.stageM(0)
        self.P.barrier()
        return self.nc

    def alloc_router(self):
        rt = self.rt = {}
        for n, w in (('lg', 36), ('gmx', 1), ('goh', 4), ('gex', 4), ('gsum', 1), ('t32', 32), ('el8', 8), ('el8m', 8), ('l1', 1), ('l2', 1),
                     ('oh1', 8), ('oh2', 8), ('w1', 1), ('w2', 1), ('E1', 32), ('E2', 32), ('Mm', 32), ('rk', 32)):
            rt[n] = self.sb([128, w], name="rt_" + n)

    def alloc_route_persist(self):
        rp = self.rp = {}
        NT = self.NT
        rp['eid'] = self.sb([128, NT * 2], name="rp_eid")
        rp['rnk'] = self.sb([128, NT * 2], name="rp_rnk")
        rp['gat'] = self.sb([128, NT * 2], name="rp_gat")
        rp['cnt'] = self.sb([128, NE], name="rp_cnt")
        rp['iota32'] = self.sb([128, NE], name="rp_iota32")
        ii = self.sb([128, NE], I32, "rp_iota32i")
        self.G(lambda e: e.iota(ii[:], pattern=[[1, NE]], base=0, channel_multiplier=0), w=[ii.key])
        self.V(lambda e: e.tensor_copy(rp['iota32'][:], ii[:]), r=[ii.key], w=[rp['iota32'].key])

    def router_tile(self, l, i):
        s, p, c = self.s, self.p, self.c
        V, A, G, M = self.V, self.A, self.G, self.M
        k = lambda t: t.key
        rt, rp = self.rt, self.rp
        if i == 0:
            G(lambda e: e.memset(rp['cnt'][:], 0.0), w=[k(rp['cnt'])])
            yield
        hT32 = s['tmp'][:].rearrange("p (a b) -> p a b", b=128)
        for half in range(2):
            bt, kt = self.bank('e')
            for j in range(4):
                kc = half * 4 + j
                M(lambda e, bt=bt, j=j, kc=kc: e.transpose(bt[:, j * 128:(j + 1) * 128], s['h1'][:, kc * 128:(kc + 1) * 128], c['identf'][:]), r=[k(s['h1']), k(c['identf'])], w=[kt])
            A(lambda e, bt=bt, half=half: e.copy(out=s['tmp'][:, half * 512:(half + 1) * 512], in_=bt[:, :]), r=[kt], w=[k(s['tmp'])])
            yield
        bl, kl = self.bank('e')
        for kc in range(8):
            M(lambda e, kc=kc: e.matmul(bl[:, 0:36], lhsT=hT32[:, kc, :], rhs=p['wr'][:, kc, :], start=(kc == 0), stop=(kc == 7)), r=[k(s['tmp']), k(p['wr'])], w=[kl])
        V(lambda e: e.tensor_tensor(rt['lg'][:], bl[:, 0:36], p['rb36'][:], ALU.add), r=[kl, k(p['rb36'])], w=[k(rt['lg'])])
        yield
        V(lambda e: e.tensor_reduce(rt['gmx'][:], rt['lg'][:, 0:4], AX.X, ALU.max), r=[k(rt['lg'])], w=[k(rt['gmx'])])
        yield
        V(lambda e: e.tensor_scalar(rt['goh'][:], rt['lg'][:, 0:4], rt['gmx'][:, 0:1], None, ALU.is_equal), r=[k(rt['lg']), k(rt['gmx'])], w=[k(rt['goh'])])
        yield
        V(lambda e: e.tensor_scalar(rt['gex'][:], rt['lg'][:, 0:4], rt['gmx'][:, 0:1], None, ALU.subtract), r=[k(rt['lg']), k(rt['gmx'])], w=[k(rt['gex'])])
        yield
        A(lambda e: e.activation(out=rt['gex'][:], in_=rt['gex'][:], func=AF.Exp), r=[k(rt['gex'])], w=[k(rt['gex'])])
        yield
        V(lambda e: e.tensor_reduce(rt['gsum'][:], rt['gex'][:], AX.X, ALU.add), r=[k(rt['gex'])], w=[k(rt['gsum'])])
        yield
        V(lambda e: e.reciprocal(rt['gsum'][:], rt['gsum'][:]), r=[k(rt['gsum'])], w=[k(rt['gsum'])])
        yield
        V(lambda e: e.tensor_tensor(rt['t32'][:].rearrange("p (g j) -> p g j", j=8), rt['lg'][:, 4:36].rearrange("p (g j) -> p g j", j=8),
                                    self.bc(rt['goh'][:, :], [128, 4, 8], 2), ALU.mult), r=[k(rt['lg']), k(rt['goh'])], w=[k(rt['t32'])])
        yield
        V(lambda e: e.tensor_reduce(rt['el8'][:], rt['t32'][:].rearrange("p (g j) -> p j g", j=8), AX.X, ALU.add), r=[k(rt['t32'])], w=[k(rt['el8'])])
        yield
        V(lambda e: e.tensor_reduce(rt['l1'][:], rt['el8'][:], AX.X, ALU.max), r=[k(rt['el8'])], w=[k(rt['l1'])])
        yield
        V(lambda e: e.tensor_scalar(rt['oh1'][:], rt['el8'][:], rt['l1'][:, 0:1], None, ALU.is_equal), r=[k(rt['el8']), k(rt['l1'])], w=[k(rt['oh1'])])
        yield
        V(lambda e: e.scalar_tensor_tensor(rt['el8m'][:], rt['oh1'][:], -1e30, rt['el8'][:], ALU.mult, ALU.add), r=[k(rt['oh1']), k(rt['el8'])], w=[k(rt['el8m'])])
        yield
        V(lambda e: e.tensor_reduce(rt['l2'][:], rt['el8m'][:], AX.X, ALU.max), r=[k(rt['el8m'])], w=[k(rt['l2'])])
        yield
        V(lambda e: e.tensor_scalar(rt['oh2'][:], rt['el8m'][:], rt['l2'][:, 0:1], None, ALU.is_equal), r=[k(rt['el8m']), k(rt['l2'])], w=[k(rt['oh2'])])
        yield
        V(lambda e: e.tensor_tensor(rt['w2'][:], rt['l2'][:], rt['l1'][:], ALU.subtract), r=[k(rt['l2']), k(rt['l1'])], w=[k(rt['w2'])])
        yield
        A(lambda e: e.activation(out=rt['w2'][:], in_=rt['w2'][:], func=AF.Exp), r=[k(rt['w2'])], w=[k(rt['w2'])])
        yield
        V(lambda e: e.tensor_scalar(rt['w1'][:], rt['w2'][:], 1.0, None, ALU.add), r=[k(rt['w2'])], w=[k(rt['w1'])])
        yield
        V(lambda e: e.reciprocal(rt['w1'][:], rt['w1'][:]), r=[k(rt['w1'])], w=[k(rt['w1'])])
        yield
        V(lambda e: e.tensor_tensor(rt['w2'][:], rt['w2'][:], rt['w1'][:], ALU.mult), r=[k(rt['w2']), k(rt['w1'])], w=[k(rt['w2'])])
        yield
        V(lambda e: e.tensor_tensor(rp['gat'][:, 2 * i:2 * i + 1], rt['w1'][:], rt['gsum'][:], ALU.mult), r=[k(rt['w1']), k(rt['gsum'])], w=[k(rp['gat'])])
        yield
        V(lambda e: e.tensor_tensor(rp['gat'][:, 2 * i + 1:2 * i + 2], rt['w2'][:], rt['gsum'][:], ALU.mult), r=[k(rt['w2']), k(rt['gsum'])], w=[k(rp['gat'])])
        yield
        for E, oh in ((rt['E1'], rt['oh1']), (rt['E2'], rt['oh2'])):
            V(lambda e, E=E, oh=oh: e.tensor_tensor(E[:].rearrange("p (g j) -> p g j", j=8), self.bc(rt['goh'][:, :], [128, 4, 8], 2), self.bc(oh[:, :], [128, 4, 8], 1), ALU.mult),
              r=[k(rt['goh']), k(oh)], w=[k(E)])
            yield
        V(lambda e: e.tensor_tensor(rt['Mm'][:], rt['E1'][:], rt['E2'][:], ALU.add), r=[k(rt['E1']), k(rt['E2'])], w=[k(rt['Mm'])])
        yield
        br, kr = self.bank('e')
        M(lambda e: e.matmul(br[:, 0:32], lhsT=c['sl'][:], rhs=rt['Mm'][:], start=True, stop=True), r=[k(c['sl']), k(rt['Mm'])], w=[kr])
        M(lambda e: e.matmul(br[:, 32:64], lhsT=c['onesf'][:], rhs=rt['Mm'][:], start=True, stop=True), r=[k(c['onesf']), k(rt['Mm'])], w=[kr])
        V(lambda e: e.tensor_tensor(rt['rk'][:], br[:, 0:32], rp['cnt'][:], ALU.add), r=[kr, k(rp['cnt'])], w=[k(rt['rk'])])
        yield
        V(lambda e: e.tensor_tensor(rp['cnt'][:], rp['cnt'][:], br[:, 32:64], ALU.add), r=[kr, k(rp['cnt'])], w=[k(rp['cnt'])])
        yield
        for j, E in ((0, rt['E1']), (1, rt['E2'])):
            V(lambda e, E=E: e.tensor_tensor(rt['t32'][:], E[:], rt['rk'][:], ALU.mult), r=[k(E), k(rt['rk'])], w=[k(rt['t32'])])
            yield
            V(lambda e, j=j: e.tensor_reduce(rp['rnk'][:, 2 * i + j:2 * i + j + 1], rt['t32'][:], AX.X, ALU.add), r=[k(rt['t32'])], w=[k(rp['rnk'])])
            yield
            V(lambda e, E=E: e.tensor_tensor(rt['t32'][:], E[:], rp['iota32'][:], ALU.mult), r=[k(E), k(rp['iota32'])], w=[k(rt['t32'])])
            yield
            V(lambda e, j=j: e.tensor_reduce(rp['eid'][:, 2 * i + j:2 * i + j + 1], rt['t32'][:], AX.X, ALU.add), r=[k(rt['t32'])], w=[k(rp['eid'])])
            yield

    def stageMoE(self, l, last):
        d, c, rp = self.d, self.c, self.rp
        V, A, G, M = self.V, self.A, self.G, self.M
        k = lambda t: t.key
        mark = self.sb_off
        sb = self.sb
        NT, NB, RB = self.NT, self.NB, self.RB
        NR = RB // 128
        NC = NT * 2
        thr_i = sb([128, 64], I32, "f_thri")
        thr = sb([128, 64], name="f_thr")
        G(lambda e: e.iota(thr_i[:], pattern=[[RB, 64]], base=0, channel_multiplier=0), w=[k(thr_i)])
        V(lambda e: e.tensor_copy(thr[:], thr_i[:]), r=[k(thr_i)], w=[k(thr)])
        big = sb([128, max(NC * NE, NE * 64, NB * NE)], name="f_big")
        nblk = sb([128, NE], name="f_nblk")
        padded = sb([128, NE], name="f_padded")
        pend = sb([128, NE], name="f_pend")
        pstart = sb([128, NE], name="f_pstart")
        cmp3 = big[:, 0:NE * 64].rearrange("p (e m) -> p e m", m=64)
        V(lambda e: e.tensor_tensor(cmp3, self.bc(rp['cnt'][:, :], [128, NE, 64], 2), self.bc(thr[:, :], [128, NE, 64], 1), ALU.is_gt), r=[k(rp['cnt']), k(thr)], w=[k(big)])
        V(lambda e: e.tensor_reduce(nblk[:], cmp3, AX.X, ALU.add), r=[k(big)], w=[k(nblk)])
        V(lambda e: e.tensor_scalar(padded[:], nblk[:], float(RB), None, ALU.mult), r=[k(nblk)], w=[k(padded)])
        V(lambda e: e.tensor_tensor_scan(pend[:], c['onesf'][:, 0:NE], padded[:], 0.0, ALU.mult, ALU.add), r=[k(c['onesf']), k(padded)], w=[k(pend)])
        V(lambda e: e.tensor_tensor(pstart[:], pend[:], padded[:], ALU.subtract), r=[k(pend), k(padded)], w=[k(pstart)])
        oh3 = big[:, 0:NC * NE].rearrange("p (n e) -> p n e", e=NE)
        destf = sb([128, NC], name="f_destf")
        dest = sb([128, NC], I32, "f_dest")
        V(lambda e: e.tensor_tensor(oh3, self.bc(rp['iota32'][:, :], [128, NC, NE], 1), self.bc(rp['eid'][:, :], [128, NC, NE], 2), ALU.is_equal), r=[k(rp['iota32']), k(rp['eid'])], w=[k(big)])
        V(lambda e: e.tensor_tensor(oh3, oh3, self.bc(pstart[:, :], [128, NC, NE], 1), ALU.mult), r=[k(big), k(pstart)], w=[k(big)])
        V(lambda e: e.tensor_reduce(destf[:], oh3, AX.X, ALU.add), r=[k(big)], w=[k(destf)])
        V(lambda e: e.tensor_tensor(destf[:], destf[:], rp['rnk'][:], ALU.add), r=[k(destf), k(rp['rnk'])], w=[k(destf)])
        V(lambda e: e.tensor_copy(dest[:], destf[:]), r=[k(destf)], w=[k(dest)])
        bs_i = sb([128, NB], I32, "f_bsi")
        bstart = sb([128, NB], name="f_bstart")
        be = sb([128, NB], name="f_be")
        G(lambda e: e.iota(bs_i[:], pattern=[[RB, NB]], base=0, channel_multiplier=0), w=[k(bs_i)])
        V(lambda e: e.tensor_copy(bstart[:], bs_i[:]), r=[k(bs_i)], w=[k(bstart)])
        cmpb = big[:, 0:NB * NE].rearrange("p (b e) -> p b e", e=NE)
        V(lambda e: e.tensor_tensor(cmpb, self.bc(pend[:, :], [128, NB, NE], 1), self.bc(bstart[:, :], [128, NB, NE], 2), ALU.is_le), r=[k(pend), k(bstart)], w=[k(big)])
        V(lambda e: e.tensor_reduce(be[:], cmpb, AX.X, ALU.add), r=[k(big)], w=[k(be)])
        V(lambda e: e.tensor_scalar(be[:], be[:], float(NE - 1), None, ALU.min), r=[k(be)], w=[k(be)])
        kp_i = sb([128, 8], I32, "f_kpi")
        kp = sb([128, 8], name="f_kp")
        G(lambda e: e.iota(kp_i[:], pattern=[[128, 8]], base=0, channel_multiplier=1), w=[k(kp_i)])
        V(lambda e: e.tensor_copy(kp[:], kp_i[:]), r=[k(kp_i)], w=[k(kp)])
        widf = sb([128, NB, 8], name="f_widf")
        wid = sb([128, NB, 8], I32, "f_wid")
        didf = sb([128, NB, 4], name="f_didf")
        did = sb([128, NB, 4], I32, "f_did")
        bew = sb([128, NB], name="f_bew")
        V(lambda e: e.tensor_scalar(bew[:], be[:], float(D), float(l * NE * D), ALU.mult, ALU.add), r=[k(be)], w=[k(bew)])
        V(lambda e: e.tensor_tensor(widf[:], self.bc(bew[:, :], [128, NB, 8], 2), self.bc(kp[:, :], [128, NB, 8], 1), ALU.add), r=[k(bew), k(kp)], w=[k(widf)])
        V(lambda e: e.tensor_copy(wid[:], widf[:]), r=[k(widf)], w=[k(wid)])
        V(lambda e: e.tensor_scalar(bew[:], be[:], float(FF), float(l * NE * FF), ALU.mult, ALU.add), r=[k(be)], w=[k(bew)])
        V(lambda e: e.tensor_tensor(didf[:], self.bc(bew[:, :], [128, NB, 4], 2), self.bc(kp[:, 0:4], [128, NB, 4], 1), ALU.add), r=[k(bew), k(kp)], w=[k(didf)])
        V(lambda e: e.tensor_copy(did[:], didf[:]), r=[k(didf)], w=[k(did)])
        hb = sb([128, D], BF16, "e_hb")
        for i in range(NT):
            self.load('sp', hb[:], self.h1b_d[i * 128:(i + 1) * 128, :], k(hb), dkeys=["h1bd_%d_%d" % (l, i)])
            for j in range(2):
                col = 2 * i + j
                self.P.dma('pool', lambda e, col=col: e.indirect_dma_start(out=self.xs_d.ap(), out_offset=bass.IndirectOffsetOnAxis(ap=dest[:, col:col + 1], axis=0),
                                                                           in_=hb[:], in_offset=None), k(hb), r=[k(hb), k(dest)], w=["xs_d"])
        self.P.barrier()
        import os
        mstop = int(os.environ.get('MOESTOP', '9'))
        if mstop <= 1:
            self.sb_off = mark
            return
        wg = [sb([128, 8, FF], BF16, "e_wg%d" % j) for j in range(2)]
        wu = [sb([128, 8, FF], BF16, "e_wu%d" % j) for j in range(2)]
        wd = [sb([128, 4, D], BF16, "e_wd%d" % j) for j in range(2)]
        xs = [sb([128, NR, D], BF16, "e_xs%d" % j) for j in range(2)]
        xsT = sb([128, 8, RB], BF16, "e_xsT")
        hT = sb([128, 4, RB], BF16, "e_hT")
        sg = sb([128, RB], name="e_sg")
        ys = [sb([128, D], name="e_ys%d" % j) for j in range(2)]
        tg, tu, td = d['moe_w_gate'].ap(), d['moe_w_up'].ap(), d['moe_w_down'].ap()
        nys = 0
        for b in range(NB):
            j = b % 2
            for kc in range(8):
                self.P.dma('pool', lambda e, j=j, b=b, kc=kc: e.indirect_dma_start(out=wg[j][:, kc, :], out_offset=None, in_=tg,
                                                                                  in_offset=bass.IndirectOffsetOnAxis(ap=wid[:, b, kc:kc + 1], axis=0)), k(wg[j]), r=[k(wid)], w=[k(wg[j])])
                self.P.dma('pool', lambda e, j=j, b=b, kc=kc: e.indirect_dma_start(out=wu[j][:, kc, :], out_offset=None, in_=tu,
                                                                                  in_offset=bass.IndirectOffsetOnAxis(ap=wid[:, b, kc:kc + 1], axis=0)), k(wu[j]), r=[k(wid)], w=[k(wu[j])])
            for fc in range(4):
                self.P.dma('pool', lambda e, j=j, b=b, fc=fc: e.indirect_dma_start(out=wd[j][:, fc, :], out_offset=None, in_=td,
                                                                                  in_offset=bass.IndirectOffsetOnAxis(ap=did[:, b, fc:fc + 1], axis=0)), k(wd[j]), r=[k(did)], w=[k(wd[j])])
            self.load('sp', xs[j][:], self.xs_d[b * RB:(b + 1) * RB, :].rearrange("(r p) n -> p r n", p=128), k(xs[j]), dkeys=["xs_d"])
            for r_ in range(NR):
                bt, kt = self.bank()
                pb = bt[:].bitcast(BF16)
                for kc in range(8):
                    M(lambda e, pb=pb, j=j, r_=r_, kc=kc: e.transpose(pb[:, kc * 128:(kc + 1) * 128], xs[j][:, r_, kc * 128:(kc + 1) * 128], c['identb'][:]), r=[k(xs[j]), k(c['identb'])], w=[kt])
                V(lambda e, pb=pb, r_=r_: e.tensor_copy(xsT[:, :, r_ * 128:(r_ + 1) * 128], pb.rearrange("p (a b) -> p a b", b=128)), r=[kt], w=[k(xsT)])
            for fc in range(4):
                bg, kg = self.bank()
                for kc in range(8):
                    M(lambda e, bg=bg, kc=kc, fc=fc, j=j: e.matmul(bg[:, 0:RB], lhsT=wg[j][:, kc, fc * 128:(fc + 1) * 128], rhs=xsT[:, kc, :], start=(kc == 0), stop=(kc == 7)),
                      r=[k(wg[j]), k(xsT)], w=[kg])
                bu, ku = self.bank()
                for kc in range(8):
                    M(lambda e, bu=bu, kc=kc, fc=fc, j=j: e.matmul(bu[:, 0:RB], lhsT=wu[j][:, kc, fc * 128:(fc + 1) * 128], rhs=xsT[:, kc, :], start=(kc == 0), stop=(kc == 7)),
                      r=[k(wu[j]), k(xsT)], w=[ku])
                A(lambda e, bg=bg: e.activation(out=sg[:], in_=bg[:, 0:RB], func=AF.Silu), r=[kg], w=[k(sg)])
                V(lambda e, bu=bu, fc=fc: e.tensor_tensor(hT[:, fc, :], sg[:], bu[:, 0:RB], ALU.mult), r=[k(sg), ku], w=[k(hT)])
            for r_ in range(NR):
                yb = ys[nys % 2]
                nys += 1
                for half in range(2):
                    bo, ko = self.bank()
                    for fc in range(4):
                        M(lambda e, bo=bo, fc=fc, r_=r_, half=half, j=j: e.matmul(bo[:, :], lhsT=hT[:, fc, r_ * 128:(r_ + 1) * 128], rhs=wd[j][:, fc, half * 512:(half + 1) * 512],
                                                                                 start=(fc == 0), stop=(fc == 3)), r=[k(hT), k(wd[j])], w=[ko])
                    if half == 0:
                        A(lambda e, bo=bo, yb=yb: e.copy(out=yb[:, 0:512], in_=bo[:, :]), r=[ko], w=[k(yb)])
                    else:
                        V(lambda e, bo=bo, yb=yb: e.tensor_copy(yb[:, 512:1024], bo[:, :]), r=[ko], w=[k(yb)])
                r0 = b * RB + r_ * 128
                self.store('sp', self.ys_d[r0:r0 + 128, :], yb[:], k(yb), dkeys=["ys_d"])
        self.P.barrier()
        if mstop <= 2:
            self.sb_off = mark
            return
        h1 = sb([128, D], name="c_h1")
        y0 = sb([128, D], name="c_y0")
        y1 = sb([128, D], name="c_y1")
        tmp = sb([128, D], name="c_tmp")
        h2 = sb([128, D], name="c_h2")
        hb2 = sb([128, D], BF16, "c_hb")
        hTo = sb([128, 8, 128], BF16, "c_hTo")
        l2g = sb([128, D], name="c_l2g")
        l2b = sb([128, D], name="c_l2b")
        self.load('sp', l2g[:], d['ln2_g'][l].partition_broadcast(128), k(l2g))
        self.load('sp', l2b[:], d['ln2_b'][l].partition_broadcast(128), k(l2b))
        for i in range(NT):
            self.load('sp', h1[:], self.h1_d[i * 128:(i + 1) * 128, :], k(h1), dkeys=["h1d_%d_%d" % (l, i)])
            for j, yt in ((0, y0), (1, y1)):
                col = 2 * i + j
                self.P.dma('pool', lambda e, col=col, yt=yt: e.indirect_dma_start(out=yt[:], out_offset=None, in_=self.ys_d.ap(),
                                                                                 in_offset=bass.IndirectOffsetOnAxis(ap=dest[:, col:col + 1], axis=0)), k(yt), r=[k(dest), "ys_d"], w=[k(yt)])
            V(lambda e, i=i: e.tensor_scalar(y0[:], y0[:], rp['gat'][:, 2 * i:2 * i + 1], None, ALU.mult), r=[k(y0), k(rp['gat'])], w=[k(y0)])
            V(lambda e, i=i: e.scalar_tensor_tensor(y0[:], y1[:], rp['gat'][:, 2 * i + 1:2 * i + 2], y0[:], ALU.mult, ALU.add), r=[k(y1), k(y0), k(rp['gat'])], w=[k(y0)])
            V(lambda e: e.scalar_tensor_tensor(h1[:], h1[:], ALPHA, y0[:], ALU.mult, ALU.add), r=[k(h1), k(y0)], w=[k(h1)])
            self.layernorm(h1, l2g, l2b, h2, tmp)
            if last:
                self.store('sp', self.out_d[i * 128:(i + 1) * 128, :], h2[:], k(h2), dkeys=["out_%d" % i])
            else:
                self.store('sp', self.h_d[i * 128:(i + 1) * 128, :], h2[:], k(h2), dkeys=["hd_%d" % i])
                self.to_fm(h2, hb2, hTo)
                self.store('sp', self.hT_d[:, :, i * 128:(i + 1) * 128], hTo[:], k(hTo), dkeys=["hTd_%d" % i])
        self.P.barrier()
        self.sb_off = mark

    def build_full(self):
        self.declare_inputs()
        T = self.T
        self.h_d = self.dscr("h_d", [T, D])
        self.hT_d = self.dscr("hT_d", [128, 8, T], BF16)
        self.h1_d = self.dscr("h1_d", [T, D])
        self.h1b_d = self.dscr("h1b_d", [T, D], BF16)
        self.xs_d = self.dscr("xs_d", [self.NB * self.RB, D], BF16)
        self.ys_d = self.dscr("ys_d", [self.NB * self.RB, D])
        self.out_d = self.dout("out", [T, D])
        if self.debug:
            self.dbg_y = self.dout("dbg_y", [T, D])
        self.consts()
        self.alloc_route_persist()
        base = self.sb_off
        for l in range(self.depth):
            self.sb_off = base
            self.alloc_params()
            self.alloc_mixer()
            self.alloc_router()
            if l == 0:
                self.stage0()
                self.P.barrier()
            self.load_params(l)
            self.stageM(l)
            self.P.barrier()
            self.sb_off = base
            self.stageMoE(l, l == self.depth - 1)
        self.P.barrier()
        return self.nc


def _host_inputs(inputs, b, T):
    m = {}
    for k, v in inputs.items():
        v = np.asarray(v)
        if k == 'x':
            m[k] = np.ascontiguousarray(v[b, :T])
        elif k == 'rwkv_r_k':
            m[k] = np.ascontiguousarray(v.reshape(DEPTH, 256))
        elif k in ('moe_w_gate', 'moe_w_up'):
            m[k] = np.ascontiguousarray(v.reshape(DEPTH * NE * D, FF))
        elif k == 'moe_w_down':
            m[k] = np.ascontiguousarray(v.reshape(DEPTH * NE * FF, D))
        else:
            m[k] = np.ascontiguousarray(v)
    return m


def kernel(**inputs):
    x = np.asarray(inputs['x'])
    Bsz, T, _ = x.shape
    bld = Builder(T)
    nc = bld.build_full()
    in_maps = [_host_inputs(inputs, b, T) for b in range(Bsz)]
    res = run_bass_kernel_spmd(nc, in_maps, core_ids=list(range(Bsz)))
    return np.stack([np.asarray(r["out"]) for r in res.results], axis=0).astype(np.float32)
```

```python
import numpy as np
import concourse.bass as bass
import concourse.mybir as mybir
from concourse.bass_utils import run_bass_kernel_spmd

F32 = mybir.dt.float32
BF16 = mybir.dt.bfloat16
I32 = mybir.dt.int32
U32 = mybir.dt.uint32
AF = mybir.ActivationFunctionType
ALU = mybir.AluOpType
AX = mybir.AxisListType

D = 1024
NIN = 2968
DEPTH = 2
ALPHA = (2 * DEPTH) ** 0.25
LN_EPS = 1e-5
RMS_EPS = 1e-6
GN_EPS = 64e-5
NE = 32
FF = 512
O_Z, O_XBC, O_DT, O_RW, O_GQ, O_GK, O_GV, O_GG, O_GA = 0, 512, 1280, 1288, 2184, 2312, 2440, 2696, 2952


class Prog:
    EPOCH = 8192
    NDMA = 40

    def __init__(self, nc):
        self.nc = nc
        self.eng = {'pe': nc.tensor, 'dve': nc.vector, 'act': nc.scalar, 'pool': nc.gpsimd, 'sp': nc.sync}
        self.esems = {n: [] for n in ('pe', 'dve', 'act', 'pool')}
        self.cnt = {n: 0 for n in ('pe', 'dve', 'act', 'pool')}
        self.dsems, self.dval, self.dkey = [], [], {}
        self.waited = {n: {} for n in self.eng}
        self.lastw, self.readers = {}, {}
        self.ninst = 0

    def _esem(self, X, ep):
        while len(self.esems[X]) <= ep:
            self.esems[X].append(self.nc.alloc_semaphore("s_%s_%d" % (X, len(self.esems[X]))))
        return self.esems[X][ep]

    def _deps(self, reads, writes):
        deps = {}

        def add(ev):
            if ev is not None and deps.get(ev[0], 0) < ev[1]:
                deps[ev[0]] = ev[1]
        for r in reads:
            add(self.lastw.get(r))
        for w in writes:
            add(self.lastw.get(w))
            for k, v in self.readers.get(w, {}).items():
                add((k, v))
        return deps

    def _wait(self, X, deps):
        e = self.eng[X]
        for k, v in deps.items():
            if k == X and X == 'pe':
                continue
            if self.waited[X].get(k, 0) >= v:
                continue
            if isinstance(k, str):
                ep = (v - 1) // self.EPOCH
                e.wait_ge(self._esem(k, ep), v - ep * self.EPOCH)
            else:
                v = self.dval[k]
                e.wait_ge(self.dsems[k], v)
            self.waited[X][k] = v
            self.ninst += 1

    def _record(self, ev, reads, writes):
        for r in reads:
            d = self.readers.setdefault(r, {})
            if d.get(ev[0], 0) < ev[1]:
                d[ev[0]] = ev[1]
        for w in writes:
            self.lastw[w] = ev
            self.readers[w] = {}

    def op(self, X, fn, r=(), w=()):
        r = [k for k in r if k is not None]
        w = [k for k in w if k is not None]
        w = w + [k for k in r if isinstance(k, str) and k.startswith('psb') and k not in w]
        self._wait(X, self._deps(r, w))
        inst = fn(self.eng[X])
        self.cnt[X] += 1
        n = self.cnt[X]
        inst.then_inc(self._esem(X, (n - 1) // self.EPOCH), 1)
        self.ninst += 1
        self._record((X, n), r, w)

    def dma(self, X, fn, semkey, r=(), w=()):
        if semkey not in self.dkey:
            i = len(self.dkey) % self.NDMA
            if i >= len(self.dsems):
                self.dsems.append(self.nc.alloc_semaphore("d_%d" % i))
                self.dval.append(0)
            self.dkey[semkey] = i
        i = self.dkey[semkey]
        self._wait(X, self._deps(r, w))
        inst = fn(self.eng[X])
        self.dval[i] += 16
        inst.then_inc(self.dsems[i], 16)
        self.ninst += 1
        self._record((i, self.dval[i]), r, w)

    def barrier(self):
        deps = {k: v for k, v in self.cnt.items() if v > 0}
        for i, v in enumerate(self.dval):
            if v > 0:
                deps[i] = v
        for X in self.eng:
            self._wait(X, dict(deps))


class Tile:
    def __init__(self, t, key):
        self.t, self.key = t, key

    def __getitem__(self, k):
        return self.t[k]


class Builder:
    def __init__(self, T, depth=DEPTH, debug=False, rb=None):
        import os
        rb = rb or int(os.environ.get('RB', '512'))
        self.T, self.depth, self.debug = T, depth, debug
        self.NT = T // 128
        self.RB = rb
        self.NB = (2 * T) // rb + NE
        nc = self.nc = bass.Bass("TRN2", target_bir_lowering=False)
        self.P = Prog(nc)
        self.nsb = 0
        self.sb_off = 16640
        self.sb_peak = 0
        self.sb_cap = 229376
        self.bank_i = 0
        self.chain_i = {}
        self.banks = [nc.alloc_psum_tensor("psb%d" % i, [128, 512], F32) for i in range(8)]
        self.dbg = {}

    def sb(self, shape, dt=F32, name=None):
        self.nsb += 1
        name = "%s_%d" % (name or "t", self.nsb)
        esz = 2 if dt == BF16 else 4
        n = 1
        for v in shape[1:]:
            n *= v
        nbytes = (n * esz + 31) // 32 * 32
        off = self.sb_off
        self.sb_off += nbytes
        assert self.sb_off <= self.sb_cap, "SBUF overflow %d" % self.sb_off
        self.sb_peak = max(self.sb_peak, self.sb_off)
        return Tile(self.nc.alloc_sbuf_tensor_at(name, list(shape), dt, offset=off), name)

    CHAIN_BANKS = {'s': [0, 1], 'r': [2, 3, 4], 'g': [5, 6], 'e': [7]}

    def bank(self, chain=None):
        if chain is None:
            i = self.bank_i
            self.bank_i = (i + 1) % 8
        else:
            lst = self.CHAIN_BANKS[chain]
            j = self.chain_i.get(chain, 0)
            self.chain_i[chain] = (j + 1) % len(lst)
            i = lst[j]
        return self.banks[i], "psb%d" % i

    def V(self, fn, r=(), w=()):
        self.P.op('dve', fn, r, w)

    def A(self, fn, r=(), w=()):
        self.P.op('act', fn, r, w)

    def G(self, fn, r=(), w=()):
        self.P.op('pool', fn, r, w)

    def M(self, fn, r=(), w=()):
        self.P.op('pe', fn, r, w)

    def din(self, name, shape, dt=F32):
        return self.nc.dram_tensor(name, list(shape), dt, kind="ExternalInput")

    def dscr(self, name, shape, dt=F32):
        return self.nc.dram_tensor(name, list(shape), dt, kind="Internal")

    def dout(self, name, shape, dt=F32):
        return self.nc.dram_tensor(name, list(shape), dt, kind="ExternalOutput")

    def load(self, q, out_ap, in_ap, key, dkeys=(), slow=False):
        if slow:
            self.P.dma(q, lambda e: e.dma_start(out=out_ap, in_=in_ap, allow_slow_non_contiguous=True), key, r=list(dkeys), w=[key])
        else:
            self.P.dma(q, lambda e: e.dma_start(out=out_ap, in_=in_ap), key, r=list(dkeys), w=[key])

    def store(self, q, out_ap, in_ap, key, dkeys=()):
        self.P.dma(q, lambda e: e.dma_start(out=out_ap, in_=in_ap), key, r=[key], w=list(dkeys))

    def consts(self):
        nc = self.nc
        c = self.c = {}
        self._ln_st = self.sb([128, 2, 6], name="ln_st")
        self._ln_mv = self.sb([128, 2], name="ln_mv")
        self._ln_rs = self.sb([128, 1], name="ln_rs")
        onesf = c['onesf'] = self.sb([128, 128], name="onesf")
        self.G(lambda e: e.memset(onesf[:], 1.0), w=[onesf.key])
        identf = c['identf'] = self.sb([128, 128], name="identf")
        self.G(lambda e: e.memset(identf[:], 0.0), w=[identf.key])
        self.G(lambda e: e.affine_select(out=identf[:], in_=identf[:], pattern=[[-1, 128]], base=0, channel_multiplier=1,
                                         compare_op=ALU.not_equal, fill=1.0), r=[identf.key], w=[identf.key])
        identb = c['identb'] = self.sb([128, 128], BF16, name="identb")
        self.V(lambda e: e.tensor_copy(identb[:], identf[:]), r=[identf.key], w=[identb.key])
        tri = c['tri'] = self.sb([128, 128], name="tri")
        self.G(lambda e: e.affine_select(out=tri[:], in_=onesf[:], pattern=[[1, 128]], base=0, channel_multiplier=-1,
                                         compare_op=ALU.is_ge, fill=0.0), r=[onesf.key], w=[tri.key])
        su = c['su'] = self.sb([128, 128], name="su")
        self.G(lambda e: e.affine_select(out=su[:], in_=onesf[:], pattern=[[-1, 128]], base=0, channel_multiplier=1,
                                         compare_op=ALU.is_gt, fill=0.0), r=[onesf.key], w=[su.key])
        sl = c['sl'] = self.sb([128, 128], name="sl")
        self.G(lambda e: e.affine_select(out=sl[:], in_=onesf[:], pattern=[[1, 128]], base=0, channel_multiplier=-1,
                                         compare_op=ALU.is_gt, fill=0.0), r=[onesf.key], w=[sl.key])
        maskb = c['maskb'] = self.sb([128, 128], name="maskb")
        self.G(lambda e: e.tensor_copy(maskb[:], tri[:]), r=[tri.key], w=[maskb.key])
        self.G(lambda e: e.memset(maskb[0:64, 64:128], 0.0), w=[maskb.key])
        rmask = c['rmask'] = self.sb([128, 256], name="rmask")
        self.G(lambda e: e.memset(rmask[:], 1.0), w=[rmask.key])
        self.G(lambda e: e.memset(rmask[:].rearrange("p (a b) -> p a b", b=64)[:, :, 0:1], 0.0), w=[rmask.key])
        hm = c['hm'] = self.sb([128, 2], name="hm")
        self.G(lambda e: e.memset(hm[:], 0.0), w=[hm.key])
        self.G(lambda e: e.memset(hm[0:64, 0:1], 1.0), w=[hm.key])
        self.G(lambda e: e.memset(hm[64:128, 1:2], 1.0), w=[hm.key])
        nhm = c['nhm'] = self.sb([128, 2], name="nhm")
        self.V(lambda e: e.tensor_scalar(nhm[:], hm[:], -1.0, None, ALU.mult), r=[hm.key], w=[nhm.key])
        qm = c['qm'] = self.sb([64, 2], name="qm")
        self.G(lambda e: e.memset(qm[:], 0.0), w=[qm.key])
        self.G(lambda e: e.memset(qm[0:32, 0:1], 32.0 ** -0.5), w=[qm.key])
        self.G(lambda e: e.memset(qm[32:64, 1:2], 32.0 ** -0.5), w=[qm.key])
        bones = c['bones'] = self.sb([128, 128], name="bones")
        self.G(lambda e: e.memset(bones[:], 0.0), w=[bones.key])
        self.G(lambda e: e.memset(bones[0:64, 0:64], 1.0), w=[bones.key])
        self.G(lambda e: e.memset(bones[64:128, 64:128], 1.0), w=[bones.key])

    def layernorm(self, xin, gk, bk, out, tmp):
        st, mv, rs = self._ln_st, self._ln_mv, self._ln_rs
        for i in range(2):
            self.V(lambda e, i=i: e.bn_stats(st[:, i, :], xin[:, i * 512:(i + 1) * 512]), r=[xin.key], w=[st.key])
        self.V(lambda e: e.bn_aggr(mv[:], st[:].rearrange("p a b -> p (a b)")), r=[st.key], w=[mv.key])
        self.V(lambda e: e.tensor_scalar(rs[:], mv[:, 1:2], LN_EPS, None, ALU.add), r=[mv.key], w=[rs.key])
        self.A(lambda e: e.activation(out=rs[:], in_=rs[:], func=AF.Sqrt), r=[rs.key], w=[rs.key])
        self.V(lambda e: e.reciprocal(rs[:], rs[:]), r=[rs.key], w=[rs.key])
        self.V(lambda e: e.tensor_scalar(tmp[:], xin[:], mv[:, 0:1], rs[:, 0:1], ALU.subtract, ALU.mult), r=[xin.key, mv.key, rs.key], w=[tmp.key])
        self.G(lambda e: e.tensor_tensor(tmp[:], tmp[:], gk[:], ALU.mult), r=[tmp.key, gk.key], w=[tmp.key])
        self.V(lambda e: e.tensor_tensor(out[:], tmp[:], bk[:], ALU.add), r=[tmp.key, bk.key], w=[out.key])

    def to_fm(self, h_tm, hb, hT):
        c = self.c
        self.A(lambda e: e.copy(out=hb[:], in_=h_tm[:]), r=[h_tm.key], w=[hb.key])
        bk, bkey = self.bank()
        pb = bk[:].bitcast(BF16)
        for kc in range(8):
            self.M(lambda e, kc=kc: e.transpose(pb[:, kc * 128:(kc + 1) * 128], hb[:, kc * 128:(kc + 1) * 128], c['identb'][:]),
                   r=[hb.key, c['identb'].key], w=[bkey])
        self.V(lambda e: e.tensor_copy(hT[:].rearrange("p a b -> p (a b)"), pb), r=[bkey], w=[hT.key])

    def declare_inputs(self):
        L = DEPTH
        d = self.d = {}
        specs = dict(x=[self.T, D], ln_in_g=[D], ln_in_b=[D], w_in=[L, D, NIN], ssd_conv_w=[L, 4, 768], ssd_conv_b=[L, 768],
                     ssd_dt_bias=[L, 8], ssd_a_log=[L, 8], ssd_d=[L, 8], ssd_norm_g=[L, 512], rwkv_mu=[L, 896], rwkv_w0=[L, 256],
                     rwkv_w2=[L, 32, 256], rwkv_a0=[L, 256], rwkv_a2=[L, 32, 256], rwkv_g2=[L, 64, 256], rwkv_k_k=[L, 256],
                     rwkv_k_a=[L, 256], rwkv_r_k=[L, 256], rwkv_ln_g=[L, 256], rwkv_ln_b=[L, 256], gla_w_a2=[L, 16, 128],
                     gla_b_a=[L, 128], gla_norm_g=[L, 256], w_out=[L, D, D], ln1_g=[L, D], ln1_b=[L, D], moe_w_rg=[L, D, 4],
                     moe_b_rg=[L, 4], moe_w_re=[L, D, 32], moe_b_re=[L, 32], moe_w_gate=[L * NE * D, FF], moe_w_up=[L * NE * D, FF],
                     moe_w_down=[L * NE * FF, D], ln2_g=[L, D], ln2_b=[L, D])
        for k, s in specs.items():
            d[k] = self.din(k, s)
        return specs

    def alloc_params(self):
        p = self.p = {}
        sb = self.sb
        p['w_in'] = sb([128, 8, NIN], BF16, "w_in_sb")
        p['w_out'] = sb([128, 8, D], BF16, "w_out_sb")
        p['cw'] = sb([128, 6, 4], name="convw")
        p['cb'] = sb([128, 6], name="convb")
        p['cdiag'] = sb([128, 24, 128], BF16, "cdiag")
        for n, w in (('dtb', 8), ('alog', 8), ('dsk8', 8), ('dsk', 512), ('sng', 512), ('rlg', 256), ('rlb', 256), ('gng', 256),
                     ('l1g', D), ('l1b', D), ('rb36', 36)):
            p[n] = sb([128, w], name="p_" + n)
        for n, w in (('mu', 7), ('omu', 7), ('w0', 2), ('a0', 2), ('kk', 2), ('ka', 2), ('omka', 2), ('rk', 2)):
            p[n] = sb([128, w], name="p_" + n)
        p['ba'] = sb([64, 2], name="p_ba")
        p['w2p'] = sb([128, 256], name="p_w2p")
        p['a2p'] = sb([128, 256], name="p_a2p")
        p['g2p'] = sb([128, 256], name="p_g2p")
        p['wa2'] = sb([32, 128], name="p_wa2")
        p['wr'] = sb([128, 8, 36], name="p_wr")

    def load_params(self, l):
        p, d, c = self.p, self.d, self.c
        q = 'pool'
        win = d['w_in'][l].rearrange("(kc p) n -> p kc n", p=128)
        for kc in range(8):
            for (a, b) in ((0, 1484), (1484, NIN)):
                self.load(q, p['w_in'][:, kc, a:b], win[:, kc, a:b], p['w_in'].key)
        wo = d['w_out'][l].rearrange("(kc p) n -> p kc n", p=128)
        for kc in range(8):
            self.load(q, p['w_out'][:, kc, :], wo[:, kc, :], p['w_out'].key)
        q = 'sp'
        for kk_ in range(4):
            self.load(q, p['cw'][:, :, kk_], d['ssd_conv_w'][l][kk_].rearrange("(cb p) -> p cb", p=128), p['cw'].key, slow=True)
        self.load(q, p['cb'][:], d['ssd_conv_b'][l].rearrange("(cb p) -> p cb", p=128), p['cb'].key, slow=True)
        for n, src in (('dtb', 'ssd_dt_bias'), ('alog', 'ssd_a_log'), ('dsk8', 'ssd_d'), ('sng', 'ssd_norm_g'), ('rlg', 'rwkv_ln_g'),
                       ('rlb', 'rwkv_ln_b'), ('gng', 'gla_norm_g'), ('l1g', 'ln1_g'), ('l1b', 'ln1_b')):
            self.load(q, p[n][:], d[src][l].partition_broadcast(128), p[n].key)
        self.load(q, p['rb36'][:, 0:4], d['moe_b_rg'][l].partition_broadcast(128), p['rb36'].key)
        self.load(q, p['rb36'][:, 4:36], d['moe_b_re'][l].partition_broadcast(128), p['rb36'].key)
        self.load(q, p['mu'][:], d['rwkv_mu'][l].rearrange("(b p) -> p b", p=128), p['mu'].key, slow=True)
        for n, src in (('w0', 'rwkv_w0'), ('a0', 'rwkv_a0'), ('kk', 'rwkv_k_k'), ('ka', 'rwkv_k_a'), ('rk', 'rwkv_r_k')):
            self.load(q, p[n][:], d[src][l].rearrange("(b p) -> p b", p=128), p[n].key, slow=True)
        self.load(q, p['ba'][:], d['gla_b_a'][l].rearrange("(b p) -> p b", p=64), p['ba'].key, slow=True)
        for n in ('w2p', 'a2p', 'g2p'):
            self.G(lambda e, n=n: e.memset(p[n][:], 0.0), w=[p[n].key])
        self.load(q, p['w2p'][0:32, :], d['rwkv_w2'][l], p['w2p'].key)
        self.load(q, p['a2p'][32:64, :], d['rwkv_a2'][l], p['a2p'].key)
        self.load(q, p['g2p'][64:128, :], d['rwkv_g2'][l], p['g2p'].key)
        self.G(lambda e: e.memset(p['wa2'][:], 0.0), w=[p['wa2'].key])
        self.load(q, p['wa2'][16:32, :], d['gla_w_a2'][l], p['wa2'].key)
        self.load(q, p['wr'][:, :, 0:4], d['moe_w_rg'][l].rearrange("(kc p) n -> p kc n", p=128), p['wr'].key, slow=True)
        self.load(q, p['wr'][:, :, 4:36], d['moe_w_re'][l].rearrange("(kc p) n -> p kc n", p=128), p['wr'].key, slow=True)
        self.V(lambda e: e.tensor_scalar(p['omu'][:], p['mu'][:], -1.0, 1.0, ALU.mult, ALU.add), r=[p['mu'].key], w=[p['omu'].key])
        self.V(lambda e: e.tensor_scalar(p['omka'][:], p['ka'][:], -1.0, 1.0, ALU.mult, ALU.add), r=[p['ka'].key], w=[p['omka'].key])
        self.A(lambda e: e.activation(out=p['alog'][:], in_=p['alog'][:], func=AF.Exp), r=[p['alog'].key], w=[p['alog'].key])
        self.V(lambda e: e.tensor_scalar(p['alog'][:], p['alog'][:], -1.0, None, ALU.mult), r=[p['alog'].key], w=[p['alog'].key])
        self.V(lambda e: e.tensor_copy(p['dsk'][:].rearrange("p (h q) -> p h q", q=64), p['dsk8'][:].unsqueeze(2).to_broadcast([128, 8, 64])),
               r=[p['dsk8'].key], w=[p['dsk'].key])
        for cb in range(6):
            for k in range(4):
                self.V(lambda e, cb=cb, k=k: e.tensor_scalar(p['cdiag'][:, cb * 4 + k, :], c['identf'][:], p['cw'][:, cb, k:k + 1], None, ALU.mult),
                       r=[c['identf'].key, p['cw'].key], w=[p['cdiag'].key])

    def alloc_mixer(self):
        s = self.s = {}
        sb = self.sb
        s['hT'] = sb([128, 8, 128], BF16, "m_hT")
        s['htm'] = sb([128, D], name="m_htm")
        s['xbc'] = sb([128, 6, 132], BF16, "m_xbc")
        s['xbB'] = sb([128, 6, 132], BF16, "m_xbB")
        s['xc'] = sb([128, 6, 128], BF16, "m_xc")
        s['xh'] = sb([128, 512], BF16, "m_xh")
        s['xdt'] = sb([128, 512], BF16, "m_xdt")
        s['btm'] = sb([128, 128], BF16, "m_btm")
        s['cm'] = sb([128, 2, 128], BF16, "m_cm")
        s['dt'] = sb([128, 8], name="m_dt")
        s['adt'] = sb([128, 8], name="m_adt")
        s['sp1'] = sb([128, 8], name="m_sp1")
        s['sp2'] = sb([128, 8], name="m_sp2")
        s['R'] = sb([128, 4, 128], name="m_R")
        s['seg'] = sb([128, 8, 128], BF16, "m_seg")
        s['ea'] = sb([128, 8], name="m_ea")
        s['cd'] = sb([128, 4], name="m_cd")
        s['cbm'] = sb([128, 2, 128], BF16, "m_cbm")
        s['toend'] = sb([128, 8], name="m_toend")
        s['S32'] = sb([128, 256], name="m_S32")
        s['Sbf'] = sb([128, 256], BF16, "m_Sbf")
        s['y1'] = sb([128, 512], name="m_y1")
        s['sz'] = sb([128, 512], BF16, "m_sz")
        s['ssq'] = sb([128, 4], name="m_ssq")
        s['ycat'] = sb([128, D], BF16, "m_ycat")
        s['yT'] = sb([128, 8, 128], BF16, "m_yT")
        s['gaT'] = sb([32, 128], name="g_gaT")
        s['gx'] = sb([64, 256], name="g_x")
        s['gt1'] = sb([64, 256], name="g_t1")
        s['gcum'] = sb([64, 256], name="g_cum")
        s['geq'] = sb([64, 256], name="g_eq")
        s['gek'] = sb([64, 256], name="g_ek")
        s['gel'] = sb([64, 4], name="g_el")
        s['gqm'] = sb([64, 2, 256], BF16, "g_qm")
        s['gkT'] = sb([64, 256], BF16, "g_kT")
        s['gktm'] = sb([128, 2, 128], BF16, "g_ktm")
        s['gv'] = sb([128, 256], BF16, "g_v")
        s['gvm'] = sb([128, 2, 256], BF16, "g_vm")
        s['gsm'] = sb([128, 4, 128], BF16, "g_sm")
        s['gS'] = sb([64, 2, 64], name="g_S")
        s['gSb'] = sb([64, 2, 2, 64], BF16, "g_Sb")
        s['gst'] = sb([64, 2, 64], name="g_st")
        s['go'] = sb([128, 256], name="g_o")
        s['gsq'] = sb([128, 256], name="g_sq")
        s['grs'] = sb([128, 4], name="g_rs")
        s['gsg'] = sb([128, 256], name="g_sg")
        s['rw'] = sb([128, 7, 129], name="r_rw")
        s['rsh'] = sb([128, 7, 128], name="r_sh")
        s['rt1'] = sb([128, 7, 128], name="r_t1")
        for n in ('ra1', 'ra2', 'ra3', 'rcw', 'recw', 'reicw', 'recwp', 'ra', 'rkk', 'rkp'):
            s[n] = sb([128, 256], name="r_" + n)
        for n in ('rKt', 'rBt'):
            s[n] = sb([128, 256], BF16, "r_" + n)
        s['rAm'] = sb([128, 2, 256], BF16, "r_Am")
        s['rRm'] = sb([128, 2, 256], BF16, "r_Rm")
        s['rtw'] = sb([128, 128], name="r_tw")
        s['rsg'] = sb([128, 128], name="r_sg")
        s['rvtm'] = sb([128, 256], name="r_vtm")
        s['rvc'] = sb([64, 2, 256], BF16, "r_vc")
        s['rBc'] = sb([64, 2, 256], BF16, "r_Bc")
        s['rKc'] = sb([64, 2, 256], BF16, "r_Kc")
        s['rP'] = sb([64, 8, 64], BF16, "r_P")
        s['rQ'] = sb([64, 8, 64], BF16, "r_Q")
        s['rP2'] = sb([64, 8, 64], BF16, "r_P2")
        s['rQ2'] = sb([64, 8, 64], BF16, "r_Q2")
        s['rTT'] = sb([64, 8, 64], BF16, "r_TT")
        s['rAak'] = sb([64, 8, 64], BF16, "r_Aak")
        s['rArb'] = sb([64, 8, 64], BF16, "r_Arb")
        s['rArk'] = sb([64, 8, 64], BF16, "r_Ark")
        s['rG'] = sb([64, 4, 64], BF16, "r_G")
        s['rU'] = sb([64, 4, 64], BF16, "r_U")
        s['rST'] = sb([128, 2, 64], name="r_ST")
        s['rSTb'] = sb([128, 2, 64], BF16, "r_STb")
        s['rt2'] = sb([128, 2, 64], name="r_t2")
        s['rewc'] = sb([128, 4], name="r_ewc")
        s['rY1'] = sb([128, 256], name="r_Y1")
        s['rY'] = sb([128, 256], name="r_Y")
        s['rm1'] = sb([128, 4], name="r_m1")
        s['rm2'] = sb([128, 4], name="r_m2")
        s['rvar'] = sb([128, 4], name="r_var")
        s['rbc'] = sb([128, 4], name="r_bc")
        s['rg'] = sb([128, 256], name="r_g")
        s['mix'] = sb([128, D], name="m_mix")
        s['tmp'] = sb([128, D], name="m_tmp")
        s['h1'] = sb([128, D], name="m_h1")

    def proj_tm(self, out_ap, okey, c0, n):
        s, p = self.s, self.p
        for kc in range(8):
            self.M(lambda e, kc=kc: e.matmul(out_ap, lhsT=s['hT'][:, kc, :], rhs=p['w_in'][:, kc, c0:c0 + n], start=(kc == 0), stop=(kc == 7)),
                   r=[s['hT'].key, p['w_in'].key], w=[okey])

    def proj_fm(self, out_ap, okey, c0, m):
        s, p = self.s, self.p
        for kc in range(8):
            self.M(lambda e, kc=kc: e.matmul(out_ap, lhsT=p['w_in'][:, kc, c0:c0 + m], rhs=s['hT'][:, kc, :], start=(kc == 0), stop=(kc == 7)),
                   r=[s['hT'].key, p['w_in'].key], w=[okey])

    def bc(self, ap, shape, axis):
        return ap.unsqueeze(axis).to_broadcast(list(shape))

    def ssd_tile(self, first):
        s, p, c = self.s, self.p, self.c
        V, A, G, M = self.V, self.A, self.G, self.M
        k = lambda t: t.key
        bz, kz = self.bank('s')
        self.proj_tm(bz[:, :], kz, O_Z, 512)
        A(lambda e: e.activation(out=s['sz'][:], in_=bz[:, :], func=AF.Silu), r=[kz], w=[k(s['sz'])])
        yield
        bd, kd = self.bank('s')
        self.proj_tm(bd[:, 0:8], kd, O_DT, 8)
        V(lambda e: e.tensor_tensor(s['sp1'][:], bd[:, 0:8], p['dtb'][:], ALU.add), r=[kd, k(p['dtb'])], w=[k(s['sp1'])])
        yield
        V(lambda e: e.scalar_tensor_tensor(s['sp2'][:], s['sp1'][:], -1.0, s['sp1'][:], ALU.mult, ALU.max), r=[k(s['sp1'])], w=[k(s['sp2'])])
        yield
        A(lambda e: e.activation(out=s['sp2'][:], in_=s['sp2'][:], func=AF.Exp, scale=-1.0), r=[k(s['sp2'])], w=[k(s['sp2'])])
        yield
        A(lambda e: e.activation(out=s['sp2'][:], in_=s['sp2'][:], func=AF.Ln, bias=1.0), r=[k(s['sp2'])], w=[k(s['sp2'])])
        yield
        V(lambda e: e.scalar_tensor_tensor(s['dt'][:], s['sp1'][:], 0.0, s['sp2'][:], ALU.max, ALU.add), r=[k(s['sp1']), k(s['sp2'])], w=[k(s['dt'])])
        yield
        V(lambda e: e.tensor_tensor(s['adt'][:], s['dt'][:], p['alog'][:], ALU.mult), r=[k(s['dt']), k(p['alog'])], w=[k(s['adt'])])
        yield
        import os
        stop = float(os.environ.get('SSDSTOP', '9'))
        if stop <= 1:
            return
        if first:
            G(lambda e: e.memset(s['xbc'][:, :, 0:4], 0.0), w=[k(s['xbc'])])
            yield
            G(lambda e: e.memset(s['xbB'][:, :, 0:2], 0.0), w=[k(s['xbB'])])
            yield
        else:
            G(lambda e: e.tensor_copy(s['xbc'][:, :, 0:3], s['xbc'][:, :, 128:131]), r=[k(s['xbc'])], w=[k(s['xbc'])])
            yield
            G(lambda e: e.tensor_copy(s['xbB'][:, :, 0:2], s['xbB'][:, :, 128:130]), r=[k(s['xbB'])], w=[k(s['xbB'])])
            yield
        for grp, nb in ((0, 4), (4, 2)):
            bx, kx = self.bank('s')
            for j in range(nb):
                self.proj_fm(bx[:, j * 128:(j + 1) * 128], kx, O_XBC + (grp + j) * 128, 128)
            A(lambda e, bx=bx, grp=grp, nb=nb: e.copy(out=s['xbc'][:, grp:grp + nb, 3:131], in_=bx[:, 0:nb * 128].rearrange("p (a b) -> p a b", b=128)),
              r=[kx], w=[k(s['xbc'])])
            yield
            V(lambda e, bx=bx, grp=grp, nb=nb: e.tensor_copy(s['xbB'][:, grp:grp + nb, 2:130], bx[:, 0:nb * 128].rearrange("p (a b) -> p a b", b=128)),
              r=[kx], w=[k(s['xbB'])])
            yield
        for grp, nb in ((0, 4), (4, 2)):
            bx, kx = self.bank('s')
            for j in range(nb):
                cb = grp + j
                for kk_ in range(4):
                    src = s['xbc'] if kk_ % 2 == 0 else s['xbB']
                    off = kk_ if kk_ % 2 == 0 else kk_ - 1
                    M(lambda e, bx=bx, j=j, cb=cb, kk_=kk_, src=src, off=off: e.matmul(bx[:, j * 128:(j + 1) * 128], lhsT=p['cdiag'][:, cb * 4 + kk_, :],
                                                                                   rhs=src[:, cb, off:off + 128], start=(kk_ == 0), stop=(kk_ == 3)),
                      r=[k(p['cdiag']), k(src)], w=[kx])
            for j in range(nb):
                cb = grp + j
                A(lambda e, bx=bx, j=j, cb=cb: e.activation(out=s['xc'][:, cb, :], in_=bx[:, j * 128:(j + 1) * 128], func=AF.Silu, bias=p['cb'][:, cb:cb + 1]),
                  r=[kx, k(p['cb'])], w=[k(s['xc'])])
                yield
        if stop <= 2:
            return
        bt, kt = self.bank('s')
        pb = bt[:].bitcast(BF16)
        for j in range(5):
            M(lambda e, j=j: e.transpose(pb[:, j * 128:(j + 1) * 128], s['xc'][:, j, :], c['identb'][:]), r=[k(s['xc']), k(c['identb'])], w=[kt])
        V(lambda e: e.tensor_copy(s['xh'][:], pb[:, 0:512]), r=[kt], w=[k(s['xh'])])
        yield
        V(lambda e: e.tensor_copy(s['btm'][:], pb[:, 512:640]), r=[kt], w=[k(s['btm'])])
        yield
        if stop <= 2.2:
            return
        G(lambda e: e.tensor_tensor(s['cm'][:], self.bc(s['xc'][:, 5, :], [128, 2, 128], 1), self.bc(c['hm'][:, :], [128, 2, 128], 2), ALU.mult),
          r=[k(s['xc']), k(c['hm'])], w=[k(s['cm'])])
        yield
        V(lambda e: e.tensor_tensor(s['xdt'][:].rearrange("p (h q) -> p h q", q=64), s['xh'][:].rearrange("p (h q) -> p h q", q=64),
                                    self.bc(s['dt'][:, :], [128, 8, 64], 2), ALU.mult), r=[k(s['xh']), k(s['dt'])], w=[k(s['xdt'])])
        yield
        if stop <= 2.4:
            return
        for half in range(2):
            G(lambda e, half=half: e.tensor_tensor(s['R'][:], self.bc(c['tri'][:, :], [128, 4, 128], 1), self.bc(s['adt'][:, half * 4:(half + 1) * 4], [128, 4, 128], 2), ALU.mult),
              r=[k(c['tri']), k(s['adt'])], w=[k(s['R'])])
            yield
            bD, kD = self.bank('s')
            for q2 in range(2):
                M(lambda e, bD=bD, q2=q2: e.matmul(bD[:, q2 * 256:(q2 + 1) * 256], lhsT=c['su'][:], rhs=s['R'][:, q2 * 2:(q2 + 1) * 2, :].rearrange("p a b -> p (a b)"), start=True, stop=True),
                  r=[k(c['su']), k(s['R'])], w=[kD])
            if stop <= 2.6:
                continue
            A(lambda e, bD=bD, half=half: e.activation(out=s['seg'][:, half * 4:(half + 1) * 4, :].rearrange("p a b -> p (a b)"), in_=bD[:, :], func=AF.Exp),
              r=[kD], w=[k(s['seg'])])
            yield
        if stop <= 2.8:
            return
        V(lambda e: e.tensor_copy(s['toend'][:], s['seg'][:, :, 127]), r=[k(s['seg'])], w=[k(s['toend'])])
        yield
        if stop <= 3:
            return
        be, ke = self.bank('s')
        M(lambda e: e.matmul(be[:, 0:8], lhsT=c['tri'][:], rhs=s['adt'][:], start=True, stop=True), r=[k(c['tri']), k(s['adt'])], w=[ke])
        for g in range(2):
            M(lambda e, g=g: e.matmul(be[g * 64:(g + 1) * 64, 8:12], lhsT=c['onesf'][:, 0:64], rhs=s['adt'][:, g * 4:(g + 1) * 4], start=True, stop=True),
              r=[k(c['onesf']), k(s['adt'])], w=[ke])
        A(lambda e: e.activation(out=s['ea'][:], in_=be[:, 0:8], func=AF.Exp), r=[ke], w=[k(s['ea'])])
        yield
        A(lambda e: e.activation(out=s['cd'][:], in_=be[:, 8:12], func=AF.Exp), r=[ke], w=[k(s['cd'])])
        yield
        if stop <= 4:
            return
        bc_, kc_ = self.bank('s')
        for g in range(2):
            M(lambda e, g=g: e.matmul(bc_[:, g * 128:(g + 1) * 128], lhsT=s['xc'][:, 4, :], rhs=s['cm'][:, g, :], start=True, stop=True),
              r=[k(s['xc']), k(s['cm'])], w=[kc_])
        V(lambda e: e.tensor_tensor(s['cbm'][:], bc_[:, 0:256].rearrange("p (a b) -> p a b", b=128), self.bc(c['tri'][:, :], [128, 2, 128], 1), ALU.mult),
          r=[kc_, k(c['tri'])], w=[k(s['cbm'])])
        yield
        for g in range(2):
            V(lambda e, g=g: e.tensor_tensor(s['seg'][:, g * 4:(g + 1) * 4, :], s['seg'][:, g * 4:(g + 1) * 4, :], self.bc(s['cbm'][:, g, :], [128, 4, 128], 1), ALU.mult),
              r=[k(s['seg']), k(s['cbm'])], w=[k(s['seg'])])
            yield
        by, ky = self.bank('s')
        for h in range(8):
            M(lambda e, h=h: e.matmul(by[:, h * 64:(h + 1) * 64], lhsT=s['seg'][:, h, :], rhs=s['xdt'][:, h * 64:(h + 1) * 64], start=True, stop=True),
              r=[k(s['seg']), k(s['xdt'])], w=[ky])
        bo, ko = self.bank('s')
        if not first:
            for g in range(2):
                M(lambda e, g=g: e.matmul(bo[:, g * 256:(g + 1) * 256], lhsT=s['cm'][:, g, :], rhs=s['Sbf'][:, :], start=True, stop=True),
                  r=[k(s['cm']), k(s['Sbf'])], w=[ko])
            V(lambda e: e.tensor_tensor(s['y1'][:].rearrange("p (h q) -> p h q", q=64), bo[:, :].rearrange("p (h q) -> p h q", q=64),
                                        self.bc(s['ea'][:, :], [128, 8, 64], 2), ALU.mult), r=[ko, k(s['ea'])], w=[k(s['y1'])])
            yield
            V(lambda e: e.tensor_tensor(s['y1'][:], s['y1'][:], by[:, :], ALU.add), r=[k(s['y1']), ky], w=[k(s['y1'])])
            yield
        else:
            V(lambda e: e.tensor_copy(s['y1'][:], by[:, :]), r=[ky], w=[k(s['y1'])])
            yield
        if stop <= 5:
            return
        V(lambda e: e.tensor_tensor(s['xdt'][:].rearrange("p (h q) -> p h q", q=64), s['xdt'][:].rearrange("p (h q) -> p h q", q=64),
                                    self.bc(s['toend'][:, :], [128, 8, 64], 2), ALU.mult), r=[k(s['xdt']), k(s['toend'])], w=[k(s['xdt'])])
        yield
        bs, ks = self.bank('s')
        for g in range(2):
            M(lambda e, g=g: e.matmul(bs[g * 64:(g + 1) * 64, 0:256], lhsT=s['btm'][:, g * 64:(g + 1) * 64], rhs=s['xdt'][:, g * 256:(g + 1) * 256], start=True, stop=True),
              r=[k(s['btm']), k(s['xdt'])], w=[ks])
        if first:
            V(lambda e: e.tensor_copy(s['S32'][:], bs[:, 0:256]), r=[ks], w=[k(s['S32'])])
            yield
        else:
            V(lambda e: e.tensor_tensor(s['S32'][:].rearrange("p (h q) -> p h q", q=64), s['S32'][:].rearrange("p (h q) -> p h q", q=64),
                                        self.bc(s['cd'][:, :], [128, 4, 64], 2), ALU.mult), r=[k(s['S32']), k(s['cd'])], w=[k(s['S32'])])
            yield
            V(lambda e: e.tensor_tensor(s['S32'][:], s['S32'][:], bs[:, 0:256], ALU.add), r=[k(s['S32']), ks], w=[k(s['S32'])])
            yield
        A(lambda e: e.copy(out=s['Sbf'][:], in_=s['S32'][:]), r=[k(s['S32'])], w=[k(s['Sbf'])])
        yield
        G(lambda e: e.tensor_tensor(s['xdt'][:], s['xh'][:], p['dsk'][:], ALU.mult), r=[k(s['xh']), k(p['dsk'])], w=[k(s['xdt'])])
        yield
        V(lambda e: e.tensor_tensor(s['y1'][:], s['y1'][:], s['xdt'][:], ALU.add), r=[k(s['y1']), k(s['xdt'])], w=[k(s['y1'])])
        yield
        V(lambda e: e.tensor_tensor(s['y1'][:], s['y1'][:], s['sz'][:], ALU.mult), r=[k(s['y1']), k(s['sz'])], w=[k(s['y1'])])
        yield
        for g in range(2):
            A(lambda e, g=g: e.activation(out=s['sz'][:, g * 256:(g + 1) * 256], in_=s['y1'][:, g * 256:(g + 1) * 256], func=AF.Square, accum_out=s['ssq'][:, g:g + 1]),
              r=[k(s['y1'])], w=[k(s['sz']), k(s['ssq'])])
            yield
        V(lambda e: e.tensor_scalar(s['ssq'][:, 0:2], s['ssq'][:, 0:2], 1.0 / 256, RMS_EPS, ALU.mult, ALU.add), r=[k(s['ssq'])], w=[k(s['ssq'])])
        yield
        A(lambda e: e.activation(out=s['ssq'][:, 0:2], in_=s['ssq'][:, 0:2], func=AF.Sqrt), r=[k(s['ssq'])], w=[k(s['ssq'])])
        yield
        V(lambda e: e.reciprocal(s['ssq'][:, 0:2], s['ssq'][:, 0:2]), r=[k(s['ssq'])], w=[k(s['ssq'])])
        yield
        for g in range(2):
            V(lambda e, g=g: e.scalar_tensor_tensor(s['ycat'][:, g * 256:(g + 1) * 256], s['y1'][:, g * 256:(g + 1) * 256], s['ssq'][:, g:g + 1],
                                                   p['sng'][:, g * 256:(g + 1) * 256], ALU.mult, ALU.mult), r=[k(s['y1']), k(s['ssq']), k(p['sng'])], w=[k(s['ycat'])])
            yield

    def gla_tile(self, first):
        s, p, c = self.s, self.p, self.c
        V, A, G, M = self.V, self.A, self.G, self.M
        k = lambda t: t.key
        bv, kv = self.bank('g')
        self.proj_tm(bv[:, :], kv, O_GV, 512)
        A(lambda e: e.copy(out=s['gv'][:], in_=bv[:, 0:256]), r=[kv], w=[k(s['gv'])])
        yield
        A(lambda e: e.activation(out=s['gsg'][:], in_=bv[:, 256:512], func=AF.Silu), r=[kv], w=[k(s['gsg'])])
        yield
        G(lambda e: e.tensor_tensor(s['gsg'][:], s['gsg'][:], p['gng'][:], ALU.mult), r=[k(s['gsg']), k(p['gng'])], w=[k(s['gsg'])])
        yield
        ba_, ka_ = self.bank('g')
        self.proj_fm(ba_[0:32, 0:128], ka_, O_GA - 16, 32)
        A(lambda e: e.copy(out=s['gaT'][:], in_=ba_[0:32, 0:128]), r=[ka_], w=[k(s['gaT'])])
        yield
        bx, kx = self.bank('g')
        for pr in range(2):
            M(lambda e, pr=pr: e.matmul(bx[0:64, pr * 128:(pr + 1) * 128], lhsT=p['wa2'][:, pr * 64:(pr + 1) * 64], rhs=s['gaT'][:], start=True, stop=True),
              r=[k(p['wa2']), k(s['gaT'])], w=[kx])
        for pr in range(2):
            A(lambda e, pr=pr: e.activation(out=s['gx'][:, pr * 128:(pr + 1) * 128], in_=bx[0:64, pr * 128:(pr + 1) * 128], func=AF.Identity, bias=p['ba'][:, pr:pr + 1]),
              r=[kx, k(p['ba'])], w=[k(s['gx'])])
            yield
        V(lambda e: e.scalar_tensor_tensor(s['gt1'][:], s['gx'][:], -1.0, s['gx'][:], ALU.mult, ALU.max), r=[k(s['gx'])], w=[k(s['gt1'])])
        yield
        A(lambda e: e.activation(out=s['gt1'][:], in_=s['gt1'][:], func=AF.Exp, scale=-1.0), r=[k(s['gt1'])], w=[k(s['gt1'])])
        yield
        A(lambda e: e.activation(out=s['gt1'][:], in_=s['gt1'][:], func=AF.Ln, bias=1.0), r=[k(s['gt1'])], w=[k(s['gt1'])])
        yield
        V(lambda e: e.scalar_tensor_tensor(s['gx'][:], s['gx'][:], 0.0, s['gt1'][:], ALU.min, ALU.subtract), r=[k(s['gx']), k(s['gt1'])], w=[k(s['gx'])])
        yield
        V(lambda e: e.tensor_tensor_scan(s['gcum'][:], c['rmask'][0:64, :], s['gx'][:], 0.0, ALU.mult, ALU.add), r=[k(c['rmask']), k(s['gx'])], w=[k(s['gcum'])])
        yield
        A(lambda e: e.activation(out=s['geq'][:], in_=s['gcum'][:], func=AF.Exp, scale=1.0 / 16), r=[k(s['gcum'])], w=[k(s['geq'])])
        yield
        A(lambda e: e.activation(out=s['gek'][:], in_=s['gcum'][:], func=AF.Exp, scale=-1.0 / 16), r=[k(s['gcum'])], w=[k(s['gek'])])
        yield
        A(lambda e: e.activation(out=s['gel'][:], in_=s['gcum'][:].rearrange("p (a b) -> p a b", b=64)[:, :, 63], func=AF.Exp, scale=1.0 / 16),
          r=[k(s['gcum'])], w=[k(s['gel'])])
        yield
        bq, kq = self.bank('g')
        for pr in range(2):
            self.proj_fm(bq[0:64, pr * 128:(pr + 1) * 128], kq, O_GQ + pr * 64, 64)
        for pr in range(2):
            self.proj_fm(bq[0:64, 256 + pr * 128:256 + (pr + 1) * 128], kq, O_GK + pr * 64, 64)
        for hh in range(2):
            V(lambda e, hh=hh: e.scalar_tensor_tensor(s['gqm'][:, hh, :], bq[0:64, 0:256], c['qm'][:, hh:hh + 1], s['geq'][:], ALU.mult, ALU.mult),
              r=[kq, k(c['qm']), k(s['geq'])], w=[k(s['gqm'])])
            yield
        V(lambda e: e.tensor_tensor(s['gkT'][:], bq[0:64, 256:512], s['gek'][:], ALU.mult), r=[kq, k(s['gek'])], w=[k(s['gkT'])])
        yield
        bt, kt = self.bank('g')
        pb = bt[:].bitcast(BF16)
        for pr in range(2):
            M(lambda e, pr=pr: e.transpose(pb[:, pr * 64:(pr + 1) * 64], s['gkT'][:, pr * 128:(pr + 1) * 128], c['identb'][0:64, 0:64]),
              r=[k(s['gkT']), k(c['identb'])], w=[kt])
        if first:
            G(lambda e: e.memset(s['gktm'][:], 0.0), w=[k(s['gktm'])])
            yield
        for hh in range(2):
            V(lambda e, hh=hh: e.tensor_copy(s['gktm'][:, hh, :].rearrange("p (a b c) -> p a b c", a=2, b=2)[:, :, hh, :],
                                             pb[:, 0:128].rearrange("p (a b c) -> p a b c", a=2, b=2)[:, :, hh, :]), r=[kt], w=[k(s['gktm'])])
            yield
        G(lambda e: e.tensor_tensor(s['gvm'][:], self.bc(s['gv'][:, :], [128, 2, 256], 1), self.bc(c['hm'][:, :], [128, 2, 256], 2), ALU.mult),
          r=[k(s['gv']), k(c['hm'])], w=[k(s['gvm'])])
        yield
        bs_, ks_ = self.bank('g')
        for h in range(4):
            pr, hh = h // 2, h % 2
            M(lambda e, h=h, pr=pr, hh=hh: e.matmul(bs_[:, h * 128:(h + 1) * 128], lhsT=s['gkT'][:, pr * 128:(pr + 1) * 128], rhs=s['gqm'][:, hh, pr * 128:(pr + 1) * 128],
                                                    start=True, stop=True), r=[k(s['gkT']), k(s['gqm'])], w=[ks_])
        V(lambda e: e.tensor_tensor(s['gsm'][:], bs_[:, :].rearrange("p (a b) -> p a b", b=128), self.bc(c['maskb'][:, :], [128, 4, 128], 1), ALU.mult),
          r=[ks_, k(c['maskb'])], w=[k(s['gsm'])])
        yield
        bu, ku = self.bank('g')
        for cc in range(2):
            for pr in range(2):
                for hh in range(2):
                    h = pr * 2 + hh
                    M(lambda e, cc=cc, h=h, pr=pr, hh=hh: e.matmul(bu[0:64, cc * 128 + pr * 64:cc * 128 + (pr + 1) * 64],
                                                                   lhsT=s['gktm'][:, hh, pr * 64:(pr + 1) * 64], rhs=s['gvm'][:, cc, h * 64:(h + 1) * 64],
                                                                   start=(hh == 0), stop=(hh == 1)), r=[k(s['gktm']), k(s['gvm'])], w=[ku])
        if first:
            G(lambda e: e.memset(s['gS'][:], 0.0), w=[k(s['gS'])])
            yield
        for cc in range(2):
            A(lambda e, cc=cc: e.copy(out=s['gSb'][:, cc, :, :], in_=s['gS'][:]), r=[k(s['gS'])], w=[k(s['gSb'])])
            yield
            V(lambda e, cc=cc: e.tensor_tensor(s['gst'][:], s['gS'][:], bu[0:64, cc * 128:(cc + 1) * 128].rearrange("p (a b) -> p a b", b=64), ALU.add),
              r=[k(s['gS']), ku], w=[k(s['gst'])])
            yield
            V(lambda e, cc=cc: e.tensor_tensor(s['gS'][:], s['gst'][:], self.bc(s['gel'][:, cc::2], [64, 2, 64], 2), ALU.mult),
              r=[k(s['gst']), k(s['gel'])], w=[k(s['gS'])])
            yield
        bo, ko = self.bank('g')
        for h in range(4):
            M(lambda e, h=h: e.matmul(bo[:, h * 64:(h + 1) * 64], lhsT=s['gsm'][:, h, :], rhs=s['gv'][:, h * 64:(h + 1) * 64], start=True, stop=True),
              r=[k(s['gsm']), k(s['gv'])], w=[ko])
        bi, ki = self.bank('g')
        for cc in range(2):
            for h in range(4):
                pr, hh = h // 2, h % 2
                M(lambda e, cc=cc, h=h, pr=pr, hh=hh: e.matmul(bi[cc * 64:(cc + 1) * 64, h * 64:(h + 1) * 64], lhsT=s['gqm'][:, hh, pr * 128 + cc * 64:pr * 128 + (cc + 1) * 64],
                                                               rhs=s['gSb'][:, cc, pr, :], start=True, stop=True), r=[k(s['gqm']), k(s['gSb'])], w=[ki])
        A(lambda e: e.copy(out=s['gsq'][:], in_=bi[:, 0:256]), r=[ki], w=[k(s['gsq'])])
        yield
        V(lambda e: e.tensor_tensor(s['go'][:], s['gsq'][:], bo[:, 0:256], ALU.add), r=[k(s['gsq']), ko], w=[k(s['go'])])
        yield
        A(lambda e: e.activation(out=s['gsq'][:], in_=s['go'][:], func=AF.Square), r=[k(s['go'])], w=[k(s['gsq'])])
        yield
        V(lambda e: e.tensor_reduce(s['grs'][:], s['gsq'][:].rearrange("p (h q) -> p h q", q=64), AX.X, ALU.add), r=[k(s['gsq'])], w=[k(s['grs'])])
        yield
        V(lambda e: e.tensor_scalar(s['grs'][:], s['grs'][:], 1.0 / 64, RMS_EPS, ALU.mult, ALU.add), r=[k(s['grs'])], w=[k(s['grs'])])
        yield
        A(lambda e: e.activation(out=s['grs'][:], in_=s['grs'][:], func=AF.Sqrt), r=[k(s['grs'])], w=[k(s['grs'])])
        yield
        V(lambda e: e.reciprocal(s['grs'][:], s['grs'][:]), r=[k(s['grs'])], w=[k(s['grs'])])
        yield
        V(lambda e: e.tensor_tensor(s['go'][:].rearrange("p (h q) -> p h q", q=64), s['go'][:].rearrange("p (h q) -> p h q", q=64),
                                    self.bc(s['grs'][:, :], [128, 4, 64], 2), ALU.mult), r=[k(s['go']), k(s['grs'])], w=[k(s['go'])])
        yield
        V(lambda e: e.tensor_tensor(s['ycat'][:, 768:1024], s['go'][:], s['gsg'][:], ALU.mult), r=[k(s['go']), k(s['gsg'])], w=[k(s['ycat'])])
        yield

    def rwkv_tile(self, first):
        s, p, c = self.s, self.p, self.c
        V, A, G, M = self.V, self.A, self.G, self.M
        k = lambda t: t.key
        f2 = lambda t, a, b: t[:, a:b, :].rearrange("p a b -> p (a b)")
        h3 = lambda ap: ap.rearrange("p (h q) -> p h q", q=64)
        if first:
            G(lambda e: e.memset(s['rw'][:, :, 0:1], 0.0), w=[k(s['rw'])])
            yield
            G(lambda e: e.memset(s['rST'][:], 0.0), w=[k(s['rST'])])
            yield
            G(lambda e: e.memset(s['rSTb'][:], 0.0), w=[k(s['rSTb'])])
            yield
        else:
            G(lambda e: e.tensor_copy(s['rw'][:, :, 0:1], s['rw'][:, :, 128:129]), r=[k(s['rw'])], w=[k(s['rw'])])
            yield
        for grp, nb in ((0, 4), (4, 3)):
            bx, kx = self.bank('r')
            for j in range(nb):
                self.proj_fm(bx[:, j * 128:(j + 1) * 128], kx, O_RW + (grp + j) * 128, 128)
            A(lambda e, bx=bx, grp=grp, nb=nb: e.copy(out=s['rw'][:, grp:grp + nb, 1:129], in_=bx[:, 0:nb * 128].rearrange("p (a b) -> p a b", b=128)),
              r=[kx], w=[k(s['rw'])])
            yield
        rt1 = s['rt1'][:]
        G(lambda e: e.tensor_tensor(rt1, s['rw'][:, :, 0:128], self.bc(p['mu'][:, :], [128, 7, 128], 2), ALU.mult), r=[k(s['rw']), k(p['mu'])], w=[k(s['rt1'])])
        yield
        V(lambda e: e.tensor_tensor(s['rsh'][:], s['rw'][:, :, 1:129], self.bc(p['omu'][:, :], [128, 7, 128], 2), ALU.mult), r=[k(s['rw']), k(p['omu'])], w=[k(s['rsh'])])
        yield
        V(lambda e: e.tensor_tensor(s['rsh'][:], s['rsh'][:], rt1, ALU.add), r=[k(s['rsh']), k(s['rt1'])], w=[k(s['rsh'])])
        yield
        rT, kT, vT, lr = f2(s['rsh'], 0, 2), f2(s['rsh'], 2, 4), f2(s['rsh'], 4, 6), s['rsh'][:, 6, :]
        ksh = k(s['rsh'])
        A(lambda e: e.activation(out=s['rtw'][:], in_=lr, func=AF.Tanh), r=[ksh], w=[k(s['rtw'])])
        yield
        bw, kw = self.bank('r')
        for b in range(2):
            M(lambda e, b=b: e.matmul(bw[:, b * 128:(b + 1) * 128], lhsT=p['w2p'][:, b * 128:(b + 1) * 128], rhs=s['rtw'][:], start=True, stop=True),
              r=[k(p['w2p']), k(s['rtw'])], w=[kw])
        for b in range(2):
            A(lambda e, b=b: e.activation(out=s['ra1'][:, b * 128:(b + 1) * 128], in_=bw[:, b * 128:(b + 1) * 128], func=AF.Identity, bias=p['w0'][:, b:b + 1]),
              r=[kw, k(p['w0'])], w=[k(s['ra1'])])
            yield
        V(lambda e: e.scalar_tensor_tensor(s['ra2'][:], s['ra1'][:], -1.0, s['ra1'][:], ALU.mult, ALU.max), r=[k(s['ra1'])], w=[k(s['ra2'])])
        yield
        A(lambda e: e.activation(out=s['ra2'][:], in_=s['ra2'][:], func=AF.Exp, scale=-1.0), r=[k(s['ra2'])], w=[k(s['ra2'])])
        yield
        A(lambda e: e.activation(out=s['ra2'][:], in_=s['ra2'][:], func=AF.Ln, bias=1.0), r=[k(s['ra2'])], w=[k(s['ra2'])])
        yield
        V(lambda e: e.tensor_scalar(s['ra3'][:], s['ra1'][:], -1.0, 0.0, ALU.mult, ALU.max), r=[k(s['ra1'])], w=[k(s['ra3'])])
        yield
        V(lambda e: e.tensor_tensor(s['ra3'][:], s['ra3'][:], s['ra2'][:], ALU.add), r=[k(s['ra3']), k(s['ra2'])], w=[k(s['ra3'])])
        yield
        A(lambda e: e.activation(out=s['ra1'][:], in_=s['ra3'][:], func=AF.Exp, scale=-1.0), r=[k(s['ra3'])], w=[k(s['ra1'])])
        yield
        V(lambda e: e.tensor_scalar(s['ra1'][:], s['ra1'][:], -float(np.exp(-0.5)), None, ALU.mult), r=[k(s['ra1'])], w=[k(s['ra1'])])
        yield
        V(lambda e: e.tensor_tensor_scan(s['rcw'][:], c['rmask'][:], s['ra1'][:], 0.0, ALU.mult, ALU.add), r=[k(c['rmask']), k(s['ra1'])], w=[k(s['rcw'])])
        yield
        V(lambda e: e.tensor_tensor(s['ra2'][:], s['rcw'][:], s['ra1'][:], ALU.subtract), r=[k(s['rcw']), k(s['ra1'])], w=[k(s['ra2'])])
        yield
        A(lambda e: e.activation(out=s['recw'][:], in_=s['rcw'][:], func=AF.Exp), r=[k(s['rcw'])], w=[k(s['recw'])])
        yield
        A(lambda e: e.activation(out=s['reicw'][:], in_=s['rcw'][:], func=AF.Exp, scale=-1.0), r=[k(s['rcw'])], w=[k(s['reicw'])])
        yield
        A(lambda e: e.activation(out=s['recwp'][:], in_=s['ra2'][:], func=AF.Exp), r=[k(s['ra2'])], w=[k(s['recwp'])])
        yield
        ba_, ka_ = self.bank('r')
        for b in range(2):
            M(lambda e, b=b: e.matmul(ba_[:, b * 128:(b + 1) * 128], lhsT=p['a2p'][:, b * 128:(b + 1) * 128], rhs=lr, start=True, stop=True),
              r=[k(p['a2p']), ksh], w=[ka_])
        for b in range(2):
            A(lambda e, b=b: e.activation(out=s['ra'][:, b * 128:(b + 1) * 128], in_=ba_[:, b * 128:(b + 1) * 128], func=AF.Sigmoid, bias=p['a0'][:, b:b + 1]),
              r=[ka_, k(p['a0'])], w=[k(s['ra'])])
            yield
        A(lambda e: e.activation(out=s['rsg'][:], in_=lr, func=AF.Sigmoid), r=[ksh], w=[k(s['rsg'])])
        yield
        bg, kg = self.bank('r')
        M(lambda e: e.matmul(bg[:, 0:256], lhsT=s['rsg'][:], rhs=p['g2p'][:], start=True, stop=True), r=[k(s['rsg']), k(p['g2p'])], w=[kg])
        A(lambda e: e.copy(out=s['rg'][:], in_=bg[:, 0:256]), r=[kg], w=[k(s['rg'])])
        yield
        V(lambda e: e.tensor_tensor(s['rkk'][:].rearrange("p (a b) -> p a b", b=128), kT.rearrange("p (a b) -> p a b", b=128), self.bc(p['kk'][:, :], [128, 2, 128], 2), ALU.mult),
          r=[ksh, k(p['kk'])], w=[k(s['rkk'])])
        yield
        A(lambda e: e.activation(out=s['ra2'][:], in_=s['rkk'][:], func=AF.Square), r=[k(s['rkk'])], w=[k(s['ra2'])])
        yield
        bn, kn = self.bank('r')
        for b in range(2):
            M(lambda e, b=b: e.matmul(bn[:, b * 128:(b + 1) * 128], lhsT=c['bones'][:], rhs=s['ra2'][:, b * 128:(b + 1) * 128], start=True, stop=True),
              r=[k(c['bones']), k(s['ra2'])], w=[kn])
        V(lambda e: e.tensor_scalar(s['ra3'][:], bn[:, 0:256], 1e-12, None, ALU.add), r=[kn], w=[k(s['ra3'])])
        yield
        A(lambda e: e.activation(out=s['ra3'][:], in_=s['ra3'][:], func=AF.Sqrt), r=[k(s['ra3'])], w=[k(s['ra3'])])
        yield
        V(lambda e: e.reciprocal(s['ra3'][:], s['ra3'][:]), r=[k(s['ra3'])], w=[k(s['ra3'])])
        yield
        V(lambda e: e.tensor_tensor(s['rkk'][:], s['rkk'][:], s['ra3'][:], ALU.mult), r=[k(s['rkk']), k(s['ra3'])], w=[k(s['rkk'])])
        yield
        for b in range(2):
            V(lambda e, b=b: e.tensor_scalar(s['ra2'][:, b * 128:(b + 1) * 128], s['ra'][:, b * 128:(b + 1) * 128], p['ka'][:, b:b + 1], p['omka'][:, b:b + 1], ALU.mult, ALU.add),
              r=[k(s['ra']), k(p['ka']), k(p['omka'])], w=[k(s['ra2'])])
            yield
        V(lambda e: e.tensor_tensor(s['rkp'][:], kT, s['ra2'][:], ALU.mult), r=[ksh, k(s['ra2'])], w=[k(s['rkp'])])
        yield
        V(lambda e: e.tensor_tensor(s['ra3'][:], s['rkk'][:], s['ra'][:], ALU.mult), r=[k(s['rkk']), k(s['ra'])], w=[k(s['ra3'])])
        yield
        for hh in range(2):
            V(lambda e, hh=hh: e.scalar_tensor_tensor(s['rAm'][:, hh, :], s['rkk'][:], c['nhm'][:, hh:hh + 1], s['recwp'][:], ALU.mult, ALU.mult),
              r=[k(s['rkk']), k(c['nhm']), k(s['recwp'])], w=[k(s['rAm'])])
            yield
            V(lambda e, hh=hh: e.scalar_tensor_tensor(s['rRm'][:, hh, :], rT, c['hm'][:, hh:hh + 1], s['recw'][:], ALU.mult, ALU.mult),
              r=[ksh, k(c['hm']), k(s['recw'])], w=[k(s['rRm'])])
            yield
        G(lambda e: e.tensor_tensor(s['rBt'][:], s['ra3'][:], s['reicw'][:], ALU.mult), r=[k(s['ra3']), k(s['reicw'])], w=[k(s['rBt'])])
        yield
        G(lambda e: e.tensor_tensor(s['rKt'][:], s['rkp'][:], s['reicw'][:], ALU.mult), r=[k(s['rkp']), k(s['reicw'])], w=[k(s['rKt'])])
        yield
        V(lambda e: e.tensor_tensor(s['ra2'][:], rT, s['rkp'][:], ALU.mult), r=[ksh, k(s['rkp'])], w=[k(s['ra2'])])
        yield
        V(lambda e: e.tensor_tensor(s['ra2'][:].rearrange("p (a b) -> p a b", b=128), s['ra2'][:].rearrange("p (a b) -> p a b", b=128), self.bc(p['rk'][:, :], [128, 2, 128], 2), ALU.mult),
          r=[k(s['ra2']), k(p['rk'])], w=[k(s['ra2'])])
        yield
        bb, kb = self.bank('r')
        for b in range(2):
            M(lambda e, b=b: e.matmul(bb[:, b * 2:(b + 1) * 2], lhsT=s['ra2'][:, b * 128:(b + 1) * 128], rhs=c['hm'][:, :], start=True, stop=True),
              r=[k(s['ra2']), k(c['hm'])], w=[kb])
        A(lambda e: e.copy(out=s['rbc'][:], in_=bb[:, 0:4]), r=[kb], w=[k(s['rbc'])])
        yield
        bt, kt = self.bank('r')
        for b in range(2):
            M(lambda e, b=b: e.transpose(bt[:, b * 128:(b + 1) * 128], s['rsh'][:, 4 + b, :], c['identf'][:]), r=[ksh, k(c['identf'])], w=[kt])
        A(lambda e: e.copy(out=s['rvtm'][:], in_=bt[:, 0:256]), r=[kt], w=[k(s['rvtm'])])
        yield
        bt, kt = self.bank('r')
        for cc in range(2):
            for b in range(2):
                M(lambda e, bt=bt, cc=cc, b=b: e.transpose(bt[0:64, cc * 256 + b * 128:cc * 256 + (b + 1) * 128], s['rsh'][:, 4 + b, cc * 64:(cc + 1) * 64], c['identf'][:]),
                  r=[ksh, k(c['identf'])], w=[kt])
        A(lambda e, bt=bt: e.copy(out=s['rvc'][:].rearrange("p a b -> p (a b)"), in_=bt[0:64, :]), r=[kt], w=[k(s['rvc'])])
        yield
        for srct, dst in ((s['rBt'], s['rBc']), (s['rKt'], s['rKc'])):
            bt, kt = self.bank('r')
            pbt = bt[:].bitcast(BF16)
            for cc in range(2):
                for b in range(2):
                    M(lambda e, pbt=pbt, srct=srct, cc=cc, b=b: e.transpose(pbt[0:64, cc * 256 + b * 128:cc * 256 + (b + 1) * 128], srct[:, b * 128 + cc * 64:b * 128 + (cc + 1) * 64], c['identb'][:]),
                      r=[k(srct), k(c['identb'])], w=[kt])
            V(lambda e, pbt=pbt, dst=dst: e.tensor_copy(dst[:].rearrange("p a b -> p (a b)"), pbt[0:64, 0:512]), r=[kt], w=[k(dst)])
            yield
        def amat(lt, lkey, lhh, rt_, rkey, rhh, mask, dst):
            bA, kA = self.bank('r')
            for cc in range(2):
                for h in range(4):
                    b, hh = h // 2, h % 2
                    sl_ = slice(b * 128 + cc * 64, b * 128 + (cc + 1) * 64)
                    la = lt[:, hh, sl_] if lhh else lt[:, sl_]
                    ra_ = rt_[:, hh, sl_] if rhh else rt_[:, sl_]
                    i8 = cc * 4 + h
                    M(lambda e, bA=bA, la=la, ra_=ra_, i8=i8: e.matmul(bA[0:64, i8 * 64:(i8 + 1) * 64], lhsT=la, rhs=ra_, start=True, stop=True), r=[lkey, rkey], w=[kA])
            V(lambda e, bA=bA: e.tensor_tensor(dst[:], h3(bA[0:64, :]), self.bc(mask, [64, 8, 64], 1), ALU.mult), r=[kA, k(c['su'])], w=[k(dst)])
            yield
        kAm, kRm, kBt, kKt = k(s['rAm']), k(s['rRm']), k(s['rBt']), k(s['rKt'])
        yield from amat(s['rAm'], kAm, True, s['rBt'], kBt, False, c['su'][0:64, 0:64], s['rP'])
        yield from amat(s['rBt'], kBt, False, s['rAm'], kAm, True, c['sl'][0:64, 0:64], s['rQ'])
        yield from amat(s['rKt'], kKt, False, s['rAm'], kAm, True, c['sl'][0:64, 0:64], s['rAak'])
        yield from amat(s['rBt'], kBt, False, s['rRm'], kRm, True, c['tri'][0:64, 0:64], s['rArb'])
        yield from amat(s['rKt'], kKt, False, s['rRm'], kRm, True, c['tri'][0:64, 0:64], s['rArk'])
        V(lambda e: e.tensor_tensor(s['rTT'][:], s['rQ'][:], self.bc(c['identf'][0:64, 0:64], [64, 8, 64], 1), ALU.add), r=[k(s['rQ']), k(c['identf'])], w=[k(s['rTT'])])
        yield
        Pc, Qc, Pn, Qn = s['rP'], s['rQ'], s['rP2'], s['rQ2']
        for lvl in range(5):
            bP, kP = self.bank('r')
            for i8 in range(8):
                M(lambda e, bP=bP, i8=i8, Pc=Pc, Qc=Qc: e.matmul(bP[0:64, i8 * 64:(i8 + 1) * 64], lhsT=Qc[:, i8, :], rhs=Pc[:, i8, :], start=True, stop=True),
                  r=[k(Pc), k(Qc)], w=[kP])
            A(lambda e, bP=bP, Pn=Pn: e.copy(out=Pn[:], in_=h3(bP[0:64, :])), r=[kP], w=[k(Pn)])
            yield
            if lvl < 4:
                bQ, kQ = self.bank('r')
                for i8 in range(8):
                    M(lambda e, bQ=bQ, i8=i8, Pc=Pc, Qc=Qc: e.matmul(bQ[0:64, i8 * 64:(i8 + 1) * 64], lhsT=Pc[:, i8, :], rhs=Qc[:, i8, :], start=True, stop=True),
                      r=[k(Pc), k(Qc)], w=[kQ])
                V(lambda e, bQ=bQ, Qn=Qn: e.tensor_copy(Qn[:], h3(bQ[0:64, :])), r=[kQ], w=[k(Qn)])
                yield
            bT, kT_ = self.bank('r')
            for i8 in range(8):
                M(lambda e, bT=bT, i8=i8, Pn=Pn: e.matmul(bT[0:64, i8 * 64:(i8 + 1) * 64], lhsT=Pn[:, i8, :], rhs=s['rTT'][:, i8, :], start=True, stop=True),
                  r=[k(Pn), k(s['rTT'])], w=[kT_])
            V(lambda e, bT=bT: e.tensor_tensor(s['rTT'][:], s['rTT'][:], h3(bT[0:64, :]), ALU.add), r=[k(s['rTT']), kT_], w=[k(s['rTT'])])
            yield
            Pc, Qc, Pn, Qn = Pn, Qn, Pc, Qc
        bG, kG = self.bank('r')
        for cc in range(2):
            for h in range(4):
                i8 = cc * 4 + h
                M(lambda e, cc=cc, h=h, i8=i8: e.matmul(bG[0:64, i8 * 64:(i8 + 1) * 64], lhsT=s['rAak'][:, i8, :], rhs=s['rvc'][:, cc, h * 64:(h + 1) * 64], start=True, stop=True),
                  r=[k(s['rAak']), k(s['rvc'])], w=[kG])
        A(lambda e: e.copy(out=s['rAak'][:], in_=h3(bG[0:64, :])), r=[kG], w=[k(s['rAak'])])
        yield
        ewc = s['recw'][:].rearrange("p (a b) -> p a b", b=64)[:, :, 63]
        for cc in range(2):
            bG1, kG1 = self.bank('r')
            for h in range(4):
                b, hh = h // 2, h % 2
                sl_ = slice(b * 128 + cc * 64, b * 128 + (cc + 1) * 64)
                M(lambda e, bG1=bG1, h=h, b=b, hh=hh, sl_=sl_: e.matmul(bG1[0:64, h * 64:(h + 1) * 64], lhsT=s['rAm'][:, hh, sl_], rhs=s['rSTb'][:, b, :], start=True, stop=True),
                  r=[kAm, k(s['rSTb'])], w=[kG1])
            bY1, kY1 = self.bank('r')
            for h in range(4):
                b, hh = h // 2, h % 2
                sl_ = slice(b * 128 + cc * 64, b * 128 + (cc + 1) * 64)
                M(lambda e, bY1=bY1, h=h, b=b, hh=hh, sl_=sl_, cc=cc: e.matmul(bY1[cc * 64:(cc + 1) * 64, h * 64:(h + 1) * 64], lhsT=s['rRm'][:, hh, sl_], rhs=s['rSTb'][:, b, :], start=True, stop=True),
                  r=[kRm, k(s['rSTb'])], w=[kY1])
            A(lambda e, bY1=bY1, cc=cc: e.copy(out=s['rY1'][cc * 64:(cc + 1) * 64, :], in_=bY1[cc * 64:(cc + 1) * 64, 0:256]), r=[kY1], w=[k(s['rY1'])])
            yield
            V(lambda e, bG1=bG1, cc=cc: e.tensor_tensor(s['rG'][:], s['rAak'][:, cc * 4:(cc + 1) * 4, :], h3(bG1[0:64, 0:256]), ALU.add), r=[k(s['rAak']), kG1], w=[k(s['rG'])])
            yield
            bU, kU = self.bank('r')
            for h in range(4):
                i8 = cc * 4 + h
                M(lambda e, bU=bU, h=h, i8=i8: e.matmul(bU[0:64, h * 64:(h + 1) * 64], lhsT=s['rTT'][:, i8, :], rhs=s['rG'][:, h, :], start=True, stop=True),
                  r=[k(s['rTT']), k(s['rG'])], w=[kU])
            A(lambda e, bU=bU: e.copy(out=s['rU'][:], in_=h3(bU[0:64, 0:256])), r=[kU], w=[k(s['rU'])])
            yield
            bY2, kY2 = self.bank('r')
            for h in range(4):
                i8 = cc * 4 + h
                M(lambda e, bY2=bY2, h=h, i8=i8, cc=cc: e.matmul(bY2[cc * 64:(cc + 1) * 64, h * 64:(h + 1) * 64], lhsT=s['rArb'][:, i8, :], rhs=s['rU'][:, h, :], start=True, stop=False),
                  r=[k(s['rArb']), k(s['rU'])], w=[kY2])
                M(lambda e, bY2=bY2, h=h, i8=i8, cc=cc: e.matmul(bY2[cc * 64:(cc + 1) * 64, h * 64:(h + 1) * 64], lhsT=s['rArk'][:, i8, :], rhs=s['rvc'][:, cc, h * 64:(h + 1) * 64], start=False, stop=True),
                  r=[k(s['rArk']), k(s['rvc'])], w=[kY2])
            V(lambda e, bY2=bY2, cc=cc: e.tensor_tensor(s['rY'][cc * 64:(cc + 1) * 64, :], s['rY1'][cc * 64:(cc + 1) * 64, :], bY2[cc * 64:(cc + 1) * 64, 0:256], ALU.add),
              r=[k(s['rY1']), kY2], w=[k(s['rY'])])
            yield
            bS, kS = self.bank('r')
            for h in range(4):
                b, hh = h // 2, h % 2
                i8 = cc * 4 + h
                M(lambda e, bS=bS, h=h, b=b, hh=hh, cc=cc: e.matmul(bS[hh * 64:(hh + 1) * 64, b * 64:(b + 1) * 64], lhsT=s['rBc'][:, cc, h * 64:(h + 1) * 64], rhs=s['rU'][:, h, :], start=True, stop=False),
                  r=[k(s['rBc']), k(s['rU'])], w=[kS])
                M(lambda e, bS=bS, h=h, b=b, hh=hh, cc=cc: e.matmul(bS[hh * 64:(hh + 1) * 64, b * 64:(b + 1) * 64], lhsT=s['rKc'][:, cc, h * 64:(h + 1) * 64], rhs=s['rvc'][:, cc, h * 64:(h + 1) * 64], start=False, stop=True),
                  r=[k(s['rKc']), k(s['rvc'])], w=[kS])
            V(lambda e, bS=bS: e.tensor_tensor(s['rt2'][:], s['rST'][:], h3(bS[:, 0:128]), ALU.add), r=[k(s['rST']), kS], w=[k(s['rt2'])])
            yield
            V(lambda e, cc=cc: e.tensor_tensor(s['rST'][:], s['rt2'][:], self.bc(ewc[:, cc::2], [128, 2, 64], 2), ALU.mult), r=[k(s['rt2']), k(s['recw'])], w=[k(s['rST'])])
            yield
            A(lambda e: e.copy(out=s['rSTb'][:], in_=s['rST'][:]), r=[k(s['rST'])], w=[k(s['rSTb'])])
            yield
        V(lambda e: e.tensor_reduce(s['rm1'][:], h3(s['rY'][:]), AX.X, ALU.add), r=[k(s['rY'])], w=[k(s['rm1'])])
        yield
        A(lambda e: e.activation(out=s['rY1'][:], in_=s['rY'][:], func=AF.Square), r=[k(s['rY'])], w=[k(s['rY1'])])
        yield
        V(lambda e: e.tensor_reduce(s['rm2'][:], h3(s['rY1'][:]), AX.X, ALU.add), r=[k(s['rY1'])], w=[k(s['rm2'])])
        yield
        V(lambda e: e.tensor_scalar(s['rm1'][:], s['rm1'][:], 1.0 / 64, None, ALU.mult), r=[k(s['rm1'])], w=[k(s['rm1'])])
        yield
        V(lambda e: e.tensor_tensor(s['rvar'][:], s['rm1'][:], s['rm1'][:], ALU.mult), r=[k(s['rm1'])], w=[k(s['rvar'])])
        yield
        V(lambda e: e.scalar_tensor_tensor(s['rvar'][:], s['rm2'][:], 1.0 / 64, s['rvar'][:], ALU.mult, ALU.subtract), r=[k(s['rm2']), k(s['rvar'])], w=[k(s['rvar'])])
        yield
        V(lambda e: e.tensor_scalar(s['rvar'][:], s['rvar'][:], GN_EPS, None, ALU.add), r=[k(s['rvar'])], w=[k(s['rvar'])])
        yield
        A(lambda e: e.activation(out=s['rvar'][:], in_=s['rvar'][:], func=AF.Sqrt), r=[k(s['rvar'])], w=[k(s['rvar'])])
        yield
        V(lambda e: e.reciprocal(s['rvar'][:], s['rvar'][:]), r=[k(s['rvar'])], w=[k(s['rvar'])])
        yield
        V(lambda e: e.tensor_tensor(h3(s['rY'][:]), h3(s['rY'][:]), self.bc(s['rm1'][:, :], [128, 4, 64], 2), ALU.subtract), r=[k(s['rY']), k(s['rm1'])], w=[k(s['rY'])])
        yield
        V(lambda e: e.tensor_tensor(h3(s['rY'][:]), h3(s['rY'][:]), self.bc(s['rvar'][:, :], [128, 4, 64], 2), ALU.mult), r=[k(s['rY']), k(s['rvar'])], w=[k(s['rY'])])
        yield
        G(lambda e: e.tensor_tensor(s['rY'][:], s['rY'][:], p['rlg'][:], ALU.mult), r=[k(s['rY']), k(p['rlg'])], w=[k(s['rY'])])
        yield
        G(lambda e: e.tensor_tensor(s['rY'][:], s['rY'][:], p['rlb'][:], ALU.add), r=[k(s['rY']), k(p['rlb'])], w=[k(s['rY'])])
        yield
        V(lambda e: e.tensor_tensor(h3(s['rY1'][:]), h3(s['rvtm'][:]), self.bc(s['rbc'][:, :], [128, 4, 64], 2), ALU.mult), r=[k(s['rvtm']), k(s['rbc'])], w=[k(s['rY1'])])
        yield
        V(lambda e: e.tensor_tensor(s['rY'][:], s['rY'][:], s['rY1'][:], ALU.add), r=[k(s['rY']), k(s['rY1'])], w=[k(s['rY'])])
        yield
        V(lambda e: e.tensor_tensor(s['ycat'][:, 512:768], s['rY'][:], s['rg'][:], ALU.mult), r=[k(s['rY']), k(s['rg'])], w=[k(s['ycat'])])
        yield

    def mixer_epilogue(self, l, i):
        s, p, c = self.s, self.p, self.c
        V, A, G, M = self.V, self.A, self.G, self.M
        k = lambda t: t.key
        self.load('sp', s['htm'][:], self.h_d[i * 128:(i + 1) * 128, :], k(s['htm']), dkeys=["hd_%d" % i])
        if self.debug:
            self.A(lambda e: e.copy(out=s['tmp'][:], in_=s['ycat'][:]), r=[k(s['ycat'])], w=[k(s['tmp'])])
            yield
            self.store('sp', self.dbg_y[i * 128:(i + 1) * 128, :], s['tmp'][:], k(s['tmp']))
            yield
        bt, kt = self.bank('e')
        pb = bt[:].bitcast(BF16)
        for kc in range(8):
            M(lambda e, kc=kc: e.transpose(pb[:, kc * 128:(kc + 1) * 128], s['ycat'][:, kc * 128:(kc + 1) * 128], c['identb'][:]), r=[k(s['ycat']), k(c['identb'])], w=[kt])
        V(lambda e: e.tensor_copy(s['yT'][:].rearrange("p a b -> p (a b)"), pb), r=[kt], w=[k(s['yT'])])
        yield
        for half in range(2):
            bo, ko = self.bank('e')
            for kc in range(8):
                M(lambda e, bo=bo, kc=kc, half=half: e.matmul(bo[:, :], lhsT=s['yT'][:, kc, :], rhs=p['w_out'][:, kc, half * 512:(half + 1) * 512], start=(kc == 0), stop=(kc == 7)),
                  r=[k(s['yT']), k(p['w_out'])], w=[ko])
            V(lambda e, bo=bo, half=half: e.scalar_tensor_tensor(s['mix'][:, half * 512:(half + 1) * 512], s['htm'][:, half * 512:(half + 1) * 512], ALPHA, bo[:, :], ALU.mult, ALU.add),
              r=[k(s['htm']), ko], w=[k(s['mix'])])
            yield
        self.layernorm(s['mix'], p['l1g'], p['l1b'], s['h1'], s['tmp'])
        yield
        tk = "h1d_%d_%d" % (l, i)
        self.store('sp', self.h1_d[i * 128:(i + 1) * 128, :], s['h1'][:], k(s['h1']), dkeys=[tk])
        yield
        self.store('pool', self.h1b_d[i * 128:(i + 1) * 128, :], s['h1'][:], k(s['h1']), dkeys=["h1bd_%d_%d" % (l, i)])
        yield
        if hasattr(self, 'xs_d'):
            yield from self.router_tile(l, i)

    def stage0(self):
        s, p, d = self.s, self.p, self.d
        k = lambda t: t.key
        self.load('sp', p['l1g'][:], d['ln_in_g'].ap().partition_broadcast(128), k(p['l1g']))
        self.load('sp', p['l1b'][:], d['ln_in_b'].ap().partition_broadcast(128), k(p['l1b']))
        for i in range(self.NT):
            self.load('sp', s['mix'][:], d['x'][i * 128:(i + 1) * 128, :], k(s['mix']))
            self.layernorm(s['mix'], p['l1g'], p['l1b'], s['htm'], s['tmp'])
            self.store('sp', self.h_d[i * 128:(i + 1) * 128, :], s['htm'][:], k(s['htm']), dkeys=["hd_%d" % i])
            self.to_fm(s['htm'], s['ycat'], s['hT'])
            self.store('sp', self.hT_d[:, :, i * 128:(i + 1) * 128], s['hT'][:], k(s['hT']), dkeys=["hTd_%d" % i])

    def stageM(self, l):
        import os
        s = self.s
        k = lambda t: t.key
        only = os.environ.get("ONLY", "srg")
        prev = None
        for i in range(self.NT + 1):
            gens = []
            if i < self.NT:
                self.load('sp', s['hT'][:], self.hT_d[:, :, i * 128:(i + 1) * 128], k(s['hT']), dkeys=["hTd_%d" % i])
                if 'r' in only:
                    gens.append(self.rwkv_tile(i == 0))
                if 's' in only:
                    gens.append(self.ssd_tile(i == 0))
                if 'g' in only:
                    gens.append(self.gla_tile(i == 0))
            if prev is not None:
                gens.append(self.mixer_epilogue(l, prev))
            prev = i if i < self.NT else None
            wts = [int(x) for x in os.environ.get("ILW", "3,1,1,1").split(",")]
            gw = {id(g_): (wts[0] if j == 0 and i < self.NT and 'r' in only else 1) for j, g_ in enumerate(gens)}
            while gens:
                for g_ in list(gens):
                    for _ in range(gw[id(g_)]):
                        try:
                            next(g_)
                        except StopIteration:
                            gens.remove(g_)
                            break

    def build_mixer_test(self):
        self.declare_inputs()
        T = self.T
        self.h_d = self.dscr("h_d", [T, D])
        self.hT_d = self.dscr("hT_d", [128, 8, T], BF16)
        self.h1_d = self.dout("h1_d", [T, D])
        self.h1b_d = self.dscr("h1b_d", [T, D], BF16)
        self.dbg_y = self.dout("dbg_y", [T, D])
        self.consts()
        self.alloc_params()
        self.alloc_mixer()
        print("sbuf peak", self.sb_peak)
        self.stage0()
        self.P.barrier()
        self.load_params(0)
        self.stageM(0)
        self.P.barrier()
        return self.nc

    def alloc_router(self):
        rt = self.rt = {}
        for n, w in (('lg', 36), ('gmx', 1), ('goh', 4), ('gex', 4), ('gsum', 1), ('t32', 32), ('el8', 8), ('el8m', 8), ('l1', 1), ('l2', 1),
                     ('oh1', 8), ('oh2', 8), ('w1', 1), ('w2', 1), ('E1', 32), ('E2', 32), ('Mm', 32), ('rk', 32)):
            rt[n] = self.sb([128, w], name="rt_" + n)

    def alloc_route_persist(self):
        rp = self.rp = {}
        NT = self.NT
        rp['eid'] = self.sb([128, NT * 2], name="rp_eid")
        rp['rnk'] = self.sb([128, NT * 2], name="rp_rnk")
        rp['gat'] = self.sb([128, NT * 2], name="rp_gat")
        rp['cnt'] = self.sb([128, NE], name="rp_cnt")
        rp['iota32'] = self.sb([128, NE], name="rp_iota32")
        ii = self.sb([128, NE], I32, "rp_iota32i")
        self.G(lambda e: e.iota(ii[:], pattern=[[1, NE]], base=0, channel_multiplier=0), w=[ii.key])
        self.V(lambda e: e.tensor_copy(rp['iota32'][:], ii[:]), r=[ii.key], w=[rp['iota32'].key])

    def router_tile(self, l, i):
        s, p, c = self.s, self.p, self.c
        V, A, G, M = self.V, self.A, self.G, self.M
        k = lambda t: t.key
        rt, rp = self.rt, self.rp
        if i == 0:
            G(lambda e: e.memset(rp['cnt'][:], 0.0), w=[k(rp['cnt'])])
            yield
        hT32 = s['tmp'][:].rearrange("p (a b) -> p a b", b=128)
        for half in range(2):
            bt, kt = self.bank('e')
            for j in range(4):
                kc = half * 4 + j
                M(lambda e, bt=bt, j=j, kc=kc: e.transpose(bt[:, j * 128:(j + 1) * 128], s['h1'][:, kc * 128:(kc + 1) * 128], c['identf'][:]), r=[k(s['h1']), k(c['identf'])], w=[kt])
            A(lambda e, bt=bt, half=half: e.copy(out=s['tmp'][:, half * 512:(half + 1) * 512], in_=bt[:, :]), r=[kt], w=[k(s['tmp'])])
            yield
        bl, kl = self.bank('e')
        for kc in range(8):
            M(lambda e, kc=kc: e.matmul(bl[:, 0:36], lhsT=hT32[:, kc, :], rhs=p['wr'][:, kc, :], start=(kc == 0), stop=(kc == 7)), r=[k(s['tmp']), k(p['wr'])], w=[kl])
        V(lambda e: e.tensor_tensor(rt['lg'][:], bl[:, 0:36], p['rb36'][:], ALU.add), r=[kl, k(p['rb36'])], w=[k(rt['lg'])])
        yield
        V(lambda e: e.tensor_reduce(rt['gmx'][:], rt['lg'][:, 0:4], AX.X, ALU.max), r=[k(rt['lg'])], w=[k(rt['gmx'])])
        yield
        V(lambda e: e.tensor_scalar(rt['goh'][:], rt['lg'][:, 0:4], rt['gmx'][:, 0:1], None, ALU.is_equal), r=[k(rt['lg']), k(rt['gmx'])], w=[k(rt['goh'])])
        yield
        V(lambda e: e.tensor_scalar(rt['gex'][:], rt['lg'][:, 0:4], rt['gmx'][:, 0:1], None, ALU.subtract), r=[k(rt['lg']), k(rt['gmx'])], w=[k(rt['gex'])])
        yield
        A(lambda e: e.activation(out=rt['gex'][:], in_=rt['gex'][:], func=AF.Exp), r=[k(rt['gex'])], w=[k(rt['gex'])])
        yield
        V(lambda e: e.tensor_reduce(rt['gsum'][:], rt['gex'][:], AX.X, ALU.add), r=[k(rt['gex'])], w=[k(rt['gsum'])])
        yield
        V(lambda e: e.reciprocal(rt['gsum'][:], rt['gsum'][:]), r=[k(rt['gsum'])], w=[k(rt['gsum'])])
        yield
        V(lambda e: e.tensor_tensor(rt['t32'][:].rearrange("p (g j) -> p g j", j=8), rt['lg'][:, 4:36].rearrange("p (g j) -> p g j", j=8),
                                    self.bc(rt['goh'][:, :], [128, 4, 8], 2), ALU.mult), r=[k(rt['lg']), k(rt['goh'])], w=[k(rt['t32'])])
        yield
        V(lambda e: e.tensor_reduce(rt['el8'][:], rt['t32'][:].rearrange("p (g j) -> p j g", j=8), AX.X, ALU.add), r=[k(rt['t32'])], w=[k(rt['el8'])])
        yield
        V(lambda e: e.tensor_reduce(rt['l1'][:], rt['el8'][:], AX.X, ALU.max), r=[k(rt['el8'])], w=[k(rt['l1'])])
        yield
        V(lambda e: e.tensor_scalar(rt['oh1'][:], rt['el8'][:], rt['l1'][:, 0:1], None, ALU.is_equal), r=[k(rt['el8']), k(rt['l1'])], w=[k(rt['oh1'])])
        yield
        V(lambda e: e.scalar_tensor_tensor(rt['el8m'][:], rt['oh1'][:], -1e30, rt['el8'][:], ALU.mult, ALU.add), r=[k(rt['oh1']), k(rt['el8'])], w=[k(rt['el8m'])])
        yield
        V(lambda e: e.tensor_reduce(rt['l2'][:], rt['el8m'][:], AX.X, ALU.max), r=[k(rt['el8m'])], w=[k(rt['l2'])])
        yield
        V(lambda e: e.tensor_scalar(rt['oh2'][:], rt['el8m'][:], rt['l2'][:, 0:1], None, ALU.is_equal), r=[k(rt['el8m']), k(rt['l2'])], w=[k(rt['oh2'])])
        yield
        V(lambda e: e.tensor_tensor(rt['w2'][:], rt['l2'][:], rt['l1'][:], ALU.subtract), r=[k(rt['l2']), k(rt['l1'])], w=[k(rt['w2'])])
        yield
        A(lambda e: e.activation(out=rt['w2'][:], in_=rt['w2'][:], func=AF.Exp), r=[k(rt['w2'])], w=[k(rt['w2'])])
        yield
        V(lambda e: e.tensor_scalar(rt['w1'][:], rt['w2'][:], 1.0, None, ALU.add), r=[k(rt['w2'])], w=[k(rt['w1'])])
        yield
        V(lambda e: e.reciprocal(rt['w1'][:], rt['w1'][:]), r=[k(rt['w1'])], w=[k(rt['w1'])])
        yield
        V(lambda e: e.tensor_tensor(rt['w2'][:], rt['w2'][:], rt['w1'][:], ALU.mult), r=[k(rt['w2']), k(rt['w1'])], w=[k(rt['w2'])])
        yield
        V(lambda e: e.tensor_tensor(rp['gat'][:, 2 * i:2 * i + 1], rt['w1'][:], rt['gsum'][:], ALU.mult), r=[k(rt['w1']), k(rt['gsum'])], w=[k(rp['gat'])])
        yield
        V(lambda e: e.tensor_tensor(rp['gat'][:, 2 * i + 1:2 * i + 2], rt['w2'][:], rt['gsum'][:], ALU.mult), r=[k(rt['w2']), k(rt['gsum'])], w=[k(rp['gat'])])
        yield
        for E, oh in ((rt['E1'], rt['oh1']), (rt['E2'], rt['oh2'])):
            V(lambda e, E=E, oh=oh: e.tensor_tensor(E[:].rearrange("p (g j) -> p g j", j=8), self.bc(rt['goh'][:, :], [128, 4, 8], 2), self.bc(oh[:, :], [128, 4, 8], 1), ALU.mult),
              r=[k(rt['goh']), k(oh)], w=[k(E)])
            yield
        V(lambda e: e.tensor_tensor(rt['Mm'][:], rt['E1'][:], rt['E2'][:], ALU.add), r=[k(rt['E1']), k(rt['E2'])], w=[k(rt['Mm'])])
        yield
        br, kr = self.bank('e')
        M(lambda e: e.matmul(br[:, 0:32], lhsT=c['sl'][:], rhs=rt['Mm'][:], start=True, stop=True), r=[k(c['sl']), k(rt['Mm'])], w=[kr])
        M(lambda e: e.matmul(br[:, 32:64], lhsT=c['onesf'][:], rhs=rt['Mm'][:], start=True, stop=True), r=[k(c['onesf']), k(rt['Mm'])], w=[kr])
        V(lambda e: e.tensor_tensor(rt['rk'][:], br[:, 0:32], rp['cnt'][:], ALU.add), r=[kr, k(rp['cnt'])], w=[k(rt['rk'])])
        yield
        V(lambda e: e.tensor_tensor(rp['cnt'][:], rp['cnt'][:], br[:, 32:64], ALU.add), r=[kr, k(rp['cnt'])], w=[k(rp['cnt'])])
        yield
        for j, E in ((0, rt['E1']), (1, rt['E2'])):
            V(lambda e, E=E: e.tensor_tensor(rt['t32'][:], E[:], rt['rk'][:], ALU.mult), r=[k(E), k(rt['rk'])], w=[k(rt['t32'])])
            yield
            V(lambda e, j=j: e.tensor_reduce(rp['rnk'][:, 2 * i + j:2 * i + j + 1], rt['t32'][:], AX.X, ALU.add), r=[k(rt['t32'])], w=[k(rp['rnk'])])
            yield
            V(lambda e, E=E: e.tensor_tensor(rt['t32'][:], E[:], rp['iota32'][:], ALU.mult), r=[k(E), k(rp['iota32'])], w=[k(rt['t32'])])
            yield
            V(lambda e, j=j: e.tensor_reduce(rp['eid'][:, 2 * i + j:2 * i + j + 1], rt['t32'][:], AX.X, ALU.add), r=[k(rt['t32'])], w=[k(rp['eid'])])
            yield

    def stageMoE(self, l, last):
        d, c, rp = self.d, self.c, self.rp
        V, A, G, M = self.V, self.A, self.G, self.M
        k = lambda t: t.key
        mark = self.sb_off
        sb = self.sb
        NT, NB, RB = self.NT, self.NB, self.RB
        NR = RB // 128
        NC = NT * 2
        thr_i = sb([128, 64], I32, "f_thri")
        thr = sb([128, 64], name="f_thr")
        G(lambda e: e.iota(thr_i[:], pattern=[[RB, 64]], base=0, channel_multiplier=0), w=[k(thr_i)])
        V(lambda e: e.tensor_copy(thr[:], thr_i[:]), r=[k(thr_i)], w=[k(thr)])
        big = sb([128, max(NC * NE, NE * 64, NB * NE)], name="f_big")
        nblk = sb([128, NE], name="f_nblk")
        padded = sb([128, NE], name="f_padded")
        pend = sb([128, NE], name="f_pend")
        pstart = sb([128, NE], name="f_pstart")
        cmp3 = big[:, 0:NE * 64].rearrange("p (e m) -> p e m", m=64)
        V(lambda e: e.tensor_tensor(cmp3, self.bc(rp['cnt'][:, :], [128, NE, 64], 2), self.bc(thr[:, :], [128, NE, 64], 1), ALU.is_gt), r=[k(rp['cnt']), k(thr)], w=[k(big)])
        V(lambda e: e.tensor_reduce(nblk[:], cmp3, AX.X, ALU.add), r=[k(big)], w=[k(nblk)])
        V(lambda e: e.tensor_scalar(padded[:], nblk[:], float(RB), None, ALU.mult), r=[k(nblk)], w=[k(padded)])
        V(lambda e: e.tensor_tensor_scan(pend[:], c['onesf'][:, 0:NE], padded[:], 0.0, ALU.mult, ALU.add), r=[k(c['onesf']), k(padded)], w=[k(pend)])
        V(lambda e: e.tensor_tensor(pstart[:], pend[:], padded[:], ALU.subtract), r=[k(pend), k(padded)], w=[k(pstart)])
        oh3 = big[:, 0:NC * NE].rearrange("p (n e) -> p n e", e=NE)
        destf = sb([128, NC], name="f_destf")
        dest = sb([128, NC], I32, "f_dest")
        V(lambda e: e.tensor_tensor(oh3, self.bc(rp['iota32'][:, :], [128, NC, NE], 1), self.bc(rp['eid'][:, :], [128, NC, NE], 2), ALU.is_equal), r=[k(rp['iota32']), k(rp['eid'])], w=[k(big)])
        V(lambda e: e.tensor_tensor(oh3, oh3, self.bc(pstart[:, :], [128, NC, NE], 1), ALU.mult), r=[k(big), k(pstart)], w=[k(big)])
        V(lambda e: e.tensor_reduce(destf[:], oh3, AX.X, ALU.add), r=[k(big)], w=[k(destf)])
        V(lambda e: e.tensor_tensor(destf[:], destf[:], rp['rnk'][:], ALU.add), r=[k(destf), k(rp['rnk'])], w=[k(destf)])
        V(lambda e: e.tensor_copy(dest[:], destf[:]), r=[k(destf)], w=[k(dest)])
        bs_i = sb([128, NB], I32, "f_bsi")
        bstart = sb([128, NB], name="f_bstart")
        be = sb([128, NB], name="f_be")
        G(lambda e: e.iota(bs_i[:], pattern=[[RB, NB]], base=0, channel_multiplier=0), w=[k(bs_i)])
        V(lambda e: e.tensor_copy(bstart[:], bs_i[:]), r=[k(bs_i)], w=[k(bstart)])
        cmpb = big[:, 0:NB * NE].rearrange("p (b e) -> p b e", e=NE)
        V(lambda e: e.tensor_tensor(cmpb, self.bc(pend[:, :], [128, NB, NE], 1), self.bc(bstart[:, :], [128, NB, NE], 2), ALU.is_le), r=[k(pend), k(bstart)], w=[k(big)])
        V(lambda e: e.tensor_reduce(be[:], cmpb, AX.X, ALU.add), r=[k(big)], w=[k(be)])
        V(lambda e: e.tensor_scalar(be[:], be[:], float(NE - 1), None, ALU.min), r=[k(be)], w=[k(be)])
        kp_i = sb([128, 8], I32, "f_kpi")
        kp = sb([128, 8], name="f_kp")
        G(lambda e: e.iota(kp_i[:], pattern=[[128, 8]], base=0, channel_multiplier=1), w=[k(kp_i)])
        V(lambda e: e.tensor_copy(kp[:], kp_i[:]), r=[k(kp_i)], w=[k(kp)])
        widf = sb([128, NB, 8], name="f_widf")
        wid = sb([128, NB, 8], I32, "f_wid")
        didf = sb([128, NB, 4], name="f_didf")
        did = sb([128, NB, 4], I32, "f_did")
        bew = sb([128, NB], name="f_bew")
        V(lambda e: e.tensor_scalar(bew[:], be[:], float(D), float(l * NE * D), ALU.mult, ALU.add), r=[k(be)], w=[k(bew)])
        V(lambda e: e.tensor_tensor(widf[:], self.bc(bew[:, :], [128, NB, 8], 2), self.bc(kp[:, :], [128, NB, 8], 1), ALU.add), r=[k(bew), k(kp)], w=[k(widf)])
        V(lambda e: e.tensor_copy(wid[:], widf[:]), r=[k(widf)], w=[k(wid)])
        V(lambda e: e.tensor_scalar(bew[:], be[:], float(FF), float(l * NE * FF), ALU.mult, ALU.add), r=[k(be)], w=[k(bew)])
        V(lambda e: e.tensor_tensor(didf[:], self.bc(bew[:, :], [128, NB, 4], 2), self.bc(kp[:, 0:4], [128, NB, 4], 1), ALU.add), r=[k(bew), k(kp)], w=[k(didf)])
        V(lambda e: e.tensor_copy(did[:], didf[:]), r=[k(didf)], w=[k(did)])
        hb = sb([128, D], BF16, "e_hb")
        for i in range(NT):
            self.load('sp', hb[:], self.h1b_d[i * 128:(i + 1) * 128, :], k(hb), dkeys=["h1bd_%d_%d" % (l, i)])
            for j in range(2):
                col = 2 * i + j
                self.P.dma('pool', lambda e, col=col: e.indirect_dma_start(out=self.xs_d.ap(), out_offset=bass.IndirectOffsetOnAxis(ap=dest[:, col:col + 1], axis=0),
                                                                           in_=hb[:], in_offset=None), k(hb), r=[k(hb), k(dest)], w=["xs_d"])
        self.P.barrier()
        import os
        mstop = int(os.environ.get('MOESTOP', '9'))
        if mstop <= 1:
            self.sb_off = mark
            return
        wg = [sb([128, 8, FF], BF16, "e_wg%d" % j) for j in range(2)]
        wu = [sb([128, 8, FF], BF16, "e_wu%d" % j) for j in range(2)]
        wd = [sb([128, 4, D], BF16, "e_wd%d" % j) for j in range(2)]
        xs = [sb([128, NR, D], BF16, "e_xs%d" % j) for j in range(2)]
        xsT = sb([128, 8, RB], BF16, "e_xsT")
        hT = sb([128, 4, RB], BF16, "e_hT")
        sg = sb([128, RB], name="e_sg")
        ys = [sb([128, D], name="e_ys%d" % j) for j in range(2)]
        tg, tu, td = d['moe_w_gate'].ap(), d['moe_w_up'].ap(), d['moe_w_down'].ap()
        nys = 0
        for b in range(NB):
            j = b % 2
            for kc in range(8):
                self.P.dma('pool', lambda e, j=j, b=b, kc=kc: e.indirect_dma_start(out=wg[j][:, kc, :], out_offset=None, in_=tg,
                                                                                  in_offset=bass.IndirectOffsetOnAxis(ap=wid[:, b, kc:kc + 1], axis=0)), k(wg[j]), r=[k(wid)], w=[k(wg[j])])
                self.P.dma('pool', lambda e, j=j, b=b, kc=kc: e.indirect_dma_start(out=wu[j][:, kc, :], out_offset=None, in_=tu,
                                                                                  in_offset=bass.IndirectOffsetOnAxis(ap=wid[:, b, kc:kc + 1], axis=0)), k(wu[j]), r=[k(wid)], w=[k(wu[j])])
            for fc in range(4):
                self.P.dma('pool', lambda e, j=j, b=b, fc=fc: e.indirect_dma_start(out=wd[j][:, fc, :], out_offset=None, in_=td,
                                                                                  in_offset=bass.IndirectOffsetOnAxis(ap=did[:, b, fc:fc + 1], axis=0)), k(wd[j]), r=[k(did)], w=[k(wd[j])])
            self.load('sp', xs[j][:], self.xs_d[b * RB:(b + 1) * RB, :].rearrange("(r p) n -> p r n", p=128), k(xs[j]), dkeys=["xs_d"])
            for r_ in range(NR):
                bt, kt = self.bank()
                pb = bt[:].bitcast(BF16)
                for kc in range(8):
                    M(lambda e, pb=pb, j=j, r_=r_, kc=kc: e.transpose(pb[:, kc * 128:(kc + 1) * 128], xs[j][:, r_, kc * 128:(kc + 1) * 128], c['identb'][:]), r=[k(xs[j]), k(c['identb'])], w=[kt])
                V(lambda e, pb=pb, r_=r_: e.tensor_copy(xsT[:, :, r_ * 128:(r_ + 1) * 128], pb.rearrange("p (a b) -> p a b", b=128)), r=[kt], w=[k(xsT)])
            for fc in range(4):
                bg, kg = self.bank()
                for kc in range(8):
                    M(lambda e, bg=bg, kc=kc, fc=fc, j=j: e.matmul(bg[:, 0:RB], lhsT=wg[j][:, kc, fc * 128:(fc + 1) * 128], rhs=xsT[:, kc, :], start=(kc == 0), stop=(kc == 7)),
                      r=[k(wg[j]), k(xsT)], w=[kg])
                bu, ku = self.bank()
                for kc in range(8):
                    M(lambda e, bu=bu, kc=kc, fc=fc, j=j: e.matmul(bu[:, 0:RB], lhsT=wu[j][:, kc, fc * 128:(fc + 1) * 128], rhs=xsT[:, kc, :], start=(kc == 0), stop=(kc == 7)),
                      r=[k(wu[j]), k(xsT)], w=[ku])
                A(lambda e, bg=bg: e.activation(out=sg[:], in_=bg[:, 0:RB], func=AF.Silu), r=[kg], w=[k(sg)])
                V(lambda e, bu=bu, fc=fc: e.tensor_tensor(hT[:, fc, :], sg[:], bu[:, 0:RB], ALU.mult), r=[k(sg), ku], w=[k(hT)])
            for r_ in range(NR):
                yb = ys[nys % 2]
                nys += 1
                for half in range(2):
                    bo, ko = self.bank()
                    for fc in range(4):
                        M(lambda e, bo=bo, fc=fc, r_=r_, half=half, j=j: e.matmul(bo[:, :], lhsT=hT[:, fc, r_ * 128:(r_ + 1) * 128], rhs=wd[j][:, fc, half * 512:(half + 1) * 512],
                                                                                 start=(fc == 0), stop=(fc == 3)), r=[k(hT), k(wd[j])], w=[ko])
                    if half == 0:
                        A(lambda e, bo=bo, yb=yb: e.copy(out=yb[:, 0:512], in_=bo[:, :]), r=[ko], w=[k(yb)])
                    else:
                        V(lambda e, bo=bo, yb=yb: e.tensor_copy(yb[:, 512:1024], bo[:, :]), r=[ko], w=[k(yb)])
                r0 = b * RB + r_ * 128
                self.store('sp', self.ys_d[r0:r0 + 128, :], yb[:], k(yb), dkeys=["ys_d"])
        self.P.barrier()
        if mstop <= 2:
            self.sb_off = mark
            return
        h1 = sb([128, D], name="c_h1")
        y0 = sb([128, D], name="c_y0")
        y1 = sb([128, D], name="c_y1")
        tmp = sb([128, D], name="c_tmp")
        h2 = sb([128, D], name="c_h2")
        hb2 = sb([128, D], BF16, "c_hb")
        hTo = sb([128, 8, 128], BF16, "c_hTo")
        l2g = sb([128, D], name="c_l2g")
        l2b = sb([128, D], name="c_l2b")
        self.load('sp', l2g[:], d['ln2_g'][l].partition_broadcast(128), k(l2g))
        self.load('sp', l2b[:], d['ln2_b'][l].partition_broadcast(128), k(l2b))
        for i in range(NT):
            self.load('sp', h1[:], self.h1_d[i * 128:(i + 1) * 128, :], k(h1), dkeys=["h1d_%d_%d" % (l, i)])
            for j, yt in ((0, y0), (1, y1)):
                col = 2 * i + j
                self.P.dma('pool', lambda e, col=col, yt=yt: e.indirect_dma_start(out=yt[:], out_offset=None, in_=self.ys_d.ap(),
                                                                                 in_offset=bass.IndirectOffsetOnAxis(ap=dest[:, col:col + 1], axis=0)), k(yt), r=[k(dest), "ys_d"], w=[k(yt)])
            V(lambda e, i=i: e.tensor_scalar(y0[:], y0[:], rp['gat'][:, 2 * i:2 * i + 1], None, ALU.mult), r=[k(y0), k(rp['gat'])], w=[k(y0)])
            V(lambda e, i=i: e.scalar_tensor_tensor(y0[:], y1[:], rp['gat'][:, 2 * i + 1:2 * i + 2], y0[:], ALU.mult, ALU.add), r=[k(y1), k(y0), k(rp['gat'])], w=[k(y0)])
            V(lambda e: e.scalar_tensor_tensor(h1[:], h1[:], ALPHA, y0[:], ALU.mult, ALU.add), r=[k(h1), k(y0)], w=[k(h1)])
            self.layernorm(h1, l2g, l2b, h2, tmp)
            if last:
                self.store('sp', self.out_d[i * 128:(i + 1) * 128, :], h2[:], k(h2), dkeys=["out_%d" % i])
            else:
                self.store('sp', self.h_d[i * 128:(i + 1) * 128, :], h2[:], k(h2), dkeys=["hd_%d" % i])
                self.to_fm(h2, hb2, hTo)
                self.store('sp', self.hT_d[:, :, i * 128:(i + 1) * 128], hTo[:], k(hTo), dkeys=["hTd_%d" % i])
        self.P.barrier()
        self.sb_off = mark

    def build_full(self):
        self.declare_inputs()
        T = self.T
        self.h_d = self.dscr("h_d", [T, D])
        self.hT_d = self.dscr("hT_d", [128, 8, T], BF16)
        self.h1_d = self.dscr("h1_d", [T, D])
        self.h1b_d = self.dscr("h1b_d", [T, D], BF16)
        self.xs_d = self.dscr("xs_d", [self.NB * self.RB, D], BF16)
        self.ys_d = self.dscr("ys_d", [self.NB * self.RB, D])
        self.out_d = self.dout("out", [T, D])
        if self.debug:
            self.dbg_y = self.dout("dbg_y", [T, D])
        self.consts()
        self.alloc_route_persist()
        base = self.sb_off
        for l in range(self.depth):
            self.sb_off = base
            self.alloc_params()
            self.alloc_mixer()
            self.alloc_router()
            if l == 0:
                self.stage0()
                self.P.barrier()
            self.load_params(l)
            self.stageM(l)
            self.P.barrier()
            self.sb_off = base
            self.stageMoE(l, l == self.depth - 1)
        self.P.barrier()
        return self.nc


def _host_inputs(inputs, b, T):
    m = {}
    for k, v in inputs.items():
        v = np.asarray(v)
        if k == 'x':
            m[k] = np.ascontiguousarray(v[b, :T])
        elif k == 'rwkv_r_k':
            m[k] = np.ascontiguousarray(v.reshape(DEPTH, 256))
        elif k in ('moe_w_gate', 'moe_w_up'):
            m[k] = np.ascontiguousarray(v.reshape(DEPTH * NE * D, FF))
        elif k == 'moe_w_down':
            m[k] = np.ascontiguousarray(v.reshape(DEPTH * NE * FF, D))
        else:
            m[k] = np.ascontiguousarray(v)
    return m


def kernel(**inputs):
    x = np.asarray(inputs['x'])
    Bsz, T, _ = x.shape
    bld = Builder(T)
    nc = bld.build_full()
    in_maps = [_host_inputs(inputs, b, T) for b in range(Bsz)]
    res = run_bass_kernel_spmd(nc, in_maps, core_ids=list(range(Bsz)))
    return np.stack([np.asarray(r["out"]) for r in res.results], axis=0).astype(np.float32)
```

```python
import numpy as np
import concourse.bass as bass
import concourse.mybir as mybir
from concourse.bass_utils import run_bass_kernel_spmd

F32 = mybir.dt.float32
BF16 = mybir.dt.bfloat16
I32 = mybir.dt.int32
U32 = mybir.dt.uint32
AF = mybir.ActivationFunctionType
ALU = mybir.AluOpType
AX = mybir.AxisListType

D = 1024
NIN = 2968
DEPTH = 2
ALPHA = (2 * DEPTH) ** 0.25
LN_EPS = 1e-5
RMS_EPS = 1e-6
GN_EPS = 64e-5
NE = 32
FF = 512
O_Z, O_XBC, O_DT, O_RW, O_GQ, O_GK, O_GV, O_GG, O_GA = 0, 512, 1280, 1288, 2184, 2312, 2440, 2696, 2952


class Prog:
    EPOCH = 8192
    NDMA = 40

    def __init__(self, nc):
        self.nc = nc
        self.eng = {'pe': nc.tensor, 'dve': nc.vector, 'act': nc.scalar, 'pool': nc.gpsimd, 'sp': nc.sync}
        self.esems = {n: [] for n in ('pe', 'dve', 'act', 'pool')}
        self.cnt = {n: 0 for n in ('pe', 'dve', 'act', 'pool')}
        self.dsems, self.dval, self.dkey = [], [], {}
        self.waited = {n: {} for n in self.eng}
        self.lastw, self.readers = {}, {}
        self.ninst = 0

    def _esem(self, X, ep):
        while len(self.esems[X]) <= ep:
            self.esems[X].append(self.nc.alloc_semaphore("s_%s_%d" % (X, len(self.esems[X]))))
        return self.esems[X][ep]

    def _deps(self, reads, writes):
        deps = {}

        def add(ev):
            if ev is not None and deps.get(ev[0], 0) < ev[1]:
                deps[ev[0]] = ev[1]
        for r in reads:
            add(self.lastw.get(r))
        for w in writes:
            add(self.lastw.get(w))
            for k, v in self.readers.get(w, {}).items():
                add((k, v))
        return deps

    def _wait(self, X, deps):
        e = self.eng[X]
        for k, v in deps.items():
            if k == X and X == 'pe':
                continue
            if self.waited[X].get(k, 0) >= v:
                continue
            if isinstance(k, str):
                ep = (v - 1) // self.EPOCH
                e.wait_ge(self._esem(k, ep), v - ep * self.EPOCH)
            else:
                v = self.dval[k]
                e.wait_ge(self.dsems[k], v)
            self.waited[X][k] = v
            self.ninst += 1

    def _record(self, ev, reads, writes):
        for r in reads:
            d = self.readers.setdefault(r, {})
            if d.get(ev[0], 0) < ev[1]:
                d[ev[0]] = ev[1]
        for w in writes:
            self.lastw[w] = ev
            self.readers[w] = {}

    def op(self, X, fn, r=(), w=()):
        r = [k for k in r if k is not None]
        w = [k for k in w if k is not None]
        w = w + [k for k in r if isinstance(k, str) and k.startswith('psb') and k not in w]
        self._wait(X, self._deps(r, w))
        inst = fn(self.eng[X])
        self.cnt[X] += 1
        n = self.cnt[X]
        inst.then_inc(self._esem(X, (n - 1) // self.EPOCH), 1)
        self.ninst += 1
        self._record((X, n), r, w)

    def dma(self, X, fn, semkey, r=(), w=()):
        if semkey not in self.dkey:
            i = len(self.dkey) % self.NDMA
            if i >= len(self.dsems):
                self.dsems.append(self.nc.alloc_semaphore("d_%d" % i))
                self.dval.append(0)
            self.dkey[semkey] = i
        i = self.dkey[semkey]
        self._wait(X, self._deps(r, w))
        inst = fn(self.eng[X])
        self.dval[i] += 16
        inst.then_inc(self.dsems[i], 16)
        self.ninst += 1
        self._record((i, self.dval[i]), r, w)

    def barrier(self):
        deps = {k: v for k, v in self.cnt.items() if v > 0}
        for i, v in enumerate(self.dval):
            if v > 0:
                deps[i] = v
        for X in self.eng:
            self._wait(X, dict(deps))


class Tile:
    def __init__(self, t, key):
        self.t, self.key = t, key

    def __getitem__(self, k):
        return self.t[k]


class Builder:
    def __init__(self, T, depth=DEPTH, debug=False, rb=None):
        import os
        rb = rb or int(os.environ.get('RB', '512'))
        self.T, self.depth, self.debug = T, depth, debug
        self.NT = T // 128
        self.RB = rb
        self.NB = (2 * T) // rb + NE
        nc = self.nc = bass.Bass("TRN2", target_bir_lowering=False)
        self.P = Prog(nc)
        self.nsb = 0
        self.sb_off = 16640
        self.sb_peak = 0
        self.sb_cap = 229376
        self.bank_i = 0
        self.chain_i = {}
        self.banks = [nc.alloc_psum_tensor("psb%d" % i, [128, 512], F32) for i in range(8)]
        self.dbg = {}

    def sb(self, shape, dt=F32, name=None):
        self.nsb += 1
        name = "%s_%d" % (name or "t", self.nsb)
        esz = 2 if dt == BF16 else 4
        n = 1
        for v in shape[1:]:
            n *= v
        nbytes = (n * esz + 31) // 32 * 32
        off = self.sb_off
        self.sb_off += nbytes
        assert self.sb_off <= self.sb_cap, "SBUF overflow %d" % self.sb_off
        self.sb_peak = max(self.sb_peak, self.sb_off)
        return Tile(self.nc.alloc_sbuf_tensor_at(name, list(shape), dt, offset=off), name)

    CHAIN_BANKS = {'s': [0, 1], 'r': [2, 3, 4], 'g': [5, 6], 'e': [7]}

    def bank(self, chain=None):
        if chain is None:
            i = self.bank_i
            self.bank_i = (i + 1) % 8
        else:
            lst = self.CHAIN_BANKS[chain]
            j = self.chain_i.get(chain, 0)
            self.chain_i[chain] = (j + 1) % len(lst)
            i = lst[j]
        return self.banks[i], "psb%d" % i

    def V(self, fn, r=(), w=()):
        self.P.op('dve', fn, r, w)

    def A(self, fn, r=(), w=()):
        self.P.op('act', fn, r, w)

    def G(self, fn, r=(), w=()):
        self.P.op('pool', fn, r, w)

    def M(self, fn, r=(), w=()):
        self.P.op('pe', fn, r, w)

    def din(self, name, shape, dt=F32):
        return self.nc.dram_tensor(name, list(shape), dt, kind="ExternalInput")

    def dscr(self, name, shape, dt=F32):
        return self.nc.dram_tensor(name, list(shape), dt, kind="Internal")

    def dout(self, name, shape, dt=F32):
        return self.nc.dram_tensor(name, list(shape), dt, kind="ExternalOutput")

    def load(self, q, out_ap, in_ap, key, dkeys=(), slow=False):
        if slow:
            self.P.dma(q, lambda e: e.dma_start(out=out_ap, in_=in_ap, allow_slow_non_contiguous=True), key, r=list(dkeys), w=[key])
        else:
            self.P.dma(q, lambda e: e.dma_start(out=out_ap, in_=in_ap), key, r=list(dkeys), w=[key])

    def store(self, q, out_ap, in_ap, key, dkeys=()):
        self.P.dma(q, lambda e: e.dma_start(out=out_ap, in_=in_ap), key, r=[key], w=list(dkeys))

    def consts(self):
        nc = self.nc
        c = self.c = {}
        self._ln_st = self.sb([128, 2, 6], name="ln_st")
        self._ln_mv = self.sb([128, 2], name="ln_mv")
        self._ln_rs = self.sb([128, 1], name="ln_rs")
        onesf = c['onesf'] = self.sb([128, 128], name="onesf")
        self.G(lambda e: e.memset(onesf[:], 1.0), w=[onesf.key])
        identf = c['identf'] = self.sb([128, 128], name="identf")
        self.G(lambda e: e.memset(identf[:], 0.0), w=[identf.key])
        self.G(lambda e: e.affine_select(out=identf[:], in_=identf[:], pattern=[[-1, 128]], base=0, channel_multiplier=1,
                                         compare_op=ALU.not_equal, fill=1.0), r=[identf.key], w=[identf.key])
        identb = c['identb'] = self.sb([128, 128], BF16, name="identb")
        self.V(lambda e: e.tensor_copy(identb[:], identf[:]), r=[identf.key], w=[identb.key])
        tri = c['tri'] = self.sb([128, 128], name="tri")
        self.G(lambda e: e.affine_select(out=tri[:], in_=onesf[:], pattern=[[1, 128]], base=0, channel_multiplier=-1,
                                         compare_op=ALU.is_ge, fill=0.0), r=[onesf.key], w=[tri.key])
        su = c['su'] = self.sb([128, 128], name="su")
        self.G(lambda e: e.affine_select(out=su[:], in_=onesf[:], pattern=[[-1, 128]], base=0, channel_multiplier=1,
                                         compare_op=ALU.is_gt, fill=0.0), r=[onesf.key], w=[su.key])
        sl = c['sl'] = self.sb([128, 128], name="sl")
        self.G(lambda e: e.affine_select(out=sl[:], in_=onesf[:], pattern=[[1, 128]], base=0, channel_multiplier=-1,
                                         compare_op=ALU.is_gt, fill=0.0), r=[onesf.key], w=[sl.key])
        maskb = c['maskb'] = self.sb([128, 128], name="maskb")
        self.G(lambda e: e.tensor_copy(maskb[:], tri[:]), r=[tri.key], w=[maskb.key])
        self.G(lambda e: e.memset(maskb[0:64, 64:128], 0.0), w=[maskb.key])
        rmask = c['rmask'] = self.sb([128, 256], name="rmask")
        self.G(lambda e: e.memset(rmask[:], 1.0), w=[rmask.key])
        self.G(lambda e: e.memset(rmask[:].rearrange("p (a b) -> p a b", b=64)[:, :, 0:1], 0.0), w=[rmask.key])
        hm = c['hm'] = self.sb([128, 2], name="hm")
        self.G(lambda e: e.memset(hm[:], 0.0), w=[hm.key])
        self.G(lambda e: e.memset(hm[0:64, 0:1], 1.0), w=[hm.key])
        self.G(lambda e: e.memset(hm[64:128, 1:2], 1.0), w=[hm.key])
        nhm = c['nhm'] = self.sb([128, 2], name="nhm")
        self.V(lambda e: e.tensor_scalar(nhm[:], hm[:], -1.0, None, ALU.mult), r=[hm.key], w=[nhm.key])
        qm = c['qm'] = self.sb([64, 2], name="qm")
        self.G(lambda e: e.memset(qm[:], 0.0), w=[qm.key])
        self.G(lambda e: e.memset(qm[0:32, 0:1], 32.0 ** -0.5), w=[qm.key])
        self.G(lambda e: e.memset(qm[32:64, 1:2], 32.0 ** -0.5), w=[qm.key])
        bones = c['bones'] = self.sb([128, 128], name="bones")
        self.G(lambda e: e.memset(bones[:], 0.0), w=[bones.key])
        self.G(lambda e: e.memset(bones[0:64, 0:64], 1.0), w=[bones.key])
        self.G(lambda e: e.memset(bones[64:128, 64:128], 1.0), w=[bones.key])

    def layernorm(self, xin, gk, bk, out, tmp, stt=None):
        st, mv, rs = stt if stt is not None else (self._ln_st, self._ln_mv, self._ln_rs)
        for i in range(2):
            self.V(lambda e, i=i: e.bn_stats(st[:, i, :], xin[:, i * 512:(i + 1) * 512]), r=[xin.key], w=[st.key])
        self.V(lambda e: e.bn_aggr(mv[:], st[:].rearrange("p a b -> p (a b)")), r=[st.key], w=[mv.key])
        self.V(lambda e: e.tensor_scalar(rs[:], mv[:, 1:2], LN_EPS, None, ALU.add), r=[mv.key], w=[rs.key])
        yield
        self.A(lambda e: e.activation(out=rs[:], in_=rs[:], func=AF.Sqrt), r=[rs.key], w=[rs.key])
        yield
        self.V(lambda e: e.reciprocal(rs[:], rs[:]), r=[rs.key], w=[rs.key])
        self.V(lambda e: e.tensor_scalar(tmp[:], xin[:], mv[:, 0:1], rs[:, 0:1], ALU.subtract, ALU.mult), r=[xin.key, mv.key, rs.key], w=[tmp.key])
        yield
        self.G(lambda e: e.tensor_tensor(tmp[:], tmp[:], gk[:], ALU.mult), r=[tmp.key, gk.key], w=[tmp.key])
        yield
        self.V(lambda e: e.tensor_tensor(out[:], tmp[:], bk[:], ALU.add), r=[tmp.key, bk.key], w=[out.key])
        yield

    def ln_stats(self, tag):
        return (self.sb([128, 2, 6], name="lnst_" + tag), self.sb([128, 2], name="lnmv_" + tag), self.sb([128, 1], name="lnrs_" + tag))

    def run_pipe(self, bodies, width=2):
        active, nxt = [], 0
        while active or nxt < len(bodies):
            while len(active) < width and nxt < len(bodies):
                active.append(bodies[nxt]())
                nxt += 1
            for g_ in list(active):
                try:
                    next(g_)
                except StopIteration:
                    active.remove(g_)

    def to_fm(self, h_tm, hb, hT):
        c = self.c
        self.A(lambda e: e.copy(out=hb[:], in_=h_tm[:]), r=[h_tm.key], w=[hb.key])
        bk, bkey = self.bank()
        pb = bk[:].bitcast(BF16)
        for kc in range(8):
            self.M(lambda e, kc=kc: e.transpose(pb[:, kc * 128:(kc + 1) * 128], hb[:, kc * 128:(kc + 1) * 128], c['identb'][:]),
                   r=[hb.key, c['identb'].key], w=[bkey])
        self.V(lambda e: e.tensor_copy(hT[:].rearrange("p a b -> p (a b)"), pb), r=[bkey], w=[hT.key])

    def declare_inputs(self):
        L = DEPTH
        d = self.d = {}
        specs = dict(x=[self.T, D], ln_in_g=[D], ln_in_b=[D], w_in=[L, D, NIN], ssd_conv_w=[L, 4, 768], ssd_conv_b=[L, 768],
                     ssd_dt_bias=[L, 8], ssd_a_log=[L, 8], ssd_d=[L, 8], ssd_norm_g=[L, 512], rwkv_mu=[L, 896], rwkv_w0=[L, 256],
                     rwkv_w2=[L, 32, 256], rwkv_a0=[L, 256], rwkv_a2=[L, 32, 256], rwkv_g2=[L, 64, 256], rwkv_k_k=[L, 256],
                     rwkv_k_a=[L, 256], rwkv_r_k=[L, 256], rwkv_ln_g=[L, 256], rwkv_ln_b=[L, 256], gla_w_a2=[L, 16, 128],
                     gla_b_a=[L, 128], gla_norm_g=[L, 256], w_out=[L, D, D], ln1_g=[L, D], ln1_b=[L, D], moe_w_rg=[L, D, 4],
                     moe_b_rg=[L, 4], moe_w_re=[L, D, 32], moe_b_re=[L, 32], moe_w_gate=[L * NE * D, FF], moe_w_up=[L * NE * D, FF],
                     moe_w_down=[L * NE * FF, D], ln2_g=[L, D], ln2_b=[L, D])
        for k, s in specs.items():
            d[k] = self.din(k, s)
        return specs

    def alloc_params(self):
        p = self.p = {}
        sb = self.sb
        p['w_in'] = sb([128, 8, NIN], BF16, "w_in_sb")
        p['w_out'] = sb([128, 8, D], BF16, "w_out_sb")
        p['cw'] = sb([128, 6, 4], name="convw")
        p['cb'] = sb([128, 6], name="convb")
        p['cdiag'] = sb([128, 24, 128], BF16, "cdiag")
        for n, w in (('dtb', 8), ('alog', 8), ('dsk8', 8), ('dsk', 512), ('sng', 512), ('rlg', 256), ('rlb', 256), ('gng', 256),
                     ('l1g', D), ('l1b', D), ('rb36', 36)):
            p[n] = sb([128, w], name="p_" + n)
        for n, w in (('mu', 7), ('omu', 7), ('w0', 2), ('a0', 2), ('kk', 2), ('ka', 2), ('omka', 2), ('rk', 2)):
            p[n] = sb([128, w], name="p_" + n)
        p['ba'] = sb([64, 2], name="p_ba")
        p['w2p'] = sb([128, 256], name="p_w2p")
        p['a2p'] = sb([128, 256], name="p_a2p")
        p['g2p'] = sb([128, 256], name="p_g2p")
        p['wa2'] = sb([32, 128], name="p_wa2")
        p['wr'] = sb([128, 8, 36], name="p_wr")

    def load_params(self, l):
        p, d, c = self.p, self.d, self.c
        q = 'pool'
        win = d['w_in'][l].rearrange("(kc p) n -> p kc n", p=128)
        for kc in range(8):
            for (a, b) in ((0, 1484), (1484, NIN)):
                self.load(q, p['w_in'][:, kc, a:b], win[:, kc, a:b], p['w_in'].key)
        wo = d['w_out'][l].rearrange("(kc p) n -> p kc n", p=128)
        for kc in range(8):
            self.load(q, p['w_out'][:, kc, :], wo[:, kc, :], p['w_out'].key)
        q = 'sp'
        for kk_ in range(4):
            self.load(q, p['cw'][:, :, kk_], d['ssd_conv_w'][l][kk_].rearrange("(cb p) -> p cb", p=128), p['cw'].key, slow=True)
        self.load(q, p['cb'][:], d['ssd_conv_b'][l].rearrange("(cb p) -> p cb", p=128), p['cb'].key, slow=True)
        for n, src in (('dtb', 'ssd_dt_bias'), ('alog', 'ssd_a_log'), ('dsk8', 'ssd_d'), ('sng', 'ssd_norm_g'), ('rlg', 'rwkv_ln_g'),
                       ('rlb', 'rwkv_ln_b'), ('gng', 'gla_norm_g'), ('l1g', 'ln1_g'), ('l1b', 'ln1_b')):
            self.load(q, p[n][:], d[src][l].partition_broadcast(128), p[n].key)
        self.load(q, p['rb36'][:, 0:4], d['moe_b_rg'][l].partition_broadcast(128), p['rb36'].key)
        self.load(q, p['rb36'][:, 4:36], d['moe_b_re'][l].partition_broadcast(128), p['rb36'].key)
        self.load(q, p['mu'][:], d['rwkv_mu'][l].rearrange("(b p) -> p b", p=128), p['mu'].key, slow=True)
        for n, src in (('w0', 'rwkv_w0'), ('a0', 'rwkv_a0'), ('kk', 'rwkv_k_k'), ('ka', 'rwkv_k_a'), ('rk', 'rwkv_r_k')):
            self.load(q, p[n][:], d[src][l].rearrange("(b p) -> p b", p=128), p[n].key, slow=True)
        self.load(q, p['ba'][:], d['gla_b_a'][l].rearrange("(b p) -> p b", p=64), p['ba'].key, slow=True)
        for n in ('w2p', 'a2p', 'g2p'):
            self.G(lambda e, n=n: e.memset(p[n][:], 0.0), w=[p[n].key])
        self.load(q, p['w2p'][0:32, :], d['rwkv_w2'][l], p['w2p'].key)
        self.load(q, p['a2p'][32:64, :], d['rwkv_a2'][l], p['a2p'].key)
        self.load(q, p['g2p'][64:128, :], d['rwkv_g2'][l], p['g2p'].key)
        self.G(lambda e: e.memset(p['wa2'][:], 0.0), w=[p['wa2'].key])
        self.load(q, p['wa2'][16:32, :], d['gla_w_a2'][l], p['wa2'].key)
        self.load(q, p['wr'][:, :, 0:4], d['moe_w_rg'][l].rearrange("(kc p) n -> p kc n", p=128), p['wr'].key, slow=True)
        self.load(q, p['wr'][:, :, 4:36], d['moe_w_re'][l].rearrange("(kc p) n -> p kc n", p=128), p['wr'].key, slow=True)
        self.V(lambda e: e.tensor_scalar(p['omu'][:], p['mu'][:], -1.0, 1.0, ALU.mult, ALU.add), r=[p['mu'].key], w=[p['omu'].key])
        self.V(lambda e: e.tensor_scalar(p['omka'][:], p['ka'][:], -1.0, 1.0, ALU.mult, ALU.add), r=[p['ka'].key], w=[p['omka'].key])
        self.A(lambda e: e.activation(out=p['alog'][:], in_=p['alog'][:], func=AF.Exp), r=[p['alog'].key], w=[p['alog'].key])
        self.V(lambda e: e.tensor_scalar(p['alog'][:], p['alog'][:], -1.0, None, ALU.mult), r=[p['alog'].key], w=[p['alog'].key])
        self.V(lambda e: e.tensor_copy(p['dsk'][:].rearrange("p (h q) -> p h q", q=64), p['dsk8'][:].unsqueeze(2).to_broadcast([128, 8, 64])),
               r=[p['dsk8'].key], w=[p['dsk'].key])
        for cb in range(6):
            for k in range(4):
                self.V(lambda e, cb=cb, k=k: e.tensor_scalar(p['cdiag'][:, cb * 4 + k, :], c['identf'][:], p['cw'][:, cb, k:k + 1], None, ALU.mult),
                       r=[c['identf'].key, p['cw'].key], w=[p['cdiag'].key])

    def alloc_mixer(self):
        s = self.s = {}
        sb = self.sb
        s['hT'] = sb([128, 8, 128], BF16, "m_hT")
        s['htm'] = sb([128, D], name="m_htm")
        s['xbc'] = sb([128, 6, 132], BF16, "m_xbc")
        s['xbB'] = sb([128, 6, 132], BF16, "m_xbB")
        s['xc'] = sb([128, 6, 128], BF16, "m_xc")
        s['xh'] = sb([128, 512], BF16, "m_xh")
        s['xdt'] = sb([128, 512], BF16, "m_xdt")
        s['btm'] = sb([128, 128], BF16, "m_btm")
        s['cm'] = sb([128, 2, 128], BF16, "m_cm")
        s['dt'] = sb([128, 8], name="m_dt")
        s['adt'] = sb([128, 8], name="m_adt")
        s['sp1'] = sb([128, 8], name="m_sp1")
        s['sp2'] = sb([128, 8], name="m_sp2")
        s['R'] = sb([128, 4, 128], name="m_R")
        s['seg'] = sb([128, 8, 128], BF16, "m_seg")
        s['ea'] = sb([128, 8], name="m_ea")
        s['cd'] = sb([128, 4], name="m_cd")
        s['cbm'] = sb([128, 2, 128], BF16, "m_cbm")
        s['toend'] = sb([128, 8], name="m_toend")
        s['S32'] = sb([128, 256], name="m_S32")
        s['Sbf'] = sb([128, 256], BF16, "m_Sbf")
        s['y1'] = sb([128, 512], name="m_y1")
        s['sz'] = sb([128, 512], BF16, "m_sz")
        s['ssq'] = sb([128, 4], name="m_ssq")
        s['ycat'] = sb([128, D], BF16, "m_ycat")
        s['yT'] = sb([128, 8, 128], BF16, "m_yT")
        s['gaT'] = sb([32, 128], name="g_gaT")
        s['gx'] = sb([64, 256], name="g_x")
        s['gt1'] = sb([64, 256], name="g_t1")
        s['gcum'] = sb([64, 256], name="g_cum")
        s['geq'] = sb([64, 256], name="g_eq")
        s['gek'] = sb([64, 256], name="g_ek")
        s['gel'] = sb([64, 4], name="g_el")
        s['gqm'] = sb([64, 2, 256], BF16, "g_qm")
        s['gkT'] = sb([64, 256], BF16, "g_kT")
        s['gktm'] = sb([128, 2, 128], BF16, "g_ktm")
        s['gv'] = sb([128, 256], BF16, "g_v")
        s['gvm'] = sb([128, 2, 256], BF16, "g_vm")
        s['gsm'] = sb([128, 4, 128], BF16, "g_sm")
        s['gS'] = sb([64, 2, 64], name="g_S")
        s['gSb'] = sb([64, 2, 2, 64], BF16, "g_Sb")
        s['gst'] = sb([64, 2, 64], name="g_st")
        s['go'] = sb([128, 256], name="g_o")
        s['gsq'] = sb([128, 256], name="g_sq")
        s['grs'] = sb([128, 4], name="g_rs")
        s['gsg'] = sb([128, 256], name="g_sg")
        s['rw'] = sb([128, 7, 129], name="r_rw")
        s['rsh'] = sb([128, 7, 128], name="r_sh")
        s['rt1'] = sb([128, 7, 128], name="r_t1")
        for n in ('ra1', 'ra2', 'ra3', 'rcw', 'recw', 'reicw', 'recwp', 'ra', 'rkk', 'rkp'):
            s[n] = sb([128, 256], name="r_" + n)
        for n in ('rKt', 'rBt'):
            s[n] = sb([128, 256], BF16, "r_" + n)
        s['rAm'] = sb([128, 2, 256], BF16, "r_Am")
        s['rRm'] = sb([128, 2, 256], BF16, "r_Rm")
        s['rtw'] = sb([128, 128], name="r_tw")
        s['rsg'] = sb([128, 128], name="r_sg")
        s['rvtm'] = sb([128, 256], name="r_vtm")
        s['rvc'] = sb([64, 2, 256], BF16, "r_vc")
        s['rBc'] = sb([64, 2, 256], BF16, "r_Bc")
        s['rKc'] = sb([64, 2, 256], BF16, "r_Kc")
        s['rP'] = sb([64, 8, 64], BF16, "r_P")
        s['rQ'] = sb([64, 8, 64], BF16, "r_Q")
        s['rP2'] = sb([64, 8, 64], BF16, "r_P2")
        s['rQ2'] = sb([64, 8, 64], BF16, "r_Q2")
        s['rTT'] = sb([64, 8, 64], BF16, "r_TT")
        s['rAak'] = sb([64, 8, 64], BF16, "r_Aak")
        s['rArb'] = sb([64, 8, 64], BF16, "r_Arb")
        s['rArk'] = sb([64, 8, 64], BF16, "r_Ark")
        s['rG'] = sb([64, 4, 64], BF16, "r_G")
        s['rU'] = sb([64, 4, 64], BF16, "r_U")
        s['rST'] = sb([128, 2, 64], name="r_ST")
        s['rSTb'] = sb([128, 2, 64], BF16, "r_STb")
        s['rt2'] = sb([128, 2, 64], name="r_t2")
        s['rewc'] = sb([128, 4], name="r_ewc")
        s['rY1'] = sb([128, 256], name="r_Y1")
        s['rY'] = sb([128, 256], name="r_Y")
        s['rm1'] = sb([128, 4], name="r_m1")
        s['rm2'] = sb([128, 4], name="r_m2")
        s['rvar'] = sb([128, 4], name="r_var")
        s['rbc'] = sb([128, 4], name="r_bc")
        s['rg'] = sb([128, 256], name="r_g")
        s['mix'] = sb([128, D], name="m_mix")
        s['tmp'] = sb([128, D], name="m_tmp")
        s['h1'] = sb([128, D], name="m_h1")

    def proj_tm(self, out_ap, okey, c0, n):
        s, p = self.s, self.p
        for kc in range(8):
            self.M(lambda e, kc=kc: e.matmul(out_ap, lhsT=s['hT'][:, kc, :], rhs=p['w_in'][:, kc, c0:c0 + n], start=(kc == 0), stop=(kc == 7)),
                   r=[s['hT'].key, p['w_in'].key], w=[okey])

    def proj_fm(self, out_ap, okey, c0, m):
        s, p = self.s, self.p
        for kc in range(8):
            self.M(lambda e, kc=kc: e.matmul(out_ap, lhsT=p['w_in'][:, kc, c0:c0 + m], rhs=s['hT'][:, kc, :], start=(kc == 0), stop=(kc == 7)),
                   r=[s['hT'].key, p['w_in'].key], w=[okey])

    def bc(self, ap, shape, axis):
        return ap.unsqueeze(axis).to_broadcast(list(shape))

    def ssd_tile(self, first):
        s, p, c = self.s, self.p, self.c
        V, A, G, M = self.V, self.A, self.G, self.M
        k = lambda t: t.key
        bz, kz = self.bank('s')
        self.proj_tm(bz[:, :], kz, O_Z, 512)
        A(lambda e: e.activation(out=s['sz'][:], in_=bz[:, :], func=AF.Silu), r=[kz], w=[k(s['sz'])])
        yield
        bd, kd = self.bank('s')
        self.proj_tm(bd[:, 0:8], kd, O_DT, 8)
        V(lambda e: e.tensor_tensor(s['sp1'][:], bd[:, 0:8], p['dtb'][:], ALU.add), r=[kd, k(p['dtb'])], w=[k(s['sp1'])])
        yield
        V(lambda e: e.scalar_tensor_tensor(s['sp2'][:], s['sp1'][:], -1.0, s['sp1'][:], ALU.mult, ALU.max), r=[k(s['sp1'])], w=[k(s['sp2'])])
        yield
        A(lambda e: e.activation(out=s['sp2'][:], in_=s['sp2'][:], func=AF.Exp, scale=-1.0), r=[k(s['sp2'])], w=[k(s['sp2'])])
        yield
        A(lambda e: e.activation(out=s['sp2'][:], in_=s['sp2'][:], func=AF.Ln, bias=1.0), r=[k(s['sp2'])], w=[k(s['sp2'])])
        yield
        V(lambda e: e.scalar_tensor_tensor(s['dt'][:], s['sp1'][:], 0.0, s['sp2'][:], ALU.max, ALU.add), r=[k(s['sp1']), k(s['sp2'])], w=[k(s['dt'])])
        yield
        V(lambda e: e.tensor_tensor(s['adt'][:], s['dt'][:], p['alog'][:], ALU.mult), r=[k(s['dt']), k(p['alog'])], w=[k(s['adt'])])
        yield
        import os
        stop = float(os.environ.get('SSDSTOP', '9'))
        if stop <= 1:
            return
        if first:
            G(lambda e: e.memset(s['xbc'][:, :, 0:4], 0.0), w=[k(s['xbc'])])
            yield
            G(lambda e: e.memset(s['xbB'][:, :, 0:2], 0.0), w=[k(s['xbB'])])
            yield
        else:
            G(lambda e: e.tensor_copy(s['xbc'][:, :, 0:3], s['xbc'][:, :, 128:131]), r=[k(s['xbc'])], w=[k(s['xbc'])])
            yield
            G(lambda e: e.tensor_copy(s['xbB'][:, :, 0:2], s['xbB'][:, :, 128:130]), r=[k(s['xbB'])], w=[k(s['xbB'])])
            yield
        for grp, nb in ((0, 4), (4, 2)):
            bx, kx = self.bank('s')
            for j in range(nb):
                self.proj_fm(bx[:, j * 128:(j + 1) * 128], kx, O_XBC + (grp + j) * 128, 128)
            A(lambda e, bx=bx, grp=grp, nb=nb: e.copy(out=s['xbc'][:, grp:grp + nb, 3:131], in_=bx[:, 0:nb * 128].rearrange("p (a b) -> p a b", b=128)),
              r=[kx], w=[k(s['xbc'])])
            yield
            V(lambda e, bx=bx, grp=grp, nb=nb: e.tensor_copy(s['xbB'][:, grp:grp + nb, 2:130], bx[:, 0:nb * 128].rearrange("p (a b) -> p a b", b=128)),
              r=[kx], w=[k(s['xbB'])])
            yield
        for grp, nb in ((0, 4), (4, 2)):
            bx, kx = self.bank('s')
            for j in range(nb):
                cb = grp + j
                for kk_ in range(4):
                    src = s['xbc'] if kk_ % 2 == 0 else s['xbB']
                    off = kk_ if kk_ % 2 == 0 else kk_ - 1
                    M(lambda e, bx=bx, j=j, cb=cb, kk_=kk_, src=src, off=off: e.matmul(bx[:, j * 128:(j + 1) * 128], lhsT=p['cdiag'][:, cb * 4 + kk_, :],
                                                                                   rhs=src[:, cb, off:off + 128], start=(kk_ == 0), stop=(kk_ == 3)),
                      r=[k(p['cdiag']), k(src)], w=[kx])
            for j in range(nb):
                cb = grp + j
                A(lambda e, bx=bx, j=j, cb=cb: e.activation(out=s['xc'][:, cb, :], in_=bx[:, j * 128:(j + 1) * 128], func=AF.Silu, bias=p['cb'][:, cb:cb + 1]),
                  r=[kx, k(p['cb'])], w=[k(s['xc'])])
                yield
        if stop <= 2:
            return
        bt, kt = self.bank('s')
        pb = bt[:].bitcast(BF16)
        for j in range(5):
            M(lambda e, j=j: e.transpose(pb[:, j * 128:(j + 1) * 128], s['xc'][:, j, :], c['identb'][:]), r=[k(s['xc']), k(c['identb'])], w=[kt])
        V(lambda e: e.tensor_copy(s['xh'][:], pb[:, 0:512]), r=[kt], w=[k(s['xh'])])
        yield
        V(lambda e: e.tensor_copy(s['btm'][:], pb[:, 512:640]), r=[kt], w=[k(s['btm'])])
        yield
        if stop <= 2.2:
            return
        G(lambda e: e.tensor_tensor(s['cm'][:], self.bc(s['xc'][:, 5, :], [128, 2, 128], 1), self.bc(c['hm'][:, :], [128, 2, 128], 2), ALU.mult),
          r=[k(s['xc']), k(c['hm'])], w=[k(s['cm'])])
        yield
        V(lambda e: e.tensor_tensor(s['xdt'][:].rearrange("p (h q) -> p h q", q=64), s['xh'][:].rearrange("p (h q) -> p h q", q=64),
                                    self.bc(s['dt'][:, :], [128, 8, 64], 2), ALU.mult), r=[k(s['xh']), k(s['dt'])], w=[k(s['xdt'])])
        yield
        if stop <= 2.4:
            return
        for half in range(2):
            G(lambda e, half=half: e.tensor_tensor(s['R'][:], self.bc(c['tri'][:, :], [128, 4, 128], 1), self.bc(s['adt'][:, half * 4:(half + 1) * 4], [128, 4, 128], 2), ALU.mult),
              r=[k(c['tri']), k(s['adt'])], w=[k(s['R'])])
            yield
            bD, kD = self.bank('s')
            for q2 in range(2):
                M(lambda e, bD=bD, q2=q2: e.matmul(bD[:, q2 * 256:(q2 + 1) * 256], lhsT=c['su'][:], rhs=s['R'][:, q2 * 2:(q2 + 1) * 2, :].rearrange("p a b -> p (a b)"), start=True, stop=True),
                  r=[k(c['su']), k(s['R'])], w=[kD])
            if stop <= 2.6:
                continue
            A(lambda e, bD=bD, half=half: e.activation(out=s['seg'][:, half * 4:(half + 1) * 4, :].rearrange("p a b -> p (a b)"), in_=bD[:, :], func=AF.Exp),
              r=[kD], w=[k(s['seg'])])
            yield
        if stop <= 2.8:
            return
        V(lambda e: e.tensor_copy(s['toend'][:], s['seg'][:, :, 127]), r=[k(s['seg'])], w=[k(s['toend'])])
        yield
        if stop <= 3:
            return
        be, ke = self.bank('s')
        M(lambda e: e.matmul(be[:, 0:8], lhsT=c['tri'][:], rhs=s['adt'][:], start=True, stop=True), r=[k(c['tri']), k(s['adt'])], w=[ke])
        for g in range(2):
            M(lambda e, g=g: e.matmul(be[g * 64:(g + 1) * 64, 8:12], lhsT=c['onesf'][:, 0:64], rhs=s['adt'][:, g * 4:(g + 1) * 4], start=True, stop=True),
              r=[k(c['onesf']), k(s['adt'])], w=[ke])
        A(lambda e: e.activation(out=s['ea'][:], in_=be[:, 0:8], func=AF.Exp), r=[ke], w=[k(s['ea'])])
        yield
        A(lambda e: e.activation(out=s['cd'][:], in_=be[:, 8:12], func=AF.Exp), r=[ke], w=[k(s['cd'])])
        yield
        if stop <= 4:
            return
        bc_, kc_ = self.bank('s')
        for g in range(2):
            M(lambda e, g=g: e.matmul(bc_[:, g * 128:(g + 1) * 128], lhsT=s['xc'][:, 4, :], rhs=s['cm'][:, g, :], start=True, stop=True),
              r=[k(s['xc']), k(s['cm'])], w=[kc_])
        V(lambda e: e.tensor_tensor(s['cbm'][:], bc_[:, 0:256].rearrange("p (a b) -> p a b", b=128), self.bc(c['tri'][:, :], [128, 2, 128], 1), ALU.mult),
          r=[kc_, k(c['tri'])], w=[k(s['cbm'])])
        yield
        for g in range(2):
            V(lambda e, g=g: e.tensor_tensor(s['seg'][:, g * 4:(g + 1) * 4, :], s['seg'][:, g * 4:(g + 1) * 4, :], self.bc(s['cbm'][:, g, :], [128, 4, 128], 1), ALU.mult),
              r=[k(s['seg']), k(s['cbm'])], w=[k(s['seg'])])
            yield
        by, ky = self.bank('s')
        for h in range(8):
            M(lambda e, h=h: e.matmul(by[:, h * 64:(h + 1) * 64], lhsT=s['seg'][:, h, :], rhs=s['xdt'][:, h * 64:(h + 1) * 64], start=True, stop=True),
              r=[k(s['seg']), k(s['xdt'])], w=[ky])
        bo, ko = self.bank('s')
        if not first:
            for g in range(2):
                M(lambda e, g=g: e.matmul(bo[:, g * 256:(g + 1) * 256], lhsT=s['cm'][:, g, :], rhs=s['Sbf'][:, :], start=True, stop=True),
                  r=[k(s['cm']), k(s['Sbf'])], w=[ko])
            V(lambda e: e.tensor_tensor(s['y1'][:].rearrange("p (h q) -> p h q", q=64), bo[:, :].rearrange("p (h q) -> p h q", q=64),
                                        self.bc(s['ea'][:, :], [128, 8, 64], 2), ALU.mult), r=[ko, k(s['ea'])], w=[k(s['y1'])])
            yield
            V(lambda e: e.tensor_tensor(s['y1'][:], s['y1'][:], by[:, :], ALU.add), r=[k(s['y1']), ky], w=[k(s['y1'])])
            yield
        else:
            V(lambda e: e.tensor_copy(s['y1'][:], by[:, :]), r=[ky], w=[k(s['y1'])])
            yield
        if stop <= 5:
            return
        V(lambda e: e.tensor_tensor(s['xdt'][:].rearrange("p (h q) -> p h q", q=64), s['xdt'][:].rearrange("p (h q) -> p h q", q=64),
                                    self.bc(s['toend'][:, :], [128, 8, 64], 2), ALU.mult), r=[k(s['xdt']), k(s['toend'])], w=[k(s['xdt'])])
        yield
        bs, ks = self.bank('s')
        for g in range(2):
            M(lambda e, g=g: e.matmul(bs[g * 64:(g + 1) * 64, 0:256], lhsT=s['btm'][:, g * 64:(g + 1) * 64], rhs=s['xdt'][:, g * 256:(g + 1) * 256], start=True, stop=True),
              r=[k(s['btm']), k(s['xdt'])], w=[ks])
        if first:
            V(lambda e: e.tensor_copy(s['S32'][:], bs[:, 0:256]), r=[ks], w=[k(s['S32'])])
            yield
        else:
            V(lambda e: e.tensor_tensor(s['S32'][:].rearrange("p (h q) -> p h q", q=64), s['S32'][:].rearrange("p (h q) -> p h q", q=64),
                                        self.bc(s['cd'][:, :], [128, 4, 64], 2), ALU.mult), r=[k(s['S32']), k(s['cd'])], w=[k(s['S32'])])
            yield
            V(lambda e: e.tensor_tensor(s['S32'][:], s['S32'][:], bs[:, 0:256], ALU.add), r=[k(s['S32']), ks], w=[k(s['S32'])])
            yield
        A(lambda e: e.copy(out=s['Sbf'][:], in_=s['S32'][:]), r=[k(s['S32'])], w=[k(s['Sbf'])])
        yield
        G(lambda e: e.tensor_tensor(s['xdt'][:], s['xh'][:], p['dsk'][:], ALU.mult), r=[k(s['xh']), k(p['dsk'])], w=[k(s['xdt'])])
        yield
        V(lambda e: e.tensor_tensor(s['y1'][:], s['y1'][:], s['xdt'][:], ALU.add), r=[k(s['y1']), k(s['xdt'])], w=[k(s['y1'])])
        yield
        V(lambda e: e.tensor_tensor(s['y1'][:], s['y1'][:], s['sz'][:], ALU.mult), r=[k(s['y1']), k(s['sz'])], w=[k(s['y1'])])
        yield
        for g in range(2):
            A(lambda e, g=g: e.activation(out=s['sz'][:, g * 256:(g + 1) * 256], in_=s['y1'][:, g * 256:(g + 1) * 256], func=AF.Square, accum_out=s['ssq'][:, g:g + 1]),
              r=[k(s['y1'])], w=[k(s['sz']), k(s['ssq'])])
            yield
        V(lambda e: e.tensor_scalar(s['ssq'][:, 0:2], s['ssq'][:, 0:2], 1.0 / 256, RMS_EPS, ALU.mult, ALU.add), r=[k(s['ssq'])], w=[k(s['ssq'])])
        yield
        A(lambda e: e.activation(out=s['ssq'][:, 0:2], in_=s['ssq'][:, 0:2], func=AF.Sqrt), r=[k(s['ssq'])], w=[k(s['ssq'])])
        yield
        V(lambda e: e.reciprocal(s['ssq'][:, 0:2], s['ssq'][:, 0:2]), r=[k(s['ssq'])], w=[k(s['ssq'])])
        yield
        for g in range(2):
            V(lambda e, g=g: e.scalar_tensor_tensor(s['ycat'][:, g * 256:(g + 1) * 256], s['y1'][:, g * 256:(g + 1) * 256], s['ssq'][:, g:g + 1],
                                                   p['sng'][:, g * 256:(g + 1) * 256], ALU.mult, ALU.mult), r=[k(s['y1']), k(s['ssq']), k(p['sng'])], w=[k(s['ycat'])])
            yield

    def gla_tile(self, first):
        s, p, c = self.s, self.p, self.c
        V, A, G, M = self.V, self.A, self.G, self.M
        k = lambda t: t.key
        bv, kv = self.bank('g')
        self.proj_tm(bv[:, :], kv, O_GV, 512)
        A(lambda e: e.copy(out=s['gv'][:], in_=bv[:, 0:256]), r=[kv], w=[k(s['gv'])])
        yield
        A(lambda e: e.activation(out=s['gsg'][:], in_=bv[:, 256:512], func=AF.Silu), r=[kv], w=[k(s['gsg'])])
        yield
        G(lambda e: e.tensor_tensor(s['gsg'][:], s['gsg'][:], p['gng'][:], ALU.mult), r=[k(s['gsg']), k(p['gng'])], w=[k(s['gsg'])])
        yield
        ba_, ka_ = self.bank('g')
        self.proj_fm(ba_[0:32, 0:128], ka_, O_GA - 16, 32)
        A(lambda e: e.copy(out=s['gaT'][:], in_=ba_[0:32, 0:128]), r=[ka_], w=[k(s['gaT'])])
        yield
        bx, kx = self.bank('g')
        for pr in range(2):
            M(lambda e, pr=pr: e.matmul(bx[0:64, pr * 128:(pr + 1) * 128], lhsT=p['wa2'][:, pr * 64:(pr + 1) * 64], rhs=s['gaT'][:], start=True, stop=True),
              r=[k(p['wa2']), k(s['gaT'])], w=[kx])
        for pr in range(2):
            A(lambda e, pr=pr: e.activation(out=s['gx'][:, pr * 128:(pr + 1) * 128], in_=bx[0:64, pr * 128:(pr + 1) * 128], func=AF.Identity, bias=p['ba'][:, pr:pr + 1]),
              r=[kx, k(p['ba'])], w=[k(s['gx'])])
            yield
        V(lambda e: e.scalar_tensor_tensor(s['gt1'][:], s['gx'][:], -1.0, s['gx'][:], ALU.mult, ALU.max), r=[k(s['gx'])], w=[k(s['gt1'])])
        yield
        A(lambda e: e.activation(out=s['gt1'][:], in_=s['gt1'][:], func=AF.Exp, scale=-1.0), r=[k(s['gt1'])], w=[k(s['gt1'])])
        yield
        A(lambda e: e.activation(out=s['gt1'][:], in_=s['gt1'][:], func=AF.Ln, bias=1.0), r=[k(s['gt1'])], w=[k(s['gt1'])])
        yield
        V(lambda e: e.scalar_tensor_tensor(s['gx'][:], s['gx'][:], 0.0, s['gt1'][:], ALU.min, ALU.subtract), r=[k(s['gx']), k(s['gt1'])], w=[k(s['gx'])])
        yield
        V(lambda e: e.tensor_tensor_scan(s['gcum'][:], c['rmask'][0:64, :], s['gx'][:], 0.0, ALU.mult, ALU.add), r=[k(c['rmask']), k(s['gx'])], w=[k(s['gcum'])])
        yield
        A(lambda e: e.activation(out=s['geq'][:], in_=s['gcum'][:], func=AF.Exp, scale=1.0 / 16), r=[k(s['gcum'])], w=[k(s['geq'])])
        yield
        A(lambda e: e.activation(out=s['gek'][:], in_=s['gcum'][:], func=AF.Exp, scale=-1.0 / 16), r=[k(s['gcum'])], w=[k(s['gek'])])
        yield
        A(lambda e: e.activation(out=s['gel'][:], in_=s['gcum'][:].rearrange("p (a b) -> p a b", b=64)[:, :, 63], func=AF.Exp, scale=1.0 / 16),
          r=[k(s['gcum'])], w=[k(s['gel'])])
        yield
        bq, kq = self.bank('g')
        for pr in range(2):
            self.proj_fm(bq[0:64, pr * 128:(pr + 1) * 128], kq, O_GQ + pr * 64, 64)
        for pr in range(2):
            self.proj_fm(bq[0:64, 256 + pr * 128:256 + (pr + 1) * 128], kq, O_GK + pr * 64, 64)
        for hh in range(2):
            V(lambda e, hh=hh: e.scalar_tensor_tensor(s['gqm'][:, hh, :], bq[0:64, 0:256], c['qm'][:, hh:hh + 1], s['geq'][:], ALU.mult, ALU.mult),
              r=[kq, k(c['qm']), k(s['geq'])], w=[k(s['gqm'])])
            yield
        V(lambda e: e.tensor_tensor(s['gkT'][:], bq[0:64, 256:512], s['gek'][:], ALU.mult), r=[kq, k(s['gek'])], w=[k(s['gkT'])])
        yield
        bt, kt = self.bank('g')
        pb = bt[:].bitcast(BF16)
        for pr in range(2):
            M(lambda e, pr=pr: e.transpose(pb[:, pr * 64:(pr + 1) * 64], s['gkT'][:, pr * 128:(pr + 1) * 128], c['identb'][0:64, 0:64]),
              r=[k(s['gkT']), k(c['identb'])], w=[kt])
        if first:
            G(lambda e: e.memset(s['gktm'][:], 0.0), w=[k(s['gktm'])])
            yield
        for hh in range(2):
            V(lambda e, hh=hh: e.tensor_copy(s['gktm'][:, hh, :].rearrange("p (a b c) -> p a b c", a=2, b=2)[:, :, hh, :],
                                             pb[:, 0:128].rearrange("p (a b c) -> p a b c", a=2, b=2)[:, :, hh, :]), r=[kt], w=[k(s['gktm'])])
            yield
        G(lambda e: e.tensor_tensor(s['gvm'][:], self.bc(s['gv'][:, :], [128, 2, 256], 1), self.bc(c['hm'][:, :], [128, 2, 256], 2), ALU.mult),
          r=[k(s['gv']), k(c['hm'])], w=[k(s['gvm'])])
        yield
        bs_, ks_ = self.bank('g')
        for h in range(4):
            pr, hh = h // 2, h % 2
            M(lambda e, h=h, pr=pr, hh=hh: e.matmul(bs_[:, h * 128:(h + 1) * 128], lhsT=s['gkT'][:, pr * 128:(pr + 1) * 128], rhs=s['gqm'][:, hh, pr * 128:(pr + 1) * 128],
                                                    start=True, stop=True), r=[k(s['gkT']), k(s['gqm'])], w=[ks_])
        V(lambda e: e.tensor_tensor(s['gsm'][:], bs_[:, :].rearrange("p (a b) -> p a b", b=128), self.bc(c['maskb'][:, :], [128, 4, 128], 1), ALU.mult),
          r=[ks_, k(c['maskb'])], w=[k(s['gsm'])])
        yield
        bu, ku = self.bank('g')
        for cc in range(2):
            for pr in range(2):
                for hh in range(2):
                    h = pr * 2 + hh
                    M(lambda e, cc=cc, h=h, pr=pr, hh=hh: e.matmul(bu[0:64, cc * 128 + pr * 64:cc * 128 + (pr + 1) * 64],
                                                                   lhsT=s['gktm'][:, hh, pr * 64:(pr + 1) * 64], rhs=s['gvm'][:, cc, h * 64:(h + 1) * 64],
                                                                   start=(hh == 0), stop=(hh == 1)), r=[k(s['gktm']), k(s['gvm'])], w=[ku])
        if first:
            G(lambda e: e.memset(s['gS'][:], 0.0), w=[k(s['gS'])])
            yield
        for cc in range(2):
            A(lambda e, cc=cc: e.copy(out=s['gSb'][:, cc, :, :], in_=s['gS'][:]), r=[k(s['gS'])], w=[k(s['gSb'])])
            yield
            V(lambda e, cc=cc: e.tensor_tensor(s['gst'][:], s['gS'][:], bu[0:64, cc * 128:(cc + 1) * 128].rearrange("p (a b) -> p a b", b=64), ALU.add),
              r=[k(s['gS']), ku], w=[k(s['gst'])])
            yield
            V(lambda e, cc=cc: e.tensor_tensor(s['gS'][:], s['gst'][:], self.bc(s['gel'][:, cc::2], [64, 2, 64], 2), ALU.mult),
              r=[k(s['gst']), k(s['gel'])], w=[k(s['gS'])])
            yield
        bo, ko = self.bank('g')
        for h in range(4):
            M(lambda e, h=h: e.matmul(bo[:, h * 64:(h + 1) * 64], lhsT=s['gsm'][:, h, :], rhs=s['gv'][:, h * 64:(h + 1) * 64], start=True, stop=True),
              r=[k(s['gsm']), k(s['gv'])], w=[ko])
        bi, ki = self.bank('g')
        for cc in range(2):
            for h in range(4):
                pr, hh = h // 2, h % 2
                M(lambda e, cc=cc, h=h, pr=pr, hh=hh: e.matmul(bi[cc * 64:(cc + 1) * 64, h * 64:(h + 1) * 64], lhsT=s['gqm'][:, hh, pr * 128 + cc * 64:pr * 128 + (cc + 1) * 64],
                                                               rhs=s['gSb'][:, cc, pr, :], start=True, stop=True), r=[k(s['gqm']), k(s['gSb'])], w=[ki])
        A(lambda e: e.copy(out=s['gsq'][:], in_=bi[:, 0:256]), r=[ki], w=[k(s['gsq'])])
        yield
        V(lambda e: e.tensor_tensor(s['go'][:], s['gsq'][:], bo[:, 0:256], ALU.add), r=[k(s['gsq']), ko], w=[k(s['go'])])
        yield
        A(lambda e: e.activation(out=s['gsq'][:], in_=s['go'][:], func=AF.Square), r=[k(s['go'])], w=[k(s['gsq'])])
        yield
        V(lambda e: e.tensor_reduce(s['grs'][:], s['gsq'][:].rearrange("p (h q) -> p h q", q=64), AX.X, ALU.add), r=[k(s['gsq'])], w=[k(s['grs'])])
        yield
        V(lambda e: e.tensor_scalar(s['grs'][:], s['grs'][:], 1.0 / 64, RMS_EPS, ALU.mult, ALU.add), r=[k(s['grs'])], w=[k(s['grs'])])
        yield
        A(lambda e: e.activation(out=s['grs'][:], in_=s['grs'][:], func=AF.Sqrt), r=[k(s['grs'])], w=[k(s['grs'])])
        yield
        V(lambda e: e.reciprocal(s['grs'][:], s['grs'][:]), r=[k(s['grs'])], w=[k(s['grs'])])
        yield
        V(lambda e: e.tensor_tensor(s['go'][:].rearrange("p (h q) -> p h q", q=64), s['go'][:].rearrange("p (h q) -> p h q", q=64),
                                    self.bc(s['grs'][:, :], [128, 4, 64], 2), ALU.mult), r=[k(s['go']), k(s['grs'])], w=[k(s['go'])])
        yield
        V(lambda e: e.tensor_tensor(s['ycat'][:, 768:1024], s['go'][:], s['gsg'][:], ALU.mult), r=[k(s['go']), k(s['gsg'])], w=[k(s['ycat'])])
        yield

    def rwkv_tile(self, first):
        s, p, c = self.s, self.p, self.c
        V, A, G, M = self.V, self.A, self.G, self.M
        k = lambda t: t.key
        f2 = lambda t, a, b: t[:, a:b, :].rearrange("p a b -> p (a b)")
        h3 = lambda ap: ap.rearrange("p (h q) -> p h q", q=64)
        if first:
            G(lambda e: e.memset(s['rw'][:, :, 0:1], 0.0), w=[k(s['rw'])])
            yield
            G(lambda e: e.memset(s['rST'][:], 0.0), w=[k(s['rST'])])
            yield
            G(lambda e: e.memset(s['rSTb'][:], 0.0), w=[k(s['rSTb'])])
            yield
        else:
            G(lambda e: e.tensor_copy(s['rw'][:, :, 0:1], s['rw'][:, :, 128:129]), r=[k(s['rw'])], w=[k(s['rw'])])
            yield
        for grp, nb in ((0, 4), (4, 3)):
            bx, kx = self.bank('r')
            for j in range(nb):
                self.proj_fm(bx[:, j * 128:(j + 1) * 128], kx, O_RW + (grp + j) * 128, 128)
            A(lambda e, bx=bx, grp=grp, nb=nb: e.copy(out=s['rw'][:, grp:grp + nb, 1:129], in_=bx[:, 0:nb * 128].rearrange("p (a b) -> p a b", b=128)),
              r=[kx], w=[k(s['rw'])])
            yield
        rt1 = s['rt1'][:]
        G(lambda e: e.tensor_tensor(rt1, s['rw'][:, :, 0:128], self.bc(p['mu'][:, :], [128, 7, 128], 2), ALU.mult), r=[k(s['rw']), k(p['mu'])], w=[k(s['rt1'])])
        yield
        V(lambda e: e.tensor_tensor(s['rsh'][:], s['rw'][:, :, 1:129], self.bc(p['omu'][:, :], [128, 7, 128], 2), ALU.mult), r=[k(s['rw']), k(p['omu'])], w=[k(s['rsh'])])
        yield
        V(lambda e: e.tensor_tensor(s['rsh'][:], s['rsh'][:], rt1, ALU.add), r=[k(s['rsh']), k(s['rt1'])], w=[k(s['rsh'])])
        yield
        rT, kT, vT, lr = f2(s['rsh'], 0, 2), f2(s['rsh'], 2, 4), f2(s['rsh'], 4, 6), s['rsh'][:, 6, :]
        ksh = k(s['rsh'])
        A(lambda e: e.activation(out=s['rtw'][:], in_=lr, func=AF.Tanh), r=[ksh], w=[k(s['rtw'])])
        yield
        bw, kw = self.bank('r')
        for b in range(2):
            M(lambda e, b=b: e.matmul(bw[:, b * 128:(b + 1) * 128], lhsT=p['w2p'][:, b * 128:(b + 1) * 128], rhs=s['rtw'][:], start=True, stop=True),
              r=[k(p['w2p']), k(s['rtw'])], w=[kw])
        for b in range(2):
            A(lambda e, b=b: e.activation(out=s['ra1'][:, b * 128:(b + 1) * 128], in_=bw[:, b * 128:(b + 1) * 128], func=AF.Identity, bias=p['w0'][:, b:b + 1]),
              r=[kw, k(p['w0'])], w=[k(s['ra1'])])
            yield
        V(lambda e: e.scalar_tensor_tensor(s['ra2'][:], s['ra1'][:], -1.0, s['ra1'][:], ALU.mult, ALU.max), r=[k(s['ra1'])], w=[k(s['ra2'])])
        yield
        A(lambda e: e.activation(out=s['ra2'][:], in_=s['ra2'][:], func=AF.Exp, scale=-1.0), r=[k(s['ra2'])], w=[k(s['ra2'])])
        yield
        A(lambda e: e.activation(out=s['ra2'][:], in_=s['ra2'][:], func=AF.Ln, bias=1.0), r=[k(s['ra2'])], w=[k(s['ra2'])])
        yield
        V(lambda e: e.tensor_scalar(s['ra3'][:], s['ra1'][:], -1.0, 0.0, ALU.mult, ALU.max), r=[k(s['ra1'])], w=[k(s['ra3'])])
        yield
        V(lambda e: e.tensor_tensor(s['ra3'][:], s['ra3'][:], s['ra2'][:], ALU.add), r=[k(s['ra3']), k(s['ra2'])], w=[k(s['ra3'])])
        yield
        A(lambda e: e.activation(out=s['ra1'][:], in_=s['ra3'][:], func=AF.Exp, scale=-1.0), r=[k(s['ra3'])], w=[k(s['ra1'])])
        yield
        V(lambda e: e.tensor_scalar(s['ra1'][:], s['ra1'][:], -float(np.exp(-0.5)), None, ALU.mult), r=[k(s['ra1'])], w=[k(s['ra1'])])
        yield
        V(lambda e: e.tensor_tensor_scan(s['rcw'][:], c['rmask'][:], s['ra1'][:], 0.0, ALU.mult, ALU.add), r=[k(c['rmask']), k(s['ra1'])], w=[k(s['rcw'])])
        yield
        V(lambda e: e.tensor_tensor(s['ra2'][:], s['rcw'][:], s['ra1'][:], ALU.subtract), r=[k(s['rcw']), k(s['ra1'])], w=[k(s['ra2'])])
        yield
        A(lambda e: e.activation(out=s['recw'][:], in_=s['rcw'][:], func=AF.Exp), r=[k(s['rcw'])], w=[k(s['recw'])])
        yield
        A(lambda e: e.activation(out=s['reicw'][:], in_=s['rcw'][:], func=AF.Exp, scale=-1.0), r=[k(s['rcw'])], w=[k(s['reicw'])])
        yield
        A(lambda e: e.activation(out=s['recwp'][:], in_=s['ra2'][:], func=AF.Exp), r=[k(s['ra2'])], w=[k(s['recwp'])])
        yield
        ba_, ka_ = self.bank('r')
        for b in range(2):
            M(lambda e, b=b: e.matmul(ba_[:, b * 128:(b + 1) * 128], lhsT=p['a2p'][:, b * 128:(b + 1) * 128], rhs=lr, start=True, stop=True),
              r=[k(p['a2p']), ksh], w=[ka_])
        for b in range(2):
            A(lambda e, b=b: e.activation(out=s['ra'][:, b * 128:(b + 1) * 128], in_=ba_[:, b * 128:(b + 1) * 128], func=AF.Sigmoid, bias=p['a0'][:, b:b + 1]),
              r=[ka_, k(p['a0'])], w=[k(s['ra'])])
            yield
        A(lambda e: e.activation(out=s['rsg'][:], in_=lr, func=AF.Sigmoid), r=[ksh], w=[k(s['rsg'])])
        yield
        bg, kg = self.bank('r')
        M(lambda e: e.matmul(bg[:, 0:256], lhsT=s['rsg'][:], rhs=p['g2p'][:], start=True, stop=True), r=[k(s['rsg']), k(p['g2p'])], w=[kg])
        A(lambda e: e.copy(out=s['rg'][:], in_=bg[:, 0:256]), r=[kg], w=[k(s['rg'])])
        yield
        V(lambda e: e.tensor_tensor(s['rkk'][:].rearrange("p (a b) -> p a b", b=128), kT.rearrange("p (a b) -> p a b", b=128), self.bc(p['kk'][:, :], [128, 2, 128], 2), ALU.mult),
          r=[ksh, k(p['kk'])], w=[k(s['rkk'])])
        yield
        A(lambda e: e.activation(out=s['ra2'][:], in_=s['rkk'][:], func=AF.Square), r=[k(s['rkk'])], w=[k(s['ra2'])])
        yield
        bn, kn = self.bank('r')
        for b in range(2):
            M(lambda e, b=b: e.matmul(bn[:, b * 128:(b + 1) * 128], lhsT=c['bones'][:], rhs=s['ra2'][:, b * 128:(b + 1) * 128], start=True, stop=True),
              r=[k(c['bones']), k(s['ra2'])], w=[kn])
        V(lambda e: e.tensor_scalar(s['ra3'][:], bn[:, 0:256], 1e-12, None, ALU.add), r=[kn], w=[k(s['ra3'])])
        yield
        A(lambda e: e.activation(out=s['ra3'][:], in_=s['ra3'][:], func=AF.Sqrt), r=[k(s['ra3'])], w=[k(s['ra3'])])
        yield
        V(lambda e: e.reciprocal(s['ra3'][:], s['ra3'][:]), r=[k(s['ra3'])], w=[k(s['ra3'])])
        yield
        V(lambda e: e.tensor_tensor(s['rkk'][:], s['rkk'][:], s['ra3'][:], ALU.mult), r=[k(s['rkk']), k(s['ra3'])], w=[k(s['rkk'])])
        yield
        for b in range(2):
            V(lambda e, b=b: e.tensor_scalar(s['ra2'][:, b * 128:(b + 1) * 128], s['ra'][:, b * 128:(b + 1) * 128], p['ka'][:, b:b + 1], p['omka'][:, b:b + 1], ALU.mult, ALU.add),
              r=[k(s['ra']), k(p['ka']), k(p['omka'])], w=[k(s['ra2'])])
            yield
        V(lambda e: e.tensor_tensor(s['rkp'][:], kT, s['ra2'][:], ALU.mult), r=[ksh, k(s['ra2'])], w=[k(s['rkp'])])
        yield
        V(lambda e: e.tensor_tensor(s['ra3'][:], s['rkk'][:], s['ra'][:], ALU.mult), r=[k(s['rkk']), k(s['ra'])], w=[k(s['ra3'])])
        yield
        for hh in range(2):
            V(lambda e, hh=hh: e.scalar_tensor_tensor(s['rAm'][:, hh, :], s['rkk'][:], c['nhm'][:, hh:hh + 1], s['recwp'][:], ALU.mult, ALU.mult),
              r=[k(s['rkk']), k(c['nhm']), k(s['recwp'])], w=[k(s['rAm'])])
            yield
            V(lambda e, hh=hh: e.scalar_tensor_tensor(s['rRm'][:, hh, :], rT, c['hm'][:, hh:hh + 1], s['recw'][:], ALU.mult, ALU.mult),
              r=[ksh, k(c['hm']), k(s['recw'])], w=[k(s['rRm'])])
            yield
        G(lambda e: e.tensor_tensor(s['rBt'][:], s['ra3'][:], s['reicw'][:], ALU.mult), r=[k(s['ra3']), k(s['reicw'])], w=[k(s['rBt'])])
        yield
        G(lambda e: e.tensor_tensor(s['rKt'][:], s['rkp'][:], s['reicw'][:], ALU.mult), r=[k(s['rkp']), k(s['reicw'])], w=[k(s['rKt'])])
        yield
        V(lambda e: e.tensor_tensor(s['ra2'][:], rT, s['rkp'][:], ALU.mult), r=[ksh, k(s['rkp'])], w=[k(s['ra2'])])
        yield
        V(lambda e: e.tensor_tensor(s['ra2'][:].rearrange("p (a b) -> p a b", b=128), s['ra2'][:].rearrange("p (a b) -> p a b", b=128), self.bc(p['rk'][:, :], [128, 2, 128], 2), ALU.mult),
          r=[k(s['ra2']), k(p['rk'])], w=[k(s['ra2'])])
        yield
        bb, kb = self.bank('r')
        for b in range(2):
            M(lambda e, b=b: e.matmul(bb[:, b * 2:(b + 1) * 2], lhsT=s['ra2'][:, b * 128:(b + 1) * 128], rhs=c['hm'][:, :], start=True, stop=True),
              r=[k(s['ra2']), k(c['hm'])], w=[kb])
        A(lambda e: e.copy(out=s['rbc'][:], in_=bb[:, 0:4]), r=[kb], w=[k(s['rbc'])])
        yield
        bt, kt = self.bank('r')
        for b in range(2):
            M(lambda e, b=b: e.transpose(bt[:, b * 128:(b + 1) * 128], s['rsh'][:, 4 + b, :], c['identf'][:]), r=[ksh, k(c['identf'])], w=[kt])
        A(lambda e: e.copy(out=s['rvtm'][:], in_=bt[:, 0:256]), r=[kt], w=[k(s['rvtm'])])
        yield
        bt, kt = self.bank('r')
        for cc in range(2):
            for b in range(2):
                M(lambda e, bt=bt, cc=cc, b=b: e.transpose(bt[0:64, cc * 256 + b * 128:cc * 256 + (b + 1) * 128], s['rsh'][:, 4 + b, cc * 64:(cc + 1) * 64], c['identf'][:]),
                  r=[ksh, k(c['identf'])], w=[kt])
        A(lambda e, bt=bt: e.copy(out=s['rvc'][:].rearrange("p a b -> p (a b)"), in_=bt[0:64, :]), r=[kt], w=[k(s['rvc'])])
        yield
        for srct, dst in ((s['rBt'], s['rBc']), (s['rKt'], s['rKc'])):
            bt, kt = self.bank('r')
            pbt = bt[:].bitcast(BF16)
            for cc in range(2):
                for b in range(2):
                    M(lambda e, pbt=pbt, srct=srct, cc=cc, b=b: e.transpose(pbt[0:64, cc * 256 + b * 128:cc * 256 + (b + 1) * 128], srct[:, b * 128 + cc * 64:b * 128 + (cc + 1) * 64], c['identb'][:]),
                      r=[k(srct), k(c['identb'])], w=[kt])
            V(lambda e, pbt=pbt, dst=dst: e.tensor_copy(dst[:].rearrange("p a b -> p (a b)"), pbt[0:64, 0:512]), r=[kt], w=[k(dst)])
            yield
        def amat(lt, lkey, lhh, rt_, rkey, rhh, mask, dst):
            bA, kA = self.bank('r')
            for cc in range(2):
                for h in range(4):
                    b, hh = h // 2, h % 2
                    sl_ = slice(b * 128 + cc * 64, b * 128 + (cc + 1) * 64)
                    la = lt[:, hh, sl_] if lhh else lt[:, sl_]
                    ra_ = rt_[:, hh, sl_] if rhh else rt_[:, sl_]
                    i8 = cc * 4 + h
                    M(lambda e, bA=bA, la=la, ra_=ra_, i8=i8: e.matmul(bA[0:64, i8 * 64:(i8 + 1) * 64], lhsT=la, rhs=ra_, start=True, stop=True), r=[lkey, rkey], w=[kA])
            V(lambda e, bA=bA: e.tensor_tensor(dst[:], h3(bA[0:64, :]), self.bc(mask, [64, 8, 64], 1), ALU.mult), r=[kA, k(c['su'])], w=[k(dst)])
            yield
        kAm, kRm, kBt, kKt = k(s['rAm']), k(s['rRm']), k(s['rBt']), k(s['rKt'])
        yield from amat(s['rAm'], kAm, True, s['rBt'], kBt, False, c['su'][0:64, 0:64], s['rP'])
        yield from amat(s['rBt'], kBt, False, s['rAm'], kAm, True, c['sl'][0:64, 0:64], s['rQ'])
        yield from amat(s['rKt'], kKt, False, s['rAm'], kAm, True, c['sl'][0:64, 0:64], s['rAak'])
        yield from amat(s['rBt'], kBt, False, s['rRm'], kRm, True, c['tri'][0:64, 0:64], s['rArb'])
        yield from amat(s['rKt'], kKt, False, s['rRm'], kRm, True, c['tri'][0:64, 0:64], s['rArk'])
        V(lambda e: e.tensor_tensor(s['rTT'][:], s['rQ'][:], self.bc(c['identf'][0:64, 0:64], [64, 8, 64], 1), ALU.add), r=[k(s['rQ']), k(c['identf'])], w=[k(s['rTT'])])
        yield
        Pc, Qc, Pn, Qn = s['rP'], s['rQ'], s['rP2'], s['rQ2']
        for lvl in range(5):
            bP, kP = self.bank('r')
            for i8 in range(8):
                M(lambda e, bP=bP, i8=i8, Pc=Pc, Qc=Qc: e.matmul(bP[0:64, i8 * 64:(i8 + 1) * 64], lhsT=Qc[:, i8, :], rhs=Pc[:, i8, :], start=True, stop=True),
                  r=[k(Pc), k(Qc)], w=[kP])
            A(lambda e, bP=bP, Pn=Pn: e.copy(out=Pn[:], in_=h3(bP[0:64, :])), r=[kP], w=[k(Pn)])
            yield
            if lvl < 4:
                bQ, kQ = self.bank('r')
                for i8 in range(8):
                    M(lambda e, bQ=bQ, i8=i8, Pc=Pc, Qc=Qc: e.matmul(bQ[0:64, i8 * 64:(i8 + 1) * 64], lhsT=Pc[:, i8, :], rhs=Qc[:, i8, :], start=True, stop=True),
                      r=[k(Pc), k(Qc)], w=[kQ])
                V(lambda e, bQ=bQ, Qn=Qn: e.tensor_copy(Qn[:], h3(bQ[0:64, :])), r=[kQ], w=[k(Qn)])
                yield
            bT, kT_ = self.bank('r')
            for i8 in range(8):
                M(lambda e, bT=bT, i8=i8, Pn=Pn: e.matmul(bT[0:64, i8 * 64:(i8 + 1) * 64], lhsT=Pn[:, i8, :], rhs=s['rTT'][:, i8, :], start=True, stop=True),
                  r=[k(Pn), k(s['rTT'])], w=[kT_])
            V(lambda e, bT=bT: e.tensor_tensor(s['rTT'][:], s['rTT'][:], h3(bT[0:64, :]), ALU.add), r=[k(s['rTT']), kT_], w=[k(s['rTT'])])
            yield
            Pc, Qc, Pn, Qn = Pn, Qn, Pc, Qc
        bG, kG = self.bank('r')
        for cc in range(2):
            for h in range(4):
                i8 = cc * 4 + h
                M(lambda e, cc=cc, h=h, i8=i8: e.matmul(bG[0:64, i8 * 64:(i8 + 1) * 64], lhsT=s['rAak'][:, i8, :], rhs=s['rvc'][:, cc, h * 64:(h + 1) * 64], start=True, stop=True),
                  r=[k(s['rAak']), k(s['rvc'])], w=[kG])
        A(lambda e: e.copy(out=s['rAak'][:], in_=h3(bG[0:64, :])), r=[kG], w=[k(s['rAak'])])
        yield
        ewc = s['recw'][:].rearrange("p (a b) -> p a b", b=64)[:, :, 63]
        for cc in range(2):
            bG1, kG1 = self.bank('r')
            for h in range(4):
                b, hh = h // 2, h % 2
                sl_ = slice(b * 128 + cc * 64, b * 128 + (cc + 1) * 64)
                M(lambda e, bG1=bG1, h=h, b=b, hh=hh, sl_=sl_: e.matmul(bG1[0:64, h * 64:(h + 1) * 64], lhsT=s['rAm'][:, hh, sl_], rhs=s['rSTb'][:, b, :], start=True, stop=True),
                  r=[kAm, k(s['rSTb'])], w=[kG1])
            bY1, kY1 = self.bank('r')
            for h in range(4):
                b, hh = h // 2, h % 2
                sl_ = slice(b * 128 + cc * 64, b * 128 + (cc + 1) * 64)
                M(lambda e, bY1=bY1, h=h, b=b, hh=hh, sl_=sl_, cc=cc: e.matmul(bY1[cc * 64:(cc + 1) * 64, h * 64:(h + 1) * 64], lhsT=s['rRm'][:, hh, sl_], rhs=s['rSTb'][:, b, :], start=True, stop=True),
                  r=[kRm, k(s['rSTb'])], w=[kY1])
            A(lambda e, bY1=bY1, cc=cc: e.copy(out=s['rY1'][cc * 64:(cc + 1) * 64, :], in_=bY1[cc * 64:(cc + 1) * 64, 0:256]), r=[kY1], w=[k(s['rY1'])])
            yield
            V(lambda e, bG1=bG1, cc=cc: e.tensor_tensor(s['rG'][:], s['rAak'][:, cc * 4:(cc + 1) * 4, :], h3(bG1[0:64, 0:256]), ALU.add), r=[k(s['rAak']), kG1], w=[k(s['rG'])])
            yield
            bU, kU = self.bank('r')
            for h in range(4):
                i8 = cc * 4 + h
                M(lambda e, bU=bU, h=h, i8=i8: e.matmul(bU[0:64, h * 64:(h + 1) * 64], lhsT=s['rTT'][:, i8, :], rhs=s['rG'][:, h, :], start=True, stop=True),
                  r=[k(s['rTT']), k(s['rG'])], w=[kU])
            A(lambda e, bU=bU: e.copy(out=s['rU'][:], in_=h3(bU[0:64, 0:256])), r=[kU], w=[k(s['rU'])])
            yield
            bY2, kY2 = self.bank('r')
            for h in range(4):
                i8 = cc * 4 + h
                M(lambda e, bY2=bY2, h=h, i8=i8, cc=cc: e.matmul(bY2[cc * 64:(cc + 1) * 64, h * 64:(h + 1) * 64], lhsT=s['rArb'][:, i8, :], rhs=s['rU'][:, h, :], start=True, stop=False),
                  r=[k(s['rArb']), k(s['rU'])], w=[kY2])
                M(lambda e, bY2=bY2, h=h, i8=i8, cc=cc: e.matmul(bY2[cc * 64:(cc + 1) * 64, h * 64:(h + 1) * 64], lhsT=s['rArk'][:, i8, :], rhs=s['rvc'][:, cc, h * 64:(h + 1) * 64], start=False, stop=True),
                  r=[k(s['rArk']), k(s['rvc'])], w=[kY2])
            V(lambda e, bY2=bY2, cc=cc: e.tensor_tensor(s['rY'][cc * 64:(cc + 1) * 64, :], s['rY1'][cc * 64:(cc + 1) * 64, :], bY2[cc * 64:(cc + 1) * 64, 0:256], ALU.add),
              r=[k(s['rY1']), kY2], w=[k(s['rY'])])
            yield
            bS, kS = self.bank('r')
            for h in range(4):
                b, hh = h // 2, h % 2
                i8 = cc * 4 + h
                M(lambda e, bS=bS, h=h, b=b, hh=hh, cc=cc: e.matmul(bS[hh * 64:(hh + 1) * 64, b * 64:(b + 1) * 64], lhsT=s['rBc'][:, cc, h * 64:(h + 1) * 64], rhs=s['rU'][:, h, :], start=True, stop=False),
                  r=[k(s['rBc']), k(s['rU'])], w=[kS])
                M(lambda e, bS=bS, h=h, b=b, hh=hh, cc=cc: e.matmul(bS[hh * 64:(hh + 1) * 64, b * 64:(b + 1) * 64], lhsT=s['rKc'][:, cc, h * 64:(h + 1) * 64], rhs=s['rvc'][:, cc, h * 64:(h + 1) * 64], start=False, stop=True),
                  r=[k(s['rKc']), k(s['rvc'])], w=[kS])
            V(lambda e, bS=bS: e.tensor_tensor(s['rt2'][:], s['rST'][:], h3(bS[:, 0:128]), ALU.add), r=[k(s['rST']), kS], w=[k(s['rt2'])])
            yield
            V(lambda e, cc=cc: e.tensor_tensor(s['rST'][:], s['rt2'][:], self.bc(ewc[:, cc::2], [128, 2, 64], 2), ALU.mult), r=[k(s['rt2']), k(s['recw'])], w=[k(s['rST'])])
            yield
            A(lambda e: e.copy(out=s['rSTb'][:], in_=s['rST'][:]), r=[k(s['rST'])], w=[k(s['rSTb'])])
            yield
        V(lambda e: e.tensor_reduce(s['rm1'][:], h3(s['rY'][:]), AX.X, ALU.add), r=[k(s['rY'])], w=[k(s['rm1'])])
        yield
        A(lambda e: e.activation(out=s['rY1'][:], in_=s['rY'][:], func=AF.Square), r=[k(s['rY'])], w=[k(s['rY1'])])
        yield
        V(lambda e: e.tensor_reduce(s['rm2'][:], h3(s['rY1'][:]), AX.X, ALU.add), r=[k(s['rY1'])], w=[k(s['rm2'])])
        yield
        V(lambda e: e.tensor_scalar(s['rm1'][:], s['rm1'][:], 1.0 / 64, None, ALU.mult), r=[k(s['rm1'])], w=[k(s['rm1'])])
        yield
        V(lambda e: e.tensor_tensor(s['rvar'][:], s['rm1'][:], s['rm1'][:], ALU.mult), r=[k(s['rm1'])], w=[k(s['rvar'])])
        yield
        V(lambda e: e.scalar_tensor_tensor(s['rvar'][:], s['rm2'][:], 1.0 / 64, s['rvar'][:], ALU.mult, ALU.subtract), r=[k(s['rm2']), k(s['rvar'])], w=[k(s['rvar'])])
        yield
        V(lambda e: e.tensor_scalar(s['rvar'][:], s['rvar'][:], GN_EPS, None, ALU.add), r=[k(s['rvar'])], w=[k(s['rvar'])])
        yield
        A(lambda e: e.activation(out=s['rvar'][:], in_=s['rvar'][:], func=AF.Sqrt), r=[k(s['rvar'])], w=[k(s['rvar'])])
        yield
        V(lambda e: e.reciprocal(s['rvar'][:], s['rvar'][:]), r=[k(s['rvar'])], w=[k(s['rvar'])])
        yield
        V(lambda e: e.tensor_tensor(h3(s['rY'][:]), h3(s['rY'][:]), self.bc(s['rm1'][:, :], [128, 4, 64], 2), ALU.subtract), r=[k(s['rY']), k(s['rm1'])], w=[k(s['rY'])])
        yield
        V(lambda e: e.tensor_tensor(h3(s['rY'][:]), h3(s['rY'][:]), self.bc(s['rvar'][:, :], [128, 4, 64], 2), ALU.mult), r=[k(s['rY']), k(s['rvar'])], w=[k(s['rY'])])
        yield
        G(lambda e: e.tensor_tensor(s['rY'][:], s['rY'][:], p['rlg'][:], ALU.mult), r=[k(s['rY']), k(p['rlg'])], w=[k(s['rY'])])
        yield
        G(lambda e: e.tensor_tensor(s['rY'][:], s['rY'][:], p['rlb'][:], ALU.add), r=[k(s['rY']), k(p['rlb'])], w=[k(s['rY'])])
        yield
        V(lambda e: e.tensor_tensor(h3(s['rY1'][:]), h3(s['rvtm'][:]), self.bc(s['rbc'][:, :], [128, 4, 64], 2), ALU.mult), r=[k(s['rvtm']), k(s['rbc'])], w=[k(s['rY1'])])
        yield
        V(lambda e: e.tensor_tensor(s['rY'][:], s['rY'][:], s['rY1'][:], ALU.add), r=[k(s['rY']), k(s['rY1'])], w=[k(s['rY'])])
        yield
        V(lambda e: e.tensor_tensor(s['ycat'][:, 512:768], s['rY'][:], s['rg'][:], ALU.mult), r=[k(s['rY']), k(s['rg'])], w=[k(s['ycat'])])
        yield

    def mixer_epilogue(self, l, i):
        s, p, c = self.s, self.p, self.c
        V, A, G, M = self.V, self.A, self.G, self.M
        k = lambda t: t.key
        self.load('sp', s['htm'][:], self.h_d[i * 128:(i + 1) * 128, :], k(s['htm']), dkeys=["hd_%d" % i])
        if self.debug:
            self.A(lambda e: e.copy(out=s['tmp'][:], in_=s['ycat'][:]), r=[k(s['ycat'])], w=[k(s['tmp'])])
            yield
            self.store('sp', self.dbg_y[i * 128:(i + 1) * 128, :], s['tmp'][:], k(s['tmp']))
            yield
        bt, kt = self.bank('e')
        pb = bt[:].bitcast(BF16)
        for kc in range(8):
            M(lambda e, kc=kc: e.transpose(pb[:, kc * 128:(kc + 1) * 128], s['ycat'][:, kc * 128:(kc + 1) * 128], c['identb'][:]), r=[k(s['ycat']), k(c['identb'])], w=[kt])
        V(lambda e: e.tensor_copy(s['yT'][:].rearrange("p a b -> p (a b)"), pb), r=[kt], w=[k(s['yT'])])
        yield
        for half in range(2):
            bo, ko = self.bank('e')
            for kc in range(8):
                M(lambda e, bo=bo, kc=kc, half=half: e.matmul(bo[:, :], lhsT=s['yT'][:, kc, :], rhs=p['w_out'][:, kc, half * 512:(half + 1) * 512], start=(kc == 0), stop=(kc == 7)),
                  r=[k(s['yT']), k(p['w_out'])], w=[ko])
            V(lambda e, bo=bo, half=half: e.scalar_tensor_tensor(s['mix'][:, half * 512:(half + 1) * 512], s['htm'][:, half * 512:(half + 1) * 512], ALPHA, bo[:, :], ALU.mult, ALU.add),
              r=[k(s['htm']), ko], w=[k(s['mix'])])
            yield
        yield from self.layernorm(s['mix'], p['l1g'], p['l1b'], s['h1'], s['tmp'])
        tk = "h1d_%d_%d" % (l, i)
        self.store('sp', self.h1_d[i * 128:(i + 1) * 128, :], s['h1'][:], k(s['h1']), dkeys=[tk])
        yield
        self.store('pool', self.h1b_d[i * 128:(i + 1) * 128, :], s['h1'][:], k(s['h1']), dkeys=["h1bd_%d_%d" % (l, i)])
        yield
        if hasattr(self, 'xs_d'):
            yield from self.router_tile(l, i)

    def stage0(self):
        s, p, d = self.s, self.p, self.d
        k = lambda t: t.key
        mark = self.sb_off
        self.load('sp', p['l1g'][:], d['ln_in_g'].ap().partition_broadcast(128), k(p['l1g']))
        self.load('sp', p['l1b'][:], d['ln_in_b'].ap().partition_broadcast(128), k(p['l1b']))
        sets = [dict(x=s['mix'], h=s['htm'], tmp=s['tmp'], hb=s['ycat'], hT=s['hT'], st=self.ln_stats("a"))]
        hb2 = Tile(s['yT'].t.ap().rearrange("p a b -> p (a b)") if False else s['yT'].t, s['yT'].key)
        sets.append(dict(x=s['h1'], h=self.sb([128, D], name="s0_h"), tmp=self.sb([128, D], name="s0_tmp"),
                         hb=hb2, hT=self.sb([128, 8, 128], BF16, "s0_hT"), st=self.ln_stats("b")))

        def body(i):
            B = sets[i % 2]
            self.load('sp', B['x'][:], d['x'][i * 128:(i + 1) * 128, :], k(B['x']))
            yield
            yield from self.layernorm(B['x'], p['l1g'], p['l1b'], B['h'], B['tmp'], B['st'])
            self.store('sp', self.h_d[i * 128:(i + 1) * 128, :], B['h'][:], k(B['h']), dkeys=["hd_%d" % i])
            yield
            hbf = B['hb'][:] if len(B['hb'][:].shape) == 2 else B['hb'][:].rearrange("p a b -> p (a b)")
            self.A(lambda e: e.copy(out=hbf, in_=B['h'][:]), r=[k(B['h'])], w=[k(B['hb'])])
            yield
            bk, bkey = self.bank()
            pb = bk[:].bitcast(BF16)
            for kc in range(8):
                self.M(lambda e, kc=kc: e.transpose(pb[:, kc * 128:(kc + 1) * 128], hbf[:, kc * 128:(kc + 1) * 128], self.c['identb'][:]),
                       r=[k(B['hb']), k(self.c['identb'])], w=[bkey])
            self.V(lambda e: e.tensor_copy(B['hT'][:].rearrange("p a b -> p (a b)"), pb), r=[bkey], w=[k(B['hT'])])
            yield
            self.store('sp', self.hT_d[:, :, i * 128:(i + 1) * 128], B['hT'][:], k(B['hT']), dkeys=["hTd_%d" % i])
            yield
        self.run_pipe([lambda i=i: body(i) for i in range(self.NT)], 2)
        self.sb_off = mark

    def stageM(self, l):
        import os
        s = self.s
        k = lambda t: t.key
        only = os.environ.get("ONLY", "srg")
        prev = None
        for i in range(self.NT + 1):
            gens = []
            if i < self.NT:
                self.load('sp', s['hT'][:], self.hT_d[:, :, i * 128:(i + 1) * 128], k(s['hT']), dkeys=["hTd_%d" % i])
                if 'r' in only:
                    gens.append(self.rwkv_tile(i == 0))
                if 's' in only:
                    gens.append(self.ssd_tile(i == 0))
                if 'g' in only:
                    gens.append(self.gla_tile(i == 0))
            if prev is not None:
                gens.append(self.mixer_epilogue(l, prev))
            prev = i if i < self.NT else None
            wts = [int(x) for x in os.environ.get("ILW", "3,1,1,1").split(",")]
            gw = {id(g_): (wts[0] if j == 0 and i < self.NT and 'r' in only else 1) for j, g_ in enumerate(gens)}
            while gens:
                for g_ in list(gens):
                    for _ in range(gw[id(g_)]):
                        try:
                            next(g_)
                        except StopIteration:
                            gens.remove(g_)
                            break

    def build_mixer_test(self):
        self.declare_inputs()
        T = self.T
        self.h_d = self.dscr("h_d", [T, D])
        self.hT_d = self.dscr("hT_d", [128, 8, T], BF16)
        self.h1_d = self.dout("h1_d", [T, D])
        self.h1b_d = self.dscr("h1b_d", [T, D], BF16)
        self.dbg_y = self.dout("dbg_y", [T, D])
        self.consts()
        self.alloc_params()
        self.alloc_mixer()
        print("sbuf peak", self.sb_peak)
        self.stage0()
        self.P.barrier()
        self.load_params(0)
        self.stageM(0)
        self.P.barrier()
        return self.nc

    def alloc_router(self):
        rt = self.rt = {}
        for n, w in (('lg', 36), ('gmx', 1), ('goh', 4), ('gex', 4), ('gsum', 1), ('t32', 32), ('el8', 8), ('el8m', 8), ('l1', 1), ('l2', 1),
                     ('oh1', 8), ('oh2', 8), ('w1', 1), ('w2', 1), ('E1', 32), ('E2', 32), ('Mm', 32), ('rk', 32)):
            rt[n] = self.sb([128, w], name="rt_" + n)

    def alloc_route_persist(self):
        rp = self.rp = {}
        NT = self.NT
        rp['eid'] = self.sb([128, NT * 2], name="rp_eid")
        rp['rnk'] = self.sb([128, NT * 2], name="rp_rnk")
        rp['gat'] = self.sb([128, NT * 2], name="rp_gat")
        rp['cnt'] = self.sb([128, NE], name="rp_cnt")
        rp['iota32'] = self.sb([128, NE], name="rp_iota32")
        ii = self.sb([128, NE], I32, "rp_iota32i")
        self.G(lambda e: e.iota(ii[:], pattern=[[1, NE]], base=0, channel_multiplier=0), w=[ii.key])
        self.V(lambda e: e.tensor_copy(rp['iota32'][:], ii[:]), r=[ii.key], w=[rp['iota32'].key])

    def router_tile(self, l, i):
        s, p, c = self.s, self.p, self.c
        V, A, G, M = self.V, self.A, self.G, self.M
        k = lambda t: t.key
        rt, rp = self.rt, self.rp
        if i == 0:
            G(lambda e: e.memset(rp['cnt'][:], 0.0), w=[k(rp['cnt'])])
            yield
        hT32 = s['tmp'][:].rearrange("p (a b) -> p a b", b=128)
        for half in range(2):
            bt, kt = self.bank('e')
            for j in range(4):
                kc = half * 4 + j
                M(lambda e, bt=bt, j=j, kc=kc: e.transpose(bt[:, j * 128:(j + 1) * 128], s['h1'][:, kc * 128:(kc + 1) * 128], c['identf'][:]), r=[k(s['h1']), k(c['identf'])], w=[kt])
            A(lambda e, bt=bt, half=half: e.copy(out=s['tmp'][:, half * 512:(half + 1) * 512], in_=bt[:, :]), r=[kt], w=[k(s['tmp'])])
            yield
        bl, kl = self.bank('e')
        for kc in range(8):
            M(lambda e, kc=kc: e.matmul(bl[:, 0:36], lhsT=hT32[:, kc, :], rhs=p['wr'][:, kc, :], start=(kc == 0), stop=(kc == 7)), r=[k(s['tmp']), k(p['wr'])], w=[kl])
        V(lambda e: e.tensor_tensor(rt['lg'][:], bl[:, 0:36], p['rb36'][:], ALU.add), r=[kl, k(p['rb36'])], w=[k(rt['lg'])])
        yield
        V(lambda e: e.tensor_reduce(rt['gmx'][:], rt['lg'][:, 0:4], AX.X, ALU.max), r=[k(rt['lg'])], w=[k(rt['gmx'])])
        yield
        V(lambda e: e.tensor_scalar(rt['goh'][:], rt['lg'][:, 0:4], rt['gmx'][:, 0:1], None, ALU.is_equal), r=[k(rt['lg']), k(rt['gmx'])], w=[k(rt['goh'])])
        yield
        V(lambda e: e.tensor_scalar(rt['gex'][:], rt['lg'][:, 0:4], rt['gmx'][:, 0:1], None, ALU.subtract), r=[k(rt['lg']), k(rt['gmx'])], w=[k(rt['gex'])])
        yield
        A(lambda e: e.activation(out=rt['gex'][:], in_=rt['gex'][:], func=AF.Exp), r=[k(rt['gex'])], w=[k(rt['gex'])])
        yield
        V(lambda e: e.tensor_reduce(rt['gsum'][:], rt['gex'][:], AX.X, ALU.add), r=[k(rt['gex'])], w=[k(rt['gsum'])])
        yield
        V(lambda e: e.reciprocal(rt['gsum'][:], rt['gsum'][:]), r=[k(rt['gsum'])], w=[k(rt['gsum'])])
        yield
        V(lambda e: e.tensor_tensor(rt['t32'][:].rearrange("p (g j) -> p g j", j=8), rt['lg'][:, 4:36].rearrange("p (g j) -> p g j", j=8),
                                    self.bc(rt['goh'][:, :], [128, 4, 8], 2), ALU.mult), r=[k(rt['lg']), k(rt['goh'])], w=[k(rt['t32'])])
        yield
        V(lambda e: e.tensor_reduce(rt['el8'][:], rt['t32'][:].rearrange("p (g j) -> p j g", j=8), AX.X, ALU.add), r=[k(rt['t32'])], w=[k(rt['el8'])])
        yield
        V(lambda e: e.tensor_reduce(rt['l1'][:], rt['el8'][:], AX.X, ALU.max), r=[k(rt['el8'])], w=[k(rt['l1'])])
        yield
        V(lambda e: e.tensor_scalar(rt['oh1'][:], rt['el8'][:], rt['l1'][:, 0:1], None, ALU.is_equal), r=[k(rt['el8']), k(rt['l1'])], w=[k(rt['oh1'])])
        yield
        V(lambda e: e.scalar_tensor_tensor(rt['el8m'][:], rt['oh1'][:], -1e30, rt['el8'][:], ALU.mult, ALU.add), r=[k(rt['oh1']), k(rt['el8'])], w=[k(rt['el8m'])])
        yield
        V(lambda e: e.tensor_reduce(rt['l2'][:], rt['el8m'][:], AX.X, ALU.max), r=[k(rt['el8m'])], w=[k(rt['l2'])])
        yield
        V(lambda e: e.tensor_scalar(rt['oh2'][:], rt['el8m'][:], rt['l2'][:, 0:1], None, ALU.is_equal), r=[k(rt['el8m']), k(rt['l2'])], w=[k(rt['oh2'])])
        yield
        V(lambda e: e.tensor_tensor(rt['w2'][:], rt['l2'][:], rt['l1'][:], ALU.subtract), r=[k(rt['l2']), k(rt['l1'])], w=[k(rt['w2'])])
        yield
        A(lambda e: e.activation(out=rt['w2'][:], in_=rt['w2'][:], func=AF.Exp), r=[k(rt['w2'])], w=[k(rt['w2'])])
        yield
        V(lambda e: e.tensor_scalar(rt['w1'][:], rt['w2'][:], 1.0, None, ALU.add), r=[k(rt['w2'])], w=[k(rt['w1'])])
        yield
        V(lambda e: e.reciprocal(rt['w1'][:], rt['w1'][:]), r=[k(rt['w1'])], w=[k(rt['w1'])])
        yield
        V(lambda e: e.tensor_tensor(rt['w2'][:], rt['w2'][:], rt['w1'][:], ALU.mult), r=[k(rt['w2']), k(rt['w1'])], w=[k(rt['w2'])])
        yield
        V(lambda e: e.tensor_tensor(rp['gat'][:, 2 * i:2 * i + 1], rt['w1'][:], rt['gsum'][:], ALU.mult), r=[k(rt['w1']), k(rt['gsum'])], w=[k(rp['gat'])])
        yield
        V(lambda e: e.tensor_tensor(rp['gat'][:, 2 * i + 1:2 * i + 2], rt['w2'][:], rt['gsum'][:], ALU.mult), r=[k(rt['w2']), k(rt['gsum'])], w=[k(rp['gat'])])
        yield
        for E, oh in ((rt['E1'], rt['oh1']), (rt['E2'], rt['oh2'])):
            V(lambda e, E=E, oh=oh: e.tensor_tensor(E[:].rearrange("p (g j) -> p g j", j=8), self.bc(rt['goh'][:, :], [128, 4, 8], 2), self.bc(oh[:, :], [128, 4, 8], 1), ALU.mult),
              r=[k(rt['goh']), k(oh)], w=[k(E)])
            yield
        V(lambda e: e.tensor_tensor(rt['Mm'][:], rt['E1'][:], rt['E2'][:], ALU.add), r=[k(rt['E1']), k(rt['E2'])], w=[k(rt['Mm'])])
        yield
        br, kr = self.bank('e')
        M(lambda e: e.matmul(br[:, 0:32], lhsT=c['sl'][:], rhs=rt['Mm'][:], start=True, stop=True), r=[k(c['sl']), k(rt['Mm'])], w=[kr])
        M(lambda e: e.matmul(br[:, 32:64], lhsT=c['onesf'][:], rhs=rt['Mm'][:], start=True, stop=True), r=[k(c['onesf']), k(rt['Mm'])], w=[kr])
        V(lambda e: e.tensor_tensor(rt['rk'][:], br[:, 0:32], rp['cnt'][:], ALU.add), r=[kr, k(rp['cnt'])], w=[k(rt['rk'])])
        yield
        V(lambda e: e.tensor_tensor(rp['cnt'][:], rp['cnt'][:], br[:, 32:64], ALU.add), r=[kr, k(rp['cnt'])], w=[k(rp['cnt'])])
        yield
        for j, E in ((0, rt['E1']), (1, rt['E2'])):
            V(lambda e, E=E: e.tensor_tensor(rt['t32'][:], E[:], rt['rk'][:], ALU.mult), r=[k(E), k(rt['rk'])], w=[k(rt['t32'])])
            yield
            V(lambda e, j=j: e.tensor_reduce(rp['rnk'][:, 2 * i + j:2 * i + j + 1], rt['t32'][:], AX.X, ALU.add), r=[k(rt['t32'])], w=[k(rp['rnk'])])
            yield
            V(lambda e, E=E: e.tensor_tensor(rt['t32'][:], E[:], rp['iota32'][:], ALU.mult), r=[k(E), k(rp['iota32'])], w=[k(rt['t32'])])
            yield
            V(lambda e, j=j: e.tensor_reduce(rp['eid'][:, 2 * i + j:2 * i + j + 1], rt['t32'][:], AX.X, ALU.add), r=[k(rt['t32'])], w=[k(rp['eid'])])
            yield

    def stageMoE(self, l, last):
        d, c, rp = self.d, self.c, self.rp
        V, A, G, M = self.V, self.A, self.G, self.M
        k = lambda t: t.key
        mark = self.sb_off
        sb = self.sb
        NT, NB, RB = self.NT, self.NB, self.RB
        NR = RB // 128
        NC = NT * 2
        thr_i = sb([128, 64], I32, "f_thri")
        thr = sb([128, 64], name="f_thr")
        G(lambda e: e.iota(thr_i[:], pattern=[[RB, 64]], base=0, channel_multiplier=0), w=[k(thr_i)])
        V(lambda e: e.tensor_copy(thr[:], thr_i[:]), r=[k(thr_i)], w=[k(thr)])
        big = sb([128, max(NC * NE, NE * 64, NB * NE)], name="f_big")
        nblk = sb([128, NE], name="f_nblk")
        padded = sb([128, NE], name="f_padded")
        pend = sb([128, NE], name="f_pend")
        pstart = sb([128, NE], name="f_pstart")
        cmp3 = big[:, 0:NE * 64].rearrange("p (e m) -> p e m", m=64)
        V(lambda e: e.tensor_tensor(cmp3, self.bc(rp['cnt'][:, :], [128, NE, 64], 2), self.bc(thr[:, :], [128, NE, 64], 1), ALU.is_gt), r=[k(rp['cnt']), k(thr)], w=[k(big)])
        V(lambda e: e.tensor_reduce(nblk[:], cmp3, AX.X, ALU.add), r=[k(big)], w=[k(nblk)])
        V(lambda e: e.tensor_scalar(padded[:], nblk[:], float(RB), None, ALU.mult), r=[k(nblk)], w=[k(padded)])
        V(lambda e: e.tensor_tensor_scan(pend[:], c['onesf'][:, 0:NE], padded[:], 0.0, ALU.mult, ALU.add), r=[k(c['onesf']), k(padded)], w=[k(pend)])
        V(lambda e: e.tensor_tensor(pstart[:], pend[:], padded[:], ALU.subtract), r=[k(pend), k(padded)], w=[k(pstart)])
        oh3 = big[:, 0:NC * NE].rearrange("p (n e) -> p n e", e=NE)
        destf = sb([128, NC], name="f_destf")
        dest = sb([128, NC], I32, "f_dest")
        V(lambda e: e.tensor_tensor(oh3, self.bc(rp['iota32'][:, :], [128, NC, NE], 1), self.bc(rp['eid'][:, :], [128, NC, NE], 2), ALU.is_equal), r=[k(rp['iota32']), k(rp['eid'])], w=[k(big)])
        V(lambda e: e.tensor_tensor(oh3, oh3, self.bc(pstart[:, :], [128, NC, NE], 1), ALU.mult), r=[k(big), k(pstart)], w=[k(big)])
        V(lambda e: e.tensor_reduce(destf[:], oh3, AX.X, ALU.add), r=[k(big)], w=[k(destf)])
        V(lambda e: e.tensor_tensor(destf[:], destf[:], rp['rnk'][:], ALU.add), r=[k(destf), k(rp['rnk'])], w=[k(destf)])
        V(lambda e: e.tensor_copy(dest[:], destf[:]), r=[k(destf)], w=[k(dest)])
        bs_i = sb([128, NB], I32, "f_bsi")
        bstart = sb([128, NB], name="f_bstart")
        be = sb([128, NB], name="f_be")
        G(lambda e: e.iota(bs_i[:], pattern=[[RB, NB]], base=0, channel_multiplier=0), w=[k(bs_i)])
        V(lambda e: e.tensor_copy(bstart[:], bs_i[:]), r=[k(bs_i)], w=[k(bstart)])
        cmpb = big[:, 0:NB * NE].rearrange("p (b e) -> p b e", e=NE)
        V(lambda e: e.tensor_tensor(cmpb, self.bc(pend[:, :], [128, NB, NE], 1), self.bc(bstart[:, :], [128, NB, NE], 2), ALU.is_le), r=[k(pend), k(bstart)], w=[k(big)])
        V(lambda e: e.tensor_reduce(be[:], cmpb, AX.X, ALU.add), r=[k(big)], w=[k(be)])
        V(lambda e: e.tensor_scalar(be[:], be[:], float(NE - 1), None, ALU.min), r=[k(be)], w=[k(be)])
        kp_i = sb([128, 8], I32, "f_kpi")
        kp = sb([128, 8], name="f_kp")
        G(lambda e: e.iota(kp_i[:], pattern=[[128, 8]], base=0, channel_multiplier=1), w=[k(kp_i)])
        V(lambda e: e.tensor_copy(kp[:], kp_i[:]), r=[k(kp_i)], w=[k(kp)])
        widf = sb([128, NB, 8], name="f_widf")
        wid = sb([128, NB, 8], I32, "f_wid")
        didf = sb([128, NB, 4], name="f_didf")
        did = sb([128, NB, 4], I32, "f_did")
        bew = sb([128, NB], name="f_bew")
        V(lambda e: e.tensor_scalar(bew[:], be[:], float(D), float(l * NE * D), ALU.mult, ALU.add), r=[k(be)], w=[k(bew)])
        V(lambda e: e.tensor_tensor(widf[:], self.bc(bew[:, :], [128, NB, 8], 2), self.bc(kp[:, :], [128, NB, 8], 1), ALU.add), r=[k(bew), k(kp)], w=[k(widf)])
        V(lambda e: e.tensor_copy(wid[:], widf[:]), r=[k(widf)], w=[k(wid)])
        V(lambda e: e.tensor_scalar(bew[:], be[:], float(FF), float(l * NE * FF), ALU.mult, ALU.add), r=[k(be)], w=[k(bew)])
        V(lambda e: e.tensor_tensor(didf[:], self.bc(bew[:, :], [128, NB, 4], 2), self.bc(kp[:, 0:4], [128, NB, 4], 1), ALU.add), r=[k(bew), k(kp)], w=[k(didf)])
        V(lambda e: e.tensor_copy(did[:], didf[:]), r=[k(didf)], w=[k(did)])
        hbs = [sb([128, D], BF16, "e_hb%d" % j) for j in range(2)]
        for i in range(NT):
            hb = hbs[i % 2]
            self.load('sp', hb[:], self.h1b_d[i * 128:(i + 1) * 128, :], k(hb), dkeys=["h1bd_%d_%d" % (l, i)])
            for j in range(2):
                col = 2 * i + j
                self.P.dma('pool', lambda e, col=col, hb=hb: e.indirect_dma_start(out=self.xs_d.ap(), out_offset=bass.IndirectOffsetOnAxis(ap=dest[:, col:col + 1], axis=0),
                                                                                  in_=hb[:], in_offset=None), k(hb), r=[k(hb), k(dest)], w=["xs_d"])
        self.P.barrier()
        import os
        mstop = int(os.environ.get('MOESTOP', '9'))
        if mstop <= 1:
            self.sb_off = mark
            return
        wg = [sb([128, 8, FF], BF16, "e_wg%d" % j) for j in range(2)]
        wu = [sb([128, 8, FF], BF16, "e_wu%d" % j) for j in range(2)]
        wd = [sb([128, 4, D], BF16, "e_wd%d" % j) for j in range(2)]
        xs = [sb([128, NR, D], BF16, "e_xs%d" % j) for j in range(2)]
        xsT = sb([128, 8, RB], BF16, "e_xsT")
        hT = sb([128, 4, RB], BF16, "e_hT")
        sg = sb([128, RB], name="e_sg")
        ys = [sb([128, D], name="e_ys%d" % j) for j in range(2)]
        tg, tu, td = d['moe_w_gate'].ap(), d['moe_w_up'].ap(), d['moe_w_down'].ap()
        nys = 0
        for b in range(NB):
            j = b % 2
            for kc in range(8):
                self.P.dma('pool', lambda e, j=j, b=b, kc=kc: e.indirect_dma_start(out=wg[j][:, kc, :], out_offset=None, in_=tg,
                                                                                  in_offset=bass.IndirectOffsetOnAxis(ap=wid[:, b, kc:kc + 1], axis=0)), k(wg[j]), r=[k(wid)], w=[k(wg[j])])
                self.P.dma('pool', lambda e, j=j, b=b, kc=kc: e.indirect_dma_start(out=wu[j][:, kc, :], out_offset=None, in_=tu,
                                                                                  in_offset=bass.IndirectOffsetOnAxis(ap=wid[:, b, kc:kc + 1], axis=0)), k(wu[j]), r=[k(wid)], w=[k(wu[j])])
            for fc in range(4):
                self.P.dma('pool', lambda e, j=j, b=b, fc=fc: e.indirect_dma_start(out=wd[j][:, fc, :], out_offset=None, in_=td,
                                                                                  in_offset=bass.IndirectOffsetOnAxis(ap=did[:, b, fc:fc + 1], axis=0)), k(wd[j]), r=[k(did)], w=[k(wd[j])])
            self.load('sp', xs[j][:], self.xs_d[b * RB:(b + 1) * RB, :].rearrange("(r p) n -> p r n", p=128), k(xs[j]), dkeys=["xs_d"])
            for r_ in range(NR):
                bt, kt = self.bank()
                pb = bt[:].bitcast(BF16)
                for kc in range(8):
                    M(lambda e, pb=pb, j=j, r_=r_, kc=kc: e.transpose(pb[:, kc * 128:(kc + 1) * 128], xs[j][:, r_, kc * 128:(kc + 1) * 128], c['identb'][:]), r=[k(xs[j]), k(c['identb'])], w=[kt])
                V(lambda e, pb=pb, r_=r_: e.tensor_copy(xsT[:, :, r_ * 128:(r_ + 1) * 128], pb.rearrange("p (a b) -> p a b", b=128)), r=[kt], w=[k(xsT)])
            for fc in range(4):
                bg, kg = self.bank()
                for kc in range(8):
                    M(lambda e, bg=bg, kc=kc, fc=fc, j=j: e.matmul(bg[:, 0:RB], lhsT=wg[j][:, kc, fc * 128:(fc + 1) * 128], rhs=xsT[:, kc, :], start=(kc == 0), stop=(kc == 7)),
                      r=[k(wg[j]), k(xsT)], w=[kg])
                bu, ku = self.bank()
                for kc in range(8):
                    M(lambda e, bu=bu, kc=kc, fc=fc, j=j: e.matmul(bu[:, 0:RB], lhsT=wu[j][:, kc, fc * 128:(fc + 1) * 128], rhs=xsT[:, kc, :], start=(kc == 0), stop=(kc == 7)),
                      r=[k(wu[j]), k(xsT)], w=[ku])
                A(lambda e, bg=bg: e.activation(out=sg[:], in_=bg[:, 0:RB], func=AF.Silu), r=[kg], w=[k(sg)])
                V(lambda e, bu=bu, fc=fc: e.tensor_tensor(hT[:, fc, :], sg[:], bu[:, 0:RB], ALU.mult), r=[k(sg), ku], w=[k(hT)])
            for r_ in range(NR):
                yb = ys[nys % 2]
                nys += 1
                for half in range(2):
                    bo, ko = self.bank()
                    for fc in range(4):
                        M(lambda e, bo=bo, fc=fc, r_=r_, half=half, j=j: e.matmul(bo[:, :], lhsT=hT[:, fc, r_ * 128:(r_ + 1) * 128], rhs=wd[j][:, fc, half * 512:(half + 1) * 512],
                                                                                 start=(fc == 0), stop=(fc == 3)), r=[k(hT), k(wd[j])], w=[ko])
                    if half == 0:
                        A(lambda e, bo=bo, yb=yb: e.copy(out=yb[:, 0:512], in_=bo[:, :]), r=[ko], w=[k(yb)])
                    else:
                        V(lambda e, bo=bo, yb=yb: e.tensor_copy(yb[:, 512:1024], bo[:, :]), r=[ko], w=[k(yb)])
                r0 = b * RB + r_ * 128
                self.store('sp', self.ys_d[r0:r0 + 128, :], yb[:], k(yb), dkeys=["ys_d"])
        self.P.barrier()
        if mstop <= 2:
            self.sb_off = mark
            return
        l2g = sb([128, D], name="c_l2g")
        l2b = sb([128, D], name="c_l2b")
        self.load('sp', l2g[:], d['ln2_g'][l].partition_broadcast(128), k(l2g))
        self.load('sp', l2b[:], d['ln2_b'][l].partition_broadcast(128), k(l2b))
        csets = []
        for j in range(2):
            csets.append(dict(h1=sb([128, D], name="c_h1%d" % j), y0=sb([128, D], name="c_y0%d" % j), y1=sb([128, D], name="c_y1%d" % j),
                              tmp=sb([128, D], name="c_tmp%d" % j), h2=sb([128, D], name="c_h2%d" % j), hb=sb([128, D], BF16, "c_hb%d" % j),
                              hT=sb([128, 8, 128], BF16, "c_hT%d" % j), st=self.ln_stats("c%d" % j)))

        def cbody(i):
            B = csets[i % 2]
            h1, y0, y1 = B['h1'], B['y0'], B['y1']
            self.load('sp', h1[:], self.h1_d[i * 128:(i + 1) * 128, :], k(h1), dkeys=["h1d_%d_%d" % (l, i)])
            for j, yt in ((0, y0), (1, y1)):
                col = 2 * i + j
                self.P.dma('pool', lambda e, col=col, yt=yt: e.indirect_dma_start(out=yt[:], out_offset=None, in_=self.ys_d.ap(),
                                                                                 in_offset=bass.IndirectOffsetOnAxis(ap=dest[:, col:col + 1], axis=0)), k(yt), r=[k(dest), "ys_d"], w=[k(yt)])
            yield
            V(lambda e: e.tensor_scalar(y0[:], y0[:], rp['gat'][:, 2 * i:2 * i + 1], None, ALU.mult), r=[k(y0), k(rp['gat'])], w=[k(y0)])
            yield
            V(lambda e: e.scalar_tensor_tensor(y0[:], y1[:], rp['gat'][:, 2 * i + 1:2 * i + 2], y0[:], ALU.mult, ALU.add), r=[k(y1), k(y0), k(rp['gat'])], w=[k(y0)])
            yield
            V(lambda e: e.scalar_tensor_tensor(h1[:], h1[:], ALPHA, y0[:], ALU.mult, ALU.add), r=[k(h1), k(y0)], w=[k(h1)])
            yield
            yield from self.layernorm(h1, l2g, l2b, B['h2'], B['tmp'], B['st'])
            if last:
                self.store('sp', self.out_d[i * 128:(i + 1) * 128, :], B['h2'][:], k(B['h2']), dkeys=["out_%d" % i])
                yield
            else:
                self.store('sp', self.h_d[i * 128:(i + 1) * 128, :], B['h2'][:], k(B['h2']), dkeys=["hd_%d" % i])
                yield
                A(lambda e: e.copy(out=B['hb'][:], in_=B['h2'][:]), r=[k(B['h2'])], w=[k(B['hb'])])
                yield
                bk, bkey = self.bank()
                pb = bk[:].bitcast(BF16)
                for kc in range(8):
                    M(lambda e, kc=kc: e.transpose(pb[:, kc * 128:(kc + 1) * 128], B['hb'][:, kc * 128:(kc + 1) * 128], c['identb'][:]), r=[k(B['hb']), k(c['identb'])], w=[bkey])
                V(lambda e: e.tensor_copy(B['hT'][:].rearrange("p a b -> p (a b)"), pb), r=[bkey], w=[k(B['hT'])])
                yield
                self.store('sp', self.hT_d[:, :, i * 128:(i + 1) * 128], B['hT'][:], k(B['hT']), dkeys=["hTd_%d" % i])
                yield
        self.run_pipe([lambda i=i: cbody(i) for i in range(NT)], 2)
        self.P.barrier()
        self.sb_off = mark

    def build_full(self):
        self.declare_inputs()
        T = self.T
        self.h_d = self.dscr("h_d", [T, D])
        self.hT_d = self.dscr("hT_d", [128, 8, T], BF16)
        self.h1_d = self.dscr("h1_d", [T, D])
        self.h1b_d = self.dscr("h1b_d", [T, D], BF16)
        self.xs_d = self.dscr("xs_d", [self.NB * self.RB, D], BF16)
        self.ys_d = self.dscr("ys_d", [self.NB * self.RB, D])
        self.out_d = self.dout("out", [T, D])
        if self.debug:
            self.dbg_y = self.dout("dbg_y", [T, D])
        self.consts()
        self.alloc_route_persist()
        base = self.sb_off
        for l in range(self.depth):
            self.sb_off = base
            self.alloc_params()
            self.alloc_mixer()
            self.alloc_router()
            if l == 0:
                self.stage0()
                self.P.barrier()
            self.load_params(l)
            self.stageM(l)
            self.P.barrier()
            self.sb_off = base
            self.stageMoE(l, l == self.depth - 1)
        self.P.barrier()
        return self.nc


def _host_inputs(inputs, b, T):
    m = {}
    for k, v in inputs.items():
        v = np.asarray(v)
        if k == 'x':
            m[k] = np.ascontiguousarray(v[b, :T])
        elif k == 'rwkv_r_k':
            m[k] = np.ascontiguousarray(v.reshape(DEPTH, 256))
        elif k in ('moe_w_gate', 'moe_w_up'):
            m[k] = np.ascontiguousarray(v.reshape(DEPTH * NE * D, FF))
        elif k == 'moe_w_down':
            m[k] = np.ascontiguousarray(v.reshape(DEPTH * NE * FF, D))
        else:
            m[k] = np.ascontiguousarray(v)
    return m


def kernel(**inputs):
    x = np.asarray(inputs['x'])
    Bsz, T, _ = x.shape
    bld = Builder(T)
    nc = bld.build_full()
    in_maps = [_host_inputs(inputs, b, T) for b in range(Bsz)]
    res = run_bass_kernel_spmd(nc, in_maps, core_ids=list(range(Bsz)))
    return np.stack([np.asarray(r["out"]) for r in res.results], axis=0).astype(np.float32)
```

```python
import numpy as np
import concourse.bass as bass
import concourse.mybir as mybir
from concourse.bass_utils import run_bass_kernel_spmd

F32 = mybir.dt.float32
BF16 = mybir.dt.bfloat16
I32 = mybir.dt.int32
U32 = mybir.dt.uint32
AF = mybir.ActivationFunctionType
ALU = mybir.AluOpType
AX = mybir.AxisListType

D = 1024
NIN = 2968
DEPTH = 2
ALPHA = (2 * DEPTH) ** 0.25
LN_EPS = 1e-5
RMS_EPS = 1e-6
GN_EPS = 64e-5
NE = 32
FF = 512
O_Z, O_XBC, O_DT, O_RW, O_GQ, O_GK, O_GV, O_GG, O_GA = 0, 512, 1280, 1288, 2184, 2312, 2440, 2696, 2952


class Prog:
    EPOCH = 8192
    NDMA = 40

    def __init__(self, nc):
        self.nc = nc
        self.eng = {'pe': nc.tensor, 'dve': nc.vector, 'act': nc.scalar, 'pool': nc.gpsimd, 'sp': nc.sync}
        self.esems = {n: [] for n in ('pe', 'dve', 'act', 'pool')}
        self.cnt = {n: 0 for n in ('pe', 'dve', 'act', 'pool')}
        self.dsems, self.dval, self.dkey = [], [], {}
        self.waited = {n: {} for n in self.eng}
        self.lastw, self.readers = {}, {}
        self.ninst = 0

    def _esem(self, X, ep):
        while len(self.esems[X]) <= ep:
            self.esems[X].append(self.nc.alloc_semaphore("s_%s_%d" % (X, len(self.esems[X]))))
        return self.esems[X][ep]

    def _deps(self, reads, writes):
        deps = {}

        def add(ev):
            if ev is not None and deps.get(ev[0], 0) < ev[1]:
                deps[ev[0]] = ev[1]
        for r in reads:
            add(self.lastw.get(r))
        for w in writes:
            add(self.lastw.get(w))
            for k, v in self.readers.get(w, {}).items():
                add((k, v))
        return deps

    def _wait(self, X, deps):
        e = self.eng[X]
        for k, v in deps.items():
            if k == X and X == 'pe':
                continue
            if self.waited[X].get(k, 0) >= v:
                continue
            if isinstance(k, str):
                ep = (v - 1) // self.EPOCH
                e.wait_ge(self._esem(k, ep), v - ep * self.EPOCH)
            else:
                v = self.dval[k]
                e.wait_ge(self.dsems[k], v)
            self.waited[X][k] = v
            self.ninst += 1

    def _record(self, ev, reads, writes):
        for r in reads:
            d = self.readers.setdefault(r, {})
            if d.get(ev[0], 0) < ev[1]:
                d[ev[0]] = ev[1]
        for w in writes:
            self.lastw[w] = ev
            self.readers[w] = {}

    def op(self, X, fn, r=(), w=()):
        r = [k for k in r if k is not None]
        w = [k for k in w if k is not None]
        w = w + [k for k in r if isinstance(k, str) and k.startswith('psb') and k not in w]
        self._wait(X, self._deps(r, w))
        inst = fn(self.eng[X])
        self.cnt[X] += 1
        n = self.cnt[X]
        inst.then_inc(self._esem(X, (n - 1) // self.EPOCH), 1)
        self.ninst += 1
        self._record((X, n), r, w)

    def dma(self, X, fn, semkey, r=(), w=()):
        base = semkey.rsplit('_', 1)[0] if semkey.rsplit('_', 1)[-1].isdigit() else semkey
        if base not in self.dkey:
            i = len(self.dsems)
            self.dsems.append(self.nc.alloc_semaphore("d_%d" % i))
            self.dval.append(0)
            self.dkey[base] = i
        i = self.dkey[base]
        self._wait(X, self._deps(r, w))
        inst = fn(self.eng[X])
        self.dval[i] += 16
        inst.then_inc(self.dsems[i], 16)
        self.ninst += 1
        self._record((i, self.dval[i]), r, w)

    def barrier(self):
        deps = {k: v for k, v in self.cnt.items() if v > 0}
        for i, v in enumerate(self.dval):
            if v > 0:
                deps[i] = v
        for X in self.eng:
            self._wait(X, dict(deps))


class Tile:
    def __init__(self, t, key):
        self.t, self.key = t, key

    def __getitem__(self, k):
        return self.t[k]


class Builder:
    def __init__(self, T, depth=DEPTH, debug=False, rb=None):
        import os
        rb = rb or int(os.environ.get('RB', '512'))
        self.T, self.depth, self.debug = T, depth, debug
        self.NT = T // 128
        self.RB = rb
        self.NB = (2 * T) // rb + NE
        nc = self.nc = bass.Bass("TRN2", target_bir_lowering=False)
        self.P = Prog(nc)
        self.nsb = 0
        self.sb_off = 16640
        self.sb_peak = 0
        self.sb_cap = 229376
        self.bank_i = 0
        self.chain_i = {}
        self.banks = [nc.alloc_psum_tensor("psb%d" % i, [128, 512], F32) for i in range(8)]
        self.dbg = {}

    def sb(self, shape, dt=F32, name=None):
        self.nsb += 1
        name = "%s_%d" % (name or "t", self.nsb)
        esz = 2 if dt == BF16 else 4
        n = 1
        for v in shape[1:]:
            n *= v
        nbytes = (n * esz + 31) // 32 * 32
        off = self.sb_off
        self.sb_off += nbytes
        assert self.sb_off <= self.sb_cap, "SBUF overflow %d" % self.sb_off
        self.sb_peak = max(self.sb_peak, self.sb_off)
        return Tile(self.nc.alloc_sbuf_tensor_at(name, list(shape), dt, offset=off), name)

    CHAIN_BANKS = {'s': [0, 1], 'r': [2, 3, 4], 'g': [5, 6], 'e': [7]}

    def bank(self, chain=None):
        if chain is None:
            i = self.bank_i
            self.bank_i = (i + 1) % 8
        else:
            lst = self.CHAIN_BANKS[chain]
            j = self.chain_i.get(chain, 0)
            self.chain_i[chain] = (j + 1) % len(lst)
            i = lst[j]
        return self.banks[i], "psb%d" % i

    def V(self, fn, r=(), w=()):
        self.P.op('dve', fn, r, w)

    def A(self, fn, r=(), w=()):
        self.P.op('act', fn, r, w)

    def G(self, fn, r=(), w=()):
        self.P.op('pool', fn, r, w)

    def M(self, fn, r=(), w=()):
        self.P.op('pe', fn, r, w)

    def din(self, name, shape, dt=F32):
        return self.nc.dram_tensor(name, list(shape), dt, kind="ExternalInput")

    def dscr(self, name, shape, dt=F32):
        return self.nc.dram_tensor(name, list(shape), dt, kind="Internal")

    def dout(self, name, shape, dt=F32):
        return self.nc.dram_tensor(name, list(shape), dt, kind="ExternalOutput")

    def load(self, q, out_ap, in_ap, key, dkeys=(), slow=False):
        if slow:
            self.P.dma(q, lambda e: e.dma_start(out=out_ap, in_=in_ap, allow_slow_non_contiguous=True), key, r=list(dkeys), w=[key])
        else:
            self.P.dma(q, lambda e: e.dma_start(out=out_ap, in_=in_ap), key, r=list(dkeys), w=[key])

    def store(self, q, out_ap, in_ap, key, dkeys=()):
        self.P.dma(q, lambda e: e.dma_start(out=out_ap, in_=in_ap), key, r=[key], w=list(dkeys))

    def consts(self):
        nc = self.nc
        c = self.c = {}
        self._ln_st = self.sb([128, 2, 6], name="ln_st")
        self._ln_mv = self.sb([128, 2], name="ln_mv")
        self._ln_rs = self.sb([128, 1], name="ln_rs")
        onesf = c['onesf'] = self.sb([128, 128], name="onesf")
        self.G(lambda e: e.memset(onesf[:], 1.0), w=[onesf.key])
        identf = c['identf'] = self.sb([128, 128], name="identf")
        self.G(lambda e: e.memset(identf[:], 0.0), w=[identf.key])
        self.G(lambda e: e.affine_select(out=identf[:], in_=identf[:], pattern=[[-1, 128]], base=0, channel_multiplier=1,
                                         compare_op=ALU.not_equal, fill=1.0), r=[identf.key], w=[identf.key])
        identb = c['identb'] = self.sb([128, 128], BF16, name="identb")
        self.V(lambda e: e.tensor_copy(identb[:], identf[:]), r=[identf.key], w=[identb.key])
        tri = c['tri'] = self.sb([128, 128], name="tri")
        self.G(lambda e: e.affine_select(out=tri[:], in_=onesf[:], pattern=[[1, 128]], base=0, channel_multiplier=-1,
                                         compare_op=ALU.is_ge, fill=0.0), r=[onesf.key], w=[tri.key])
        su = c['su'] = self.sb([128, 128], name="su")
        self.G(lambda e: e.affine_select(out=su[:], in_=onesf[:], pattern=[[-1, 128]], base=0, channel_multiplier=1,
                                         compare_op=ALU.is_gt, fill=0.0), r=[onesf.key], w=[su.key])
        sl = c['sl'] = self.sb([128, 128], name="sl")
        self.G(lambda e: e.affine_select(out=sl[:], in_=onesf[:], pattern=[[1, 128]], base=0, channel_multiplier=-1,
                                         compare_op=ALU.is_gt, fill=0.0), r=[onesf.key], w=[sl.key])
        maskb = c['maskb'] = self.sb([128, 128], name="maskb")
        self.G(lambda e: e.tensor_copy(maskb[:], tri[:]), r=[tri.key], w=[maskb.key])
        self.G(lambda e: e.memset(maskb[0:64, 64:128], 0.0), w=[maskb.key])
        rmask = c['rmask'] = self.sb([128, 256], name="rmask")
        self.G(lambda e: e.memset(rmask[:], 1.0), w=[rmask.key])
        self.G(lambda e: e.memset(rmask[:].rearrange("p (a b) -> p a b", b=64)[:, :, 0:1], 0.0), w=[rmask.key])
        hm = c['hm'] = self.sb([128, 2], name="hm")
        self.G(lambda e: e.memset(hm[:], 0.0), w=[hm.key])
        self.G(lambda e: e.memset(hm[0:64, 0:1], 1.0), w=[hm.key])
        self.G(lambda e: e.memset(hm[64:128, 1:2], 1.0), w=[hm.key])
        nhm = c['nhm'] = self.sb([128, 2], name="nhm")
        self.V(lambda e: e.tensor_scalar(nhm[:], hm[:], -1.0, None, ALU.mult), r=[hm.key], w=[nhm.key])
        qm = c['qm'] = self.sb([64, 2], name="qm")
        self.G(lambda e: e.memset(qm[:], 0.0), w=[qm.key])
        self.G(lambda e: e.memset(qm[0:32, 0:1], 32.0 ** -0.5), w=[qm.key])
        self.G(lambda e: e.memset(qm[32:64, 1:2], 32.0 ** -0.5), w=[qm.key])
        bones = c['bones'] = self.sb([128, 128], name="bones")
        self.G(lambda e: e.memset(bones[:], 0.0), w=[bones.key])
        self.G(lambda e: e.memset(bones[0:64, 0:64], 1.0), w=[bones.key])
        self.G(lambda e: e.memset(bones[64:128, 64:128], 1.0), w=[bones.key])

    def layernorm(self, xin, gk, bk, out, tmp, stt=None):
        st, mv, rs = stt if stt is not None else (self._ln_st, self._ln_mv, self._ln_rs)
        for i in range(2):
            self.V(lambda e, i=i: e.bn_stats(st[:, i, :], xin[:, i * 512:(i + 1) * 512]), r=[xin.key], w=[st.key])
        self.V(lambda e: e.bn_aggr(mv[:], st[:].rearrange("p a b -> p (a b)")), r=[st.key], w=[mv.key])
        self.V(lambda e: e.tensor_scalar(rs[:], mv[:, 1:2], LN_EPS, None, ALU.add), r=[mv.key], w=[rs.key])
        yield
        self.A(lambda e: e.activation(out=rs[:], in_=rs[:], func=AF.Sqrt), r=[rs.key], w=[rs.key])
        yield
        self.V(lambda e: e.reciprocal(rs[:], rs[:]), r=[rs.key], w=[rs.key])
        self.V(lambda e: e.tensor_scalar(tmp[:], xin[:], mv[:, 0:1], rs[:, 0:1], ALU.subtract, ALU.mult), r=[xin.key, mv.key, rs.key], w=[tmp.key])
        yield
        self.G(lambda e: e.tensor_tensor(tmp[:], tmp[:], gk[:], ALU.mult), r=[tmp.key, gk.key], w=[tmp.key])
        yield
        self.V(lambda e: e.tensor_tensor(out[:], tmp[:], bk[:], ALU.add), r=[tmp.key, bk.key], w=[out.key])
        yield

    def ln_stats(self, tag):
        return (self.sb([128, 2, 6], name="lnst_" + tag), self.sb([128, 2], name="lnmv_" + tag), self.sb([128, 1], name="lnrs_" + tag))

    def run_pipe(self, bodies, width=2):
        active, nxt = [], 0
        while active or nxt < len(bodies):
            while len(active) < width and nxt < len(bodies):
                active.append(bodies[nxt]())
                nxt += 1
            for g_ in list(active):
                try:
                    next(g_)
                except StopIteration:
                    active.remove(g_)

    def to_fm(self, h_tm, hb, hT):
        c = self.c
        self.A(lambda e: e.copy(out=hb[:], in_=h_tm[:]), r=[h_tm.key], w=[hb.key])
        bk, bkey = self.bank()
        pb = bk[:].bitcast(BF16)
        for kc in range(8):
            self.M(lambda e, kc=kc: e.transpose(pb[:, kc * 128:(kc + 1) * 128], hb[:, kc * 128:(kc + 1) * 128], c['identb'][:]),
                   r=[hb.key, c['identb'].key], w=[bkey])
        self.V(lambda e: e.tensor_copy(hT[:].rearrange("p a b -> p (a b)"), pb), r=[bkey], w=[hT.key])

    def declare_inputs(self):
        L = DEPTH
        d = self.d = {}
        specs = dict(x=[self.T, D], ln_in_g=[D], ln_in_b=[D], w_in=[L, D, NIN], ssd_conv_w=[L, 4, 768], ssd_conv_b=[L, 768],
                     ssd_dt_bias=[L, 8], ssd_a_log=[L, 8], ssd_d=[L, 8], ssd_norm_g=[L, 512], rwkv_mu=[L, 896], rwkv_w0=[L, 256],
                     rwkv_w2=[L, 32, 256], rwkv_a0=[L, 256], rwkv_a2=[L, 32, 256], rwkv_g2=[L, 64, 256], rwkv_k_k=[L, 256],
                     rwkv_k_a=[L, 256], rwkv_r_k=[L, 256], rwkv_ln_g=[L, 256], rwkv_ln_b=[L, 256], gla_w_a2=[L, 16, 128],
                     gla_b_a=[L, 128], gla_norm_g=[L, 256], w_out=[L, D, D], ln1_g=[L, D], ln1_b=[L, D], moe_w_rg=[L, D, 4],
                     moe_b_rg=[L, 4], moe_w_re=[L, D, 32], moe_b_re=[L, 32], moe_w_gate=[L * NE * D, FF], moe_w_up=[L * NE * D, FF],
                     moe_w_down=[L * NE * FF, D], ln2_g=[L, D], ln2_b=[L, D])
        for k, s in specs.items():
            d[k] = self.din(k, s)
        return specs

    def alloc_params(self):
        p = self.p = {}
        sb = self.sb
        p['w_in'] = sb([128, 8, NIN], BF16, "w_in_sb")
        p['w_out'] = sb([128, 8, D], BF16, "w_out_sb")
        p['cw'] = sb([128, 6, 4], name="convw")
        p['cb'] = sb([128, 6], name="convb")
        p['cdiag'] = sb([128, 24, 128], BF16, "cdiag")
        for n, w in (('dtb', 8), ('alog', 8), ('dsk8', 8), ('dsk', 512), ('sng', 512), ('rlg', 256), ('rlb', 256), ('gng', 256),
                     ('l1g', D), ('l1b', D), ('rb36', 36)):
            p[n] = sb([128, w], name="p_" + n)
        for n, w in (('mu', 7), ('omu', 7), ('w0', 2), ('a0', 2), ('kk', 2), ('ka', 2), ('omka', 2), ('rk', 2)):
            p[n] = sb([128, w], name="p_" + n)
        p['ba'] = sb([64, 2], name="p_ba")
        p['w2p'] = sb([128, 256], name="p_w2p")
        p['a2p'] = sb([128, 256], name="p_a2p")
        p['g2p'] = sb([128, 256], name="p_g2p")
        p['wa2'] = sb([32, 128], name="p_wa2")
        p['wr'] = sb([128, 8, 36], name="p_wr")

    def load_params(self, l):
        p, d, c = self.p, self.d, self.c
        q = 'pool'
        win = d['w_in'][l].rearrange("(kc p) n -> p kc n", p=128)
        for kc in range(8):
            for (a, b) in ((0, 1484), (1484, NIN)):
                self.load(q, p['w_in'][:, kc, a:b], win[:, kc, a:b], p['w_in'].key)
        wo = d['w_out'][l].rearrange("(kc p) n -> p kc n", p=128)
        for kc in range(8):
            self.load(q, p['w_out'][:, kc, :], wo[:, kc, :], p['w_out'].key)
        q = 'sp'
        for kk_ in range(4):
            self.load(q, p['cw'][:, :, kk_], d['ssd_conv_w'][l][kk_].rearrange("(cb p) -> p cb", p=128), p['cw'].key, slow=True)
        self.load(q, p['cb'][:], d['ssd_conv_b'][l].rearrange("(cb p) -> p cb", p=128), p['cb'].key, slow=True)
        for n, src in (('dtb', 'ssd_dt_bias'), ('alog', 'ssd_a_log'), ('dsk8', 'ssd_d'), ('sng', 'ssd_norm_g'), ('rlg', 'rwkv_ln_g'),
                       ('rlb', 'rwkv_ln_b'), ('gng', 'gla_norm_g'), ('l1g', 'ln1_g'), ('l1b', 'ln1_b')):
            self.load(q, p[n][:], d[src][l].partition_broadcast(128), p[n].key)
        self.load(q, p['rb36'][:, 0:4], d['moe_b_rg'][l].partition_broadcast(128), p['rb36'].key)
        self.load(q, p['rb36'][:, 4:36], d['moe_b_re'][l].partition_broadcast(128), p['rb36'].key)
        self.load(q, p['mu'][:], d['rwkv_mu'][l].rearrange("(b p) -> p b", p=128), p['mu'].key, slow=True)
        for n, src in (('w0', 'rwkv_w0'), ('a0', 'rwkv_a0'), ('kk', 'rwkv_k_k'), ('ka', 'rwkv_k_a'), ('rk', 'rwkv_r_k')):
            self.load(q, p[n][:], d[src][l].rearrange("(b p) -> p b", p=128), p[n].key, slow=True)
        self.load(q, p['ba'][:], d['gla_b_a'][l].rearrange("(b p) -> p b", p=64), p['ba'].key, slow=True)
        for n in ('w2p', 'a2p', 'g2p'):
            self.G(lambda e, n=n: e.memset(p[n][:], 0.0), w=[p[n].key])
        self.load(q, p['w2p'][0:32, :], d['rwkv_w2'][l], p['w2p'].key)
        self.load(q, p['a2p'][32:64, :], d['rwkv_a2'][l], p['a2p'].key)
        self.load(q, p['g2p'][64:128, :], d['rwkv_g2'][l], p['g2p'].key)
        self.G(lambda e: e.memset(p['wa2'][:], 0.0), w=[p['wa2'].key])
        self.load(q, p['wa2'][16:32, :], d['gla_w_a2'][l], p['wa2'].key)
        self.load(q, p['wr'][:, :, 0:4], d['moe_w_rg'][l].rearrange("(kc p) n -> p kc n", p=128), p['wr'].key, slow=True)
        self.load(q, p['wr'][:, :, 4:36], d['moe_w_re'][l].rearrange("(kc p) n -> p kc n", p=128), p['wr'].key, slow=True)
        self.V(lambda e: e.tensor_scalar(p['omu'][:], p['mu'][:], -1.0, 1.0, ALU.mult, ALU.add), r=[p['mu'].key], w=[p['omu'].key])
        self.V(lambda e: e.tensor_scalar(p['omka'][:], p['ka'][:], -1.0, 1.0, ALU.mult, ALU.add), r=[p['ka'].key], w=[p['omka'].key])
        self.A(lambda e: e.activation(out=p['alog'][:], in_=p['alog'][:], func=AF.Exp), r=[p['alog'].key], w=[p['alog'].key])
        self.V(lambda e: e.tensor_scalar(p['alog'][:], p['alog'][:], -1.0, None, ALU.mult), r=[p['alog'].key], w=[p['alog'].key])
        self.V(lambda e: e.tensor_copy(p['dsk'][:].rearrange("p (h q) -> p h q", q=64), p['dsk8'][:].unsqueeze(2).to_broadcast([128, 8, 64])),
               r=[p['dsk8'].key], w=[p['dsk'].key])
        for cb in range(6):
            for k in range(4):
                self.V(lambda e, cb=cb, k=k: e.tensor_scalar(p['cdiag'][:, cb * 4 + k, :], c['identf'][:], p['cw'][:, cb, k:k + 1], None, ALU.mult),
                       r=[c['identf'].key, p['cw'].key], w=[p['cdiag'].key])

    def alloc_mixer(self):
        s = self.s = {}
        sb = self.sb
        s['hT'] = sb([128, 8, 128], BF16, "m_hT")
        s['htm'] = sb([128, D], name="m_htm")
        s['xbc'] = sb([128, 6, 132], BF16, "m_xbc")
        s['xbB'] = sb([128, 6, 132], BF16, "m_xbB")
        s['xc'] = sb([128, 6, 128], BF16, "m_xc")
        s['xh'] = sb([128, 512], BF16, "m_xh")
        s['xdt'] = sb([128, 512], BF16, "m_xdt")
        s['btm'] = sb([128, 128], BF16, "m_btm")
        s['cm'] = sb([128, 2, 128], BF16, "m_cm")
        s['dt'] = sb([128, 8], name="m_dt")
        s['adt'] = sb([128, 8], name="m_adt")
        s['sp1'] = sb([128, 8], name="m_sp1")
        s['sp2'] = sb([128, 8], name="m_sp2")
        s['R'] = sb([128, 4, 128], name="m_R")
        s['seg'] = sb([128, 8, 128], BF16, "m_seg")
        s['ea'] = sb([128, 8], name="m_ea")
        s['cd'] = sb([128, 4], name="m_cd")
        s['cbm'] = sb([128, 2, 128], BF16, "m_cbm")
        s['toend'] = sb([128, 8], name="m_toend")
        s['S32'] = sb([128, 256], name="m_S32")
        s['Sbf'] = sb([128, 256], BF16, "m_Sbf")
        s['y1'] = sb([128, 512], name="m_y1")
        s['sz'] = sb([128, 512], BF16, "m_sz")
        s['ssq'] = sb([128, 4], name="m_ssq")
        s['ycat'] = sb([128, D], BF16, "m_ycat")
        s['yT'] = sb([128, 8, 128], BF16, "m_yT")
        s['gaT'] = sb([32, 128], name="g_gaT")
        s['gx'] = sb([64, 256], name="g_x")
        s['gt1'] = sb([64, 256], name="g_t1")
        s['gcum'] = sb([64, 256], name="g_cum")
        s['geq'] = sb([64, 256], name="g_eq")
        s['gek'] = sb([64, 256], name="g_ek")
        s['gel'] = sb([64, 4], name="g_el")
        s['gqm'] = sb([64, 2, 256], BF16, "g_qm")
        s['gkT'] = sb([64, 256], BF16, "g_kT")
        s['gktm'] = sb([128, 2, 128], BF16, "g_ktm")
        s['gv'] = sb([128, 256], BF16, "g_v")
        s['gvm'] = sb([128, 2, 256], BF16, "g_vm")
        s['gsm'] = sb([128, 4, 128], BF16, "g_sm")
        s['gS'] = sb([64, 2, 64], name="g_S")
        s['gSb'] = sb([64, 2, 2, 64], BF16, "g_Sb")
        s['gst'] = sb([64, 2, 64], name="g_st")
        s['go'] = sb([128, 256], name="g_o")
        s['gsq'] = sb([128, 256], name="g_sq")
        s['grs'] = sb([128, 4], name="g_rs")
        s['gsg'] = sb([128, 256], name="g_sg")
        s['rw'] = sb([128, 7, 129], name="r_rw")
        s['rsh'] = sb([128, 7, 128], name="r_sh")
        s['rt1'] = sb([128, 7, 128], name="r_t1")
        for n in ('ra1', 'ra2', 'ra3', 'rcw', 'recw', 'reicw', 'recwp', 'ra', 'rkk', 'rkp'):
            s[n] = sb([128, 256], name="r_" + n)
        for n in ('rKt', 'rBt'):
            s[n] = sb([128, 256], BF16, "r_" + n)
        s['rAm'] = sb([128, 2, 256], BF16, "r_Am")
        s['rRm'] = sb([128, 2, 256], BF16, "r_Rm")
        s['rtw'] = sb([128, 128], name="r_tw")
        s['rsg'] = sb([128, 128], name="r_sg")
        s['rvtm'] = sb([128, 256], name="r_vtm")
        s['rvc'] = sb([64, 2, 256], BF16, "r_vc")
        s['rBc'] = sb([64, 2, 256], BF16, "r_Bc")
        s['rKc'] = sb([64, 2, 256], BF16, "r_Kc")
        s['rP'] = sb([64, 8, 64], BF16, "r_P")
        s['rQ'] = sb([64, 8, 64], BF16, "r_Q")
        s['rP2'] = sb([64, 8, 64], BF16, "r_P2")
        s['rQ2'] = sb([64, 8, 64], BF16, "r_Q2")
        s['rTT'] = sb([64, 8, 64], BF16, "r_TT")
        s['rAak'] = sb([64, 8, 64], BF16, "r_Aak")
        s['rArb'] = sb([64, 8, 64], BF16, "r_Arb")
        s['rArk'] = sb([64, 8, 64], BF16, "r_Ark")
        s['rG'] = sb([64, 4, 64], BF16, "r_G")
        s['rU'] = sb([64, 4, 64], BF16, "r_U")
        s['rST'] = sb([128, 2, 64], name="r_ST")
        s['rSTb'] = sb([128, 2, 64], BF16, "r_STb")
        s['rt2'] = sb([128, 2, 64], name="r_t2")
        s['rewc'] = sb([128, 4], name="r_ewc")
        s['rY1'] = sb([128, 256], name="r_Y1")
        s['rY'] = sb([128, 256], name="r_Y")
        s['rm1'] = sb([128, 4], name="r_m1")
        s['rm2'] = sb([128, 4], name="r_m2")
        s['rvar'] = sb([128, 4], name="r_var")
        s['rbc'] = sb([128, 4], name="r_bc")
        s['rg'] = sb([128, 256], name="r_g")
        s['mix'] = sb([128, D], name="m_mix")
        s['tmp'] = sb([128, D], name="m_tmp")
        s['h1'] = sb([128, D], name="m_h1")

    def proj_tm(self, out_ap, okey, c0, n):
        s, p = self.s, self.p
        for kc in range(8):
            self.M(lambda e, kc=kc: e.matmul(out_ap, lhsT=s['hT'][:, kc, :], rhs=p['w_in'][:, kc, c0:c0 + n], start=(kc == 0), stop=(kc == 7)),
                   r=[s['hT'].key, p['w_in'].key], w=[okey])

    def proj_fm(self, out_ap, okey, c0, m):
        s, p = self.s, self.p
        for kc in range(8):
            self.M(lambda e, kc=kc: e.matmul(out_ap, lhsT=p['w_in'][:, kc, c0:c0 + m], rhs=s['hT'][:, kc, :], start=(kc == 0), stop=(kc == 7)),
                   r=[s['hT'].key, p['w_in'].key], w=[okey])

    def bc(self, ap, shape, axis):
        return ap.unsqueeze(axis).to_broadcast(list(shape))

    def ssd_tile(self, first):
        s, p, c = self.s, self.p, self.c
        V, A, G, M = self.V, self.A, self.G, self.M
        k = lambda t: t.key
        bz, kz = self.bank('s')
        self.proj_tm(bz[:, :], kz, O_Z, 512)
        A(lambda e: e.activation(out=s['sz'][:], in_=bz[:, :], func=AF.Silu), r=[kz], w=[k(s['sz'])])
        yield
        bd, kd = self.bank('s')
        self.proj_tm(bd[:, 0:8], kd, O_DT, 8)
        V(lambda e: e.tensor_tensor(s['sp1'][:], bd[:, 0:8], p['dtb'][:], ALU.add), r=[kd, k(p['dtb'])], w=[k(s['sp1'])])
        yield
        V(lambda e: e.scalar_tensor_tensor(s['sp2'][:], s['sp1'][:], -1.0, s['sp1'][:], ALU.mult, ALU.max), r=[k(s['sp1'])], w=[k(s['sp2'])])
        yield
        A(lambda e: e.activation(out=s['sp2'][:], in_=s['sp2'][:], func=AF.Exp, scale=-1.0), r=[k(s['sp2'])], w=[k(s['sp2'])])
        yield
        A(lambda e: e.activation(out=s['sp2'][:], in_=s['sp2'][:], func=AF.Ln, bias=1.0), r=[k(s['sp2'])], w=[k(s['sp2'])])
        yield
        V(lambda e: e.scalar_tensor_tensor(s['dt'][:], s['sp1'][:], 0.0, s['sp2'][:], ALU.max, ALU.add), r=[k(s['sp1']), k(s['sp2'])], w=[k(s['dt'])])
        yield
        V(lambda e: e.tensor_tensor(s['adt'][:], s['dt'][:], p['alog'][:], ALU.mult), r=[k(s['dt']), k(p['alog'])], w=[k(s['adt'])])
        yield
        import os
        stop = float(os.environ.get('SSDSTOP', '9'))
        if stop <= 1:
            return
        if first:
            G(lambda e: e.memset(s['xbc'][:, :, 0:4], 0.0), w=[k(s['xbc'])])
            yield
            G(lambda e: e.memset(s['xbB'][:, :, 0:2], 0.0), w=[k(s['xbB'])])
            yield
        else:
            G(lambda e: e.tensor_copy(s['xbc'][:, :, 0:3], s['xbc'][:, :, 128:131]), r=[k(s['xbc'])], w=[k(s['xbc'])])
            yield
            G(lambda e: e.tensor_copy(s['xbB'][:, :, 0:2], s['xbB'][:, :, 128:130]), r=[k(s['xbB'])], w=[k(s['xbB'])])
            yield
        for grp, nb in ((0, 4), (4, 2)):
            bx, kx = self.bank('s')
            for j in range(nb):
                self.proj_fm(bx[:, j * 128:(j + 1) * 128], kx, O_XBC + (grp + j) * 128, 128)
            A(lambda e, bx=bx, grp=grp, nb=nb: e.copy(out=s['xbc'][:, grp:grp + nb, 3:131], in_=bx[:, 0:nb * 128].rearrange("p (a b) -> p a b", b=128)),
              r=[kx], w=[k(s['xbc'])])
            yield
            V(lambda e, bx=bx, grp=grp, nb=nb: e.tensor_copy(s['xbB'][:, grp:grp + nb, 2:130], bx[:, 0:nb * 128].rearrange("p (a b) -> p a b", b=128)),
              r=[kx], w=[k(s['xbB'])])
            yield
        for grp, nb in ((0, 4), (4, 2)):
            bx, kx = self.bank('s')
            for j in range(nb):
                cb = grp + j
                for kk_ in range(4):
                    src = s['xbc'] if kk_ % 2 == 0 else s['xbB']
                    off = kk_ if kk_ % 2 == 0 else kk_ - 1
                    M(lambda e, bx=bx, j=j, cb=cb, kk_=kk_, src=src, off=off: e.matmul(bx[:, j * 128:(j + 1) * 128], lhsT=p['cdiag'][:, cb * 4 + kk_, :],
                                                                                   rhs=src[:, cb, off:off + 128], start=(kk_ == 0), stop=(kk_ == 3)),
                      r=[k(p['cdiag']), k(src)], w=[kx])
            for j in range(nb):
                cb = grp + j
                A(lambda e, bx=bx, j=j, cb=cb: e.activation(out=s['xc'][:, cb, :], in_=bx[:, j * 128:(j + 1) * 128], func=AF.Silu, bias=p['cb'][:, cb:cb + 1]),
                  r=[kx, k(p['cb'])], w=[k(s['xc'])])
                yield
        if stop <= 2:
            return
        bt, kt = self.bank('s')
        pb = bt[:].bitcast(BF16)
        for j in range(5):
            M(lambda e, j=j: e.transpose(pb[:, j * 128:(j + 1) * 128], s['xc'][:, j, :], c['identb'][:]), r=[k(s['xc']), k(c['identb'])], w=[kt])
        V(lambda e: e.tensor_copy(s['xh'][:], pb[:, 0:512]), r=[kt], w=[k(s['xh'])])
        yield
        V(lambda e: e.tensor_copy(s['btm'][:], pb[:, 512:640]), r=[kt], w=[k(s['btm'])])
        yield
        if stop <= 2.2:
            return
        G(lambda e: e.tensor_tensor(s['cm'][:], self.bc(s['xc'][:, 5, :], [128, 2, 128], 1), self.bc(c['hm'][:, :], [128, 2, 128], 2), ALU.mult),
          r=[k(s['xc']), k(c['hm'])], w=[k(s['cm'])])
        yield
        V(lambda e: e.tensor_tensor(s['xdt'][:].rearrange("p (h q) -> p h q", q=64), s['xh'][:].rearrange("p (h q) -> p h q", q=64),
                                    self.bc(s['dt'][:, :], [128, 8, 64], 2), ALU.mult), r=[k(s['xh']), k(s['dt'])], w=[k(s['xdt'])])
        yield
        if stop <= 2.4:
            return
        for half in range(2):
            G(lambda e, half=half: e.tensor_tensor(s['R'][:], self.bc(c['tri'][:, :], [128, 4, 128], 1), self.bc(s['adt'][:, half * 4:(half + 1) * 4], [128, 4, 128], 2), ALU.mult),
              r=[k(c['tri']), k(s['adt'])], w=[k(s['R'])])
            yield
            bD, kD = self.bank('s')
            for q2 in range(2):
                M(lambda e, bD=bD, q2=q2: e.matmul(bD[:, q2 * 256:(q2 + 1) * 256], lhsT=c['su'][:], rhs=s['R'][:, q2 * 2:(q2 + 1) * 2, :].rearrange("p a b -> p (a b)"), start=True, stop=True),
                  r=[k(c['su']), k(s['R'])], w=[kD])
            if stop <= 2.6:
                continue
            A(lambda e, bD=bD, half=half: e.activation(out=s['seg'][:, half * 4:(half + 1) * 4, :].rearrange("p a b -> p (a b)"), in_=bD[:, :], func=AF.Exp),
              r=[kD], w=[k(s['seg'])])
            yield
        if stop <= 2.8:
            return
        V(lambda e: e.tensor_copy(s['toend'][:], s['seg'][:, :, 127]), r=[k(s['seg'])], w=[k(s['toend'])])
        yield
        if stop <= 3:
            return
        be, ke = self.bank('s')
        M(lambda e: e.matmul(be[:, 0:8], lhsT=c['tri'][:], rhs=s['adt'][:], start=True, stop=True), r=[k(c['tri']), k(s['adt'])], w=[ke])
        for g in range(2):
            M(lambda e, g=g: e.matmul(be[g * 64:(g + 1) * 64, 8:12], lhsT=c['onesf'][:, 0:64], rhs=s['adt'][:, g * 4:(g + 1) * 4], start=True, stop=True),
              r=[k(c['onesf']), k(s['adt'])], w=[ke])
        A(lambda e: e.activation(out=s['ea'][:], in_=be[:, 0:8], func=AF.Exp), r=[ke], w=[k(s['ea'])])
        yield
        A(lambda e: e.activation(out=s['cd'][:], in_=be[:, 8:12], func=AF.Exp), r=[ke], w=[k(s['cd'])])
        yield
        if stop <= 4:
            return
        bc_, kc_ = self.bank('s')
        for g in range(2):
            M(lambda e, g=g: e.matmul(bc_[:, g * 128:(g + 1) * 128], lhsT=s['xc'][:, 4, :], rhs=s['cm'][:, g, :], start=True, stop=True),
              r=[k(s['xc']), k(s['cm'])], w=[kc_])
        V(lambda e: e.tensor_tensor(s['cbm'][:], bc_[:, 0:256].rearrange("p (a b) -> p a b", b=128), self.bc(c['tri'][:, :], [128, 2, 128], 1), ALU.mult),
          r=[kc_, k(c['tri'])], w=[k(s['cbm'])])
        yield
        for g in range(2):
            V(lambda e, g=g: e.tensor_tensor(s['seg'][:, g * 4:(g + 1) * 4, :], s['seg'][:, g * 4:(g + 1) * 4, :], self.bc(s['cbm'][:, g, :], [128, 4, 128], 1), ALU.mult),
              r=[k(s['seg']), k(s['cbm'])], w=[k(s['seg'])])
            yield
        by, ky = self.bank('s')
        for h in range(8):
            M(lambda e, h=h: e.matmul(by[:, h * 64:(h + 1) * 64], lhsT=s['seg'][:, h, :], rhs=s['xdt'][:, h * 64:(h + 1) * 64], start=True, stop=True),
              r=[k(s['seg']), k(s['xdt'])], w=[ky])
        bo, ko = self.bank('s')
        if not first:
            for g in range(2):
                M(lambda e, g=g: e.matmul(bo[:, g * 256:(g + 1) * 256], lhsT=s['cm'][:, g, :], rhs=s['Sbf'][:, :], start=True, stop=True),
                  r=[k(s['cm']), k(s['Sbf'])], w=[ko])
            V(lambda e: e.tensor_tensor(s['y1'][:].rearrange("p (h q) -> p h q", q=64), bo[:, :].rearrange("p (h q) -> p h q", q=64),
                                        self.bc(s['ea'][:, :], [128, 8, 64], 2), ALU.mult), r=[ko, k(s['ea'])], w=[k(s['y1'])])
            yield
            V(lambda e: e.tensor_tensor(s['y1'][:], s['y1'][:], by[:, :], ALU.add), r=[k(s['y1']), ky], w=[k(s['y1'])])
            yield
        else:
            V(lambda e: e.tensor_copy(s['y1'][:], by[:, :]), r=[ky], w=[k(s['y1'])])
            yield
        if stop <= 5:
            return
        V(lambda e: e.tensor_tensor(s['xdt'][:].rearrange("p (h q) -> p h q", q=64), s['xdt'][:].rearrange("p (h q) -> p h q", q=64),
                                    self.bc(s['toend'][:, :], [128, 8, 64], 2), ALU.mult), r=[k(s['xdt']), k(s['toend'])], w=[k(s['xdt'])])
        yield
        bs, ks = self.bank('s')
        for g in range(2):
            M(lambda e, g=g: e.matmul(bs[g * 64:(g + 1) * 64, 0:256], lhsT=s['btm'][:, g * 64:(g + 1) * 64], rhs=s['xdt'][:, g * 256:(g + 1) * 256], start=True, stop=True),
              r=[k(s['btm']), k(s['xdt'])], w=[ks])
        if first:
            V(lambda e: e.tensor_copy(s['S32'][:], bs[:, 0:256]), r=[ks], w=[k(s['S32'])])
            yield
        else:
            V(lambda e: e.tensor_tensor(s['S32'][:].rearrange("p (h q) -> p h q", q=64), s['S32'][:].rearrange("p (h q) -> p h q", q=64),
                                        self.bc(s['cd'][:, :], [128, 4, 64], 2), ALU.mult), r=[k(s['S32']), k(s['cd'])], w=[k(s['S32'])])
            yield
            V(lambda e: e.tensor_tensor(s['S32'][:], s['S32'][:], bs[:, 0:256], ALU.add), r=[k(s['S32']), ks], w=[k(s['S32'])])
            yield
        A(lambda e: e.copy(out=s['Sbf'][:], in_=s['S32'][:]), r=[k(s['S32'])], w=[k(s['Sbf'])])
        yield
        G(lambda e: e.tensor_tensor(s['xdt'][:], s['xh'][:], p['dsk'][:], ALU.mult), r=[k(s['xh']), k(p['dsk'])], w=[k(s['xdt'])])
        yield
        V(lambda e: e.tensor_tensor(s['y1'][:], s['y1'][:], s['xdt'][:], ALU.add), r=[k(s['y1']), k(s['xdt'])], w=[k(s['y1'])])
        yield
        V(lambda e: e.tensor_tensor(s['y1'][:], s['y1'][:], s['sz'][:], ALU.mult), r=[k(s['y1']), k(s['sz'])], w=[k(s['y1'])])
        yield
        for g in range(2):
            A(lambda e, g=g: e.activation(out=s['sz'][:, g * 256:(g + 1) * 256], in_=s['y1'][:, g * 256:(g + 1) * 256], func=AF.Square, accum_out=s['ssq'][:, g:g + 1]),
              r=[k(s['y1'])], w=[k(s['sz']), k(s['ssq'])])
            yield
        V(lambda e: e.tensor_scalar(s['ssq'][:, 0:2], s['ssq'][:, 0:2], 1.0 / 256, RMS_EPS, ALU.mult, ALU.add), r=[k(s['ssq'])], w=[k(s['ssq'])])
        yield
        A(lambda e: e.activation(out=s['ssq'][:, 0:2], in_=s['ssq'][:, 0:2], func=AF.Sqrt), r=[k(s['ssq'])], w=[k(s['ssq'])])
        yield
        V(lambda e: e.reciprocal(s['ssq'][:, 0:2], s['ssq'][:, 0:2]), r=[k(s['ssq'])], w=[k(s['ssq'])])
        yield
        for g in range(2):
            V(lambda e, g=g: e.scalar_tensor_tensor(s['ycat'][:, g * 256:(g + 1) * 256], s['y1'][:, g * 256:(g + 1) * 256], s['ssq'][:, g:g + 1],
                                                   p['sng'][:, g * 256:(g + 1) * 256], ALU.mult, ALU.mult), r=[k(s['y1']), k(s['ssq']), k(p['sng'])], w=[k(s['ycat'])])
            yield

    def gla_tile(self, first):
        s, p, c = self.s, self.p, self.c
        V, A, G, M = self.V, self.A, self.G, self.M
        k = lambda t: t.key
        bv, kv = self.bank('g')
        self.proj_tm(bv[:, :], kv, O_GV, 512)
        A(lambda e: e.copy(out=s['gv'][:], in_=bv[:, 0:256]), r=[kv], w=[k(s['gv'])])
        yield
        A(lambda e: e.activation(out=s['gsg'][:], in_=bv[:, 256:512], func=AF.Silu), r=[kv], w=[k(s['gsg'])])
        yield
        G(lambda e: e.tensor_tensor(s['gsg'][:], s['gsg'][:], p['gng'][:], ALU.mult), r=[k(s['gsg']), k(p['gng'])], w=[k(s['gsg'])])
        yield
        ba_, ka_ = self.bank('g')
        self.proj_fm(ba_[0:32, 0:128], ka_, O_GA - 16, 32)
        A(lambda e: e.copy(out=s['gaT'][:], in_=ba_[0:32, 0:128]), r=[ka_], w=[k(s['gaT'])])
        yield
        bx, kx = self.bank('g')
        for pr in range(2):
            M(lambda e, pr=pr: e.matmul(bx[0:64, pr * 128:(pr + 1) * 128], lhsT=p['wa2'][:, pr * 64:(pr + 1) * 64], rhs=s['gaT'][:], start=True, stop=True),
              r=[k(p['wa2']), k(s['gaT'])], w=[kx])
        for pr in range(2):
            A(lambda e, pr=pr: e.activation(out=s['gx'][:, pr * 128:(pr + 1) * 128], in_=bx[0:64, pr * 128:(pr + 1) * 128], func=AF.Identity, bias=p['ba'][:, pr:pr + 1]),
              r=[kx, k(p['ba'])], w=[k(s['gx'])])
            yield
        V(lambda e: e.scalar_tensor_tensor(s['gt1'][:], s['gx'][:], -1.0, s['gx'][:], ALU.mult, ALU.max), r=[k(s['gx'])], w=[k(s['gt1'])])
        yield
        A(lambda e: e.activation(out=s['gt1'][:], in_=s['gt1'][:], func=AF.Exp, scale=-1.0), r=[k(s['gt1'])], w=[k(s['gt1'])])
        yield
        A(lambda e: e.activation(out=s['gt1'][:], in_=s['gt1'][:], func=AF.Ln, bias=1.0), r=[k(s['gt1'])], w=[k(s['gt1'])])
        yield
        V(lambda e: e.scalar_tensor_tensor(s['gx'][:], s['gx'][:], 0.0, s['gt1'][:], ALU.min, ALU.subtract), r=[k(s['gx']), k(s['gt1'])], w=[k(s['gx'])])
        yield
        V(lambda e: e.tensor_tensor_scan(s['gcum'][:], c['rmask'][0:64, :], s['gx'][:], 0.0, ALU.mult, ALU.add), r=[k(c['rmask']), k(s['gx'])], w=[k(s['gcum'])])
        yield
        A(lambda e: e.activation(out=s['geq'][:], in_=s['gcum'][:], func=AF.Exp, scale=1.0 / 16), r=[k(s['gcum'])], w=[k(s['geq'])])
        yield
        A(lambda e: e.activation(out=s['gek'][:], in_=s['gcum'][:], func=AF.Exp, scale=-1.0 / 16), r=[k(s['gcum'])], w=[k(s['gek'])])
        yield
        A(lambda e: e.activation(out=s['gel'][:], in_=s['gcum'][:].rearrange("p (a b) -> p a b", b=64)[:, :, 63], func=AF.Exp, scale=1.0 / 16),
          r=[k(s['gcum'])], w=[k(s['gel'])])
        yield
        bq, kq = self.bank('g')
        for pr in range(2):
            self.proj_fm(bq[0:64, pr * 128:(pr + 1) * 128], kq, O_GQ + pr * 64, 64)
        for pr in range(2):
            self.proj_fm(bq[0:64, 256 + pr * 128:256 + (pr + 1) * 128], kq, O_GK + pr * 64, 64)
        for hh in range(2):
            V(lambda e, hh=hh: e.scalar_tensor_tensor(s['gqm'][:, hh, :], bq[0:64, 0:256], c['qm'][:, hh:hh + 1], s['geq'][:], ALU.mult, ALU.mult),
              r=[kq, k(c['qm']), k(s['geq'])], w=[k(s['gqm'])])
            yield
        V(lambda e: e.tensor_tensor(s['gkT'][:], bq[0:64, 256:512], s['gek'][:], ALU.mult), r=[kq, k(s['gek'])], w=[k(s['gkT'])])
        yield
        bt, kt = self.bank('g')
        pb = bt[:].bitcast(BF16)
        for pr in range(2):
            M(lambda e, pr=pr: e.transpose(pb[:, pr * 64:(pr + 1) * 64], s['gkT'][:, pr * 128:(pr + 1) * 128], c['identb'][0:64, 0:64]),
              r=[k(s['gkT']), k(c['identb'])], w=[kt])
        if first:
            G(lambda e: e.memset(s['gktm'][:], 0.0), w=[k(s['gktm'])])
            yield
        for hh in range(2):
            V(lambda e, hh=hh: e.tensor_copy(s['gktm'][:, hh, :].rearrange("p (a b c) -> p a b c", a=2, b=2)[:, :, hh, :],
                                             pb[:, 0:128].rearrange("p (a b c) -> p a b c", a=2, b=2)[:, :, hh, :]), r=[kt], w=[k(s['gktm'])])
            yield
        G(lambda e: e.tensor_tensor(s['gvm'][:], self.bc(s['gv'][:, :], [128, 2, 256], 1), self.bc(c['hm'][:, :], [128, 2, 256], 2), ALU.mult),
          r=[k(s['gv']), k(c['hm'])], w=[k(s['gvm'])])
        yield
        bs_, ks_ = self.bank('g')
        for h in range(4):
            pr, hh = h // 2, h % 2
            M(lambda e, h=h, pr=pr, hh=hh: e.matmul(bs_[:, h * 128:(h + 1) * 128], lhsT=s['gkT'][:, pr * 128:(pr + 1) * 128], rhs=s['gqm'][:, hh, pr * 128:(pr + 1) * 128],
                                                    start=True, stop=True), r=[k(s['gkT']), k(s['gqm'])], w=[ks_])
        V(lambda e: e.tensor_tensor(s['gsm'][:], bs_[:, :].rearrange("p (a b) -> p a b", b=128), self.bc(c['maskb'][:, :], [128, 4, 128], 1), ALU.mult),
          r=[ks_, k(c['maskb'])], w=[k(s['gsm'])])
        yield
        bu, ku = self.bank('g')
        for cc in range(2):
            for pr in range(2):
                for hh in range(2):
                    h = pr * 2 + hh
                    M(lambda e, cc=cc, h=h, pr=pr, hh=hh: e.matmul(bu[0:64, cc * 128 + pr * 64:cc * 128 + (pr + 1) * 64],
                                                                   lhsT=s['gktm'][:, hh, pr * 64:(pr + 1) * 64], rhs=s['gvm'][:, cc, h * 64:(h + 1) * 64],
                                                                   start=(hh == 0), stop=(hh == 1)), r=[k(s['gktm']), k(s['gvm'])], w=[ku])
        if first:
            G(lambda e: e.memset(s['gS'][:], 0.0), w=[k(s['gS'])])
            yield
        for cc in range(2):
            A(lambda e, cc=cc: e.copy(out=s['gSb'][:, cc, :, :], in_=s['gS'][:]), r=[k(s['gS'])], w=[k(s['gSb'])])
            yield
            V(lambda e, cc=cc: e.tensor_tensor(s['gst'][:], s['gS'][:], bu[0:64, cc * 128:(cc + 1) * 128].rearrange("p (a b) -> p a b", b=64), ALU.add),
              r=[k(s['gS']), ku], w=[k(s['gst'])])
            yield
            V(lambda e, cc=cc: e.tensor_tensor(s['gS'][:], s['gst'][:], self.bc(s['gel'][:, cc::2], [64, 2, 64], 2), ALU.mult),
              r=[k(s['gst']), k(s['gel'])], w=[k(s['gS'])])
            yield
        bo, ko = self.bank('g')
        for h in range(4):
            M(lambda e, h=h: e.matmul(bo[:, h * 64:(h + 1) * 64], lhsT=s['gsm'][:, h, :], rhs=s['gv'][:, h * 64:(h + 1) * 64], start=True, stop=True),
              r=[k(s['gsm']), k(s['gv'])], w=[ko])
        bi, ki = self.bank('g')
        for cc in range(2):
            for h in range(4):
                pr, hh = h // 2, h % 2
                M(lambda e, cc=cc, h=h, pr=pr, hh=hh: e.matmul(bi[cc * 64:(cc + 1) * 64, h * 64:(h + 1) * 64], lhsT=s['gqm'][:, hh, pr * 128 + cc * 64:pr * 128 + (cc + 1) * 64],
                                                               rhs=s['gSb'][:, cc, pr, :], start=True, stop=True), r=[k(s['gqm']), k(s['gSb'])], w=[ki])
        A(lambda e: e.copy(out=s['gsq'][:], in_=bi[:, 0:256]), r=[ki], w=[k(s['gsq'])])
        yield
        V(lambda e: e.tensor_tensor(s['go'][:], s['gsq'][:], bo[:, 0:256], ALU.add), r=[k(s['gsq']), ko], w=[k(s['go'])])
        yield
        A(lambda e: e.activation(out=s['gsq'][:], in_=s['go'][:], func=AF.Square), r=[k(s['go'])], w=[k(s['gsq'])])
        yield
        V(lambda e: e.tensor_reduce(s['grs'][:], s['gsq'][:].rearrange("p (h q) -> p h q", q=64), AX.X, ALU.add), r=[k(s['gsq'])], w=[k(s['grs'])])
        yield
        V(lambda e: e.tensor_scalar(s['grs'][:], s['grs'][:], 1.0 / 64, RMS_EPS, ALU.mult, ALU.add), r=[k(s['grs'])], w=[k(s['grs'])])
        yield
        A(lambda e: e.activation(out=s['grs'][:], in_=s['grs'][:], func=AF.Sqrt), r=[k(s['grs'])], w=[k(s['grs'])])
        yield
        V(lambda e: e.reciprocal(s['grs'][:], s['grs'][:]), r=[k(s['grs'])], w=[k(s['grs'])])
        yield
        V(lambda e: e.tensor_tensor(s['go'][:].rearrange("p (h q) -> p h q", q=64), s['go'][:].rearrange("p (h q) -> p h q", q=64),
                                    self.bc(s['grs'][:, :], [128, 4, 64], 2), ALU.mult), r=[k(s['go']), k(s['grs'])], w=[k(s['go'])])
        yield
        V(lambda e: e.tensor_tensor(s['ycat'][:, 768:1024], s['go'][:], s['gsg'][:], ALU.mult), r=[k(s['go']), k(s['gsg'])], w=[k(s['ycat'])])
        yield

    def rwkv_tile(self, first):
        s, p, c = self.s, self.p, self.c
        V, A, G, M = self.V, self.A, self.G, self.M
        k = lambda t: t.key
        f2 = lambda t, a, b: t[:, a:b, :].rearrange("p a b -> p (a b)")
        h3 = lambda ap: ap.rearrange("p (h q) -> p h q", q=64)
        if first:
            G(lambda e: e.memset(s['rw'][:, :, 0:1], 0.0), w=[k(s['rw'])])
            yield
            G(lambda e: e.memset(s['rST'][:], 0.0), w=[k(s['rST'])])
            yield
            G(lambda e: e.memset(s['rSTb'][:], 0.0), w=[k(s['rSTb'])])
            yield
        else:
            G(lambda e: e.tensor_copy(s['rw'][:, :, 0:1], s['rw'][:, :, 128:129]), r=[k(s['rw'])], w=[k(s['rw'])])
            yield
        for grp, nb in ((0, 4), (4, 3)):
            bx, kx = self.bank('r')
            for j in range(nb):
                self.proj_fm(bx[:, j * 128:(j + 1) * 128], kx, O_RW + (grp + j) * 128, 128)
            A(lambda e, bx=bx, grp=grp, nb=nb: e.copy(out=s['rw'][:, grp:grp + nb, 1:129], in_=bx[:, 0:nb * 128].rearrange("p (a b) -> p a b", b=128)),
              r=[kx], w=[k(s['rw'])])
            yield
        rt1 = s['rt1'][:]
        G(lambda e: e.tensor_tensor(rt1, s['rw'][:, :, 0:128], self.bc(p['mu'][:, :], [128, 7, 128], 2), ALU.mult), r=[k(s['rw']), k(p['mu'])], w=[k(s['rt1'])])
        yield
        V(lambda e: e.tensor_tensor(s['rsh'][:], s['rw'][:, :, 1:129], self.bc(p['omu'][:, :], [128, 7, 128], 2), ALU.mult), r=[k(s['rw']), k(p['omu'])], w=[k(s['rsh'])])
        yield
        V(lambda e: e.tensor_tensor(s['rsh'][:], s['rsh'][:], rt1, ALU.add), r=[k(s['rsh']), k(s['rt1'])], w=[k(s['rsh'])])
        yield
        rT, kT, vT, lr = f2(s['rsh'], 0, 2), f2(s['rsh'], 2, 4), f2(s['rsh'], 4, 6), s['rsh'][:, 6, :]
        ksh = k(s['rsh'])
        A(lambda e: e.activation(out=s['rtw'][:], in_=lr, func=AF.Tanh), r=[ksh], w=[k(s['rtw'])])
        yield
        bw, kw = self.bank('r')
        for b in range(2):
            M(lambda e, b=b: e.matmul(bw[:, b * 128:(b + 1) * 128], lhsT=p['w2p'][:, b * 128:(b + 1) * 128], rhs=s['rtw'][:], start=True, stop=True),
              r=[k(p['w2p']), k(s['rtw'])], w=[kw])
        for b in range(2):
            A(lambda e, b=b: e.activation(out=s['ra1'][:, b * 128:(b + 1) * 128], in_=bw[:, b * 128:(b + 1) * 128], func=AF.Identity, bias=p['w0'][:, b:b + 1]),
              r=[kw, k(p['w0'])], w=[k(s['ra1'])])
            yield
        V(lambda e: e.scalar_tensor_tensor(s['ra2'][:], s['ra1'][:], -1.0, s['ra1'][:], ALU.mult, ALU.max), r=[k(s['ra1'])], w=[k(s['ra2'])])
        yield
        A(lambda e: e.activation(out=s['ra2'][:], in_=s['ra2'][:], func=AF.Exp, scale=-1.0), r=[k(s['ra2'])], w=[k(s['ra2'])])
        yield
        A(lambda e: e.activation(out=s['ra2'][:], in_=s['ra2'][:], func=AF.Ln, bias=1.0), r=[k(s['ra2'])], w=[k(s['ra2'])])
        yield
        V(lambda e: e.tensor_scalar(s['ra3'][:], s['ra1'][:], -1.0, 0.0, ALU.mult, ALU.max), r=[k(s['ra1'])], w=[k(s['ra3'])])
        yield
        V(lambda e: e.tensor_tensor(s['ra3'][:], s['ra3'][:], s['ra2'][:], ALU.add), r=[k(s['ra3']), k(s['ra2'])], w=[k(s['ra3'])])
        yield
        A(lambda e: e.activation(out=s['ra1'][:], in_=s['ra3'][:], func=AF.Exp, scale=-1.0), r=[k(s['ra3'])], w=[k(s['ra1'])])
        yield
        V(lambda e: e.tensor_scalar(s['ra1'][:], s['ra1'][:], -float(np.exp(-0.5)), None, ALU.mult), r=[k(s['ra1'])], w=[k(s['ra1'])])
        yield
        V(lambda e: e.tensor_tensor_scan(s['rcw'][:], c['rmask'][:], s['ra1'][:], 0.0, ALU.mult, ALU.add), r=[k(c['rmask']), k(s['ra1'])], w=[k(s['rcw'])])
        yield
        V(lambda e: e.tensor_tensor(s['ra2'][:], s['rcw'][:], s['ra1'][:], ALU.subtract), r=[k(s['rcw']), k(s['ra1'])], w=[k(s['ra2'])])
        yield
        A(lambda e: e.activation(out=s['recw'][:], in_=s['rcw'][:], func=AF.Exp), r=[k(s['rcw'])], w=[k(s['recw'])])
        yield
        A(lambda e: e.activation(out=s['reicw'][:], in_=s['rcw'][:], func=AF.Exp, scale=-1.0), r=[k(s['rcw'])], w=[k(s['reicw'])])
        yield
        A(lambda e: e.activation(out=s['recwp'][:], in_=s['ra2'][:], func=AF.Exp), r=[k(s['ra2'])], w=[k(s['recwp'])])
        yield
        ba_, ka_ = self.bank('r')
        for b in range(2):
            M(lambda e, b=b: e.matmul(ba_[:, b * 128:(b + 1) * 128], lhsT=p['a2p'][:, b * 128:(b + 1) * 128], rhs=lr, start=True, stop=True),
              r=[k(p['a2p']), ksh], w=[ka_])
        for b in range(2):
            A(lambda e, b=b: e.activation(out=s['ra'][:, b * 128:(b + 1) * 128], in_=ba_[:, b * 128:(b + 1) * 128], func=AF.Sigmoid, bias=p['a0'][:, b:b + 1]),
              r=[ka_, k(p['a0'])], w=[k(s['ra'])])
            yield
        A(lambda e: e.activation(out=s['rsg'][:], in_=lr, func=AF.Sigmoid), r=[ksh], w=[k(s['rsg'])])
        yield
        bg, kg = self.bank('r')
        M(lambda e: e.matmul(bg[:, 0:256], lhsT=s['rsg'][:], rhs=p['g2p'][:], start=True, stop=True), r=[k(s['rsg']), k(p['g2p'])], w=[kg])
        A(lambda e: e.copy(out=s['rg'][:], in_=bg[:, 0:256]), r=[kg], w=[k(s['rg'])])
        yield
        V(lambda e: e.tensor_tensor(s['rkk'][:].rearrange("p (a b) -> p a b", b=128), kT.rearrange("p (a b) -> p a b", b=128), self.bc(p['kk'][:, :], [128, 2, 128], 2), ALU.mult),
          r=[ksh, k(p['kk'])], w=[k(s['rkk'])])
        yield
        A(lambda e: e.activation(out=s['ra2'][:], in_=s['rkk'][:], func=AF.Square), r=[k(s['rkk'])], w=[k(s['ra2'])])
        yield
        bn, kn = self.bank('r')
        for b in range(2):
            M(lambda e, b=b: e.matmul(bn[:, b * 128:(b + 1) * 128], lhsT=c['bones'][:], rhs=s['ra2'][:, b * 128:(b + 1) * 128], start=True, stop=True),
              r=[k(c['bones']), k(s['ra2'])], w=[kn])
        V(lambda e: e.tensor_scalar(s['ra3'][:], bn[:, 0:256], 1e-12, None, ALU.add), r=[kn], w=[k(s['ra3'])])
        yield
        A(lambda e: e.activation(out=s['ra3'][:], in_=s['ra3'][:], func=AF.Sqrt), r=[k(s['ra3'])], w=[k(s['ra3'])])
        yield
        V(lambda e: e.reciprocal(s['ra3'][:], s['ra3'][:]), r=[k(s['ra3'])], w=[k(s['ra3'])])
        yield
        V(lambda e: e.tensor_tensor(s['rkk'][:], s['rkk'][:], s['ra3'][:], ALU.mult), r=[k(s['rkk']), k(s['ra3'])], w=[k(s['rkk'])])
        yield
        for b in range(2):
            V(lambda e, b=b: e.tensor_scalar(s['ra2'][:, b * 128:(b + 1) * 128], s['ra'][:, b * 128:(b + 1) * 128], p['ka'][:, b:b + 1], p['omka'][:, b:b + 1], ALU.mult, ALU.add),
              r=[k(s['ra']), k(p['ka']), k(p['omka'])], w=[k(s['ra2'])])
            yield
        V(lambda e: e.tensor_tensor(s['rkp'][:], kT, s['ra2'][:], ALU.mult), r=[ksh, k(s['ra2'])], w=[k(s['rkp'])])
        yield
        V(lambda e: e.tensor_tensor(s['ra3'][:], s['rkk'][:], s['ra'][:], ALU.mult), r=[k(s['rkk']), k(s['ra'])], w=[k(s['ra3'])])
        yield
        for hh in range(2):
            V(lambda e, hh=hh: e.scalar_tensor_tensor(s['rAm'][:, hh, :], s['rkk'][:], c['nhm'][:, hh:hh + 1], s['recwp'][:], ALU.mult, ALU.mult),
              r=[k(s['rkk']), k(c['nhm']), k(s['recwp'])], w=[k(s['rAm'])])
            yield
            V(lambda e, hh=hh: e.scalar_tensor_tensor(s['rRm'][:, hh, :], rT, c['hm'][:, hh:hh + 1], s['recw'][:], ALU.mult, ALU.mult),
              r=[ksh, k(c['hm']), k(s['recw'])], w=[k(s['rRm'])])
            yield
        G(lambda e: e.tensor_tensor(s['rBt'][:], s['ra3'][:], s['reicw'][:], ALU.mult), r=[k(s['ra3']), k(s['reicw'])], w=[k(s['rBt'])])
        yield
        G(lambda e: e.tensor_tensor(s['rKt'][:], s['rkp'][:], s['reicw'][:], ALU.mult), r=[k(s['rkp']), k(s['reicw'])], w=[k(s['rKt'])])
        yield
        V(lambda e: e.tensor_tensor(s['ra2'][:], rT, s['rkp'][:], ALU.mult), r=[ksh, k(s['rkp'])], w=[k(s['ra2'])])
        yield
        V(lambda e: e.tensor_tensor(s['ra2'][:].rearrange("p (a b) -> p a b", b=128), s['ra2'][:].rearrange("p (a b) -> p a b", b=128), self.bc(p['rk'][:, :], [128, 2, 128], 2), ALU.mult),
          r=[k(s['ra2']), k(p['rk'])], w=[k(s['ra2'])])
        yield
        bb, kb = self.bank('r')
        for b in range(2):
            M(lambda e, b=b: e.matmul(bb[:, b * 2:(b + 1) * 2], lhsT=s['ra2'][:, b * 128:(b + 1) * 128], rhs=c['hm'][:, :], start=True, stop=True),
              r=[k(s['ra2']), k(c['hm'])], w=[kb])
        A(lambda e: e.copy(out=s['rbc'][:], in_=bb[:, 0:4]), r=[kb], w=[k(s['rbc'])])
        yield
        bt, kt = self.bank('r')
        for b in range(2):
            M(lambda e, b=b: e.transpose(bt[:, b * 128:(b + 1) * 128], s['rsh'][:, 4 + b, :], c['identf'][:]), r=[ksh, k(c['identf'])], w=[kt])
        A(lambda e: e.copy(out=s['rvtm'][:], in_=bt[:, 0:256]), r=[kt], w=[k(s['rvtm'])])
        yield
        bt, kt = self.bank('r')
        for cc in range(2):
            for b in range(2):
                M(lambda e, bt=bt, cc=cc, b=b: e.transpose(bt[0:64, cc * 256 + b * 128:cc * 256 + (b + 1) * 128], s['rsh'][:, 4 + b, cc * 64:(cc + 1) * 64], c['identf'][:]),
                  r=[ksh, k(c['identf'])], w=[kt])
        A(lambda e, bt=bt: e.copy(out=s['rvc'][:].rearrange("p a b -> p (a b)"), in_=bt[0:64, :]), r=[kt], w=[k(s['rvc'])])
        yield
        for srct, dst in ((s['rBt'], s['rBc']), (s['rKt'], s['rKc'])):
            bt, kt = self.bank('r')
            pbt = bt[:].bitcast(BF16)
            for cc in range(2):
                for b in range(2):
                    M(lambda e, pbt=pbt, srct=srct, cc=cc, b=b: e.transpose(pbt[0:64, cc * 256 + b * 128:cc * 256 + (b + 1) * 128], srct[:, b * 128 + cc * 64:b * 128 + (cc + 1) * 64], c['identb'][:]),
                      r=[k(srct), k(c['identb'])], w=[kt])
            V(lambda e, pbt=pbt, dst=dst: e.tensor_copy(dst[:].rearrange("p a b -> p (a b)"), pbt[0:64, 0:512]), r=[kt], w=[k(dst)])
            yield
        def amat(lt, lkey, lhh, rt_, rkey, rhh, mask, dst):
            bA, kA = self.bank('r')
            for cc in range(2):
                for h in range(4):
                    b, hh = h // 2, h % 2
                    sl_ = slice(b * 128 + cc * 64, b * 128 + (cc + 1) * 64)
                    la = lt[:, hh, sl_] if lhh else lt[:, sl_]
                    ra_ = rt_[:, hh, sl_] if rhh else rt_[:, sl_]
                    i8 = cc * 4 + h
                    M(lambda e, bA=bA, la=la, ra_=ra_, i8=i8: e.matmul(bA[0:64, i8 * 64:(i8 + 1) * 64], lhsT=la, rhs=ra_, start=True, stop=True), r=[lkey, rkey], w=[kA])
            V(lambda e, bA=bA: e.tensor_tensor(dst[:], h3(bA[0:64, :]), self.bc(mask, [64, 8, 64], 1), ALU.mult), r=[kA, k(c['su'])], w=[k(dst)])
            yield
        kAm, kRm, kBt, kKt = k(s['rAm']), k(s['rRm']), k(s['rBt']), k(s['rKt'])
        yield from amat(s['rAm'], kAm, True, s['rBt'], kBt, False, c['su'][0:64, 0:64], s['rP'])
        yield from amat(s['rBt'], kBt, False, s['rAm'], kAm, True, c['sl'][0:64, 0:64], s['rQ'])
        yield from amat(s['rKt'], kKt, False, s['rAm'], kAm, True, c['sl'][0:64, 0:64], s['rAak'])
        yield from amat(s['rBt'], kBt, False, s['rRm'], kRm, True, c['tri'][0:64, 0:64], s['rArb'])
        yield from amat(s['rKt'], kKt, False, s['rRm'], kRm, True, c['tri'][0:64, 0:64], s['rArk'])
        V(lambda e: e.tensor_tensor(s['rTT'][:], s['rQ'][:], self.bc(c['identf'][0:64, 0:64], [64, 8, 64], 1), ALU.add), r=[k(s['rQ']), k(c['identf'])], w=[k(s['rTT'])])
        yield
        Pc, Qc, Pn, Qn = s['rP'], s['rQ'], s['rP2'], s['rQ2']
        for lvl in range(5):
            bP, kP = self.bank('r')
            for i8 in range(8):
                M(lambda e, bP=bP, i8=i8, Pc=Pc, Qc=Qc: e.matmul(bP[0:64, i8 * 64:(i8 + 1) * 64], lhsT=Qc[:, i8, :], rhs=Pc[:, i8, :], start=True, stop=True),
                  r=[k(Pc), k(Qc)], w=[kP])
            A(lambda e, bP=bP, Pn=Pn: e.copy(out=Pn[:], in_=h3(bP[0:64, :])), r=[kP], w=[k(Pn)])
            yield
            if lvl < 4:
                bQ, kQ = self.bank('r')
                for i8 in range(8):
                    M(lambda e, bQ=bQ, i8=i8, Pc=Pc, Qc=Qc: e.matmul(bQ[0:64, i8 * 64:(i8 + 1) * 64], lhsT=Pc[:, i8, :], rhs=Qc[:, i8, :], start=True, stop=True),
                      r=[k(Pc), k(Qc)], w=[kQ])
                V(lambda e, bQ=bQ, Qn=Qn: e.tensor_copy(Qn[:], h3(bQ[0:64, :])), r=[kQ], w=[k(Qn)])
                yield
            bT, kT_ = self.bank('r')
            for i8 in range(8):
                M(lambda e, bT=bT, i8=i8, Pn=Pn: e.matmul(bT[0:64, i8 * 64:(i8 + 1) * 64], lhsT=Pn[:, i8, :], rhs=s['rTT'][:, i8, :], start=True, stop=True),
                  r=[k(Pn), k(s['rTT'])], w=[kT_])
            V(lambda e, bT=bT: e.tensor_tensor(s['rTT'][:], s['rTT'][:], h3(bT[0:64, :]), ALU.add), r=[k(s['rTT']), kT_], w=[k(s['rTT'])])
            yield
            Pc, Qc, Pn, Qn = Pn, Qn, Pc, Qc
        bG, kG = self.bank('r')
        for cc in range(2):
            for h in range(4):
                i8 = cc * 4 + h
                M(lambda e, cc=cc, h=h, i8=i8: e.matmul(bG[0:64, i8 * 64:(i8 + 1) * 64], lhsT=s['rAak'][:, i8, :], rhs=s['rvc'][:, cc, h * 64:(h + 1) * 64], start=True, stop=True),
                  r=[k(s['rAak']), k(s['rvc'])], w=[kG])
        A(lambda e: e.copy(out=s['rAak'][:], in_=h3(bG[0:64, :])), r=[kG], w=[k(s['rAak'])])
        yield
        ewc = s['recw'][:].rearrange("p (a b) -> p a b", b=64)[:, :, 63]
        for cc in range(2):
            bG1, kG1 = self.bank('r')
            for h in range(4):
                b, hh = h // 2, h % 2
                sl_ = slice(b * 128 + cc * 64, b * 128 + (cc + 1) * 64)
                M(lambda e, bG1=bG1, h=h, b=b, hh=hh, sl_=sl_: e.matmul(bG1[0:64, h * 64:(h + 1) * 64], lhsT=s['rAm'][:, hh, sl_], rhs=s['rSTb'][:, b, :], start=True, stop=True),
                  r=[kAm, k(s['rSTb'])], w=[kG1])
            bY1, kY1 = self.bank('r')
            for h in range(4):
                b, hh = h // 2, h % 2
                sl_ = slice(b * 128 + cc * 64, b * 128 + (cc + 1) * 64)
                M(lambda e, bY1=bY1, h=h, b=b, hh=hh, sl_=sl_, cc=cc: e.matmul(bY1[cc * 64:(cc + 1) * 64, h * 64:(h + 1) * 64], lhsT=s['rRm'][:, hh, sl_], rhs=s['rSTb'][:, b, :], start=True, stop=True),
                  r=[kRm, k(s['rSTb'])], w=[kY1])
            A(lambda e, bY1=bY1, cc=cc: e.copy(out=s['rY1'][cc * 64:(cc + 1) * 64, :], in_=bY1[cc * 64:(cc + 1) * 64, 0:256]), r=[kY1], w=[k(s['rY1'])])
            yield
            V(lambda e, bG1=bG1, cc=cc: e.tensor_tensor(s['rG'][:], s['rAak'][:, cc * 4:(cc + 1) * 4, :], h3(bG1[0:64, 0:256]), ALU.add), r=[k(s['rAak']), kG1], w=[k(s['rG'])])
            yield
            bU, kU = self.bank('r')
            for h in range(4):
                i8 = cc * 4 + h
                M(lambda e, bU=bU, h=h, i8=i8: e.matmul(bU[0:64, h * 64:(h + 1) * 64], lhsT=s['rTT'][:, i8, :], rhs=s['rG'][:, h, :], start=True, stop=True),
                  r=[k(s['rTT']), k(s['rG'])], w=[kU])
            A(lambda e, bU=bU: e.copy(out=s['rU'][:], in_=h3(bU[0:64, 0:256])), r=[kU], w=[k(s['rU'])])
            yield
            bY2, kY2 = self.bank('r')
            for h in range(4):
                i8 = cc * 4 + h
                M(lambda e, bY2=bY2, h=h, i8=i8, cc=cc: e.matmul(bY2[cc * 64:(cc + 1) * 64, h * 64:(h + 1) * 64], lhsT=s['rArb'][:, i8, :], rhs=s['rU'][:, h, :], start=True, stop=False),
                  r=[k(s['rArb']), k(s['rU'])], w=[kY2])
                M(lambda e, bY2=bY2, h=h, i8=i8, cc=cc: e.matmul(bY2[cc * 64:(cc + 1) * 64, h * 64:(h + 1) * 64], lhsT=s['rArk'][:, i8, :], rhs=s['rvc'][:, cc, h * 64:(h + 1) * 64], start=False, stop=True),
                  r=[k(s['rArk']), k(s['rvc'])], w=[kY2])
            V(lambda e, bY2=bY2, cc=cc: e.tensor_tensor(s['rY'][cc * 64:(cc + 1) * 64, :], s['rY1'][cc * 64:(cc + 1) * 64, :], bY2[cc * 64:(cc + 1) * 64, 0:256], ALU.add),
              r=[k(s['rY1']), kY2], w=[k(s['rY'])])
            yield
            bS, kS = self.bank('r')
            for h in range(4):
                b, hh = h // 2, h % 2
                i8 = cc * 4 + h
                M(lambda e, bS=bS, h=h, b=b, hh=hh, cc=cc: e.matmul(bS[hh * 64:(hh + 1) * 64, b * 64:(b + 1) * 64], lhsT=s['rBc'][:, cc, h * 64:(h + 1) * 64], rhs=s['rU'][:, h, :], start=True, stop=False),
                  r=[k(s['rBc']), k(s['rU'])], w=[kS])
                M(lambda e, bS=bS, h=h, b=b, hh=hh, cc=cc: e.matmul(bS[hh * 64:(hh + 1) * 64, b * 64:(b + 1) * 64], lhsT=s['rKc'][:, cc, h * 64:(h + 1) * 64], rhs=s['rvc'][:, cc, h * 64:(h + 1) * 64], start=False, stop=True),
                  r=[k(s['rKc']), k(s['rvc'])], w=[kS])
            V(lambda e, bS=bS: e.tensor_tensor(s['rt2'][:], s['rST'][:], h3(bS[:, 0:128]), ALU.add), r=[k(s['rST']), kS], w=[k(s['rt2'])])
            yield
            V(lambda e, cc=cc: e.tensor_tensor(s['rST'][:], s['rt2'][:], self.bc(ewc[:, cc::2], [128, 2, 64], 2), ALU.mult), r=[k(s['rt2']), k(s['recw'])], w=[k(s['rST'])])
            yield
            A(lambda e: e.copy(out=s['rSTb'][:], in_=s['rST'][:]), r=[k(s['rST'])], w=[k(s['rSTb'])])
            yield
        V(lambda e: e.tensor_reduce(s['rm1'][:], h3(s['rY'][:]), AX.X, ALU.add), r=[k(s['rY'])], w=[k(s['rm1'])])
        yield
        A(lambda e: e.activation(out=s['rY1'][:], in_=s['rY'][:], func=AF.Square), r=[k(s['rY'])], w=[k(s['rY1'])])
        yield
        V(lambda e: e.tensor_reduce(s['rm2'][:], h3(s['rY1'][:]), AX.X, ALU.add), r=[k(s['rY1'])], w=[k(s['rm2'])])
        yield
        V(lambda e: e.tensor_scalar(s['rm1'][:], s['rm1'][:], 1.0 / 64, None, ALU.mult), r=[k(s['rm1'])], w=[k(s['rm1'])])
        yield
        V(lambda e: e.tensor_tensor(s['rvar'][:], s['rm1'][:], s['rm1'][:], ALU.mult), r=[k(s['rm1'])], w=[k(s['rvar'])])
        yield
        V(lambda e: e.scalar_tensor_tensor(s['rvar'][:], s['rm2'][:], 1.0 / 64, s['rvar'][:], ALU.mult, ALU.subtract), r=[k(s['rm2']), k(s['rvar'])], w=[k(s['rvar'])])
        yield
        V(lambda e: e.tensor_scalar(s['rvar'][:], s['rvar'][:], GN_EPS, None, ALU.add), r=[k(s['rvar'])], w=[k(s['rvar'])])
        yield
        A(lambda e: e.activation(out=s['rvar'][:], in_=s['rvar'][:], func=AF.Sqrt), r=[k(s['rvar'])], w=[k(s['rvar'])])
        yield
        V(lambda e: e.reciprocal(s['rvar'][:], s['rvar'][:]), r=[k(s['rvar'])], w=[k(s['rvar'])])
        yield
        V(lambda e: e.tensor_tensor(h3(s['rY'][:]), h3(s['rY'][:]), self.bc(s['rm1'][:, :], [128, 4, 64], 2), ALU.subtract), r=[k(s['rY']), k(s['rm1'])], w=[k(s['rY'])])
        yield
        V(lambda e: e.tensor_tensor(h3(s['rY'][:]), h3(s['rY'][:]), self.bc(s['rvar'][:, :], [128, 4, 64], 2), ALU.mult), r=[k(s['rY']), k(s['rvar'])], w=[k(s['rY'])])
        yield
        G(lambda e: e.tensor_tensor(s['rY'][:], s['rY'][:], p['rlg'][:], ALU.mult), r=[k(s['rY']), k(p['rlg'])], w=[k(s['rY'])])
        yield
        G(lambda e: e.tensor_tensor(s['rY'][:], s['rY'][:], p['rlb'][:], ALU.add), r=[k(s['rY']), k(p['rlb'])], w=[k(s['rY'])])
        yield
        V(lambda e: e.tensor_tensor(h3(s['rY1'][:]), h3(s['rvtm'][:]), self.bc(s['rbc'][:, :], [128, 4, 64], 2), ALU.mult), r=[k(s['rvtm']), k(s['rbc'])], w=[k(s['rY1'])])
        yield
        V(lambda e: e.tensor_tensor(s['rY'][:], s['rY'][:], s['rY1'][:], ALU.add), r=[k(s['rY']), k(s['rY1'])], w=[k(s['rY'])])
        yield
        V(lambda e: e.tensor_tensor(s['ycat'][:, 512:768], s['rY'][:], s['rg'][:], ALU.mult), r=[k(s['rY']), k(s['rg'])], w=[k(s['ycat'])])
        yield

    def mixer_epilogue(self, l, i):
        s, p, c = self.s, self.p, self.c
        V, A, G, M = self.V, self.A, self.G, self.M
        k = lambda t: t.key
        self.load('sp', s['htm'][:], self.h_d[i * 128:(i + 1) * 128, :], k(s['htm']), dkeys=["hd_%d" % i])
        if self.debug:
            self.A(lambda e: e.copy(out=s['tmp'][:], in_=s['ycat'][:]), r=[k(s['ycat'])], w=[k(s['tmp'])])
            yield
            self.store('sp', self.dbg_y[i * 128:(i + 1) * 128, :], s['tmp'][:], k(s['tmp']))
            yield
        bt, kt = self.bank('e')
        pb = bt[:].bitcast(BF16)
        for kc in range(8):
            M(lambda e, kc=kc: e.transpose(pb[:, kc * 128:(kc + 1) * 128], s['ycat'][:, kc * 128:(kc + 1) * 128], c['identb'][:]), r=[k(s['ycat']), k(c['identb'])], w=[kt])
        V(lambda e: e.tensor_copy(s['yT'][:].rearrange("p a b -> p (a b)"), pb), r=[kt], w=[k(s['yT'])])
        yield
        for half in range(2):
            bo, ko = self.bank('e')
            for kc in range(8):
                M(lambda e, bo=bo, kc=kc, half=half: e.matmul(bo[:, :], lhsT=s['yT'][:, kc, :], rhs=p['w_out'][:, kc, half * 512:(half + 1) * 512], start=(kc == 0), stop=(kc == 7)),
                  r=[k(s['yT']), k(p['w_out'])], w=[ko])
            V(lambda e, bo=bo, half=half: e.scalar_tensor_tensor(s['mix'][:, half * 512:(half + 1) * 512], s['htm'][:, half * 512:(half + 1) * 512], ALPHA, bo[:, :], ALU.mult, ALU.add),
              r=[k(s['htm']), ko], w=[k(s['mix'])])
            yield
        yield from self.layernorm(s['mix'], p['l1g'], p['l1b'], s['h1'], s['tmp'])
        tk = "h1d_%d_%d" % (l, i)
        self.store('sp', self.h1_d[i * 128:(i + 1) * 128, :], s['h1'][:], k(s['h1']), dkeys=[tk])
        yield
        self.store('pool', self.h1b_d[i * 128:(i + 1) * 128, :], s['h1'][:], k(s['h1']), dkeys=["h1bd_%d_%d" % (l, i)])
        yield
        if hasattr(self, 'xs_d'):
            yield from self.router_tile(l, i)

    def stage0(self):
        s, p, d = self.s, self.p, self.d
        k = lambda t: t.key
        mark = self.sb_off
        self.load('sp', p['l1g'][:], d['ln_in_g'].ap().partition_broadcast(128), k(p['l1g']))
        self.load('sp', p['l1b'][:], d['ln_in_b'].ap().partition_broadcast(128), k(p['l1b']))
        sets = [dict(x=s['mix'], h=s['htm'], tmp=s['tmp'], hb=s['ycat'], hT=s['hT'], st=self.ln_stats("a"))]
        hb2 = Tile(s['yT'].t.ap().rearrange("p a b -> p (a b)") if False else s['yT'].t, s['yT'].key)
        sets.append(dict(x=s['h1'], h=self.sb([128, D], name="s0_h"), tmp=self.sb([128, D], name="s0_tmp"),
                         hb=hb2, hT=self.sb([128, 8, 128], BF16, "s0_hT"), st=self.ln_stats("b")))

        def body(i):
            B = sets[i % 2]
            self.load('sp', B['x'][:], d['x'][i * 128:(i + 1) * 128, :], k(B['x']))
            yield
            yield from self.layernorm(B['x'], p['l1g'], p['l1b'], B['h'], B['tmp'], B['st'])
            self.store('sp', self.h_d[i * 128:(i + 1) * 128, :], B['h'][:], k(B['h']), dkeys=["hd_%d" % i])
            yield
            hbf = B['hb'][:] if len(B['hb'][:].shape) == 2 else B['hb'][:].rearrange("p a b -> p (a b)")
            self.A(lambda e: e.copy(out=hbf, in_=B['h'][:]), r=[k(B['h'])], w=[k(B['hb'])])
            yield
            bk, bkey = self.bank()
            pb = bk[:].bitcast(BF16)
            for kc in range(8):
                self.M(lambda e, kc=kc: e.transpose(pb[:, kc * 128:(kc + 1) * 128], hbf[:, kc * 128:(kc + 1) * 128], self.c['identb'][:]),
                       r=[k(B['hb']), k(self.c['identb'])], w=[bkey])
            self.V(lambda e: e.tensor_copy(B['hT'][:].rearrange("p a b -> p (a b)"), pb), r=[bkey], w=[k(B['hT'])])
            yield
            self.store('sp', self.hT_d[:, :, i * 128:(i + 1) * 128], B['hT'][:], k(B['hT']), dkeys=["hTd_%d" % i])
            yield
        self.run_pipe([lambda i=i: body(i) for i in range(self.NT)], 2)
        self.sb_off = mark

    def stageM(self, l):
        import os
        s = self.s
        k = lambda t: t.key
        only = os.environ.get("ONLY", "srg")
        prev = None
        for i in range(self.NT + 1):
            gens = []
            if i < self.NT:
                self.load('sp', s['hT'][:], self.hT_d[:, :, i * 128:(i + 1) * 128], k(s['hT']), dkeys=["hTd_%d" % i])
                if 'r' in only:
                    gens.append(self.rwkv_tile(i == 0))
                if 's' in only:
                    gens.append(self.ssd_tile(i == 0))
                if 'g' in only:
                    gens.append(self.gla_tile(i == 0))
            if prev is not None:
                gens.append(self.mixer_epilogue(l, prev))
            prev = i if i < self.NT else None
            wts = [int(x) for x in os.environ.get("ILW", "3,1,1,1").split(",")]
            gw = {id(g_): (wts[0] if j == 0 and i < self.NT and 'r' in only else 1) for j, g_ in enumerate(gens)}
            while gens:
                for g_ in list(gens):
                    for _ in range(gw[id(g_)]):
                        try:
                            next(g_)
                        except StopIteration:
                            gens.remove(g_)
                            break

    def build_mixer_test(self):
        self.declare_inputs()
        T = self.T
        self.h_d = self.dscr("h_d", [T, D])
        self.hT_d = self.dscr("hT_d", [128, 8, T], BF16)
        self.h1_d = self.dout("h1_d", [T, D])
        self.h1b_d = self.dscr("h1b_d", [T, D], BF16)
        self.dbg_y = self.dout("dbg_y", [T, D])
        self.consts()
        self.alloc_params()
        self.alloc_mixer()
        print("sbuf peak", self.sb_peak)
        self.stage0()
        self.P.barrier()
        self.load_params(0)
        self.stageM(0)
        self.P.barrier()
        return self.nc

    def alloc_router(self):
        rt = self.rt = {}
        for n, w in (('lg', 36), ('gmx', 1), ('goh', 4), ('gex', 4), ('gsum', 1), ('t32', 32), ('el8', 8), ('el8m', 8), ('l1', 1), ('l2', 1),
                     ('oh1', 8), ('oh2', 8), ('w1', 1), ('w2', 1), ('E1', 32), ('E2', 32), ('Mm', 32), ('rk', 32)):
            rt[n] = self.sb([128, w], name="rt_" + n)

    def alloc_route_persist(self):
        rp = self.rp = {}
        NT = self.NT
        rp['eid'] = self.sb([128, NT * 2], name="rp_eid")
        rp['rnk'] = self.sb([128, NT * 2], name="rp_rnk")
        rp['gat'] = self.sb([128, NT * 2], name="rp_gat")
        rp['cnt'] = self.sb([128, NE], name="rp_cnt")
        rp['iota32'] = self.sb([128, NE], name="rp_iota32")
        ii = self.sb([128, NE], I32, "rp_iota32i")
        self.G(lambda e: e.iota(ii[:], pattern=[[1, NE]], base=0, channel_multiplier=0), w=[ii.key])
        self.V(lambda e: e.tensor_copy(rp['iota32'][:], ii[:]), r=[ii.key], w=[rp['iota32'].key])

    def router_tile(self, l, i):
        s, p, c = self.s, self.p, self.c
        V, A, G, M = self.V, self.A, self.G, self.M
        k = lambda t: t.key
        rt, rp = self.rt, self.rp
        if i == 0:
            G(lambda e: e.memset(rp['cnt'][:], 0.0), w=[k(rp['cnt'])])
            yield
        hT32 = s['tmp'][:].rearrange("p (a b) -> p a b", b=128)
        for half in range(2):
            bt, kt = self.bank('e')
            for j in range(4):
                kc = half * 4 + j
                M(lambda e, bt=bt, j=j, kc=kc: e.transpose(bt[:, j * 128:(j + 1) * 128], s['h1'][:, kc * 128:(kc + 1) * 128], c['identf'][:]), r=[k(s['h1']), k(c['identf'])], w=[kt])
            A(lambda e, bt=bt, half=half: e.copy(out=s['tmp'][:, half * 512:(half + 1) * 512], in_=bt[:, :]), r=[kt], w=[k(s['tmp'])])
            yield
        bl, kl = self.bank('e')
        for kc in range(8):
            M(lambda e, kc=kc: e.matmul(bl[:, 0:36], lhsT=hT32[:, kc, :], rhs=p['wr'][:, kc, :], start=(kc == 0), stop=(kc == 7)), r=[k(s['tmp']), k(p['wr'])], w=[kl])
        V(lambda e: e.tensor_tensor(rt['lg'][:], bl[:, 0:36], p['rb36'][:], ALU.add), r=[kl, k(p['rb36'])], w=[k(rt['lg'])])
        yield
        V(lambda e: e.tensor_reduce(rt['gmx'][:], rt['lg'][:, 0:4], AX.X, ALU.max), r=[k(rt['lg'])], w=[k(rt['gmx'])])
        yield
        V(lambda e: e.tensor_scalar(rt['goh'][:], rt['lg'][:, 0:4], rt['gmx'][:, 0:1], None, ALU.is_equal), r=[k(rt['lg']), k(rt['gmx'])], w=[k(rt['goh'])])
        yield
        V(lambda e: e.tensor_scalar(rt['gex'][:], rt['lg'][:, 0:4], rt['gmx'][:, 0:1], None, ALU.subtract), r=[k(rt['lg']), k(rt['gmx'])], w=[k(rt['gex'])])
        yield
        A(lambda e: e.activation(out=rt['gex'][:], in_=rt['gex'][:], func=AF.Exp), r=[k(rt['gex'])], w=[k(rt['gex'])])
        yield
        V(lambda e: e.tensor_reduce(rt['gsum'][:], rt['gex'][:], AX.X, ALU.add), r=[k(rt['gex'])], w=[k(rt['gsum'])])
        yield
        V(lambda e: e.reciprocal(rt['gsum'][:], rt['gsum'][:]), r=[k(rt['gsum'])], w=[k(rt['gsum'])])
        yield
        V(lambda e: e.tensor_tensor(rt['t32'][:].rearrange("p (g j) -> p g j", j=8), rt['lg'][:, 4:36].rearrange("p (g j) -> p g j", j=8),
                                    self.bc(rt['goh'][:, :], [128, 4, 8], 2), ALU.mult), r=[k(rt['lg']), k(rt['goh'])], w=[k(rt['t32'])])
        yield
        V(lambda e: e.tensor_reduce(rt['el8'][:], rt['t32'][:].rearrange("p (g j) -> p j g", j=8), AX.X, ALU.add), r=[k(rt['t32'])], w=[k(rt['el8'])])
        yield
        V(lambda e: e.tensor_reduce(rt['l1'][:], rt['el8'][:], AX.X, ALU.max), r=[k(rt['el8'])], w=[k(rt['l1'])])
        yield
        V(lambda e: e.tensor_scalar(rt['oh1'][:], rt['el8'][:], rt['l1'][:, 0:1], None, ALU.is_equal), r=[k(rt['el8']), k(rt['l1'])], w=[k(rt['oh1'])])
        yield
        V(lambda e: e.scalar_tensor_tensor(rt['el8m'][:], rt['oh1'][:], -1e30, rt['el8'][:], ALU.mult, ALU.add), r=[k(rt['oh1']), k(rt['el8'])], w=[k(rt['el8m'])])
        yield
        V(lambda e: e.tensor_reduce(rt['l2'][:], rt['el8m'][:], AX.X, ALU.max), r=[k(rt['el8m'])], w=[k(rt['l2'])])
        yield
        V(lambda e: e.tensor_scalar(rt['oh2'][:], rt['el8m'][:], rt['l2'][:, 0:1], None, ALU.is_equal), r=[k(rt['el8m']), k(rt['l2'])], w=[k(rt['oh2'])])
        yield
        V(lambda e: e.tensor_tensor(rt['w2'][:], rt['l2'][:], rt['l1'][:], ALU.subtract), r=[k(rt['l2']), k(rt['l1'])], w=[k(rt['w2'])])
        yield
        A(lambda e: e.activation(out=rt['w2'][:], in_=rt['w2'][:], func=AF.Exp), r=[k(rt['w2'])], w=[k(rt['w2'])])
        yield
        V(lambda e: e.tensor_scalar(rt['w1'][:], rt['w2'][:], 1.0, None, ALU.add), r=[k(rt['w2'])], w=[k(rt['w1'])])
        yield
        V(lambda e: e.reciprocal(rt['w1'][:], rt['w1'][:]), r=[k(rt['w1'])], w=[k(rt['w1'])])
        yield
        V(lambda e: e.tensor_tensor(rt['w2'][:], rt['w2'][:], rt['w1'][:], ALU.mult), r=[k(rt['w2']), k(rt['w1'])], w=[k(rt['w2'])])
        yield
        V(lambda e: e.tensor_tensor(rp['gat'][:, 2 * i:2 * i + 1], rt['w1'][:], rt['gsum'][:], ALU.mult), r=[k(rt['w1']), k(rt['gsum'])], w=[k(rp['gat'])])
        yield
        V(lambda e: e.tensor_tensor(rp['gat'][:, 2 * i + 1:2 * i + 2], rt['w2'][:], rt['gsum'][:], ALU.mult), r=[k(rt['w2']), k(rt['gsum'])], w=[k(rp['gat'])])
        yield
        for E, oh in ((rt['E1'], rt['oh1']), (rt['E2'], rt['oh2'])):
            V(lambda e, E=E, oh=oh: e.tensor_tensor(E[:].rearrange("p (g j) -> p g j", j=8), self.bc(rt['goh'][:, :], [128, 4, 8], 2), self.bc(oh[:, :], [128, 4, 8], 1), ALU.mult),
              r=[k(rt['goh']), k(oh)], w=[k(E)])
            yield
        V(lambda e: e.tensor_tensor(rt['Mm'][:], rt['E1'][:], rt['E2'][:], ALU.add), r=[k(rt['E1']), k(rt['E2'])], w=[k(rt['Mm'])])
        yield
        br, kr = self.bank('e')
        M(lambda e: e.matmul(br[:, 0:32], lhsT=c['sl'][:], rhs=rt['Mm'][:], start=True, stop=True), r=[k(c['sl']), k(rt['Mm'])], w=[kr])
        M(lambda e: e.matmul(br[:, 32:64], lhsT=c['onesf'][:], rhs=rt['Mm'][:], start=True, stop=True), r=[k(c['onesf']), k(rt['Mm'])], w=[kr])
        V(lambda e: e.tensor_tensor(rt['rk'][:], br[:, 0:32], rp['cnt'][:], ALU.add), r=[kr, k(rp['cnt'])], w=[k(rt['rk'])])
        yield
        V(lambda e: e.tensor_tensor(rp['cnt'][:], rp['cnt'][:], br[:, 32:64], ALU.add), r=[kr, k(rp['cnt'])], w=[k(rp['cnt'])])
        yield
        for j, E in ((0, rt['E1']), (1, rt['E2'])):
            V(lambda e, E=E: e.tensor_tensor(rt['t32'][:], E[:], rt['rk'][:], ALU.mult), r=[k(E), k(rt['rk'])], w=[k(rt['t32'])])
            yield
            V(lambda e, j=j: e.tensor_reduce(rp['rnk'][:, 2 * i + j:2 * i + j + 1], rt['t32'][:], AX.X, ALU.add), r=[k(rt['t32'])], w=[k(rp['rnk'])])
            yield
            V(lambda e, E=E: e.tensor_tensor(rt['t32'][:], E[:], rp['iota32'][:], ALU.mult), r=[k(E), k(rp['iota32'])], w=[k(rt['t32'])])
            yield
            V(lambda e, j=j: e.tensor_reduce(rp['eid'][:, 2 * i + j:2 * i + j + 1], rt['t32'][:], AX.X, ALU.add), r=[k(rt['t32'])], w=[k(rp['eid'])])
            yield

    def stageMoE(self, l, last):
        d, c, rp = self.d, self.c, self.rp
        V, A, G, M = self.V, self.A, self.G, self.M
        k = lambda t: t.key
        mark = self.sb_off
        sb = self.sb
        NT, NB, RB = self.NT, self.NB, self.RB
        NR = RB // 128
        NC = NT * 2
        thr_i = sb([128, 64], I32, "f_thri")
        thr = sb([128, 64], name="f_thr")
        G(lambda e: e.iota(thr_i[:], pattern=[[RB, 64]], base=0, channel_multiplier=0), w=[k(thr_i)])
        V(lambda e: e.tensor_copy(thr[:], thr_i[:]), r=[k(thr_i)], w=[k(thr)])
        big = sb([128, max(NC * NE, NE * 64, NB * NE)], name="f_big")
        nblk = sb([128, NE], name="f_nblk")
        padded = sb([128, NE], name="f_padded")
        pend = sb([128, NE], name="f_pend")
        pstart = sb([128, NE], name="f_pstart")
        cmp3 = big[:, 0:NE * 64].rearrange("p (e m) -> p e m", m=64)
        V(lambda e: e.tensor_tensor(cmp3, self.bc(rp['cnt'][:, :], [128, NE, 64], 2), self.bc(thr[:, :], [128, NE, 64], 1), ALU.is_gt), r=[k(rp['cnt']), k(thr)], w=[k(big)])
        V(lambda e: e.tensor_reduce(nblk[:], cmp3, AX.X, ALU.add), r=[k(big)], w=[k(nblk)])
        V(lambda e: e.tensor_scalar(padded[:], nblk[:], float(RB), None, ALU.mult), r=[k(nblk)], w=[k(padded)])
        V(lambda e: e.tensor_tensor_scan(pend[:], c['onesf'][:, 0:NE], padded[:], 0.0, ALU.mult, ALU.add), r=[k(c['onesf']), k(padded)], w=[k(pend)])
        V(lambda e: e.tensor_tensor(pstart[:], pend[:], padded[:], ALU.subtract), r=[k(pend), k(padded)], w=[k(pstart)])
        oh3 = big[:, 0:NC * NE].rearrange("p (n e) -> p n e", e=NE)
        destf = sb([128, NC], name="f_destf")
        dest = sb([128, NC], I32, "f_dest")
        V(lambda e: e.tensor_tensor(oh3, self.bc(rp['iota32'][:, :], [128, NC, NE], 1), self.bc(rp['eid'][:, :], [128, NC, NE], 2), ALU.is_equal), r=[k(rp['iota32']), k(rp['eid'])], w=[k(big)])
        V(lambda e: e.tensor_tensor(oh3, oh3, self.bc(pstart[:, :], [128, NC, NE], 1), ALU.mult), r=[k(big), k(pstart)], w=[k(big)])
        V(lambda e: e.tensor_reduce(destf[:], oh3, AX.X, ALU.add), r=[k(big)], w=[k(destf)])
        V(lambda e: e.tensor_tensor(destf[:], destf[:], rp['rnk'][:], ALU.add), r=[k(destf), k(rp['rnk'])], w=[k(destf)])
        V(lambda e: e.tensor_copy(dest[:], destf[:]), r=[k(destf)], w=[k(dest)])
        bs_i = sb([128, NB], I32, "f_bsi")
        bstart = sb([128, NB], name="f_bstart")
        be = sb([128, NB], name="f_be")
        G(lambda e: e.iota(bs_i[:], pattern=[[RB, NB]], base=0, channel_multiplier=0), w=[k(bs_i)])
        V(lambda e: e.tensor_copy(bstart[:], bs_i[:]), r=[k(bs_i)], w=[k(bstart)])
        cmpb = big[:, 0:NB * NE].rearrange("p (b e) -> p b e", e=NE)
        V(lambda e: e.tensor_tensor(cmpb, self.bc(pend[:, :], [128, NB, NE], 1), self.bc(bstart[:, :], [128, NB, NE], 2), ALU.is_le), r=[k(pend), k(bstart)], w=[k(big)])
        V(lambda e: e.tensor_reduce(be[:], cmpb, AX.X, ALU.add), r=[k(big)], w=[k(be)])
        V(lambda e: e.tensor_scalar(be[:], be[:], float(NE - 1), None, ALU.min), r=[k(be)], w=[k(be)])
        kp_i = sb([128, 8], I32, "f_kpi")
        kp = sb([128, 8], name="f_kp")
        G(lambda e: e.iota(kp_i[:], pattern=[[128, 8]], base=0, channel_multiplier=1), w=[k(kp_i)])
        V(lambda e: e.tensor_copy(kp[:], kp_i[:]), r=[k(kp_i)], w=[k(kp)])
        widf = sb([128, NB, 8], name="f_widf")
        wid = sb([128, NB, 8], I32, "f_wid")
        didf = sb([128, NB, 4], name="f_didf")
        did = sb([128, NB, 4], I32, "f_did")
        bew = sb([128, NB], name="f_bew")
        V(lambda e: e.tensor_scalar(bew[:], be[:], float(D), float(l * NE * D), ALU.mult, ALU.add), r=[k(be)], w=[k(bew)])
        V(lambda e: e.tensor_tensor(widf[:], self.bc(bew[:, :], [128, NB, 8], 2), self.bc(kp[:, :], [128, NB, 8], 1), ALU.add), r=[k(bew), k(kp)], w=[k(widf)])
        V(lambda e: e.tensor_copy(wid[:], widf[:]), r=[k(widf)], w=[k(wid)])
        V(lambda e: e.tensor_scalar(bew[:], be[:], float(FF), float(l * NE * FF), ALU.mult, ALU.add), r=[k(be)], w=[k(bew)])
        V(lambda e: e.tensor_tensor(didf[:], self.bc(bew[:, :], [128, NB, 4], 2), self.bc(kp[:, 0:4], [128, NB, 4], 1), ALU.add), r=[k(bew), k(kp)], w=[k(didf)])
        V(lambda e: e.tensor_copy(did[:], didf[:]), r=[k(didf)], w=[k(did)])
        hbs = [sb([128, D], BF16, "e_hb%d" % j) for j in range(2)]
        for i in range(NT):
            hb = hbs[i % 2]
            self.load('sp', hb[:], self.h1b_d[i * 128:(i + 1) * 128, :], k(hb), dkeys=["h1bd_%d_%d" % (l, i)])
            for j in range(2):
                col = 2 * i + j
                self.P.dma('pool', lambda e, col=col, hb=hb: e.indirect_dma_start(out=self.xs_d.ap(), out_offset=bass.IndirectOffsetOnAxis(ap=dest[:, col:col + 1], axis=0),
                                                                                  in_=hb[:], in_offset=None), k(hb), r=[k(hb), k(dest)], w=["xs_d"])
        self.P.barrier()
        import os
        mstop = int(os.environ.get('MOESTOP', '9'))
        if mstop <= 1:
            self.sb_off = mark
            return
        wg = [sb([128, 8, FF], BF16, "e_wg%d" % j) for j in range(2)]
        wu = [sb([128, 8, FF], BF16, "e_wu%d" % j) for j in range(2)]
        wd = [sb([128, 4, D], BF16, "e_wd%d" % j) for j in range(2)]
        xs = [sb([128, NR, D], BF16, "e_xs%d" % j) for j in range(2)]
        xsT = sb([128, 8, RB], BF16, "e_xsT")
        hT = sb([128, 4, RB], BF16, "e_hT")
        sg = sb([128, RB], name="e_sg")
        ys = [sb([128, D], name="e_ys%d" % j) for j in range(2)]
        tg, tu, td = d['moe_w_gate'].ap(), d['moe_w_up'].ap(), d['moe_w_down'].ap()
        nys = 0
        for b in range(NB):
            j = b % 2
            for kc in range(8):
                self.P.dma('pool', lambda e, j=j, b=b, kc=kc: e.indirect_dma_start(out=wg[j][:, kc, :], out_offset=None, in_=tg,
                                                                                  in_offset=bass.IndirectOffsetOnAxis(ap=wid[:, b, kc:kc + 1], axis=0)), k(wg[j]), r=[k(wid)], w=[k(wg[j])])
                self.P.dma('pool', lambda e, j=j, b=b, kc=kc: e.indirect_dma_start(out=wu[j][:, kc, :], out_offset=None, in_=tu,
                                                                                  in_offset=bass.IndirectOffsetOnAxis(ap=wid[:, b, kc:kc + 1], axis=0)), k(wu[j]), r=[k(wid)], w=[k(wu[j])])
            for fc in range(4):
                self.P.dma('pool', lambda e, j=j, b=b, fc=fc: e.indirect_dma_start(out=wd[j][:, fc, :], out_offset=None, in_=td,
                                                                                  in_offset=bass.IndirectOffsetOnAxis(ap=did[:, b, fc:fc + 1], axis=0)), k(wd[j]), r=[k(did)], w=[k(wd[j])])
            self.load('sp', xs[j][:], self.xs_d[b * RB:(b + 1) * RB, :].rearrange("(r p) n -> p r n", p=128), k(xs[j]), dkeys=["xs_d"])
            for r_ in range(NR):
                bt, kt = self.bank()
                pb = bt[:].bitcast(BF16)
                for kc in range(8):
                    M(lambda e, pb=pb, j=j, r_=r_, kc=kc: e.transpose(pb[:, kc * 128:(kc + 1) * 128], xs[j][:, r_, kc * 128:(kc + 1) * 128], c['identb'][:]), r=[k(xs[j]), k(c['identb'])], w=[kt])
                V(lambda e, pb=pb, r_=r_: e.tensor_copy(xsT[:, :, r_ * 128:(r_ + 1) * 128], pb.rearrange("p (a b) -> p a b", b=128)), r=[kt], w=[k(xsT)])
            for fc in range(4):
                bg, kg = self.bank()
                for kc in range(8):
                    M(lambda e, bg=bg, kc=kc, fc=fc, j=j: e.matmul(bg[:, 0:RB], lhsT=wg[j][:, kc, fc * 128:(fc + 1) * 128], rhs=xsT[:, kc, :], start=(kc == 0), stop=(kc == 7)),
                      r=[k(wg[j]), k(xsT)], w=[kg])
                bu, ku = self.bank()
                for kc in range(8):
                    M(lambda e, bu=bu, kc=kc, fc=fc, j=j: e.matmul(bu[:, 0:RB], lhsT=wu[j][:, kc, fc * 128:(fc + 1) * 128], rhs=xsT[:, kc, :], start=(kc == 0), stop=(kc == 7)),
                      r=[k(wu[j]), k(xsT)], w=[ku])
                A(lambda e, bg=bg: e.activation(out=sg[:], in_=bg[:, 0:RB], func=AF.Silu), r=[kg], w=[k(sg)])
                V(lambda e, bu=bu, fc=fc: e.tensor_tensor(hT[:, fc, :], sg[:], bu[:, 0:RB], ALU.mult), r=[k(sg), ku], w=[k(hT)])
            for r_ in range(NR):
                yb = ys[nys % 2]
                nys += 1
                for half in range(2):
                    bo, ko = self.bank()
                    for fc in range(4):
                        M(lambda e, bo=bo, fc=fc, r_=r_, half=half, j=j: e.matmul(bo[:, :], lhsT=hT[:, fc, r_ * 128:(r_ + 1) * 128], rhs=wd[j][:, fc, half * 512:(half + 1) * 512],
                                                                                 start=(fc == 0), stop=(fc == 3)), r=[k(hT), k(wd[j])], w=[ko])
                    if half == 0:
                        A(lambda e, bo=bo, yb=yb: e.copy(out=yb[:, 0:512], in_=bo[:, :]), r=[ko], w=[k(yb)])
                    else:
                        V(lambda e, bo=bo, yb=yb: e.tensor_copy(yb[:, 512:1024], bo[:, :]), r=[ko], w=[k(yb)])
                r0 = b * RB + r_ * 128
                self.store('sp', self.ys_d[r0:r0 + 128, :], yb[:], k(yb), dkeys=["ys_d"])
        self.P.barrier()
        if mstop <= 2:
            self.sb_off = mark
            return
        l2g = sb([128, D], name="c_l2g")
        l2b = sb([128, D], name="c_l2b")
        self.load('sp', l2g[:], d['ln2_g'][l].partition_broadcast(128), k(l2g))
        self.load('sp', l2b[:], d['ln2_b'][l].partition_broadcast(128), k(l2b))
        csets = []
        for j in range(2):
            csets.append(dict(h1=sb([128, D], name="c_h1%d" % j), y0=sb([128, D], name="c_y0%d" % j), y1=sb([128, D], name="c_y1%d" % j),
                              tmp=sb([128, D], name="c_tmp%d" % j), h2=sb([128, D], name="c_h2%d" % j), hb=sb([128, D], BF16, "c_hb%d" % j),
                              hT=sb([128, 8, 128], BF16, "c_hT%d" % j), st=self.ln_stats("c%d" % j)))

        def cbody(i):
            B = csets[i % 2]
            h1, y0, y1 = B['h1'], B['y0'], B['y1']
            self.load('sp', h1[:], self.h1_d[i * 128:(i + 1) * 128, :], k(h1), dkeys=["h1d_%d_%d" % (l, i)])
            for j, yt in ((0, y0), (1, y1)):
                col = 2 * i + j
                self.P.dma('pool', lambda e, col=col, yt=yt: e.indirect_dma_start(out=yt[:], out_offset=None, in_=self.ys_d.ap(),
                                                                                 in_offset=bass.IndirectOffsetOnAxis(ap=dest[:, col:col + 1], axis=0)), k(yt), r=[k(dest), "ys_d"], w=[k(yt)])
            yield
            V(lambda e: e.tensor_scalar(y0[:], y0[:], rp['gat'][:, 2 * i:2 * i + 1], None, ALU.mult), r=[k(y0), k(rp['gat'])], w=[k(y0)])
            yield
            V(lambda e: e.scalar_tensor_tensor(y0[:], y1[:], rp['gat'][:, 2 * i + 1:2 * i + 2], y0[:], ALU.mult, ALU.add), r=[k(y1), k(y0), k(rp['gat'])], w=[k(y0)])
            yield
            V(lambda e: e.scalar_tensor_tensor(h1[:], h1[:], ALPHA, y0[:], ALU.mult, ALU.add), r=[k(h1), k(y0)], w=[k(h1)])
            yield
            yield from self.layernorm(h1, l2g, l2b, B['h2'], B['tmp'], B['st'])
            if last:
                self.store('sp', self.out_d[i * 128:(i + 1) * 128, :], B['h2'][:], k(B['h2']), dkeys=["out_%d" % i])
                yield
            else:
                self.store('sp', self.h_d[i * 128:(i + 1) * 128, :], B['h2'][:], k(B['h2']), dkeys=["hd_%d" % i])
                yield
                A(lambda e: e.copy(out=B['hb'][:], in_=B['h2'][:]), r=[k(B['h2'])], w=[k(B['hb'])])
                yield
                bk, bkey = self.bank()
                pb = bk[:].bitcast(BF16)
                for kc in range(8):
                    M(lambda e, kc=kc: e.transpose(pb[:, kc * 128:(kc + 1) * 128], B['hb'][:, kc * 128:(kc + 1) * 128], c['identb'][:]), r=[k(B['hb']), k(c['identb'])], w=[bkey])
                V(lambda e: e.tensor_copy(B['hT'][:].rearrange("p a b -> p (a b)"), pb), r=[bkey], w=[k(B['hT'])])
                yield
                self.store('sp', self.hT_d[:, :, i * 128:(i + 1) * 128], B['hT'][:], k(B['hT']), dkeys=["hTd_%d" % i])
                yield
        self.run_pipe([lambda i=i: cbody(i) for i in range(NT)], 2)
        self.P.barrier()
        self.sb_off = mark

    def build_full(self):
        self.declare_inputs()
        T = self.T
        self.h_d = self.dscr("h_d", [T, D])
        self.hT_d = self.dscr("hT_d", [128, 8, T], BF16)
        self.h1_d = self.dscr("h1_d", [T, D])
        self.h1b_d = self.dscr("h1b_d", [T, D], BF16)
        self.xs_d = self.dscr("xs_d", [self.NB * self.RB, D], BF16)
        self.ys_d = self.dscr("ys_d", [self.NB * self.RB, D])
        self.out_d = self.dout("out", [T, D])
        if self.debug:
            self.dbg_y = self.dout("dbg_y", [T, D])
        self.consts()
        self.alloc_route_persist()
        base = self.sb_off
        for l in range(self.depth):
            self.sb_off = base
            self.alloc_params()
            self.alloc_mixer()
            self.alloc_router()
            if l == 0:
                self.stage0()
                self.P.barrier()
            self.load_params(l)
            self.stageM(l)
            self.P.barrier()
            self.sb_off = base
            self.stageMoE(l, l == self.depth - 1)
        self.P.barrier()
        return self.nc


def _host_inputs(inputs, b, T):
    m = {}
    for k, v in inputs.items():
        v = np.asarray(v)
        if k == 'x':
            m[k] = np.ascontiguousarray(v[b, :T])
        elif k == 'rwkv_r_k':
            m[k] = np.ascontiguousarray(v.reshape(DEPTH, 256))
        elif k in ('moe_w_gate', 'moe_w_up'):
            m[k] = np.ascontiguousarray(v.reshape(DEPTH * NE * D, FF))
        elif k == 'moe_w_down':
            m[k] = np.ascontiguousarray(v.reshape(DEPTH * NE * FF, D))
        else:
            m[k] = np.ascontiguousarray(v)
    return m


def kernel(**inputs):
    x = np.asarray(inputs['x'])
    Bsz, T, _ = x.shape
    bld = Builder(T)
    nc = bld.build_full()
    in_maps = [_host_inputs(inputs, b, T) for b in range(Bsz)]
    res = run_bass_kernel_spmd(nc, in_maps, core_ids=list(range(Bsz)))
    return np.stack([np.asarray(r["out"]) for r in res.results], axis=0).astype(np.float32)
```

```python
import numpy as np
import concourse.bass as bass
import concourse.mybir as mybir
from concourse.bass_utils import run_bass_kernel_spmd

F32 = mybir.dt.float32
BF16 = mybir.dt.bfloat16
I32 = mybir.dt.int32
U32 = mybir.dt.uint32
AF = mybir.ActivationFunctionType
ALU = mybir.AluOpType
AX = mybir.AxisListType

D = 1024
NIN = 2968
DEPTH = 2
ALPHA = (2 * DEPTH) ** 0.25
LN_EPS = 1e-5
RMS_EPS = 1e-6
GN_EPS = 64e-5
NE = 32
FF = 512
O_Z, O_XBC, O_DT, O_RW, O_GQ, O_GK, O_GV, O_GG, O_GA = 0, 512, 1280, 1288, 2184, 2312, 2440, 2696, 2952


class Prog:
    EPOCH = 8192
    NDMA = 40

    def __init__(self, nc):
        self.nc = nc
        self.eng = {'pe': nc.tensor, 'dve': nc.vector, 'act': nc.scalar, 'pool': nc.gpsimd, 'sp': nc.sync}
        self.esems = {n: [] for n in ('pe', 'dve', 'act', 'pool')}
        self.cnt = {n: 0 for n in ('pe', 'dve', 'act', 'pool')}
        self.dsems, self.dval, self.dkey = [], [], {}
        self.waited = {n: {} for n in self.eng}
        self.lastw, self.readers = {}, {}
        self.ninst = 0

    def _esem(self, X, ep):
        while len(self.esems[X]) <= ep:
            self.esems[X].append(self.nc.alloc_semaphore("s_%s_%d" % (X, len(self.esems[X]))))
        return self.esems[X][ep]

    def _deps(self, reads, writes):
        deps = {}

        def add(ev):
            if ev is not None and deps.get(ev[0], 0) < ev[1]:
                deps[ev[0]] = ev[1]
        for r in reads:
            add(self.lastw.get(r))
        for w in writes:
            add(self.lastw.get(w))
            for k, v in self.readers.get(w, {}).items():
                add((k, v))
        return deps

    def _wait(self, X, deps):
        e = self.eng[X]
        for k, v in deps.items():
            if k == X and X == 'pe':
                continue
            if self.waited[X].get(k, 0) >= v:
                continue
            if isinstance(k, str):
                ep = (v - 1) // self.EPOCH
                e.wait_ge(self._esem(k, ep), v - ep * self.EPOCH)
            else:
                v = self.dval[k]
                e.wait_ge(self.dsems[k], v)
            self.waited[X][k] = v
            self.ninst += 1

    def _record(self, ev, reads, writes):
        for r in reads:
            d = self.readers.setdefault(r, {})
            if d.get(ev[0], 0) < ev[1]:
                d[ev[0]] = ev[1]
        for w in writes:
            self.lastw[w] = ev
            self.readers[w] = {}

    def op(self, X, fn, r=(), w=()):
        r = [k for k in r if k is not None]
        w = [k for k in w if k is not None]
        w = w + [k for k in r if isinstance(k, str) and k.startswith('psb') and k not in w]
        self._wait(X, self._deps(r, w))
        inst = fn(self.eng[X])
        self.cnt[X] += 1
        n = self.cnt[X]
        inst.then_inc(self._esem(X, (n - 1) // self.EPOCH), 1)
        self.ninst += 1
        self._record((X, n), r, w)

    def dma(self, X, fn, semkey, r=(), w=()):
        base = semkey.rsplit('_', 1)[0] if semkey.rsplit('_', 1)[-1].isdigit() else semkey
        if base not in self.dkey:
            i = len(self.dsems)
            self.dsems.append(self.nc.alloc_semaphore("d_%d" % i))
            self.dval.append(0)
            self.dkey[base] = i
        i = self.dkey[base]
        self._wait(X, self._deps(r, w))
        inst = fn(self.eng[X])
        self.dval[i] += 16
        inst.then_inc(self.dsems[i], 16)
        self.ninst += 1
        self._record((i, self.dval[i]), r, w)

    def barrier(self):
        deps = {k: v for k, v in self.cnt.items() if v > 0}
        for i, v in enumerate(self.dval):
            if v > 0:
                deps[i] = v
        for X in self.eng:
            self._wait(X, dict(deps))


class Tile:
    def __init__(self, t, key):
        self.t, self.key = t, key

    def __getitem__(self, k):
        return self.t[k]


class Builder:
    def __init__(self, T, depth=DEPTH, debug=False, rb=None):
        import os
        rb = rb or int(os.environ.get('RB', '512'))
        self.T, self.depth, self.debug = T, depth, debug
        self.NT = T // 128
        self.RB = rb
        self.NB = (2 * T) // rb + NE
        nc = self.nc = bass.Bass("TRN2", target_bir_lowering=False)
        self.P = Prog(nc)
        self.nsb = 0
        self.sb_off = 16640
        self.sb_peak = 0
        self.sb_cap = 229376
        self.bank_i = 0
        self.chain_i = {}
        self.banks = [nc.alloc_psum_tensor("psb%d" % i, [128, 512], F32) for i in range(8)]
        self.dbg = {}

    def sb(self, shape, dt=F32, name=None):
        self.nsb += 1
        name = "%s_%d" % (name or "t", self.nsb)
        esz = 2 if dt == BF16 else 4
        n = 1
        for v in shape[1:]:
            n *= v
        nbytes = (n * esz + 31) // 32 * 32
        off = self.sb_off
        self.sb_off += nbytes
        assert self.sb_off <= self.sb_cap, "SBUF overflow %d" % self.sb_off
        self.sb_peak = max(self.sb_peak, self.sb_off)
        return Tile(self.nc.alloc_sbuf_tensor_at(name, list(shape), dt, offset=off), name)

    CHAIN_BANKS = {'s': [0, 1], 'r': [2, 3, 4], 'g': [5, 6], 'e': [7]}

    def bank(self, chain=None):
        if chain is None:
            i = self.bank_i
            self.bank_i = (i + 1) % 8
        else:
            lst = self.CHAIN_BANKS[chain]
            j = self.chain_i.get(chain, 0)
            self.chain_i[chain] = (j + 1) % len(lst)
            i = lst[j]
        return self.banks[i], "psb%d" % i

    def V(self, fn, r=(), w=()):
        self.P.op('dve', fn, r, w)

    def A(self, fn, r=(), w=()):
        self.P.op('act', fn, r, w)

    def G(self, fn, r=(), w=()):
        self.P.op('pool', fn, r, w)

    def M(self, fn, r=(), w=()):
        self.P.op('pe', fn, r, w)

    def din(self, name, shape, dt=F32):
        return self.nc.dram_tensor(name, list(shape), dt, kind="ExternalInput")

    def dscr(self, name, shape, dt=F32):
        return self.nc.dram_tensor(name, list(shape), dt, kind="Internal")

    def dout(self, name, shape, dt=F32):
        return self.nc.dram_tensor(name, list(shape), dt, kind="ExternalOutput")

    def load(self, q, out_ap, in_ap, key, dkeys=(), slow=False):
        if slow:
            self.P.dma(q, lambda e: e.dma_start(out=out_ap, in_=in_ap, allow_slow_non_contiguous=True), key, r=list(dkeys), w=[key])
        else:
            self.P.dma(q, lambda e: e.dma_start(out=out_ap, in_=in_ap), key, r=list(dkeys), w=[key])

    def store(self, q, out_ap, in_ap, key, dkeys=()):
        self.P.dma(q, lambda e: e.dma_start(out=out_ap, in_=in_ap), key, r=[key], w=list(dkeys))

    def consts(self):
        nc = self.nc
        c = self.c = {}
        self._ln_st = self.sb([128, 2, 6], name="ln_st")
        self._ln_mv = self.sb([128, 2], name="ln_mv")
        self._ln_rs = self.sb([128, 1], name="ln_rs")
        onesf = c['onesf'] = self.sb([128, 128], name="onesf")
        self.G(lambda e: e.memset(onesf[:], 1.0), w=[onesf.key])
        identf = c['identf'] = self.sb([128, 128], name="identf")
        self.G(lambda e: e.memset(identf[:], 0.0), w=[identf.key])
        self.G(lambda e: e.affine_select(out=identf[:], in_=identf[:], pattern=[[-1, 128]], base=0, channel_multiplier=1,
                                         compare_op=ALU.not_equal, fill=1.0), r=[identf.key], w=[identf.key])
        identb = c['identb'] = self.sb([128, 128], BF16, name="identb")
        self.V(lambda e: e.tensor_copy(identb[:], identf[:]), r=[identf.key], w=[identb.key])
        tri = c['tri'] = self.sb([128, 128], name="tri")
        self.G(lambda e: e.affine_select(out=tri[:], in_=onesf[:], pattern=[[1, 128]], base=0, channel_multiplier=-1,
                                         compare_op=ALU.is_ge, fill=0.0), r=[onesf.key], w=[tri.key])
        su = c['su'] = self.sb([128, 128], name="su")
        self.G(lambda e: e.affine_select(out=su[:], in_=onesf[:], pattern=[[-1, 128]], base=0, channel_multiplier=1,
                                         compare_op=ALU.is_gt, fill=0.0), r=[onesf.key], w=[su.key])
        sl = c['sl'] = self.sb([128, 128], name="sl")
        self.G(lambda e: e.affine_select(out=sl[:], in_=onesf[:], pattern=[[1, 128]], base=0, channel_multiplier=-1,
                                         compare_op=ALU.is_gt, fill=0.0), r=[onesf.key], w=[sl.key])
        maskb = c['maskb'] = self.sb([128, 128], name="maskb")
        self.G(lambda e: e.tensor_copy(maskb[:], tri[:]), r=[tri.key], w=[maskb.key])
        self.G(lambda e: e.memset(maskb[0:64, 64:128], 0.0), w=[maskb.key])
        rmask = c['rmask'] = self.sb([128, 256], name="rmask")
        self.G(lambda e: e.memset(rmask[:], 1.0), w=[rmask.key])
        self.G(lambda e: e.memset(rmask[:].rearrange("p (a b) -> p a b", b=64)[:, :, 0:1], 0.0), w=[rmask.key])
        hm = c['hm'] = self.sb([128, 2], name="hm")
        self.G(lambda e: e.memset(hm[:], 0.0), w=[hm.key])
        self.G(lambda e: e.memset(hm[0:64, 0:1], 1.0), w=[hm.key])
        self.G(lambda e: e.memset(hm[64:128, 1:2], 1.0), w=[hm.key])
        nhm = c['nhm'] = self.sb([128, 2], name="nhm")
        self.V(lambda e: e.tensor_scalar(nhm[:], hm[:], -1.0, None, ALU.mult), r=[hm.key], w=[nhm.key])
        qm = c['qm'] = self.sb([64, 2], name="qm")
        self.G(lambda e: e.memset(qm[:], 0.0), w=[qm.key])
        self.G(lambda e: e.memset(qm[0:32, 0:1], 32.0 ** -0.5), w=[qm.key])
        self.G(lambda e: e.memset(qm[32:64, 1:2], 32.0 ** -0.5), w=[qm.key])
        bones = c['bones'] = self.sb([128, 128], name="bones")
        self.G(lambda e: e.memset(bones[:], 0.0), w=[bones.key])
        self.G(lambda e: e.memset(bones[0:64, 0:64], 1.0), w=[bones.key])
        self.G(lambda e: e.memset(bones[64:128, 64:128], 1.0), w=[bones.key])

    def layernorm(self, xin, gk, bk, out, tmp, stt=None):
        st, mv, rs = stt if stt is not None else (self._ln_st, self._ln_mv, self._ln_rs)
        for i in range(2):
            self.V(lambda e, i=i: e.bn_stats(st[:, i, :], xin[:, i * 512:(i + 1) * 512]), r=[xin.key], w=[st.key])
        self.V(lambda e: e.bn_aggr(mv[:], st[:].rearrange("p a b -> p (a b)")), r=[st.key], w=[mv.key])
        self.V(lambda e: e.tensor_scalar(rs[:], mv[:, 1:2], LN_EPS, None, ALU.add), r=[mv.key], w=[rs.key])
        yield
        self.A(lambda e: e.activation(out=rs[:], in_=rs[:], func=AF.Sqrt), r=[rs.key], w=[rs.key])
        yield
        self.V(lambda e: e.reciprocal(rs[:], rs[:]), r=[rs.key], w=[rs.key])
        self.V(lambda e: e.tensor_scalar(tmp[:], xin[:], mv[:, 0:1], rs[:, 0:1], ALU.subtract, ALU.mult), r=[xin.key, mv.key, rs.key], w=[tmp.key])
        yield
        self.G(lambda e: e.tensor_tensor(tmp[:], tmp[:], gk[:], ALU.mult), r=[tmp.key, gk.key], w=[tmp.key])
        yield
        self.V(lambda e: e.tensor_tensor(out[:], tmp[:], bk[:], ALU.add), r=[tmp.key, bk.key], w=[out.key])
        yield

    def ln_stats(self, tag):
        return (self.sb([128, 2, 6], name="lnst_" + tag), self.sb([128, 2], name="lnmv_" + tag), self.sb([128, 1], name="lnrs_" + tag))

    def run_pipe(self, bodies, width=2):
        active, nxt = [], 0
        while active or nxt < len(bodies):
            while len(active) < width and nxt < len(bodies):
                active.append(bodies[nxt]())
                nxt += 1
            for g_ in list(active):
                try:
                    next(g_)
                except StopIteration:
                    active.remove(g_)

    def to_fm(self, h_tm, hb, hT):
        c = self.c
        self.A(lambda e: e.copy(out=hb[:], in_=h_tm[:]), r=[h_tm.key], w=[hb.key])
        bk, bkey = self.bank()
        pb = bk[:].bitcast(BF16)
        for kc in range(8):
            self.M(lambda e, kc=kc: e.transpose(pb[:, kc * 128:(kc + 1) * 128], hb[:, kc * 128:(kc + 1) * 128], c['identb'][:]),
                   r=[hb.key, c['identb'].key], w=[bkey])
        self.V(lambda e: e.tensor_copy(hT[:].rearrange("p a b -> p (a b)"), pb), r=[bkey], w=[hT.key])

    def declare_inputs(self):
        L = DEPTH
        d = self.d = {}
        specs = dict(x=[self.T, D], ln_in_g=[D], ln_in_b=[D], w_in=[L, D, NIN], ssd_conv_w=[L, 4, 768], ssd_conv_b=[L, 768],
                     ssd_dt_bias=[L, 8], ssd_a_log=[L, 8], ssd_d=[L, 8], ssd_norm_g=[L, 512], rwkv_mu=[L, 896], rwkv_w0=[L, 256],
                     rwkv_w2=[L, 32, 256], rwkv_a0=[L, 256], rwkv_a2=[L, 32, 256], rwkv_g2=[L, 64, 256], rwkv_k_k=[L, 256],
                     rwkv_k_a=[L, 256], rwkv_r_k=[L, 256], rwkv_ln_g=[L, 256], rwkv_ln_b=[L, 256], gla_w_a2=[L, 16, 128],
                     gla_b_a=[L, 128], gla_norm_g=[L, 256], w_out=[L, D, D], ln1_g=[L, D], ln1_b=[L, D], moe_w_rg=[L, D, 4],
                     moe_b_rg=[L, 4], moe_w_re=[L, D, 32], moe_b_re=[L, 32], moe_w_gate=[L * NE * D, FF], moe_w_up=[L * NE * D, FF],
                     moe_w_down=[L * NE * FF, D], ln2_g=[L, D], ln2_b=[L, D])
        for k, s in specs.items():
            d[k] = self.din(k, s)
        return specs

    def alloc_params(self):
        p = self.p = {}
        sb = self.sb
        p['w_in'] = sb([128, 8, NIN], BF16, "w_in_sb")
        p['w_out'] = sb([128, 8, D], BF16, "w_out_sb")
        p['cw'] = sb([128, 6, 4], name="convw")
        p['cb'] = sb([128, 6], name="convb")
        p['cdiag'] = sb([128, 24, 128], BF16, "cdiag")
        for n, w in (('dtb', 8), ('alog', 8), ('dsk8', 8), ('dsk', 512), ('sng', 512), ('rlg', 256), ('rlb', 256), ('gng', 256),
                     ('l1g', D), ('l1b', D), ('rb36', 36)):
            p[n] = sb([128, w], name="p_" + n)
        for n, w in (('mu', 7), ('omu', 7), ('w0', 2), ('a0', 2), ('kk', 2), ('ka', 2), ('omka', 2), ('rk', 2)):
            p[n] = sb([128, w], name="p_" + n)
        p['ba'] = sb([64, 2], name="p_ba")
        p['w2p'] = sb([128, 256], name="p_w2p")
        p['a2p'] = sb([128, 256], name="p_a2p")
        p['g2p'] = sb([128, 256], name="p_g2p")
        p['wa2'] = sb([32, 128], name="p_wa2")
        p['wr'] = sb([128, 8, 36], name="p_wr")

    def load_params(self, l):
        p, d, c = self.p, self.d, self.c
        q = 'pool'
        win = d['w_in'][l].rearrange("(kc p) n -> p kc n", p=128)
        for kc in range(8):
            for (a, b) in ((0, 1484), (1484, NIN)):
                self.load(q, p['w_in'][:, kc, a:b], win[:, kc, a:b], p['w_in'].key)
        wo = d['w_out'][l].rearrange("(kc p) n -> p kc n", p=128)
        for kc in range(8):
            self.load(q, p['w_out'][:, kc, :], wo[:, kc, :], p['w_out'].key)
        q = 'sp'
        for kk_ in range(4):
            self.load(q, p['cw'][:, :, kk_], d['ssd_conv_w'][l][kk_].rearrange("(cb p) -> p cb", p=128), p['cw'].key, slow=True)
        self.load(q, p['cb'][:], d['ssd_conv_b'][l].rearrange("(cb p) -> p cb", p=128), p['cb'].key, slow=True)
        for n, src in (('dtb', 'ssd_dt_bias'), ('alog', 'ssd_a_log'), ('dsk8', 'ssd_d'), ('sng', 'ssd_norm_g'), ('rlg', 'rwkv_ln_g'),
                       ('rlb', 'rwkv_ln_b'), ('gng', 'gla_norm_g'), ('l1g', 'ln1_g'), ('l1b', 'ln1_b')):
            self.load(q, p[n][:], d[src][l].partition_broadcast(128), p[n].key)
        self.load(q, p['rb36'][:, 0:4], d['moe_b_rg'][l].partition_broadcast(128), p['rb36'].key)
        self.load(q, p['rb36'][:, 4:36], d['moe_b_re'][l].partition_broadcast(128), p['rb36'].key)
        self.load(q, p['mu'][:], d['rwkv_mu'][l].rearrange("(b p) -> p b", p=128), p['mu'].key, slow=True)
        for n, src in (('w0', 'rwkv_w0'), ('a0', 'rwkv_a0'), ('kk', 'rwkv_k_k'), ('ka', 'rwkv_k_a'), ('rk', 'rwkv_r_k')):
            self.load(q, p[n][:], d[src][l].rearrange("(b p) -> p b", p=128), p[n].key, slow=True)
        self.load(q, p['ba'][:], d['gla_b_a'][l].rearrange("(b p) -> p b", p=64), p['ba'].key, slow=True)
        for n in ('w2p', 'a2p', 'g2p'):
            self.G(lambda e, n=n: e.memset(p[n][:], 0.0), w=[p[n].key])
        self.load(q, p['w2p'][0:32, :], d['rwkv_w2'][l], p['w2p'].key)
        self.load(q, p['a2p'][32:64, :], d['rwkv_a2'][l], p['a2p'].key)
        self.load(q, p['g2p'][64:128, :], d['rwkv_g2'][l], p['g2p'].key)
        self.G(lambda e: e.memset(p['wa2'][:], 0.0), w=[p['wa2'].key])
        self.load(q, p['wa2'][16:32, :], d['gla_w_a2'][l], p['wa2'].key)
        self.load(q, p['wr'][:, :, 0:4], d['moe_w_rg'][l].rearrange("(kc p) n -> p kc n", p=128), p['wr'].key, slow=True)
        self.load(q, p['wr'][:, :, 4:36], d['moe_w_re'][l].rearrange("(kc p) n -> p kc n", p=128), p['wr'].key, slow=True)
        self.V(lambda e: e.tensor_scalar(p['omu'][:], p['mu'][:], -1.0, 1.0, ALU.mult, ALU.add), r=[p['mu'].key], w=[p['omu'].key])
        self.V(lambda e: e.tensor_scalar(p['omka'][:], p['ka'][:], -1.0, 1.0, ALU.mult, ALU.add), r=[p['ka'].key], w=[p['omka'].key])
        self.A(lambda e: e.activation(out=p['alog'][:], in_=p['alog'][:], func=AF.Exp), r=[p['alog'].key], w=[p['alog'].key])
        self.V(lambda e: e.tensor_scalar(p['alog'][:], p['alog'][:], -1.0, None, ALU.mult), r=[p['alog'].key], w=[p['alog'].key])
        self.V(lambda e: e.tensor_copy(p['dsk'][:].rearrange("p (h q) -> p h q", q=64), p['dsk8'][:].unsqueeze(2).to_broadcast([128, 8, 64])),
               r=[p['dsk8'].key], w=[p['dsk'].key])
        for cb in range(6):
            for k in range(4):
                self.V(lambda e, cb=cb, k=k: e.tensor_scalar(p['cdiag'][:, cb * 4 + k, :], c['identf'][:], p['cw'][:, cb, k:k + 1], None, ALU.mult),
                       r=[c['identf'].key, p['cw'].key], w=[p['cdiag'].key])

    def alloc_mixer(self):
        s = self.s = {}
        sb = self.sb
        s['hT'] = sb([128, 8, 128], BF16, "m_hT")
        s['htm'] = sb([128, D], name="m_htm")
        s['xbc'] = sb([128, 6, 132], BF16, "m_xbc")
        s['xbB'] = sb([128, 6, 132], BF16, "m_xbB")
        s['xc'] = sb([128, 6, 128], BF16, "m_xc")
        s['xh'] = sb([128, 512], BF16, "m_xh")
        s['xdt'] = sb([128, 512], BF16, "m_xdt")
        s['btm'] = sb([128, 128], BF16, "m_btm")
        s['cm'] = sb([128, 2, 128], BF16, "m_cm")
        s['dt'] = sb([128, 8], name="m_dt")
        s['adt'] = sb([128, 8], name="m_adt")
        s['sp1'] = sb([128, 8], name="m_sp1")
        s['sp2'] = sb([128, 8], name="m_sp2")
        s['R'] = sb([128, 4, 128], name="m_R")
        s['seg'] = sb([128, 8, 128], BF16, "m_seg")
        s['ea'] = sb([128, 8], name="m_ea")
        s['cd'] = sb([128, 4], name="m_cd")
        s['cbm'] = sb([128, 2, 128], BF16, "m_cbm")
        s['toend'] = sb([128, 8], name="m_toend")
        s['S32'] = sb([128, 256], name="m_S32")
        s['Sbf'] = sb([128, 256], BF16, "m_Sbf")
        s['y1'] = sb([128, 512], name="m_y1")
        s['sz'] = sb([128, 512], BF16, "m_sz")
        s['ssq'] = sb([128, 4], name="m_ssq")
        s['ycat'] = sb([128, D], BF16, "m_ycat")
        s['yT'] = sb([128, 8, 128], BF16, "m_yT")
        s['gaT'] = sb([32, 128], name="g_gaT")
        s['gx'] = sb([64, 256], name="g_x")
        s['gt1'] = sb([64, 256], name="g_t1")
        s['gcum'] = sb([64, 256], name="g_cum")
        s['geq'] = sb([64, 256], name="g_eq")
        s['gek'] = sb([64, 256], name="g_ek")
        s['gel'] = sb([64, 4], name="g_el")
        s['gqm'] = sb([64, 2, 256], BF16, "g_qm")
        s['gkT'] = sb([64, 256], BF16, "g_kT")
        s['gktm'] = sb([128, 2, 128], BF16, "g_ktm")
        s['gv'] = sb([128, 256], BF16, "g_v")
        s['gvm'] = sb([128, 2, 256], BF16, "g_vm")
        s['gsm'] = sb([128, 4, 128], BF16, "g_sm")
        s['gS'] = sb([64, 2, 64], name="g_S")
        s['gSb'] = sb([64, 2, 2, 64], BF16, "g_Sb")
        s['gst'] = sb([64, 2, 64], name="g_st")
        s['go'] = sb([128, 256], name="g_o")
        s['gsq'] = sb([128, 256], name="g_sq")
        s['grs'] = sb([128, 4], name="g_rs")
        s['gsg'] = sb([128, 256], name="g_sg")
        s['rw'] = sb([128, 7, 129], name="r_rw")
        s['rsh'] = sb([128, 7, 128], name="r_sh")
        s['rt1'] = sb([128, 7, 128], name="r_t1")
        for n in ('ra1', 'ra2', 'ra3', 'rcw', 'recw', 'reicw', 'recwp', 'ra', 'rkk', 'rkp'):
            s[n] = sb([128, 256], name="r_" + n)
        for n in ('rKt', 'rBt'):
            s[n] = sb([128, 256], BF16, "r_" + n)
        s['rAm'] = sb([128, 2, 256], BF16, "r_Am")
        s['rRm'] = sb([128, 2, 256], BF16, "r_Rm")
        s['rtw'] = sb([128, 128], name="r_tw")
        s['rsg'] = sb([128, 128], name="r_sg")
        s['rvtm'] = sb([128, 256], name="r_vtm")
        s['rvc'] = sb([64, 2, 256], BF16, "r_vc")
        s['rBc'] = sb([64, 2, 256], BF16, "r_Bc")
        s['rKc'] = sb([64, 2, 256], BF16, "r_Kc")
        s['rP'] = sb([64, 8, 64], BF16, "r_P")
        s['rQ'] = sb([64, 8, 64], BF16, "r_Q")
        s['rP2'] = sb([64, 8, 64], BF16, "r_P2")
        s['rQ2'] = sb([64, 8, 64], BF16, "r_Q2")
        s['rTT'] = sb([64, 8, 64], BF16, "r_TT")
        s['rAak'] = sb([64, 8, 64], BF16, "r_Aak")
        s['rArb'] = sb([64, 8, 64], BF16, "r_Arb")
        s['rArk'] = sb([64, 8, 64], BF16, "r_Ark")
        s['rG'] = sb([64, 4, 64], BF16, "r_G")
        s['rU'] = sb([64, 4, 64], BF16, "r_U")
        s['rST'] = sb([128, 2, 64], name="r_ST")
        s['rSTb'] = sb([128, 2, 64], BF16, "r_STb")
        s['rt2'] = sb([128, 2, 64], name="r_t2")
        s['rewc'] = sb([128, 4], name="r_ewc")
        s['rY1'] = sb([128, 256], name="r_Y1")
        s['rY'] = sb([128, 256], name="r_Y")
        s['rm1'] = sb([128, 4], name="r_m1")
        s['rm2'] = sb([128, 4], name="r_m2")
        s['rvar'] = sb([128, 4], name="r_var")
        s['rbc'] = sb([128, 4], name="r_bc")
        s['rg'] = sb([128, 256], name="r_g")
        s['mix'] = sb([128, D], name="m_mix")
        s['tmp'] = sb([128, D], name="m_tmp")
        s['h1'] = sb([128, D], name="m_h1")

    def proj_tm(self, out_ap, okey, c0, n):
        s, p = self.s, self.p
        for kc in range(8):
            self.M(lambda e, kc=kc: e.matmul(out_ap, lhsT=s['hT'][:, kc, :], rhs=p['w_in'][:, kc, c0:c0 + n], start=(kc == 0), stop=(kc == 7)),
                   r=[s['hT'].key, p['w_in'].key], w=[okey])

    def proj_fm(self, out_ap, okey, c0, m):
        s, p = self.s, self.p
        for kc in range(8):
            self.M(lambda e, kc=kc: e.matmul(out_ap, lhsT=p['w_in'][:, kc, c0:c0 + m], rhs=s['hT'][:, kc, :], start=(kc == 0), stop=(kc == 7)),
                   r=[s['hT'].key, p['w_in'].key], w=[okey])

    def bc(self, ap, shape, axis):
        return ap.unsqueeze(axis).to_broadcast(list(shape))

    def ssd_tile(self, first):
        s, p, c = self.s, self.p, self.c
        V, A, G, M = self.V, self.A, self.G, self.M
        k = lambda t: t.key
        bz, kz = self.bank('s')
        self.proj_tm(bz[:, :], kz, O_Z, 512)
        A(lambda e: e.activation(out=s['sz'][:], in_=bz[:, :], func=AF.Silu), r=[kz], w=[k(s['sz'])])
        yield
        bd, kd = self.bank('s')
        self.proj_tm(bd[:, 0:8], kd, O_DT, 8)
        V(lambda e: e.tensor_tensor(s['sp1'][:], bd[:, 0:8], p['dtb'][:], ALU.add), r=[kd, k(p['dtb'])], w=[k(s['sp1'])])
        yield
        V(lambda e: e.scalar_tensor_tensor(s['sp2'][:], s['sp1'][:], -1.0, s['sp1'][:], ALU.mult, ALU.max), r=[k(s['sp1'])], w=[k(s['sp2'])])
        yield
        A(lambda e: e.activation(out=s['sp2'][:], in_=s['sp2'][:], func=AF.Exp, scale=-1.0), r=[k(s['sp2'])], w=[k(s['sp2'])])
        yield
        A(lambda e: e.activation(out=s['sp2'][:], in_=s['sp2'][:], func=AF.Ln, bias=1.0), r=[k(s['sp2'])], w=[k(s['sp2'])])
        yield
        V(lambda e: e.scalar_tensor_tensor(s['dt'][:], s['sp1'][:], 0.0, s['sp2'][:], ALU.max, ALU.add), r=[k(s['sp1']), k(s['sp2'])], w=[k(s['dt'])])
        yield
        V(lambda e: e.tensor_tensor(s['adt'][:], s['dt'][:], p['alog'][:], ALU.mult), r=[k(s['dt']), k(p['alog'])], w=[k(s['adt'])])
        yield
        import os
        stop = float(os.environ.get('SSDSTOP', '9'))
        if stop <= 1:
            return
        if first:
            G(lambda e: e.memset(s['xbc'][:, :, 0:4], 0.0), w=[k(s['xbc'])])
            yield
            G(lambda e: e.memset(s['xbB'][:, :, 0:2], 0.0), w=[k(s['xbB'])])
            yield
        else:
            G(lambda e: e.tensor_copy(s['xbc'][:, :, 0:3], s['xbc'][:, :, 128:131]), r=[k(s['xbc'])], w=[k(s['xbc'])])
            yield
            G(lambda e: e.tensor_copy(s['xbB'][:, :, 0:2], s['xbB'][:, :, 128:130]), r=[k(s['xbB'])], w=[k(s['xbB'])])
            yield
        for grp, nb in ((0, 4), (4, 2)):
            bx, kx = self.bank('s')
            for j in range(nb):
                self.proj_fm(bx[:, j * 128:(j + 1) * 128], kx, O_XBC + (grp + j) * 128, 128)
            A(lambda e, bx=bx, grp=grp, nb=nb: e.copy(out=s['xbc'][:, grp:grp + nb, 3:131], in_=bx[:, 0:nb * 128].rearrange("p (a b) -> p a b", b=128)),
              r=[kx], w=[k(s['xbc'])])
            yield
            V(lambda e, bx=bx, grp=grp, nb=nb: e.tensor_copy(s['xbB'][:, grp:grp + nb, 2:130], bx[:, 0:nb * 128].rearrange("p (a b) -> p a b", b=128)),
              r=[kx], w=[k(s['xbB'])])
            yield
        for grp, nb in ((0, 4), (4, 2)):
            bx, kx = self.bank('s')
            for j in range(nb):
                cb = grp + j
                for kk_ in range(4):
                    src = s['xbc'] if kk_ % 2 == 0 else s['xbB']
                    off = kk_ if kk_ % 2 == 0 else kk_ - 1
                    M(lambda e, bx=bx, j=j, cb=cb, kk_=kk_, src=src, off=off: e.matmul(bx[:, j * 128:(j + 1) * 128], lhsT=p['cdiag'][:, cb * 4 + kk_, :],
                                                                                   rhs=src[:, cb, off:off + 128], start=(kk_ == 0), stop=(kk_ == 3)),
                      r=[k(p['cdiag']), k(src)], w=[kx])
            for j in range(nb):
                cb = grp + j
                A(lambda e, bx=bx, j=j, cb=cb: e.activation(out=s['xc'][:, cb, :], in_=bx[:, j * 128:(j + 1) * 128], func=AF.Silu, bias=p['cb'][:, cb:cb + 1]),
                  r=[kx, k(p['cb'])], w=[k(s['xc'])])
                yield
        if stop <= 2:
            return
        bt, kt = self.bank('s')
        pb = bt[:].bitcast(BF16)
        for j in range(5):
            M(lambda e, j=j: e.transpose(pb[:, j * 128:(j + 1) * 128], s['xc'][:, j, :], c['identb'][:]), r=[k(s['xc']), k(c['identb'])], w=[kt])
        V(lambda e: e.tensor_copy(s['xh'][:], pb[:, 0:512]), r=[kt], w=[k(s['xh'])])
        yield
        V(lambda e: e.tensor_copy(s['btm'][:], pb[:, 512:640]), r=[kt], w=[k(s['btm'])])
        yield
        if stop <= 2.2:
            return
        G(lambda e: e.tensor_tensor(s['cm'][:], self.bc(s['xc'][:, 5, :], [128, 2, 128], 1), self.bc(c['hm'][:, :], [128, 2, 128], 2), ALU.mult),
          r=[k(s['xc']), k(c['hm'])], w=[k(s['cm'])])
        yield
        V(lambda e: e.tensor_tensor(s['xdt'][:].rearrange("p (h q) -> p h q", q=64), s['xh'][:].rearrange("p (h q) -> p h q", q=64),
                                    self.bc(s['dt'][:, :], [128, 8, 64], 2), ALU.mult), r=[k(s['xh']), k(s['dt'])], w=[k(s['xdt'])])
        yield
        if stop <= 2.4:
            return
        for half in range(2):
            G(lambda e, half=half: e.tensor_tensor(s['R'][:], self.bc(c['tri'][:, :], [128, 4, 128], 1), self.bc(s['adt'][:, half * 4:(half + 1) * 4], [128, 4, 128], 2), ALU.mult),
              r=[k(c['tri']), k(s['adt'])], w=[k(s['R'])])
            yield
            bD, kD = self.bank('s')
            for q2 in range(2):
                M(lambda e, bD=bD, q2=q2: e.matmul(bD[:, q2 * 256:(q2 + 1) * 256], lhsT=c['su'][:], rhs=s['R'][:, q2 * 2:(q2 + 1) * 2, :].rearrange("p a b -> p (a b)"), start=True, stop=True),
                  r=[k(c['su']), k(s['R'])], w=[kD])
            if stop <= 2.6:
                continue
            A(lambda e, bD=bD, half=half: e.activation(out=s['seg'][:, half * 4:(half + 1) * 4, :].rearrange("p a b -> p (a b)"), in_=bD[:, :], func=AF.Exp),
              r=[kD], w=[k(s['seg'])])
            yield
        if stop <= 2.8:
            return
        V(lambda e: e.tensor_copy(s['toend'][:], s['seg'][:, :, 127]), r=[k(s['seg'])], w=[k(s['toend'])])
        yield
        if stop <= 3:
            return
        be, ke = self.bank('s')
        M(lambda e: e.matmul(be[:, 0:8], lhsT=c['tri'][:], rhs=s['adt'][:], start=True, stop=True), r=[k(c['tri']), k(s['adt'])], w=[ke])
        for g in range(2):
            M(lambda e, g=g: e.matmul(be[g * 64:(g + 1) * 64, 8:12], lhsT=c['onesf'][:, 0:64], rhs=s['adt'][:, g * 4:(g + 1) * 4], start=True, stop=True),
              r=[k(c['onesf']), k(s['adt'])], w=[ke])
        A(lambda e: e.activation(out=s['ea'][:], in_=be[:, 0:8], func=AF.Exp), r=[ke], w=[k(s['ea'])])
        yield
        A(lambda e: e.activation(out=s['cd'][:], in_=be[:, 8:12], func=AF.Exp), r=[ke], w=[k(s['cd'])])
        yield
        if stop <= 4:
            return
        bc_, kc_ = self.bank('s')
        for g in range(2):
            M(lambda e, g=g: e.matmul(bc_[:, g * 128:(g + 1) * 128], lhsT=s['xc'][:, 4, :], rhs=s['cm'][:, g, :], start=True, stop=True),
              r=[k(s['xc']), k(s['cm'])], w=[kc_])
        V(lambda e: e.tensor_tensor(s['cbm'][:], bc_[:, 0:256].rearrange("p (a b) -> p a b", b=128), self.bc(c['tri'][:, :], [128, 2, 128], 1), ALU.mult),
          r=[kc_, k(c['tri'])], w=[k(s['cbm'])])
        yield
        for g in range(2):
            V(lambda e, g=g: e.tensor_tensor(s['seg'][:, g * 4:(g + 1) * 4, :], s['seg'][:, g * 4:(g + 1) * 4, :], self.bc(s['cbm'][:, g, :], [128, 4, 128], 1), ALU.mult),
              r=[k(s['seg']), k(s['cbm'])], w=[k(s['seg'])])
            yield
        by, ky = self.bank('s')
        for h in range(8):
            M(lambda e, h=h: e.matmul(by[:, h * 64:(h + 1) * 64], lhsT=s['seg'][:, h, :], rhs=s['xdt'][:, h * 64:(h + 1) * 64], start=True, stop=True),
              r=[k(s['seg']), k(s['xdt'])], w=[ky])
        bo, ko = self.bank('s')
        if not first:
            for g in range(2):
                M(lambda e, g=g: e.matmul(bo[:, g * 256:(g + 1) * 256], lhsT=s['cm'][:, g, :], rhs=s['Sbf'][:, :], start=True, stop=True),
                  r=[k(s['cm']), k(s['Sbf'])], w=[ko])
            V(lambda e: e.tensor_tensor(s['y1'][:].rearrange("p (h q) -> p h q", q=64), bo[:, :].rearrange("p (h q) -> p h q", q=64),
                                        self.bc(s['ea'][:, :], [128, 8, 64], 2), ALU.mult), r=[ko, k(s['ea'])], w=[k(s['y1'])])
            yield
            V(lambda e: e.tensor_tensor(s['y1'][:], s['y1'][:], by[:, :], ALU.add), r=[k(s['y1']), ky], w=[k(s['y1'])])
            yield
        else:
            V(lambda e: e.tensor_copy(s['y1'][:], by[:, :]), r=[ky], w=[k(s['y1'])])
            yield
        if stop <= 5:
            return
        V(lambda e: e.tensor_tensor(s['xdt'][:].rearrange("p (h q) -> p h q", q=64), s['xdt'][:].rearrange("p (h q) -> p h q", q=64),
                                    self.bc(s['toend'][:, :], [128, 8, 64], 2), ALU.mult), r=[k(s['xdt']), k(s['toend'])], w=[k(s['xdt'])])
        yield
        bs, ks = self.bank('s')
        for g in range(2):
            M(lambda e, g=g: e.matmul(bs[g * 64:(g + 1) * 64, 0:256], lhsT=s['btm'][:, g * 64:(g + 1) * 64], rhs=s['xdt'][:, g * 256:(g + 1) * 256], start=True, stop=True),
              r=[k(s['btm']), k(s['xdt'])], w=[ks])
        if first:
            V(lambda e: e.tensor_copy(s['S32'][:], bs[:, 0:256]), r=[ks], w=[k(s['S32'])])
            yield
        else:
            V(lambda e: e.tensor_tensor(s['S32'][:].rearrange("p (h q) -> p h q", q=64), s['S32'][:].rearrange("p (h q) -> p h q", q=64),
                                        self.bc(s['cd'][:, :], [128, 4, 64], 2), ALU.mult), r=[k(s['S32']), k(s['cd'])], w=[k(s['S32'])])
            yield
            V(lambda e: e.tensor_tensor(s['S32'][:], s['S32'][:], bs[:, 0:256], ALU.add), r=[k(s['S32']), ks], w=[k(s['S32'])])
            yield
        A(lambda e: e.copy(out=s['Sbf'][:], in_=s['S32'][:]), r=[k(s['S32'])], w=[k(s['Sbf'])])
        yield
        G(lambda e: e.tensor_tensor(s['xdt'][:], s['xh'][:], p['dsk'][:], ALU.mult), r=[k(s['xh']), k(p['dsk'])], w=[k(s['xdt'])])
        yield
        V(lambda e: e.tensor_tensor(s['y1'][:], s['y1'][:], s['xdt'][:], ALU.add), r=[k(s['y1']), k(s['xdt'])], w=[k(s['y1'])])
        yield
        V(lambda e: e.tensor_tensor(s['y1'][:], s['y1'][:], s['sz'][:], ALU.mult), r=[k(s['y1']), k(s['sz'])], w=[k(s['y1'])])
        yield
        for g in range(2):
            A(lambda e, g=g: e.activation(out=s['sz'][:, g * 256:(g + 1) * 256], in_=s['y1'][:, g * 256:(g + 1) * 256], func=AF.Square, accum_out=s['ssq'][:, g:g + 1]),
              r=[k(s['y1'])], w=[k(s['sz']), k(s['ssq'])])
            yield
        V(lambda e: e.tensor_scalar(s['ssq'][:, 0:2], s['ssq'][:, 0:2], 1.0 / 256, RMS_EPS, ALU.mult, ALU.add), r=[k(s['ssq'])], w=[k(s['ssq'])])
        yield
        A(lambda e: e.activation(out=s['ssq'][:, 0:2], in_=s['ssq'][:, 0:2], func=AF.Sqrt), r=[k(s['ssq'])], w=[k(s['ssq'])])
        yield
        V(lambda e: e.reciprocal(s['ssq'][:, 0:2], s['ssq'][:, 0:2]), r=[k(s['ssq'])], w=[k(s['ssq'])])
        yield
        for g in range(2):
            V(lambda e, g=g: e.scalar_tensor_tensor(s['ycat'][:, g * 256:(g + 1) * 256], s['y1'][:, g * 256:(g + 1) * 256], s['ssq'][:, g:g + 1],
                                                   p['sng'][:, g * 256:(g + 1) * 256], ALU.mult, ALU.mult), r=[k(s['y1']), k(s['ssq']), k(p['sng'])], w=[k(s['ycat'])])
            yield

    def gla_tile(self, first):
        s, p, c = self.s, self.p, self.c
        V, A, G, M = self.V, self.A, self.G, self.M
        k = lambda t: t.key
        bv, kv = self.bank('g')
        self.proj_tm(bv[:, :], kv, O_GV, 512)
        A(lambda e: e.copy(out=s['gv'][:], in_=bv[:, 0:256]), r=[kv], w=[k(s['gv'])])
        yield
        A(lambda e: e.activation(out=s['gsg'][:], in_=bv[:, 256:512], func=AF.Silu), r=[kv], w=[k(s['gsg'])])
        yield
        G(lambda e: e.tensor_tensor(s['gsg'][:], s['gsg'][:], p['gng'][:], ALU.mult), r=[k(s['gsg']), k(p['gng'])], w=[k(s['gsg'])])
        yield
        ba_, ka_ = self.bank('g')
        self.proj_fm(ba_[0:32, 0:128], ka_, O_GA - 16, 32)
        A(lambda e: e.copy(out=s['gaT'][:], in_=ba_[0:32, 0:128]), r=[ka_], w=[k(s['gaT'])])
        yield
        bx, kx = self.bank('g')
        for pr in range(2):
            M(lambda e, pr=pr: e.matmul(bx[0:64, pr * 128:(pr + 1) * 128], lhsT=p['wa2'][:, pr * 64:(pr + 1) * 64], rhs=s['gaT'][:], start=True, stop=True),
              r=[k(p['wa2']), k(s['gaT'])], w=[kx])
        for pr in range(2):
            A(lambda e, pr=pr: e.activation(out=s['gx'][:, pr * 128:(pr + 1) * 128], in_=bx[0:64, pr * 128:(pr + 1) * 128], func=AF.Identity, bias=p['ba'][:, pr:pr + 1]),
              r=[kx, k(p['ba'])], w=[k(s['gx'])])
            yield
        V(lambda e: e.scalar_tensor_tensor(s['gt1'][:], s['gx'][:], -1.0, s['gx'][:], ALU.mult, ALU.max), r=[k(s['gx'])], w=[k(s['gt1'])])
        yield
        A(lambda e: e.activation(out=s['gt1'][:], in_=s['gt1'][:], func=AF.Exp, scale=-1.0), r=[k(s['gt1'])], w=[k(s['gt1'])])
        yield
        A(lambda e: e.activation(out=s['gt1'][:], in_=s['gt1'][:], func=AF.Ln, bias=1.0), r=[k(s['gt1'])], w=[k(s['gt1'])])
        yield
        V(lambda e: e.scalar_tensor_tensor(s['gx'][:], s['gx'][:], 0.0, s['gt1'][:], ALU.min, ALU.subtract), r=[k(s['gx']), k(s['gt1'])], w=[k(s['gx'])])
        yield
        V(lambda e: e.tensor_tensor_scan(s['gcum'][:], c['rmask'][0:64, :], s['gx'][:], 0.0, ALU.mult, ALU.add), r=[k(c['rmask']), k(s['gx'])], w=[k(s['gcum'])])
        yield
        A(lambda e: e.activation(out=s['geq'][:], in_=s['gcum'][:], func=AF.Exp, scale=1.0 / 16), r=[k(s['gcum'])], w=[k(s['geq'])])
        yield
        A(lambda e: e.activation(out=s['gek'][:], in_=s['gcum'][:], func=AF.Exp, scale=-1.0 / 16), r=[k(s['gcum'])], w=[k(s['gek'])])
        yield
        A(lambda e: e.activation(out=s['gel'][:], in_=s['gcum'][:].rearrange("p (a b) -> p a b", b=64)[:, :, 63], func=AF.Exp, scale=1.0 / 16),
          r=[k(s['gcum'])], w=[k(s['gel'])])
        yield
        bq, kq = self.bank('g')
        for pr in range(2):
            self.proj_fm(bq[0:64, pr * 128:(pr + 1) * 128], kq, O_GQ + pr * 64, 64)
        for pr in range(2):
            self.proj_fm(bq[0:64, 256 + pr * 128:256 + (pr + 1) * 128], kq, O_GK + pr * 64, 64)
        for hh in range(2):
            V(lambda e, hh=hh: e.scalar_tensor_tensor(s['gqm'][:, hh, :], bq[0:64, 0:256], c['qm'][:, hh:hh + 1], s['geq'][:], ALU.mult, ALU.mult),
              r=[kq, k(c['qm']), k(s['geq'])], w=[k(s['gqm'])])
            yield
        V(lambda e: e.tensor_tensor(s['gkT'][:], bq[0:64, 256:512], s['gek'][:], ALU.mult), r=[kq, k(s['gek'])], w=[k(s['gkT'])])
        yield
        bt, kt = self.bank('g')
        pb = bt[:].bitcast(BF16)
        for pr in range(2):
            M(lambda e, pr=pr: e.transpose(pb[:, pr * 64:(pr + 1) * 64], s['gkT'][:, pr * 128:(pr + 1) * 128], c['identb'][0:64, 0:64]),
              r=[k(s['gkT']), k(c['identb'])], w=[kt])
        if first:
            G(lambda e: e.memset(s['gktm'][:], 0.0), w=[k(s['gktm'])])
            yield
        for hh in range(2):
            V(lambda e, hh=hh: e.tensor_copy(s['gktm'][:, hh, :].rearrange("p (a b c) -> p a b c", a=2, b=2)[:, :, hh, :],
                                             pb[:, 0:128].rearrange("p (a b c) -> p a b c", a=2, b=2)[:, :, hh, :]), r=[kt], w=[k(s['gktm'])])
            yield
        G(lambda e: e.tensor_tensor(s['gvm'][:], self.bc(s['gv'][:, :], [128, 2, 256], 1), self.bc(c['hm'][:, :], [128, 2, 256], 2), ALU.mult),
          r=[k(s['gv']), k(c['hm'])], w=[k(s['gvm'])])
        yield
        bs_, ks_ = self.bank('g')
        for h in range(4):
            pr, hh = h // 2, h % 2
            M(lambda e, h=h, pr=pr, hh=hh: e.matmul(bs_[:, h * 128:(h + 1) * 128], lhsT=s['gkT'][:, pr * 128:(pr + 1) * 128], rhs=s['gqm'][:, hh, pr * 128:(pr + 1) * 128],
                                                    start=True, stop=True), r=[k(s['gkT']), k(s['gqm'])], w=[ks_])
        V(lambda e: e.tensor_tensor(s['gsm'][:], bs_[:, :].rearrange("p (a b) -> p a b", b=128), self.bc(c['maskb'][:, :], [128, 4, 128], 1), ALU.mult),
          r=[ks_, k(c['maskb'])], w=[k(s['gsm'])])
        yield
        bu, ku = self.bank('g')
        for cc in range(2):
            for pr in range(2):
                for hh in range(2):
                    h = pr * 2 + hh
                    M(lambda e, cc=cc, h=h, pr=pr, hh=hh: e.matmul(bu[0:64, cc * 128 + pr * 64:cc * 128 + (pr + 1) * 64],
                                                                   lhsT=s['gktm'][:, hh, pr * 64:(pr + 1) * 64], rhs=s['gvm'][:, cc, h * 64:(h + 1) * 64],
                                                                   start=(hh == 0), stop=(hh == 1)), r=[k(s['gktm']), k(s['gvm'])], w=[ku])
        if first:
            G(lambda e: e.memset(s['gS'][:], 0.0), w=[k(s['gS'])])
            yield
        for cc in range(2):
            A(lambda e, cc=cc: e.copy(out=s['gSb'][:, cc, :, :], in_=s['gS'][:]), r=[k(s['gS'])], w=[k(s['gSb'])])
            yield
            V(lambda e, cc=cc: e.tensor_tensor(s['gst'][:], s['gS'][:], bu[0:64, cc * 128:(cc + 1) * 128].rearrange("p (a b) -> p a b", b=64), ALU.add),
              r=[k(s['gS']), ku], w=[k(s['gst'])])
            yield
            V(lambda e, cc=cc: e.tensor_tensor(s['gS'][:], s['gst'][:], self.bc(s['gel'][:, cc::2], [64, 2, 64], 2), ALU.mult),
              r=[k(s['gst']), k(s['gel'])], w=[k(s['gS'])])
            yield
        bo, ko = self.bank('g')
        for h in range(4):
            M(lambda e, h=h: e.matmul(bo[:, h * 64:(h + 1) * 64], lhsT=s['gsm'][:, h, :], rhs=s['gv'][:, h * 64:(h + 1) * 64], start=True, stop=True),
              r=[k(s['gsm']), k(s['gv'])], w=[ko])
        bi, ki = self.bank('g')
        for cc in range(2):
            for h in range(4):
                pr, hh = h // 2, h % 2
                M(lambda e, cc=cc, h=h, pr=pr, hh=hh: e.matmul(bi[cc * 64:(cc + 1) * 64, h * 64:(h + 1) * 64], lhsT=s['gqm'][:, hh, pr * 128 + cc * 64:pr * 128 + (cc + 1) * 64],
                                                               rhs=s['gSb'][:, cc, pr, :], start=True, stop=True), r=[k(s['gqm']), k(s['gSb'])], w=[ki])
        A(lambda e: e.copy(out=s['gsq'][:], in_=bi[:, 0:256]), r=[ki], w=[k(s['gsq'])])
        yield
        V(lambda e: e.tensor_tensor(s['go'][:], s['gsq'][:], bo[:, 0:256], ALU.add), r=[k(s['gsq']), ko], w=[k(s['go'])])
        yield
        A(lambda e: e.activation(out=s['gsq'][:], in_=s['go'][:], func=AF.Square), r=[k(s['go'])], w=[k(s['gsq'])])
        yield
        V(lambda e: e.tensor_reduce(s['grs'][:], s['gsq'][:].rearrange("p (h q) -> p h q", q=64), AX.X, ALU.add), r=[k(s['gsq'])], w=[k(s['grs'])])
        yield
        V(lambda e: e.tensor_scalar(s['grs'][:], s['grs'][:], 1.0 / 64, RMS_EPS, ALU.mult, ALU.add), r=[k(s['grs'])], w=[k(s['grs'])])
        yield
        A(lambda e: e.activation(out=s['grs'][:], in_=s['grs'][:], func=AF.Sqrt), r=[k(s['grs'])], w=[k(s['grs'])])
        yield
        V(lambda e: e.reciprocal(s['grs'][:], s['grs'][:]), r=[k(s['grs'])], w=[k(s['grs'])])
        yield
        V(lambda e: e.tensor_tensor(s['go'][:].rearrange("p (h q) -> p h q", q=64), s['go'][:].rearrange("p (h q) -> p h q", q=64),
                                    self.bc(s['grs'][:, :], [128, 4, 64], 2), ALU.mult), r=[k(s['go']), k(s['grs'])], w=[k(s['go'])])
        yield
        V(lambda e: e.tensor_tensor(s['ycat'][:, 768:1024], s['go'][:], s['gsg'][:], ALU.mult), r=[k(s['go']), k(s['gsg'])], w=[k(s['ycat'])])
        yield

    def rwkv_tile(self, first):
        s, p, c = self.s, self.p, self.c
        V, A, G, M = self.V, self.A, self.G, self.M
        k = lambda t: t.key
        f2 = lambda t, a, b: t[:, a:b, :].rearrange("p a b -> p (a b)")
        h3 = lambda ap: ap.rearrange("p (h q) -> p h q", q=64)
        if first:
            G(lambda e: e.memset(s['rw'][:, :, 0:1], 0.0), w=[k(s['rw'])])
            yield
            G(lambda e: e.memset(s['rST'][:], 0.0), w=[k(s['rST'])])
            yield
            G(lambda e: e.memset(s['rSTb'][:], 0.0), w=[k(s['rSTb'])])
            yield
        else:
            G(lambda e: e.tensor_copy(s['rw'][:, :, 0:1], s['rw'][:, :, 128:129]), r=[k(s['rw'])], w=[k(s['rw'])])
            yield
        for grp, nb in ((0, 4), (4, 3)):
            bx, kx = self.bank('r')
            for j in range(nb):
                self.proj_fm(bx[:, j * 128:(j + 1) * 128], kx, O_RW + (grp + j) * 128, 128)
            A(lambda e, bx=bx, grp=grp, nb=nb: e.copy(out=s['rw'][:, grp:grp + nb, 1:129], in_=bx[:, 0:nb * 128].rearrange("p (a b) -> p a b", b=128)),
              r=[kx], w=[k(s['rw'])])
            yield
        rt1 = s['rt1'][:]
        G(lambda e: e.tensor_tensor(rt1, s['rw'][:, :, 0:128], self.bc(p['mu'][:, :], [128, 7, 128], 2), ALU.mult), r=[k(s['rw']), k(p['mu'])], w=[k(s['rt1'])])
        yield
        V(lambda e: e.tensor_tensor(s['rsh'][:], s['rw'][:, :, 1:129], self.bc(p['omu'][:, :], [128, 7, 128], 2), ALU.mult), r=[k(s['rw']), k(p['omu'])], w=[k(s['rsh'])])
        yield
        V(lambda e: e.tensor_tensor(s['rsh'][:], s['rsh'][:], rt1, ALU.add), r=[k(s['rsh']), k(s['rt1'])], w=[k(s['rsh'])])
        yield
        rT, kT, vT, lr = f2(s['rsh'], 0, 2), f2(s['rsh'], 2, 4), f2(s['rsh'], 4, 6), s['rsh'][:, 6, :]
        ksh = k(s['rsh'])
        A(lambda e: e.activation(out=s['rtw'][:], in_=lr, func=AF.Tanh), r=[ksh], w=[k(s['rtw'])])
        yield
        bw, kw = self.bank('r')
        for b in range(2):
            M(lambda e, b=b: e.matmul(bw[:, b * 128:(b + 1) * 128], lhsT=p['w2p'][:, b * 128:(b + 1) * 128], rhs=s['rtw'][:], start=True, stop=True),
              r=[k(p['w2p']), k(s['rtw'])], w=[kw])
        for b in range(2):
            A(lambda e, b=b: e.activation(out=s['ra1'][:, b * 128:(b + 1) * 128], in_=bw[:, b * 128:(b + 1) * 128], func=AF.Identity, bias=p['w0'][:, b:b + 1]),
              r=[kw, k(p['w0'])], w=[k(s['ra1'])])
            yield
        V(lambda e: e.scalar_tensor_tensor(s['ra2'][:], s['ra1'][:], -1.0, s['ra1'][:], ALU.mult, ALU.max), r=[k(s['ra1'])], w=[k(s['ra2'])])
        yield
        A(lambda e: e.activation(out=s['ra2'][:], in_=s['ra2'][:], func=AF.Exp, scale=-1.0), r=[k(s['ra2'])], w=[k(s['ra2'])])
        yield
        A(lambda e: e.activation(out=s['ra2'][:], in_=s['ra2'][:], func=AF.Ln, bias=1.0), r=[k(s['ra2'])], w=[k(s['ra2'])])
        yield
        V(lambda e: e.tensor_scalar(s['ra3'][:], s['ra1'][:], -1.0, 0.0, ALU.mult, ALU.max), r=[k(s['ra1'])], w=[k(s['ra3'])])
        yield
        V(lambda e: e.tensor_tensor(s['ra3'][:], s['ra3'][:], s['ra2'][:], ALU.add), r=[k(s['ra3']), k(s['ra2'])], w=[k(s['ra3'])])
        yield
        A(lambda e: e.activation(out=s['ra1'][:], in_=s['ra3'][:], func=AF.Exp, scale=-1.0), r=[k(s['ra3'])], w=[k(s['ra1'])])
        yield
        V(lambda e: e.tensor_scalar(s['ra1'][:], s['ra1'][:], -float(np.exp(-0.5)), None, ALU.mult), r=[k(s['ra1'])], w=[k(s['ra1'])])
        yield
        V(lambda e: e.tensor_tensor_scan(s['rcw'][:], c['rmask'][:], s['ra1'][:], 0.0, ALU.mult, ALU.add), r=[k(c['rmask']), k(s['ra1'])], w=[k(s['rcw'])])
        yield
        V(lambda e: e.tensor_tensor(s['ra2'][:], s['rcw'][:], s['ra1'][:], ALU.subtract), r=[k(s['rcw']), k(s['ra1'])], w=[k(s['ra2'])])
        yield
        A(lambda e: e.activation(out=s['recw'][:], in_=s['rcw'][:], func=AF.Exp), r=[k(s['rcw'])], w=[k(s['recw'])])
        yield
        A(lambda e: e.activation(out=s['reicw'][:], in_=s['rcw'][:], func=AF.Exp, scale=-1.0), r=[k(s['rcw'])], w=[k(s['reicw'])])
        yield
        A(lambda e: e.activation(out=s['recwp'][:], in_=s['ra2'][:], func=AF.Exp), r=[k(s['ra2'])], w=[k(s['recwp'])])
        yield
        ba_, ka_ = self.bank('r')
        for b in range(2):
            M(lambda e, b=b: e.matmul(ba_[:, b * 128:(b + 1) * 128], lhsT=p['a2p'][:, b * 128:(b + 1) * 128], rhs=lr, start=True, stop=True),
              r=[k(p['a2p']), ksh], w=[ka_])
        for b in range(2):
            A(lambda e, b=b: e.activation(out=s['ra'][:, b * 128:(b + 1) * 128], in_=ba_[:, b * 128:(b + 1) * 128], func=AF.Sigmoid, bias=p['a0'][:, b:b + 1]),
              r=[ka_, k(p['a0'])], w=[k(s['ra'])])
            yield
        A(lambda e: e.activation(out=s['rsg'][:], in_=lr, func=AF.Sigmoid), r=[ksh], w=[k(s['rsg'])])
        yield
        bg, kg = self.bank('r')
        M(lambda e: e.matmul(bg[:, 0:256], lhsT=s['rsg'][:], rhs=p['g2p'][:], start=True, stop=True), r=[k(s['rsg']), k(p['g2p'])], w=[kg])
        A(lambda e: e.copy(out=s['rg'][:], in_=bg[:, 0:256]), r=[kg], w=[k(s['rg'])])
        yield
        V(lambda e: e.tensor_tensor(s['rkk'][:].rearrange("p (a b) -> p a b", b=128), kT.rearrange("p (a b) -> p a b", b=128), self.bc(p['kk'][:, :], [128, 2, 128], 2), ALU.mult),
          r=[ksh, k(p['kk'])], w=[k(s['rkk'])])
        yield
        A(lambda e: e.activation(out=s['ra2'][:], in_=s['rkk'][:], func=AF.Square), r=[k(s['rkk'])], w=[k(s['ra2'])])
        yield
        bn, kn = self.bank('r')
        for b in range(2):
            M(lambda e, b=b: e.matmul(bn[:, b * 128:(b + 1) * 128], lhsT=c['bones'][:], rhs=s['ra2'][:, b * 128:(b + 1) * 128], start=True, stop=True),
              r=[k(c['bones']), k(s['ra2'])], w=[kn])
        V(lambda e: e.tensor_scalar(s['ra3'][:], bn[:, 0:256], 1e-12, None, ALU.add), r=[kn], w=[k(s['ra3'])])
        yield
        A(lambda e: e.activation(out=s['ra3'][:], in_=s['ra3'][:], func=AF.Sqrt), r=[k(s['ra3'])], w=[k(s['ra3'])])
        yield
        V(lambda e: e.reciprocal(s['ra3'][:], s['ra3'][:]), r=[k(s['ra3'])], w=[k(s['ra3'])])
        yield
        V(lambda e: e.tensor_tensor(s['rkk'][:], s['rkk'][:], s['ra3'][:], ALU.mult), r=[k(s['rkk']), k(s['ra3'])], w=[k(s['rkk'])])
        yield
        for b in range(2):
            V(lambda e, b=b: e.tensor_scalar(s['ra2'][:, b * 128:(b + 1) * 128], s['ra'][:, b * 128:(b + 1) * 128], p['ka'][:, b:b + 1], p['omka'][:, b:b + 1], ALU.mult, ALU.add),
              r=[k(s['ra']), k(p['ka']), k(p['omka'])], w=[k(s['ra2'])])
            yield
        V(lambda e: e.tensor_tensor(s['rkp'][:], kT, s['ra2'][:], ALU.mult), r=[ksh, k(s['ra2'])], w=[k(s['rkp'])])
        yield
        V(lambda e: e.tensor_tensor(s['ra3'][:], s['rkk'][:], s['ra'][:], ALU.mult), r=[k(s['rkk']), k(s['ra'])], w=[k(s['ra3'])])
        yield
        for hh in range(2):
            V(lambda e, hh=hh: e.scalar_tensor_tensor(s['rAm'][:, hh, :], s['rkk'][:], c['nhm'][:, hh:hh + 1], s['recwp'][:], ALU.mult, ALU.mult),
              r=[k(s['rkk']), k(c['nhm']), k(s['recwp'])], w=[k(s['rAm'])])
            yield
            V(lambda e, hh=hh: e.scalar_tensor_tensor(s['rRm'][:, hh, :], rT, c['hm'][:, hh:hh + 1], s['recw'][:], ALU.mult, ALU.mult),
              r=[ksh, k(c['hm']), k(s['recw'])], w=[k(s['rRm'])])
            yield
        G(lambda e: e.tensor_tensor(s['rBt'][:], s['ra3'][:], s['reicw'][:], ALU.mult), r=[k(s['ra3']), k(s['reicw'])], w=[k(s['rBt'])])
        yield
        G(lambda e: e.tensor_tensor(s['rKt'][:], s['rkp'][:], s['reicw'][:], ALU.mult), r=[k(s['rkp']), k(s['reicw'])], w=[k(s['rKt'])])
        yield
        V(lambda e: e.tensor_tensor(s['ra2'][:], rT, s['rkp'][:], ALU.mult), r=[ksh, k(s['rkp'])], w=[k(s['ra2'])])
        yield
        V(lambda e: e.tensor_tensor(s['ra2'][:].rearrange("p (a b) -> p a b", b=128), s['ra2'][:].rearrange("p (a b) -> p a b", b=128), self.bc(p['rk'][:, :], [128, 2, 128], 2), ALU.mult),
          r=[k(s['ra2']), k(p['rk'])], w=[k(s['ra2'])])
        yield
        bb, kb = self.bank('r')
        for b in range(2):
            M(lambda e, b=b: e.matmul(bb[:, b * 2:(b + 1) * 2], lhsT=s['ra2'][:, b * 128:(b + 1) * 128], rhs=c['hm'][:, :], start=True, stop=True),
              r=[k(s['ra2']), k(c['hm'])], w=[kb])
        A(lambda e: e.copy(out=s['rbc'][:], in_=bb[:, 0:4]), r=[kb], w=[k(s['rbc'])])
        yield
        bt, kt = self.bank('r')
        for b in range(2):
            M(lambda e, b=b: e.transpose(bt[:, b * 128:(b + 1) * 128], s['rsh'][:, 4 + b, :], c['identf'][:]), r=[ksh, k(c['identf'])], w=[kt])
        A(lambda e: e.copy(out=s['rvtm'][:], in_=bt[:, 0:256]), r=[kt], w=[k(s['rvtm'])])
        yield
        bt, kt = self.bank('r')
        for cc in range(2):
            for b in range(2):
                M(lambda e, bt=bt, cc=cc, b=b: e.transpose(bt[0:64, cc * 256 + b * 128:cc * 256 + (b + 1) * 128], s['rsh'][:, 4 + b, cc * 64:(cc + 1) * 64], c['identf'][:]),
                  r=[ksh, k(c['identf'])], w=[kt])
        A(lambda e, bt=bt: e.copy(out=s['rvc'][:].rearrange("p a b -> p (a b)"), in_=bt[0:64, :]), r=[kt], w=[k(s['rvc'])])
        yield
        for srct, dst in ((s['rBt'], s['rBc']), (s['rKt'], s['rKc'])):
            bt, kt = self.bank('r')
            pbt = bt[:].bitcast(BF16)
            for cc in range(2):
                for b in range(2):
                    M(lambda e, pbt=pbt, srct=srct, cc=cc, b=b: e.transpose(pbt[0:64, cc * 256 + b * 128:cc * 256 + (b + 1) * 128], srct[:, b * 128 + cc * 64:b * 128 + (cc + 1) * 64], c['identb'][:]),
                      r=[k(srct), k(c['identb'])], w=[kt])
            V(lambda e, pbt=pbt, dst=dst: e.tensor_copy(dst[:].rearrange("p a b -> p (a b)"), pbt[0:64, 0:512]), r=[kt], w=[k(dst)])
            yield
        def amat(lt, lkey, lhh, rt_, rkey, rhh, mask, dst):
            bA, kA = self.bank('r')
            for cc in range(2):
                for h in range(4):
                    b, hh = h // 2, h % 2
                    sl_ = slice(b * 128 + cc * 64, b * 128 + (cc + 1) * 64)
                    la = lt[:, hh, sl_] if lhh else lt[:, sl_]
                    ra_ = rt_[:, hh, sl_] if rhh else rt_[:, sl_]
                    i8 = cc * 4 + h
                    M(lambda e, bA=bA, la=la, ra_=ra_, i8=i8: e.matmul(bA[0:64, i8 * 64:(i8 + 1) * 64], lhsT=la, rhs=ra_, start=True, stop=True), r=[lkey, rkey], w=[kA])
            V(lambda e, bA=bA: e.tensor_tensor(dst[:], h3(bA[0:64, :]), self.bc(mask, [64, 8, 64], 1), ALU.mult), r=[kA, k(c['su'])], w=[k(dst)])
            yield
        kAm, kRm, kBt, kKt = k(s['rAm']), k(s['rRm']), k(s['rBt']), k(s['rKt'])
        yield from amat(s['rAm'], kAm, True, s['rBt'], kBt, False, c['su'][0:64, 0:64], s['rP'])
        yield from amat(s['rBt'], kBt, False, s['rAm'], kAm, True, c['sl'][0:64, 0:64], s['rQ'])
        yield from amat(s['rKt'], kKt, False, s['rAm'], kAm, True, c['sl'][0:64, 0:64], s['rAak'])
        yield from amat(s['rBt'], kBt, False, s['rRm'], kRm, True, c['tri'][0:64, 0:64], s['rArb'])
        yield from amat(s['rKt'], kKt, False, s['rRm'], kRm, True, c['tri'][0:64, 0:64], s['rArk'])
        V(lambda e: e.tensor_tensor(s['rTT'][:], s['rQ'][:], self.bc(c['identf'][0:64, 0:64], [64, 8, 64], 1), ALU.add), r=[k(s['rQ']), k(c['identf'])], w=[k(s['rTT'])])
        yield
        Pc, Qc, Pn, Qn = s['rP'], s['rQ'], s['rP2'], s['rQ2']
        for lvl in range(5):
            bP, kP = self.bank('r')
            for i8 in range(8):
                M(lambda e, bP=bP, i8=i8, Pc=Pc, Qc=Qc: e.matmul(bP[0:64, i8 * 64:(i8 + 1) * 64], lhsT=Qc[:, i8, :], rhs=Pc[:, i8, :], start=True, stop=True),
                  r=[k(Pc), k(Qc)], w=[kP])
            A(lambda e, bP=bP, Pn=Pn: e.copy(out=Pn[:], in_=h3(bP[0:64, :])), r=[kP], w=[k(Pn)])
            yield
            if lvl < 4:
                bQ, kQ = self.bank('r')
                for i8 in range(8):
                    M(lambda e, bQ=bQ, i8=i8, Pc=Pc, Qc=Qc: e.matmul(bQ[0:64, i8 * 64:(i8 + 1) * 64], lhsT=Pc[:, i8, :], rhs=Qc[:, i8, :], start=True, stop=True),
                      r=[k(Pc), k(Qc)], w=[kQ])
                V(lambda e, bQ=bQ, Qn=Qn: e.tensor_copy(Qn[:], h3(bQ[0:64, :])), r=[kQ], w=[k(Qn)])
                yield
            bT, kT_ = self.bank('r')
            for i8 in range(8):
                M(lambda e, bT=bT, i8=i8, Pn=Pn: e.matmul(bT[0:64, i8 * 64:(i8 + 1) * 64], lhsT=Pn[:, i8, :], rhs=s['rTT'][:, i8, :], start=True, stop=True),
                  r=[k(Pn), k(s['rTT'])], w=[kT_])
            V(lambda e, bT=bT: e.tensor_tensor(s['rTT'][:], s['rTT'][:], h3(bT[0:64, :]), ALU.add), r=[k(s['rTT']), kT_], w=[k(s['rTT'])])
            yield
            Pc, Qc, Pn, Qn = Pn, Qn, Pc, Qc
        bG, kG = self.bank('r')
        for cc in range(2):
            for h in range(4):
                i8 = cc * 4 + h
                M(lambda e, cc=cc, h=h, i8=i8: e.matmul(bG[0:64, i8 * 64:(i8 + 1) * 64], lhsT=s['rAak'][:, i8, :], rhs=s['rvc'][:, cc, h * 64:(h + 1) * 64], start=True, stop=True),
                  r=[k(s['rAak']), k(s['rvc'])], w=[kG])
        A(lambda e: e.copy(out=s['rAak'][:], in_=h3(bG[0:64, :])), r=[kG], w=[k(s['rAak'])])
        yield
        ewc = s['recw'][:].rearrange("p (a b) -> p a b", b=64)[:, :, 63]
        for cc in range(2):
            bG1, kG1 = self.bank('r')
            for h in range(4):
                b, hh = h // 2, h % 2
                sl_ = slice(b * 128 + cc * 64, b * 128 + (cc + 1) * 64)
                M(lambda e, bG1=bG1, h=h, b=b, hh=hh, sl_=sl_: e.matmul(bG1[0:64, h * 64:(h + 1) * 64], lhsT=s['rAm'][:, hh, sl_], rhs=s['rSTb'][:, b, :], start=True, stop=True),
                  r=[kAm, k(s['rSTb'])], w=[kG1])
            bY1, kY1 = self.bank('r')
            for h in range(4):
                b, hh = h // 2, h % 2
                sl_ = slice(b * 128 + cc * 64, b * 128 + (cc + 1) * 64)
                M(lambda e, bY1=bY1, h=h, b=b, hh=hh, sl_=sl_, cc=cc: e.matmul(bY1[cc * 64:(cc + 1) * 64, h * 64:(h + 1) * 64], lhsT=s['rRm'][:, hh, sl_], rhs=s['rSTb'][:, b, :], start=True, stop=True),
                  r=[kRm, k(s['rSTb'])], w=[kY1])
            A(lambda e, bY1=bY1, cc=cc: e.copy(out=s['rY1'][cc * 64:(cc + 1) * 64, :], in_=bY1[cc * 64:(cc + 1) * 64, 0:256]), r=[kY1], w=[k(s['rY1'])])
            yield
            V(lambda e, bG1=bG1, cc=cc: e.tensor_tensor(s['rG'][:], s['rAak'][:, cc * 4:(cc + 1) * 4, :], h3(bG1[0:64, 0:256]), ALU.add), r=[k(s['rAak']), kG1], w=[k(s['rG'])])
            yield
            bU, kU = self.bank('r')
            for h in range(4):
                i8 = cc * 4 + h
                M(lambda e, bU=bU, h=h, i8=i8: e.matmul(bU[0:64, h * 64:(h + 1) * 64], lhsT=s['rTT'][:, i8, :], rhs=s['rG'][:, h, :], start=True, stop=True),
                  r=[k(s['rTT']), k(s['rG'])], w=[kU])
            A(lambda e, bU=bU: e.copy(out=s['rU'][:], in_=h3(bU[0:64, 0:256])), r=[kU], w=[k(s['rU'])])
            yield
            bY2, kY2 = self.bank('r')
            for h in range(4):
                i8 = cc * 4 + h
                M(lambda e, bY2=bY2, h=h, i8=i8, cc=cc: e.matmul(bY2[cc * 64:(cc + 1) * 64, h * 64:(h + 1) * 64], lhsT=s['rArb'][:, i8, :], rhs=s['rU'][:, h, :], start=True, stop=False),
                  r=[k(s['rArb']), k(s['rU'])], w=[kY2])
                M(lambda e, bY2=bY2, h=h, i8=i8, cc=cc: e.matmul(bY2[cc * 64:(cc + 1) * 64, h * 64:(h + 1) * 64], lhsT=s['rArk'][:, i8, :], rhs=s['rvc'][:, cc, h * 64:(h + 1) * 64], start=False, stop=True),
                  r=[k(s['rArk']), k(s['rvc'])], w=[kY2])
            V(lambda e, bY2=bY2, cc=cc: e.tensor_tensor(s['rY'][cc * 64:(cc + 1) * 64, :], s['rY1'][cc * 64:(cc + 1) * 64, :], bY2[cc * 64:(cc + 1) * 64, 0:256], ALU.add),
              r=[k(s['rY1']), kY2], w=[k(s['rY'])])
            yield
            bS, kS = self.bank('r')
            for h in range(4):
                b, hh = h // 2, h % 2
                i8 = cc * 4 + h
                M(lambda e, bS=bS, h=h, b=b, hh=hh, cc=cc: e.matmul(bS[hh * 64:(hh + 1) * 64, b * 64:(b + 1) * 64], lhsT=s['rBc'][:, cc, h * 64:(h + 1) * 64], rhs=s['rU'][:, h, :], start=True, stop=False),
                  r=[k(s['rBc']), k(s['rU'])], w=[kS])
                M(lambda e, bS=bS, h=h, b=b, hh=hh, cc=cc: e.matmul(bS[hh * 64:(hh + 1) * 64, b * 64:(b + 1) * 64], lhsT=s['rKc'][:, cc, h * 64:(h + 1) * 64], rhs=s['rvc'][:, cc, h * 64:(h + 1) * 64], start=False, stop=True),
                  r=[k(s['rKc']), k(s['rvc'])], w=[kS])
            V(lambda e, bS=bS: e.tensor_tensor(s['rt2'][:], s['rST'][:], h3(bS[:, 0:128]), ALU.add), r=[k(s['rST']), kS], w=[k(s['rt2'])])
            yield
            V(lambda e, cc=cc: e.tensor_tensor(s['rST'][:], s['rt2'][:], self.bc(ewc[:, cc::2], [128, 2, 64], 2), ALU.mult), r=[k(s['rt2']), k(s['recw'])], w=[k(s['rST'])])
            yield
            A(lambda e: e.copy(out=s['rSTb'][:], in_=s['rST'][:]), r=[k(s['rST'])], w=[k(s['rSTb'])])
            yield
        V(lambda e: e.tensor_reduce(s['rm1'][:], h3(s['rY'][:]), AX.X, ALU.add), r=[k(s['rY'])], w=[k(s['rm1'])])
        yield
        A(lambda e: e.activation(out=s['rY1'][:], in_=s['rY'][:], func=AF.Square), r=[k(s['rY'])], w=[k(s['rY1'])])
        yield
        V(lambda e: e.tensor_reduce(s['rm2'][:], h3(s['rY1'][:]), AX.X, ALU.add), r=[k(s['rY1'])], w=[k(s['rm2'])])
        yield
        V(lambda e: e.tensor_scalar(s['rm1'][:], s['rm1'][:], 1.0 / 64, None, ALU.mult), r=[k(s['rm1'])], w=[k(s['rm1'])])
        yield
        V(lambda e: e.tensor_tensor(s['rvar'][:], s['rm1'][:], s['rm1'][:], ALU.mult), r=[k(s['rm1'])], w=[k(s['rvar'])])
        yield
        V(lambda e: e.scalar_tensor_tensor(s['rvar'][:], s['rm2'][:], 1.0 / 64, s['rvar'][:], ALU.mult, ALU.subtract), r=[k(s['rm2']), k(s['rvar'])], w=[k(s['rvar'])])
        yield
        V(lambda e: e.tensor_scalar(s['rvar'][:], s['rvar'][:], GN_EPS, None, ALU.add), r=[k(s['rvar'])], w=[k(s['rvar'])])
        yield
        A(lambda e: e.activation(out=s['rvar'][:], in_=s['rvar'][:], func=AF.Sqrt), r=[k(s['rvar'])], w=[k(s['rvar'])])
        yield
        V(lambda e: e.reciprocal(s['rvar'][:], s['rvar'][:]), r=[k(s['rvar'])], w=[k(s['rvar'])])
        yield
        V(lambda e: e.tensor_tensor(h3(s['rY'][:]), h3(s['rY'][:]), self.bc(s['rm1'][:, :], [128, 4, 64], 2), ALU.subtract), r=[k(s['rY']), k(s['rm1'])], w=[k(s['rY'])])
        yield
        V(lambda e: e.tensor_tensor(h3(s['rY'][:]), h3(s['rY'][:]), self.bc(s['rvar'][:, :], [128, 4, 64], 2), ALU.mult), r=[k(s['rY']), k(s['rvar'])], w=[k(s['rY'])])
        yield
        G(lambda e: e.tensor_tensor(s['rY'][:], s['rY'][:], p['rlg'][:], ALU.mult), r=[k(s['rY']), k(p['rlg'])], w=[k(s['rY'])])
        yield
        G(lambda e: e.tensor_tensor(s['rY'][:], s['rY'][:], p['rlb'][:], ALU.add), r=[k(s['rY']), k(p['rlb'])], w=[k(s['rY'])])
        yield
        V(lambda e: e.tensor_tensor(h3(s['rY1'][:]), h3(s['rvtm'][:]), self.bc(s['rbc'][:, :], [128, 4, 64], 2), ALU.mult), r=[k(s['rvtm']), k(s['rbc'])], w=[k(s['rY1'])])
        yield
        V(lambda e: e.tensor_tensor(s['rY'][:], s['rY'][:], s['rY1'][:], ALU.add), r=[k(s['rY']), k(s['rY1'])], w=[k(s['rY'])])
        yield
        V(lambda e: e.tensor_tensor(s['ycat'][:, 512:768], s['rY'][:], s['rg'][:], ALU.mult), r=[k(s['rY']), k(s['rg'])], w=[k(s['ycat'])])
        yield

    def mixer_epilogue(self, l, i):
        s, p, c = self.s, self.p, self.c
        V, A, G, M = self.V, self.A, self.G, self.M
        k = lambda t: t.key
        self.load('sp', s['htm'][:], self.h_d[i * 128:(i + 1) * 128, :], k(s['htm']), dkeys=["hd_%d" % i])
        if self.debug:
            self.A(lambda e: e.copy(out=s['tmp'][:], in_=s['ycat'][:]), r=[k(s['ycat'])], w=[k(s['tmp'])])
            yield
            self.store('sp', self.dbg_y[i * 128:(i + 1) * 128, :], s['tmp'][:], k(s['tmp']))
            yield
        bt, kt = self.bank('e')
        pb = bt[:].bitcast(BF16)
        for kc in range(8):
            M(lambda e, kc=kc: e.transpose(pb[:, kc * 128:(kc + 1) * 128], s['ycat'][:, kc * 128:(kc + 1) * 128], c['identb'][:]), r=[k(s['ycat']), k(c['identb'])], w=[kt])
        V(lambda e: e.tensor_copy(s['yT'][:].rearrange("p a b -> p (a b)"), pb), r=[kt], w=[k(s['yT'])])
        yield
        for half in range(2):
            bo, ko = self.bank('e')
            for kc in range(8):
                M(lambda e, bo=bo, kc=kc, half=half: e.matmul(bo[:, :], lhsT=s['yT'][:, kc, :], rhs=p['w_out'][:, kc, half * 512:(half + 1) * 512], start=(kc == 0), stop=(kc == 7)),
                  r=[k(s['yT']), k(p['w_out'])], w=[ko])
            V(lambda e, bo=bo, half=half: e.scalar_tensor_tensor(s['mix'][:, half * 512:(half + 1) * 512], s['htm'][:, half * 512:(half + 1) * 512], ALPHA, bo[:, :], ALU.mult, ALU.add),
              r=[k(s['htm']), ko], w=[k(s['mix'])])
            yield
        yield from self.layernorm(s['mix'], p['l1g'], p['l1b'], s['h1'], s['tmp'])
        tk = "h1d_%d_%d" % (l, i)
        self.store('sp', self.h1_d[i * 128:(i + 1) * 128, :], s['h1'][:], k(s['h1']), dkeys=[tk])
        yield
        self.store('pool', self.h1b_d[i * 128:(i + 1) * 128, :], s['h1'][:], k(s['h1']), dkeys=["h1bd_%d_%d" % (l, i)])
        yield
        if hasattr(self, 'xs_d'):
            yield from self.router_tile(l, i)

    def stage0(self):
        s, p, d = self.s, self.p, self.d
        k = lambda t: t.key
        mark = self.sb_off
        self.load('sp', p['l1g'][:], d['ln_in_g'].ap().partition_broadcast(128), k(p['l1g']))
        self.load('sp', p['l1b'][:], d['ln_in_b'].ap().partition_broadcast(128), k(p['l1b']))
        sets = [dict(x=s['mix'], h=s['htm'], tmp=s['tmp'], hb=s['ycat'], hT=s['hT'], st=self.ln_stats("a"))]
        hb2 = Tile(s['yT'].t.ap().rearrange("p a b -> p (a b)") if False else s['yT'].t, s['yT'].key)
        sets.append(dict(x=s['h1'], h=self.sb([128, D], name="s0_h"), tmp=self.sb([128, D], name="s0_tmp"),
                         hb=hb2, hT=self.sb([128, 8, 128], BF16, "s0_hT"), st=self.ln_stats("b")))

        def body(i):
            B = sets[i % 2]
            self.load('sp', B['x'][:], d['x'][i * 128:(i + 1) * 128, :], k(B['x']))
            yield
            yield from self.layernorm(B['x'], p['l1g'], p['l1b'], B['h'], B['tmp'], B['st'])
            self.store('sp', self.h_d[i * 128:(i + 1) * 128, :], B['h'][:], k(B['h']), dkeys=["hd_%d" % i])
            yield
            hbf = B['hb'][:] if len(B['hb'][:].shape) == 2 else B['hb'][:].rearrange("p a b -> p (a b)")
            self.A(lambda e: e.copy(out=hbf, in_=B['h'][:]), r=[k(B['h'])], w=[k(B['hb'])])
            yield
            bk, bkey = self.bank()
            pb = bk[:].bitcast(BF16)
            for kc in range(8):
                self.M(lambda e, kc=kc: e.transpose(pb[:, kc * 128:(kc + 1) * 128], hbf[:, kc * 128:(kc + 1) * 128], self.c['identb'][:]),
                       r=[k(B['hb']), k(self.c['identb'])], w=[bkey])
            self.V(lambda e: e.tensor_copy(B['hT'][:].rearrange("p a b -> p (a b)"), pb), r=[bkey], w=[k(B['hT'])])
            yield
            self.store('sp', self.hT_d[:, :, i * 128:(i + 1) * 128], B['hT'][:], k(B['hT']), dkeys=["hTd_%d" % i])
            yield
        self.run_pipe([lambda i=i: body(i) for i in range(self.NT)], 2)
        self.sb_off = mark

    def stageM(self, l):
        import os
        s = self.s
        k = lambda t: t.key
        only = os.environ.get("ONLY", "srg")
        prev = None
        for i in range(self.NT + 1):
            gens = []
            if i < self.NT:
                self.load('sp', s['hT'][:], self.hT_d[:, :, i * 128:(i + 1) * 128], k(s['hT']), dkeys=["hTd_%d" % i])
                if 'r' in only:
                    gens.append(self.rwkv_tile(i == 0))
                if 's' in only:
                    gens.append(self.ssd_tile(i == 0))
                if 'g' in only:
                    gens.append(self.gla_tile(i == 0))
            if prev is not None:
                gens.append(self.mixer_epilogue(l, prev))
            prev = i if i < self.NT else None
            wts = [int(x) for x in os.environ.get("ILW", "3,1,1,1").split(",")]
            gw = {id(g_): (wts[0] if j == 0 and i < self.NT and 'r' in only else 1) for j, g_ in enumerate(gens)}
            while gens:
                for g_ in list(gens):
                    for _ in range(gw[id(g_)]):
                        try:
                            next(g_)
                        except StopIteration:
                            gens.remove(g_)
                            break

    def build_mixer_test(self):
        self.declare_inputs()
        T = self.T
        self.h_d = self.dscr("h_d", [T, D])
        self.hT_d = self.dscr("hT_d", [128, 8, T], BF16)
        self.h1_d = self.dout("h1_d", [T, D])
        self.h1b_d = self.dscr("h1b_d", [T, D], BF16)
        self.dbg_y = self.dout("dbg_y", [T, D])
        self.consts()
        self.alloc_params()
        self.alloc_mixer()
        print("sbuf peak", self.sb_peak)
        self.stage0()
        self.P.barrier()
        self.load_params(0)
        self.stageM(0)
        self.P.barrier()
        return self.nc

    def alloc_router(self):
        rt = self.rt = {}
        for n, w in (('lg', 36), ('gmx', 1), ('goh', 4), ('gex', 4), ('gsum', 1), ('t32', 32), ('el8', 8), ('el8m', 8), ('l1', 1), ('l2', 1),
                     ('oh1', 8), ('oh2', 8), ('w1', 1), ('w2', 1), ('E1', 32), ('E2', 32), ('Mm', 32), ('rk', 32)):
            rt[n] = self.sb([128, w], name="rt_" + n)

    def alloc_route_persist(self):
        rp = self.rp = {}
        NT = self.NT
        rp['eid'] = self.sb([128, NT * 2], name="rp_eid")
        rp['rnk'] = self.sb([128, NT * 2], name="rp_rnk")
        rp['gat'] = self.sb([128, NT * 2], name="rp_gat")
        rp['cnt'] = self.sb([128, NE], name="rp_cnt")
        rp['iota32'] = self.sb([128, NE], name="rp_iota32")
        ii = self.sb([128, NE], I32, "rp_iota32i")
        self.G(lambda e: e.iota(ii[:], pattern=[[1, NE]], base=0, channel_multiplier=0), w=[ii.key])
        self.V(lambda e: e.tensor_copy(rp['iota32'][:], ii[:]), r=[ii.key], w=[rp['iota32'].key])

    def router_tile(self, l, i):
        s, p, c = self.s, self.p, self.c
        V, A, G, M = self.V, self.A, self.G, self.M
        k = lambda t: t.key
        rt, rp = self.rt, self.rp
        if i == 0:
            G(lambda e: e.memset(rp['cnt'][:], 0.0), w=[k(rp['cnt'])])
            yield
        hT32 = s['tmp'][:].rearrange("p (a b) -> p a b", b=128)
        for half in range(2):
            bt, kt = self.bank('e')
            for j in range(4):
                kc = half * 4 + j
                M(lambda e, bt=bt, j=j, kc=kc: e.transpose(bt[:, j * 128:(j + 1) * 128], s['h1'][:, kc * 128:(kc + 1) * 128], c['identf'][:]), r=[k(s['h1']), k(c['identf'])], w=[kt])
            A(lambda e, bt=bt, half=half: e.copy(out=s['tmp'][:, half * 512:(half + 1) * 512], in_=bt[:, :]), r=[kt], w=[k(s['tmp'])])
            yield
        bl, kl = self.bank('e')
        for kc in range(8):
            M(lambda e, kc=kc: e.matmul(bl[:, 0:36], lhsT=hT32[:, kc, :], rhs=p['wr'][:, kc, :], start=(kc == 0), stop=(kc == 7)), r=[k(s['tmp']), k(p['wr'])], w=[kl])
        V(lambda e: e.tensor_tensor(rt['lg'][:], bl[:, 0:36], p['rb36'][:], ALU.add), r=[kl, k(p['rb36'])], w=[k(rt['lg'])])
        yield
        V(lambda e: e.tensor_reduce(rt['gmx'][:], rt['lg'][:, 0:4], AX.X, ALU.max), r=[k(rt['lg'])], w=[k(rt['gmx'])])
        yield
        V(lambda e: e.tensor_scalar(rt['goh'][:], rt['lg'][:, 0:4], rt['gmx'][:, 0:1], None, ALU.is_equal), r=[k(rt['lg']), k(rt['gmx'])], w=[k(rt['goh'])])
        yield
        V(lambda e: e.tensor_scalar(rt['gex'][:], rt['lg'][:, 0:4], rt['gmx'][:, 0:1], None, ALU.subtract), r=[k(rt['lg']), k(rt['gmx'])], w=[k(rt['gex'])])
        yield
        A(lambda e: e.activation(out=rt['gex'][:], in_=rt['gex'][:], func=AF.Exp), r=[k(rt['gex'])], w=[k(rt['gex'])])
        yield
        V(lambda e: e.tensor_reduce(rt['gsum'][:], rt['gex'][:], AX.X, ALU.add), r=[k(rt['gex'])], w=[k(rt['gsum'])])
        yield
        V(lambda e: e.reciprocal(rt['gsum'][:], rt['gsum'][:]), r=[k(rt['gsum'])], w=[k(rt['gsum'])])
        yield
        V(lambda e: e.tensor_tensor(rt['t32'][:].rearrange("p (g j) -> p g j", j=8), rt['lg'][:, 4:36].rearrange("p (g j) -> p g j", j=8),
                                    self.bc(rt['goh'][:, :], [128, 4, 8], 2), ALU.mult), r=[k(rt['lg']), k(rt['goh'])], w=[k(rt['t32'])])
        yield
        V(lambda e: e.tensor_reduce(rt['el8'][:], rt['t32'][:].rearrange("p (g j) -> p j g", j=8), AX.X, ALU.add), r=[k(rt['t32'])], w=[k(rt['el8'])])
        yield
        V(lambda e: e.tensor_reduce(rt['l1'][:], rt['el8'][:], AX.X, ALU.max), r=[k(rt['el8'])], w=[k(rt['l1'])])
        yield
        V(lambda e: e.tensor_scalar(rt['oh1'][:], rt['el8'][:], rt['l1'][:, 0:1], None, ALU.is_equal), r=[k(rt['el8']), k(rt['l1'])], w=[k(rt['oh1'])])
        yield
        V(lambda e: e.scalar_tensor_tensor(rt['el8m'][:], rt['oh1'][:], -1e30, rt['el8'][:], ALU.mult, ALU.add), r=[k(rt['oh1']), k(rt['el8'])], w=[k(rt['el8m'])])
        yield
        V(lambda e: e.tensor_reduce(rt['l2'][:], rt['el8m'][:], AX.X, ALU.max), r=[k(rt['el8m'])], w=[k(rt['l2'])])
        yield
        V(lambda e: e.tensor_scalar(rt['oh2'][:], rt['el8m'][:], rt['l2'][:, 0:1], None, ALU.is_equal), r=[k(rt['el8m']), k(rt['l2'])], w=[k(rt['oh2'])])
        yield
        V(lambda e: e.tensor_tensor(rt['w2'][:], rt['l2'][:], rt['l1'][:], ALU.subtract), r=[k(rt['l2']), k(rt['l1'])], w=[k(rt['w2'])])
        yield
        A(lambda e: e.activation(out=rt['w2'][:], in_=rt['w2'][:], func=AF.Exp), r=[k(rt['w2'])], w=[k(rt['w2'])])
        yield
        V(lambda e: e.tensor_scalar(rt['w1'][:], rt['w2'][:], 1.0, None, ALU.add), r=[k(rt['w2'])], w=[k(rt['w1'])])
        yield
        V(lambda e: e.reciprocal(rt['w1'][:], rt['w1'][:]), r=[k(rt['w1'])], w=[k(rt['w1'])])
        yield
        V(lambda e: e.tensor_tensor(rt['w2'][:], rt['w2'][:], rt['w1'][:], ALU.mult), r=[k(rt['w2']), k(rt['w1'])], w=[k(rt['w2'])])
        yield
        V(lambda e: e.tensor_tensor(rp['gat'][:, 2 * i:2 * i + 1], rt['w1'][:], rt['gsum'][:], ALU.mult), r=[k(rt['w1']), k(rt['gsum'])], w=[k(rp['gat'])])
        yield
        V(lambda e: e.tensor_tensor(rp['gat'][:, 2 * i + 1:2 * i + 2], rt['w2'][:], rt['gsum'][:], ALU.mult), r=[k(rt['w2']), k(rt['gsum'])], w=[k(rp['gat'])])
        yield
        for E, oh in ((rt['E1'], rt['oh1']), (rt['E2'], rt['oh2'])):
            V(lambda e, E=E, oh=oh: e.tensor_tensor(E[:].rearrange("p (g j) -> p g j", j=8), self.bc(rt['goh'][:, :], [128, 4, 8], 2), self.bc(oh[:, :], [128, 4, 8], 1), ALU.mult),
              r=[k(rt['goh']), k(oh)], w=[k(E)])
            yield
        V(lambda e: e.tensor_tensor(rt['Mm'][:], rt['E1'][:], rt['E2'][:], ALU.add), r=[k(rt['E1']), k(rt['E2'])], w=[k(rt['Mm'])])
        yield
        br, kr = self.bank('e')
        M(lambda e: e.matmul(br[:, 0:32], lhsT=c['sl'][:], rhs=rt['Mm'][:], start=True, stop=True), r=[k(c['sl']), k(rt['Mm'])], w=[kr])
        M(lambda e: e.matmul(br[:, 32:64], lhsT=c['onesf'][:], rhs=rt['Mm'][:], start=True, stop=True), r=[k(c['onesf']), k(rt['Mm'])], w=[kr])
        V(lambda e: e.tensor_tensor(rt['rk'][:], br[:, 0:32], rp['cnt'][:], ALU.add), r=[kr, k(rp['cnt'])], w=[k(rt['rk'])])
        yield
        V(lambda e: e.tensor_tensor(rp['cnt'][:], rp['cnt'][:], br[:, 32:64], ALU.add), r=[kr, k(rp['cnt'])], w=[k(rp['cnt'])])
        yield
        for j, E in ((0, rt['E1']), (1, rt['E2'])):
            V(lambda e, E=E: e.tensor_tensor(rt['t32'][:], E[:], rt['rk'][:], ALU.mult), r=[k(E), k(rt['rk'])], w=[k(rt['t32'])])
            yield
            V(lambda e, j=j: e.tensor_reduce(rp['rnk'][:, 2 * i + j:2 * i + j + 1], rt['t32'][:], AX.X, ALU.add), r=[k(rt['t32'])], w=[k(rp['rnk'])])
            yield
            V(lambda e, E=E: e.tensor_tensor(rt['t32'][:], E[:], rp['iota32'][:], ALU.mult), r=[k(E), k(rp['iota32'])], w=[k(rt['t32'])])
            yield
            V(lambda e, j=j: e.tensor_reduce(rp['eid'][:, 2 * i + j:2 * i + j + 1], rt['t32'][:], AX.X, ALU.add), r=[k(rt['t32'])], w=[k(rp['eid'])])
            yield

    def stageMoE(self, l, last):
        d, c, rp = self.d, self.c, self.rp
        V, A, G, M = self.V, self.A, self.G, self.M
        k = lambda t: t.key
        mark = self.sb_off
        sb = self.sb
        NT, NB, RB = self.NT, self.NB, self.RB
        NR = RB // 128
        NC = NT * 2
        thr_i = sb([128, 64], I32, "f_thri")
        thr = sb([128, 64], name="f_thr")
        G(lambda e: e.iota(thr_i[:], pattern=[[RB, 64]], base=0, channel_multiplier=0), w=[k(thr_i)])
        V(lambda e: e.tensor_copy(thr[:], thr_i[:]), r=[k(thr_i)], w=[k(thr)])
        big = sb([128, max(NC * NE, NE * 64, NB * NE)], name="f_big")
        nblk = sb([128, NE], name="f_nblk")
        padded = sb([128, NE], name="f_padded")
        pend = sb([128, NE], name="f_pend")
        pstart = sb([128, NE], name="f_pstart")
        cmp3 = big[:, 0:NE * 64].rearrange("p (e m) -> p e m", m=64)
        V(lambda e: e.tensor_tensor(cmp3, self.bc(rp['cnt'][:, :], [128, NE, 64], 2), self.bc(thr[:, :], [128, NE, 64], 1), ALU.is_gt), r=[k(rp['cnt']), k(thr)], w=[k(big)])
        V(lambda e: e.tensor_reduce(nblk[:], cmp3, AX.X, ALU.add), r=[k(big)], w=[k(nblk)])
        V(lambda e: e.tensor_scalar(padded[:], nblk[:], float(RB), None, ALU.mult), r=[k(nblk)], w=[k(padded)])
        V(lambda e: e.tensor_tensor_scan(pend[:], c['onesf'][:, 0:NE], padded[:], 0.0, ALU.mult, ALU.add), r=[k(c['onesf']), k(padded)], w=[k(pend)])
        V(lambda e: e.tensor_tensor(pstart[:], pend[:], padded[:], ALU.subtract), r=[k(pend), k(padded)], w=[k(pstart)])
        oh3 = big[:, 0:NC * NE].rearrange("p (n e) -> p n e", e=NE)
        destf = sb([128, NC], name="f_destf")
        dest = sb([128, NC], I32, "f_dest")
        V(lambda e: e.tensor_tensor(oh3, self.bc(rp['iota32'][:, :], [128, NC, NE], 1), self.bc(rp['eid'][:, :], [128, NC, NE], 2), ALU.is_equal), r=[k(rp['iota32']), k(rp['eid'])], w=[k(big)])
        V(lambda e: e.tensor_tensor(oh3, oh3, self.bc(pstart[:, :], [128, NC, NE], 1), ALU.mult), r=[k(big), k(pstart)], w=[k(big)])
        V(lambda e: e.tensor_reduce(destf[:], oh3, AX.X, ALU.add), r=[k(big)], w=[k(destf)])
        V(lambda e: e.tensor_tensor(destf[:], destf[:], rp['rnk'][:], ALU.add), r=[k(destf), k(rp['rnk'])], w=[k(destf)])
        V(lambda e: e.tensor_copy(dest[:], destf[:]), r=[k(destf)], w=[k(dest)])
        bs_i = sb([128, NB], I32, "f_bsi")
        bstart = sb([128, NB], name="f_bstart")
        be = sb([128, NB], name="f_be")
        G(lambda e: e.iota(bs_i[:], pattern=[[RB, NB]], base=0, channel_multiplier=0), w=[k(bs_i)])
        V(lambda e: e.tensor_copy(bstart[:], bs_i[:]), r=[k(bs_i)], w=[k(bstart)])
        cmpb = big[:, 0:NB * NE].rearrange("p (b e) -> p b e", e=NE)
        V(lambda e: e.tensor_tensor(cmpb, self.bc(pend[:, :], [128, NB, NE], 1), self.bc(bstart[:, :], [128, NB, NE], 2), ALU.is_le), r=[k(pend), k(bstart)], w=[k(big)])
        V(lambda e: e.tensor_reduce(be[:], cmpb, AX.X, ALU.add), r=[k(big)], w=[k(be)])
        V(lambda e: e.tensor_scalar(be[:], be[:], float(NE - 1), None, ALU.min), r=[k(be)], w=[k(be)])
        kp_i = sb([128, 8], I32, "f_kpi")
        kp = sb([128, 8], name="f_kp")
        G(lambda e: e.iota(kp_i[:], pattern=[[128, 8]], base=0, channel_multiplier=1), w=[k(kp_i)])
        V(lambda e: e.tensor_copy(kp[:], kp_i[:]), r=[k(kp_i)], w=[k(kp)])
        widf = sb([128, NB, 8], name="f_widf")
        wid = sb([128, NB, 8], I32, "f_wid")
        didf = sb([128, NB, 4], name="f_didf")
        did = sb([128, NB, 4], I32, "f_did")
        bew = sb([128, NB], name="f_bew")
        inval = sb([128, NB], name="f_inval")
        V(lambda e: e.tensor_scalar(inval[:], bstart[:], pend[:, NE - 1:NE], 1.0e9, ALU.is_ge, ALU.mult), r=[k(bstart), k(pend)], w=[k(inval)])
        V(lambda e: e.tensor_scalar(bew[:], be[:], float(D), float(l * NE * D), ALU.mult, ALU.add), r=[k(be)], w=[k(bew)])
        V(lambda e: e.tensor_tensor(bew[:], bew[:], inval[:], ALU.add), r=[k(bew), k(inval)], w=[k(bew)])
        V(lambda e: e.tensor_tensor(widf[:], self.bc(bew[:, :], [128, NB, 8], 2), self.bc(kp[:, :], [128, NB, 8], 1), ALU.add), r=[k(bew), k(kp)], w=[k(widf)])
        V(lambda e: e.tensor_copy(wid[:], widf[:]), r=[k(widf)], w=[k(wid)])
        V(lambda e: e.tensor_scalar(bew[:], be[:], float(FF), float(l * NE * FF), ALU.mult, ALU.add), r=[k(be)], w=[k(bew)])
        V(lambda e: e.tensor_tensor(bew[:], bew[:], inval[:], ALU.add), r=[k(bew), k(inval)], w=[k(bew)])
        V(lambda e: e.tensor_tensor(didf[:], self.bc(bew[:, :], [128, NB, 4], 2), self.bc(kp[:, 0:4], [128, NB, 4], 1), ALU.add), r=[k(bew), k(kp)], w=[k(didf)])
        V(lambda e: e.tensor_copy(did[:], didf[:]), r=[k(didf)], w=[k(did)])
        hbs = [sb([128, D], BF16, "e_hb%d" % j) for j in range(2)]
        for i in range(NT):
            hb = hbs[i % 2]
            self.load('sp', hb[:], self.h1b_d[i * 128:(i + 1) * 128, :], k(hb), dkeys=["h1bd_%d_%d" % (l, i)])
            for j in range(2):
                col = 2 * i + j
                self.P.dma('pool', lambda e, col=col, hb=hb: e.indirect_dma_start(out=self.xs_d.ap(), out_offset=bass.IndirectOffsetOnAxis(ap=dest[:, col:col + 1], axis=0),
                                                                                  in_=hb[:], in_offset=None), k(hb), r=[k(hb), k(dest)], w=["xs_d"])
        self.P.barrier()
        import os
        mstop = int(os.environ.get('MOESTOP', '9'))
        if mstop <= 1:
            self.sb_off = mark
            return
        wg = [sb([128, 8, FF], BF16, "e_wg%d" % j) for j in range(2)]
        wu = [sb([128, 8, FF], BF16, "e_wu%d" % j) for j in range(2)]
        wd = [sb([128, 4, D], BF16, "e_wd%d" % j) for j in range(2)]
        xs = [sb([128, NR, D], BF16, "e_xs%d" % j) for j in range(2)]
        xsT = sb([128, 8, RB], BF16, "e_xsT")
        hT = sb([128, 4, RB], BF16, "e_hT")
        sg = sb([128, RB], name="e_sg")
        ys = [sb([128, D], name="e_ys%d" % j) for j in range(2)]
        tg, tu, td = d['moe_w_gate'].ap(), d['moe_w_up'].ap(), d['moe_w_down'].ap()
        if not hasattr(self, '_bc_regs'):
            self._bc_regs = (self.nc.gpsimd.to_reg(DEPTH * NE * D - 1), self.nc.gpsimd.to_reg(DEPTH * NE * FF - 1))
        bc_g, bc_d = self._bc_regs
        nys = 0
        for b in range(NB):
            j = b % 2
            for kc in range(8):
                self.P.dma('pool', lambda e, j=j, b=b, kc=kc: e.indirect_dma_start(out=wg[j][:, kc, :], out_offset=None, in_=tg,
                                                                                  in_offset=bass.IndirectOffsetOnAxis(ap=wid[:, b, kc:kc + 1], axis=0), bounds_check=bc_g, oob_is_err=False), k(wg[j]), r=[k(wid)], w=[k(wg[j])])
                self.P.dma('pool', lambda e, j=j, b=b, kc=kc: e.indirect_dma_start(out=wu[j][:, kc, :], out_offset=None, in_=tu,
                                                                                  in_offset=bass.IndirectOffsetOnAxis(ap=wid[:, b, kc:kc + 1], axis=0), bounds_check=bc_g, oob_is_err=False), k(wu[j]), r=[k(wid)], w=[k(wu[j])])
            for fc in range(4):
                self.P.dma('pool', lambda e, j=j, b=b, fc=fc: e.indirect_dma_start(out=wd[j][:, fc, :], out_offset=None, in_=td,
                                                                                  in_offset=bass.IndirectOffsetOnAxis(ap=did[:, b, fc:fc + 1], axis=0), bounds_check=bc_d, oob_is_err=False), k(wd[j]), r=[k(did)], w=[k(wd[j])])
            self.load('sp', xs[j][:], self.xs_d[b * RB:(b + 1) * RB, :].rearrange("(r p) n -> p r n", p=128), k(xs[j]), dkeys=["xs_d"])
            for r_ in range(NR):
                bt, kt = self.bank()
                pb = bt[:].bitcast(BF16)
                for kc in range(8):
                    M(lambda e, pb=pb, j=j, r_=r_, kc=kc: e.transpose(pb[:, kc * 128:(kc + 1) * 128], xs[j][:, r_, kc * 128:(kc + 1) * 128], c['identb'][:]), r=[k(xs[j]), k(c['identb'])], w=[kt])
                V(lambda e, pb=pb, r_=r_: e.tensor_copy(xsT[:, :, r_ * 128:(r_ + 1) * 128], pb.rearrange("p (a b) -> p a b", b=128)), r=[kt], w=[k(xsT)])
            for fc in range(4):
                bg, kg = self.bank()
                for kc in range(8):
                    M(lambda e, bg=bg, kc=kc, fc=fc, j=j: e.matmul(bg[:, 0:RB], lhsT=wg[j][:, kc, fc * 128:(fc + 1) * 128], rhs=xsT[:, kc, :], start=(kc == 0), stop=(kc == 7)),
                      r=[k(wg[j]), k(xsT)], w=[kg])
                bu, ku = self.bank()
                for kc in range(8):
                    M(lambda e, bu=bu, kc=kc, fc=fc, j=j: e.matmul(bu[:, 0:RB], lhsT=wu[j][:, kc, fc * 128:(fc + 1) * 128], rhs=xsT[:, kc, :], start=(kc == 0), stop=(kc == 7)),
                      r=[k(wu[j]), k(xsT)], w=[ku])
                A(lambda e, bg=bg: e.activation(out=sg[:], in_=bg[:, 0:RB], func=AF.Silu), r=[kg], w=[k(sg)])
                V(lambda e, bu=bu, fc=fc: e.tensor_tensor(hT[:, fc, :], sg[:], bu[:, 0:RB], ALU.mult), r=[k(sg), ku], w=[k(hT)])
            for r_ in range(NR):
                yb = ys[nys % 2]
                nys += 1
                for half in range(2):
                    bo, ko = self.bank()
                    for fc in range(4):
                        M(lambda e, bo=bo, fc=fc, r_=r_, half=half, j=j: e.matmul(bo[:, :], lhsT=hT[:, fc, r_ * 128:(r_ + 1) * 128], rhs=wd[j][:, fc, half * 512:(half + 1) * 512],
                                                                                 start=(fc == 0), stop=(fc == 3)), r=[k(hT), k(wd[j])], w=[ko])
                    if half == 0:
                        A(lambda e, bo=bo, yb=yb: e.copy(out=yb[:, 0:512], in_=bo[:, :]), r=[ko], w=[k(yb)])
                    else:
                        V(lambda e, bo=bo, yb=yb: e.tensor_copy(yb[:, 512:1024], bo[:, :]), r=[ko], w=[k(yb)])
                r0 = b * RB + r_ * 128
                self.store('sp', self.ys_d[r0:r0 + 128, :], yb[:], k(yb), dkeys=["ys_d"])
        self.P.barrier()
        if mstop <= 2:
            self.sb_off = mark
            return
        l2g = sb([128, D], name="c_l2g")
        l2b = sb([128, D], name="c_l2b")
        self.load('sp', l2g[:], d['ln2_g'][l].partition_broadcast(128), k(l2g))
        self.load('sp', l2b[:], d['ln2_b'][l].partition_broadcast(128), k(l2b))
        csets = []
        for j in range(2):
            csets.append(dict(h1=sb([128, D], name="c_h1%d" % j), y0=sb([128, D], name="c_y0%d" % j), y1=sb([128, D], name="c_y1%d" % j),
                              tmp=sb([128, D], name="c_tmp%d" % j), h2=sb([128, D], name="c_h2%d" % j), hb=sb([128, D], BF16, "c_hb%d" % j),
                              hT=sb([128, 8, 128], BF16, "c_hT%d" % j), st=self.ln_stats("c%d" % j)))

        def cbody(i):
            B = csets[i % 2]
            h1, y0, y1 = B['h1'], B['y0'], B['y1']
            self.load('sp', h1[:], self.h1_d[i * 128:(i + 1) * 128, :], k(h1), dkeys=["h1d_%d_%d" % (l, i)])
            for j, yt in ((0, y0), (1, y1)):
                col = 2 * i + j
                self.P.dma('pool', lambda e, col=col, yt=yt: e.indirect_dma_start(out=yt[:], out_offset=None, in_=self.ys_d.ap(),
                                                                                 in_offset=bass.IndirectOffsetOnAxis(ap=dest[:, col:col + 1], axis=0)), k(yt), r=[k(dest), "ys_d"], w=[k(yt)])
            yield
            V(lambda e: e.tensor_scalar(y0[:], y0[:], rp['gat'][:, 2 * i:2 * i + 1], None, ALU.mult), r=[k(y0), k(rp['gat'])], w=[k(y0)])
            yield
            V(lambda e: e.scalar_tensor_tensor(y0[:], y1[:], rp['gat'][:, 2 * i + 1:2 * i + 2], y0[:], ALU.mult, ALU.add), r=[k(y1), k(y0), k(rp['gat'])], w=[k(y0)])
            yield
            V(lambda e: e.scalar_tensor_tensor(h1[:], h1[:], ALPHA, y0[:], ALU.mult, ALU.add), r=[k(h1), k(y0)], w=[k(h1)])
            yield
            yield from self.layernorm(h1, l2g, l2b, B['h2'], B['tmp'], B['st'])
            if last:
                self.store('sp', self.out_d[i * 128:(i + 1) * 128, :], B['h2'][:], k(B['h2']), dkeys=["out_%d" % i])
                yield
            else:
                self.store('sp', self.h_d[i * 128:(i + 1) * 128, :], B['h2'][:], k(B['h2']), dkeys=["hd_%d" % i])
                yield
                A(lambda e: e.copy(out=B['hb'][:], in_=B['h2'][:]), r=[k(B['h2'])], w=[k(B['hb'])])
                yield
                bk, bkey = self.bank()
                pb = bk[:].bitcast(BF16)
                for kc in range(8):
                    M(lambda e, kc=kc: e.transpose(pb[:, kc * 128:(kc + 1) * 128], B['hb'][:, kc * 128:(kc + 1) * 128], c['identb'][:]), r=[k(B['hb']), k(c['identb'])], w=[bkey])
                V(lambda e: e.tensor_copy(B['hT'][:].rearrange("p a b -> p (a b)"), pb), r=[bkey], w=[k(B['hT'])])
                yield
                self.store('sp', self.hT_d[:, :, i * 128:(i + 1) * 128], B['hT'][:], k(B['hT']), dkeys=["hTd_%d" % i])
                yield
        self.run_pipe([lambda i=i: cbody(i) for i in range(NT)], 2)
        self.P.barrier()
        self.sb_off = mark

    def build_full(self):
        self.declare_inputs()
        T = self.T
        self.h_d = self.dscr("h_d", [T, D])
        self.hT_d = self.dscr("hT_d", [128, 8, T], BF16)
        self.h1_d = self.dscr("h1_d", [T, D])
        self.h1b_d = self.dscr("h1b_d", [T, D], BF16)
        self.xs_d = self.dscr("xs_d", [self.NB * self.RB, D], BF16)
        self.ys_d = self.dscr("ys_d", [self.NB * self.RB, D])
        self.out_d = self.dout("out", [T, D])
        if self.debug:
            self.dbg_y = self.dout("dbg_y", [T, D])
        self.consts()
        self.alloc_route_persist()
        base = self.sb_off
        for l in range(self.depth):
            self.sb_off = base
            self.alloc_params()
            self.alloc_mixer()
            self.alloc_router()
            if l == 0:
                self.stage0()
                self.P.barrier()
            self.load_params(l)
            self.stageM(l)
            self.P.barrier()
            self.sb_off = base
            self.stageMoE(l, l == self.depth - 1)
        self.P.barrier()
        return self.nc


def _host_inputs(inputs, b, T):
    m = {}
    for k, v in inputs.items():
        v = np.asarray(v)
        if k == 'x':
            m[k] = np.ascontiguousarray(v[b, :T])
        elif k == 'rwkv_r_k':
            m[k] = np.ascontiguousarray(v.reshape(DEPTH, 256))
        elif k in ('moe_w_gate', 'moe_w_up'):
            m[k] = np.ascontiguousarray(v.reshape(DEPTH * NE * D, FF))
        elif k == 'moe_w_down':
            m[k] = np.ascontiguousarray(v.reshape(DEPTH * NE * FF, D))
        else:
            m[k] = np.ascontiguousarray(v)
    return m


def kernel(**inputs):
    x = np.asarray(inputs['x'])
    Bsz, T, _ = x.shape
    bld = Builder(T)
    nc = bld.build_full()
    in_maps = [_host_inputs(inputs, b, T) for b in range(Bsz)]
    res = run_bass_kernel_spmd(nc, in_maps, core_ids=list(range(Bsz)))
    return np.stack([np.asarray(r["out"]) for r in res.results], axis=0).astype(np.float32)
```

```python
import numpy as np
import concourse.bass as bass
import concourse.mybir as mybir
from concourse.bass_utils import run_bass_kernel_spmd

F32 = mybir.dt.float32
BF16 = mybir.dt.bfloat16
I32 = mybir.dt.int32
U32 = mybir.dt.uint32
AF = mybir.ActivationFunctionType
ALU = mybir.AluOpType
AX = mybir.AxisListType

D = 1024
NIN = 2968
DEPTH = 2
ALPHA = (2 * DEPTH) ** 0.25
LN_EPS = 1e-5
RMS_EPS = 1e-6
GN_EPS = 64e-5
NE = 32
FF = 512
O_Z, O_XBC, O_DT, O_RW, O_GQ, O_GK, O_GV, O_GG, O_GA = 0, 512, 1280, 1288, 2184, 2312, 2440, 2696, 2952


class Prog:
    EPOCH = 8192
    NDMA = 40

    def __init__(self, nc):
        self.nc = nc
        self.eng = {'pe': nc.tensor, 'dve': nc.vector, 'act': nc.scalar, 'pool': nc.gpsimd, 'sp': nc.sync}
        self.esems = {n: [] for n in ('pe', 'dve', 'act', 'pool')}
        self.cnt = {n: 0 for n in ('pe', 'dve', 'act', 'pool')}
        self.dsems, self.dval, self.dkey = [], [], {}
        self.waited = {n: {} for n in self.eng}
        self.lastw, self.readers = {}, {}
        self.ninst = 0

    def _esem(self, X, ep):
        while len(self.esems[X]) <= ep:
            self.esems[X].append(self.nc.alloc_semaphore("s_%s_%d" % (X, len(self.esems[X]))))
        return self.esems[X][ep]

    def _deps(self, reads, writes):
        deps = {}

        def add(ev):
            if ev is not None and deps.get(ev[0], 0) < ev[1]:
                deps[ev[0]] = ev[1]
        for r in reads:
            add(self.lastw.get(r))
        for w in writes:
            add(self.lastw.get(w))
            for k, v in self.readers.get(w, {}).items():
                add((k, v))
        return deps

    def _wait(self, X, deps):
        e = self.eng[X]
        for k, v in deps.items():
            if k == X and X == 'pe':
                continue
            if self.waited[X].get(k, 0) >= v:
                continue
            if isinstance(k, str):
                ep = (v - 1) // self.EPOCH
                e.wait_ge(self._esem(k, ep), v - ep * self.EPOCH)
            else:
                v = self.dval[k]
                e.wait_ge(self.dsems[k], v)
            self.waited[X][k] = v
            self.ninst += 1

    def _record(self, ev, reads, writes):
        for r in reads:
            d = self.readers.setdefault(r, {})
            if d.get(ev[0], 0) < ev[1]:
                d[ev[0]] = ev[1]
        for w in writes:
            self.lastw[w] = ev
            self.readers[w] = {}

    def op(self, X, fn, r=(), w=()):
        r = [k for k in r if k is not None]
        w = [k for k in w if k is not None]
        w = w + [k for k in r if isinstance(k, str) and k.startswith('psb') and k not in w]
        self._wait(X, self._deps(r, w))
        inst = fn(self.eng[X])
        self.cnt[X] += 1
        n = self.cnt[X]
        inst.then_inc(self._esem(X, (n - 1) // self.EPOCH), 1)
        self.ninst += 1
        self._record((X, n), r, w)

    def dma(self, X, fn, semkey, r=(), w=()):
        base = semkey.rsplit('_', 1)[0] if semkey.rsplit('_', 1)[-1].isdigit() else semkey
        if base not in self.dkey:
            i = len(self.dsems)
            self.dsems.append(self.nc.alloc_semaphore("d_%d" % i))
            self.dval.append(0)
            self.dkey[base] = i
        i = self.dkey[base]
        self._wait(X, self._deps(r, w))
        inst = fn(self.eng[X])
        self.dval[i] += 16
        inst.then_inc(self.dsems[i], 16)
        self.ninst += 1
        self._record((i, self.dval[i]), r, w)

    def barrier(self):
        deps = {k: v for k, v in self.cnt.items() if v > 0}
        for i, v in enumerate(self.dval):
            if v > 0:
                deps[i] = v
        for X in self.eng:
            self._wait(X, dict(deps))


class Tile:
    def __init__(self, t, key):
        self.t, self.key = t, key

    def __getitem__(self, k):
        return self.t[k]


class Builder:
    def __init__(self, T, depth=DEPTH, debug=False, rb=None):
        import os
        rb = rb or int(os.environ.get('RB', '512'))
        self.T, self.depth, self.debug = T, depth, debug
        self.NT = T // 128
        self.RB = rb
        self.NB = (2 * T) // rb + NE
        nc = self.nc = bass.Bass("TRN2", target_bir_lowering=False)
        self.P = Prog(nc)
        self.nsb = 0
        self.sb_off = 16640
        self.sb_peak = 0
        self.sb_cap = 229376
        self.bank_i = 0
        self.chain_i = {}
        self.banks = [nc.alloc_psum_tensor("psb%d" % i, [128, 512], F32) for i in range(8)]
        self.dbg = {}

    def sb(self, shape, dt=F32, name=None):
        self.nsb += 1
        name = "%s_%d" % (name or "t", self.nsb)
        esz = 2 if dt == BF16 else 4
        n = 1
        for v in shape[1:]:
            n *= v
        nbytes = (n * esz + 31) // 32 * 32
        off = self.sb_off
        self.sb_off += nbytes
        assert self.sb_off <= self.sb_cap, "SBUF overflow %d" % self.sb_off
        self.sb_peak = max(self.sb_peak, self.sb_off)
        return Tile(self.nc.alloc_sbuf_tensor_at(name, list(shape), dt, offset=off), name)

    CHAIN_BANKS = {'s': [0, 1], 'r': [2, 3, 4], 'g': [5, 6], 'e': [7]}

    def bank(self, chain=None):
        if chain is None:
            i = self.bank_i
            self.bank_i = (i + 1) % 8
        else:
            lst = self.CHAIN_BANKS[chain]
            j = self.chain_i.get(chain, 0)
            self.chain_i[chain] = (j + 1) % len(lst)
            i = lst[j]
        return self.banks[i], "psb%d" % i

    def V(self, fn, r=(), w=()):
        self.P.op('dve', fn, r, w)

    def A(self, fn, r=(), w=()):
        self.P.op('act', fn, r, w)

    def G(self, fn, r=(), w=()):
        self.P.op('pool', fn, r, w)

    def M(self, fn, r=(), w=()):
        self.P.op('pe', fn, r, w)

    def din(self, name, shape, dt=F32):
        return self.nc.dram_tensor(name, list(shape), dt, kind="ExternalInput")

    def dscr(self, name, shape, dt=F32):
        return self.nc.dram_tensor(name, list(shape), dt, kind="Internal")

    def dout(self, name, shape, dt=F32):
        return self.nc.dram_tensor(name, list(shape), dt, kind="ExternalOutput")

    def load(self, q, out_ap, in_ap, key, dkeys=(), slow=False):
        if slow:
            self.P.dma(q, lambda e: e.dma_start(out=out_ap, in_=in_ap, allow_slow_non_contiguous=True), key, r=list(dkeys), w=[key])
        else:
            self.P.dma(q, lambda e: e.dma_start(out=out_ap, in_=in_ap), key, r=list(dkeys), w=[key])

    def store(self, q, out_ap, in_ap, key, dkeys=()):
        self.P.dma(q, lambda e: e.dma_start(out=out_ap, in_=in_ap), key, r=[key], w=list(dkeys))

    def consts(self):
        nc = self.nc
        c = self.c = {}
        self._ln_st = self.sb([128, 2, 6], name="ln_st")
        self._ln_mv = self.sb([128, 2], name="ln_mv")
        self._ln_rs = self.sb([128, 1], name="ln_rs")
        onesf = c['onesf'] = self.sb([128, 128], name="onesf")
        self.G(lambda e: e.memset(onesf[:], 1.0), w=[onesf.key])
        identf = c['identf'] = self.sb([128, 128], name="identf")
        self.G(lambda e: e.memset(identf[:], 0.0), w=[identf.key])
        self.G(lambda e: e.affine_select(out=identf[:], in_=identf[:], pattern=[[-1, 128]], base=0, channel_multiplier=1,
                                         compare_op=ALU.not_equal, fill=1.0), r=[identf.key], w=[identf.key])
        identb = c['identb'] = self.sb([128, 128], BF16, name="identb")
        self.V(lambda e: e.tensor_copy(identb[:], identf[:]), r=[identf.key], w=[identb.key])
        tri = c['tri'] = self.sb([128, 128], name="tri")
        self.G(lambda e: e.affine_select(out=tri[:], in_=onesf[:], pattern=[[1, 128]], base=0, channel_multiplier=-1,
                                         compare_op=ALU.is_ge, fill=0.0), r=[onesf.key], w=[tri.key])
        su = c['su'] = self.sb([128, 128], name="su")
        self.G(lambda e: e.affine_select(out=su[:], in_=onesf[:], pattern=[[-1, 128]], base=0, channel_multiplier=1,
                                         compare_op=ALU.is_gt, fill=0.0), r=[onesf.key], w=[su.key])
        sl = c['sl'] = self.sb([128, 128], name="sl")
        self.G(lambda e: e.affine_select(out=sl[:], in_=onesf[:], pattern=[[1, 128]], base=0, channel_multiplier=-1,
                                         compare_op=ALU.is_gt, fill=0.0), r=[onesf.key], w=[sl.key])
        maskb = c['maskb'] = self.sb([128, 128], name="maskb")
        self.G(lambda e: e.tensor_copy(maskb[:], tri[:]), r=[tri.key], w=[maskb.key])
        self.G(lambda e: e.memset(maskb[0:64, 64:128], 0.0), w=[maskb.key])
        rmask = c['rmask'] = self.sb([128, 256], name="rmask")
        self.G(lambda e: e.memset(rmask[:], 1.0), w=[rmask.key])
        self.G(lambda e: e.memset(rmask[:].rearrange("p (a b) -> p a b", b=64)[:, :, 0:1], 0.0), w=[rmask.key])
        hm = c['hm'] = self.sb([128, 2], name="hm")
        self.G(lambda e: e.memset(hm[:], 0.0), w=[hm.key])
        self.G(lambda e: e.memset(hm[0:64, 0:1], 1.0), w=[hm.key])
        self.G(lambda e: e.memset(hm[64:128, 1:2], 1.0), w=[hm.key])
        nhm = c['nhm'] = self.sb([128, 2], name="nhm")
        self.V(lambda e: e.tensor_scalar(nhm[:], hm[:], -1.0, None, ALU.mult), r=[hm.key], w=[nhm.key])
        qm = c['qm'] = self.sb([64, 2], name="qm")
        self.G(lambda e: e.memset(qm[:], 0.0), w=[qm.key])
        self.G(lambda e: e.memset(qm[0:32, 0:1], 32.0 ** -0.5), w=[qm.key])
        self.G(lambda e: e.memset(qm[32:64, 1:2], 32.0 ** -0.5), w=[qm.key])
        bones = c['bones'] = self.sb([128, 128], name="bones")
        self.G(lambda e: e.memset(bones[:], 0.0), w=[bones.key])
        self.G(lambda e: e.memset(bones[0:64, 0:64], 1.0), w=[bones.key])
        self.G(lambda e: e.memset(bones[64:128, 64:128], 1.0), w=[bones.key])

    def layernorm(self, xin, gk, bk, out, tmp, stt=None):
        st, mv, rs = stt if stt is not None else (self._ln_st, self._ln_mv, self._ln_rs)
        for i in range(2):
            self.V(lambda e, i=i: e.bn_stats(st[:, i, :], xin[:, i * 512:(i + 1) * 512]), r=[xin.key], w=[st.key])
        self.V(lambda e: e.bn_aggr(mv[:], st[:].rearrange("p a b -> p (a b)")), r=[st.key], w=[mv.key])
        self.V(lambda e: e.tensor_scalar(rs[:], mv[:, 1:2], LN_EPS, None, ALU.add), r=[mv.key], w=[rs.key])
        yield
        self.A(lambda e: e.activation(out=rs[:], in_=rs[:], func=AF.Sqrt), r=[rs.key], w=[rs.key])
        yield
        self.V(lambda e: e.reciprocal(rs[:], rs[:]), r=[rs.key], w=[rs.key])
        self.V(lambda e: e.tensor_scalar(tmp[:], xin[:], mv[:, 0:1], rs[:, 0:1], ALU.subtract, ALU.mult), r=[xin.key, mv.key, rs.key], w=[tmp.key])
        yield
        self.G(lambda e: e.tensor_tensor(tmp[:], tmp[:], gk[:], ALU.mult), r=[tmp.key, gk.key], w=[tmp.key])
        yield
        self.V(lambda e: e.tensor_tensor(out[:], tmp[:], bk[:], ALU.add), r=[tmp.key, bk.key], w=[out.key])
        yield

    def ln_stats(self, tag):
        return (self.sb([128, 2, 6], name="lnst_" + tag), self.sb([128, 2], name="lnmv_" + tag), self.sb([128, 1], name="lnrs_" + tag))

    def run_pipe(self, bodies, width=2):
        active, nxt = [], 0
        while active or nxt < len(bodies):
            while len(active) < width and nxt < len(bodies):
                active.append(bodies[nxt]())
                nxt += 1
            for g_ in list(active):
                try:
                    next(g_)
                except StopIteration:
                    active.remove(g_)

    def to_fm(self, h_tm, hb, hT):
        c = self.c
        self.A(lambda e: e.copy(out=hb[:], in_=h_tm[:]), r=[h_tm.key], w=[hb.key])
        bk, bkey = self.bank()
        pb = bk[:].bitcast(BF16)
        for kc in range(8):
            self.M(lambda e, kc=kc: e.transpose(pb[:, kc * 128:(kc + 1) * 128], hb[:, kc * 128:(kc + 1) * 128], c['identb'][:]),
                   r=[hb.key, c['identb'].key], w=[bkey])
        self.V(lambda e: e.tensor_copy(hT[:].rearrange("p a b -> p (a b)"), pb), r=[bkey], w=[hT.key])

    def declare_inputs(self):
        L = DEPTH
        d = self.d = {}
        specs = dict(x=[self.T, D], ln_in_g=[D], ln_in_b=[D], w_in=[L, D, NIN], ssd_conv_w=[L, 4, 768], ssd_conv_b=[L, 768],
                     ssd_dt_bias=[L, 8], ssd_a_log=[L, 8], ssd_d=[L, 8], ssd_norm_g=[L, 512], rwkv_mu=[L, 896], rwkv_w0=[L, 256],
                     rwkv_w2=[L, 32, 256], rwkv_a0=[L, 256], rwkv_a2=[L, 32, 256], rwkv_g2=[L, 64, 256], rwkv_k_k=[L, 256],
                     rwkv_k_a=[L, 256], rwkv_r_k=[L, 256], rwkv_ln_g=[L, 256], rwkv_ln_b=[L, 256], gla_w_a2=[L, 16, 128],
                     gla_b_a=[L, 128], gla_norm_g=[L, 256], w_out=[L, D, D], ln1_g=[L, D], ln1_b=[L, D], moe_w_rg=[L, D, 4],
                     moe_b_rg=[L, 4], moe_w_re=[L, D, 32], moe_b_re=[L, 32], moe_w_gu=[L * NE * D, 2 * FF],
                     moe_w_down=[L * NE * FF, D], ln2_g=[L, D], ln2_b=[L, D])
        for k, s in specs.items():
            d[k] = self.din(k, s)
        return specs

    def alloc_params(self):
        p = self.p = {}
        sb = self.sb
        p['w_in'] = sb([128, 8, NIN], BF16, "w_in_sb")
        p['w_out'] = sb([128, 8, D], BF16, "w_out_sb")
        p['cw'] = sb([128, 6, 4], name="convw")
        p['cb'] = sb([128, 6], name="convb")
        p['cdiag'] = sb([128, 24, 128], BF16, "cdiag")
        for n, w in (('dtb', 8), ('alog', 8), ('dsk8', 8), ('dsk', 512), ('sng', 512), ('rlg', 256), ('rlb', 256), ('gng', 256),
                     ('l1g', D), ('l1b', D), ('rb36', 36)):
            p[n] = sb([128, w], name="p_" + n)
        for n, w in (('mu', 7), ('omu', 7), ('w0', 2), ('a0', 2), ('kk', 2), ('ka', 2), ('omka', 2), ('rk', 2)):
            p[n] = sb([128, w], name="p_" + n)
        p['ba'] = sb([64, 2], name="p_ba")
        p['w2p'] = sb([128, 256], name="p_w2p")
        p['a2p'] = sb([128, 256], name="p_a2p")
        p['g2p'] = sb([128, 256], name="p_g2p")
        p['wa2'] = sb([32, 128], name="p_wa2")
        p['wr'] = sb([128, 8, 36], name="p_wr")

    def load_params(self, l):
        p, d, c = self.p, self.d, self.c
        q = 'pool'
        win = d['w_in'][l].rearrange("(kc p) n -> p kc n", p=128)
        for kc in range(8):
            for (a, b) in ((0, 1484), (1484, NIN)):
                self.load(q, p['w_in'][:, kc, a:b], win[:, kc, a:b], p['w_in'].key)
        wo = d['w_out'][l].rearrange("(kc p) n -> p kc n", p=128)
        for kc in range(8):
            self.load(q, p['w_out'][:, kc, :], wo[:, kc, :], p['w_out'].key)
        q = 'sp'
        for kk_ in range(4):
            self.load(q, p['cw'][:, :, kk_], d['ssd_conv_w'][l][kk_].rearrange("(cb p) -> p cb", p=128), p['cw'].key, slow=True)
        self.load(q, p['cb'][:], d['ssd_conv_b'][l].rearrange("(cb p) -> p cb", p=128), p['cb'].key, slow=True)
        for n, src in (('dtb', 'ssd_dt_bias'), ('alog', 'ssd_a_log'), ('dsk8', 'ssd_d'), ('sng', 'ssd_norm_g'), ('rlg', 'rwkv_ln_g'),
                       ('rlb', 'rwkv_ln_b'), ('gng', 'gla_norm_g'), ('l1g', 'ln1_g'), ('l1b', 'ln1_b')):
            self.load(q, p[n][:], d[src][l].partition_broadcast(128), p[n].key)
        self.load(q, p['rb36'][:, 0:4], d['moe_b_rg'][l].partition_broadcast(128), p['rb36'].key)
        self.load(q, p['rb36'][:, 4:36], d['moe_b_re'][l].partition_broadcast(128), p['rb36'].key)
        self.load(q, p['mu'][:], d['rwkv_mu'][l].rearrange("(b p) -> p b", p=128), p['mu'].key, slow=True)
        for n, src in (('w0', 'rwkv_w0'), ('a0', 'rwkv_a0'), ('kk', 'rwkv_k_k'), ('ka', 'rwkv_k_a'), ('rk', 'rwkv_r_k')):
            self.load(q, p[n][:], d[src][l].rearrange("(b p) -> p b", p=128), p[n].key, slow=True)
        self.load(q, p['ba'][:], d['gla_b_a'][l].rearrange("(b p) -> p b", p=64), p['ba'].key, slow=True)
        for n in ('w2p', 'a2p', 'g2p'):
            self.G(lambda e, n=n: e.memset(p[n][:], 0.0), w=[p[n].key])
        self.load(q, p['w2p'][0:32, :], d['rwkv_w2'][l], p['w2p'].key)
        self.load(q, p['a2p'][32:64, :], d['rwkv_a2'][l], p['a2p'].key)
        self.load(q, p['g2p'][64:128, :], d['rwkv_g2'][l], p['g2p'].key)
        self.G(lambda e: e.memset(p['wa2'][:], 0.0), w=[p['wa2'].key])
        self.load(q, p['wa2'][16:32, :], d['gla_w_a2'][l], p['wa2'].key)
        self.load(q, p['wr'][:, :, 0:4], d['moe_w_rg'][l].rearrange("(kc p) n -> p kc n", p=128), p['wr'].key, slow=True)
        self.load(q, p['wr'][:, :, 4:36], d['moe_w_re'][l].rearrange("(kc p) n -> p kc n", p=128), p['wr'].key, slow=True)
        self.V(lambda e: e.tensor_scalar(p['omu'][:], p['mu'][:], -1.0, 1.0, ALU.mult, ALU.add), r=[p['mu'].key], w=[p['omu'].key])
        self.V(lambda e: e.tensor_scalar(p['omka'][:], p['ka'][:], -1.0, 1.0, ALU.mult, ALU.add), r=[p['ka'].key], w=[p['omka'].key])
        self.A(lambda e: e.activation(out=p['alog'][:], in_=p['alog'][:], func=AF.Exp), r=[p['alog'].key], w=[p['alog'].key])
        self.V(lambda e: e.tensor_scalar(p['alog'][:], p['alog'][:], -1.0, None, ALU.mult), r=[p['alog'].key], w=[p['alog'].key])
        self.V(lambda e: e.tensor_copy(p['dsk'][:].rearrange("p (h q) -> p h q", q=64), p['dsk8'][:].unsqueeze(2).to_broadcast([128, 8, 64])),
               r=[p['dsk8'].key], w=[p['dsk'].key])
        for cb in range(6):
            for k in range(4):
                self.V(lambda e, cb=cb, k=k: e.tensor_scalar(p['cdiag'][:, cb * 4 + k, :], c['identf'][:], p['cw'][:, cb, k:k + 1], None, ALU.mult),
                       r=[c['identf'].key, p['cw'].key], w=[p['cdiag'].key])

    def alloc_mixer(self):
        s = self.s = {}
        sb = self.sb
        s['hT'] = sb([128, 8, 128], BF16, "m_hT")
        s['htm'] = sb([128, D], name="m_htm")
        s['xbc'] = sb([128, 6, 132], BF16, "m_xbc")
        s['xbB'] = sb([128, 6, 132], BF16, "m_xbB")
        s['xc'] = sb([128, 6, 128], BF16, "m_xc")
        s['xh'] = sb([128, 512], BF16, "m_xh")
        s['xdt'] = sb([128, 512], BF16, "m_xdt")
        s['btm'] = sb([128, 128], BF16, "m_btm")
        s['cm'] = sb([128, 2, 128], BF16, "m_cm")
        s['dt'] = sb([128, 8], name="m_dt")
        s['adt'] = sb([128, 8], name="m_adt")
        s['sp1'] = sb([128, 8], name="m_sp1")
        s['sp2'] = sb([128, 8], name="m_sp2")
        s['R'] = sb([128, 4, 128], name="m_R")
        s['seg'] = sb([128, 8, 128], BF16, "m_seg")
        s['ea'] = sb([128, 8], name="m_ea")
        s['cd'] = sb([128, 4], name="m_cd")
        s['cbm'] = sb([128, 2, 128], BF16, "m_cbm")
        s['toend'] = sb([128, 8], name="m_toend")
        s['S32'] = sb([128, 256], name="m_S32")
        s['Sbf'] = sb([128, 256], BF16, "m_Sbf")
        s['y1'] = sb([128, 512], name="m_y1")
        s['sz'] = sb([128, 512], BF16, "m_sz")
        s['ssq'] = sb([128, 4], name="m_ssq")
        s['ycat'] = sb([128, D], BF16, "m_ycat")
        s['yT'] = sb([128, 8, 128], BF16, "m_yT")
        s['gaT'] = sb([32, 128], name="g_gaT")
        s['gx'] = sb([64, 256], name="g_x")
        s['gt1'] = sb([64, 256], name="g_t1")
        s['gcum'] = sb([64, 256], name="g_cum")
        s['geq'] = sb([64, 256], name="g_eq")
        s['gek'] = sb([64, 256], name="g_ek")
        s['gel'] = sb([64, 4], name="g_el")
        s['gqm'] = sb([64, 2, 256], BF16, "g_qm")
        s['gkT'] = sb([64, 256], BF16, "g_kT")
        s['gktm'] = sb([128, 2, 128], BF16, "g_ktm")
        s['gv'] = sb([128, 256], BF16, "g_v")
        s['gvm'] = sb([128, 2, 256], BF16, "g_vm")
        s['gsm'] = sb([128, 4, 128], BF16, "g_sm")
        s['gS'] = sb([64, 2, 64], name="g_S")
        s['gSb'] = sb([64, 2, 2, 64], BF16, "g_Sb")
        s['gst'] = sb([64, 2, 64], name="g_st")
        s['go'] = sb([128, 256], name="g_o")
        s['gsq'] = sb([128, 256], name="g_sq")
        s['grs'] = sb([128, 4], name="g_rs")
        s['gsg'] = sb([128, 256], name="g_sg")
        s['rw'] = sb([128, 7, 129], name="r_rw")
        s['rsh'] = sb([128, 7, 128], name="r_sh")
        s['rt1'] = sb([128, 7, 128], name="r_t1")
        for n in ('ra1', 'ra2', 'ra3', 'rcw', 'recw', 'reicw', 'recwp', 'ra', 'rkk', 'rkp'):
            s[n] = sb([128, 256], name="r_" + n)
        for n in ('rKt', 'rBt'):
            s[n] = sb([128, 256], BF16, "r_" + n)
        s['rAm'] = sb([128, 2, 256], BF16, "r_Am")
        s['rRm'] = sb([128, 2, 256], BF16, "r_Rm")
        s['rtw'] = sb([128, 128], name="r_tw")
        s['rsg'] = sb([128, 128], name="r_sg")
        s['rvtm'] = sb([128, 256], name="r_vtm")
        s['rvc'] = sb([64, 2, 256], BF16, "r_vc")
        s['rBc'] = sb([64, 2, 256], BF16, "r_Bc")
        s['rKc'] = sb([64, 2, 256], BF16, "r_Kc")
        s['rP'] = sb([64, 8, 64], BF16, "r_P")
        s['rQ'] = sb([64, 8, 64], BF16, "r_Q")
        s['rP2'] = sb([64, 8, 64], BF16, "r_P2")
        s['rQ2'] = sb([64, 8, 64], BF16, "r_Q2")
        s['rTT'] = sb([64, 8, 64], BF16, "r_TT")
        s['rAak'] = sb([64, 8, 64], BF16, "r_Aak")
        s['rArb'] = sb([64, 8, 64], BF16, "r_Arb")
        s['rArk'] = sb([64, 8, 64], BF16, "r_Ark")
        s['rG'] = sb([64, 4, 64], BF16, "r_G")
        s['rU'] = sb([64, 4, 64], BF16, "r_U")
        s['rST'] = sb([128, 2, 64], name="r_ST")
        s['rSTb'] = sb([128, 2, 64], BF16, "r_STb")
        s['rt2'] = sb([128, 2, 64], name="r_t2")
        s['rewc'] = sb([128, 4], name="r_ewc")
        s['rY1'] = sb([128, 256], name="r_Y1")
        s['rY'] = sb([128, 256], name="r_Y")
        s['rm1'] = sb([128, 4], name="r_m1")
        s['rm2'] = sb([128, 4], name="r_m2")
        s['rvar'] = sb([128, 4], name="r_var")
        s['rbc'] = sb([128, 4], name="r_bc")
        s['rg'] = sb([128, 256], name="r_g")
        s['mix'] = sb([128, D], name="m_mix")
        s['tmp'] = sb([128, D], name="m_tmp")
        s['h1'] = sb([128, D], name="m_h1")

    def proj_tm(self, out_ap, okey, c0, n):
        s, p = self.s, self.p
        for kc in range(8):
            self.M(lambda e, kc=kc: e.matmul(out_ap, lhsT=s['hT'][:, kc, :], rhs=p['w_in'][:, kc, c0:c0 + n], start=(kc == 0), stop=(kc == 7)),
                   r=[s['hT'].key, p['w_in'].key], w=[okey])

    def proj_fm(self, out_ap, okey, c0, m):
        s, p = self.s, self.p
        for kc in range(8):
            self.M(lambda e, kc=kc: e.matmul(out_ap, lhsT=p['w_in'][:, kc, c0:c0 + m], rhs=s['hT'][:, kc, :], start=(kc == 0), stop=(kc == 7)),
                   r=[s['hT'].key, p['w_in'].key], w=[okey])

    def bc(self, ap, shape, axis):
        return ap.unsqueeze(axis).to_broadcast(list(shape))

    def ssd_tile(self, first):
        s, p, c = self.s, self.p, self.c
        V, A, G, M = self.V, self.A, self.G, self.M
        k = lambda t: t.key
        bz, kz = self.bank('s')
        self.proj_tm(bz[:, :], kz, O_Z, 512)
        A(lambda e: e.activation(out=s['sz'][:], in_=bz[:, :], func=AF.Silu), r=[kz], w=[k(s['sz'])])
        yield
        bd, kd = self.bank('s')
        self.proj_tm(bd[:, 0:8], kd, O_DT, 8)
        V(lambda e: e.tensor_tensor(s['sp1'][:], bd[:, 0:8], p['dtb'][:], ALU.add), r=[kd, k(p['dtb'])], w=[k(s['sp1'])])
        yield
        V(lambda e: e.scalar_tensor_tensor(s['sp2'][:], s['sp1'][:], -1.0, s['sp1'][:], ALU.mult, ALU.max), r=[k(s['sp1'])], w=[k(s['sp2'])])
        yield
        A(lambda e: e.activation(out=s['sp2'][:], in_=s['sp2'][:], func=AF.Exp, scale=-1.0), r=[k(s['sp2'])], w=[k(s['sp2'])])
        yield
        A(lambda e: e.activation(out=s['sp2'][:], in_=s['sp2'][:], func=AF.Ln, bias=1.0), r=[k(s['sp2'])], w=[k(s['sp2'])])
        yield
        V(lambda e: e.scalar_tensor_tensor(s['dt'][:], s['sp1'][:], 0.0, s['sp2'][:], ALU.max, ALU.add), r=[k(s['sp1']), k(s['sp2'])], w=[k(s['dt'])])
        yield
        V(lambda e: e.tensor_tensor(s['adt'][:], s['dt'][:], p['alog'][:], ALU.mult), r=[k(s['dt']), k(p['alog'])], w=[k(s['adt'])])
        yield
        import os
        stop = float(os.environ.get('SSDSTOP', '9'))
        if stop <= 1:
            return
        if first:
            G(lambda e: e.memset(s['xbc'][:, :, 0:4], 0.0), w=[k(s['xbc'])])
            yield
            G(lambda e: e.memset(s['xbB'][:, :, 0:2], 0.0), w=[k(s['xbB'])])
            yield
        else:
            G(lambda e: e.tensor_copy(s['xbc'][:, :, 0:3], s['xbc'][:, :, 128:131]), r=[k(s['xbc'])], w=[k(s['xbc'])])
            yield
            G(lambda e: e.tensor_copy(s['xbB'][:, :, 0:2], s['xbB'][:, :, 128:130]), r=[k(s['xbB'])], w=[k(s['xbB'])])
            yield
        for grp, nb in ((0, 4), (4, 2)):
            bx, kx = self.bank('s')
            for j in range(nb):
                self.proj_fm(bx[:, j * 128:(j + 1) * 128], kx, O_XBC + (grp + j) * 128, 128)
            A(lambda e, bx=bx, grp=grp, nb=nb: e.copy(out=s['xbc'][:, grp:grp + nb, 3:131], in_=bx[:, 0:nb * 128].rearrange("p (a b) -> p a b", b=128)),
              r=[kx], w=[k(s['xbc'])])
            yield
            V(lambda e, bx=bx, grp=grp, nb=nb: e.tensor_copy(s['xbB'][:, grp:grp + nb, 2:130], bx[:, 0:nb * 128].rearrange("p (a b) -> p a b", b=128)),
              r=[kx], w=[k(s['xbB'])])
            yield
        for grp, nb in ((0, 4), (4, 2)):
            bx, kx = self.bank('s')
            for j in range(nb):
                cb = grp + j
                for kk_ in range(4):
                    src = s['xbc'] if kk_ % 2 == 0 else s['xbB']
                    off = kk_ if kk_ % 2 == 0 else kk_ - 1
                    M(lambda e, bx=bx, j=j, cb=cb, kk_=kk_, src=src, off=off: e.matmul(bx[:, j * 128:(j + 1) * 128], lhsT=p['cdiag'][:, cb * 4 + kk_, :],
                                                                                   rhs=src[:, cb, off:off + 128], start=(kk_ == 0), stop=(kk_ == 3)),
                      r=[k(p['cdiag']), k(src)], w=[kx])
            for j in range(nb):
                cb = grp + j
                A(lambda e, bx=bx, j=j, cb=cb: e.activation(out=s['xc'][:, cb, :], in_=bx[:, j * 128:(j + 1) * 128], func=AF.Silu, bias=p['cb'][:, cb:cb + 1]),
                  r=[kx, k(p['cb'])], w=[k(s['xc'])])
                yield
        if stop <= 2:
            return
        bt, kt = self.bank('s')
        pb = bt[:].bitcast(BF16)
        for j in range(5):
            M(lambda e, j=j: e.transpose(pb[:, j * 128:(j + 1) * 128], s['xc'][:, j, :], c['identb'][:]), r=[k(s['xc']), k(c['identb'])], w=[kt])
        V(lambda e: e.tensor_copy(s['xh'][:], pb[:, 0:512]), r=[kt], w=[k(s['xh'])])
        yield
        V(lambda e: e.tensor_copy(s['btm'][:], pb[:, 512:640]), r=[kt], w=[k(s['btm'])])
        yield
        if stop <= 2.2:
            return
        G(lambda e: e.tensor_tensor(s['cm'][:], self.bc(s['xc'][:, 5, :], [128, 2, 128], 1), self.bc(c['hm'][:, :], [128, 2, 128], 2), ALU.mult),
          r=[k(s['xc']), k(c['hm'])], w=[k(s['cm'])])
        yield
        V(lambda e: e.tensor_tensor(s['xdt'][:].rearrange("p (h q) -> p h q", q=64), s['xh'][:].rearrange("p (h q) -> p h q", q=64),
                                    self.bc(s['dt'][:, :], [128, 8, 64], 2), ALU.mult), r=[k(s['xh']), k(s['dt'])], w=[k(s['xdt'])])
        yield
        if stop <= 2.4:
            return
        for half in range(2):
            G(lambda e, half=half: e.tensor_tensor(s['R'][:], self.bc(c['tri'][:, :], [128, 4, 128], 1), self.bc(s['adt'][:, half * 4:(half + 1) * 4], [128, 4, 128], 2), ALU.mult),
              r=[k(c['tri']), k(s['adt'])], w=[k(s['R'])])
            yield
            bD, kD = self.bank('s')
            for q2 in range(2):
                M(lambda e, bD=bD, q2=q2: e.matmul(bD[:, q2 * 256:(q2 + 1) * 256], lhsT=c['su'][:], rhs=s['R'][:, q2 * 2:(q2 + 1) * 2, :].rearrange("p a b -> p (a b)"), start=True, stop=True),
                  r=[k(c['su']), k(s['R'])], w=[kD])
            if stop <= 2.6:
                continue
            A(lambda e, bD=bD, half=half: e.activation(out=s['seg'][:, half * 4:(half + 1) * 4, :].rearrange("p a b -> p (a b)"), in_=bD[:, :], func=AF.Exp),
              r=[kD], w=[k(s['seg'])])
            yield
        if stop <= 2.8:
            return
        V(lambda e: e.tensor_copy(s['toend'][:], s['seg'][:, :, 127]), r=[k(s['seg'])], w=[k(s['toend'])])
        yield
        if stop <= 3:
            return
        be, ke = self.bank('s')
        M(lambda e: e.matmul(be[:, 0:8], lhsT=c['tri'][:], rhs=s['adt'][:], start=True, stop=True), r=[k(c['tri']), k(s['adt'])], w=[ke])
        for g in range(2):
            M(lambda e, g=g: e.matmul(be[g * 64:(g + 1) * 64, 8:12], lhsT=c['onesf'][:, 0:64], rhs=s['adt'][:, g * 4:(g + 1) * 4], start=True, stop=True),
              r=[k(c['onesf']), k(s['adt'])], w=[ke])
        A(lambda e: e.activation(out=s['ea'][:], in_=be[:, 0:8], func=AF.Exp), r=[ke], w=[k(s['ea'])])
        yield
        A(lambda e: e.activation(out=s['cd'][:], in_=be[:, 8:12], func=AF.Exp), r=[ke], w=[k(s['cd'])])
        yield
        if stop <= 4:
            return
        bc_, kc_ = self.bank('s')
        for g in range(2):
            M(lambda e, g=g: e.matmul(bc_[:, g * 128:(g + 1) * 128], lhsT=s['xc'][:, 4, :], rhs=s['cm'][:, g, :], start=True, stop=True),
              r=[k(s['xc']), k(s['cm'])], w=[kc_])
        V(lambda e: e.tensor_tensor(s['cbm'][:], bc_[:, 0:256].rearrange("p (a b) -> p a b", b=128), self.bc(c['tri'][:, :], [128, 2, 128], 1), ALU.mult),
          r=[kc_, k(c['tri'])], w=[k(s['cbm'])])
        yield
        for g in range(2):
            V(lambda e, g=g: e.tensor_tensor(s['seg'][:, g * 4:(g + 1) * 4, :], s['seg'][:, g * 4:(g + 1) * 4, :], self.bc(s['cbm'][:, g, :], [128, 4, 128], 1), ALU.mult),
              r=[k(s['seg']), k(s['cbm'])], w=[k(s['seg'])])
            yield
        by, ky = self.bank('s')
        for h in range(8):
            M(lambda e, h=h: e.matmul(by[:, h * 64:(h + 1) * 64], lhsT=s['seg'][:, h, :], rhs=s['xdt'][:, h * 64:(h + 1) * 64], start=True, stop=True),
              r=[k(s['seg']), k(s['xdt'])], w=[ky])
        bo, ko = self.bank('s')
        if not first:
            for g in range(2):
                M(lambda e, g=g: e.matmul(bo[:, g * 256:(g + 1) * 256], lhsT=s['cm'][:, g, :], rhs=s['Sbf'][:, :], start=True, stop=True),
                  r=[k(s['cm']), k(s['Sbf'])], w=[ko])
            V(lambda e: e.tensor_tensor(s['y1'][:].rearrange("p (h q) -> p h q", q=64), bo[:, :].rearrange("p (h q) -> p h q", q=64),
                                        self.bc(s['ea'][:, :], [128, 8, 64], 2), ALU.mult), r=[ko, k(s['ea'])], w=[k(s['y1'])])
            yield
            V(lambda e: e.tensor_tensor(s['y1'][:], s['y1'][:], by[:, :], ALU.add), r=[k(s['y1']), ky], w=[k(s['y1'])])
            yield
        else:
            V(lambda e: e.tensor_copy(s['y1'][:], by[:, :]), r=[ky], w=[k(s['y1'])])
            yield
        if stop <= 5:
            return
        V(lambda e: e.tensor_tensor(s['xdt'][:].rearrange("p (h q) -> p h q", q=64), s['xdt'][:].rearrange("p (h q) -> p h q", q=64),
                                    self.bc(s['toend'][:, :], [128, 8, 64], 2), ALU.mult), r=[k(s['xdt']), k(s['toend'])], w=[k(s['xdt'])])
        yield
        bs, ks = self.bank('s')
        for g in range(2):
            M(lambda e, g=g: e.matmul(bs[g * 64:(g + 1) * 64, 0:256], lhsT=s['btm'][:, g * 64:(g + 1) * 64], rhs=s['xdt'][:, g * 256:(g + 1) * 256], start=True, stop=True),
              r=[k(s['btm']), k(s['xdt'])], w=[ks])
        if first:
            V(lambda e: e.tensor_copy(s['S32'][:], bs[:, 0:256]), r=[ks], w=[k(s['S32'])])
            yield
        else:
            V(lambda e: e.tensor_tensor(s['S32'][:].rearrange("p (h q) -> p h q", q=64), s['S32'][:].rearrange("p (h q) -> p h q", q=64),
                                        self.bc(s['cd'][:, :], [128, 4, 64], 2), ALU.mult), r=[k(s['S32']), k(s['cd'])], w=[k(s['S32'])])
            yield
            V(lambda e: e.tensor_tensor(s['S32'][:], s['S32'][:], bs[:, 0:256], ALU.add), r=[k(s['S32']), ks], w=[k(s['S32'])])
            yield
        A(lambda e: e.copy(out=s['Sbf'][:], in_=s['S32'][:]), r=[k(s['S32'])], w=[k(s['Sbf'])])
        yield
        G(lambda e: e.tensor_tensor(s['xdt'][:], s['xh'][:], p['dsk'][:], ALU.mult), r=[k(s['xh']), k(p['dsk'])], w=[k(s['xdt'])])
        yield
        V(lambda e: e.tensor_tensor(s['y1'][:], s['y1'][:], s['xdt'][:], ALU.add), r=[k(s['y1']), k(s['xdt'])], w=[k(s['y1'])])
        yield
        V(lambda e: e.tensor_tensor(s['y1'][:], s['y1'][:], s['sz'][:], ALU.mult), r=[k(s['y1']), k(s['sz'])], w=[k(s['y1'])])
        yield
        for g in range(2):
            A(lambda e, g=g: e.activation(out=s['sz'][:, g * 256:(g + 1) * 256], in_=s['y1'][:, g * 256:(g + 1) * 256], func=AF.Square, accum_out=s['ssq'][:, g:g + 1]),
              r=[k(s['y1'])], w=[k(s['sz']), k(s['ssq'])])
            yield
        V(lambda e: e.tensor_scalar(s['ssq'][:, 0:2], s['ssq'][:, 0:2], 1.0 / 256, RMS_EPS, ALU.mult, ALU.add), r=[k(s['ssq'])], w=[k(s['ssq'])])
        yield
        A(lambda e: e.activation(out=s['ssq'][:, 0:2], in_=s['ssq'][:, 0:2], func=AF.Sqrt), r=[k(s['ssq'])], w=[k(s['ssq'])])
        yield
        V(lambda e: e.reciprocal(s['ssq'][:, 0:2], s['ssq'][:, 0:2]), r=[k(s['ssq'])], w=[k(s['ssq'])])
        yield
        for g in range(2):
            V(lambda e, g=g: e.scalar_tensor_tensor(s['ycat'][:, g * 256:(g + 1) * 256], s['y1'][:, g * 256:(g + 1) * 256], s['ssq'][:, g:g + 1],
                                                   p['sng'][:, g * 256:(g + 1) * 256], ALU.mult, ALU.mult), r=[k(s['y1']), k(s['ssq']), k(p['sng'])], w=[k(s['ycat'])])
            yield

    def gla_tile(self, first):
        s, p, c = self.s, self.p, self.c
        V, A, G, M = self.V, self.A, self.G, self.M
        k = lambda t: t.key
        bv, kv = self.bank('g')
        self.proj_tm(bv[:, :], kv, O_GV, 512)
        A(lambda e: e.copy(out=s['gv'][:], in_=bv[:, 0:256]), r=[kv], w=[k(s['gv'])])
        yield
        A(lambda e: e.activation(out=s['gsg'][:], in_=bv[:, 256:512], func=AF.Silu), r=[kv], w=[k(s['gsg'])])
        yield
        G(lambda e: e.tensor_tensor(s['gsg'][:], s['gsg'][:], p['gng'][:], ALU.mult), r=[k(s['gsg']), k(p['gng'])], w=[k(s['gsg'])])
        yield
        ba_, ka_ = self.bank('g')
        self.proj_fm(ba_[0:32, 0:128], ka_, O_GA - 16, 32)
        A(lambda e: e.copy(out=s['gaT'][:], in_=ba_[0:32, 0:128]), r=[ka_], w=[k(s['gaT'])])
        yield
        bx, kx = self.bank('g')
        for pr in range(2):
            M(lambda e, pr=pr: e.matmul(bx[0:64, pr * 128:(pr + 1) * 128], lhsT=p['wa2'][:, pr * 64:(pr + 1) * 64], rhs=s['gaT'][:], start=True, stop=True),
              r=[k(p['wa2']), k(s['gaT'])], w=[kx])
        for pr in range(2):
            A(lambda e, pr=pr: e.activation(out=s['gx'][:, pr * 128:(pr + 1) * 128], in_=bx[0:64, pr * 128:(pr + 1) * 128], func=AF.Identity, bias=p['ba'][:, pr:pr + 1]),
              r=[kx, k(p['ba'])], w=[k(s['gx'])])
            yield
        V(lambda e: e.scalar_tensor_tensor(s['gt1'][:], s['gx'][:], -1.0, s['gx'][:], ALU.mult, ALU.max), r=[k(s['gx'])], w=[k(s['gt1'])])
        yield
        A(lambda e: e.activation(out=s['gt1'][:], in_=s['gt1'][:], func=AF.Exp, scale=-1.0), r=[k(s['gt1'])], w=[k(s['gt1'])])
        yield
        A(lambda e: e.activation(out=s['gt1'][:], in_=s['gt1'][:], func=AF.Ln, bias=1.0), r=[k(s['gt1'])], w=[k(s['gt1'])])
        yield
        V(lambda e: e.scalar_tensor_tensor(s['gx'][:], s['gx'][:], 0.0, s['gt1'][:], ALU.min, ALU.subtract), r=[k(s['gx']), k(s['gt1'])], w=[k(s['gx'])])
        yield
        V(lambda e: e.tensor_tensor_scan(s['gcum'][:], c['rmask'][0:64, :], s['gx'][:], 0.0, ALU.mult, ALU.add), r=[k(c['rmask']), k(s['gx'])], w=[k(s['gcum'])])
        yield
        A(lambda e: e.activation(out=s['geq'][:], in_=s['gcum'][:], func=AF.Exp, scale=1.0 / 16), r=[k(s['gcum'])], w=[k(s['geq'])])
        yield
        A(lambda e: e.activation(out=s['gek'][:], in_=s['gcum'][:], func=AF.Exp, scale=-1.0 / 16), r=[k(s['gcum'])], w=[k(s['gek'])])
        yield
        A(lambda e: e.activation(out=s['gel'][:], in_=s['gcum'][:].rearrange("p (a b) -> p a b", b=64)[:, :, 63], func=AF.Exp, scale=1.0 / 16),
          r=[k(s['gcum'])], w=[k(s['gel'])])
        yield
        bq, kq = self.bank('g')
        for pr in range(2):
            self.proj_fm(bq[0:64, pr * 128:(pr + 1) * 128], kq, O_GQ + pr * 64, 64)
        for pr in range(2):
            self.proj_fm(bq[0:64, 256 + pr * 128:256 + (pr + 1) * 128], kq, O_GK + pr * 64, 64)
        for hh in range(2):
            V(lambda e, hh=hh: e.scalar_tensor_tensor(s['gqm'][:, hh, :], bq[0:64, 0:256], c['qm'][:, hh:hh + 1], s['geq'][:], ALU.mult, ALU.mult),
              r=[kq, k(c['qm']), k(s['geq'])], w=[k(s['gqm'])])
            yield
        V(lambda e: e.tensor_tensor(s['gkT'][:], bq[0:64, 256:512], s['gek'][:], ALU.mult), r=[kq, k(s['gek'])], w=[k(s['gkT'])])
        yield
        bt, kt = self.bank('g')
        pb = bt[:].bitcast(BF16)
        for pr in range(2):
            M(lambda e, pr=pr: e.transpose(pb[:, pr * 64:(pr + 1) * 64], s['gkT'][:, pr * 128:(pr + 1) * 128], c['identb'][0:64, 0:64]),
              r=[k(s['gkT']), k(c['identb'])], w=[kt])
        if first:
            G(lambda e: e.memset(s['gktm'][:], 0.0), w=[k(s['gktm'])])
            yield
        for hh in range(2):
            V(lambda e, hh=hh: e.tensor_copy(s['gktm'][:, hh, :].rearrange("p (a b c) -> p a b c", a=2, b=2)[:, :, hh, :],
                                             pb[:, 0:128].rearrange("p (a b c) -> p a b c", a=2, b=2)[:, :, hh, :]), r=[kt], w=[k(s['gktm'])])
            yield
        G(lambda e: e.tensor_tensor(s['gvm'][:], self.bc(s['gv'][:, :], [128, 2, 256], 1), self.bc(c['hm'][:, :], [128, 2, 256], 2), ALU.mult),
          r=[k(s['gv']), k(c['hm'])], w=[k(s['gvm'])])
        yield
        bs_, ks_ = self.bank('g')
        for h in range(4):
            pr, hh = h // 2, h % 2
            M(lambda e, h=h, pr=pr, hh=hh: e.matmul(bs_[:, h * 128:(h + 1) * 128], lhsT=s['gkT'][:, pr * 128:(pr + 1) * 128], rhs=s['gqm'][:, hh, pr * 128:(pr + 1) * 128],
                                                    start=True, stop=True), r=[k(s['gkT']), k(s['gqm'])], w=[ks_])
        V(lambda e: e.tensor_tensor(s['gsm'][:], bs_[:, :].rearrange("p (a b) -> p a b", b=128), self.bc(c['maskb'][:, :], [128, 4, 128], 1), ALU.mult),
          r=[ks_, k(c['maskb'])], w=[k(s['gsm'])])
        yield
        bu, ku = self.bank('g')
        for cc in range(2):
            for pr in range(2):
                for hh in range(2):
                    h = pr * 2 + hh
                    M(lambda e, cc=cc, h=h, pr=pr, hh=hh: e.matmul(bu[0:64, cc * 128 + pr * 64:cc * 128 + (pr + 1) * 64],
                                                                   lhsT=s['gktm'][:, hh, pr * 64:(pr + 1) * 64], rhs=s['gvm'][:, cc, h * 64:(h + 1) * 64],
                                                                   start=(hh == 0), stop=(hh == 1)), r=[k(s['gktm']), k(s['gvm'])], w=[ku])
        if first:
            G(lambda e: e.memset(s['gS'][:], 0.0), w=[k(s['gS'])])
            yield
        for cc in range(2):
            A(lambda e, cc=cc: e.copy(out=s['gSb'][:, cc, :, :], in_=s['gS'][:]), r=[k(s['gS'])], w=[k(s['gSb'])])
            yield
            V(lambda e, cc=cc: e.tensor_tensor(s['gst'][:], s['gS'][:], bu[0:64, cc * 128:(cc + 1) * 128].rearrange("p (a b) -> p a b", b=64), ALU.add),
              r=[k(s['gS']), ku], w=[k(s['gst'])])
            yield
            V(lambda e, cc=cc: e.tensor_tensor(s['gS'][:], s['gst'][:], self.bc(s['gel'][:, cc::2], [64, 2, 64], 2), ALU.mult),
              r=[k(s['gst']), k(s['gel'])], w=[k(s['gS'])])
            yield
        bo, ko = self.bank('g')
        for h in range(4):
            M(lambda e, h=h: e.matmul(bo[:, h * 64:(h + 1) * 64], lhsT=s['gsm'][:, h, :], rhs=s['gv'][:, h * 64:(h + 1) * 64], start=True, stop=True),
              r=[k(s['gsm']), k(s['gv'])], w=[ko])
        bi, ki = self.bank('g')
        for cc in range(2):
            for h in range(4):
                pr, hh = h // 2, h % 2
                M(lambda e, cc=cc, h=h, pr=pr, hh=hh: e.matmul(bi[cc * 64:(cc + 1) * 64, h * 64:(h + 1) * 64], lhsT=s['gqm'][:, hh, pr * 128 + cc * 64:pr * 128 + (cc + 1) * 64],
                                                               rhs=s['gSb'][:, cc, pr, :], start=True, stop=True), r=[k(s['gqm']), k(s['gSb'])], w=[ki])
        A(lambda e: e.copy(out=s['gsq'][:], in_=bi[:, 0:256]), r=[ki], w=[k(s['gsq'])])
        yield
        V(lambda e: e.tensor_tensor(s['go'][:], s['gsq'][:], bo[:, 0:256], ALU.add), r=[k(s['gsq']), ko], w=[k(s['go'])])
        yield
        A(lambda e: e.activation(out=s['gsq'][:], in_=s['go'][:], func=AF.Square), r=[k(s['go'])], w=[k(s['gsq'])])
        yield
        V(lambda e: e.tensor_reduce(s['grs'][:], s['gsq'][:].rearrange("p (h q) -> p h q", q=64), AX.X, ALU.add), r=[k(s['gsq'])], w=[k(s['grs'])])
        yield
        V(lambda e: e.tensor_scalar(s['grs'][:], s['grs'][:], 1.0 / 64, RMS_EPS, ALU.mult, ALU.add), r=[k(s['grs'])], w=[k(s['grs'])])
        yield
        A(lambda e: e.activation(out=s['grs'][:], in_=s['grs'][:], func=AF.Sqrt), r=[k(s['grs'])], w=[k(s['grs'])])
        yield
        V(lambda e: e.reciprocal(s['grs'][:], s['grs'][:]), r=[k(s['grs'])], w=[k(s['grs'])])
        yield
        V(lambda e: e.tensor_tensor(s['go'][:].rearrange("p (h q) -> p h q", q=64), s['go'][:].rearrange("p (h q) -> p h q", q=64),
                                    self.bc(s['grs'][:, :], [128, 4, 64], 2), ALU.mult), r=[k(s['go']), k(s['grs'])], w=[k(s['go'])])
        yield
        V(lambda e: e.tensor_tensor(s['ycat'][:, 768:1024], s['go'][:], s['gsg'][:], ALU.mult), r=[k(s['go']), k(s['gsg'])], w=[k(s['ycat'])])
        yield

    def rwkv_tile(self, first):
        s, p, c = self.s, self.p, self.c
        V, A, G, M = self.V, self.A, self.G, self.M
        k = lambda t: t.key
        f2 = lambda t, a, b: t[:, a:b, :].rearrange("p a b -> p (a b)")
        h3 = lambda ap: ap.rearrange("p (h q) -> p h q", q=64)
        if first:
            G(lambda e: e.memset(s['rw'][:, :, 0:1], 0.0), w=[k(s['rw'])])
            yield
            G(lambda e: e.memset(s['rST'][:], 0.0), w=[k(s['rST'])])
            yield
            G(lambda e: e.memset(s['rSTb'][:], 0.0), w=[k(s['rSTb'])])
            yield
        else:
            G(lambda e: e.tensor_copy(s['rw'][:, :, 0:1], s['rw'][:, :, 128:129]), r=[k(s['rw'])], w=[k(s['rw'])])
            yield
        for grp, nb in ((0, 4), (4, 3)):
            bx, kx = self.bank('r')
            for j in range(nb):
                self.proj_fm(bx[:, j * 128:(j + 1) * 128], kx, O_RW + (grp + j) * 128, 128)
            A(lambda e, bx=bx, grp=grp, nb=nb: e.copy(out=s['rw'][:, grp:grp + nb, 1:129], in_=bx[:, 0:nb * 128].rearrange("p (a b) -> p a b", b=128)),
              r=[kx], w=[k(s['rw'])])
            yield
        rt1 = s['rt1'][:]
        G(lambda e: e.tensor_tensor(rt1, s['rw'][:, :, 0:128], self.bc(p['mu'][:, :], [128, 7, 128], 2), ALU.mult), r=[k(s['rw']), k(p['mu'])], w=[k(s['rt1'])])
        yield
        V(lambda e: e.tensor_tensor(s['rsh'][:], s['rw'][:, :, 1:129], self.bc(p['omu'][:, :], [128, 7, 128], 2), ALU.mult), r=[k(s['rw']), k(p['omu'])], w=[k(s['rsh'])])
        yield
        V(lambda e: e.tensor_tensor(s['rsh'][:], s['rsh'][:], rt1, ALU.add), r=[k(s['rsh']), k(s['rt1'])], w=[k(s['rsh'])])
        yield
        rT, kT, vT, lr = f2(s['rsh'], 0, 2), f2(s['rsh'], 2, 4), f2(s['rsh'], 4, 6), s['rsh'][:, 6, :]
        ksh = k(s['rsh'])
        A(lambda e: e.activation(out=s['rtw'][:], in_=lr, func=AF.Tanh), r=[ksh], w=[k(s['rtw'])])
        yield
        bw, kw = self.bank('r')
        for b in range(2):
            M(lambda e, b=b: e.matmul(bw[:, b * 128:(b + 1) * 128], lhsT=p['w2p'][:, b * 128:(b + 1) * 128], rhs=s['rtw'][:], start=True, stop=True),
              r=[k(p['w2p']), k(s['rtw'])], w=[kw])
        for b in range(2):
            A(lambda e, b=b: e.activation(out=s['ra1'][:, b * 128:(b + 1) * 128], in_=bw[:, b * 128:(b + 1) * 128], func=AF.Identity, bias=p['w0'][:, b:b + 1]),
              r=[kw, k(p['w0'])], w=[k(s['ra1'])])
            yield
        V(lambda e: e.scalar_tensor_tensor(s['ra2'][:], s['ra1'][:], -1.0, s['ra1'][:], ALU.mult, ALU.max), r=[k(s['ra1'])], w=[k(s['ra2'])])
        yield
        A(lambda e: e.activation(out=s['ra2'][:], in_=s['ra2'][:], func=AF.Exp, scale=-1.0), r=[k(s['ra2'])], w=[k(s['ra2'])])
        yield
        A(lambda e: e.activation(out=s['ra2'][:], in_=s['ra2'][:], func=AF.Ln, bias=1.0), r=[k(s['ra2'])], w=[k(s['ra2'])])
        yield
        V(lambda e: e.tensor_scalar(s['ra3'][:], s['ra1'][:], -1.0, 0.0, ALU.mult, ALU.max), r=[k(s['ra1'])], w=[k(s['ra3'])])
        yield
        V(lambda e: e.tensor_tensor(s['ra3'][:], s['ra3'][:], s['ra2'][:], ALU.add), r=[k(s['ra3']), k(s['ra2'])], w=[k(s['ra3'])])
        yield
        A(lambda e: e.activation(out=s['ra1'][:], in_=s['ra3'][:], func=AF.Exp, scale=-1.0), r=[k(s['ra3'])], w=[k(s['ra1'])])
        yield
        V(lambda e: e.tensor_scalar(s['ra1'][:], s['ra1'][:], -float(np.exp(-0.5)), None, ALU.mult), r=[k(s['ra1'])], w=[k(s['ra1'])])
        yield
        V(lambda e: e.tensor_tensor_scan(s['rcw'][:], c['rmask'][:], s['ra1'][:], 0.0, ALU.mult, ALU.add), r=[k(c['rmask']), k(s['ra1'])], w=[k(s['rcw'])])
        yield
        V(lambda e: e.tensor_tensor(s['ra2'][:], s['rcw'][:], s['ra1'][:], ALU.subtract), r=[k(s['rcw']), k(s['ra1'])], w=[k(s['ra2'])])
        yield
        A(lambda e: e.activation(out=s['recw'][:], in_=s['rcw'][:], func=AF.Exp), r=[k(s['rcw'])], w=[k(s['recw'])])
        yield
        A(lambda e: e.activation(out=s['reicw'][:], in_=s['rcw'][:], func=AF.Exp, scale=-1.0), r=[k(s['rcw'])], w=[k(s['reicw'])])
        yield
        A(lambda e: e.activation(out=s['recwp'][:], in_=s['ra2'][:], func=AF.Exp), r=[k(s['ra2'])], w=[k(s['recwp'])])
        yield
        ba_, ka_ = self.bank('r')
        for b in range(2):
            M(lambda e, b=b: e.matmul(ba_[:, b * 128:(b + 1) * 128], lhsT=p['a2p'][:, b * 128:(b + 1) * 128], rhs=lr, start=True, stop=True),
              r=[k(p['a2p']), ksh], w=[ka_])
        for b in range(2):
            A(lambda e, b=b: e.activation(out=s['ra'][:, b * 128:(b + 1) * 128], in_=ba_[:, b * 128:(b + 1) * 128], func=AF.Sigmoid, bias=p['a0'][:, b:b + 1]),
              r=[ka_, k(p['a0'])], w=[k(s['ra'])])
            yield
        A(lambda e: e.activation(out=s['rsg'][:], in_=lr, func=AF.Sigmoid), r=[ksh], w=[k(s['rsg'])])
        yield
        bg, kg = self.bank('r')
        M(lambda e: e.matmul(bg[:, 0:256], lhsT=s['rsg'][:], rhs=p['g2p'][:], start=True, stop=True), r=[k(s['rsg']), k(p['g2p'])], w=[kg])
        A(lambda e: e.copy(out=s['rg'][:], in_=bg[:, 0:256]), r=[kg], w=[k(s['rg'])])
        yield
        V(lambda e: e.tensor_tensor(s['rkk'][:].rearrange("p (a b) -> p a b", b=128), kT.rearrange("p (a b) -> p a b", b=128), self.bc(p['kk'][:, :], [128, 2, 128], 2), ALU.mult),
          r=[ksh, k(p['kk'])], w=[k(s['rkk'])])
        yield
        A(lambda e: e.activation(out=s['ra2'][:], in_=s['rkk'][:], func=AF.Square), r=[k(s['rkk'])], w=[k(s['ra2'])])
        yield
        bn, kn = self.bank('r')
        for b in range(2):
            M(lambda e, b=b: e.matmul(bn[:, b * 128:(b + 1) * 128], lhsT=c['bones'][:], rhs=s['ra2'][:, b * 128:(b + 1) * 128], start=True, stop=True),
              r=[k(c['bones']), k(s['ra2'])], w=[kn])
        V(lambda e: e.tensor_scalar(s['ra3'][:], bn[:, 0:256], 1e-12, None, ALU.add), r=[kn], w=[k(s['ra3'])])
        yield
        A(lambda e: e.activation(out=s['ra3'][:], in_=s['ra3'][:], func=AF.Sqrt), r=[k(s['ra3'])], w=[k(s['ra3'])])
        yield
        V(lambda e: e.reciprocal(s['ra3'][:], s['ra3'][:]), r=[k(s['ra3'])], w=[k(s['ra3'])])
        yield
        V(lambda e: e.tensor_tensor(s['rkk'][:], s['rkk'][:], s['ra3'][:], ALU.mult), r=[k(s['rkk']), k(s['ra3'])], w=[k(s['rkk'])])
        yield
        for b in range(2):
            V(lambda e, b=b: e.tensor_scalar(s['ra2'][:, b * 128:(b + 1) * 128], s['ra'][:, b * 128:(b + 1) * 128], p['ka'][:, b:b + 1], p['omka'][:, b:b + 1], ALU.mult, ALU.add),
              r=[k(s['ra']), k(p['ka']), k(p['omka'])], w=[k(s['ra2'])])
            yield
        V(lambda e: e.tensor_tensor(s['rkp'][:], kT, s['ra2'][:], ALU.mult), r=[ksh, k(s['ra2'])], w=[k(s['rkp'])])
        yield
        V(lambda e: e.tensor_tensor(s['ra3'][:], s['rkk'][:], s['ra'][:], ALU.mult), r=[k(s['rkk']), k(s['ra'])], w=[k(s['ra3'])])
        yield
        for hh in range(2):
            V(lambda e, hh=hh: e.scalar_tensor_tensor(s['rAm'][:, hh, :], s['rkk'][:], c['nhm'][:, hh:hh + 1], s['recwp'][:], ALU.mult, ALU.mult),
              r=[k(s['rkk']), k(c['nhm']), k(s['recwp'])], w=[k(s['rAm'])])
            yield
            V(lambda e, hh=hh: e.scalar_tensor_tensor(s['rRm'][:, hh, :], rT, c['hm'][:, hh:hh + 1], s['recw'][:], ALU.mult, ALU.mult),
              r=[ksh, k(c['hm']), k(s['recw'])], w=[k(s['rRm'])])
            yield
        G(lambda e: e.tensor_tensor(s['rBt'][:], s['ra3'][:], s['reicw'][:], ALU.mult), r=[k(s['ra3']), k(s['reicw'])], w=[k(s['rBt'])])
        yield
        G(lambda e: e.tensor_tensor(s['rKt'][:], s['rkp'][:], s['reicw'][:], ALU.mult), r=[k(s['rkp']), k(s['reicw'])], w=[k(s['rKt'])])
        yield
        V(lambda e: e.tensor_tensor(s['ra2'][:], rT, s['rkp'][:], ALU.mult), r=[ksh, k(s['rkp'])], w=[k(s['ra2'])])
        yield
        V(lambda e: e.tensor_tensor(s['ra2'][:].rearrange("p (a b) -> p a b", b=128), s['ra2'][:].rearrange("p (a b) -> p a b", b=128), self.bc(p['rk'][:, :], [128, 2, 128], 2), ALU.mult),
          r=[k(s['ra2']), k(p['rk'])], w=[k(s['ra2'])])
        yield
        bb, kb = self.bank('r')
        for b in range(2):
            M(lambda e, b=b: e.matmul(bb[:, b * 2:(b + 1) * 2], lhsT=s['ra2'][:, b * 128:(b + 1) * 128], rhs=c['hm'][:, :], start=True, stop=True),
              r=[k(s['ra2']), k(c['hm'])], w=[kb])
        A(lambda e: e.copy(out=s['rbc'][:], in_=bb[:, 0:4]), r=[kb], w=[k(s['rbc'])])
        yield
        bt, kt = self.bank('r')
        for b in range(2):
            M(lambda e, b=b: e.transpose(bt[:, b * 128:(b + 1) * 128], s['rsh'][:, 4 + b, :], c['identf'][:]), r=[ksh, k(c['identf'])], w=[kt])
        A(lambda e: e.copy(out=s['rvtm'][:], in_=bt[:, 0:256]), r=[kt], w=[k(s['rvtm'])])
        yield
        bt, kt = self.bank('r')
        for cc in range(2):
            for b in range(2):
                M(lambda e, bt=bt, cc=cc, b=b: e.transpose(bt[0:64, cc * 256 + b * 128:cc * 256 + (b + 1) * 128], s['rsh'][:, 4 + b, cc * 64:(cc + 1) * 64], c['identf'][:]),
                  r=[ksh, k(c['identf'])], w=[kt])
        A(lambda e, bt=bt: e.copy(out=s['rvc'][:].rearrange("p a b -> p (a b)"), in_=bt[0:64, :]), r=[kt], w=[k(s['rvc'])])
        yield
        for srct, dst in ((s['rBt'], s['rBc']), (s['rKt'], s['rKc'])):
            bt, kt = self.bank('r')
            pbt = bt[:].bitcast(BF16)
            for cc in range(2):
                for b in range(2):
                    M(lambda e, pbt=pbt, srct=srct, cc=cc, b=b: e.transpose(pbt[0:64, cc * 256 + b * 128:cc * 256 + (b + 1) * 128], srct[:, b * 128 + cc * 64:b * 128 + (cc + 1) * 64], c['identb'][:]),
                      r=[k(srct), k(c['identb'])], w=[kt])
            V(lambda e, pbt=pbt, dst=dst: e.tensor_copy(dst[:].rearrange("p a b -> p (a b)"), pbt[0:64, 0:512]), r=[kt], w=[k(dst)])
            yield
        def amat(lt, lkey, lhh, rt_, rkey, rhh, mask, dst):
            bA, kA = self.bank('r')
            for cc in range(2):
                for h in range(4):
                    b, hh = h // 2, h % 2
                    sl_ = slice(b * 128 + cc * 64, b * 128 + (cc + 1) * 64)
                    la = lt[:, hh, sl_] if lhh else lt[:, sl_]
                    ra_ = rt_[:, hh, sl_] if rhh else rt_[:, sl_]
                    i8 = cc * 4 + h
                    M(lambda e, bA=bA, la=la, ra_=ra_, i8=i8: e.matmul(bA[0:64, i8 * 64:(i8 + 1) * 64], lhsT=la, rhs=ra_, start=True, stop=True), r=[lkey, rkey], w=[kA])
            V(lambda e, bA=bA: e.tensor_tensor(dst[:], h3(bA[0:64, :]), self.bc(mask, [64, 8, 64], 1), ALU.mult), r=[kA, k(c['su'])], w=[k(dst)])
            yield
        kAm, kRm, kBt, kKt = k(s['rAm']), k(s['rRm']), k(s['rBt']), k(s['rKt'])
        yield from amat(s['rAm'], kAm, True, s['rBt'], kBt, False, c['su'][0:64, 0:64], s['rP'])
        yield from amat(s['rBt'], kBt, False, s['rAm'], kAm, True, c['sl'][0:64, 0:64], s['rQ'])
        yield from amat(s['rKt'], kKt, False, s['rAm'], kAm, True, c['sl'][0:64, 0:64], s['rAak'])
        yield from amat(s['rBt'], kBt, False, s['rRm'], kRm, True, c['tri'][0:64, 0:64], s['rArb'])
        yield from amat(s['rKt'], kKt, False, s['rRm'], kRm, True, c['tri'][0:64, 0:64], s['rArk'])
        V(lambda e: e.tensor_tensor(s['rTT'][:], s['rQ'][:], self.bc(c['identf'][0:64, 0:64], [64, 8, 64], 1), ALU.add), r=[k(s['rQ']), k(c['identf'])], w=[k(s['rTT'])])
        yield
        Pc, Qc, Pn, Qn = s['rP'], s['rQ'], s['rP2'], s['rQ2']
        for lvl in range(5):
            bP, kP = self.bank('r')
            for i8 in range(8):
                M(lambda e, bP=bP, i8=i8, Pc=Pc, Qc=Qc: e.matmul(bP[0:64, i8 * 64:(i8 + 1) * 64], lhsT=Qc[:, i8, :], rhs=Pc[:, i8, :], start=True, stop=True),
                  r=[k(Pc), k(Qc)], w=[kP])
            A(lambda e, bP=bP, Pn=Pn: e.copy(out=Pn[:], in_=h3(bP[0:64, :])), r=[kP], w=[k(Pn)])
            yield
            if lvl < 4:
                bQ, kQ = self.bank('r')
                for i8 in range(8):
                    M(lambda e, bQ=bQ, i8=i8, Pc=Pc, Qc=Qc: e.matmul(bQ[0:64, i8 * 64:(i8 + 1) * 64], lhsT=Pc[:, i8, :], rhs=Qc[:, i8, :], start=True, stop=True),
                      r=[k(Pc), k(Qc)], w=[kQ])
                V(lambda e, bQ=bQ, Qn=Qn: e.tensor_copy(Qn[:], h3(bQ[0:64, :])), r=[kQ], w=[k(Qn)])
                yield
            bT, kT_ = self.bank('r')
            for i8 in range(8):
                M(lambda e, bT=bT, i8=i8, Pn=Pn: e.matmul(bT[0:64, i8 * 64:(i8 + 1) * 64], lhsT=Pn[:, i8, :], rhs=s['rTT'][:, i8, :], start=True, stop=True),
                  r=[k(Pn), k(s['rTT'])], w=[kT_])
            V(lambda e, bT=bT: e.tensor_tensor(s['rTT'][:], s['rTT'][:], h3(bT[0:64, :]), ALU.add), r=[k(s['rTT']), kT_], w=[k(s['rTT'])])
            yield
            Pc, Qc, Pn, Qn = Pn, Qn, Pc, Qc
        bG, kG = self.bank('r')
        for cc in range(2):
            for h in range(4):
                i8 = cc * 4 + h
                M(lambda e, cc=cc, h=h, i8=i8: e.matmul(bG[0:64, i8 * 64:(i8 + 1) * 64], lhsT=s['rAak'][:, i8, :], rhs=s['rvc'][:, cc, h * 64:(h + 1) * 64], start=True, stop=True),
                  r=[k(s['rAak']), k(s['rvc'])], w=[kG])
        A(lambda e: e.copy(out=s['rAak'][:], in_=h3(bG[0:64, :])), r=[kG], w=[k(s['rAak'])])
        yield
        ewc = s['recw'][:].rearrange("p (a b) -> p a b", b=64)[:, :, 63]
        for cc in range(2):
            bG1, kG1 = self.bank('r')
            for h in range(4):
                b, hh = h // 2, h % 2
                sl_ = slice(b * 128 + cc * 64, b * 128 + (cc + 1) * 64)
                M(lambda e, bG1=bG1, h=h, b=b, hh=hh, sl_=sl_: e.matmul(bG1[0:64, h * 64:(h + 1) * 64], lhsT=s['rAm'][:, hh, sl_], rhs=s['rSTb'][:, b, :], start=True, stop=True),
                  r=[kAm, k(s['rSTb'])], w=[kG1])
            bY1, kY1 = self.bank('r')
            for h in range(4):
                b, hh = h // 2, h % 2
                sl_ = slice(b * 128 + cc * 64, b * 128 + (cc + 1) * 64)
                M(lambda e, bY1=bY1, h=h, b=b, hh=hh, sl_=sl_, cc=cc: e.matmul(bY1[cc * 64:(cc + 1) * 64, h * 64:(h + 1) * 64], lhsT=s['rRm'][:, hh, sl_], rhs=s['rSTb'][:, b, :], start=True, stop=True),
                  r=[kRm, k(s['rSTb'])], w=[kY1])
            A(lambda e, bY1=bY1, cc=cc: e.copy(out=s['rY1'][cc * 64:(cc + 1) * 64, :], in_=bY1[cc * 64:(cc + 1) * 64, 0:256]), r=[kY1], w=[k(s['rY1'])])
            yield
            V(lambda e, bG1=bG1, cc=cc: e.tensor_tensor(s['rG'][:], s['rAak'][:, cc * 4:(cc + 1) * 4, :], h3(bG1[0:64, 0:256]), ALU.add), r=[k(s['rAak']), kG1], w=[k(s['rG'])])
            yield
            bU, kU = self.bank('r')
            for h in range(4):
                i8 = cc * 4 + h
                M(lambda e, bU=bU, h=h, i8=i8: e.matmul(bU[0:64, h * 64:(h + 1) * 64], lhsT=s['rTT'][:, i8, :], rhs=s['rG'][:, h, :], start=True, stop=True),
                  r=[k(s['rTT']), k(s['rG'])], w=[kU])
            A(lambda e, bU=bU: e.copy(out=s['rU'][:], in_=h3(bU[0:64, 0:256])), r=[kU], w=[k(s['rU'])])
            yield
            bY2, kY2 = self.bank('r')
            for h in range(4):
                i8 = cc * 4 + h
                M(lambda e, bY2=bY2, h=h, i8=i8, cc=cc: e.matmul(bY2[cc * 64:(cc + 1) * 64, h * 64:(h + 1) * 64], lhsT=s['rArb'][:, i8, :], rhs=s['rU'][:, h, :], start=True, stop=False),
                  r=[k(s['rArb']), k(s['rU'])], w=[kY2])
                M(lambda e, bY2=bY2, h=h, i8=i8, cc=cc: e.matmul(bY2[cc * 64:(cc + 1) * 64, h * 64:(h + 1) * 64], lhsT=s['rArk'][:, i8, :], rhs=s['rvc'][:, cc, h * 64:(h + 1) * 64], start=False, stop=True),
                  r=[k(s['rArk']), k(s['rvc'])], w=[kY2])
            V(lambda e, bY2=bY2, cc=cc: e.tensor_tensor(s['rY'][cc * 64:(cc + 1) * 64, :], s['rY1'][cc * 64:(cc + 1) * 64, :], bY2[cc * 64:(cc + 1) * 64, 0:256], ALU.add),
              r=[k(s['rY1']), kY2], w=[k(s['rY'])])
            yield
            bS, kS = self.bank('r')
            for h in range(4):
                b, hh = h // 2, h % 2
                i8 = cc * 4 + h
                M(lambda e, bS=bS, h=h, b=b, hh=hh, cc=cc: e.matmul(bS[hh * 64:(hh + 1) * 64, b * 64:(b + 1) * 64], lhsT=s['rBc'][:, cc, h * 64:(h + 1) * 64], rhs=s['rU'][:, h, :], start=True, stop=False),
                  r=[k(s['rBc']), k(s['rU'])], w=[kS])
                M(lambda e, bS=bS, h=h, b=b, hh=hh, cc=cc: e.matmul(bS[hh * 64:(hh + 1) * 64, b * 64:(b + 1) * 64], lhsT=s['rKc'][:, cc, h * 64:(h + 1) * 64], rhs=s['rvc'][:, cc, h * 64:(h + 1) * 64], start=False, stop=True),
                  r=[k(s['rKc']), k(s['rvc'])], w=[kS])
            V(lambda e, bS=bS: e.tensor_tensor(s['rt2'][:], s['rST'][:], h3(bS[:, 0:128]), ALU.add), r=[k(s['rST']), kS], w=[k(s['rt2'])])
            yield
            V(lambda e, cc=cc: e.tensor_tensor(s['rST'][:], s['rt2'][:], self.bc(ewc[:, cc::2], [128, 2, 64], 2), ALU.mult), r=[k(s['rt2']), k(s['recw'])], w=[k(s['rST'])])
            yield
            A(lambda e: e.copy(out=s['rSTb'][:], in_=s['rST'][:]), r=[k(s['rST'])], w=[k(s['rSTb'])])
            yield
        V(lambda e: e.tensor_reduce(s['rm1'][:], h3(s['rY'][:]), AX.X, ALU.add), r=[k(s['rY'])], w=[k(s['rm1'])])
        yield
        A(lambda e: e.activation(out=s['rY1'][:], in_=s['rY'][:], func=AF.Square), r=[k(s['rY'])], w=[k(s['rY1'])])
        yield
        V(lambda e: e.tensor_reduce(s['rm2'][:], h3(s['rY1'][:]), AX.X, ALU.add), r=[k(s['rY1'])], w=[k(s['rm2'])])
        yield
        V(lambda e: e.tensor_scalar(s['rm1'][:], s['rm1'][:], 1.0 / 64, None, ALU.mult), r=[k(s['rm1'])], w=[k(s['rm1'])])
        yield
        V(lambda e: e.tensor_tensor(s['rvar'][:], s['rm1'][:], s['rm1'][:], ALU.mult), r=[k(s['rm1'])], w=[k(s['rvar'])])
        yield
        V(lambda e: e.scalar_tensor_tensor(s['rvar'][:], s['rm2'][:], 1.0 / 64, s['rvar'][:], ALU.mult, ALU.subtract), r=[k(s['rm2']), k(s['rvar'])], w=[k(s['rvar'])])
        yield
        V(lambda e: e.tensor_scalar(s['rvar'][:], s['rvar'][:], GN_EPS, None, ALU.add), r=[k(s['rvar'])], w=[k(s['rvar'])])
        yield
        A(lambda e: e.activation(out=s['rvar'][:], in_=s['rvar'][:], func=AF.Sqrt), r=[k(s['rvar'])], w=[k(s['rvar'])])
        yield
        V(lambda e: e.reciprocal(s['rvar'][:], s['rvar'][:]), r=[k(s['rvar'])], w=[k(s['rvar'])])
        yield
        V(lambda e: e.tensor_tensor(h3(s['rY'][:]), h3(s['rY'][:]), self.bc(s['rm1'][:, :], [128, 4, 64], 2), ALU.subtract), r=[k(s['rY']), k(s['rm1'])], w=[k(s['rY'])])
        yield
        V(lambda e: e.tensor_tensor(h3(s['rY'][:]), h3(s['rY'][:]), self.bc(s['rvar'][:, :], [128, 4, 64], 2), ALU.mult), r=[k(s['rY']), k(s['rvar'])], w=[k(s['rY'])])
        yield
        G(lambda e: e.tensor_tensor(s['rY'][:], s['rY'][:], p['rlg'][:], ALU.mult), r=[k(s['rY']), k(p['rlg'])], w=[k(s['rY'])])
        yield
        G(lambda e: e.tensor_tensor(s['rY'][:], s['rY'][:], p['rlb'][:], ALU.add), r=[k(s['rY']), k(p['rlb'])], w=[k(s['rY'])])
        yield
        V(lambda e: e.tensor_tensor(h3(s['rY1'][:]), h3(s['rvtm'][:]), self.bc(s['rbc'][:, :], [128, 4, 64], 2), ALU.mult), r=[k(s['rvtm']), k(s['rbc'])], w=[k(s['rY1'])])
        yield
        V(lambda e: e.tensor_tensor(s['rY'][:], s['rY'][:], s['rY1'][:], ALU.add), r=[k(s['rY']), k(s['rY1'])], w=[k(s['rY'])])
        yield
        V(lambda e: e.tensor_tensor(s['ycat'][:, 512:768], s['rY'][:], s['rg'][:], ALU.mult), r=[k(s['rY']), k(s['rg'])], w=[k(s['ycat'])])
        yield

    def mixer_epilogue(self, l, i):
        s, p, c = self.s, self.p, self.c
        V, A, G, M = self.V, self.A, self.G, self.M
        k = lambda t: t.key
        self.load('sp', s['htm'][:], self.h_d[i * 128:(i + 1) * 128, :], k(s['htm']), dkeys=["hd_%d" % i])
        if self.debug:
            self.A(lambda e: e.copy(out=s['tmp'][:], in_=s['ycat'][:]), r=[k(s['ycat'])], w=[k(s['tmp'])])
            yield
            self.store('sp', self.dbg_y[i * 128:(i + 1) * 128, :], s['tmp'][:], k(s['tmp']))
            yield
        bt, kt = self.bank('e')
        pb = bt[:].bitcast(BF16)
        for kc in range(8):
            M(lambda e, kc=kc: e.transpose(pb[:, kc * 128:(kc + 1) * 128], s['ycat'][:, kc * 128:(kc + 1) * 128], c['identb'][:]), r=[k(s['ycat']), k(c['identb'])], w=[kt])
        V(lambda e: e.tensor_copy(s['yT'][:].rearrange("p a b -> p (a b)"), pb), r=[kt], w=[k(s['yT'])])
        yield
        for half in range(2):
            bo, ko = self.bank('e')
            for kc in range(8):
                M(lambda e, bo=bo, kc=kc, half=half: e.matmul(bo[:, :], lhsT=s['yT'][:, kc, :], rhs=p['w_out'][:, kc, half * 512:(half + 1) * 512], start=(kc == 0), stop=(kc == 7)),
                  r=[k(s['yT']), k(p['w_out'])], w=[ko])
            V(lambda e, bo=bo, half=half: e.scalar_tensor_tensor(s['mix'][:, half * 512:(half + 1) * 512], s['htm'][:, half * 512:(half + 1) * 512], ALPHA, bo[:, :], ALU.mult, ALU.add),
              r=[k(s['htm']), ko], w=[k(s['mix'])])
            yield
        yield from self.layernorm(s['mix'], p['l1g'], p['l1b'], s['h1'], s['tmp'])
        tk = "h1d_%d_%d" % (l, i)
        self.store('sp', self.h1_d[i * 128:(i + 1) * 128, :], s['h1'][:], k(s['h1']), dkeys=[tk])
        yield
        self.store('pool', self.h1b_d[i * 128:(i + 1) * 128, :], s['h1'][:], k(s['h1']), dkeys=["h1bd_%d_%d" % (l, i)])
        yield
        if hasattr(self, 'xs_d'):
            yield from self.router_tile(l, i)

    def stage0(self):
        s, p, d = self.s, self.p, self.d
        k = lambda t: t.key
        mark = self.sb_off
        self.load('sp', p['l1g'][:], d['ln_in_g'].ap().partition_broadcast(128), k(p['l1g']))
        self.load('sp', p['l1b'][:], d['ln_in_b'].ap().partition_broadcast(128), k(p['l1b']))
        sets = [dict(x=s['mix'], h=s['htm'], tmp=s['tmp'], hb=s['ycat'], hT=s['hT'], st=self.ln_stats("a"))]
        hb2 = Tile(s['yT'].t.ap().rearrange("p a b -> p (a b)") if False else s['yT'].t, s['yT'].key)
        sets.append(dict(x=s['h1'], h=self.sb([128, D], name="s0_h"), tmp=self.sb([128, D], name="s0_tmp"),
                         hb=hb2, hT=self.sb([128, 8, 128], BF16, "s0_hT"), st=self.ln_stats("b")))

        def body(i):
            B = sets[i % 2]
            self.load('sp', B['x'][:], d['x'][i * 128:(i + 1) * 128, :], k(B['x']))
            yield
            yield from self.layernorm(B['x'], p['l1g'], p['l1b'], B['h'], B['tmp'], B['st'])
            self.store('sp', self.h_d[i * 128:(i + 1) * 128, :], B['h'][:], k(B['h']), dkeys=["hd_%d" % i])
            yield
            hbf = B['hb'][:] if len(B['hb'][:].shape) == 2 else B['hb'][:].rearrange("p a b -> p (a b)")
            self.A(lambda e: e.copy(out=hbf, in_=B['h'][:]), r=[k(B['h'])], w=[k(B['hb'])])
            yield
            bk, bkey = self.bank()
            pb = bk[:].bitcast(BF16)
            for kc in range(8):
                self.M(lambda e, kc=kc: e.transpose(pb[:, kc * 128:(kc + 1) * 128], hbf[:, kc * 128:(kc + 1) * 128], self.c['identb'][:]),
                       r=[k(B['hb']), k(self.c['identb'])], w=[bkey])
            self.V(lambda e: e.tensor_copy(B['hT'][:].rearrange("p a b -> p (a b)"), pb), r=[bkey], w=[k(B['hT'])])
            yield
            self.store('sp', self.hT_d[:, :, i * 128:(i + 1) * 128], B['hT'][:], k(B['hT']), dkeys=["hTd_%d" % i])
            yield
        self.run_pipe([lambda i=i: body(i) for i in range(self.NT)], 2)
        self.sb_off = mark

    def stageM(self, l):
        import os
        s = self.s
        k = lambda t: t.key
        only = os.environ.get("ONLY", "srg")
        prev = None
        for i in range(self.NT + 1):
            gens = []
            if i < self.NT:
                self.load('sp', s['hT'][:], self.hT_d[:, :, i * 128:(i + 1) * 128], k(s['hT']), dkeys=["hTd_%d" % i])
                if 'r' in only:
                    gens.append(self.rwkv_tile(i == 0))
                if 's' in only:
                    gens.append(self.ssd_tile(i == 0))
                if 'g' in only:
                    gens.append(self.gla_tile(i == 0))
            if prev is not None:
                gens.append(self.mixer_epilogue(l, prev))
            prev = i if i < self.NT else None
            wts = [int(x) for x in os.environ.get("ILW", "3,1,1,1").split(",")]
            gw = {id(g_): (wts[0] if j == 0 and i < self.NT and 'r' in only else 1) for j, g_ in enumerate(gens)}
            while gens:
                for g_ in list(gens):
                    for _ in range(gw[id(g_)]):
                        try:
                            next(g_)
                        except StopIteration:
                            gens.remove(g_)
                            break

    def build_mixer_test(self):
        self.declare_inputs()
        T = self.T
        self.h_d = self.dscr("h_d", [T, D])
        self.hT_d = self.dscr("hT_d", [128, 8, T], BF16)
        self.h1_d = self.dout("h1_d", [T, D])
        self.h1b_d = self.dscr("h1b_d", [T, D], BF16)
        self.dbg_y = self.dout("dbg_y", [T, D])
        self.consts()
        self.alloc_params()
        self.alloc_mixer()
        print("sbuf peak", self.sb_peak)
        self.stage0()
        self.P.barrier()
        self.load_params(0)
        self.stageM(0)
        self.P.barrier()
        return self.nc

    def alloc_router(self):
        rt = self.rt = {}
        for n, w in (('lg', 36), ('gmx', 1), ('goh', 4), ('gex', 4), ('gsum', 1), ('t32', 32), ('el8', 8), ('el8m', 8), ('l1', 1), ('l2', 1),
                     ('oh1', 8), ('oh2', 8), ('w1', 1), ('w2', 1), ('E1', 32), ('E2', 32), ('Mm', 32), ('rk', 32)):
            rt[n] = self.sb([128, w], name="rt_" + n)

    def alloc_route_persist(self):
        rp = self.rp = {}
        NT = self.NT
        rp['eid'] = self.sb([128, NT * 2], name="rp_eid")
        rp['rnk'] = self.sb([128, NT * 2], name="rp_rnk")
        rp['gat'] = self.sb([128, NT * 2], name="rp_gat")
        rp['cnt'] = self.sb([128, NE], name="rp_cnt")
        rp['iota32'] = self.sb([128, NE], name="rp_iota32")
        ii = self.sb([128, NE], I32, "rp_iota32i")
        self.G(lambda e: e.iota(ii[:], pattern=[[1, NE]], base=0, channel_multiplier=0), w=[ii.key])
        self.V(lambda e: e.tensor_copy(rp['iota32'][:], ii[:]), r=[ii.key], w=[rp['iota32'].key])

    def router_tile(self, l, i):
        s, p, c = self.s, self.p, self.c
        V, A, G, M = self.V, self.A, self.G, self.M
        k = lambda t: t.key
        rt, rp = self.rt, self.rp
        if i == 0:
            G(lambda e: e.memset(rp['cnt'][:], 0.0), w=[k(rp['cnt'])])
            yield
        hT32 = s['tmp'][:].rearrange("p (a b) -> p a b", b=128)
        for half in range(2):
            bt, kt = self.bank('e')
            for j in range(4):
                kc = half * 4 + j
                M(lambda e, bt=bt, j=j, kc=kc: e.transpose(bt[:, j * 128:(j + 1) * 128], s['h1'][:, kc * 128:(kc + 1) * 128], c['identf'][:]), r=[k(s['h1']), k(c['identf'])], w=[kt])
            A(lambda e, bt=bt, half=half: e.copy(out=s['tmp'][:, half * 512:(half + 1) * 512], in_=bt[:, :]), r=[kt], w=[k(s['tmp'])])
            yield
        bl, kl = self.bank('e')
        for kc in range(8):
            M(lambda e, kc=kc: e.matmul(bl[:, 0:36], lhsT=hT32[:, kc, :], rhs=p['wr'][:, kc, :], start=(kc == 0), stop=(kc == 7)), r=[k(s['tmp']), k(p['wr'])], w=[kl])
        V(lambda e: e.tensor_tensor(rt['lg'][:], bl[:, 0:36], p['rb36'][:], ALU.add), r=[kl, k(p['rb36'])], w=[k(rt['lg'])])
        yield
        V(lambda e: e.tensor_reduce(rt['gmx'][:], rt['lg'][:, 0:4], AX.X, ALU.max), r=[k(rt['lg'])], w=[k(rt['gmx'])])
        yield
        V(lambda e: e.tensor_scalar(rt['goh'][:], rt['lg'][:, 0:4], rt['gmx'][:, 0:1], None, ALU.is_equal), r=[k(rt['lg']), k(rt['gmx'])], w=[k(rt['goh'])])
        yield
        V(lambda e: e.tensor_scalar(rt['gex'][:], rt['lg'][:, 0:4], rt['gmx'][:, 0:1], None, ALU.subtract), r=[k(rt['lg']), k(rt['gmx'])], w=[k(rt['gex'])])
        yield
        A(lambda e: e.activation(out=rt['gex'][:], in_=rt['gex'][:], func=AF.Exp), r=[k(rt['gex'])], w=[k(rt['gex'])])
        yield
        V(lambda e: e.tensor_reduce(rt['gsum'][:], rt['gex'][:], AX.X, ALU.add), r=[k(rt['gex'])], w=[k(rt['gsum'])])
        yield
        V(lambda e: e.reciprocal(rt['gsum'][:], rt['gsum'][:]), r=[k(rt['gsum'])], w=[k(rt['gsum'])])
        yield
        V(lambda e: e.tensor_tensor(rt['t32'][:].rearrange("p (g j) -> p g j", j=8), rt['lg'][:, 4:36].rearrange("p (g j) -> p g j", j=8),
                                    self.bc(rt['goh'][:, :], [128, 4, 8], 2), ALU.mult), r=[k(rt['lg']), k(rt['goh'])], w=[k(rt['t32'])])
        yield
        V(lambda e: e.tensor_reduce(rt['el8'][:], rt['t32'][:].rearrange("p (g j) -> p j g", j=8), AX.X, ALU.add), r=[k(rt['t32'])], w=[k(rt['el8'])])
        yield
        V(lambda e: e.tensor_reduce(rt['l1'][:], rt['el8'][:], AX.X, ALU.max), r=[k(rt['el8'])], w=[k(rt['l1'])])
        yield
        V(lambda e: e.tensor_scalar(rt['oh1'][:], rt['el8'][:], rt['l1'][:, 0:1], None, ALU.is_equal), r=[k(rt['el8']), k(rt['l1'])], w=[k(rt['oh1'])])
        yield
        V(lambda e: e.scalar_tensor_tensor(rt['el8m'][:], rt['oh1'][:], -1e30, rt['el8'][:], ALU.mult, ALU.add), r=[k(rt['oh1']), k(rt['el8'])], w=[k(rt['el8m'])])
        yield
        V(lambda e: e.tensor_reduce(rt['l2'][:], rt['el8m'][:], AX.X, ALU.max), r=[k(rt['el8m'])], w=[k(rt['l2'])])
        yield
        V(lambda e: e.tensor_scalar(rt['oh2'][:], rt['el8m'][:], rt['l2'][:, 0:1], None, ALU.is_equal), r=[k(rt['el8m']), k(rt['l2'])], w=[k(rt['oh2'])])
        yield
        V(lambda e: e.tensor_tensor(rt['w2'][:], rt['l2'][:], rt['l1'][:], ALU.subtract), r=[k(rt['l2']), k(rt['l1'])], w=[k(rt['w2'])])
        yield
        A(lambda e: e.activation(out=rt['w2'][:], in_=rt['w2'][:], func=AF.Exp), r=[k(rt['w2'])], w=[k(rt['w2'])])
        yield
        V(lambda e: e.tensor_scalar(rt['w1'][:], rt['w2'][:], 1.0, None, ALU.add), r=[k(rt['w2'])], w=[k(rt['w1'])])
        yield
        V(lambda e: e.reciprocal(rt['w1'][:], rt['w1'][:]), r=[k(rt['w1'])], w=[k(rt['w1'])])
        yield
        V(lambda e: e.tensor_tensor(rt['w2'][:], rt['w2'][:], rt['w1'][:], ALU.mult), r=[k(rt['w2']), k(rt['w1'])], w=[k(rt['w2'])])
        yield
        V(lambda e: e.tensor_tensor(rp['gat'][:, 2 * i:2 * i + 1], rt['w1'][:], rt['gsum'][:], ALU.mult), r=[k(rt['w1']), k(rt['gsum'])], w=[k(rp['gat'])])
        yield
        V(lambda e: e.tensor_tensor(rp['gat'][:, 2 * i + 1:2 * i + 2], rt['w2'][:], rt['gsum'][:], ALU.mult), r=[k(rt['w2']), k(rt['gsum'])], w=[k(rp['gat'])])
        yield
        for E, oh in ((rt['E1'], rt['oh1']), (rt['E2'], rt['oh2'])):
            V(lambda e, E=E, oh=oh: e.tensor_tensor(E[:].rearrange("p (g j) -> p g j", j=8), self.bc(rt['goh'][:, :], [128, 4, 8], 2), self.bc(oh[:, :], [128, 4, 8], 1), ALU.mult),
              r=[k(rt['goh']), k(oh)], w=[k(E)])
            yield
        V(lambda e: e.tensor_tensor(rt['Mm'][:], rt['E1'][:], rt['E2'][:], ALU.add), r=[k(rt['E1']), k(rt['E2'])], w=[k(rt['Mm'])])
        yield
        br, kr = self.bank('e')
        M(lambda e: e.matmul(br[:, 0:32], lhsT=c['sl'][:], rhs=rt['Mm'][:], start=True, stop=True), r=[k(c['sl']), k(rt['Mm'])], w=[kr])
        M(lambda e: e.matmul(br[:, 32:64], lhsT=c['onesf'][:], rhs=rt['Mm'][:], start=True, stop=True), r=[k(c['onesf']), k(rt['Mm'])], w=[kr])
        V(lambda e: e.tensor_tensor(rt['rk'][:], br[:, 0:32], rp['cnt'][:], ALU.add), r=[kr, k(rp['cnt'])], w=[k(rt['rk'])])
        yield
        V(lambda e: e.tensor_tensor(rp['cnt'][:], rp['cnt'][:], br[:, 32:64], ALU.add), r=[kr, k(rp['cnt'])], w=[k(rp['cnt'])])
        yield
        for j, E in ((0, rt['E1']), (1, rt['E2'])):
            V(lambda e, E=E: e.tensor_tensor(rt['t32'][:], E[:], rt['rk'][:], ALU.mult), r=[k(E), k(rt['rk'])], w=[k(rt['t32'])])
            yield
            V(lambda e, j=j: e.tensor_reduce(rp['rnk'][:, 2 * i + j:2 * i + j + 1], rt['t32'][:], AX.X, ALU.add), r=[k(rt['t32'])], w=[k(rp['rnk'])])
            yield
            V(lambda e, E=E: e.tensor_tensor(rt['t32'][:], E[:], rp['iota32'][:], ALU.mult), r=[k(E), k(rp['iota32'])], w=[k(rt['t32'])])
            yield
            V(lambda e, j=j: e.tensor_reduce(rp['eid'][:, 2 * i + j:2 * i + j + 1], rt['t32'][:], AX.X, ALU.add), r=[k(rt['t32'])], w=[k(rp['eid'])])
            yield

    def stageMoE(self, l, last):
        d, c, rp = self.d, self.c, self.rp
        V, A, G, M = self.V, self.A, self.G, self.M
        k = lambda t: t.key
        mark = self.sb_off
        sb = self.sb
        NT, NB, RB = self.NT, self.NB, self.RB
        NR = RB // 128
        NC = NT * 2
        thr_i = sb([128, 64], I32, "f_thri")
        thr = sb([128, 64], name="f_thr")
        G(lambda e: e.iota(thr_i[:], pattern=[[RB, 64]], base=0, channel_multiplier=0), w=[k(thr_i)])
        V(lambda e: e.tensor_copy(thr[:], thr_i[:]), r=[k(thr_i)], w=[k(thr)])
        big = sb([128, max(NC * NE, NE * 64, NB * NE)], name="f_big")
        nblk = sb([128, NE], name="f_nblk")
        padded = sb([128, NE], name="f_padded")
        pend = sb([128, NE], name="f_pend")
        pstart = sb([128, NE], name="f_pstart")
        cmp3 = big[:, 0:NE * 64].rearrange("p (e m) -> p e m", m=64)
        V(lambda e: e.tensor_tensor(cmp3, self.bc(rp['cnt'][:, :], [128, NE, 64], 2), self.bc(thr[:, :], [128, NE, 64], 1), ALU.is_gt), r=[k(rp['cnt']), k(thr)], w=[k(big)])
        V(lambda e: e.tensor_reduce(nblk[:], cmp3, AX.X, ALU.add), r=[k(big)], w=[k(nblk)])
        V(lambda e: e.tensor_scalar(padded[:], nblk[:], float(RB), None, ALU.mult), r=[k(nblk)], w=[k(padded)])
        V(lambda e: e.tensor_tensor_scan(pend[:], c['onesf'][:, 0:NE], padded[:], 0.0, ALU.mult, ALU.add), r=[k(c['onesf']), k(padded)], w=[k(pend)])
        V(lambda e: e.tensor_tensor(pstart[:], pend[:], padded[:], ALU.subtract), r=[k(pend), k(padded)], w=[k(pstart)])
        oh3 = big[:, 0:NC * NE].rearrange("p (n e) -> p n e", e=NE)
        destf = sb([128, NC], name="f_destf")
        dest = sb([128, NC], I32, "f_dest")
        V(lambda e: e.tensor_tensor(oh3, self.bc(rp['iota32'][:, :], [128, NC, NE], 1), self.bc(rp['eid'][:, :], [128, NC, NE], 2), ALU.is_equal), r=[k(rp['iota32']), k(rp['eid'])], w=[k(big)])
        V(lambda e: e.tensor_tensor(oh3, oh3, self.bc(pstart[:, :], [128, NC, NE], 1), ALU.mult), r=[k(big), k(pstart)], w=[k(big)])
        V(lambda e: e.tensor_reduce(destf[:], oh3, AX.X, ALU.add), r=[k(big)], w=[k(destf)])
        V(lambda e: e.tensor_tensor(destf[:], destf[:], rp['rnk'][:], ALU.add), r=[k(destf), k(rp['rnk'])], w=[k(destf)])
        V(lambda e: e.tensor_copy(dest[:], destf[:]), r=[k(destf)], w=[k(dest)])
        bs_i = sb([128, NB], I32, "f_bsi")
        bstart = sb([128, NB], name="f_bstart")
        be = sb([128, NB], name="f_be")
        G(lambda e: e.iota(bs_i[:], pattern=[[RB, NB]], base=0, channel_multiplier=0), w=[k(bs_i)])
        V(lambda e: e.tensor_copy(bstart[:], bs_i[:]), r=[k(bs_i)], w=[k(bstart)])
        cmpb = big[:, 0:NB * NE].rearrange("p (b e) -> p b e", e=NE)
        V(lambda e: e.tensor_tensor(cmpb, self.bc(pend[:, :], [128, NB, NE], 1), self.bc(bstart[:, :], [128, NB, NE], 2), ALU.is_le), r=[k(pend), k(bstart)], w=[k(big)])
        V(lambda e: e.tensor_reduce(be[:], cmpb, AX.X, ALU.add), r=[k(big)], w=[k(be)])
        V(lambda e: e.tensor_scalar(be[:], be[:], float(NE - 1), None, ALU.min), r=[k(be)], w=[k(be)])
        kp_i = sb([128, 8], I32, "f_kpi")
        kp = sb([128, 8], name="f_kp")
        G(lambda e: e.iota(kp_i[:], pattern=[[128, 8]], base=0, channel_multiplier=1), w=[k(kp_i)])
        V(lambda e: e.tensor_copy(kp[:], kp_i[:]), r=[k(kp_i)], w=[k(kp)])
        widf = sb([128, NB, 8], name="f_widf")
        wid = sb([128, NB, 8], I32, "f_wid")
        didf = sb([128, NB, 4], name="f_didf")
        did = sb([128, NB, 4], I32, "f_did")
        bew = sb([128, NB], name="f_bew")
        inval = sb([128, NB], name="f_inval")
        V(lambda e: e.tensor_scalar(inval[:], bstart[:], pend[:, NE - 1:NE], 1.0e9, ALU.is_ge, ALU.mult), r=[k(bstart), k(pend)], w=[k(inval)])
        V(lambda e: e.tensor_scalar(bew[:], be[:], float(D), float(l * NE * D), ALU.mult, ALU.add), r=[k(be)], w=[k(bew)])
        V(lambda e: e.tensor_tensor(bew[:], bew[:], inval[:], ALU.add), r=[k(bew), k(inval)], w=[k(bew)])
        V(lambda e: e.tensor_tensor(widf[:], self.bc(bew[:, :], [128, NB, 8], 2), self.bc(kp[:, :], [128, NB, 8], 1), ALU.add), r=[k(bew), k(kp)], w=[k(widf)])
        V(lambda e: e.tensor_copy(wid[:], widf[:]), r=[k(widf)], w=[k(wid)])
        V(lambda e: e.tensor_scalar(bew[:], be[:], float(FF), float(l * NE * FF), ALU.mult, ALU.add), r=[k(be)], w=[k(bew)])
        V(lambda e: e.tensor_tensor(bew[:], bew[:], inval[:], ALU.add), r=[k(bew), k(inval)], w=[k(bew)])
        V(lambda e: e.tensor_tensor(didf[:], self.bc(bew[:, :], [128, NB, 4], 2), self.bc(kp[:, 0:4], [128, NB, 4], 1), ALU.add), r=[k(bew), k(kp)], w=[k(didf)])
        V(lambda e: e.tensor_copy(did[:], didf[:]), r=[k(didf)], w=[k(did)])
        hbs = [sb([128, D], BF16, "e_hb%d" % j) for j in range(2)]
        for i in range(NT):
            hb = hbs[i % 2]
            self.load('sp', hb[:], self.h1b_d[i * 128:(i + 1) * 128, :], k(hb), dkeys=["h1bd_%d_%d" % (l, i)])
            for j in range(2):
                col = 2 * i + j
                self.P.dma('pool', lambda e, col=col, hb=hb: e.indirect_dma_start(out=self.xs_d.ap(), out_offset=bass.IndirectOffsetOnAxis(ap=dest[:, col:col + 1], axis=0),
                                                                                  in_=hb[:], in_offset=None), k(hb), r=[k(hb), k(dest)], w=["xs_d"])
        self.P.barrier()
        import os
        mstop = int(os.environ.get('MOESTOP', '9'))
        if mstop <= 1:
            self.sb_off = mark
            return
        wgu = [sb([128, 8, 2 * FF], BF16, "e_wgu%d" % j) for j in range(2)]
        wd = [sb([128, 4, D], BF16, "e_wd%d" % j) for j in range(2)]
        xs = [sb([128, NR, D], BF16, "e_xs%d" % j) for j in range(2)]
        xsT = sb([128, 8, RB], BF16, "e_xsT")
        hT = sb([128, 4, RB], BF16, "e_hT")
        sg = sb([128, RB], name="e_sg")
        ys = [sb([128, D], name="e_ys%d" % j) for j in range(2)]
        tgu, td = d['moe_w_gu'].ap(), d['moe_w_down'].ap()
        if not hasattr(self, '_bc_regs'):
            self._bc_regs = (self.nc.gpsimd.to_reg(DEPTH * NE * D - 1), self.nc.gpsimd.to_reg(DEPTH * NE * FF - 1))
        bc_g, bc_d = self._bc_regs
        nys = 0
        for b in range(NB):
            j = b % 2
            for kc in range(8):
                self.P.dma('pool', lambda e, j=j, b=b, kc=kc: e.indirect_dma_start(out=wgu[j][:, kc, :], out_offset=None, in_=tgu,
                                                                                  in_offset=bass.IndirectOffsetOnAxis(ap=wid[:, b, kc:kc + 1], axis=0), bounds_check=bc_g, oob_is_err=False), k(wgu[j]), r=[k(wid)], w=[k(wgu[j])])
            for fc in range(4):
                self.P.dma('pool', lambda e, j=j, b=b, fc=fc: e.indirect_dma_start(out=wd[j][:, fc, :], out_offset=None, in_=td,
                                                                                  in_offset=bass.IndirectOffsetOnAxis(ap=did[:, b, fc:fc + 1], axis=0), bounds_check=bc_d, oob_is_err=False), k(wd[j]), r=[k(did)], w=[k(wd[j])])
            self.load('sp', xs[j][:], self.xs_d[b * RB:(b + 1) * RB, :].rearrange("(r p) n -> p r n", p=128), k(xs[j]), dkeys=["xs_d"])
            for r_ in range(NR):
                bt, kt = self.bank()
                pb = bt[:].bitcast(BF16)
                for kc in range(8):
                    M(lambda e, pb=pb, j=j, r_=r_, kc=kc: e.transpose(pb[:, kc * 128:(kc + 1) * 128], xs[j][:, r_, kc * 128:(kc + 1) * 128], c['identb'][:]), r=[k(xs[j]), k(c['identb'])], w=[kt])
                V(lambda e, pb=pb, r_=r_: e.tensor_copy(xsT[:, :, r_ * 128:(r_ + 1) * 128], pb.rearrange("p (a b) -> p a b", b=128)), r=[kt], w=[k(xsT)])
            for fc in range(4):
                bg, kg = self.bank()
                for kc in range(8):
                    M(lambda e, bg=bg, kc=kc, fc=fc, j=j: e.matmul(bg[:, 0:RB], lhsT=wgu[j][:, kc, fc * 128:(fc + 1) * 128], rhs=xsT[:, kc, :], start=(kc == 0), stop=(kc == 7)),
                      r=[k(wgu[j]), k(xsT)], w=[kg])
                bu, ku = self.bank()
                for kc in range(8):
                    M(lambda e, bu=bu, kc=kc, fc=fc, j=j: e.matmul(bu[:, 0:RB], lhsT=wgu[j][:, kc, FF + fc * 128:FF + (fc + 1) * 128], rhs=xsT[:, kc, :], start=(kc == 0), stop=(kc == 7)),
                      r=[k(wgu[j]), k(xsT)], w=[ku])
                A(lambda e, bg=bg: e.activation(out=sg[:], in_=bg[:, 0:RB], func=AF.Silu), r=[kg], w=[k(sg)])
                V(lambda e, bu=bu, fc=fc: e.tensor_tensor(hT[:, fc, :], sg[:], bu[:, 0:RB], ALU.mult), r=[k(sg), ku], w=[k(hT)])
            for r_ in range(NR):
                yb = ys[nys % 2]
                nys += 1
                for half in range(2):
                    bo, ko = self.bank()
                    for fc in range(4):
                        M(lambda e, bo=bo, fc=fc, r_=r_, half=half, j=j: e.matmul(bo[:, :], lhsT=hT[:, fc, r_ * 128:(r_ + 1) * 128], rhs=wd[j][:, fc, half * 512:(half + 1) * 512],
                                                                                 start=(fc == 0), stop=(fc == 3)), r=[k(hT), k(wd[j])], w=[ko])
                    if half == 0:
                        A(lambda e, bo=bo, yb=yb: e.copy(out=yb[:, 0:512], in_=bo[:, :]), r=[ko], w=[k(yb)])
                    else:
                        V(lambda e, bo=bo, yb=yb: e.tensor_copy(yb[:, 512:1024], bo[:, :]), r=[ko], w=[k(yb)])
                r0 = b * RB + r_ * 128
                self.store('sp', self.ys_d[r0:r0 + 128, :], yb[:], k(yb), dkeys=["ys_d"])
        self.P.barrier()
        if mstop <= 2:
            self.sb_off = mark
            return
        l2g = sb([128, D], name="c_l2g")
        l2b = sb([128, D], name="c_l2b")
        self.load('sp', l2g[:], d['ln2_g'][l].partition_broadcast(128), k(l2g))
        self.load('sp', l2b[:], d['ln2_b'][l].partition_broadcast(128), k(l2b))
        csets = []
        for j in range(2):
            csets.append(dict(h1=sb([128, D], name="c_h1%d" % j), y0=sb([128, D], name="c_y0%d" % j), y1=sb([128, D], name="c_y1%d" % j),
                              tmp=sb([128, D], name="c_tmp%d" % j), h2=sb([128, D], name="c_h2%d" % j), hb=sb([128, D], BF16, "c_hb%d" % j),
                              hT=sb([128, 8, 128], BF16, "c_hT%d" % j), st=self.ln_stats("c%d" % j)))

        def cbody(i):
            B = csets[i % 2]
            h1, y0, y1 = B['h1'], B['y0'], B['y1']
            self.load('sp', h1[:], self.h1_d[i * 128:(i + 1) * 128, :], k(h1), dkeys=["h1d_%d_%d" % (l, i)])
            for j, yt in ((0, y0), (1, y1)):
                col = 2 * i + j
                self.P.dma('pool', lambda e, col=col, yt=yt: e.indirect_dma_start(out=yt[:], out_offset=None, in_=self.ys_d.ap(),
                                                                                 in_offset=bass.IndirectOffsetOnAxis(ap=dest[:, col:col + 1], axis=0)), k(yt), r=[k(dest), "ys_d"], w=[k(yt)])
            yield
            V(lambda e: e.tensor_scalar(y0[:], y0[:], rp['gat'][:, 2 * i:2 * i + 1], None, ALU.mult), r=[k(y0), k(rp['gat'])], w=[k(y0)])
            yield
            V(lambda e: e.scalar_tensor_tensor(y0[:], y1[:], rp['gat'][:, 2 * i + 1:2 * i + 2], y0[:], ALU.mult, ALU.add), r=[k(y1), k(y0), k(rp['gat'])], w=[k(y0)])
            yield
            V(lambda e: e.scalar_tensor_tensor(h1[:], h1[:], ALPHA, y0[:], ALU.mult, ALU.add), r=[k(h1), k(y0)], w=[k(h1)])
            yield
            yield from self.layernorm(h1, l2g, l2b, B['h2'], B['tmp'], B['st'])
            if last:
                self.store('sp', self.out_d[i * 128:(i + 1) * 128, :], B['h2'][:], k(B['h2']), dkeys=["out_%d" % i])
                yield
            else:
                self.store('sp', self.h_d[i * 128:(i + 1) * 128, :], B['h2'][:], k(B['h2']), dkeys=["hd_%d" % i])
                yield
                A(lambda e: e.copy(out=B['hb'][:], in_=B['h2'][:]), r=[k(B['h2'])], w=[k(B['hb'])])
                yield
                bk, bkey = self.bank()
                pb = bk[:].bitcast(BF16)
                for kc in range(8):
                    M(lambda e, kc=kc: e.transpose(pb[:, kc * 128:(kc + 1) * 128], B['hb'][:, kc * 128:(kc + 1) * 128], c['identb'][:]), r=[k(B['hb']), k(c['identb'])], w=[bkey])
                V(lambda e: e.tensor_copy(B['hT'][:].rearrange("p a b -> p (a b)"), pb), r=[bkey], w=[k(B['hT'])])
                yield
                self.store('sp', self.hT_d[:, :, i * 128:(i + 1) * 128], B['hT'][:], k(B['hT']), dkeys=["hTd_%d" % i])
                yield
        self.run_pipe([lambda i=i: cbody(i) for i in range(NT)], 2)
        self.P.barrier()
        self.sb_off = mark

    def build_full(self):
        self.declare_inputs()
        T = self.T
        self.h_d = self.dscr("h_d", [T, D])
        self.hT_d = self.dscr("hT_d", [128, 8, T], BF16)
        self.h1_d = self.dscr("h1_d", [T, D])
        self.h1b_d = self.dscr("h1b_d", [T, D], BF16)
        self.xs_d = self.dscr("xs_d", [self.NB * self.RB, D], BF16)
        self.ys_d = self.dscr("ys_d", [self.NB * self.RB, D])
        self.out_d = self.dout("out", [T, D])
        if self.debug:
            self.dbg_y = self.dout("dbg_y", [T, D])
        self.consts()
        self.alloc_route_persist()
        base = self.sb_off
        for l in range(self.depth):
            self.sb_off = base
            self.alloc_params()
            self.alloc_mixer()
            self.alloc_router()
            if l == 0:
                self.stage0()
                self.P.barrier()
            self.load_params(l)
            self.stageM(l)
            self.P.barrier()
            self.sb_off = base
            self.stageMoE(l, l == self.depth - 1)
        self.P.barrier()
        return self.nc


def _host_inputs(inputs, b, T):
    m = {}
    for k, v in inputs.items():
        v = np.asarray(v)
        if k == 'x':
            m[k] = np.ascontiguousarray(v[b, :T])
        elif k == 'rwkv_r_k':
            m[k] = np.ascontiguousarray(v.reshape(DEPTH, 256))
        elif k == 'moe_w_gate':
            m['moe_w_gu'] = np.concatenate([v.reshape(DEPTH * NE * D, FF), np.asarray(inputs['moe_w_up']).reshape(DEPTH * NE * D, FF)], axis=1)
        elif k == 'moe_w_up':
            pass
        elif k == 'moe_w_down':
            m[k] = np.ascontiguousarray(v.reshape(DEPTH * NE * FF, D))
        else:
            m[k] = np.ascontiguousarray(v)
    return m


def kernel(**inputs):
    x = np.asarray(inputs['x'])
    Bsz, T, _ = x.shape
    bld = Builder(T)
    nc = bld.build_full()
    in_maps = [_host_inputs(inputs, b, T) for b in range(Bsz)]
    res = run_bass_kernel_spmd(nc, in_maps, core_ids=list(range(Bsz)))
    return np.stack([np.asarray(r["out"]) for r in res.results], axis=0).astype(np.float32)
```

```python
import numpy as np
import concourse.bass as bass
import concourse.mybir as mybir
from concourse.bass_utils import run_bass_kernel_spmd

F32 = mybir.dt.float32
BF16 = mybir.dt.bfloat16
I32 = mybir.dt.int32
U32 = mybir.dt.uint32
AF = mybir.ActivationFunctionType
ALU = mybir.AluOpType
AX = mybir.AxisListType

D = 1024
NIN = 2968
DEPTH = 2
ALPHA = (2 * DEPTH) ** 0.25
LN_EPS = 1e-5
RMS_EPS = 1e-6
GN_EPS = 64e-5
NE = 32
FF = 512
O_Z, O_XBC, O_DT, O_RW, O_GQ, O_GK, O_GV, O_GG, O_GA = 0, 512, 1280, 1288, 2184, 2312, 2440, 2696, 2952


class Prog:
    EPOCH = 8192
    NDMA = 40

    def __init__(self, nc):
        self.nc = nc
        self.eng = {'pe': nc.tensor, 'dve': nc.vector, 'act': nc.scalar, 'pool': nc.gpsimd, 'sp': nc.sync}
        self.esems = {n: [] for n in ('pe', 'dve', 'act', 'pool')}
        self.cnt = {n: 0 for n in ('pe', 'dve', 'act', 'pool')}
        self.dsems, self.dval, self.dkey = [], [], {}
        self.waited = {n: {} for n in self.eng}
        self.lastw, self.readers = {}, {}
        self.ps_last = {}
        self.ninst = 0

    def _esem(self, X, ep):
        while len(self.esems[X]) <= ep:
            self.esems[X].append(self.nc.alloc_semaphore("s_%s_%d" % (X, len(self.esems[X]))))
        return self.esems[X][ep]

    def _deps(self, reads, writes):
        deps = {}

        def add(ev):
            if ev is not None and deps.get(ev[0], 0) < ev[1]:
                deps[ev[0]] = ev[1]
        for r in reads:
            add(self.lastw.get(r))
        for w in writes:
            add(self.lastw.get(w))
            for k, v in self.readers.get(w, {}).items():
                add((k, v))
        return deps

    def _wait(self, X, deps):
        e = self.eng[X]
        for k, v in deps.items():
            if k == X and X == 'pe':
                continue
            if self.waited[X].get(k, 0) >= v:
                continue
            if isinstance(k, str):
                ep = (v - 1) // self.EPOCH
                e.wait_ge(self._esem(k, ep), v - ep * self.EPOCH)
            else:
                v = self.dval[k]
                e.wait_ge(self.dsems[k], v)
            self.waited[X][k] = v
            self.ninst += 1

    def _record(self, ev, reads, writes):
        for r in reads:
            d = self.readers.setdefault(r, {})
            if d.get(ev[0], 0) < ev[1]:
                d[ev[0]] = ev[1]
        for w in writes:
            self.lastw[w] = ev
            self.readers[w] = {}

    def op(self, X, fn, r=(), w=()):
        r = [k for k in r if k is not None]
        w = [k for k in w if k is not None]
        deps = self._deps(r, w)
        pkeys = [k for k in list(r) + list(w) if isinstance(k, str) and k.startswith('psb')]
        for k in pkeys:
            ev = self.ps_last.get(k)
            if ev is not None and ev[0] != X and deps.get(ev[0], 0) < ev[1]:
                deps[ev[0]] = ev[1]
        self._wait(X, deps)
        inst = fn(self.eng[X])
        self.cnt[X] += 1
        n = self.cnt[X]
        for k in pkeys:
            self.ps_last[k] = (X, n)
        inst.then_inc(self._esem(X, (n - 1) // self.EPOCH), 1)
        self.ninst += 1
        self._record((X, n), r, w)

    def dma(self, X, fn, semkey, r=(), w=()):
        base = semkey.rsplit('_', 1)[0] if semkey.rsplit('_', 1)[-1].isdigit() else semkey
        if base not in self.dkey:
            i = len(self.dsems)
            self.dsems.append(self.nc.alloc_semaphore("d_%d" % i))
            self.dval.append(0)
            self.dkey[base] = i
        i = self.dkey[base]
        self._wait(X, self._deps(r, w))
        inst = fn(self.eng[X])
        self.dval[i] += 16
        inst.then_inc(self.dsems[i], 16)
        self.ninst += 1
        self._record((i, self.dval[i]), r, w)

    def barrier(self):
        deps = {k: v for k, v in self.cnt.items() if v > 0}
        for i, v in enumerate(self.dval):
            if v > 0:
                deps[i] = v
        for X in self.eng:
            self._wait(X, dict(deps))


class Tile:
    def __init__(self, t, key):
        self.t, self.key = t, key

    def __getitem__(self, k):
        return self.t[k]


class Builder:
    def __init__(self, T, depth=DEPTH, debug=False, rb=None):
        import os
        rb = rb or int(os.environ.get('RB', '512'))
        self.T, self.depth, self.debug = T, depth, debug
        self.NT = T // 128
        self.RB = rb
        self.NB = (2 * T) // rb + NE
        nc = self.nc = bass.Bass("TRN2", target_bir_lowering=False)
        self.P = Prog(nc)
        self.nsb = 0
        self.sb_off = 16640
        self.sb_peak = 0
        self.sb_cap = 229376
        self.bank_i = 0
        self.chain_i = {}
        self.banks = [nc.alloc_psum_tensor("psb%d" % i, [128, 512], F32) for i in range(8)]
        self.dbg = {}

    def sb(self, shape, dt=F32, name=None):
        self.nsb += 1
        name = "%s_%d" % (name or "t", self.nsb)
        esz = 2 if dt == BF16 else 4
        n = 1
        for v in shape[1:]:
            n *= v
        nbytes = (n * esz + 31) // 32 * 32
        off = self.sb_off
        self.sb_off += nbytes
        assert self.sb_off <= self.sb_cap, "SBUF overflow %d" % self.sb_off
        self.sb_peak = max(self.sb_peak, self.sb_off)
        return Tile(self.nc.alloc_sbuf_tensor_at(name, list(shape), dt, offset=off), name)

    CHAIN_BANKS = {'s': [0, 1], 'r': [2, 3, 4], 'g': [5, 6], 'e': [7]}

    def bank(self, chain=None):
        if chain is None:
            i = self.bank_i
            self.bank_i = (i + 1) % 8
        else:
            lst = self.CHAIN_BANKS[chain]
            j = self.chain_i.get(chain, 0)
            self.chain_i[chain] = (j + 1) % len(lst)
            i = lst[j]
        return self.banks[i], "psb%d" % i

    def V(self, fn, r=(), w=()):
        self.P.op('dve', fn, r, w)

    def A(self, fn, r=(), w=()):
        self.P.op('act', fn, r, w)

    def G(self, fn, r=(), w=()):
        self.P.op('pool', fn, r, w)

    def M(self, fn, r=(), w=()):
        self.P.op('pe', fn, r, w)

    def din(self, name, shape, dt=F32):
        return self.nc.dram_tensor(name, list(shape), dt, kind="ExternalInput")

    def dscr(self, name, shape, dt=F32):
        return self.nc.dram_tensor(name, list(shape), dt, kind="Internal")

    def dout(self, name, shape, dt=F32):
        return self.nc.dram_tensor(name, list(shape), dt, kind="ExternalOutput")

    def load(self, q, out_ap, in_ap, key, dkeys=(), slow=False):
        if slow:
            self.P.dma(q, lambda e: e.dma_start(out=out_ap, in_=in_ap, allow_slow_non_contiguous=True), key, r=list(dkeys), w=[key])
        else:
            self.P.dma(q, lambda e: e.dma_start(out=out_ap, in_=in_ap), key, r=list(dkeys), w=[key])

    def store(self, q, out_ap, in_ap, key, dkeys=()):
        self.P.dma(q, lambda e: e.dma_start(out=out_ap, in_=in_ap), key, r=[key], w=list(dkeys))

    def consts(self):
        nc = self.nc
        c = self.c = {}
        self._ln_st = self.sb([128, 2, 6], name="ln_st")
        self._ln_mv = self.sb([128, 2], name="ln_mv")
        self._ln_rs = self.sb([128, 1], name="ln_rs")
        onesf = c['onesf'] = self.sb([128, 128], name="onesf")
        self.G(lambda e: e.memset(onesf[:], 1.0), w=[onesf.key])
        identf = c['identf'] = self.sb([128, 128], name="identf")
        self.G(lambda e: e.memset(identf[:], 0.0), w=[identf.key])
        self.G(lambda e: e.affine_select(out=identf[:], in_=identf[:], pattern=[[-1, 128]], base=0, channel_multiplier=1,
                                         compare_op=ALU.not_equal, fill=1.0), r=[identf.key], w=[identf.key])
        identb = c['identb'] = self.sb([128, 128], BF16, name="identb")
        self.V(lambda e: e.tensor_copy(identb[:], identf[:]), r=[identf.key], w=[identb.key])
        tri = c['tri'] = self.sb([128, 128], name="tri")
        self.G(lambda e: e.affine_select(out=tri[:], in_=onesf[:], pattern=[[1, 128]], base=0, channel_multiplier=-1,
                                         compare_op=ALU.is_ge, fill=0.0), r=[onesf.key], w=[tri.key])
        su = c['su'] = self.sb([128, 128], name="su")
        self.G(lambda e: e.affine_select(out=su[:], in_=onesf[:], pattern=[[-1, 128]], base=0, channel_multiplier=1,
                                         compare_op=ALU.is_gt, fill=0.0), r=[onesf.key], w=[su.key])
        sl = c['sl'] = self.sb([128, 128], name="sl")
        self.G(lambda e: e.affine_select(out=sl[:], in_=onesf[:], pattern=[[1, 128]], base=0, channel_multiplier=-1,
                                         compare_op=ALU.is_gt, fill=0.0), r=[onesf.key], w=[sl.key])
        maskb = c['maskb'] = self.sb([128, 128], name="maskb")
        self.G(lambda e: e.tensor_copy(maskb[:], tri[:]), r=[tri.key], w=[maskb.key])
        self.G(lambda e: e.memset(maskb[0:64, 64:128], 0.0), w=[maskb.key])
        rmask = c['rmask'] = self.sb([128, 256], name="rmask")
        self.G(lambda e: e.memset(rmask[:], 1.0), w=[rmask.key])
        self.G(lambda e: e.memset(rmask[:].rearrange("p (a b) -> p a b", b=64)[:, :, 0:1], 0.0), w=[rmask.key])
        hm = c['hm'] = self.sb([128, 2], name="hm")
        self.G(lambda e: e.memset(hm[:], 0.0), w=[hm.key])
        self.G(lambda e: e.memset(hm[0:64, 0:1], 1.0), w=[hm.key])
        self.G(lambda e: e.memset(hm[64:128, 1:2], 1.0), w=[hm.key])
        nhm = c['nhm'] = self.sb([128, 2], name="nhm")
        self.V(lambda e: e.tensor_scalar(nhm[:], hm[:], -1.0, None, ALU.mult), r=[hm.key], w=[nhm.key])
        qm = c['qm'] = self.sb([64, 2], name="qm")
        self.G(lambda e: e.memset(qm[:], 0.0), w=[qm.key])
        self.G(lambda e: e.memset(qm[0:32, 0:1], 32.0 ** -0.5), w=[qm.key])
        self.G(lambda e: e.memset(qm[32:64, 1:2], 32.0 ** -0.5), w=[qm.key])
        bones = c['bones'] = self.sb([128, 128], name="bones")
        self.G(lambda e: e.memset(bones[:], 0.0), w=[bones.key])
        self.G(lambda e: e.memset(bones[0:64, 0:64], 1.0), w=[bones.key])
        self.G(lambda e: e.memset(bones[64:128, 64:128], 1.0), w=[bones.key])

    def layernorm(self, xin, gk, bk, out, tmp, stt=None):
        st, mv, rs = stt if stt is not None else (self._ln_st, self._ln_mv, self._ln_rs)
        for i in range(2):
            self.V(lambda e, i=i: e.bn_stats(st[:, i, :], xin[:, i * 512:(i + 1) * 512]), r=[xin.key], w=[st.key])
        self.V(lambda e: e.bn_aggr(mv[:], st[:].rearrange("p a b -> p (a b)")), r=[st.key], w=[mv.key])
        self.V(lambda e: e.tensor_scalar(rs[:], mv[:, 1:2], LN_EPS, None, ALU.add), r=[mv.key], w=[rs.key])
        yield
        self.A(lambda e: e.activation(out=rs[:], in_=rs[:], func=AF.Sqrt), r=[rs.key], w=[rs.key])
        yield
        self.V(lambda e: e.reciprocal(rs[:], rs[:]), r=[rs.key], w=[rs.key])
        self.V(lambda e: e.tensor_scalar(tmp[:], xin[:], mv[:, 0:1], rs[:, 0:1], ALU.subtract, ALU.mult), r=[xin.key, mv.key, rs.key], w=[tmp.key])
        yield
        self.G(lambda e: e.tensor_tensor(tmp[:], tmp[:], gk[:], ALU.mult), r=[tmp.key, gk.key], w=[tmp.key])
        yield
        self.V(lambda e: e.tensor_tensor(out[:], tmp[:], bk[:], ALU.add), r=[tmp.key, bk.key], w=[out.key])
        yield

    def ln_stats(self, tag):
        return (self.sb([128, 2, 6], name="lnst_" + tag), self.sb([128, 2], name="lnmv_" + tag), self.sb([128, 1], name="lnrs_" + tag))

    def run_pipe(self, bodies, width=2):
        active, nxt = [], 0
        while active or nxt < len(bodies):
            while len(active) < width and nxt < len(bodies):
                active.append(bodies[nxt]())
                nxt += 1
            for g_ in list(active):
                try:
                    next(g_)
                except StopIteration:
                    active.remove(g_)

    def to_fm(self, h_tm, hb, hT):
        c = self.c
        self.A(lambda e: e.copy(out=hb[:], in_=h_tm[:]), r=[h_tm.key], w=[hb.key])
        bk, bkey = self.bank()
        pb = bk[:].bitcast(BF16)
        for kc in range(8):
            self.M(lambda e, kc=kc: e.transpose(pb[:, kc * 128:(kc + 1) * 128], hb[:, kc * 128:(kc + 1) * 128], c['identb'][:]),
                   r=[hb.key, c['identb'].key], w=[bkey])
        self.V(lambda e: e.tensor_copy(hT[:].rearrange("p a b -> p (a b)"), pb), r=[bkey], w=[hT.key])

    def declare_inputs(self):
        L = DEPTH
        d = self.d = {}
        specs = dict(x=[self.T, D], ln_in_g=[D], ln_in_b=[D], w_in=[L, D, NIN], ssd_conv_w=[L, 4, 768], ssd_conv_b=[L, 768],
                     ssd_dt_bias=[L, 8], ssd_a_log=[L, 8], ssd_d=[L, 8], ssd_norm_g=[L, 512], rwkv_mu=[L, 896], rwkv_w0=[L, 256],
                     rwkv_w2=[L, 32, 256], rwkv_a0=[L, 256], rwkv_a2=[L, 32, 256], rwkv_g2=[L, 64, 256], rwkv_k_k=[L, 256],
                     rwkv_k_a=[L, 256], rwkv_r_k=[L, 256], rwkv_ln_g=[L, 256], rwkv_ln_b=[L, 256], gla_w_a2=[L, 16, 128],
                     gla_b_a=[L, 128], gla_norm_g=[L, 256], w_out=[L, D, D], ln1_g=[L, D], ln1_b=[L, D], moe_w_rg=[L, D, 4],
                     moe_b_rg=[L, 4], moe_w_re=[L, D, 32], moe_b_re=[L, 32], moe_w_gate=[L * NE * D, FF], moe_w_up=[L * NE * D, FF],
                     moe_w_down=[L * NE * FF, D], ln2_g=[L, D], ln2_b=[L, D])
        for k, s in specs.items():
            d[k] = self.din(k, s)
        return specs

    def alloc_params(self):
        p = self.p = {}
        sb = self.sb
        p['w_in'] = sb([128, 8, NIN], BF16, "w_in_sb")
        p['w_out'] = sb([128, 8, D], BF16, "w_out_sb")
        p['cw'] = sb([128, 6, 4], name="convw")
        p['cb'] = sb([128, 6], name="convb")
        p['cdiag'] = sb([128, 24, 128], BF16, "cdiag")
        for n, w in (('dtb', 8), ('alog', 8), ('dsk8', 8), ('dsk', 512), ('sng', 512), ('rlg', 256), ('rlb', 256), ('gng', 256),
                     ('l1g', D), ('l1b', D), ('rb36', 36)):
            p[n] = sb([128, w], name="p_" + n)
        for n, w in (('mu', 7), ('omu', 7), ('w0', 2), ('a0', 2), ('kk', 2), ('ka', 2), ('omka', 2), ('rk', 2)):
            p[n] = sb([128, w], name="p_" + n)
        p['ba'] = sb([64, 2], name="p_ba")
        p['w2p'] = sb([128, 256], name="p_w2p")
        p['a2p'] = sb([128, 256], name="p_a2p")
        p['g2p'] = sb([128, 256], name="p_g2p")
        p['wa2'] = sb([32, 128], name="p_wa2")
        p['wr'] = sb([128, 8, 36], name="p_wr")

    def load_params(self, l):
        p, d, c = self.p, self.d, self.c
        q = 'pool'
        win = d['w_in'][l].rearrange("(kc p) n -> p kc n", p=128)
        for kc in range(8):
            for (a, b) in ((0, 1484), (1484, NIN)):
                self.load(q, p['w_in'][:, kc, a:b], win[:, kc, a:b], p['w_in'].key)
        wo = d['w_out'][l].rearrange("(kc p) n -> p kc n", p=128)
        for kc in range(8):
            self.load(q, p['w_out'][:, kc, :], wo[:, kc, :], p['w_out'].key)
        q = 'sp'
        for kk_ in range(4):
            self.load(q, p['cw'][:, :, kk_], d['ssd_conv_w'][l][kk_].rearrange("(cb p) -> p cb", p=128), p['cw'].key, slow=True)
        self.load(q, p['cb'][:], d['ssd_conv_b'][l].rearrange("(cb p) -> p cb", p=128), p['cb'].key, slow=True)
        for n, src in (('dtb', 'ssd_dt_bias'), ('alog', 'ssd_a_log'), ('dsk8', 'ssd_d'), ('sng', 'ssd_norm_g'), ('rlg', 'rwkv_ln_g'),
                       ('rlb', 'rwkv_ln_b'), ('gng', 'gla_norm_g'), ('l1g', 'ln1_g'), ('l1b', 'ln1_b')):
            self.load(q, p[n][:], d[src][l].partition_broadcast(128), p[n].key)
        self.load(q, p['rb36'][:, 0:4], d['moe_b_rg'][l].partition_broadcast(128), p['rb36'].key)
        self.load(q, p['rb36'][:, 4:36], d['moe_b_re'][l].partition_broadcast(128), p['rb36'].key)
        self.load(q, p['mu'][:], d['rwkv_mu'][l].rearrange("(b p) -> p b", p=128), p['mu'].key, slow=True)
        for n, src in (('w0', 'rwkv_w0'), ('a0', 'rwkv_a0'), ('kk', 'rwkv_k_k'), ('ka', 'rwkv_k_a'), ('rk', 'rwkv_r_k')):
            self.load(q, p[n][:], d[src][l].rearrange("(b p) -> p b", p=128), p[n].key, slow=True)
        self.load(q, p['ba'][:], d['gla_b_a'][l].rearrange("(b p) -> p b", p=64), p['ba'].key, slow=True)
        for n in ('w2p', 'a2p', 'g2p'):
            self.G(lambda e, n=n: e.memset(p[n][:], 0.0), w=[p[n].key])
        self.load(q, p['w2p'][0:32, :], d['rwkv_w2'][l], p['w2p'].key)
        self.load(q, p['a2p'][32:64, :], d['rwkv_a2'][l], p['a2p'].key)
        self.load(q, p['g2p'][64:128, :], d['rwkv_g2'][l], p['g2p'].key)
        self.G(lambda e: e.memset(p['wa2'][:], 0.0), w=[p['wa2'].key])
        self.load(q, p['wa2'][16:32, :], d['gla_w_a2'][l], p['wa2'].key)
        self.load(q, p['wr'][:, :, 0:4], d['moe_w_rg'][l].rearrange("(kc p) n -> p kc n", p=128), p['wr'].key, slow=True)
        self.load(q, p['wr'][:, :, 4:36], d['moe_w_re'][l].rearrange("(kc p) n -> p kc n", p=128), p['wr'].key, slow=True)
        self.V(lambda e: e.tensor_scalar(p['omu'][:], p['mu'][:], -1.0, 1.0, ALU.mult, ALU.add), r=[p['mu'].key], w=[p['omu'].key])
        self.V(lambda e: e.tensor_scalar(p['omka'][:], p['ka'][:], -1.0, 1.0, ALU.mult, ALU.add), r=[p['ka'].key], w=[p['omka'].key])
        self.A(lambda e: e.activation(out=p['alog'][:], in_=p['alog'][:], func=AF.Exp), r=[p['alog'].key], w=[p['alog'].key])
        self.V(lambda e: e.tensor_scalar(p['alog'][:], p['alog'][:], -1.0, None, ALU.mult), r=[p['alog'].key], w=[p['alog'].key])
        self.V(lambda e: e.tensor_copy(p['dsk'][:].rearrange("p (h q) -> p h q", q=64), p['dsk8'][:].unsqueeze(2).to_broadcast([128, 8, 64])),
               r=[p['dsk8'].key], w=[p['dsk'].key])
        for cb in range(6):
            for k in range(4):
                self.V(lambda e, cb=cb, k=k: e.tensor_scalar(p['cdiag'][:, cb * 4 + k, :], c['identf'][:], p['cw'][:, cb, k:k + 1], None, ALU.mult),
                       r=[c['identf'].key, p['cw'].key], w=[p['cdiag'].key])

    def alloc_mixer(self):
        s = self.s = {}
        sb = self.sb
        s['hT'] = sb([128, 8, 128], BF16, "m_hT")
        s['htm'] = sb([128, D], name="m_htm")
        s['xbc'] = sb([128, 6, 132], BF16, "m_xbc")
        s['xbB'] = sb([128, 6, 132], BF16, "m_xbB")
        s['xc'] = sb([128, 6, 128], BF16, "m_xc")
        s['xh'] = sb([128, 512], BF16, "m_xh")
        s['xdt'] = sb([128, 512], BF16, "m_xdt")
        s['btm'] = sb([128, 128], BF16, "m_btm")
        s['cm'] = sb([128, 2, 128], BF16, "m_cm")
        s['dt'] = sb([128, 8], name="m_dt")
        s['adt'] = sb([128, 8], name="m_adt")
        s['sp1'] = sb([128, 8], name="m_sp1")
        s['sp2'] = sb([128, 8], name="m_sp2")
        s['R'] = sb([128, 4, 128], name="m_R")
        s['seg'] = sb([128, 8, 128], BF16, "m_seg")
        s['ea'] = sb([128, 8], name="m_ea")
        s['cd'] = sb([128, 4], name="m_cd")
        s['cbm'] = sb([128, 2, 128], BF16, "m_cbm")
        s['toend'] = sb([128, 8], name="m_toend")
        s['S32'] = sb([128, 256], name="m_S32")
        s['Sbf'] = sb([128, 256], BF16, "m_Sbf")
        s['y1'] = sb([128, 512], name="m_y1")
        s['sz'] = sb([128, 512], BF16, "m_sz")
        s['ssq'] = sb([128, 4], name="m_ssq")
        s['ycat'] = sb([128, D], BF16, "m_ycat")
        s['yT'] = sb([128, 8, 128], BF16, "m_yT")
        s['gaT'] = sb([32, 128], name="g_gaT")
        s['gx'] = sb([64, 256], name="g_x")
        s['gt1'] = sb([64, 256], name="g_t1")
        s['gcum'] = sb([64, 256], name="g_cum")
        s['geq'] = sb([64, 256], name="g_eq")
        s['gek'] = sb([64, 256], name="g_ek")
        s['gel'] = sb([64, 4], name="g_el")
        s['gqm'] = sb([64, 2, 256], BF16, "g_qm")
        s['gkT'] = sb([64, 256], BF16, "g_kT")
        s['gktm'] = sb([128, 2, 128], BF16, "g_ktm")
        s['gv'] = sb([128, 256], BF16, "g_v")
        s['gvm'] = sb([128, 2, 256], BF16, "g_vm")
        s['gsm'] = sb([128, 4, 128], BF16, "g_sm")
        s['gS'] = sb([64, 2, 64], name="g_S")
        s['gSb'] = sb([64, 2, 2, 64], BF16, "g_Sb")
        s['gst'] = sb([64, 2, 64], name="g_st")
        s['go'] = sb([128, 256], name="g_o")
        s['gsq'] = sb([128, 256], name="g_sq")
        s['grs'] = sb([128, 4], name="g_rs")
        s['gsg'] = sb([128, 256], name="g_sg")
        s['rw'] = sb([128, 7, 129], name="r_rw")
        s['rsh'] = sb([128, 7, 128], name="r_sh")
        s['rt1'] = sb([128, 7, 128], name="r_t1")
        for n in ('ra1', 'ra2', 'ra3', 'rcw', 'recw', 'reicw', 'recwp', 'ra', 'rkk', 'rkp'):
            s[n] = sb([128, 256], name="r_" + n)
        for n in ('rKt', 'rBt'):
            s[n] = sb([128, 256], BF16, "r_" + n)
        s['rAm'] = sb([128, 2, 256], BF16, "r_Am")
        s['rRm'] = sb([128, 2, 256], BF16, "r_Rm")
        s['rtw'] = sb([128, 128], name="r_tw")
        s['rsg'] = sb([128, 128], name="r_sg")
        s['rvtm'] = sb([128, 256], name="r_vtm")
        s['rvc'] = sb([64, 2, 256], BF16, "r_vc")
        s['rBc'] = sb([64, 2, 256], BF16, "r_Bc")
        s['rKc'] = sb([64, 2, 256], BF16, "r_Kc")
        s['rP'] = sb([64, 8, 64], BF16, "r_P")
        s['rQ'] = sb([64, 8, 64], BF16, "r_Q")
        s['rP2'] = sb([64, 8, 64], BF16, "r_P2")
        s['rQ2'] = sb([64, 8, 64], BF16, "r_Q2")
        s['rTT'] = sb([64, 8, 64], BF16, "r_TT")
        s['rAak'] = sb([64, 8, 64], BF16, "r_Aak")
        s['rArb'] = sb([64, 8, 64], BF16, "r_Arb")
        s['rArk'] = sb([64, 8, 64], BF16, "r_Ark")
        s['rG'] = sb([64, 4, 64], BF16, "r_G")
        s['rU'] = sb([64, 4, 64], BF16, "r_U")
        s['rST'] = sb([128, 2, 64], name="r_ST")
        s['rSTb'] = sb([128, 2, 64], BF16, "r_STb")
        s['rt2'] = sb([128, 2, 64], name="r_t2")
        s['rewc'] = sb([128, 4], name="r_ewc")
        s['rY1'] = sb([128, 256], name="r_Y1")
        s['rY'] = sb([128, 256], name="r_Y")
        s['rm1'] = sb([128, 4], name="r_m1")
        s['rm2'] = sb([128, 4], name="r_m2")
        s['rvar'] = sb([128, 4], name="r_var")
        s['rbc'] = sb([128, 4], name="r_bc")
        s['rg'] = sb([128, 256], name="r_g")
        s['mix'] = sb([128, D], name="m_mix")
        s['tmp'] = sb([128, D], name="m_tmp")
        s['h1'] = sb([128, D], name="m_h1")

    def proj_tm(self, out_ap, okey, c0, n):
        s, p = self.s, self.p
        for kc in range(8):
            self.M(lambda e, kc=kc: e.matmul(out_ap, lhsT=s['hT'][:, kc, :], rhs=p['w_in'][:, kc, c0:c0 + n], start=(kc == 0), stop=(kc == 7)),
                   r=[s['hT'].key, p['w_in'].key], w=[okey])

    def proj_fm(self, out_ap, okey, c0, m):
        s, p = self.s, self.p
        for kc in range(8):
            self.M(lambda e, kc=kc: e.matmul(out_ap, lhsT=p['w_in'][:, kc, c0:c0 + m], rhs=s['hT'][:, kc, :], start=(kc == 0), stop=(kc == 7)),
                   r=[s['hT'].key, p['w_in'].key], w=[okey])

    def bc(self, ap, shape, axis):
        return ap.unsqueeze(axis).to_broadcast(list(shape))

    def ssd_tile(self, first):
        s, p, c = self.s, self.p, self.c
        V, A, G, M = self.V, self.A, self.G, self.M
        k = lambda t: t.key
        bz, kz = self.bank('s')
        self.proj_tm(bz[:, :], kz, O_Z, 512)
        A(lambda e: e.activation(out=s['sz'][:], in_=bz[:, :], func=AF.Silu), r=[kz], w=[k(s['sz'])])
        yield
        bd, kd = self.bank('s')
        self.proj_tm(bd[:, 0:8], kd, O_DT, 8)
        V(lambda e: e.tensor_tensor(s['sp1'][:], bd[:, 0:8], p['dtb'][:], ALU.add), r=[kd, k(p['dtb'])], w=[k(s['sp1'])])
        yield
        V(lambda e: e.scalar_tensor_tensor(s['sp2'][:], s['sp1'][:], -1.0, s['sp1'][:], ALU.mult, ALU.max), r=[k(s['sp1'])], w=[k(s['sp2'])])
        yield
        A(lambda e: e.activation(out=s['sp2'][:], in_=s['sp2'][:], func=AF.Exp, scale=-1.0), r=[k(s['sp2'])], w=[k(s['sp2'])])
        yield
        A(lambda e: e.activation(out=s['sp2'][:], in_=s['sp2'][:], func=AF.Ln, bias=1.0), r=[k(s['sp2'])], w=[k(s['sp2'])])
        yield
        V(lambda e: e.scalar_tensor_tensor(s['dt'][:], s['sp1'][:], 0.0, s['sp2'][:], ALU.max, ALU.add), r=[k(s['sp1']), k(s['sp2'])], w=[k(s['dt'])])
        yield
        V(lambda e: e.tensor_tensor(s['adt'][:], s['dt'][:], p['alog'][:], ALU.mult), r=[k(s['dt']), k(p['alog'])], w=[k(s['adt'])])
        yield
        import os
        stop = float(os.environ.get('SSDSTOP', '9'))
        if stop <= 1:
            return
        if first:
            G(lambda e: e.memset(s['xbc'][:, :, 0:4], 0.0), w=[k(s['xbc'])])
            yield
            G(lambda e: e.memset(s['xbB'][:, :, 0:2], 0.0), w=[k(s['xbB'])])
            yield
        else:
            G(lambda e: e.tensor_copy(s['xbc'][:, :, 0:3], s['xbc'][:, :, 128:131]), r=[k(s['xbc'])], w=[k(s['xbc'])])
            yield
            G(lambda e: e.tensor_copy(s['xbB'][:, :, 0:2], s['xbB'][:, :, 128:130]), r=[k(s['xbB'])], w=[k(s['xbB'])])
            yield
        for grp, nb in ((0, 4), (4, 2)):
            bx, kx = self.bank('s')
            for j in range(nb):
                self.proj_fm(bx[:, j * 128:(j + 1) * 128], kx, O_XBC + (grp + j) * 128, 128)
            A(lambda e, bx=bx, grp=grp, nb=nb: e.copy(out=s['xbc'][:, grp:grp + nb, 3:131], in_=bx[:, 0:nb * 128].rearrange("p (a b) -> p a b", b=128)),
              r=[kx], w=[k(s['xbc'])])
            yield
            V(lambda e, bx=bx, grp=grp, nb=nb: e.tensor_copy(s['xbB'][:, grp:grp + nb, 2:130], bx[:, 0:nb * 128].rearrange("p (a b) -> p a b", b=128)),
              r=[kx], w=[k(s['xbB'])])
            yield
        for grp, nb in ((0, 4), (4, 2)):
            bx, kx = self.bank('s')
            for j in range(nb):
                cb = grp + j
                for kk_ in range(4):
                    src = s['xbc'] if kk_ % 2 == 0 else s['xbB']
                    off = kk_ if kk_ % 2 == 0 else kk_ - 1
                    M(lambda e, bx=bx, j=j, cb=cb, kk_=kk_, src=src, off=off: e.matmul(bx[:, j * 128:(j + 1) * 128], lhsT=p['cdiag'][:, cb * 4 + kk_, :],
                                                                                   rhs=src[:, cb, off:off + 128], start=(kk_ == 0), stop=(kk_ == 3)),
                      r=[k(p['cdiag']), k(src)], w=[kx])
            for j in range(nb):
                cb = grp + j
                A(lambda e, bx=bx, j=j, cb=cb: e.activation(out=s['xc'][:, cb, :], in_=bx[:, j * 128:(j + 1) * 128], func=AF.Silu, bias=p['cb'][:, cb:cb + 1]),
                  r=[kx, k(p['cb'])], w=[k(s['xc'])])
                yield
        if stop <= 2:
            return
        bt, kt = self.bank('s')
        pb = bt[:].bitcast(BF16)
        for j in range(5):
            M(lambda e, j=j: e.transpose(pb[:, j * 128:(j + 1) * 128], s['xc'][:, j, :], c['identb'][:]), r=[k(s['xc']), k(c['identb'])], w=[kt])
        V(lambda e: e.tensor_copy(s['xh'][:], pb[:, 0:512]), r=[kt], w=[k(s['xh'])])
        yield
        V(lambda e: e.tensor_copy(s['btm'][:], pb[:, 512:640]), r=[kt], w=[k(s['btm'])])
        yield
        if stop <= 2.2:
            return
        G(lambda e: e.tensor_tensor(s['cm'][:], self.bc(s['xc'][:, 5, :], [128, 2, 128], 1), self.bc(c['hm'][:, :], [128, 2, 128], 2), ALU.mult),
          r=[k(s['xc']), k(c['hm'])], w=[k(s['cm'])])
        yield
        V(lambda e: e.tensor_tensor(s['xdt'][:].rearrange("p (h q) -> p h q", q=64), s['xh'][:].rearrange("p (h q) -> p h q", q=64),
                                    self.bc(s['dt'][:, :], [128, 8, 64], 2), ALU.mult), r=[k(s['xh']), k(s['dt'])], w=[k(s['xdt'])])
        yield
        if stop <= 2.4:
            return
        for half in range(2):
            G(lambda e, half=half: e.tensor_tensor(s['R'][:], self.bc(c['tri'][:, :], [128, 4, 128], 1), self.bc(s['adt'][:, half * 4:(half + 1) * 4], [128, 4, 128], 2), ALU.mult),
              r=[k(c['tri']), k(s['adt'])], w=[k(s['R'])])
            yield
            bD, kD = self.bank('s')
            for q2 in range(2):
                M(lambda e, bD=bD, q2=q2: e.matmul(bD[:, q2 * 256:(q2 + 1) * 256], lhsT=c['su'][:], rhs=s['R'][:, q2 * 2:(q2 + 1) * 2, :].rearrange("p a b -> p (a b)"), start=True, stop=True),
                  r=[k(c['su']), k(s['R'])], w=[kD])
            if stop <= 2.6:
                continue
            A(lambda e, bD=bD, half=half: e.activation(out=s['seg'][:, half * 4:(half + 1) * 4, :].rearrange("p a b -> p (a b)"), in_=bD[:, :], func=AF.Exp),
              r=[kD], w=[k(s['seg'])])
            yield
        if stop <= 2.8:
            return
        V(lambda e: e.tensor_copy(s['toend'][:], s['seg'][:, :, 127]), r=[k(s['seg'])], w=[k(s['toend'])])
        yield
        if stop <= 3:
            return
        be, ke = self.bank('s')
        M(lambda e: e.matmul(be[:, 0:8], lhsT=c['tri'][:], rhs=s['adt'][:], start=True, stop=True), r=[k(c['tri']), k(s['adt'])], w=[ke])
        for g in range(2):
            M(lambda e, g=g: e.matmul(be[g * 64:(g + 1) * 64, 8:12], lhsT=c['onesf'][:, 0:64], rhs=s['adt'][:, g * 4:(g + 1) * 4], start=True, stop=True),
              r=[k(c['onesf']), k(s['adt'])], w=[ke])
        A(lambda e: e.activation(out=s['ea'][:], in_=be[:, 0:8], func=AF.Exp), r=[ke], w=[k(s['ea'])])
        yield
        A(lambda e: e.activation(out=s['cd'][:], in_=be[:, 8:12], func=AF.Exp), r=[ke], w=[k(s['cd'])])
        yield
        if stop <= 4:
            return
        bc_, kc_ = self.bank('s')
        for g in range(2):
            M(lambda e, g=g: e.matmul(bc_[:, g * 128:(g + 1) * 128], lhsT=s['xc'][:, 4, :], rhs=s['cm'][:, g, :], start=True, stop=True),
              r=[k(s['xc']), k(s['cm'])], w=[kc_])
        V(lambda e: e.tensor_tensor(s['cbm'][:], bc_[:, 0:256].rearrange("p (a b) -> p a b", b=128), self.bc(c['tri'][:, :], [128, 2, 128], 1), ALU.mult),
          r=[kc_, k(c['tri'])], w=[k(s['cbm'])])
        yield
        for g in range(2):
            V(lambda e, g=g: e.tensor_tensor(s['seg'][:, g * 4:(g + 1) * 4, :], s['seg'][:, g * 4:(g + 1) * 4, :], self.bc(s['cbm'][:, g, :], [128, 4, 128], 1), ALU.mult),
              r=[k(s['seg']), k(s['cbm'])], w=[k(s['seg'])])
            yield
        by, ky = self.bank('s')
        for h in range(8):
            M(lambda e, h=h: e.matmul(by[:, h * 64:(h + 1) * 64], lhsT=s['seg'][:, h, :], rhs=s['xdt'][:, h * 64:(h + 1) * 64], start=True, stop=True),
              r=[k(s['seg']), k(s['xdt'])], w=[ky])
        bo, ko = self.bank('s')
        if not first:
            for g in range(2):
                M(lambda e, g=g: e.matmul(bo[:, g * 256:(g + 1) * 256], lhsT=s['cm'][:, g, :], rhs=s['Sbf'][:, :], start=True, stop=True),
                  r=[k(s['cm']), k(s['Sbf'])], w=[ko])
            V(lambda e: e.tensor_tensor(s['y1'][:].rearrange("p (h q) -> p h q", q=64), bo[:, :].rearrange("p (h q) -> p h q", q=64),
                                        self.bc(s['ea'][:, :], [128, 8, 64], 2), ALU.mult), r=[ko, k(s['ea'])], w=[k(s['y1'])])
            yield
            V(lambda e: e.tensor_tensor(s['y1'][:], s['y1'][:], by[:, :], ALU.add), r=[k(s['y1']), ky], w=[k(s['y1'])])
            yield
        else:
            V(lambda e: e.tensor_copy(s['y1'][:], by[:, :]), r=[ky], w=[k(s['y1'])])
            yield
        if stop <= 5:
            return
        V(lambda e: e.tensor_tensor(s['xdt'][:].rearrange("p (h q) -> p h q", q=64), s['xdt'][:].rearrange("p (h q) -> p h q", q=64),
                                    self.bc(s['toend'][:, :], [128, 8, 64], 2), ALU.mult), r=[k(s['xdt']), k(s['toend'])], w=[k(s['xdt'])])
        yield
        bs, ks = self.bank('s')
        for g in range(2):
            M(lambda e, g=g: e.matmul(bs[g * 64:(g + 1) * 64, 0:256], lhsT=s['btm'][:, g * 64:(g + 1) * 64], rhs=s['xdt'][:, g * 256:(g + 1) * 256], start=True, stop=True),
              r=[k(s['btm']), k(s['xdt'])], w=[ks])
        if first:
            V(lambda e: e.tensor_copy(s['S32'][:], bs[:, 0:256]), r=[ks], w=[k(s['S32'])])
            yield
        else:
            V(lambda e: e.tensor_tensor(s['S32'][:].rearrange("p (h q) -> p h q", q=64), s['S32'][:].rearrange("p (h q) -> p h q", q=64),
                                        self.bc(s['cd'][:, :], [128, 4, 64], 2), ALU.mult), r=[k(s['S32']), k(s['cd'])], w=[k(s['S32'])])
            yield
            V(lambda e: e.tensor_tensor(s['S32'][:], s['S32'][:], bs[:, 0:256], ALU.add), r=[k(s['S32']), ks], w=[k(s['S32'])])
            yield
        A(lambda e: e.copy(out=s['Sbf'][:], in_=s['S32'][:]), r=[k(s['S32'])], w=[k(s['Sbf'])])
        yield
        G(lambda e: e.tensor_tensor(s['xdt'][:], s['xh'][:], p['dsk'][:], ALU.mult), r=[k(s['xh']), k(p['dsk'])], w=[k(s['xdt'])])
        yield
        V(lambda e: e.tensor_tensor(s['y1'][:], s['y1'][:], s['xdt'][:], ALU.add), r=[k(s['y1']), k(s['xdt'])], w=[k(s['y1'])])
        yield
        V(lambda e: e.tensor_tensor(s['y1'][:], s['y1'][:], s['sz'][:], ALU.mult), r=[k(s['y1']), k(s['sz'])], w=[k(s['y1'])])
        yield
        for g in range(2):
            A(lambda e, g=g: e.activation(out=s['sz'][:, g * 256:(g + 1) * 256], in_=s['y1'][:, g * 256:(g + 1) * 256], func=AF.Square, accum_out=s['ssq'][:, g:g + 1]),
              r=[k(s['y1'])], w=[k(s['sz']), k(s['ssq'])])
            yield
        V(lambda e: e.tensor_scalar(s['ssq'][:, 0:2], s['ssq'][:, 0:2], 1.0 / 256, RMS_EPS, ALU.mult, ALU.add), r=[k(s['ssq'])], w=[k(s['ssq'])])
        yield
        A(lambda e: e.activation(out=s['ssq'][:, 0:2], in_=s['ssq'][:, 0:2], func=AF.Sqrt), r=[k(s['ssq'])], w=[k(s['ssq'])])
        yield
        V(lambda e: e.reciprocal(s['ssq'][:, 0:2], s['ssq'][:, 0:2]), r=[k(s['ssq'])], w=[k(s['ssq'])])
        yield
        for g in range(2):
            V(lambda e, g=g: e.scalar_tensor_tensor(s['ycat'][:, g * 256:(g + 1) * 256], s['y1'][:, g * 256:(g + 1) * 256], s['ssq'][:, g:g + 1],
                                                   p['sng'][:, g * 256:(g + 1) * 256], ALU.mult, ALU.mult), r=[k(s['y1']), k(s['ssq']), k(p['sng'])], w=[k(s['ycat'])])
            yield

    def gla_tile(self, first):
        s, p, c = self.s, self.p, self.c
        V, A, G, M = self.V, self.A, self.G, self.M
        k = lambda t: t.key
        bv, kv = self.bank('g')
        self.proj_tm(bv[:, :], kv, O_GV, 512)
        A(lambda e: e.copy(out=s['gv'][:], in_=bv[:, 0:256]), r=[kv], w=[k(s['gv'])])
        yield
        A(lambda e: e.activation(out=s['gsg'][:], in_=bv[:, 256:512], func=AF.Silu), r=[kv], w=[k(s['gsg'])])
        yield
        G(lambda e: e.tensor_tensor(s['gsg'][:], s['gsg'][:], p['gng'][:], ALU.mult), r=[k(s['gsg']), k(p['gng'])], w=[k(s['gsg'])])
        yield
        ba_, ka_ = self.bank('g')
        self.proj_fm(ba_[0:32, 0:128], ka_, O_GA - 16, 32)
        A(lambda e: e.copy(out=s['gaT'][:], in_=ba_[0:32, 0:128]), r=[ka_], w=[k(s['gaT'])])
        yield
        bx, kx = self.bank('g')
        for pr in range(2):
            M(lambda e, pr=pr: e.matmul(bx[0:64, pr * 128:(pr + 1) * 128], lhsT=p['wa2'][:, pr * 64:(pr + 1) * 64], rhs=s['gaT'][:], start=True, stop=True),
              r=[k(p['wa2']), k(s['gaT'])], w=[kx])
        for pr in range(2):
            A(lambda e, pr=pr: e.activation(out=s['gx'][:, pr * 128:(pr + 1) * 128], in_=bx[0:64, pr * 128:(pr + 1) * 128], func=AF.Identity, bias=p['ba'][:, pr:pr + 1]),
              r=[kx, k(p['ba'])], w=[k(s['gx'])])
            yield
        V(lambda e: e.scalar_tensor_tensor(s['gt1'][:], s['gx'][:], -1.0, s['gx'][:], ALU.mult, ALU.max), r=[k(s['gx'])], w=[k(s['gt1'])])
        yield
        A(lambda e: e.activation(out=s['gt1'][:], in_=s['gt1'][:], func=AF.Exp, scale=-1.0), r=[k(s['gt1'])], w=[k(s['gt1'])])
        yield
        A(lambda e: e.activation(out=s['gt1'][:], in_=s['gt1'][:], func=AF.Ln, bias=1.0), r=[k(s['gt1'])], w=[k(s['gt1'])])
        yield
        V(lambda e: e.scalar_tensor_tensor(s['gx'][:], s['gx'][:], 0.0, s['gt1'][:], ALU.min, ALU.subtract), r=[k(s['gx']), k(s['gt1'])], w=[k(s['gx'])])
        yield
        V(lambda e: e.tensor_tensor_scan(s['gcum'][:], c['rmask'][0:64, :], s['gx'][:], 0.0, ALU.mult, ALU.add), r=[k(c['rmask']), k(s['gx'])], w=[k(s['gcum'])])
        yield
        A(lambda e: e.activation(out=s['geq'][:], in_=s['gcum'][:], func=AF.Exp, scale=1.0 / 16), r=[k(s['gcum'])], w=[k(s['geq'])])
        yield
        A(lambda e: e.activation(out=s['gek'][:], in_=s['gcum'][:], func=AF.Exp, scale=-1.0 / 16), r=[k(s['gcum'])], w=[k(s['gek'])])
        yield
        A(lambda e: e.activation(out=s['gel'][:], in_=s['gcum'][:].rearrange("p (a b) -> p a b", b=64)[:, :, 63], func=AF.Exp, scale=1.0 / 16),
          r=[k(s['gcum'])], w=[k(s['gel'])])
        yield
        bq, kq = self.bank('g')
        for pr in range(2):
            self.proj_fm(bq[0:64, pr * 128:(pr + 1) * 128], kq, O_GQ + pr * 64, 64)
        for pr in range(2):
            self.proj_fm(bq[0:64, 256 + pr * 128:256 + (pr + 1) * 128], kq, O_GK + pr * 64, 64)
        for hh in range(2):
            V(lambda e, hh=hh: e.scalar_tensor_tensor(s['gqm'][:, hh, :], bq[0:64, 0:256], c['qm'][:, hh:hh + 1], s['geq'][:], ALU.mult, ALU.mult),
              r=[kq, k(c['qm']), k(s['geq'])], w=[k(s['gqm'])])
            yield
        V(lambda e: e.tensor_tensor(s['gkT'][:], bq[0:64, 256:512], s['gek'][:], ALU.mult), r=[kq, k(s['gek'])], w=[k(s['gkT'])])
        yield
        bt, kt = self.bank('g')
        pb = bt[:].bitcast(BF16)
        for pr in range(2):
            M(lambda e, pr=pr: e.transpose(pb[:, pr * 64:(pr + 1) * 64], s['gkT'][:, pr * 128:(pr + 1) * 128], c['identb'][0:64, 0:64]),
              r=[k(s['gkT']), k(c['identb'])], w=[kt])
        if first:
            G(lambda e: e.memset(s['gktm'][:], 0.0), w=[k(s['gktm'])])
            yield
        for hh in range(2):
            V(lambda e, hh=hh: e.tensor_copy(s['gktm'][:, hh, :].rearrange("p (a b c) -> p a b c", a=2, b=2)[:, :, hh, :],
                                             pb[:, 0:128].rearrange("p (a b c) -> p a b c", a=2, b=2)[:, :, hh, :]), r=[kt], w=[k(s['gktm'])])
            yield
        G(lambda e: e.tensor_tensor(s['gvm'][:], self.bc(s['gv'][:, :], [128, 2, 256], 1), self.bc(c['hm'][:, :], [128, 2, 256], 2), ALU.mult),
          r=[k(s['gv']), k(c['hm'])], w=[k(s['gvm'])])
        yield
        bs_, ks_ = self.bank('g')
        for h in range(4):
            pr, hh = h // 2, h % 2
            M(lambda e, h=h, pr=pr, hh=hh: e.matmul(bs_[:, h * 128:(h + 1) * 128], lhsT=s['gkT'][:, pr * 128:(pr + 1) * 128], rhs=s['gqm'][:, hh, pr * 128:(pr + 1) * 128],
                                                    start=True, stop=True), r=[k(s['gkT']), k(s['gqm'])], w=[ks_])
        V(lambda e: e.tensor_tensor(s['gsm'][:], bs_[:, :].rearrange("p (a b) -> p a b", b=128), self.bc(c['maskb'][:, :], [128, 4, 128], 1), ALU.mult),
          r=[ks_, k(c['maskb'])], w=[k(s['gsm'])])
        yield
        bu, ku = self.bank('g')
        for cc in range(2):
            for pr in range(2):
                for hh in range(2):
                    h = pr * 2 + hh
                    M(lambda e, cc=cc, h=h, pr=pr, hh=hh: e.matmul(bu[0:64, cc * 128 + pr * 64:cc * 128 + (pr + 1) * 64],
                                                                   lhsT=s['gktm'][:, hh, pr * 64:(pr + 1) * 64], rhs=s['gvm'][:, cc, h * 64:(h + 1) * 64],
                                                                   start=(hh == 0), stop=(hh == 1)), r=[k(s['gktm']), k(s['gvm'])], w=[ku])
        if first:
            G(lambda e: e.memset(s['gS'][:], 0.0), w=[k(s['gS'])])
            yield
        for cc in range(2):
            A(lambda e, cc=cc: e.copy(out=s['gSb'][:, cc, :, :], in_=s['gS'][:]), r=[k(s['gS'])], w=[k(s['gSb'])])
            yield
            V(lambda e, cc=cc: e.tensor_tensor(s['gst'][:], s['gS'][:], bu[0:64, cc * 128:(cc + 1) * 128].rearrange("p (a b) -> p a b", b=64), ALU.add),
              r=[k(s['gS']), ku], w=[k(s['gst'])])
            yield
            V(lambda e, cc=cc: e.tensor_tensor(s['gS'][:], s['gst'][:], self.bc(s['gel'][:, cc::2], [64, 2, 64], 2), ALU.mult),
              r=[k(s['gst']), k(s['gel'])], w=[k(s['gS'])])
            yield
        bo, ko = self.bank('g')
        for h in range(4):
            M(lambda e, h=h: e.matmul(bo[:, h * 64:(h + 1) * 64], lhsT=s['gsm'][:, h, :], rhs=s['gv'][:, h * 64:(h + 1) * 64], start=True, stop=True),
              r=[k(s['gsm']), k(s['gv'])], w=[ko])
        bi, ki = self.bank('g')
        for cc in range(2):
            for h in range(4):
                pr, hh = h // 2, h % 2
                M(lambda e, cc=cc, h=h, pr=pr, hh=hh: e.matmul(bi[cc * 64:(cc + 1) * 64, h * 64:(h + 1) * 64], lhsT=s['gqm'][:, hh, pr * 128 + cc * 64:pr * 128 + (cc + 1) * 64],
                                                               rhs=s['gSb'][:, cc, pr, :], start=True, stop=True), r=[k(s['gqm']), k(s['gSb'])], w=[ki])
        A(lambda e: e.copy(out=s['gsq'][:], in_=bi[:, 0:256]), r=[ki], w=[k(s['gsq'])])
        yield
        V(lambda e: e.tensor_tensor(s['go'][:], s['gsq'][:], bo[:, 0:256], ALU.add), r=[k(s['gsq']), ko], w=[k(s['go'])])
        yield
        A(lambda e: e.activation(out=s['gsq'][:], in_=s['go'][:], func=AF.Square), r=[k(s['go'])], w=[k(s['gsq'])])
        yield
        V(lambda e: e.tensor_reduce(s['grs'][:], s['gsq'][:].rearrange("p (h q) -> p h q", q=64), AX.X, ALU.add), r=[k(s['gsq'])], w=[k(s['grs'])])
        yield
        V(lambda e: e.tensor_scalar(s['grs'][:], s['grs'][:], 1.0 / 64, RMS_EPS, ALU.mult, ALU.add), r=[k(s['grs'])], w=[k(s['grs'])])
        yield
        A(lambda e: e.activation(out=s['grs'][:], in_=s['grs'][:], func=AF.Sqrt), r=[k(s['grs'])], w=[k(s['grs'])])
        yield
        V(lambda e: e.reciprocal(s['grs'][:], s['grs'][:]), r=[k(s['grs'])], w=[k(s['grs'])])
        yield
        V(lambda e: e.tensor_tensor(s['go'][:].rearrange("p (h q) -> p h q", q=64), s['go'][:].rearrange("p (h q) -> p h q", q=64),
                                    self.bc(s['grs'][:, :], [128, 4, 64], 2), ALU.mult), r=[k(s['go']), k(s['grs'])], w=[k(s['go'])])
        yield
        V(lambda e: e.tensor_tensor(s['ycat'][:, 768:1024], s['go'][:], s['gsg'][:], ALU.mult), r=[k(s['go']), k(s['gsg'])], w=[k(s['ycat'])])
        yield

    def rwkv_tile(self, first):
        s, p, c = self.s, self.p, self.c
        V, A, G, M = self.V, self.A, self.G, self.M
        k = lambda t: t.key
        f2 = lambda t, a, b: t[:, a:b, :].rearrange("p a b -> p (a b)")
        h3 = lambda ap: ap.rearrange("p (h q) -> p h q", q=64)
        if first:
            G(lambda e: e.memset(s['rw'][:, :, 0:1], 0.0), w=[k(s['rw'])])
            yield
            G(lambda e: e.memset(s['rST'][:], 0.0), w=[k(s['rST'])])
            yield
            G(lambda e: e.memset(s['rSTb'][:], 0.0), w=[k(s['rSTb'])])
            yield
        else:
            G(lambda e: e.tensor_copy(s['rw'][:, :, 0:1], s['rw'][:, :, 128:129]), r=[k(s['rw'])], w=[k(s['rw'])])
            yield
        for grp, nb in ((0, 4), (4, 3)):
            bx, kx = self.bank('r')
            for j in range(nb):
                self.proj_fm(bx[:, j * 128:(j + 1) * 128], kx, O_RW + (grp + j) * 128, 128)
            A(lambda e, bx=bx, grp=grp, nb=nb: e.copy(out=s['rw'][:, grp:grp + nb, 1:129], in_=bx[:, 0:nb * 128].rearrange("p (a b) -> p a b", b=128)),
              r=[kx], w=[k(s['rw'])])
            yield
        rt1 = s['rt1'][:]
        G(lambda e: e.tensor_tensor(rt1, s['rw'][:, :, 0:128], self.bc(p['mu'][:, :], [128, 7, 128], 2), ALU.mult), r=[k(s['rw']), k(p['mu'])], w=[k(s['rt1'])])
        yield
        V(lambda e: e.tensor_tensor(s['rsh'][:], s['rw'][:, :, 1:129], self.bc(p['omu'][:, :], [128, 7, 128], 2), ALU.mult), r=[k(s['rw']), k(p['omu'])], w=[k(s['rsh'])])
        yield
        V(lambda e: e.tensor_tensor(s['rsh'][:], s['rsh'][:], rt1, ALU.add), r=[k(s['rsh']), k(s['rt1'])], w=[k(s['rsh'])])
        yield
        rT, kT, vT, lr = f2(s['rsh'], 0, 2), f2(s['rsh'], 2, 4), f2(s['rsh'], 4, 6), s['rsh'][:, 6, :]
        ksh = k(s['rsh'])
        A(lambda e: e.activation(out=s['rtw'][:], in_=lr, func=AF.Tanh), r=[ksh], w=[k(s['rtw'])])
        yield
        bw, kw = self.bank('r')
        for b in range(2):
            M(lambda e, b=b: e.matmul(bw[:, b * 128:(b + 1) * 128], lhsT=p['w2p'][:, b * 128:(b + 1) * 128], rhs=s['rtw'][:], start=True, stop=True),
              r=[k(p['w2p']), k(s['rtw'])], w=[kw])
        for b in range(2):
            A(lambda e, b=b: e.activation(out=s['ra1'][:, b * 128:(b + 1) * 128], in_=bw[:, b * 128:(b + 1) * 128], func=AF.Identity, bias=p['w0'][:, b:b + 1]),
              r=[kw, k(p['w0'])], w=[k(s['ra1'])])
            yield
        V(lambda e: e.scalar_tensor_tensor(s['ra2'][:], s['ra1'][:], -1.0, s['ra1'][:], ALU.mult, ALU.max), r=[k(s['ra1'])], w=[k(s['ra2'])])
        yield
        A(lambda e: e.activation(out=s['ra2'][:], in_=s['ra2'][:], func=AF.Exp, scale=-1.0), r=[k(s['ra2'])], w=[k(s['ra2'])])
        yield
        A(lambda e: e.activation(out=s['ra2'][:], in_=s['ra2'][:], func=AF.Ln, bias=1.0), r=[k(s['ra2'])], w=[k(s['ra2'])])
        yield
        V(lambda e: e.tensor_scalar(s['ra3'][:], s['ra1'][:], -1.0, 0.0, ALU.mult, ALU.max), r=[k(s['ra1'])], w=[k(s['ra3'])])
        yield
        V(lambda e: e.tensor_tensor(s['ra3'][:], s['ra3'][:], s['ra2'][:], ALU.add), r=[k(s['ra3']), k(s['ra2'])], w=[k(s['ra3'])])
        yield
        A(lambda e: e.activation(out=s['ra1'][:], in_=s['ra3'][:], func=AF.Exp, scale=-1.0), r=[k(s['ra3'])], w=[k(s['ra1'])])
        yield
        V(lambda e: e.tensor_scalar(s['ra1'][:], s['ra1'][:], -float(np.exp(-0.5)), None, ALU.mult), r=[k(s['ra1'])], w=[k(s['ra1'])])
        yield
        V(lambda e: e.tensor_tensor_scan(s['rcw'][:], c['rmask'][:], s['ra1'][:], 0.0, ALU.mult, ALU.add), r=[k(c['rmask']), k(s['ra1'])], w=[k(s['rcw'])])
        yield
        V(lambda e: e.tensor_tensor(s['ra2'][:], s['rcw'][:], s['ra1'][:], ALU.subtract), r=[k(s['rcw']), k(s['ra1'])], w=[k(s['ra2'])])
        yield
        A(lambda e: e.activation(out=s['recw'][:], in_=s['rcw'][:], func=AF.Exp), r=[k(s['rcw'])], w=[k(s['recw'])])
        yield
        A(lambda e: e.activation(out=s['reicw'][:], in_=s['rcw'][:], func=AF.Exp, scale=-1.0), r=[k(s['rcw'])], w=[k(s['reicw'])])
        yield
        A(lambda e: e.activation(out=s['recwp'][:], in_=s['ra2'][:], func=AF.Exp), r=[k(s['ra2'])], w=[k(s['recwp'])])
        yield
        ba_, ka_ = self.bank('r')
        for b in range(2):
            M(lambda e, b=b: e.matmul(ba_[:, b * 128:(b + 1) * 128], lhsT=p['a2p'][:, b * 128:(b + 1) * 128], rhs=lr, start=True, stop=True),
              r=[k(p['a2p']), ksh], w=[ka_])
        for b in range(2):
            A(lambda e, b=b: e.activation(out=s['ra'][:, b * 128:(b + 1) * 128], in_=ba_[:, b * 128:(b + 1) * 128], func=AF.Sigmoid, bias=p['a0'][:, b:b + 1]),
              r=[ka_, k(p['a0'])], w=[k(s['ra'])])
            yield
        A(lambda e: e.activation(out=s['rsg'][:], in_=lr, func=AF.Sigmoid), r=[ksh], w=[k(s['rsg'])])
        yield
        bg, kg = self.bank('r')
        M(lambda e: e.matmul(bg[:, 0:256], lhsT=s['rsg'][:], rhs=p['g2p'][:], start=True, stop=True), r=[k(s['rsg']), k(p['g2p'])], w=[kg])
        A(lambda e: e.copy(out=s['rg'][:], in_=bg[:, 0:256]), r=[kg], w=[k(s['rg'])])
        yield
        V(lambda e: e.tensor_tensor(s['rkk'][:].rearrange("p (a b) -> p a b", b=128), kT.rearrange("p (a b) -> p a b", b=128), self.bc(p['kk'][:, :], [128, 2, 128], 2), ALU.mult),
          r=[ksh, k(p['kk'])], w=[k(s['rkk'])])
        yield
        A(lambda e: e.activation(out=s['ra2'][:], in_=s['rkk'][:], func=AF.Square), r=[k(s['rkk'])], w=[k(s['ra2'])])
        yield
        bn, kn = self.bank('r')
        for b in range(2):
            M(lambda e, b=b: e.matmul(bn[:, b * 128:(b + 1) * 128], lhsT=c['bones'][:], rhs=s['ra2'][:, b * 128:(b + 1) * 128], start=True, stop=True),
              r=[k(c['bones']), k(s['ra2'])], w=[kn])
        V(lambda e: e.tensor_scalar(s['ra3'][:], bn[:, 0:256], 1e-12, None, ALU.add), r=[kn], w=[k(s['ra3'])])
        yield
        A(lambda e: e.activation(out=s['ra3'][:], in_=s['ra3'][:], func=AF.Sqrt), r=[k(s['ra3'])], w=[k(s['ra3'])])
        yield
        V(lambda e: e.reciprocal(s['ra3'][:], s['ra3'][:]), r=[k(s['ra3'])], w=[k(s['ra3'])])
        yield
        V(lambda e: e.tensor_tensor(s['rkk'][:], s['rkk'][:], s['ra3'][:], ALU.mult), r=[k(s['rkk']), k(s['ra3'])], w=[k(s['rkk'])])
        yield
        for b in range(2):
            V(lambda e, b=b: e.tensor_scalar(s['ra2'][:, b * 128:(b + 1) * 128], s['ra'][:, b * 128:(b + 1) * 128], p['ka'][:, b:b + 1], p['omka'][:, b:b + 1], ALU.mult, ALU.add),
              r=[k(s['ra']), k(p['ka']), k(p['omka'])], w=[k(s['ra2'])])
            yield
        V(lambda e: e.tensor_tensor(s['rkp'][:], kT, s['ra2'][:], ALU.mult), r=[ksh, k(s['ra2'])], w=[k(s['rkp'])])
        yield
        V(lambda e: e.tensor_tensor(s['ra3'][:], s['rkk'][:], s['ra'][:], ALU.mult), r=[k(s['rkk']), k(s['ra'])], w=[k(s['ra3'])])
        yield
        for hh in range(2):
            V(lambda e, hh=hh: e.scalar_tensor_tensor(s['rAm'][:, hh, :], s['rkk'][:], c['nhm'][:, hh:hh + 1], s['recwp'][:], ALU.mult, ALU.mult),
              r=[k(s['rkk']), k(c['nhm']), k(s['recwp'])], w=[k(s['rAm'])])
            yield
            V(lambda e, hh=hh: e.scalar_tensor_tensor(s['rRm'][:, hh, :], rT, c['hm'][:, hh:hh + 1], s['recw'][:], ALU.mult, ALU.mult),
              r=[ksh, k(c['hm']), k(s['recw'])], w=[k(s['rRm'])])
            yield
        G(lambda e: e.tensor_tensor(s['rBt'][:], s['ra3'][:], s['reicw'][:], ALU.mult), r=[k(s['ra3']), k(s['reicw'])], w=[k(s['rBt'])])
        yield
        G(lambda e: e.tensor_tensor(s['rKt'][:], s['rkp'][:], s['reicw'][:], ALU.mult), r=[k(s['rkp']), k(s['reicw'])], w=[k(s['rKt'])])
        yield
        V(lambda e: e.tensor_tensor(s['ra2'][:], rT, s['rkp'][:], ALU.mult), r=[ksh, k(s['rkp'])], w=[k(s['ra2'])])
        yield
        V(lambda e: e.tensor_tensor(s['ra2'][:].rearrange("p (a b) -> p a b", b=128), s['ra2'][:].rearrange("p (a b) -> p a b", b=128), self.bc(p['rk'][:, :], [128, 2, 128], 2), ALU.mult),
          r=[k(s['ra2']), k(p['rk'])], w=[k(s['ra2'])])
        yield
        bb, kb = self.bank('r')
        for b in range(2):
            M(lambda e, b=b: e.matmul(bb[:, b * 2:(b + 1) * 2], lhsT=s['ra2'][:, b * 128:(b + 1) * 128], rhs=c['hm'][:, :], start=True, stop=True),
              r=[k(s['ra2']), k(c['hm'])], w=[kb])
        A(lambda e: e.copy(out=s['rbc'][:], in_=bb[:, 0:4]), r=[kb], w=[k(s['rbc'])])
        yield
        bt, kt = self.bank('r')
        for b in range(2):
            M(lambda e, b=b: e.transpose(bt[:, b * 128:(b + 1) * 128], s['rsh'][:, 4 + b, :], c['identf'][:]), r=[ksh, k(c['identf'])], w=[kt])
        A(lambda e: e.copy(out=s['rvtm'][:], in_=bt[:, 0:256]), r=[kt], w=[k(s['rvtm'])])
        yield
        bt, kt = self.bank('r')
        for cc in range(2):
            for b in range(2):
                M(lambda e, bt=bt, cc=cc, b=b: e.transpose(bt[0:64, cc * 256 + b * 128:cc * 256 + (b + 1) * 128], s['rsh'][:, 4 + b, cc * 64:(cc + 1) * 64], c['identf'][:]),
                  r=[ksh, k(c['identf'])], w=[kt])
        A(lambda e, bt=bt: e.copy(out=s['rvc'][:].rearrange("p a b -> p (a b)"), in_=bt[0:64, :]), r=[kt], w=[k(s['rvc'])])
        yield
        for srct, dst in ((s['rBt'], s['rBc']), (s['rKt'], s['rKc'])):
            bt, kt = self.bank('r')
            pbt = bt[:].bitcast(BF16)
            for cc in range(2):
                for b in range(2):
                    M(lambda e, pbt=pbt, srct=srct, cc=cc, b=b: e.transpose(pbt[0:64, cc * 256 + b * 128:cc * 256 + (b + 1) * 128], srct[:, b * 128 + cc * 64:b * 128 + (cc + 1) * 64], c['identb'][:]),
                      r=[k(srct), k(c['identb'])], w=[kt])
            V(lambda e, pbt=pbt, dst=dst: e.tensor_copy(dst[:].rearrange("p a b -> p (a b)"), pbt[0:64, 0:512]), r=[kt], w=[k(dst)])
            yield
        def amat(lt, lkey, lhh, rt_, rkey, rhh, mask, dst):
            bA, kA = self.bank('r')
            for cc in range(2):
                for h in range(4):
                    b, hh = h // 2, h % 2
                    sl_ = slice(b * 128 + cc * 64, b * 128 + (cc + 1) * 64)
                    la = lt[:, hh, sl_] if lhh else lt[:, sl_]
                    ra_ = rt_[:, hh, sl_] if rhh else rt_[:, sl_]
                    i8 = cc * 4 + h
                    M(lambda e, bA=bA, la=la, ra_=ra_, i8=i8: e.matmul(bA[0:64, i8 * 64:(i8 + 1) * 64], lhsT=la, rhs=ra_, start=True, stop=True), r=[lkey, rkey], w=[kA])
            V(lambda e, bA=bA: e.tensor_tensor(dst[:], h3(bA[0:64, :]), self.bc(mask, [64, 8, 64], 1), ALU.mult), r=[kA, k(c['su'])], w=[k(dst)])
            yield
        kAm, kRm, kBt, kKt = k(s['rAm']), k(s['rRm']), k(s['rBt']), k(s['rKt'])
        yield from amat(s['rAm'], kAm, True, s['rBt'], kBt, False, c['su'][0:64, 0:64], s['rP'])
        yield from amat(s['rBt'], kBt, False, s['rAm'], kAm, True, c['sl'][0:64, 0:64], s['rQ'])
        yield from amat(s['rKt'], kKt, False, s['rAm'], kAm, True, c['sl'][0:64, 0:64], s['rAak'])
        yield from amat(s['rBt'], kBt, False, s['rRm'], kRm, True, c['tri'][0:64, 0:64], s['rArb'])
        yield from amat(s['rKt'], kKt, False, s['rRm'], kRm, True, c['tri'][0:64, 0:64], s['rArk'])
        V(lambda e: e.tensor_tensor(s['rTT'][:], s['rQ'][:], self.bc(c['identf'][0:64, 0:64], [64, 8, 64], 1), ALU.add), r=[k(s['rQ']), k(c['identf'])], w=[k(s['rTT'])])
        yield
        Pc, Qc, Pn, Qn = s['rP'], s['rQ'], s['rP2'], s['rQ2']
        for lvl in range(5):
            bP, kP = self.bank('r')
            for i8 in range(8):
                M(lambda e, bP=bP, i8=i8, Pc=Pc, Qc=Qc: e.matmul(bP[0:64, i8 * 64:(i8 + 1) * 64], lhsT=Qc[:, i8, :], rhs=Pc[:, i8, :], start=True, stop=True),
                  r=[k(Pc), k(Qc)], w=[kP])
            A(lambda e, bP=bP, Pn=Pn: e.copy(out=Pn[:], in_=h3(bP[0:64, :])), r=[kP], w=[k(Pn)])
            yield
            if lvl < 4:
                bQ, kQ = self.bank('r')
                for i8 in range(8):
                    M(lambda e, bQ=bQ, i8=i8, Pc=Pc, Qc=Qc: e.matmul(bQ[0:64, i8 * 64:(i8 + 1) * 64], lhsT=Pc[:, i8, :], rhs=Qc[:, i8, :], start=True, stop=True),
                      r=[k(Pc), k(Qc)], w=[kQ])
                V(lambda e, bQ=bQ, Qn=Qn: e.tensor_copy(Qn[:], h3(bQ[0:64, :])), r=[kQ], w=[k(Qn)])
                yield
            bT, kT_ = self.bank('r')
            for i8 in range(8):
                M(lambda e, bT=bT, i8=i8, Pn=Pn: e.matmul(bT[0:64, i8 * 64:(i8 + 1) * 64], lhsT=Pn[:, i8, :], rhs=s['rTT'][:, i8, :], start=True, stop=True),
                  r=[k(Pn), k(s['rTT'])], w=[kT_])
            V(lambda e, bT=bT: e.tensor_tensor(s['rTT'][:], s['rTT'][:], h3(bT[0:64, :]), ALU.add), r=[k(s['rTT']), kT_], w=[k(s['rTT'])])
            yield
            Pc, Qc, Pn, Qn = Pn, Qn, Pc, Qc
        bG, kG = self.bank('r')
        for cc in range(2):
            for h in range(4):
                i8 = cc * 4 + h
                M(lambda e, cc=cc, h=h, i8=i8: e.matmul(bG[0:64, i8 * 64:(i8 + 1) * 64], lhsT=s['rAak'][:, i8, :], rhs=s['rvc'][:, cc, h * 64:(h + 1) * 64], start=True, stop=True),
                  r=[k(s['rAak']), k(s['rvc'])], w=[kG])
        A(lambda e: e.copy(out=s['rAak'][:], in_=h3(bG[0:64, :])), r=[kG], w=[k(s['rAak'])])
        yield
        ewc = s['recw'][:].rearrange("p (a b) -> p a b", b=64)[:, :, 63]
        for cc in range(2):
            bG1, kG1 = self.bank('r')
            for h in range(4):
                b, hh = h // 2, h % 2
                sl_ = slice(b * 128 + cc * 64, b * 128 + (cc + 1) * 64)
                M(lambda e, bG1=bG1, h=h, b=b, hh=hh, sl_=sl_: e.matmul(bG1[0:64, h * 64:(h + 1) * 64], lhsT=s['rAm'][:, hh, sl_], rhs=s['rSTb'][:, b, :], start=True, stop=True),
                  r=[kAm, k(s['rSTb'])], w=[kG1])
            bY1, kY1 = self.bank('r')
            for h in range(4):
                b, hh = h // 2, h % 2
                sl_ = slice(b * 128 + cc * 64, b * 128 + (cc + 1) * 64)
                M(lambda e, bY1=bY1, h=h, b=b, hh=hh, sl_=sl_, cc=cc: e.matmul(bY1[cc * 64:(cc + 1) * 64, h * 64:(h + 1) * 64], lhsT=s['rRm'][:, hh, sl_], rhs=s['rSTb'][:, b, :], start=True, stop=True),
                  r=[kRm, k(s['rSTb'])], w=[kY1])
            A(lambda e, bY1=bY1, cc=cc: e.copy(out=s['rY1'][cc * 64:(cc + 1) * 64, :], in_=bY1[cc * 64:(cc + 1) * 64, 0:256]), r=[kY1], w=[k(s['rY1'])])
            yield
            V(lambda e, bG1=bG1, cc=cc: e.tensor_tensor(s['rG'][:], s['rAak'][:, cc * 4:(cc + 1) * 4, :], h3(bG1[0:64, 0:256]), ALU.add), r=[k(s['rAak']), kG1], w=[k(s['rG'])])
            yield
            bU, kU = self.bank('r')
            for h in range(4):
                i8 = cc * 4 + h
                M(lambda e, bU=bU, h=h, i8=i8: e.matmul(bU[0:64, h * 64:(h + 1) * 64], lhsT=s['rTT'][:, i8, :], rhs=s['rG'][:, h, :], start=True, stop=True),
                  r=[k(s['rTT']), k(s['rG'])], w=[kU])
            A(lambda e, bU=bU: e.copy(out=s['rU'][:], in_=h3(bU[0:64, 0:256])), r=[kU], w=[k(s['rU'])])
            yield
            bY2, kY2 = self.bank('r')
            for h in range(4):
                i8 = cc * 4 + h
                M(lambda e, bY2=bY2, h=h, i8=i8, cc=cc: e.matmul(bY2[cc * 64:(cc + 1) * 64, h * 64:(h + 1) * 64], lhsT=s['rArb'][:, i8, :], rhs=s['rU'][:, h, :], start=True, stop=False),
                  r=[k(s['rArb']), k(s['rU'])], w=[kY2])
                M(lambda e, bY2=bY2, h=h, i8=i8, cc=cc: e.matmul(bY2[cc * 64:(cc + 1) * 64, h * 64:(h + 1) * 64], lhsT=s['rArk'][:, i8, :], rhs=s['rvc'][:, cc, h * 64:(h + 1) * 64], start=False, stop=True),
                  r=[k(s['rArk']), k(s['rvc'])], w=[kY2])
            V(lambda e, bY2=bY2, cc=cc: e.tensor_tensor(s['rY'][cc * 64:(cc + 1) * 64, :], s['rY1'][cc * 64:(cc + 1) * 64, :], bY2[cc * 64:(cc + 1) * 64, 0:256], ALU.add),
              r=[k(s['rY1']), kY2], w=[k(s['rY'])])
            yield
            bS, kS = self.bank('r')
            for h in range(4):
                b, hh = h // 2, h % 2
                i8 = cc * 4 + h
                M(lambda e, bS=bS, h=h, b=b, hh=hh, cc=cc: e.matmul(bS[hh * 64:(hh + 1) * 64, b * 64:(b + 1) * 64], lhsT=s['rBc'][:, cc, h * 64:(h + 1) * 64], rhs=s['rU'][:, h, :], start=True, stop=False),
                  r=[k(s['rBc']), k(s['rU'])], w=[kS])
                M(lambda e, bS=bS, h=h, b=b, hh=hh, cc=cc: e.matmul(bS[hh * 64:(hh + 1) * 64, b * 64:(b + 1) * 64], lhsT=s['rKc'][:, cc, h * 64:(h + 1) * 64], rhs=s['rvc'][:, cc, h * 64:(h + 1) * 64], start=False, stop=True),
                  r=[k(s['rKc']), k(s['rvc'])], w=[kS])
            V(lambda e, bS=bS: e.tensor_tensor(s['rt2'][:], s['rST'][:], h3(bS[:, 0:128]), ALU.add), r=[k(s['rST']), kS], w=[k(s['rt2'])])
            yield
            V(lambda e, cc=cc: e.tensor_tensor(s['rST'][:], s['rt2'][:], self.bc(ewc[:, cc::2], [128, 2, 64], 2), ALU.mult), r=[k(s['rt2']), k(s['recw'])], w=[k(s['rST'])])
            yield
            A(lambda e: e.copy(out=s['rSTb'][:], in_=s['rST'][:]), r=[k(s['rST'])], w=[k(s['rSTb'])])
            yield
        V(lambda e: e.tensor_reduce(s['rm1'][:], h3(s['rY'][:]), AX.X, ALU.add), r=[k(s['rY'])], w=[k(s['rm1'])])
        yield
        A(lambda e: e.activation(out=s['rY1'][:], in_=s['rY'][:], func=AF.Square), r=[k(s['rY'])], w=[k(s['rY1'])])
        yield
        V(lambda e: e.tensor_reduce(s['rm2'][:], h3(s['rY1'][:]), AX.X, ALU.add), r=[k(s['rY1'])], w=[k(s['rm2'])])
        yield
        V(lambda e: e.tensor_scalar(s['rm1'][:], s['rm1'][:], 1.0 / 64, None, ALU.mult), r=[k(s['rm1'])], w=[k(s['rm1'])])
        yield
        V(lambda e: e.tensor_tensor(s['rvar'][:], s['rm1'][:], s['rm1'][:], ALU.mult), r=[k(s['rm1'])], w=[k(s['rvar'])])
        yield
        V(lambda e: e.scalar_tensor_tensor(s['rvar'][:], s['rm2'][:], 1.0 / 64, s['rvar'][:], ALU.mult, ALU.subtract), r=[k(s['rm2']), k(s['rvar'])], w=[k(s['rvar'])])
        yield
        V(lambda e: e.tensor_scalar(s['rvar'][:], s['rvar'][:], GN_EPS, None, ALU.add), r=[k(s['rvar'])], w=[k(s['rvar'])])
        yield
        A(lambda e: e.activation(out=s['rvar'][:], in_=s['rvar'][:], func=AF.Sqrt), r=[k(s['rvar'])], w=[k(s['rvar'])])
        yield
        V(lambda e: e.reciprocal(s['rvar'][:], s['rvar'][:]), r=[k(s['rvar'])], w=[k(s['rvar'])])
        yield
        V(lambda e: e.tensor_tensor(h3(s['rY'][:]), h3(s['rY'][:]), self.bc(s['rm1'][:, :], [128, 4, 64], 2), ALU.subtract), r=[k(s['rY']), k(s['rm1'])], w=[k(s['rY'])])
        yield
        V(lambda e: e.tensor_tensor(h3(s['rY'][:]), h3(s['rY'][:]), self.bc(s['rvar'][:, :], [128, 4, 64], 2), ALU.mult), r=[k(s['rY']), k(s['rvar'])], w=[k(s['rY'])])
        yield
        G(lambda e: e.tensor_tensor(s['rY'][:], s['rY'][:], p['rlg'][:], ALU.mult), r=[k(s['rY']), k(p['rlg'])], w=[k(s['rY'])])
        yield
        G(lambda e: e.tensor_tensor(s['rY'][:], s['rY'][:], p['rlb'][:], ALU.add), r=[k(s['rY']), k(p['rlb'])], w=[k(s['rY'])])
        yield
        V(lambda e: e.tensor_tensor(h3(s['rY1'][:]), h3(s['rvtm'][:]), self.bc(s['rbc'][:, :], [128, 4, 64], 2), ALU.mult), r=[k(s['rvtm']), k(s['rbc'])], w=[k(s['rY1'])])
        yield
        V(lambda e: e.tensor_tensor(s['rY'][:], s['rY'][:], s['rY1'][:], ALU.add), r=[k(s['rY']), k(s['rY1'])], w=[k(s['rY'])])
        yield
        V(lambda e: e.tensor_tensor(s['ycat'][:, 512:768], s['rY'][:], s['rg'][:], ALU.mult), r=[k(s['rY']), k(s['rg'])], w=[k(s['ycat'])])
        yield

    def mixer_epilogue(self, l, i):
        s, p, c = self.s, self.p, self.c
        V, A, G, M = self.V, self.A, self.G, self.M
        k = lambda t: t.key
        self.load('sp', s['htm'][:], self.h_d[i * 128:(i + 1) * 128, :], k(s['htm']), dkeys=["hd_%d" % i])
        if self.debug:
            self.A(lambda e: e.copy(out=s['tmp'][:], in_=s['ycat'][:]), r=[k(s['ycat'])], w=[k(s['tmp'])])
            yield
            self.store('sp', self.dbg_y[i * 128:(i + 1) * 128, :], s['tmp'][:], k(s['tmp']))
            yield
        bt, kt = self.bank('e')
        pb = bt[:].bitcast(BF16)
        for kc in range(8):
            M(lambda e, kc=kc: e.transpose(pb[:, kc * 128:(kc + 1) * 128], s['ycat'][:, kc * 128:(kc + 1) * 128], c['identb'][:]), r=[k(s['ycat']), k(c['identb'])], w=[kt])
        V(lambda e: e.tensor_copy(s['yT'][:].rearrange("p a b -> p (a b)"), pb), r=[kt], w=[k(s['yT'])])
        yield
        for half in range(2):
            bo, ko = self.bank('e')
            for kc in range(8):
                M(lambda e, bo=bo, kc=kc, half=half: e.matmul(bo[:, :], lhsT=s['yT'][:, kc, :], rhs=p['w_out'][:, kc, half * 512:(half + 1) * 512], start=(kc == 0), stop=(kc == 7)),
                  r=[k(s['yT']), k(p['w_out'])], w=[ko])
            V(lambda e, bo=bo, half=half: e.scalar_tensor_tensor(s['mix'][:, half * 512:(half + 1) * 512], s['htm'][:, half * 512:(half + 1) * 512], ALPHA, bo[:, :], ALU.mult, ALU.add),
              r=[k(s['htm']), ko], w=[k(s['mix'])])
            yield
        yield from self.layernorm(s['mix'], p['l1g'], p['l1b'], s['h1'], s['tmp'])
        tk = "h1d_%d_%d" % (l, i)
        self.store('sp', self.h1_d[i * 128:(i + 1) * 128, :], s['h1'][:], k(s['h1']), dkeys=[tk])
        yield
        self.store('pool', self.h1b_d[i * 128:(i + 1) * 128, :], s['h1'][:], k(s['h1']), dkeys=["h1bd_%d_%d" % (l, i)])
        yield
        if hasattr(self, 'xs_d'):
            yield from self.router_tile(l, i)

    def stage0(self):
        s, p, d = self.s, self.p, self.d
        k = lambda t: t.key
        mark = self.sb_off
        self.load('sp', p['l1g'][:], d['ln_in_g'].ap().partition_broadcast(128), k(p['l1g']))
        self.load('sp', p['l1b'][:], d['ln_in_b'].ap().partition_broadcast(128), k(p['l1b']))
        sets = [dict(x=s['mix'], h=s['htm'], tmp=s['tmp'], hb=s['ycat'], hT=s['hT'], st=self.ln_stats("a"))]
        hb2 = Tile(s['yT'].t.ap().rearrange("p a b -> p (a b)") if False else s['yT'].t, s['yT'].key)
        sets.append(dict(x=s['h1'], h=self.sb([128, D], name="s0_h"), tmp=self.sb([128, D], name="s0_tmp"),
                         hb=hb2, hT=self.sb([128, 8, 128], BF16, "s0_hT"), st=self.ln_stats("b")))

        def body(i):
            B = sets[i % 2]
            self.load('sp', B['x'][:], d['x'][i * 128:(i + 1) * 128, :], k(B['x']))
            yield
            yield from self.layernorm(B['x'], p['l1g'], p['l1b'], B['h'], B['tmp'], B['st'])
            self.store('sp', self.h_d[i * 128:(i + 1) * 128, :], B['h'][:], k(B['h']), dkeys=["hd_%d" % i])
            yield
            hbf = B['hb'][:] if len(B['hb'][:].shape) == 2 else B['hb'][:].rearrange("p a b -> p (a b)")
            self.A(lambda e: e.copy(out=hbf, in_=B['h'][:]), r=[k(B['h'])], w=[k(B['hb'])])
            yield
            bk, bkey = self.bank()
            pb = bk[:].bitcast(BF16)
            for kc in range(8):
                self.M(lambda e, kc=kc: e.transpose(pb[:, kc * 128:(kc + 1) * 128], hbf[:, kc * 128:(kc + 1) * 128], self.c['identb'][:]),
                       r=[k(B['hb']), k(self.c['identb'])], w=[bkey])
            self.V(lambda e: e.tensor_copy(B['hT'][:].rearrange("p a b -> p (a b)"), pb), r=[bkey], w=[k(B['hT'])])
            yield
            self.store('sp', self.hT_d[:, :, i * 128:(i + 1) * 128], B['hT'][:], k(B['hT']), dkeys=["hTd_%d" % i])
            yield
        self.run_pipe([lambda i=i: body(i) for i in range(self.NT)], 2)
        self.sb_off = mark

    def stageM(self, l):
        import os
        s = self.s
        k = lambda t: t.key
        only = os.environ.get("ONLY", "srg")
        prev = None
        for i in range(self.NT + 1):
            gens = []
            if i < self.NT:
                self.load('sp', s['hT'][:], self.hT_d[:, :, i * 128:(i + 1) * 128], k(s['hT']), dkeys=["hTd_%d" % i])
                if 'r' in only:
                    gens.append(self.rwkv_tile(i == 0))
                if 's' in only:
                    gens.append(self.ssd_tile(i == 0))
                if 'g' in only:
                    gens.append(self.gla_tile(i == 0))
            if prev is not None:
                gens.append(self.mixer_epilogue(l, prev))
            prev = i if i < self.NT else None
            wts = [int(x) for x in os.environ.get("ILW", "3,1,1,1").split(",")]
            gw = {id(g_): (wts[0] if j == 0 and i < self.NT and 'r' in only else 1) for j, g_ in enumerate(gens)}
            while gens:
                for g_ in list(gens):
                    for _ in range(gw[id(g_)]):
                        try:
                            next(g_)
                        except StopIteration:
                            gens.remove(g_)
                            break

    def build_mixer_test(self):
        self.declare_inputs()
        T = self.T
        self.h_d = self.dscr("h_d", [T, D])
        self.hT_d = self.dscr("hT_d", [128, 8, T], BF16)
        self.h1_d = self.dout("h1_d", [T, D])
        self.h1b_d = self.dscr("h1b_d", [T, D], BF16)
        self.dbg_y = self.dout("dbg_y", [T, D])
        self.consts()
        self.alloc_params()
        self.alloc_mixer()
        print("sbuf peak", self.sb_peak)
        self.stage0()
        self.P.barrier()
        self.load_params(0)
        self.stageM(0)
        self.P.barrier()
        return self.nc

    def alloc_router(self):
        rt = self.rt = {}
        for n, w in (('lg', 36), ('gmx', 1), ('goh', 4), ('gex', 4), ('gsum', 1), ('t32', 32), ('el8', 8), ('el8m', 8), ('l1', 1), ('l2', 1),
                     ('oh1', 8), ('oh2', 8), ('w1', 1), ('w2', 1), ('E1', 32), ('E2', 32), ('Mm', 32), ('rk', 32)):
            rt[n] = self.sb([128, w], name="rt_" + n)

    def alloc_route_persist(self):
        rp = self.rp = {}
        NT = self.NT
        rp['eid'] = self.sb([128, NT * 2], name="rp_eid")
        rp['rnk'] = self.sb([128, NT * 2], name="rp_rnk")
        rp['gat'] = self.sb([128, NT * 2], name="rp_gat")
        rp['cnt'] = self.sb([128, NE], name="rp_cnt")
        rp['iota32'] = self.sb([128, NE], name="rp_iota32")
        ii = self.sb([128, NE], I32, "rp_iota32i")
        self.G(lambda e: e.iota(ii[:], pattern=[[1, NE]], base=0, channel_multiplier=0), w=[ii.key])
        self.V(lambda e: e.tensor_copy(rp['iota32'][:], ii[:]), r=[ii.key], w=[rp['iota32'].key])

    def router_tile(self, l, i):
        s, p, c = self.s, self.p, self.c
        V, A, G, M = self.V, self.A, self.G, self.M
        k = lambda t: t.key
        rt, rp = self.rt, self.rp
        if i == 0:
            G(lambda e: e.memset(rp['cnt'][:], 0.0), w=[k(rp['cnt'])])
            yield
        hT32 = s['tmp'][:].rearrange("p (a b) -> p a b", b=128)
        for half in range(2):
            bt, kt = self.bank('e')
            for j in range(4):
                kc = half * 4 + j
                M(lambda e, bt=bt, j=j, kc=kc: e.transpose(bt[:, j * 128:(j + 1) * 128], s['h1'][:, kc * 128:(kc + 1) * 128], c['identf'][:]), r=[k(s['h1']), k(c['identf'])], w=[kt])
            A(lambda e, bt=bt, half=half: e.copy(out=s['tmp'][:, half * 512:(half + 1) * 512], in_=bt[:, :]), r=[kt], w=[k(s['tmp'])])
            yield
        bl, kl = self.bank('e')
        for kc in range(8):
            M(lambda e, kc=kc: e.matmul(bl[:, 0:36], lhsT=hT32[:, kc, :], rhs=p['wr'][:, kc, :], start=(kc == 0), stop=(kc == 7)), r=[k(s['tmp']), k(p['wr'])], w=[kl])
        V(lambda e: e.tensor_tensor(rt['lg'][:], bl[:, 0:36], p['rb36'][:], ALU.add), r=[kl, k(p['rb36'])], w=[k(rt['lg'])])
        yield
        V(lambda e: e.tensor_reduce(rt['gmx'][:], rt['lg'][:, 0:4], AX.X, ALU.max), r=[k(rt['lg'])], w=[k(rt['gmx'])])
        yield
        V(lambda e: e.tensor_scalar(rt['goh'][:], rt['lg'][:, 0:4], rt['gmx'][:, 0:1], None, ALU.is_equal), r=[k(rt['lg']), k(rt['gmx'])], w=[k(rt['goh'])])
        yield
        V(lambda e: e.tensor_scalar(rt['gex'][:], rt['lg'][:, 0:4], rt['gmx'][:, 0:1], None, ALU.subtract), r=[k(rt['lg']), k(rt['gmx'])], w=[k(rt['gex'])])
        yield
        A(lambda e: e.activation(out=rt['gex'][:], in_=rt['gex'][:], func=AF.Exp), r=[k(rt['gex'])], w=[k(rt['gex'])])
        yield
        V(lambda e: e.tensor_reduce(rt['gsum'][:], rt['gex'][:], AX.X, ALU.add), r=[k(rt['gex'])], w=[k(rt['gsum'])])
        yield
        V(lambda e: e.reciprocal(rt['gsum'][:], rt['gsum'][:]), r=[k(rt['gsum'])], w=[k(rt['gsum'])])
        yield
        V(lambda e: e.tensor_tensor(rt['t32'][:].rearrange("p (g j) -> p g j", j=8), rt['lg'][:, 4:36].rearrange("p (g j) -> p g j", j=8),
                                    self.bc(rt['goh'][:, :], [128, 4, 8], 2), ALU.mult), r=[k(rt['lg']), k(rt['goh'])], w=[k(rt['t32'])])
        yield
        V(lambda e: e.tensor_reduce(rt['el8'][:], rt['t32'][:].rearrange("p (g j) -> p j g", j=8), AX.X, ALU.add), r=[k(rt['t32'])], w=[k(rt['el8'])])
        yield
        V(lambda e: e.tensor_reduce(rt['l1'][:], rt['el8'][:], AX.X, ALU.max), r=[k(rt['el8'])], w=[k(rt['l1'])])
        yield
        V(lambda e: e.tensor_scalar(rt['oh1'][:], rt['el8'][:], rt['l1'][:, 0:1], None, ALU.is_equal), r=[k(rt['el8']), k(rt['l1'])], w=[k(rt['oh1'])])
        yield
        V(lambda e: e.scalar_tensor_tensor(rt['el8m'][:], rt['oh1'][:], -1e30, rt['el8'][:], ALU.mult, ALU.add), r=[k(rt['oh1']), k(rt['el8'])], w=[k(rt['el8m'])])
        yield
        V(lambda e: e.tensor_reduce(rt['l2'][:], rt['el8m'][:], AX.X, ALU.max), r=[k(rt['el8m'])], w=[k(rt['l2'])])
        yield
        V(lambda e: e.tensor_scalar(rt['oh2'][:], rt['el8m'][:], rt['l2'][:, 0:1], None, ALU.is_equal), r=[k(rt['el8m']), k(rt['l2'])], w=[k(rt['oh2'])])
        yield
        V(lambda e: e.tensor_tensor(rt['w2'][:], rt['l2'][:], rt['l1'][:], ALU.subtract), r=[k(rt['l2']), k(rt['l1'])], w=[k(rt['w2'])])
        yield
        A(lambda e: e.activation(out=rt['w2'][:], in_=rt['w2'][:], func=AF.Exp), r=[k(rt['w2'])], w=[k(rt['w2'])])
        yield
        V(lambda e: e.tensor_scalar(rt['w1'][:], rt['w2'][:], 1.0, None, ALU.add), r=[k(rt['w2'])], w=[k(rt['w1'])])
        yield
        V(lambda e: e.reciprocal(rt['w1'][:], rt['w1'][:]), r=[k(rt['w1'])], w=[k(rt['w1'])])
        yield
        V(lambda e: e.tensor_tensor(rt['w2'][:], rt['w2'][:], rt['w1'][:], ALU.mult), r=[k(rt['w2']), k(rt['w1'])], w=[k(rt['w2'])])
        yield
        V(lambda e: e.tensor_tensor(rp['gat'][:, 2 * i:2 * i + 1], rt['w1'][:], rt['gsum'][:], ALU.mult), r=[k(rt['w1']), k(rt['gsum'])], w=[k(rp['gat'])])
        yield
        V(lambda e: e.tensor_tensor(rp['gat'][:, 2 * i + 1:2 * i + 2], rt['w2'][:], rt['gsum'][:], ALU.mult), r=[k(rt['w2']), k(rt['gsum'])], w=[k(rp['gat'])])
        yield
        for E, oh in ((rt['E1'], rt['oh1']), (rt['E2'], rt['oh2'])):
            V(lambda e, E=E, oh=oh: e.tensor_tensor(E[:].rearrange("p (g j) -> p g j", j=8), self.bc(rt['goh'][:, :], [128, 4, 8], 2), self.bc(oh[:, :], [128, 4, 8], 1), ALU.mult),
              r=[k(rt['goh']), k(oh)], w=[k(E)])
            yield
        V(lambda e: e.tensor_tensor(rt['Mm'][:], rt['E1'][:], rt['E2'][:], ALU.add), r=[k(rt['E1']), k(rt['E2'])], w=[k(rt['Mm'])])
        yield
        br, kr = self.bank('e')
        M(lambda e: e.matmul(br[:, 0:32], lhsT=c['sl'][:], rhs=rt['Mm'][:], start=True, stop=True), r=[k(c['sl']), k(rt['Mm'])], w=[kr])
        M(lambda e: e.matmul(br[:, 32:64], lhsT=c['onesf'][:], rhs=rt['Mm'][:], start=True, stop=True), r=[k(c['onesf']), k(rt['Mm'])], w=[kr])
        V(lambda e: e.tensor_tensor(rt['rk'][:], br[:, 0:32], rp['cnt'][:], ALU.add), r=[kr, k(rp['cnt'])], w=[k(rt['rk'])])
        yield
        V(lambda e: e.tensor_tensor(rp['cnt'][:], rp['cnt'][:], br[:, 32:64], ALU.add), r=[kr, k(rp['cnt'])], w=[k(rp['cnt'])])
        yield
        for j, E in ((0, rt['E1']), (1, rt['E2'])):
            V(lambda e, E=E: e.tensor_tensor(rt['t32'][:], E[:], rt['rk'][:], ALU.mult), r=[k(E), k(rt['rk'])], w=[k(rt['t32'])])
            yield
            V(lambda e, j=j: e.tensor_reduce(rp['rnk'][:, 2 * i + j:2 * i + j + 1], rt['t32'][:], AX.X, ALU.add), r=[k(rt['t32'])], w=[k(rp['rnk'])])
            yield
            V(lambda e, E=E: e.tensor_tensor(rt['t32'][:], E[:], rp['iota32'][:], ALU.mult), r=[k(E), k(rp['iota32'])], w=[k(rt['t32'])])
            yield
            V(lambda e, j=j: e.tensor_reduce(rp['eid'][:, 2 * i + j:2 * i + j + 1], rt['t32'][:], AX.X, ALU.add), r=[k(rt['t32'])], w=[k(rp['eid'])])
            yield

    def stageMoE(self, l, last):
        d, c, rp = self.d, self.c, self.rp
        V, A, G, M = self.V, self.A, self.G, self.M
        k = lambda t: t.key
        mark = self.sb_off
        sb = self.sb
        NT, NB, RB = self.NT, self.NB, self.RB
        NR = RB // 128
        NC = NT * 2
        thr_i = sb([128, 64], I32, "f_thri")
        thr = sb([128, 64], name="f_thr")
        G(lambda e: e.iota(thr_i[:], pattern=[[RB, 64]], base=0, channel_multiplier=0), w=[k(thr_i)])
        V(lambda e: e.tensor_copy(thr[:], thr_i[:]), r=[k(thr_i)], w=[k(thr)])
        big = sb([128, max(NC * NE, NE * 64, NB * NE)], name="f_big")
        nblk = sb([128, NE], name="f_nblk")
        padded = sb([128, NE], name="f_padded")
        pend = sb([128, NE], name="f_pend")
        pstart = sb([128, NE], name="f_pstart")
        cmp3 = big[:, 0:NE * 64].rearrange("p (e m) -> p e m", m=64)
        V(lambda e: e.tensor_tensor(cmp3, self.bc(rp['cnt'][:, :], [128, NE, 64], 2), self.bc(thr[:, :], [128, NE, 64], 1), ALU.is_gt), r=[k(rp['cnt']), k(thr)], w=[k(big)])
        V(lambda e: e.tensor_reduce(nblk[:], cmp3, AX.X, ALU.add), r=[k(big)], w=[k(nblk)])
        V(lambda e: e.tensor_scalar(padded[:], nblk[:], float(RB), None, ALU.mult), r=[k(nblk)], w=[k(padded)])
        V(lambda e: e.tensor_tensor_scan(pend[:], c['onesf'][:, 0:NE], padded[:], 0.0, ALU.mult, ALU.add), r=[k(c['onesf']), k(padded)], w=[k(pend)])
        V(lambda e: e.tensor_tensor(pstart[:], pend[:], padded[:], ALU.subtract), r=[k(pend), k(padded)], w=[k(pstart)])
        oh3 = big[:, 0:NC * NE].rearrange("p (n e) -> p n e", e=NE)
        destf = sb([128, NC], name="f_destf")
        dest = sb([128, NC], I32, "f_dest")
        V(lambda e: e.tensor_tensor(oh3, self.bc(rp['iota32'][:, :], [128, NC, NE], 1), self.bc(rp['eid'][:, :], [128, NC, NE], 2), ALU.is_equal), r=[k(rp['iota32']), k(rp['eid'])], w=[k(big)])
        V(lambda e: e.tensor_tensor(oh3, oh3, self.bc(pstart[:, :], [128, NC, NE], 1), ALU.mult), r=[k(big), k(pstart)], w=[k(big)])
        V(lambda e: e.tensor_reduce(destf[:], oh3, AX.X, ALU.add), r=[k(big)], w=[k(destf)])
        V(lambda e: e.tensor_tensor(destf[:], destf[:], rp['rnk'][:], ALU.add), r=[k(destf), k(rp['rnk'])], w=[k(destf)])
        V(lambda e: e.tensor_copy(dest[:], destf[:]), r=[k(destf)], w=[k(dest)])
        bs_i = sb([128, NB], I32, "f_bsi")
        bstart = sb([128, NB], name="f_bstart")
        be = sb([128, NB], name="f_be")
        G(lambda e: e.iota(bs_i[:], pattern=[[RB, NB]], base=0, channel_multiplier=0), w=[k(bs_i)])
        V(lambda e: e.tensor_copy(bstart[:], bs_i[:]), r=[k(bs_i)], w=[k(bstart)])
        cmpb = big[:, 0:NB * NE].rearrange("p (b e) -> p b e", e=NE)
        V(lambda e: e.tensor_tensor(cmpb, self.bc(pend[:, :], [128, NB, NE], 1), self.bc(bstart[:, :], [128, NB, NE], 2), ALU.is_le), r=[k(pend), k(bstart)], w=[k(big)])
        V(lambda e: e.tensor_reduce(be[:], cmpb, AX.X, ALU.add), r=[k(big)], w=[k(be)])
        V(lambda e: e.tensor_scalar(be[:], be[:], float(NE - 1), None, ALU.min), r=[k(be)], w=[k(be)])
        kp_i = sb([128, 8], I32, "f_kpi")
        kp = sb([128, 8], name="f_kp")
        G(lambda e: e.iota(kp_i[:], pattern=[[128, 8]], base=0, channel_multiplier=1), w=[k(kp_i)])
        V(lambda e: e.tensor_copy(kp[:], kp_i[:]), r=[k(kp_i)], w=[k(kp)])
        widf = sb([128, NB, 8], name="f_widf")
        wid = sb([128, NB, 8], I32, "f_wid")
        didf = sb([128, NB, 4], name="f_didf")
        did = sb([128, NB, 4], I32, "f_did")
        bew = sb([128, NB], name="f_bew")
        inval = sb([128, NB], name="f_inval")
        V(lambda e: e.tensor_scalar(inval[:], bstart[:], pend[:, NE - 1:NE], 1.0e9, ALU.is_ge, ALU.mult), r=[k(bstart), k(pend)], w=[k(inval)])
        V(lambda e: e.tensor_scalar(bew[:], be[:], float(D), float(l * NE * D), ALU.mult, ALU.add), r=[k(be)], w=[k(bew)])
        V(lambda e: e.tensor_tensor(bew[:], bew[:], inval[:], ALU.add), r=[k(bew), k(inval)], w=[k(bew)])
        V(lambda e: e.tensor_tensor(widf[:], self.bc(bew[:, :], [128, NB, 8], 2), self.bc(kp[:, :], [128, NB, 8], 1), ALU.add), r=[k(bew), k(kp)], w=[k(widf)])
        V(lambda e: e.tensor_copy(wid[:], widf[:]), r=[k(widf)], w=[k(wid)])
        V(lambda e: e.tensor_scalar(bew[:], be[:], float(FF), float(l * NE * FF), ALU.mult, ALU.add), r=[k(be)], w=[k(bew)])
        V(lambda e: e.tensor_tensor(bew[:], bew[:], inval[:], ALU.add), r=[k(bew), k(inval)], w=[k(bew)])
        V(lambda e: e.tensor_tensor(didf[:], self.bc(bew[:, :], [128, NB, 4], 2), self.bc(kp[:, 0:4], [128, NB, 4], 1), ALU.add), r=[k(bew), k(kp)], w=[k(didf)])
        V(lambda e: e.tensor_copy(did[:], didf[:]), r=[k(didf)], w=[k(did)])
        hbs = [sb([128, D], BF16, "e_hb%d" % j) for j in range(2)]
        for i in range(NT):
            hb = hbs[i % 2]
            self.load('sp', hb[:], self.h1b_d[i * 128:(i + 1) * 128, :], k(hb), dkeys=["h1bd_%d_%d" % (l, i)])
            for j in range(2):
                col = 2 * i + j
                self.P.dma('pool', lambda e, col=col, hb=hb: e.indirect_dma_start(out=self.xs_d.ap(), out_offset=bass.IndirectOffsetOnAxis(ap=dest[:, col:col + 1], axis=0),
                                                                                  in_=hb[:], in_offset=None), k(hb), r=[k(hb), k(dest)], w=["xs_d"])
        self.P.barrier()
        import os
        mstop = int(os.environ.get('MOESTOP', '9'))
        if mstop <= 1:
            self.sb_off = mark
            return
        wg = [sb([128, 8, FF], BF16, "e_wg%d" % j) for j in range(2)]
        wu = [sb([128, 8, FF], BF16, "e_wu%d" % j) for j in range(2)]
        wd = [sb([128, 4, D], BF16, "e_wd%d" % j) for j in range(2)]
        xs = [sb([128, NR, D], BF16, "e_xs%d" % j) for j in range(2)]
        xsT = sb([128, 8, RB], BF16, "e_xsT")
        hT = sb([128, 4, RB], BF16, "e_hT")
        sg = sb([128, RB], name="e_sg")
        ys = [sb([128, D], name="e_ys%d" % j) for j in range(2)]
        tg, tu, td = d['moe_w_gate'].ap(), d['moe_w_up'].ap(), d['moe_w_down'].ap()
        if not hasattr(self, '_bc_regs'):
            self._bc_regs = (self.nc.gpsimd.to_reg(DEPTH * NE * D - 1), self.nc.gpsimd.to_reg(DEPTH * NE * FF - 1))
        bc_g, bc_d = self._bc_regs
        nys = 0
        for b in range(NB):
            j = b % 2
            for kc in range(8):
                self.P.dma('pool', lambda e, j=j, b=b, kc=kc: e.indirect_dma_start(out=wg[j][:, kc, :], out_offset=None, in_=tg,
                                                                                  in_offset=bass.IndirectOffsetOnAxis(ap=wid[:, b, kc:kc + 1], axis=0), bounds_check=bc_g, oob_is_err=False), k(wg[j]), r=[k(wid)], w=[k(wg[j])])
                self.P.dma('pool', lambda e, j=j, b=b, kc=kc: e.indirect_dma_start(out=wu[j][:, kc, :], out_offset=None, in_=tu,
                                                                                  in_offset=bass.IndirectOffsetOnAxis(ap=wid[:, b, kc:kc + 1], axis=0), bounds_check=bc_g, oob_is_err=False), k(wu[j]), r=[k(wid)], w=[k(wu[j])])
            for fc in range(4):
                self.P.dma('pool', lambda e, j=j, b=b, fc=fc: e.indirect_dma_start(out=wd[j][:, fc, :], out_offset=None, in_=td,
                                                                                  in_offset=bass.IndirectOffsetOnAxis(ap=did[:, b, fc:fc + 1], axis=0), bounds_check=bc_d, oob_is_err=False), k(wd[j]), r=[k(did)], w=[k(wd[j])])
            self.load('sp', xs[j][:], self.xs_d[b * RB:(b + 1) * RB, :].rearrange("(r p) n -> p r n", p=128), k(xs[j]), dkeys=["xs_d"])
            for r_ in range(NR):
                bt, kt = self.bank()
                pb = bt[:].bitcast(BF16)
                for kc in range(8):
                    M(lambda e, pb=pb, j=j, r_=r_, kc=kc: e.transpose(pb[:, kc * 128:(kc + 1) * 128], xs[j][:, r_, kc * 128:(kc + 1) * 128], c['identb'][:]), r=[k(xs[j]), k(c['identb'])], w=[kt])
                V(lambda e, pb=pb, r_=r_: e.tensor_copy(xsT[:, :, r_ * 128:(r_ + 1) * 128], pb.rearrange("p (a b) -> p a b", b=128)), r=[kt], w=[k(xsT)])
            for fc in range(4):
                bg, kg = self.bank()
                for kc in range(8):
                    M(lambda e, bg=bg, kc=kc, fc=fc, j=j: e.matmul(bg[:, 0:RB], lhsT=wg[j][:, kc, fc * 128:(fc + 1) * 128], rhs=xsT[:, kc, :], start=(kc == 0), stop=(kc == 7)),
                      r=[k(wg[j]), k(xsT)], w=[kg])
                bu, ku = self.bank()
                for kc in range(8):
                    M(lambda e, bu=bu, kc=kc, fc=fc, j=j: e.matmul(bu[:, 0:RB], lhsT=wu[j][:, kc, fc * 128:(fc + 1) * 128], rhs=xsT[:, kc, :], start=(kc == 0), stop=(kc == 7)),
                      r=[k(wu[j]), k(xsT)], w=[ku])
                A(lambda e, bg=bg: e.activation(out=sg[:], in_=bg[:, 0:RB], func=AF.Silu), r=[kg], w=[k(sg)])
                V(lambda e, bu=bu, fc=fc: e.tensor_tensor(hT[:, fc, :], sg[:], bu[:, 0:RB], ALU.mult), r=[k(sg), ku], w=[k(hT)])
            for r_ in range(NR):
                yb = ys[nys % 2]
                nys += 1
                for half in range(2):
                    bo, ko = self.bank()
                    for fc in range(4):
                        M(lambda e, bo=bo, fc=fc, r_=r_, half=half, j=j: e.matmul(bo[:, :], lhsT=hT[:, fc, r_ * 128:(r_ + 1) * 128], rhs=wd[j][:, fc, half * 512:(half + 1) * 512],
                                                                                 start=(fc == 0), stop=(fc == 3)), r=[k(hT), k(wd[j])], w=[ko])
                    if half == 0:
                        A(lambda e, bo=bo, yb=yb: e.copy(out=yb[:, 0:512], in_=bo[:, :]), r=[ko], w=[k(yb)])
                    else:
                        V(lambda e, bo=bo, yb=yb: e.tensor_copy(yb[:, 512:1024], bo[:, :]), r=[ko], w=[k(yb)])
                r0 = b * RB + r_ * 128
                self.store('sp', self.ys_d[r0:r0 + 128, :], yb[:], k(yb), dkeys=["ys_d"])
        self.P.barrier()
        if mstop <= 2:
            self.sb_off = mark
            return
        l2g = sb([128, D], name="c_l2g")
        l2b = sb([128, D], name="c_l2b")
        self.load('sp', l2g[:], d['ln2_g'][l].partition_broadcast(128), k(l2g))
        self.load('sp', l2b[:], d['ln2_b'][l].partition_broadcast(128), k(l2b))
        csets = []
        for j in range(2):
            csets.append(dict(h1=sb([128, D], name="c_h1%d" % j), y0=sb([128, D], name="c_y0%d" % j), y1=sb([128, D], name="c_y1%d" % j),
                              tmp=sb([128, D], name="c_tmp%d" % j), h2=sb([128, D], name="c_h2%d" % j), hb=sb([128, D], BF16, "c_hb%d" % j),
                              hT=sb([128, 8, 128], BF16, "c_hT%d" % j), st=self.ln_stats("c%d" % j)))

        def cbody(i):
            B = csets[i % 2]
            h1, y0, y1 = B['h1'], B['y0'], B['y1']
            self.load('sp', h1[:], self.h1_d[i * 128:(i + 1) * 128, :], k(h1), dkeys=["h1d_%d_%d" % (l, i)])
            for j, yt in ((0, y0), (1, y1)):
                col = 2 * i + j
                self.P.dma('pool', lambda e, col=col, yt=yt: e.indirect_dma_start(out=yt[:], out_offset=None, in_=self.ys_d.ap(),
                                                                                 in_offset=bass.IndirectOffsetOnAxis(ap=dest[:, col:col + 1], axis=0)), k(yt), r=[k(dest), "ys_d"], w=[k(yt)])
            yield
            V(lambda e: e.tensor_scalar(y0[:], y0[:], rp['gat'][:, 2 * i:2 * i + 1], None, ALU.mult), r=[k(y0), k(rp['gat'])], w=[k(y0)])
            yield
            V(lambda e: e.scalar_tensor_tensor(y0[:], y1[:], rp['gat'][:, 2 * i + 1:2 * i + 2], y0[:], ALU.mult, ALU.add), r=[k(y1), k(y0), k(rp['gat'])], w=[k(y0)])
            yield
            V(lambda e: e.scalar_tensor_tensor(h1[:], h1[:], ALPHA, y0[:], ALU.mult, ALU.add), r=[k(h1), k(y0)], w=[k(h1)])
            yield
            yield from self.layernorm(h1, l2g, l2b, B['h2'], B['tmp'], B['st'])
            if last:
                self.store('sp', self.out_d[i * 128:(i + 1) * 128, :], B['h2'][:], k(B['h2']), dkeys=["out_%d" % i])
                yield
            else:
                self.store('sp', self.h_d[i * 128:(i + 1) * 128, :], B['h2'][:], k(B['h2']), dkeys=["hd_%d" % i])
                yield
                A(lambda e: e.copy(out=B['hb'][:], in_=B['h2'][:]), r=[k(B['h2'])], w=[k(B['hb'])])
                yield
                bk, bkey = self.bank()
                pb = bk[:].bitcast(BF16)
                for kc in range(8):
                    M(lambda e, kc=kc: e.transpose(pb[:, kc * 128:(kc + 1) * 128], B['hb'][:, kc * 128:(kc + 1) * 128], c['identb'][:]), r=[k(B['hb']), k(c['identb'])], w=[bkey])
                V(lambda e: e.tensor_copy(B['hT'][:].rearrange("p a b -> p (a b)"), pb), r=[bkey], w=[k(B['hT'])])
                yield
                self.store('sp', self.hT_d[:, :, i * 128:(i + 1) * 128], B['hT'][:], k(B['hT']), dkeys=["hTd_%d" % i])
                yield
        self.run_pipe([lambda i=i: cbody(i) for i in range(NT)], 2)
        self.P.barrier()
        self.sb_off = mark

    def build_full(self):
        self.declare_inputs()
        T = self.T
        self.h_d = self.dscr("h_d", [T, D])
        self.hT_d = self.dscr("hT_d", [128, 8, T], BF16)
        self.h1_d = self.dscr("h1_d", [T, D])
        self.h1b_d = self.dscr("h1b_d", [T, D], BF16)
        self.xs_d = self.dscr("xs_d", [self.NB * self.RB, D], BF16)
        self.ys_d = self.dscr("ys_d", [self.NB * self.RB, D])
        self.out_d = self.dout("out", [T, D])
        if self.debug:
            self.dbg_y = self.dout("dbg_y", [T, D])
        self.consts()
        self.alloc_route_persist()
        base = self.sb_off
        for l in range(self.depth):
            self.sb_off = base
            self.alloc_params()
            self.alloc_mixer()
            self.alloc_router()
            if l == 0:
                self.stage0()
                self.P.barrier()
            self.load_params(l)
            self.stageM(l)
            self.P.barrier()
            self.sb_off = base
            self.stageMoE(l, l == self.depth - 1)
        self.P.barrier()
        return self.nc


def _host_inputs(inputs, b, T):
    m = {}
    for k, v in inputs.items():
        v = np.asarray(v)
        if k == 'x':
            m[k] = np.ascontiguousarray(v[b, :T])
        elif k == 'rwkv_r_k':
            m[k] = np.ascontiguousarray(v.reshape(DEPTH, 256))
        elif k in ('moe_w_gate', 'moe_w_up'):
            m[k] = np.ascontiguousarray(v.reshape(DEPTH * NE * D, FF))
        elif k == 'moe_w_down':
            m[k] = np.ascontiguousarray(v.reshape(DEPTH * NE * FF, D))
        else:
            m[k] = np.ascontiguousarray(v)
    return m


def kernel(**inputs):
    x = np.asarray(inputs['x'])
    Bsz, T, _ = x.shape
    bld = Builder(T)
    nc = bld.build_full()
    in_maps = [_host_inputs(inputs, b, T) for b in range(Bsz)]
    res = run_bass_kernel_spmd(nc, in_maps, core_ids=list(range(Bsz)))
    return np.stack([np.asarray(r["out"]) for r in res.results], axis=0).astype(np.float32)
```
